# Optimizing a Trainium2 kernel written in Bass

```python
import jax
import jax.numpy as jnp
from jax import lax
import numpy as np

D_MODEL = 1024
BATCH = 8
SEQ = 2048
DEPTH = 2

GRID_W = 64
CTX_LEN = 256
HEAD_DIM = 64
MLA_HEADS = D_MODEL // (2 * HEAD_DIM)
MLA_Q_RANK = 3 * D_MODEL // 8
MLA_KV_RANK = D_MODEL // 4
MLA_NOPE = HEAD_DIM
MLA_ROPE = HEAD_DIM // 2
MLA_QK = MLA_NOPE + MLA_ROPE
MLA_V = HEAD_DIM
NA_HEADS = D_MODEL // (2 * HEAD_DIM)
NA_WIN_ROWS = 8
NA_WIN_COLS = 16
D_IN = MLA_Q_RANK + MLA_KV_RANK + MLA_ROPE + 3 * NA_HEADS * HEAD_DIM
ROPE_THETA = 10000.0
Q_BLOCK = 128
CONV_WIDTH = 31
PEER_HEADS = 8
PEER_N_KEYS = 128
PEER_N_EXPERTS = PEER_N_KEYS * PEER_N_KEYS
PEER_TOPK = 16
PEER_KEY_DIM = 256
PEER_TOKEN_BLOCK = 128
EPS = 1e-6

kernel_name = "hybrid_mla_natten_conformer_peer_dit"


def rms_norm(x, g):
    xf = x.astype(jnp.float32)
    y = xf * lax.rsqrt(jnp.mean(xf * xf, axis=-1, keepdims=True) + EPS)
    return (y * g.astype(jnp.float32)).astype(x.dtype)


def layer_norm(x, g, b):
    xf = x.astype(jnp.float32)
    mu = jnp.mean(xf, axis=-1, keepdims=True)
    var = jnp.mean(jnp.square(xf - mu), axis=-1, keepdims=True)
    y = (xf - mu) * lax.rsqrt(var + EPS)
    return (y * g.astype(jnp.float32) + b.astype(jnp.float32)).astype(x.dtype)


def modulate(x, shift, scale):
    return x * (1 + scale[:, None, :]) + shift[:, None, :]


def ada_params(cond, w, b):
    return jnp.split(jax.nn.silu(cond) @ w + b, 6, axis=-1)


def axial_rope_tables(n_tokens, rot_dim):
    t = jnp.arange(n_tokens)
    row = (t // GRID_W).astype(jnp.float32)
    col = (t % GRID_W).astype(jnp.float32)
    n_freq = rot_dim // 4
    inv_freq = 1.0 / (ROPE_THETA ** (jnp.arange(n_freq, dtype=jnp.float32) / n_freq))
    ang = jnp.concatenate([row[:, None] * inv_freq, col[:, None] * inv_freq], axis=-1)
    return jnp.cos(ang)[:, None, :], jnp.sin(ang)[:, None, :]


def apply_rope(x, cos, sin):
    x1, x2 = jnp.split(x, 2, axis=-1)
    cos = cos.astype(x.dtype)
    sin = sin.astype(x.dtype)
    return jnp.concatenate([x1 * cos - x2 * sin, x1 * sin + x2 * cos], axis=-1)


def to_heads(x):
    return x.transpose(0, 2, 1, 3)


def merge_heads(o):
    b, h, t, d = o.shape
    return o.transpose(0, 2, 1, 3).reshape(b, t, h * d)


def split_combined(z):
    a = MLA_Q_RANK
    b = a + MLA_KV_RANK
    c = b + MLA_ROPE
    return z[..., :a], z[..., a:b], z[..., b:c], z[..., c:]


def mla_queries(cq, g_qa, w_q_up, g_q, rope):
    b, t, _ = cq.shape
    q = (rms_norm(cq, g_qa) @ w_q_up).reshape(b, t, MLA_HEADS, MLA_QK)
    q = rms_norm(q, g_q)
    if rope is not None:
        q = jnp.concatenate([q[..., :MLA_NOPE], apply_rope(q[..., MLA_NOPE:], *rope)], axis=-1)
    return to_heads(q)


def mla_keys_values(ckv, k_rope, g_kva, w_kv_up, g_k, rope):
    b, t, _ = ckv.shape
    kv = (rms_norm(ckv, g_kva) @ w_kv_up).reshape(b, t, MLA_HEADS, MLA_NOPE + MLA_V)
    k_rope = jnp.broadcast_to(k_rope[:, :, None, :], (b, t, MLA_HEADS, MLA_ROPE))
    k = rms_norm(jnp.concatenate([kv[..., :MLA_NOPE], k_rope], axis=-1), g_k)
    if rope is not None:
        k = jnp.concatenate([k[..., :MLA_NOPE], apply_rope(k[..., MLA_NOPE:], *rope)], axis=-1)
    return to_heads(k), to_heads(kv[..., MLA_NOPE:])


def na_split(z_na):
    b, t, _ = z_na.shape
    qkv = z_na.reshape(b, t, 3, NA_HEADS, HEAD_DIM)
    return qkv[:, :, 0], qkv[:, :, 1], qkv[:, :, 2]


def latent_global_attention(q, k_lat, v_lat, k_ctx, v_ctx):
    b, h, s, dq = q.shape
    k = jnp.concatenate([k_ctx, k_lat], axis=2)
    v = jnp.concatenate([v_ctx, v_lat], axis=2)
    scale = dq ** -0.5
    nb = s // Q_BLOCK
    q_blocks = q.reshape(b, h, nb, Q_BLOCK, dq).transpose(2, 0, 1, 3, 4)

    def block(qb):
        sc = jnp.einsum("bhqd,bhkd->bhqk", qb, k).astype(jnp.float32) * scale
        p = jax.nn.softmax(sc, axis=-1).astype(v.dtype)
        return jnp.einsum("bhqk,bhkd->bhqd", p, v)

    o = lax.map(block, q_blocks)
    return o.transpose(1, 2, 0, 3, 4).reshape(b, h, s, v.shape[-1])


def neighbourhood_index(s):
    rows = s // GRID_W
    wr = min(NA_WIN_ROWS, rows)
    t = jnp.arange(s)
    r, col = t // GRID_W, t % GRID_W
    r0 = jnp.clip(r - wr // 2, 0, rows - wr)
    c0 = jnp.clip(col - NA_WIN_COLS // 2, 0, GRID_W - NA_WIN_COLS)
    kr = r0[:, None, None] + jnp.arange(wr)[None, :, None]
    kc = c0[:, None, None] + jnp.arange(NA_WIN_COLS)[None, None, :]
    nk = wr * NA_WIN_COLS
    idx = (kr * GRID_W + kc).reshape(s, nk)
    rel_r = jnp.broadcast_to(kr - r[:, None, None] + NA_WIN_ROWS - 1, (s, wr, NA_WIN_COLS)).reshape(s, nk)
    rel_c = jnp.broadcast_to(kc - col[:, None, None] + NA_WIN_COLS - 1, (s, wr, NA_WIN_COLS)).reshape(s, nk)
    return idx, rel_r, rel_c


def neighbourhood_attention(q, k, v, k_ctx, v_ctx, rpb):
    b, h, s, dh = q.shape
    idx, rel_r, rel_c = neighbourhood_index(s)
    nk = idx.shape[-1]
    nb = s // GRID_W
    scale = dh ** -0.5
    q_blocks = q.reshape(b, h, nb, GRID_W, dh).transpose(2, 0, 1, 3, 4)

    def block(args):
        qb, ib, rb, cb = args
        k_sel = k[:, :, ib]
        v_sel = v[:, :, ib]
        s_loc = (jnp.einsum("bhqd,bhqkd->bhqk", qb, k_sel).astype(jnp.float32) * scale
                 + rpb[:, rb, cb].astype(jnp.float32))
        s_ctx = jnp.einsum("bhqd,bhkd->bhqk", qb, k_ctx).astype(jnp.float32) * scale
        p = jax.nn.softmax(jnp.concatenate([s_loc, s_ctx], axis=-1), axis=-1).astype(v.dtype)
        return (jnp.einsum("bhqk,bhqkd->bhqd", p[..., :nk], v_sel)
                + jnp.einsum("bhqk,bhkd->bhqd", p[..., nk:], v_ctx))

    o = lax.map(block, (q_blocks, idx.reshape(nb, GRID_W, nk),
                        rel_r.reshape(nb, GRID_W, nk), rel_c.reshape(nb, GRID_W, nk)))
    return o.transpose(1, 2, 0, 3, 4).reshape(b, h, s, dh)


def context_attention(q, k, v):
    sc = jnp.einsum("bhqd,bhkd->bhqk", q, k).astype(jnp.float32) * (q.shape[-1] ** -0.5)
    p = jax.nn.softmax(sc, axis=-1).astype(v.dtype)
    return jnp.einsum("bhqk,bhkd->bhqd", p, v)


def attention_mixer(a_lat, a_ctx, w_in, g_qa, w_q_up, g_kva, w_kv_up, g_q, g_k,
                    na_g_q, na_g_k, rpb, w_out, rope, ctx_out):
    cq_l, ckv_l, kr_l, na_l = split_combined(a_lat @ w_in)
    cq_c, ckv_c, kr_c, na_c = split_combined(a_ctx @ w_in)
    q_m = mla_queries(cq_l, g_qa, w_q_up, g_q, rope)
    k_m, v_m = mla_keys_values(ckv_l, kr_l, g_kva, w_kv_up, g_k, rope)
    k_mc, v_mc = mla_keys_values(ckv_c, kr_c, g_kva, w_kv_up, g_k, None)
    q_n, k_n, v_n = na_split(na_l)
    qc_n, kc_n, vc_n = na_split(na_c)
    q_n = to_heads(rms_norm(q_n, na_g_q))
    k_n = to_heads(rms_norm(k_n, na_g_k))
    v_n = to_heads(v_n)
    k_nc = to_heads(rms_norm(kc_n, na_g_k))
    v_nc = to_heads(vc_n)
    o_m = latent_global_attention(q_m, k_m, v_m, k_mc, v_mc)
    o_n = neighbourhood_attention(q_n, k_n, v_n, k_nc, v_nc, rpb)
    y_lat = jnp.concatenate([merge_heads(o_m), merge_heads(o_n)], axis=-1) @ w_out
    if not ctx_out:
        return y_lat, None
    q_mc = mla_queries(cq_c, g_qa, w_q_up, g_q, None)
    q_nc = to_heads(rms_norm(qc_n, na_g_q))
    y_ctx = jnp.concatenate([merge_heads(context_attention(q_mc, k_mc, v_mc)),
                             merge_heads(context_attention(q_nc, k_nc, v_nc))], axis=-1) @ w_out
    return y_lat, y_ctx


def conformer_conv(h, w_pw1, b_pw1, w_dw, b_dw, g_ln, b_ln, w_pw2, b_pw2):
    d = h.shape[-1]
    a = h @ w_pw1 + b_pw1
    a = a[..., :d] * jax.nn.sigmoid(a[..., d:])
    a = lax.conv_general_dilated(a, w_dw[:, None, :], window_strides=(1,),
                                 padding=[(CONV_WIDTH // 2, CONV_WIDTH // 2)],
                                 dimension_numbers=("NWC", "WIO", "NWC"),
                                 feature_group_count=d) + b_dw
    a = jax.nn.silu(layer_norm(a, g_ln, b_ln))
    return a @ w_pw2 + b_pw2


def peer_ffn(h, w_query, sub_keys, u_tab, v_tab):
    b, t, d = h.shape
    half = PEER_KEY_DIM // 2
    h_blocks = h.reshape(-1, PEER_TOKEN_BLOCK, d)

    def block(hb):
        q = (hb @ w_query).reshape(PEER_TOKEN_BLOCK, PEER_HEADS, 2, half)
        s = jnp.einsum("thpd,hpnd->thpn", q, sub_keys).astype(jnp.float32)
        s_top, i_top = lax.top_k(s, PEER_TOPK)
        cand = (s_top[:, :, 0, :, None] + s_top[:, :, 1, None, :]).reshape(
            PEER_TOKEN_BLOCK, PEER_HEADS, PEER_TOPK * PEER_TOPK)
        cand_idx = (i_top[:, :, 0, :, None] * PEER_N_KEYS + i_top[:, :, 1, None, :]).reshape(
            PEER_TOKEN_BLOCK, PEER_HEADS, PEER_TOPK * PEER_TOPK)
        best, pos = lax.top_k(cand, PEER_TOPK)
        expert = jnp.take_along_axis(cand_idx, pos, axis=-1)
        gate = jax.nn.softmax(best, axis=-1)
        u = jnp.take(u_tab, expert, axis=0)
        act = jax.nn.gelu(jnp.einsum("thkd,td->thk", u, hb).astype(jnp.float32), approximate=False)
        w = (gate * act).astype(hb.dtype)
        return jnp.einsum("thk,thkd->td", w, jnp.take(v_tab, expert, axis=0))

    return lax.map(block, h_blocks).reshape(b, t, d)


def setup_inputs(seed: int = 0) -> dict:
    key = jax.random.key(seed)
    ks = iter(jax.random.split(key, 32))
    f32 = jnp.float32
    d = D_MODEL
    n_attn = (DEPTH + 1) // 2
    n_conv = DEPTH // 2

    def normal(shape, scale):
        return jax.random.normal(next(ks), shape, f32) * scale

    def gain(shape):
        return 1.0 + 0.05 * jax.random.normal(next(ks), shape, f32)

    inp = {}
    inp["x"] = normal((BATCH, SEQ, d), 1.0)
    inp["c"] = normal((BATCH, d), 1.0)
    inp["ctx"] = normal((BATCH, CTX_LEN, d), 1.0)
    inp["c_ctx"] = normal((d,), 1.0)
    inp["w_ada"] = normal((DEPTH, d, 6 * d), 0.5 * d ** -0.5)
    inp["b_ada"] = normal((DEPTH, 6 * d), 0.02)
    inp["g_norm1"] = gain((DEPTH, d))
    inp["g_norm2"] = gain((DEPTH, d))
    inp["attn_w_in"] = normal((n_attn, d, D_IN), d ** -0.5)
    inp["mla_g_qa"] = gain((n_attn, MLA_Q_RANK))
    inp["mla_w_q_up"] = normal((n_attn, MLA_Q_RANK, MLA_HEADS * MLA_QK), MLA_Q_RANK ** -0.5)
    inp["mla_g_kva"] = gain((n_attn, MLA_KV_RANK))
    inp["mla_w_kv_up"] = normal((n_attn, MLA_KV_RANK, MLA_HEADS * (MLA_NOPE + MLA_V)), MLA_KV_RANK ** -0.5)
    inp["mla_g_q"] = gain((n_attn, MLA_QK))
    inp["mla_g_k"] = gain((n_attn, MLA_QK))
    inp["na_g_q"] = gain((n_attn, HEAD_DIM))
    inp["na_g_k"] = gain((n_attn, HEAD_DIM))
    inp["na_rpb"] = normal((n_attn, NA_HEADS, 2 * NA_WIN_ROWS - 1, 2 * NA_WIN_COLS - 1), 0.2)
    inp["attn_w_out"] = normal((n_attn, d, d), d ** -0.5)
    inp["conv_w_pw1"] = normal((n_conv, d, 2 * d), d ** -0.5)
    inp["conv_b_pw1"] = normal((n_conv, 2 * d), 0.02)
    inp["conv_w_dw"] = normal((n_conv, CONV_WIDTH, d), CONV_WIDTH ** -0.5)
    inp["conv_b_dw"] = normal((n_conv, d), 0.02)
    inp["conv_g_ln"] = gain((n_conv, d))
    inp["conv_b_ln"] = normal((n_conv, d), 0.02)
    inp["conv_w_pw2"] = normal((n_conv, d, d), d ** -0.5)
    inp["conv_b_pw2"] = normal((n_conv, d), 0.02)
    inp["peer_w_query"] = normal((DEPTH, d, PEER_HEADS * PEER_KEY_DIM), d ** -0.5)
    inp["peer_sub_keys"] = normal((DEPTH, PEER_HEADS, 2, PEER_N_KEYS, PEER_KEY_DIM // 2), (PEER_KEY_DIM // 2) ** -0.5)
    inp["peer_u"] = normal((DEPTH, PEER_N_EXPERTS, d), d ** -0.5)
    inp["peer_v"] = normal((DEPTH, PEER_N_EXPERTS, d), PEER_HEADS ** -0.5)
    return inp


def reference(x, c, ctx, c_ctx, w_ada, b_ada, g_norm1, g_norm2,
              attn_w_in, mla_g_qa, mla_w_q_up, mla_g_kva, mla_w_kv_up, mla_g_q, mla_g_k,
              na_g_q, na_g_k, na_rpb, attn_w_out,
              conv_w_pw1, conv_b_pw1, conv_w_dw, conv_b_dw, conv_g_ln, conv_b_ln, conv_w_pw2, conv_b_pw2,
              peer_w_query, peer_sub_keys, peer_u, peer_v):
    rope = axial_rope_tables(x.shape[1], MLA_ROPE)
    c_ctx_row = c_ctx[None, :]
    h_lat, h_ctx = x, ctx
    for l in range(DEPTH):
        ctx_live = any(j % 2 == 0 for j in range(l + 1, DEPTH))
        ctx_in = (l % 2 == 0) or ctx_live
        sh1, sc1, g1, sh2, sc2, g2 = ada_params(c, w_ada[l], b_ada[l])
        a_lat = modulate(rms_norm(h_lat, g_norm1[l]), sh1, sc1)
        a_ctx = None
        if ctx_in:
            sh1c, sc1c, g1c, sh2c, sc2c, g2c = ada_params(c_ctx_row, w_ada[l], b_ada[l])
            a_ctx = modulate(rms_norm(h_ctx, g_norm1[l]), sh1c, sc1c)
        i = l // 2
        if l % 2 == 0:
            y_lat, y_ctx = attention_mixer(a_lat, a_ctx, attn_w_in[i], mla_g_qa[i], mla_w_q_up[i],
                                           mla_g_kva[i], mla_w_kv_up[i], mla_g_q[i], mla_g_k[i],
                                           na_g_q[i], na_g_k[i], na_rpb[i], attn_w_out[i], rope, ctx_live)
        else:
            conv_p = (conv_w_pw1[i], conv_b_pw1[i], conv_w_dw[i], conv_b_dw[i],
                      conv_g_ln[i], conv_b_ln[i], conv_w_pw2[i], conv_b_pw2[i])
            y_lat = conformer_conv(a_lat, *conv_p)
            y_ctx = conformer_conv(a_ctx, *conv_p) if ctx_live else None
        peer_p = (peer_w_query[l], peer_sub_keys[l], peer_u[l], peer_v[l])
        h_lat = h_lat + g1[:, None, :] * y_lat
        h_lat = h_lat + g2[:, None, :] * peer_ffn(modulate(rms_norm(h_lat, g_norm2[l]), sh2, sc2), *peer_p)
        if ctx_live:
            h_ctx = h_ctx + g1c[:, None, :] * y_ctx
            h_ctx = h_ctx + g2c[:, None, :] * peer_ffn(modulate(rms_norm(h_ctx, g_norm2[l]), sh2c, sc2c), *peer_p)
    return h_lat
```

```python
import numpy as np
from contextlib import ExitStack
import concourse.bass as bass
import concourse.mybir as mybir
from concourse.bass_utils import run_bass_kernel_spmd

F32 = mybir.dt.float32
BF16 = mybir.dt.bfloat16
I32 = mybir.dt.int32
U32 = mybir.dt.uint32
ALU = mybir.AluOpType
AF = mybir.ActivationFunctionType
AX = mybir.AxisListType

D = 1024
SEQ = 2048
NT = SEQ // 128
CTX = 256
NCT = CTX // 128
EPS = 1e-6
NEG = -30000.0


class Buf:
    __slots__ = ("w", "r")

    def __init__(self):
        self.w = None
        self.r = {}


class Sched:
    RING = 12

    def __init__(self, nc, es):
        self.nc = nc
        self.eng = {"pe": nc.tensor, "act": nc.scalar, "dve": nc.vector, "pool": nc.gpsimd, "sp": nc.sync}
        self.semobj = {}
        self.cnt = {}
        for k in self.eng:
            self.semobj[k] = es.enter_context(nc.semaphore("s_" + k))
            self.cnt[k] = 0
        self.waited = {k: {} for k in self.eng}
        self.dq = {}
        for q in ("sp", "pool", "act"):
            slots = []
            for i in range(self.RING):
                key = ("d", q, i)
                self.semobj[key] = es.enter_context(nc.semaphore(f"d_{q}_{i}"))
                slots.append(key)
            self.dq[q] = {"slots": slots, "uses": [0] * self.RING, "next": 0}

    def _wait(self, ek, tok):
        if tok is None:
            return
        sk, v = tok
        if self.waited[ek].get(sk, 0) >= v:
            return
        self.eng[ek].wait_ge(self.semobj[sk], v)
        self.waited[ek][sk] = v

    def _deps(self, ek, reads, writes):
        for b in reads:
            self._wait(ek, b.w)
        for b in writes:
            self._wait(ek, b.w)
            for sk, v in b.r.items():
                self._wait(ek, (sk, v))

    def _mark(self, tok, reads, writes):
        sk, v = tok
        for b in reads:
            if b.r.get(sk, 0) < v:
                b.r[sk] = v
        for b in writes:
            b.w = tok
            b.r = {}

    def op(self, ek, fn, reads=(), writes=()):
        self._deps(ek, reads, writes)
        ins = fn(self.eng[ek])
        self.cnt[ek] += 1
        ins.then_inc(self.semobj[ek], 1)
        tok = (ek, self.cnt[ek])
        self._mark(tok, reads, writes)
        return tok

    def dma(self, q, fn, reads=(), writes=()):
        dq = self.dq[q]
        slot = dq["next"]
        dq["next"] = (slot + 1) % self.RING
        key = dq["slots"][slot]
        uses = dq["uses"][slot]
        if uses:
            self._wait(q, (key, 16 * uses))
        self._deps(q, reads, writes)
        ins = fn(self.eng[q])
        ins.then_inc(self.semobj[key], 16)
        dq["uses"][slot] = uses + 1
        tok = (key, 16 * (uses + 1))
        self._mark(tok, reads, writes)
        return tok

    def barrier(self):
        toks = [(k, self.cnt[k]) for k in self.eng if self.cnt[k]]
        for q, dq in self.dq.items():
            for key, u in zip(dq["slots"], dq["uses"]):
                if u:
                    toks.append((key, 16 * u))
        for ek in self.eng:
            for t in toks:
                self._wait(ek, t)


class K:
    def __init__(self, nc, es):
        self.nc = nc
        self.es = es
        self.s = Sched(nc, es)
        self.banks = []
        for i in range(8):
            t = es.enter_context(nc.psum_tensor(f"bank{i}", [128, 512], F32))
            self.banks.append((t, Buf()))
        self.ident = None

    def sb(self, es, name, shape, dt):
        t = es.enter_context(self.nc.sbuf_tensor(name, list(shape), dt))
        return t, Buf()


def bcast_row(ap_row, parts):
    return ap_row.partition_broadcast(parts) if len(ap_row.shape) == 1 else ap_row.to_broadcast([parts, ap_row.shape[-1]])


def stage_ada(k, cc, w_ada, b_ada, g_norm, modv):
    nc, s = k.nc, k.s
    with ExitStack() as es:
        cct, ccb = k.sb(es, "ada_cc", [128, 16], F32)
        sil, silb = k.sb(es, "ada_sil", [128, 16], F32)
        wt = [k.sb(es, f"ada_w{i}", [128, 8, 512], F32) for i in range(2)]
        brow, browb = k.sb(es, "ada_b", [2, 6144], F32)
        grow, growb = k.sb(es, "ada_g", [2, 2, 1024], F32)
        mrow, mrowb = k.sb(es, "ada_m", [2, 6144], F32)
        orow, orowb = k.sb(es, "ada_o", [2, 6, 1024], F32)

        s.dma("sp", lambda e: e.dma_start(out=cct[:], in_=cc), writes=[ccb])
        s.op("act", lambda e: e.activation(out=sil[:], in_=cct[:], func=AF.Silu), reads=[ccb], writes=[silb])
        for l in range(2):
            s.dma("sp", lambda e: e.dma_start(out=brow[:], in_=b_ada[l:l + 1, :].to_broadcast([2, 6144])), writes=[browb])
            s.dma("sp", lambda e: e.dma_start(out=grow[:], in_=g_norm[2 * l:2 * l + 2, :].rearrange("(o a) d -> o a d", o=1).to_broadcast([2, 2, 1024])), writes=[growb])
            wv = w_ada[l].rearrange("(kc p) n -> p kc n", p=128)
            for g in range(12):
                wtile, wbuf = wt[g % 2]
                q = "sp" if g % 2 == 0 else "pool"
                s.dma(q, lambda e: e.dma_start(out=wtile[:], in_=wv[:, :, g * 512:(g + 1) * 512]), writes=[wbuf])
                bank, bb = k.banks[g % 2]
                for kc in range(8):
                    s.op("pe", lambda e: e.matmul(bank[0:2, :], lhsT=sil[:, 2 * kc:2 * kc + 2], rhs=wtile[:, kc, :],
                                                  start=(kc == 0), stop=(kc == 7)),
                         reads=[silb, wbuf], writes=[bb])
                s.op("dve", lambda e: e.tensor_tensor(out=mrow[:, g * 512:(g + 1) * 512], in0=bank[0:2, :],
                                                      in1=brow[:, g * 512:(g + 1) * 512], op=ALU.add),
                     reads=[bb, browb], writes=[mrowb])
            for j in range(2):
                sh = mrow[:, (3 * j) * 1024:(3 * j + 1) * 1024]
                sc = mrow[:, (3 * j + 1) * 1024:(3 * j + 2) * 1024]
                gt = mrow[:, (3 * j + 2) * 1024:(3 * j + 3) * 1024]
                s.op("dve", lambda e: e.scalar_tensor_tensor(out=orow[:, 3 * j, :], in0=sc, scalar=1.0, in1=grow[:, j, :],
                                                             op0=ALU.add, op1=ALU.mult),
                     reads=[mrowb, growb], writes=[orowb])
                s.op("dve", lambda e: e.tensor_copy(out=orow[:, 3 * j + 1, :], in_=sh), reads=[mrowb], writes=[orowb])
                s.op("dve", lambda e: e.tensor_copy(out=orow[:, 3 * j + 2, :], in_=gt), reads=[mrowb], writes=[orowb])
            s.dma("sp", lambda e: e.dma_start(out=modv[l], in_=orow[:]), reads=[orowb], writes=[k.modv_buf])
        s.barrier()


def load_cast(k, stg, dst, dstb, src, n, qi=0):
    s = k.s
    st, stb = stg[qi % len(stg)]
    s.dma("sp" if qi % 2 == 0 else "pool", lambda e: e.dma_start(out=st[:, 0:n], in_=src), writes=[stb])
    ek = ("act", "pool", "dve")[qi % 3]
    if ek == "act":
        s.op("act", lambda e: e.copy(out=dst, in_=st[:, 0:n]), reads=[stb], writes=[dstb])
    else:
        s.op(ek, lambda e: e.tensor_copy(out=dst, in_=st[:, 0:n]), reads=[stb], writes=[dstb])


def load_w_bf16(k, stg, wt, wb, wdram, kc, n, q0=0):
    qi = q0
    v = wdram.rearrange("(kc p) n -> p kc n", p=128)
    cw = stg[0][0].shape[1]
    for c in range(kc):
        for c0 in range(0, n, cw):
            c1 = min(n, c0 + cw)
            load_cast(k, stg, wt[:, c, c0:c1], wb, v[:, c, c0:c1], c1 - c0, qi)
            qi += 1
    return qi


def rstd_from_ss(k, ss, ssb, rs, rsb, inv_n, w):
    s = k.s
    s.op("act", lambda e: e.activation(out=rs[:, 0:w], in_=ss[:, 0:w], func=AF.Sqrt, scale=inv_n, bias=k.eps[:, 0:1]),
         reads=[ssb], writes=[rsb])
    s.op("dve", lambda e: e.reciprocal(out=rs[:, 0:w], in_=rs[:, 0:w]), reads=[rsb], writes=[rsb])


def norm_mod(k, xt, xb, A, B, ABb, scr, scrb, st, stb, out, outb, out_f32=None):
    s = k.s
    s.op("act", lambda e: e.activation(out=scr[:], in_=xt[:], func=AF.Square, accum_out=st[:, 0:1]),
         reads=[xb], writes=[scrb, stb])
    rstd_from_ss(k, st, stb, st, stb, 1.0 / D, 1)
    s.op("dve", lambda e: e.scalar_tensor_tensor(out=scr[:], in0=xt[:], scalar=st[:, 0:1], in1=A, op0=ALU.mult, op1=ALU.mult),
         reads=[xb, stb, ABb], writes=[scrb])
    if out_f32 is not None:
        of, ofb = out_f32
        s.op("pool", lambda e: e.tensor_tensor(out=of[:], in0=scr[:], in1=B, op=ALU.add), reads=[scrb, ABb], writes=[ofb])
        s.op("act", lambda e: e.copy(out=out[:], in_=of[:]), reads=[ofb], writes=[outb])
    else:
        s.op("pool", lambda e: e.tensor_tensor(out=out[:], in0=scr[:], in1=B, op=ALU.add), reads=[scrb, ABb], writes=[outb])


def transpose_chunks(k, bank, bankb, src_fn, nchunks, rows, dst, dstb, srcb, dst_view=None):
    s = k.s
    bv = bank[:].bitcast(BF16)
    for c in range(nchunks):
        s.op("pe", lambda e: e.transpose(out=bv[0:rows, c * 128:(c + 1) * 128], in_=src_fn(c), identity=k.ident[:]),
             reads=[srcb, k.identb], writes=[bankb])
    src = bv[0:rows, 0:nchunks * 128]
    if dst_view is not None:
        src = src.rearrange("p (c t) -> p c t", t=128)
    s.op("act", lambda e: e.copy(out=dst, in_=src), reads=[bankb], writes=[dstb])


def setup_consts(k, es, ident_d, identf_d=None):
    s = k.s
    k.ident, k.identb = k.sb(es, "ident_sb", [128, 128], BF16)
    k.eps, k.epsb = k.sb(es, "epsc", [128, 1], F32)
    s.dma("sp", lambda e: e.dma_start(out=k.ident[:], in_=ident_d), writes=[k.identb])
    if identf_d is not None:
        k.identf, _ = k.sb(es, "identf_sb", [128, 128], F32)
        s.dma("sp", lambda e: e.dma_start(out=k.identf[:], in_=identf_d), writes=[k.identb])
    s.op("dve", lambda e: e.memset(k.eps[:], EPS), writes=[k.epsb])


def load_bcast(k, tile, tb, row, n):
    k.s.dma("sp", lambda e: e.dma_start(out=tile, in_=row.to_broadcast([128, n])), writes=[tb])


def na_blocks(qt):
    if 2 <= qt <= 13:
        return [(qt - 2 + j, j) for j in range(5)]
    if qt == 0:
        return [(j, 5 + j) for j in range(4)]
    if qt == 1:
        return [(j, 9 + j) for j in range(4)]
    if qt == 14:
        return [(12 + j, 13 + j) for j in range(4)]
    return [(12 + j, 17 + j) for j in range(4)]


def stage_attn(k, x, ctx, modv0, W, hout, houtb):
    nc, s = k.nc, k.s
    banks = k.banks
    with ExitStack() as es:
        AB, ABb = k.sb(es, "at_AB", [128, 4, 1024], F32)
        osb, osbb = k.sb(es, "at_o", [128, NT, 1024], BF16)
        qT, qTb = k.sb(es, "at_qT", [96, 8, SEQ], BF16)
        kT, kTb = k.sb(es, "at_kT", [96, 8, SEQ + CTX], BF16)
        Vs, Vsb = k.sb(es, "at_V", [128, NT + NCT, 8, 65], BF16)
        st, stb = k.sb(es, "at_st", [128, 4], F32)
        st2, st2b = k.sb(es, "at_st2", [128, 16], F32)
        st3, st3b = k.sb(es, "at_st3", [128, 16], F32)
        xts = [k.sb(es, f"at_x{i}", [128, 1024], F32) for i in range(2)]
        scrs = [k.sb(es, f"at_scr{i}", [128, 1024], F32) for i in range(2)]
        abfs = [k.sb(es, f"at_a{i}", [128, 1024], BF16) for i in range(2)]
        aTs = [k.sb(es, f"at_aT{i}", [128, 8, 128], BF16) for i in range(2)]

        load_bcast(k, AB[:, 0, :], ABb, modv0[0:1, 0, :], 1024)
        load_bcast(k, AB[:, 1, :], ABb, modv0[0:1, 1, :], 1024)
        load_bcast(k, AB[:, 2, :], ABb, modv0[1:2, 0, :], 1024)
        load_bcast(k, AB[:, 3, :], ABb, modv0[1:2, 1, :], 1024)
        s.op("pool", lambda e: e.memset(Vs[:, :, :, 64:65], 1.0), writes=[Vsb])

        def src_tile(t):
            return ctx[t * 128:(t + 1) * 128, :] if t < NCT else x[(t - NCT) * 128:(t - NCT + 1) * 128, :]

        def front(t):
            xt, xb = xts[t % 2]
            scr, scrb = scrs[t % 2]
            abf, abfb = abfs[t % 2]
            aT, aTb = aTs[t % 2]
            s.dma("sp", lambda e: e.dma_start(out=xt[:], in_=src_tile(t)), writes=[xb])
            j = 2 if t < NCT else 0
            norm_mod(k, xt, xb, AB[:, j, :], AB[:, j + 1, :], ABb, scr, scrb, st, stb, abf, abfb)
            bank, bb = banks[7]
            transpose_chunks(k, bank, bb, lambda c: abf[:, c * 128:(c + 1) * 128], 8, 128, aT[:], aTb, abfb, dst_view=True)
            return aT, aTb, scr, scrb

        with ExitStack() as es1:
            stg = [k.sb(es1, f"p1_stg{i}", [128, 1024], F32) for i in range(2)]
            w_in, w_inb = k.sb(es1, "p1_win", [128, 8, 672], BF16)
            w_q, w_qb = k.sb(es1, "p1_wq", [128, 3, 768], BF16)
            w_kv, w_kvb = k.sb(es1, "p1_wkv", [128, 2, 1024], BF16)
            gcn, gcnb = k.sb(es1, "p1_gcn", [128, 640], F32)
            gq, gqb = k.sb(es1, "p1_gq", [128, 96], F32)
            gk, gkb = k.sb(es1, "p1_gk", [128, 96], F32)
            zsb, zsbb = k.sb(es1, "p1_z", [128, 672], F32)
            cn, cnb = k.sb(es1, "p1_cn", [128, 640], BF16)
            cnT, cnTb = k.sb(es1, "p1_cnT", [128, 5, 128], BF16)
            qn, qnb = k.sb(es1, "p1_qn", [128, 8, 96], F32)
            kn, knb = k.sb(es1, "p1_kn", [128, 8, 64], F32)
            qr, qrb = k.sb(es1, "p1_qr", [128, 8, 32], F32)
            rt, rtb = k.sb(es1, "p1_rt", [128, 4, 8, 16], F32)
            krg, krgb = k.sb(es1, "p1_krg", [128, 32], F32)
            krr, krrb = k.sb(es1, "p1_krr", [128, 32], F32)
            kt4, kt4b = k.sb(es1, "p1_kt4", [128, 4, 16], F32)
            qf, qfb = k.sb(es1, "p1_qf", [128, 8, 96], BF16)
            kf, kfb = k.sb(es1, "p1_kf", [128, 8, 96], BF16)
            ropes = [k.sb(es1, f"p1_rope{i}", [128, 32], F32) for i in range(2)]

            qi = 0
            wv = W["attn_w_in"].rearrange("(kc p) n -> p kc n", p=128)
            for c in range(8):
                load_cast(k, stg, w_in[:, c, :], w_inb, wv[:, c, 0:672], 672, qi); qi += 1
            wv = W["mla_w_q_up"].rearrange("(kc p) n -> p kc n", p=128)
            for c in range(3):
                load_cast(k, stg, w_q[:, c, :], w_qb, wv[:, c, :], 768, qi); qi += 1
            wv = W["mla_w_kv_up"].rearrange("(kc p) n -> p kc n", p=128)
            for c in range(2):
                load_cast(k, stg, w_kv[:, c, :], w_kvb, wv[:, c, :], 1024, qi); qi += 1
            load_bcast(k, gcn[:, 0:384], gcnb, W["mla_g_qa"], 384)
            load_bcast(k, gcn[:, 384:640], gcnb, W["mla_g_kva"], 256)
            load_bcast(k, gq[:], gqb, W["mla_g_q"], 96)
            load_bcast(k, gk[:], gkb, W["mla_g_k"], 96)
            s.op("dve", lambda e: e.tensor_scalar_mul(out=gq[:], in0=gq[:], scalar1=96.0 ** -0.5), reads=[gqb], writes=[gqb])

            for t in range(NT + NCT):
                lat = t >= NCT
                aT, aTb, scr, scrb = front(t)
                if lat:
                    rp, rpb = ropes[t % 2]
                    s.dma("sp", lambda e: e.dma_start(out=rp[:], in_=W["rope"][(t - NCT) * 128:(t - NCT + 1) * 128, :]), writes=[rpb])
                (z0, z0b), (z1, z1b) = banks[0], banks[1]
                for (zb, zbb, c0, c1) in ((z0, z0b, 0, 512), (z1, z1b, 512, 672)):
                    for c in range(8):
                        s.op("pe", lambda e: e.matmul(zb[:, 0:c1 - c0], lhsT=aT[:, c, :], rhs=w_in[:, c, c0:c1], start=(c == 0), stop=(c == 7)),
                             reads=[aTb, w_inb], writes=[zbb])
                    s.op("act", lambda e: e.copy(out=zsb[:, c0:c1], in_=zb[:, 0:c1 - c0]), reads=[zbb], writes=[zsbb])
                if lat:
                    s.op("act", lambda e: e.activation(out=scr[:, 0:384], in_=zsb[:, 0:384], func=AF.Square, accum_out=st2[:, 0:1]),
                         reads=[zsbb], writes=[scrb, st2b])
                    rstd_from_ss(k, st2, st2b, st3, st3b, 1.0 / 384, 1)
                    s.op("dve", lambda e: e.scalar_tensor_tensor(out=cn[:, 0:384], in0=zsb[:, 0:384], scalar=st3[:, 0:1], in1=gcn[:, 0:384],
                                                                 op0=ALU.mult, op1=ALU.mult), reads=[zsbb, st3b, gcnb], writes=[cnb])
                s.op("act", lambda e: e.activation(out=scr[:, 384:640], in_=zsb[:, 384:640], func=AF.Square, accum_out=st2[:, 1:2]),
                     reads=[zsbb], writes=[scrb, st2b])
                s.op("act", lambda e: e.activation(out=st3[:, 1:2], in_=st2[:, 1:2], func=AF.Sqrt, scale=1.0 / 256, bias=k.eps[:, 0:1]),
                     reads=[st2b], writes=[st3b])
                s.op("dve", lambda e: e.reciprocal(out=st3[:, 1:2], in_=st3[:, 1:2]), reads=[st3b], writes=[st3b])
                s.op("dve", lambda e: e.scalar_tensor_tensor(out=cn[:, 384:640], in0=zsb[:, 384:640], scalar=st3[:, 1:2], in1=gcn[:, 384:640],
                                                             op0=ALU.mult, op1=ALU.mult), reads=[zsbb, st3b, gcnb], writes=[cnb])
                s.op("act", lambda e: e.activation(out=scr[:, 640:672], in_=zsb[:, 640:672], func=AF.Square, accum_out=st2[:, 2:3]),
                     reads=[zsbb], writes=[scrb, st2b])
                c_lo = 0 if lat else 3
                bank, bb = banks[7]
                bv = bank[:].bitcast(BF16)
                for c in range(c_lo, 5):
                    s.op("pe", lambda e: e.transpose(out=bv[:, c * 128:(c + 1) * 128], in_=cn[:, c * 128:(c + 1) * 128], identity=k.ident[:]),
                         reads=[cnb, k.identb], writes=[bb])
                s.op("act", lambda e: e.copy(out=cnT[:, c_lo:5, :], in_=bv[:, c_lo * 128:640].rearrange("p (c t) -> p c t", t=128)),
                     reads=[bb], writes=[cnTb])
                if lat:
                    (qa, qab), (qb_, qbb) = banks[2], banks[3]
                    for (qk, qkb, h0, h1) in ((qa, qab, 0, 5), (qb_, qbb, 5, 8)):
                        n = (h1 - h0) * 96
                        for c in range(3):
                            s.op("pe", lambda e: e.matmul(qk[:, 0:n], lhsT=cnT[:, c, :], rhs=w_q[:, c, h0 * 96:h1 * 96], start=(c == 0), stop=(c == 2)),
                                 reads=[cnTb, w_qb], writes=[qkb])
                        s.op("act", lambda e: e.activation(out=scr[:, h0 * 96:h1 * 96], in_=qk[:, 0:n], func=AF.Square), reads=[qkb], writes=[scrb])
                    s.op("dve", lambda e: e.tensor_reduce(out=st2[:, 4:12], in_=scr[:, 0:768].rearrange("p (h d) -> p h d", d=96), axis=AX.X, op=ALU.add),
                         reads=[scrb], writes=[st2b])
                    s.op("act", lambda e: e.activation(out=st3[:, 4:12], in_=st2[:, 4:12], func=AF.Sqrt, scale=1.0 / 96, bias=k.eps[:, 0:1]),
                         reads=[st2b], writes=[st3b])
                    s.op("dve", lambda e: e.reciprocal(out=st3[:, 4:12], in_=st3[:, 4:12]), reads=[st3b], writes=[st3b])
                    for (qk, qkb, h0, h1) in ((qa, qab, 0, 5), (qb_, qbb, 5, 8)):
                        n = (h1 - h0) * 96
                        s.op("dve", lambda e: e.tensor_tensor(out=qn[:, h0:h1, :], in0=qk[:, 0:n].rearrange("p (h d) -> p h d", d=96),
                                                              in1=st3[:, 4 + h0:4 + h1].unsqueeze(2).to_broadcast([128, h1 - h0, 96]), op=ALU.mult),
                             reads=[qkb, st3b], writes=[qnb])
                    s.op("pool", lambda e: e.tensor_tensor(out=qf[:, :, 0:64], in0=qn[:, :, 0:64],
                                                           in1=gq[:, 0:64].unsqueeze(1).to_broadcast([128, 8, 64]), op=ALU.mult),
                         reads=[qnb, gqb], writes=[qfb])
                    s.op("dve", lambda e: e.tensor_tensor(out=qr[:], in0=qn[:, :, 64:96],
                                                          in1=gq[:, 64:96].unsqueeze(1).to_broadcast([128, 8, 32]), op=ALU.mult),
                         reads=[qnb, gqb], writes=[qrb])
                    cosb = rp[:, 0:16].unsqueeze(1).to_broadcast([128, 8, 16])
                    sinb = rp[:, 16:32].unsqueeze(1).to_broadcast([128, 8, 16])
                    s.op("dve", lambda e: e.tensor_tensor(out=rt[:, 0], in0=qr[:, :, 0:16], in1=cosb, op=ALU.mult), reads=[qrb, rpb], writes=[rtb])
                    s.op("dve", lambda e: e.tensor_tensor(out=rt[:, 1], in0=qr[:, :, 16:32], in1=sinb, op=ALU.mult), reads=[qrb, rpb], writes=[rtb])
                    s.op("dve", lambda e: e.tensor_tensor(out=rt[:, 2], in0=qr[:, :, 0:16], in1=sinb, op=ALU.mult), reads=[qrb, rpb], writes=[rtb])
                    s.op("dve", lambda e: e.tensor_tensor(out=rt[:, 3], in0=qr[:, :, 16:32], in1=cosb, op=ALU.mult), reads=[qrb, rpb], writes=[rtb])
                    s.op("dve", lambda e: e.tensor_tensor(out=qf[:, :, 64:80], in0=rt[:, 0], in1=rt[:, 1], op=ALU.subtract), reads=[rtb], writes=[qfb])
                    s.op("dve", lambda e: e.tensor_tensor(out=qf[:, :, 80:96], in0=rt[:, 2], in1=rt[:, 3], op=ALU.add), reads=[rtb], writes=[qfb])
                (ka, kab), (kb_, kbb) = banks[4], banks[5]
                for g, (kk, kkb) in enumerate(((ka, kab), (kb_, kbb))):
                    for c in range(2):
                        s.op("pe", lambda e: e.matmul(kk[:, :], lhsT=cnT[:, 3 + c, :], rhs=w_kv[:, c, g * 512:(g + 1) * 512], start=(c == 0), stop=(c == 1)),
                             reads=[cnTb, w_kvb], writes=[kkb])
                    kv3 = kk[:, :].rearrange("p (h d) -> p h d", d=128)
                    s.op("act", lambda e: e.activation(out=scr[:, g * 256:(g + 1) * 256].rearrange("p (h d) -> p h d", d=64), in_=kv3[:, :, 0:64], func=AF.Square),
                         reads=[kkb], writes=[scrb])
                    s.op("act", lambda e: e.copy(out=Vs[:, t, g * 4:(g + 1) * 4, 0:64], in_=kv3[:, :, 64:128]), reads=[kkb], writes=[Vsb])
                s.op("dve", lambda e: e.tensor_reduce(out=st2[:, 4:12], in_=scr[:, 0:512].rearrange("p (h d) -> p h d", d=64), axis=AX.X, op=ALU.add),
                     reads=[scrb], writes=[st2b])
                s.op("dve", lambda e: e.tensor_scalar(out=st2[:, 4:12], in0=st2[:, 4:12], scalar1=st2[:, 2:3], scalar2=None, op0=ALU.add),
                     reads=[st2b], writes=[st2b])
                s.op("act", lambda e: e.activation(out=st3[:, 4:12], in_=st2[:, 4:12], func=AF.Sqrt, scale=1.0 / 96, bias=k.eps[:, 0:1]),
                     reads=[st2b], writes=[st3b])
                s.op("dve", lambda e: e.reciprocal(out=st3[:, 4:12], in_=st3[:, 4:12]), reads=[st3b], writes=[st3b])
                for g, (kk, kkb) in enumerate(((ka, kab), (kb_, kbb))):
                    kv3 = kk[:, :].rearrange("p (h d) -> p h d", d=128)
                    s.op("dve", lambda e: e.tensor_tensor(out=kn[:, g * 4:(g + 1) * 4, :], in0=kv3[:, :, 0:64],
                                                          in1=st3[:, 4 + g * 4:8 + g * 4].unsqueeze(2).to_broadcast([128, 4, 64]), op=ALU.mult),
                         reads=[kkb, st3b], writes=[knb])
                s.op("pool", lambda e: e.tensor_tensor(out=kf[:, :, 0:64], in0=kn[:], in1=gk[:, 0:64].unsqueeze(1).to_broadcast([128, 8, 64]), op=ALU.mult),
                     reads=[knb, gkb], writes=[kfb])
                s.op("dve", lambda e: e.tensor_tensor(out=krg[:], in0=zsb[:, 640:672], in1=gk[:, 64:96], op=ALU.mult), reads=[zsbb, gkb], writes=[krgb])
                if lat:
                    s.op("dve", lambda e: e.tensor_tensor(out=kt4[:, 0], in0=krg[:, 0:16], in1=rp[:, 0:16], op=ALU.mult), reads=[krgb, rpb], writes=[kt4b])
                    s.op("dve", lambda e: e.tensor_tensor(out=kt4[:, 1], in0=krg[:, 16:32], in1=rp[:, 16:32], op=ALU.mult), reads=[krgb, rpb], writes=[kt4b])
                    s.op("dve", lambda e: e.tensor_tensor(out=kt4[:, 2], in0=krg[:, 0:16], in1=rp[:, 16:32], op=ALU.mult), reads=[krgb, rpb], writes=[kt4b])
                    s.op("dve", lambda e: e.tensor_tensor(out=kt4[:, 3], in0=krg[:, 16:32], in1=rp[:, 0:16], op=ALU.mult), reads=[krgb, rpb], writes=[kt4b])
                    s.op("dve", lambda e: e.tensor_tensor(out=krr[:, 0:16], in0=kt4[:, 0], in1=kt4[:, 1], op=ALU.subtract), reads=[kt4b], writes=[krrb])
                    s.op("dve", lambda e: e.tensor_tensor(out=krr[:, 16:32], in0=kt4[:, 2], in1=kt4[:, 3], op=ALU.add), reads=[kt4b], writes=[krrb])
                else:
                    s.op("dve", lambda e: e.tensor_copy(out=krr[:], in_=krg[:]), reads=[krgb], writes=[krrb])
                s.op("dve", lambda e: e.tensor_tensor(out=kf[:, :, 64:96], in0=krr[:].unsqueeze(1).to_broadcast([128, 8, 32]),
                                                      in1=st3[:, 4:12].unsqueeze(2).to_broadcast([128, 8, 32]), op=ALU.mult),
                     reads=[krrb, st3b], writes=[kfb])
                if lat:
                    tb_, tbb = banks[6]
                    tl = t - NCT
                    transpose_chunks(k, tb_, tbb, lambda h: qf[:, h, :], 8, 96, qT[:, :, tl * 128:(tl + 1) * 128], qTb, qfb, dst_view=True)
                tb_, tbb = banks[0]
                transpose_chunks(k, tb_, tbb, lambda h: kf[:, h, :], 8, 96, kT[:, :, t * 128:(t + 1) * 128], kTb, kfb, dst_view=True)
            s.barrier()

        with ExitStack() as es2:
            pTs = [k.sb(es2, f"p2_pT{i}", [128, 512], BF16) for i in range(3)]
            rc, rcb = k.sb(es2, "p2_rc", [128, 8], F32)
            it = 0
            for h in range(8):
                for g in range(4):
                    for kt in range(NT + NCT):
                        sbk, sbkb = banks[it % 2]
                        pT, pTb = pTs[it % 3]
                        it += 1
                        s.op("pe", lambda e: e.matmul(sbk[:, :], lhsT=kT[:, h, kt * 128:(kt + 1) * 128], rhs=qT[:, h, g * 512:(g + 1) * 512], start=True, stop=True),
                             reads=[kTb, qTb], writes=[sbkb])
                        s.op("act", lambda e: e.activation(out=pT[:], in_=sbk[:, :], func=AF.Exp), reads=[sbkb], writes=[pTb])
                        for qs in range(4):
                            ob, obb = banks[2 + qs]
                            s.op("pe", lambda e: e.matmul(ob[:, 0:65], lhsT=pT[:, qs * 128:(qs + 1) * 128], rhs=Vs[:, kt, h, :],
                                                          start=(kt == 0), stop=(kt == NT + NCT - 1)), reads=[pTb, Vsb], writes=[obb])
                    for qs in range(4):
                        ob, obb = banks[2 + qs]
                        j = (g * 4 + qs) % 8
                        s.op("dve", lambda e: e.reciprocal(out=rc[:, j:j + 1], in_=ob[:, 64:65]), reads=[obb], writes=[rcb])
                        s.op("dve", lambda e: e.tensor_scalar(out=osb[:, g * 4 + qs, h * 64:(h + 1) * 64], in0=ob[:, 0:64], scalar1=rc[:, j:j + 1],
                                                              scalar2=None, op0=ALU.mult), reads=[obb, rcb], writes=[osbb])
            s.barrier()

        with ExitStack() as es3:
            stg = [k.sb(es3, f"p3_stg{i}", [128, 1536], F32) for i in range(2)]
            w_in, w_inb = k.sb(es3, "p3_win", [128, 8, 1536], BF16)
            gqn, gqnb = k.sb(es3, "p3_gq", [128, 64], F32)
            gkn, gknb = k.sb(es3, "p3_gk", [128, 64], F32)
            tn, tnb = k.sb(es3, "p3_tn", [128, 16, 64], F32)
            qkf, qkfb = k.sb(es3, "p3_qkf", [128, 16, 64], BF16)
            wv = W["attn_w_in"].rearrange("(kc p) n -> p kc n", p=128)
            for c in range(8):
                load_cast(k, stg, w_in[:, c, :], w_inb, wv[:, c, 672:2208], 1536, c)
            load_bcast(k, gqn[:], gqnb, W["na_g_q"], 64)
            load_bcast(k, gkn[:], gknb, W["na_g_k"], 64)
            s.op("dve", lambda e: e.tensor_scalar_mul(out=gqn[:], in0=gqn[:], scalar1=64.0 ** -0.5), reads=[gqnb], writes=[gqnb])
            for t in range(NT + NCT):
                lat = t >= NCT
                aT, aTb, scr, scrb = front(t)
                grp = (0, 1, 2) if lat else (1, 2)
                for g in grp:
                    zb, zbb = banks[g]
                    for c in range(8):
                        s.op("pe", lambda e: e.matmul(zb[:, :], lhsT=aT[:, c, :], rhs=w_in[:, c, g * 512:(g + 1) * 512], start=(c == 0), stop=(c == 7)),
                             reads=[aTb, w_inb], writes=[zbb])
                    if g < 2:
                        s.op("act", lambda e: e.activation(out=scr[:, g * 512:(g + 1) * 512], in_=zb[:, :], func=AF.Square), reads=[zbb], writes=[scrb])
                    else:
                        s.op("act", lambda e: e.copy(out=Vs[:, t, :, 0:64], in_=zb[:, :].rearrange("p (h d) -> p h d", d=64)), reads=[zbb], writes=[Vsb])
                g0 = 0 if lat else 1
                s.op("dve", lambda e: e.tensor_reduce(out=st2[:, g0 * 8:16], in_=scr[:, g0 * 512:1024].rearrange("p (h d) -> p h d", d=64), axis=AX.X, op=ALU.add),
                     reads=[scrb], writes=[st2b])
                s.op("act", lambda e: e.activation(out=st3[:, g0 * 8:16], in_=st2[:, g0 * 8:16], func=AF.Sqrt, scale=1.0 / 64, bias=k.eps[:, 0:1]),
                     reads=[st2b], writes=[st3b])
                s.op("dve", lambda e: e.reciprocal(out=st3[:, g0 * 8:16], in_=st3[:, g0 * 8:16]), reads=[st3b], writes=[st3b])
                for g in grp[:-1]:
                    zb, zbb = banks[g]
                    gg, ggb = (gqn, gqnb) if g == 0 else (gkn, gknb)
                    s.op("dve", lambda e: e.tensor_tensor(out=tn[:, g * 8:(g + 1) * 8, :], in0=zb[:, :].rearrange("p (h d) -> p h d", d=64),
                                                          in1=st3[:, g * 8:(g + 1) * 8].unsqueeze(2).to_broadcast([128, 8, 64]), op=ALU.mult),
                         reads=[zbb, st3b], writes=[tnb])
                    s.op("pool", lambda e: e.tensor_tensor(out=qkf[:, g * 8:(g + 1) * 8, :], in0=tn[:, g * 8:(g + 1) * 8, :],
                                                           in1=gg[:].unsqueeze(1).to_broadcast([128, 8, 64]), op=ALU.mult),
                         reads=[tnb, ggb], writes=[qkfb])
                if lat:
                    tb_, tbb = banks[3]
                    tl = t - NCT
                    transpose_chunks(k, tb_, tbb, lambda h: qkf[:, h, :], 8, 64, qT[0:64, :, tl * 128:(tl + 1) * 128], qTb, qkfb, dst_view=True)
                tb_, tbb = banks[4]
                transpose_chunks(k, tb_, tbb, lambda h: qkf[:, 8 + h, :], 8, 64, kT[0:64, :, t * 128:(t + 1) * 128], kTb, qkfb, dst_view=True)
            s.barrier()

        with ExitStack() as es4:
            nbs = [k.sb(es4, f"p4_nb{i}", [128, 21, 128], F32) for i in range(2)]
            sfs = [k.sb(es4, f"p4_sf{i}", [128, 640], F32) for i in range(2)]
            pTs = [k.sb(es4, f"p4_pT{i}", [128, 896], BF16) for i in range(2)]
            rc, rcb = k.sb(es4, "p4_rc", [128, 8], F32)
            it = 0
            for h in range(8):
                nb, nbb = nbs[h % 2]
                s.dma("sp", lambda e: e.dma_start(out=nb[:], in_=W["nabias"][h]), writes=[nbb])
                for qt in range(NT):
                    blocks = na_blocks(qt)
                    nloc = len(blocks)
                    (sa, sab), (sb_, sbb) = banks[(it % 2) * 2], banks[(it % 2) * 2 + 1]
                    sf, sfb = sfs[it % 2]
                    pT, pTb = pTs[it % 2]
                    ob, obb = banks[4 + it % 2]
                    it += 1
                    qsl = qT[0:64, h, qt * 128:(qt + 1) * 128]
                    for j, (kt, bi) in enumerate(blocks):
                        dstb_, dstbb = (sa, sab) if j < 4 else (sb_, sbb)
                        col = (j % 4) * 128
                        s.op("pe", lambda e: e.matmul(dstb_[:, col:col + 128], lhsT=kT[0:64, h, (NCT + kt) * 128:(NCT + kt + 1) * 128], rhs=qsl, start=True, stop=True),
                             reads=[kTb, qTb], writes=[dstbb])
                    for c in range(NCT):
                        s.op("pe", lambda e: e.matmul(sb_[:, 128 + c * 128:256 + c * 128], lhsT=kT[0:64, h, c * 128:(c + 1) * 128], rhs=qsl, start=True, stop=True),
                             reads=[kTb, qTb], writes=[sbb])
                    b0 = blocks[0][1]
                    s.op("dve", lambda e: e.tensor_tensor(out=sf[:, 0:512], in0=sa[:, :], in1=nb[:, b0:b0 + 4, :].rearrange("p b q -> p (b q)"), op=ALU.add),
                         reads=[sab, nbb], writes=[sfb])
                    if nloc == 5:
                        s.op("dve", lambda e: e.tensor_tensor(out=sf[:, 512:640], in0=sb_[:, 0:128], in1=nb[:, b0 + 4, :], op=ALU.add),
                             reads=[sbb, nbb], writes=[sfb])
                    s.op("act", lambda e: e.activation(out=pT[:, 0:nloc * 128], in_=sf[:, 0:nloc * 128], func=AF.Exp), reads=[sfb], writes=[pTb])
                    s.op("act", lambda e: e.activation(out=pT[:, 640:896], in_=sb_[:, 128:384], func=AF.Exp), reads=[sbb], writes=[pTb])
                    nblk = nloc + NCT
                    for j, (kt, bi) in enumerate(blocks):
                        s.op("pe", lambda e: e.matmul(ob[:, 0:65], lhsT=pT[:, j * 128:(j + 1) * 128], rhs=Vs[:, NCT + kt, h, :], start=(j == 0), stop=False),
                             reads=[pTb, Vsb], writes=[obb])
                    for c in range(NCT):
                        s.op("pe", lambda e: e.matmul(ob[:, 0:65], lhsT=pT[:, 640 + c * 128:768 + c * 128], rhs=Vs[:, c, h, :], start=False, stop=(c == NCT - 1)),
                             reads=[pTb, Vsb], writes=[obb])
                    j8 = it % 8
                    s.op("dve", lambda e: e.reciprocal(out=rc[:, j8:j8 + 1], in_=ob[:, 64:65]), reads=[obb], writes=[rcb])
                    s.op("dve", lambda e: e.tensor_scalar(out=osb[:, qt, 512 + h * 64:512 + (h + 1) * 64], in0=ob[:, 0:64], scalar1=rc[:, j8:j8 + 1],
                                                          scalar2=None, op0=ALU.mult), reads=[obb, rcb], writes=[osbb])
            s.barrier()

        with ExitStack() as es5:
            stg = [k.sb(es5, f"p5_stg{i}", [128, 1024], F32) for i in range(2)]
            w_o, w_ob = k.sb(es5, "p5_wo", [128, 8, 1024], BF16)
            G1, G1b = k.sb(es5, "p5_g1", [128, 1024], F32)
            outs = [k.sb(es5, f"p5_out{i}", [128, 1024], F32) for i in range(2)]
            wv = W["attn_w_out"].rearrange("(kc p) n -> p kc n", p=128)
            for c in range(8):
                load_cast(k, stg, w_o[:, c, :], w_ob, wv[:, c, :], 1024, c)
            load_bcast(k, G1[:], G1b, modv0[0:1, 2, :], 1024)
            for t in range(NT):
                xt, xb = xts[t % 2]
                scr, scrb = scrs[t % 2]
                aT, aTb = aTs[t % 2]
                ot, otb = outs[t % 2]
                s.dma("sp", lambda e: e.dma_start(out=xt[:], in_=x[t * 128:(t + 1) * 128, :]), writes=[xb])
                bank, bb = banks[7]
                transpose_chunks(k, bank, bb, lambda c: osb[:, t, c * 128:(c + 1) * 128], 8, 128, aT[:], aTb, osbb, dst_view=True)
                for g in range(2):
                    yb, ybb = banks[g]
                    for c in range(8):
                        s.op("pe", lambda e: e.matmul(yb[:, :], lhsT=aT[:, c, :], rhs=w_o[:, c, g * 512:(g + 1) * 512], start=(c == 0), stop=(c == 7)),
                             reads=[aTb, w_ob], writes=[ybb])
                    s.op("dve", lambda e: e.tensor_tensor(out=scr[:, g * 512:(g + 1) * 512], in0=yb[:, :], in1=G1[:, g * 512:(g + 1) * 512], op=ALU.mult),
                         reads=[ybb, G1b], writes=[scrb])
                s.op("pool", lambda e: e.tensor_tensor(out=ot[:], in0=scr[:], in1=xt[:], op=ALU.add), reads=[scrb, xb], writes=[otb])
                s.dma("sp", lambda e: e.dma_start(out=hout[t * 128:(t + 1) * 128, :], in_=ot[:]), reads=[otb], writes=[houtb[t]])
            s.barrier()


def host_rope_table():
    t = np.arange(SEQ)
    row = (t // 64).astype(np.float32)
    col = (t % 64).astype(np.float32)
    inv = (np.float32(1.0) / (np.float32(10000.0) ** (np.arange(8, dtype=np.float32) / np.float32(8)))).astype(np.float32)
    ang = np.concatenate([row[:, None] * inv, col[:, None] * inv], axis=-1).astype(np.float32)
    return np.concatenate([np.cos(ang), np.sin(ang)], axis=-1).astype(np.float32)


def host_na_bias(rpb):
    pairs = [(2, j) for j in range(5)] + [(0, j) for j in range(4)] + [(1, j) for j in range(4)] \
        + [(14, 12 + j) for j in range(4)] + [(15, 12 + j) for j in range(4)]
    out = np.full((8, 128, 21, 128), NEG, np.float32)
    p = np.arange(128)
    for bi, (qt, kt) in enumerate(pairs):
        tq = qt * 128 + p
        tk = kt * 128 + p
        r, c = tq // 64, tq % 64
        kr, kc = tk // 64, tk % 64
        r0 = np.clip(r - 4, 0, 24)
        c0 = np.clip(c - 8, 0, 48)
        inside = (kr[:, None] >= r0[None, :]) & (kr[:, None] < r0[None, :] + 8) & (kc[:, None] >= c0[None, :]) & (kc[:, None] < c0[None, :] + 16)
        rr = np.clip(kr[:, None] - r[None, :] + 7, 0, 14)
        rc = np.clip(kc[:, None] - c[None, :] + 15, 0, 30)
        vals = rpb[:, rr, rc]
        out[:, :, bi, :] = np.where(inside[None], vals, np.float32(NEG))
    return out


NROW = 8


def stage_peer(k, hin, hinb, modv_l, w_query, skT_d, u_tab, v_tab, hout, houtb, tag):
    nc, s = k.nc, k.s
    banks = k.banks
    with ExitStack() as es:
        stg = [k.sb(es, f"{tag}_stg{i}", [128, 2048], F32) for i in range(2)]
        wq, wqb = k.sb(es, f"{tag}_wq", [128, 8, 2048], BF16)
        skT, skTb = k.sb(es, f"{tag}_skT", [128, 16, 128], BF16)
        ABG, ABGb = k.sb(es, f"{tag}_ABG", [128, 3, 1024], F32)
        iota_i, iota_ib = k.sb(es, f"{tag}_iotai", [128, 16], I32)
        iota, iotab = k.sb(es, f"{tag}_iota", [128, 16], F32)
        st, stb = k.sb(es, f"{tag}_st", [128, 4], F32)
        xts = [k.sb(es, f"{tag}_x{i}", [128, 1024], F32) for i in range(2)]
        scrs = [k.sb(es, f"{tag}_scr{i}", [128, 1024], F32) for i in range(2)]
        hms = [k.sb(es, f"{tag}_hm{i}", [128, 1024], F32) for i in range(2)]
        hbf, hbfb = k.sb(es, f"{tag}_hbf", [128, 1024], BF16)
        hT, hTb = k.sb(es, f"{tag}_hT", [128, 8, 128], BF16)
        qbf, qbfb = k.sb(es, f"{tag}_qbf", [128, 2048], BF16)
        qT, qTb = k.sb(es, f"{tag}_qT", [128, 16, 128], BF16)
        ssb, ssbb = k.sb(es, f"{tag}_s", [128, 16, 128], F32)
        s2, _ = k.sb(es, f"{tag}_s2", [128, 16, 128], F32)
        m16, _ = k.sb(es, f"{tag}_m16", [128, 16, 16], F32)
        i16, _ = k.sb(es, f"{tag}_i16", [128, 16, 16], U32)
        i16f, i16fb = k.sb(es, f"{tag}_i16f", [128, 16, 16], F32)
        cand, candb = k.sb(es, f"{tag}_cand", [128, 8, 256], F32)
        cand2, _ = k.sb(es, f"{tag}_cand2", [128, 8, 256], F32)
        best, _ = k.sb(es, f"{tag}_best", [128, 8, 16], F32)
        pos, _ = k.sb(es, f"{tag}_pos", [128, 8, 16], U32)
        ab_i, ab_ib = k.sb(es, f"{tag}_abi", [128, 2, 128], I32)
        ab_f, ab_fb = k.sb(es, f"{tag}_abf", [128, 2, 128], F32)
        oh, ohb = k.sb(es, f"{tag}_oh", [128, 8, 16, 16], F32)
        e01, e01b = k.sb(es, f"{tag}_e01", [128, 2, 128], F32)
        idxs = [k.sb(es, f"{tag}_idx{i}", [128, 128], I32) for i in range(2)]
        gts = [k.sb(es, f"{tag}_gate{i}", [128, 8, 16], F32) for i in range(2)]
        gsum, gsumb = k.sb(es, f"{tag}_gsum", [128, 8], F32)
        actv, _ = k.sb(es, f"{tag}_act", [128, 128], F32)
        wgt, wgtb = k.sb(es, f"{tag}_wgt", [128, 128], F32)
        junk, _ = k.sb(es, f"{tag}_junk", [128, 1024], BF16)
        rows = [k.sb(es, f"{tag}_row{i}", [128, 1024], F32) for i in range(NROW)]
        accs = [k.sb(es, f"{tag}_acc{i}", [128, 1024], F32) for i in range(4)]
        ot, otb = k.sb(es, f"{tag}_ot", [128, 1024], F32)
        hpb = [[Buf() for _ in range(16)] for _ in range(3)]
        hb = [[Buf() for _ in range(8)] for _ in range(3)]
        actb = [Buf() for _ in range(16)]

        qi = load_w_bf16(k, stg, wq, wqb, w_query, 8, 2048)
        load_cast(k, stg, skT[:].rearrange("p a b -> p (a b)"), skTb, skT_d.rearrange("p a b -> p (a b)"), 2048, qi)
        for j in range(3):
            load_bcast(k, ABG[:, j, :], ABGb, modv_l[0:1, 3 + j, :], 1024)
        s.op("pool", lambda e: e.iota(out=iota_i[:], pattern=[[1, 16]], base=0, channel_multiplier=0), writes=[iota_ib])
        s.op("dve", lambda e: e.tensor_copy(out=iota[:], in_=iota_i[:]), reads=[iota_ib], writes=[iotab])

        def front(t):
            xt, xb = xts[t % 2]
            scr, scrb = scrs[t % 2]
            hm, hmb = hms[t % 2]
            idx, idxb = idxs[t % 2]
            gate, gateb = gts[t % 2]
            s.dma("sp", lambda e: e.dma_start(out=xt[:], in_=hin[t * 128:(t + 1) * 128, :]), reads=[hinb[t]], writes=[xb])
            norm_mod(k, xt, xb, ABG[:, 0, :], ABG[:, 1, :], ABGb, scr, scrb, st, stb, hbf, hbfb, out_f32=(hm, hmb))
            bank, bb = banks[7]
            transpose_chunks(k, bank, bb, lambda c: hbf[:, c * 128:(c + 1) * 128], 8, 128, hT[:], hTb, hbfb, dst_view=True)
            for g in range(4):
                qb_, qbb = banks[g]
                for c in range(8):
                    s.op("pe", lambda e: e.matmul(qb_[:, :], lhsT=hT[:, c, :], rhs=wq[:, c, g * 512:(g + 1) * 512], start=(c == 0), stop=(c == 7)),
                         reads=[hTb, wqb], writes=[qbb])
                s.op("act", lambda e: e.copy(out=qbf[:, g * 512:(g + 1) * 512], in_=qb_[:, :]), reads=[qbb], writes=[qbfb])
            for half in range(2):
                tb_, tbb = banks[4 + half]
                transpose_chunks(k, tb_, tbb, lambda c: qbf[:, (half * 8 + c) * 128:(half * 8 + c + 1) * 128], 8, 128,
                                 qT[:, half * 8:(half + 1) * 8, :], qTb, qbfb, dst_view=True)
            for g in range(4):
                sb_, sbb = banks[g]
                for j in range(4):
                    hp = g * 4 + j
                    s.op("pe", lambda e: e.matmul(sb_[:, j * 128:(j + 1) * 128], lhsT=qT[:, hp, :], rhs=skT[:, hp, :], start=True, stop=True),
                         reads=[qTb, skTb], writes=[sbb])
                s.op("act", lambda e: e.copy(out=ssb[:, g * 4:(g + 1) * 4, :], in_=sb_[:, :].rearrange("p (a b) -> p a b", b=128)), reads=[sbb], writes=[ssbb])
            for hp in range(16):
                s.op("dve", lambda e: e.max(out=m16[:, hp, 0:8], in_=ssb[:, hp, :]), reads=[ssbb], writes=[hpb[0][hp]])
            for hp in range(16):
                s.op("dve", lambda e: e.max_index(out=i16[:, hp, 0:8], in_max=m16[:, hp, 0:8], in_values=ssb[:, hp, :]),
                     reads=[ssbb, hpb[0][hp]], writes=[hpb[1][hp]])
            for hp in range(16):
                s.op("dve", lambda e: e.match_replace(out=s2[:, hp, :], in_to_replace=m16[:, hp, 0:8], in_values=ssb[:, hp, :], imm_value=-1e30),
                     reads=[ssbb, hpb[0][hp]], writes=[hpb[2][hp]])
            for hp in range(16):
                s.op("dve", lambda e: e.max(out=m16[:, hp, 8:16], in_=s2[:, hp, :]), reads=[hpb[2][hp]], writes=[hpb[0][hp]])
            for hp in range(16):
                s.op("dve", lambda e: e.max_index(out=i16[:, hp, 8:16], in_max=m16[:, hp, 8:16], in_values=s2[:, hp, :]),
                     reads=[hpb[2][hp], hpb[0][hp]], writes=[hpb[1][hp]])
            s.op("dve", lambda e: e.tensor_copy(out=i16f[:], in_=i16[:]), reads=hpb[1], writes=[i16fb])
            m4 = m16[:].rearrange("p (h t) a -> p h t a", t=2)
            s.op("dve", lambda e: e.tensor_tensor(out=cand[:].rearrange("p h (a b) -> p h a b", b=16),
                                                  in0=m4[:, :, 0, :].unsqueeze(3).to_broadcast([128, 8, 16, 16]),
                                                  in1=m4[:, :, 1, :].unsqueeze(2).to_broadcast([128, 8, 16, 16]), op=ALU.add),
                 reads=hpb[0], writes=[candb])
            for h in range(8):
                s.op("dve", lambda e: e.max(out=best[:, h, 0:8], in_=cand[:, h, :]), reads=[candb], writes=[hb[0][h]])
            for h in range(8):
                s.op("dve", lambda e: e.max_index(out=pos[:, h, 0:8], in_max=best[:, h, 0:8], in_values=cand[:, h, :]),
                     reads=[candb, hb[0][h]], writes=[hb[1][h]])
            for h in range(8):
                s.op("dve", lambda e: e.match_replace(out=cand2[:, h, :], in_to_replace=best[:, h, 0:8], in_values=cand[:, h, :], imm_value=-1e30),
                     reads=[candb, hb[0][h]], writes=[hb[2][h]])
            for h in range(8):
                s.op("dve", lambda e: e.max(out=best[:, h, 8:16], in_=cand2[:, h, :]), reads=[hb[2][h]], writes=[hb[0][h]])
            for h in range(8):
                s.op("dve", lambda e: e.max_index(out=pos[:, h, 8:16], in_max=best[:, h, 8:16], in_values=cand2[:, h, :]),
                     reads=[hb[2][h], hb[0][h]], writes=[hb[1][h]])
            posi = pos[:].rearrange("p h k -> p (h k)").bitcast(I32)
            s.op("dve", lambda e: e.tensor_single_scalar(out=ab_i[:, 0, :], in_=posi, scalar=4, op=ALU.arith_shift_right), reads=hb[1], writes=[ab_ib])
            s.op("dve", lambda e: e.tensor_single_scalar(out=ab_i[:, 1, :], in_=posi, scalar=15, op=ALU.bitwise_and), reads=hb[1], writes=[ab_ib])
            s.op("dve", lambda e: e.tensor_copy(out=ab_f[:], in_=ab_i[:]), reads=[ab_ib], writes=[ab_fb])
            i4 = i16f[:].rearrange("p (h t) a -> p h t a", t=2)
            for p_ in range(2):
                s.op("dve", lambda e: e.tensor_tensor(out=oh[:], in0=ab_f[:, p_, :].rearrange("p (h k) -> p h k", k=16).unsqueeze(3).to_broadcast([128, 8, 16, 16]),
                                                      in1=iota[:].unsqueeze(1).unsqueeze(1).to_broadcast([128, 8, 16, 16]), op=ALU.is_equal),
                     reads=[ab_fb, iotab], writes=[ohb])
                s.op("dve", lambda e: e.tensor_tensor(out=oh[:], in0=oh[:], in1=i4[:, :, p_, :].unsqueeze(2).to_broadcast([128, 8, 16, 16]), op=ALU.mult),
                     reads=[ohb, i16fb], writes=[ohb])
                s.op("dve", lambda e: e.tensor_reduce(out=e01[:, p_, :].rearrange("p (h k) -> p h k", k=16), in_=oh[:], axis=AX.X, op=ALU.add),
                     reads=[ohb], writes=[e01b])
            s.op("dve", lambda e: e.scalar_tensor_tensor(out=e01[:, 0, :], in0=e01[:, 0, :], scalar=128.0, in1=e01[:, 1, :], op0=ALU.mult, op1=ALU.add),
                 reads=[e01b], writes=[e01b])
            s.op("dve", lambda e: e.tensor_copy(out=idx[:], in_=e01[:, 0, :]), reads=[e01b], writes=[idxb])
            s.op("dve", lambda e: e.tensor_tensor(out=gate[:], in0=best[:], in1=best[:, :, 0:1].to_broadcast([128, 8, 16]), op=ALU.subtract),
                 reads=hb[0], writes=[gateb])
            s.op("act", lambda e: e.activation(out=gate[:], in_=gate[:], func=AF.Exp), reads=[gateb], writes=[gateb])
            s.op("dve", lambda e: e.tensor_reduce(out=gsum[:], in_=gate[:], axis=AX.X, op=ALU.add), reads=[gateb], writes=[gsumb])
            s.op("dve", lambda e: e.reciprocal(out=gsum[:], in_=gsum[:]), reads=[gsumb], writes=[gsumb])
            s.op("dve", lambda e: e.tensor_tensor(out=gate[:], in0=gate[:], in1=gsum[:].unsqueeze(2).to_broadcast([128, 8, 16]), op=ALU.mult),
                 reads=[gateb, gsumb], writes=[gateb])

        ring = [0]

        def gather(tab, idx, idxb, hk):
            rw, rwb = rows[ring[0] % NROW]
            ring[0] += 1
            s.dma("pool", lambda e: e.indirect_dma_start(out=rw[:], out_offset=None, in_=tab,
                                                         in_offset=bass.IndirectOffsetOnAxis(ap=idx[:, hk:hk + 1], axis=0)),
                  reads=[idxb], writes=[rwb])
            return rw, rwb

        def back(t):
            xt, xb = xts[t % 2]
            scr, scrb = scrs[t % 2]
            hm, hmb = hms[t % 2]
            idx, idxb = idxs[t % 2]
            gate, gateb = gts[t % 2]
            for hk in range(128):
                rw, rwb = gather(u_tab, idx, idxb, hk)
                s.op("dve", lambda e: e.scalar_tensor_tensor(out=junk[:], in0=rw[:], scalar=1.0, in1=hm[:], op0=ALU.mult, op1=ALU.mult,
                                                             accum_out=actv[:, hk:hk + 1]), reads=[rwb, hmb], writes=[actb[hk % 16]])
            s.op("act", lambda e: e.activation(out=wgt[:], in_=actv[:], func=AF.Gelu), reads=actb, writes=[wgtb])
            s.op("dve", lambda e: e.tensor_tensor(out=wgt[:], in0=wgt[:], in1=gate[:].rearrange("p h k -> p (h k)"), op=ALU.mult),
                 reads=[wgtb, gateb], writes=[wgtb])
            for hk in range(128):
                rw, rwb = gather(v_tab, idx, idxb, hk)
                ac, acb = accs[hk % 4]
                if hk < 4:
                    s.op("dve", lambda e: e.tensor_scalar(out=ac[:], in0=rw[:], scalar1=wgt[:, hk:hk + 1], scalar2=None, op0=ALU.mult),
                         reads=[rwb, wgtb], writes=[acb])
                else:
                    s.op("dve", lambda e: e.scalar_tensor_tensor(out=ac[:], in0=rw[:], scalar=wgt[:, hk:hk + 1], in1=ac[:], op0=ALU.mult, op1=ALU.add),
                         reads=[rwb, wgtb, acb], writes=[acb])
            (a0, a0b), (a1, a1b), (a2, a2b), (a3, a3b) = accs
            s.op("pool", lambda e: e.tensor_tensor(out=a0[:], in0=a0[:], in1=a1[:], op=ALU.add), reads=[a0b, a1b], writes=[a0b])
            s.op("pool", lambda e: e.tensor_tensor(out=a2[:], in0=a2[:], in1=a3[:], op=ALU.add), reads=[a2b, a3b], writes=[a2b])
            s.op("pool", lambda e: e.tensor_tensor(out=a0[:], in0=a0[:], in1=a2[:], op=ALU.add), reads=[a0b, a2b], writes=[a0b])
            s.op("dve", lambda e: e.tensor_tensor(out=scr[:], in0=a0[:], in1=ABG[:, 2, :], op=ALU.mult), reads=[a0b, ABGb], writes=[scrb])
            s.op("pool", lambda e: e.tensor_tensor(out=ot[:], in0=scr[:], in1=xt[:], op=ALU.add), reads=[scrb, xb], writes=[otb])
            s.dma("sp", lambda e: e.dma_start(out=hout[t * 128:(t + 1) * 128, :], in_=ot[:]), reads=[otb], writes=[houtb[t]])

        front(0)
        for t in range(NT):
            if t + 1 < NT:
                front(t + 1)
            back(t)
        s.barrier()


def stage_conv(k, hin, hinb, modv_l, W, hout, houtb):
    nc, s = k.nc, k.s
    banks = k.banks
    PADW = SEQ + 30
    with ExitStack() as es:
        cbuf, cbufb = k.sb(es, "cv_cbuf", [128, NT, 1024], F32)
        ABG, ABGb = k.sb(es, "cv_ABG", [128, 2, 1024], F32)
        st, stb = k.sb(es, "cv_st", [128, 8], F32)
        xts = [k.sb(es, f"cv_x{i}", [128, 1024], F32) for i in range(2)]
        scrs = [k.sb(es, f"cv_scr{i}", [128, 1024], F32) for i in range(2)]
        for j in range(2):
            load_bcast(k, ABG[:, j, :], ABGb, modv_l[0:1, j, :], 1024)
        cbt = [Buf() for _ in range(NT // 4)]
        with ExitStack() as es1:
            stg = [k.sb(es1, f"cv_stg{i}", [128, 1024], F32) for i in range(2)]
            aTa, aTab = k.sb(es1, "cv_aT", [128, 8, SEQ], BF16)
            w1, w1b = k.sb(es1, "cv_w1", [128, 8, 2048], BF16)
            b1T, b1Tb = k.sb(es1, "cv_b1T", [128, 16], F32)
            wdw, wdwb = k.sb(es1, "cv_wdw", [128, 8, 31], F32)
            bdw, bdwb = k.sb(es1, "cv_bdw", [128, 8], F32)
            abfs = [k.sb(es1, f"cv_a{i}", [128, 1024], BF16) for i in range(2)]
            upads = [k.sb(es1, f"cv_up{i}", [128, PADW], F32) for i in range(2)]
            accs = [k.sb(es1, f"cv_acc{i}", [128, SEQ], F32) for i in range(2)]
            sgs = [k.sb(es1, f"cv_sg{i}", [128, 512], F32) for i in range(2)]
            load_w_bf16(k, stg, w1, w1b, W["conv_w_pw1"], 8, 2048)
            s.dma("sp", lambda e: e.dma_start(out=b1T[:], in_=W["conv_b1T"]), writes=[b1Tb])
            s.dma("sp", lambda e: e.dma_start(out=wdw[:], in_=W["conv_wdwT"]), writes=[wdwb])
            s.dma("sp", lambda e: e.dma_start(out=bdw[:], in_=W["conv_bdwT"]), writes=[bdwb])
            for up, upb in upads:
                s.op("pool", lambda e: e.memset(up[:, 0:15], 0.0), writes=[upb])
                s.op("pool", lambda e: e.memset(up[:, 15 + SEQ:PADW], 0.0), writes=[upb])
            for t in range(NT):
                xt, xb = xts[t % 2]
                scr, scrb = scrs[t % 2]
                abf, abfb = abfs[t % 2]
                s.dma("sp", lambda e: e.dma_start(out=xt[:], in_=hin[t * 128:(t + 1) * 128, :]), reads=[hinb[t]], writes=[xb])
                norm_mod(k, xt, xb, ABG[:, 0, :], ABG[:, 1, :], ABGb, scr, scrb, st, stb, abf, abfb)
                bank, bb = banks[6 + t % 2]
                transpose_chunks(k, bank, bb, lambda c: abf[:, c * 128:(c + 1) * 128], 8, 128, aTa[:, :, t * 128:(t + 1) * 128], aTab, abfb, dst_view=True)
            accbs = [[Buf() for _ in range(4)] for _ in range(2)]
            it = 0
            for m in range(8):
                up, upb = upads[m % 2]
                acc, _ = accs[m % 2]
                accb = accbs[m % 2]
                for tg in range(4):
                    (bv_, bvb), (bg_, bgb) = banks[(it % 2) * 2], banks[(it % 2) * 2 + 1]
                    sg, sgb = sgs[it % 2]
                    it += 1
                    for (bk, bkb, c0) in ((bv_, bvb, m * 128), (bg_, bgb, 1024 + m * 128)):
                        for c in range(8):
                            s.op("pe", lambda e: e.matmul(bk[:, :], lhsT=w1[:, c, c0:c0 + 128], rhs=aTa[:, c, tg * 512:(tg + 1) * 512], start=(c == 0), stop=(c == 7)),
                                 reads=[w1b, aTab], writes=[bkb])
                    s.op("act", lambda e: e.activation(out=sg[:], in_=bg_[:, :], func=AF.Sigmoid, bias=b1T[:, 8 + m:9 + m]), reads=[bgb, b1Tb], writes=[sgb])
                    s.op("dve", lambda e: e.scalar_tensor_tensor(out=up[:, 15 + tg * 512:15 + (tg + 1) * 512], in0=bv_[:, :], scalar=b1T[:, m:m + 1], in1=sg[:],
                                                                 op0=ALU.add, op1=ALU.mult), reads=[bvb, sgb, b1Tb], writes=[upb])
                for j in range(31):
                    for ch in range(4):
                        src = up[:, ch * 512 + j:ch * 512 + j + 512]
                        dst = acc[:, ch * 512:(ch + 1) * 512]
                        if j == 0:
                            s.op("dve", lambda e: e.tensor_scalar(out=dst, in0=src, scalar1=wdw[:, m, 0:1], scalar2=bdw[:, m:m + 1], op0=ALU.mult, op1=ALU.add),
                                 reads=[upb, wdwb, bdwb], writes=[accb[ch]])
                        else:
                            s.op("dve", lambda e: e.scalar_tensor_tensor(out=dst, in0=src, scalar=wdw[:, m, j:j + 1], in1=dst, op0=ALU.mult, op1=ALU.add),
                                 reads=[upb, wdwb, accb[ch]], writes=[accb[ch]])
                for g in range(NT // 4):
                    tb_, tbb = banks[4 + g % 2]
                    for j in range(4):
                        t = g * 4 + j
                        s.op("pe", lambda e: e.transpose(out=tb_[:, j * 128:(j + 1) * 128], in_=acc[:, t * 128:(t + 1) * 128], identity=k.identf[:]),
                             reads=[accb[t // 4], k.identb], writes=[tbb])
                    s.op("act", lambda e: e.copy(out=cbuf[:, g * 4:(g + 1) * 4, m * 128:(m + 1) * 128], in_=tb_[:, :].rearrange("p (a b) -> p a b", b=128)),
                         reads=[tbb], writes=[cbt[g]])
            s.barrier()
        with ExitStack() as es3:
            stg = [k.sb(es3, f"cv3_stg{i}", [128, 1024], F32) for i in range(2)]
            w2, w2b = k.sb(es3, "cv3_w2", [128, 8, 1024], BF16)
            gl, glb = k.sb(es3, "cv3_gl", [128, 4, 1024], F32)
            sbfs = [k.sb(es3, f"cv3_s{i}", [128, 1024], BF16) for i in range(2)]
            sTs = [k.sb(es3, f"cv3_sT{i}", [128, 8, 128], BF16) for i in range(2)]
            outs = [k.sb(es3, f"cv3_o{i}", [128, 1024], F32) for i in range(2)]
            wv = W["conv_w_pw2"].rearrange("(kc p) n -> p kc n", p=128)
            for c in range(8):
                load_cast(k, stg, w2[:, c, :], w2b, wv[:, c, :], 1024, c)
            load_bcast(k, gl[:, 0, :], glb, W["conv_g_ln"], 1024)
            load_bcast(k, gl[:, 1, :], glb, W["conv_b_ln"], 1024)
            load_bcast(k, gl[:, 2, :], glb, W["conv_b_pw2"], 1024)
            load_bcast(k, gl[:, 3, :], glb, modv_l[0:1, 2, :], 1024)
            for t in range(NT):
                xt, xb = xts[t % 2]
                scr, scrb = scrs[t % 2]
                sbf, sbfb = sbfs[t % 2]
                sT, sTb = sTs[t % 2]
                ot, otb = outs[t % 2]
                cb = cbt[t // 4]
                c_t = cbuf[:, t, :]
                s.dma("sp", lambda e: e.dma_start(out=xt[:], in_=hin[t * 128:(t + 1) * 128, :]), reads=[hinb[t]], writes=[xb])
                s.op("act", lambda e: e.activation(out=scr[:], in_=c_t, func=AF.Identity, accum_out=st[:, 0:1]), reads=[cb], writes=[scrb, stb])
                s.op("act", lambda e: e.activation(out=scr[:], in_=c_t, func=AF.Square, accum_out=st[:, 1:2]), reads=[cb], writes=[scrb, stb])
                s.op("dve", lambda e: e.tensor_scalar(out=st[:, 2:3], in0=st[:, 0:1], scalar1=1.0 / D, scalar2=None, op0=ALU.mult), reads=[stb], writes=[stb])
                s.op("dve", lambda e: e.scalar_tensor_tensor(out=st[:, 3:4], in0=st[:, 2:3], scalar=-1.0, in1=st[:, 2:3], op0=ALU.mult, op1=ALU.mult),
                     reads=[stb], writes=[stb])
                s.op("dve", lambda e: e.scalar_tensor_tensor(out=st[:, 4:5], in0=st[:, 1:2], scalar=1.0 / D, in1=st[:, 3:4], op0=ALU.mult, op1=ALU.add),
                     reads=[stb], writes=[stb])
                s.op("act", lambda e: e.activation(out=st[:, 5:6], in_=st[:, 4:5], func=AF.Sqrt, scale=1.0, bias=k.eps[:, 0:1]), reads=[stb], writes=[stb])
                s.op("dve", lambda e: e.reciprocal(out=st[:, 5:6], in_=st[:, 5:6]), reads=[stb], writes=[stb])
                s.op("dve", lambda e: e.tensor_scalar(out=scr[:], in0=c_t, scalar1=st[:, 2:3], scalar2=st[:, 5:6], op0=ALU.subtract, op1=ALU.mult),
                     reads=[cb, stb], writes=[scrb])
                s.op("dve", lambda e: e.tensor_tensor(out=scr[:], in0=scr[:], in1=gl[:, 0, :], op=ALU.mult), reads=[scrb, glb], writes=[scrb])
                s.op("pool", lambda e: e.tensor_tensor(out=scr[:], in0=scr[:], in1=gl[:, 1, :], op=ALU.add), reads=[scrb, glb], writes=[scrb])
                s.op("act", lambda e: e.activation(out=sbf[:], in_=scr[:], func=AF.Silu), reads=[scrb], writes=[sbfb])
                bank, bb = banks[7]
                transpose_chunks(k, bank, bb, lambda c: sbf[:, c * 128:(c + 1) * 128], 8, 128, sT[:], sTb, sbfb, dst_view=True)
                for g in range(2):
                    yb, ybb = banks[g]
                    for c in range(8):
                        s.op("pe", lambda e: e.matmul(yb[:, :], lhsT=sT[:, c, :], rhs=w2[:, c, g * 512:(g + 1) * 512], start=(c == 0), stop=(c == 7)),
                             reads=[sTb, w2b], writes=[ybb])
                    s.op("dve", lambda e: e.tensor_tensor(out=scr[:, g * 512:(g + 1) * 512], in0=yb[:, :], in1=gl[:, 2, g * 512:(g + 1) * 512], op=ALU.add),
                         reads=[ybb, glb], writes=[scrb])
                s.op("pool", lambda e: e.tensor_tensor(out=scr[:], in0=scr[:], in1=gl[:, 3, :], op=ALU.mult), reads=[scrb, glb], writes=[scrb])
                s.op("pool", lambda e: e.tensor_tensor(out=ot[:], in0=scr[:], in1=xt[:], op=ALU.add), reads=[scrb, xb], writes=[otb])
                s.dma("sp", lambda e: e.dma_start(out=hout[t * 128:(t + 1) * 128, :], in_=ot[:]), reads=[otb], writes=[houtb[t]])
            s.barrier()


IN_SPECS = {
    "x": ([SEQ, D], F32), "ctx": ([CTX, D], F32), "cc": ([128, 16], F32),
    "w_ada": ([2, D, 6 * D], F32), "b_ada": ([2, 6 * D], F32), "g_norm": ([4, D], F32),
    "ident": ([128, 128], BF16), "identf": ([128, 128], F32),
    "attn_w_in": ([D, 2208], F32), "mla_w_q_up": ([384, 768], F32), "mla_w_kv_up": ([256, 1024], F32),
    "mla_g_qa": ([1, 384], F32), "mla_g_kva": ([1, 256], F32), "mla_g_q": ([1, 96], F32), "mla_g_k": ([1, 96], F32),
    "na_g_q": ([1, 64], F32), "na_g_k": ([1, 64], F32), "attn_w_out": ([D, D], F32),
    "rope": ([SEQ, 32], F32), "nabias": ([8, 128, 21, 128], F32),
    "conv_w_pw1": ([D, 2 * D], F32), "conv_b1T": ([128, 16], F32), "conv_wdwT": ([128, 8, 31], F32), "conv_bdwT": ([128, 8], F32),
    "conv_g_ln": ([1, D], F32), "conv_b_ln": ([1, D], F32), "conv_w_pw2": ([D, D], F32), "conv_b_pw2": ([1, D], F32),
    "wq0": ([D, 2048], F32), "wq1": ([D, 2048], F32), "skT0": ([128, 16, 128], F32), "skT1": ([128, 16, 128], F32),
    "u0": ([16384, D], F32), "u1": ([16384, D], F32), "v0": ([16384, D], F32), "v1": ([16384, D], F32),
}


def build_program():
    nc = bass.Bass("TRN2", target_bir_lowering=False)
    A = {n: nc.dram_tensor(n, sh, dt, kind="ExternalInput").ap() for n, (sh, dt) in IN_SPECS.items()}
    out = nc.dram_tensor("out", [SEQ, D], F32, kind="ExternalOutput").ap()
    modv = nc.dram_tensor("modv_scr", [2, 2, 6, D], F32, kind="Internal").ap()
    hs = [nc.dram_tensor(f"h_scr{i}", [SEQ, D], F32, kind="Internal").ap() for i in range(3)]
    hb = [[Buf() for _ in range(NT)] for _ in range(4)]
    with ExitStack() as es:
        k = K(nc, es)
        k.modv_buf = Buf()
        setup_consts(k, es, A["ident"], A["identf"])
        stage_ada(k, A["cc"], A["w_ada"], A["b_ada"], A["g_norm"], modv)
        stage_attn(k, A["x"], A["ctx"], modv[0], A, hs[0], hb[0])
        stage_peer(k, hs[0], hb[0], modv[0], A["wq0"], A["skT0"], A["u0"], A["v0"], hs[1], hb[1], "pra")
        stage_conv(k, hs[1], hb[1], modv[1], A, hs[2], hb[2])
        stage_peer(k, hs[2], hb[2], modv[1], A["wq1"], A["skT1"], A["u1"], A["v1"], out, hb[3], "prb")
        k.s.barrier()
    return nc


def kernel(**inp):
    import ml_dtypes
    f = lambda a: np.ascontiguousarray(np.asarray(a, dtype=np.float32))
    nb = inp["x"].shape[0]
    shared = {
        "w_ada": f(inp["w_ada"]), "b_ada": f(inp["b_ada"]),
        "g_norm": f(np.stack([inp["g_norm1"][0], inp["g_norm2"][0], inp["g_norm1"][1], inp["g_norm2"][1]])),
        "ident": np.eye(128).astype(ml_dtypes.bfloat16), "identf": np.eye(128, dtype=np.float32),
        "attn_w_in": f(inp["attn_w_in"][0]), "mla_w_q_up": f(inp["mla_w_q_up"][0]), "mla_w_kv_up": f(inp["mla_w_kv_up"][0]),
        "mla_g_qa": f(inp["mla_g_qa"]), "mla_g_kva": f(inp["mla_g_kva"]), "mla_g_q": f(inp["mla_g_q"]), "mla_g_k": f(inp["mla_g_k"]),
        "na_g_q": f(inp["na_g_q"]), "na_g_k": f(inp["na_g_k"]), "attn_w_out": f(inp["attn_w_out"][0]),
        "rope": host_rope_table(), "nabias": host_na_bias(np.asarray(inp["na_rpb"][0], np.float32)),
        "conv_w_pw1": f(inp["conv_w_pw1"][0]), "conv_b1T": f(np.asarray(inp["conv_b_pw1"][0]).reshape(16, 128).T),
        "conv_wdwT": f(np.asarray(inp["conv_w_dw"][0]).reshape(31, 8, 128).transpose(2, 1, 0)),
        "conv_bdwT": f(np.asarray(inp["conv_b_dw"][0]).reshape(8, 128).T),
        "conv_g_ln": f(inp["conv_g_ln"]), "conv_b_ln": f(inp["conv_b_ln"]), "conv_w_pw2": f(inp["conv_w_pw2"][0]), "conv_b_pw2": f(inp["conv_b_pw2"]),
    }
    for l in range(2):
        shared[f"wq{l}"] = f(inp["peer_w_query"][l])
        shared[f"skT{l}"] = f(np.asarray(inp["peer_sub_keys"][l]).reshape(16, 128, 128).transpose(2, 0, 1))
        shared[f"u{l}"] = f(inp["peer_u"][l])
        shared[f"v{l}"] = f(inp["peer_v"][l])
    in_maps = []
    for b in range(nb):
        cc = np.zeros((128, 16), np.float32)
        cc[:, 0::2] = np.asarray(inp["c"][b], np.float32).reshape(8, 128).T
        cc[:, 1::2] = np.asarray(inp["c_ctx"], np.float32).reshape(8, 128).T
        m = dict(shared)
        m["x"] = f(inp["x"][b])
        m["ctx"] = f(inp["ctx"][b])
        m["cc"] = cc
        in_maps.append(m)
    nc = build_program()
    res = run_bass_kernel_spmd(nc, in_maps, core_ids=list(range(nb)))
    return np.stack([np.asarray(r["out"], dtype=np.float32) for r in res.results], axis=0)
```

```python
import numpy as np
from contextlib import ExitStack
import concourse.bass as bass
import concourse.mybir as mybir
from concourse.bass_utils import run_bass_kernel_spmd

F32 = mybir.dt.float32
BF16 = mybir.dt.bfloat16
I32 = mybir.dt.int32
U32 = mybir.dt.uint32
ALU = mybir.AluOpType
AF = mybir.ActivationFunctionType
AX = mybir.AxisListType

D = 1024
SEQ = 2048
NT = SEQ // 128
CTX = 256
NCT = CTX // 128
EPS = 1e-6
NEG = -30000.0


class Buf:
    __slots__ = ("w", "r")

    def __init__(self):
        self.w = None
        self.r = {}


class Sched:
    RING = 12

    def __init__(self, nc, es):
        self.nc = nc
        self.eng = {"pe": nc.tensor, "act": nc.scalar, "dve": nc.vector, "pool": nc.gpsimd, "sp": nc.sync}
        self.semobj = {}
        self.cnt = {}
        for k in self.eng:
            self.semobj[k] = es.enter_context(nc.semaphore("s_" + k))
            self.cnt[k] = 0
        self.waited = {k: {} for k in self.eng}
        self.bulk = []
        self.dq = {}
        for q in ("sp", "pool", "act"):
            slots = []
            for i in range(self.RING):
                key = ("d", q, i)
                self.semobj[key] = es.enter_context(nc.semaphore(f"d_{q}_{i}"))
                slots.append(key)
            self.dq[q] = {"slots": slots, "uses": [0] * self.RING, "next": 0}

    def _wait(self, ek, tok):
        if tok is None:
            return
        sk, v = tok
        if self.waited[ek].get(sk, 0) >= v:
            return
        self.eng[ek].wait_ge(self.semobj[sk], v)
        self.waited[ek][sk] = v

    def _deps(self, ek, reads, writes):
        for b in reads:
            self._wait(ek, b.w)
        for b in writes:
            self._wait(ek, b.w)
            for sk, v in b.r.items():
                self._wait(ek, (sk, v))

    def _mark(self, tok, reads, writes):
        sk, v = tok
        for b in reads:
            if b.r.get(sk, 0) < v:
                b.r[sk] = v
        for b in writes:
            b.w = tok
            b.r = {}

    def op(self, ek, fn, reads=(), writes=()):
        self._deps(ek, reads, writes)
        ins = fn(self.eng[ek])
        self.cnt[ek] += 1
        ins.then_inc(self.semobj[ek], 1)
        tok = (ek, self.cnt[ek])
        self._mark(tok, reads, writes)
        return tok

    def dma(self, q, fn, reads=(), writes=()):
        dq = self.dq[q]
        slot = dq["next"]
        dq["next"] = (slot + 1) % self.RING
        key = dq["slots"][slot]
        uses = dq["uses"][slot]
        if uses:
            self._wait(q, (key, 16 * uses))
        self._deps(q, reads, writes)
        ins = fn(self.eng[q])
        ins.then_inc(self.semobj[key], 16)
        dq["uses"][slot] = uses + 1
        tok = (key, 16 * (uses + 1))
        self._mark(tok, reads, writes)
        return tok

    def bulk_dma(self, q, fn, reads=(), writes=(), es=None):
        key = ("bulk", len(self.semobj))
        self.semobj[key] = es.enter_context(self.nc.semaphore(f"bulk{len(self.semobj)}"))
        self._deps(q, reads, writes)
        ins = fn(self.eng[q])
        ins.then_inc(self.semobj[key], 16)
        tok = (key, 16)
        self.bulk.append(tok)
        self._mark(tok, reads, writes)
        return tok

    def barrier(self):
        toks = [(k, self.cnt[k]) for k in self.eng if self.cnt[k]]
        for q, dq in self.dq.items():
            for key, u in zip(dq["slots"], dq["uses"]):
                if u:
                    toks.append((key, 16 * u))
        toks.extend(self.bulk)
        for ek in self.eng:
            for t in toks:
                self._wait(ek, t)


class K:
    def __init__(self, nc, es):
        self.nc = nc
        self.es = es
        self.s = Sched(nc, es)
        self.banks = []
        for i in range(8):
            t = es.enter_context(nc.psum_tensor(f"bank{i}", [128, 512], F32))
            self.banks.append((t, Buf()))
        self.ident = None

    def sb(self, es, name, shape, dt):
        t = es.enter_context(self.nc.sbuf_tensor(name, list(shape), dt))
        return t, Buf()


def bcast_row(ap_row, parts):
    return ap_row.partition_broadcast(parts) if len(ap_row.shape) == 1 else ap_row.to_broadcast([parts, ap_row.shape[-1]])


def stage_ada(k, cc, w_ada, b_ada, g_norm, modv):
    nc, s = k.nc, k.s
    with ExitStack() as es:
        cct, ccb = k.sb(es, "ada_cc", [128, 16], F32)
        sil, silb = k.sb(es, "ada_sil", [128, 16], F32)
        wt = [k.sb(es, f"ada_w{i}", [128, 8, 512], F32) for i in range(2)]
        brow, browb = k.sb(es, "ada_b", [2, 6144], F32)
        grow, growb = k.sb(es, "ada_g", [2, 2, 1024], F32)
        mrow, mrowb = k.sb(es, "ada_m", [2, 6144], F32)
        orow, orowb = k.sb(es, "ada_o", [2, 6, 1024], F32)

        s.dma("sp", lambda e: e.dma_start(out=cct[:], in_=cc), writes=[ccb])
        s.op("act", lambda e: e.activation(out=sil[:], in_=cct[:], func=AF.Silu), reads=[ccb], writes=[silb])
        for l in range(2):
            s.dma("sp", lambda e: e.dma_start(out=brow[:], in_=b_ada[l:l + 1, :].to_broadcast([2, 6144])), writes=[browb])
            s.dma("sp", lambda e: e.dma_start(out=grow[:], in_=g_norm[2 * l:2 * l + 2, :].rearrange("(o a) d -> o a d", o=1).to_broadcast([2, 2, 1024])), writes=[growb])
            wv = w_ada[l].rearrange("(kc p) n -> p kc n", p=128)
            for g in range(12):
                wtile, wbuf = wt[g % 2]
                q = "sp" if g % 2 == 0 else "pool"
                s.dma(q, lambda e: e.dma_start(out=wtile[:], in_=wv[:, :, g * 512:(g + 1) * 512]), writes=[wbuf])
                bank, bb = k.banks[g % 2]
                for kc in range(8):
                    s.op("pe", lambda e: e.matmul(bank[0:2, :], lhsT=sil[:, 2 * kc:2 * kc + 2], rhs=wtile[:, kc, :],
                                                  start=(kc == 0), stop=(kc == 7)),
                         reads=[silb, wbuf], writes=[bb])
                s.op("dve", lambda e: e.tensor_tensor(out=mrow[:, g * 512:(g + 1) * 512], in0=bank[0:2, :],
                                                      in1=brow[:, g * 512:(g + 1) * 512], op=ALU.add),
                     reads=[bb, browb], writes=[mrowb])
            for j in range(2):
                sh = mrow[:, (3 * j) * 1024:(3 * j + 1) * 1024]
                sc = mrow[:, (3 * j + 1) * 1024:(3 * j + 2) * 1024]
                gt = mrow[:, (3 * j + 2) * 1024:(3 * j + 3) * 1024]
                s.op("dve", lambda e: e.scalar_tensor_tensor(out=orow[:, 3 * j, :], in0=sc, scalar=1.0, in1=grow[:, j, :],
                                                             op0=ALU.add, op1=ALU.mult),
                     reads=[mrowb, growb], writes=[orowb])
                s.op("dve", lambda e: e.tensor_copy(out=orow[:, 3 * j + 1, :], in_=sh), reads=[mrowb], writes=[orowb])
                s.op("dve", lambda e: e.tensor_copy(out=orow[:, 3 * j + 2, :], in_=gt), reads=[mrowb], writes=[orowb])
            s.dma("sp", lambda e: e.dma_start(out=modv[l], in_=orow[:]), reads=[orowb], writes=[k.modv_buf])
        s.barrier()


def load_cast(k, stg, dst, dstb, src, n, qi=0):
    s = k.s
    st, stb = stg[qi % len(stg)]
    s.dma("sp" if qi % 2 == 0 else "pool", lambda e: e.dma_start(out=st[:, 0:n], in_=src), writes=[stb])
    ek = ("act", "pool", "dve")[qi % 3]
    if ek == "act":
        s.op("act", lambda e: e.copy(out=dst, in_=st[:, 0:n]), reads=[stb], writes=[dstb])
    else:
        s.op(ek, lambda e: e.tensor_copy(out=dst, in_=st[:, 0:n]), reads=[stb], writes=[dstb])


def load_w_bf16(k, stg, wt, wb, wdram, kc, n, q0=0):
    qi = q0
    v = wdram.rearrange("(kc p) n -> p kc n", p=128)
    cw = stg[0][0].shape[1]
    for c in range(kc):
        for c0 in range(0, n, cw):
            c1 = min(n, c0 + cw)
            load_cast(k, stg, wt[:, c, c0:c1], wb, v[:, c, c0:c1], c1 - c0, qi)
            qi += 1
    return qi


def rstd_from_ss(k, ss, ssb, rs, rsb, inv_n, w):
    s = k.s
    s.op("act", lambda e: e.activation(out=rs[:, 0:w], in_=ss[:, 0:w], func=AF.Sqrt, scale=inv_n, bias=k.eps[:, 0:1]),
         reads=[ssb], writes=[rsb])
    s.op("dve", lambda e: e.reciprocal(out=rs[:, 0:w], in_=rs[:, 0:w]), reads=[rsb], writes=[rsb])


def norm_mod(k, xt, xb, A, B, ABb, scr, scrb, st, stb, out, outb, out_f32=None):
    s = k.s
    s.op("act", lambda e: e.activation(out=scr[:], in_=xt[:], func=AF.Square, accum_out=st[:, 0:1]),
         reads=[xb], writes=[scrb, stb])
    rstd_from_ss(k, st, stb, st, stb, 1.0 / D, 1)
    s.op("dve", lambda e: e.scalar_tensor_tensor(out=scr[:], in0=xt[:], scalar=st[:, 0:1], in1=A, op0=ALU.mult, op1=ALU.mult),
         reads=[xb, stb, ABb], writes=[scrb])
    if out_f32 is not None:
        of, ofb = out_f32
        s.op("pool", lambda e: e.tensor_tensor(out=of[:], in0=scr[:], in1=B, op=ALU.add), reads=[scrb, ABb], writes=[ofb])
        s.op("act", lambda e: e.copy(out=out[:], in_=of[:]), reads=[ofb], writes=[outb])
    else:
        s.op("pool", lambda e: e.tensor_tensor(out=out[:], in0=scr[:], in1=B, op=ALU.add), reads=[scrb, ABb], writes=[outb])


def transpose_chunks(k, bank, bankb, src_fn, nchunks, rows, dst, dstb, srcb, dst_view=None):
    s = k.s
    bv = bank[:].bitcast(BF16)
    for c in range(nchunks):
        s.op("pe", lambda e: e.transpose(out=bv[0:rows, c * 128:(c + 1) * 128], in_=src_fn(c), identity=k.ident[:]),
             reads=[srcb, k.identb], writes=[bankb])
    src = bv[0:rows, 0:nchunks * 128]
    if dst_view is not None:
        src = src.rearrange("p (c t) -> p c t", t=128)
    s.op("act", lambda e: e.copy(out=dst, in_=src), reads=[bankb], writes=[dstb])


def setup_consts(k, es, ident_d, identf_d=None):
    s = k.s
    k.ident, k.identb = k.sb(es, "ident_sb", [128, 128], BF16)
    k.eps, k.epsb = k.sb(es, "epsc", [128, 1], F32)
    s.dma("sp", lambda e: e.dma_start(out=k.ident[:], in_=ident_d), writes=[k.identb])
    if identf_d is not None:
        k.identf, _ = k.sb(es, "identf_sb", [128, 128], F32)
        s.dma("sp", lambda e: e.dma_start(out=k.identf[:], in_=identf_d), writes=[k.identb])
    s.op("dve", lambda e: e.memset(k.eps[:], EPS), writes=[k.epsb])


def load_bcast(k, tile, tb, row, n):
    k.s.dma("sp", lambda e: e.dma_start(out=tile, in_=row.to_broadcast([128, n])), writes=[tb])


def na_blocks(qt):
    if 2 <= qt <= 13:
        return [(qt - 2 + j, j) for j in range(5)]
    if qt == 0:
        return [(j, 5 + j) for j in range(4)]
    if qt == 1:
        return [(j, 9 + j) for j in range(4)]
    if qt == 14:
        return [(12 + j, 13 + j) for j in range(4)]
    return [(12 + j, 17 + j) for j in range(4)]


def stage_attn(k, x, ctx, modv0, W, hout, houtb):
    nc, s = k.nc, k.s
    banks = k.banks
    with ExitStack() as es:
        AB, ABb = k.sb(es, "at_AB", [128, 4, 1024], F32)
        osb, osbb = k.sb(es, "at_o", [128, NT, 1024], BF16)
        qT, qTb = k.sb(es, "at_qT", [96, 8, SEQ], BF16)
        kT, kTb = k.sb(es, "at_kT", [96, 8, SEQ + CTX], BF16)
        Vs, Vsb = k.sb(es, "at_V", [128, NT + NCT, 8, 65], BF16)
        st, stb = k.sb(es, "at_st", [128, 4], F32)
        st2, st2b = k.sb(es, "at_st2", [128, 16], F32)
        st3, st3b = k.sb(es, "at_st3", [128, 16], F32)
        xts = [k.sb(es, f"at_x{i}", [128, 1024], F32) for i in range(2)]
        scrs = [k.sb(es, f"at_scr{i}", [128, 1024], F32) for i in range(2)]
        abfs = [k.sb(es, f"at_a{i}", [128, 1024], BF16) for i in range(2)]
        aTs = [k.sb(es, f"at_aT{i}", [128, 8, 128], BF16) for i in range(2)]

        load_bcast(k, AB[:, 0, :], ABb, modv0[0:1, 0, :], 1024)
        load_bcast(k, AB[:, 1, :], ABb, modv0[0:1, 1, :], 1024)
        load_bcast(k, AB[:, 2, :], ABb, modv0[1:2, 0, :], 1024)
        load_bcast(k, AB[:, 3, :], ABb, modv0[1:2, 1, :], 1024)
        s.op("pool", lambda e: e.memset(Vs[:, :, :, 64:65], 1.0), writes=[Vsb])

        def src_tile(t):
            return ctx[t * 128:(t + 1) * 128, :] if t < NCT else x[(t - NCT) * 128:(t - NCT + 1) * 128, :]

        def front(t):
            xt, xb = xts[t % 2]
            scr, scrb = scrs[t % 2]
            abf, abfb = abfs[t % 2]
            aT, aTb = aTs[t % 2]
            s.dma("sp", lambda e: e.dma_start(out=xt[:], in_=src_tile(t)), writes=[xb])
            j = 2 if t < NCT else 0
            norm_mod(k, xt, xb, AB[:, j, :], AB[:, j + 1, :], ABb, scr, scrb, st, stb, abf, abfb)
            bank, bb = banks[7]
            transpose_chunks(k, bank, bb, lambda c: abf[:, c * 128:(c + 1) * 128], 8, 128, aT[:], aTb, abfb, dst_view=True)
            return aT, aTb, scr, scrb

        with ExitStack() as es1:
            stg = [k.sb(es1, f"p1_stg{i}", [128, 1024], F32) for i in range(2)]
            w_in, w_inb = k.sb(es1, "p1_win", [128, 8, 672], BF16)
            w_q, w_qb = k.sb(es1, "p1_wq", [128, 3, 768], BF16)
            w_kv, w_kvb = k.sb(es1, "p1_wkv", [128, 2, 1024], BF16)
            gcn, gcnb = k.sb(es1, "p1_gcn", [128, 640], F32)
            gq, gqb = k.sb(es1, "p1_gq", [128, 96], F32)
            gk, gkb = k.sb(es1, "p1_gk", [128, 96], F32)
            zsb, zsbb = k.sb(es1, "p1_z", [128, 672], F32)
            cn, cnb = k.sb(es1, "p1_cn", [128, 640], BF16)
            cnT, cnTb = k.sb(es1, "p1_cnT", [128, 5, 128], BF16)
            qn, qnb = k.sb(es1, "p1_qn", [128, 8, 96], F32)
            kn, knb = k.sb(es1, "p1_kn", [128, 8, 64], F32)
            qr, qrb = k.sb(es1, "p1_qr", [128, 8, 32], F32)
            rt, rtb = k.sb(es1, "p1_rt", [128, 4, 8, 16], F32)
            krg, krgb = k.sb(es1, "p1_krg", [128, 32], F32)
            krr, krrb = k.sb(es1, "p1_krr", [128, 32], F32)
            kt4, kt4b = k.sb(es1, "p1_kt4", [128, 4, 16], F32)
            qf, qfb = k.sb(es1, "p1_qf", [128, 8, 96], BF16)
            kf, kfb = k.sb(es1, "p1_kf", [128, 8, 96], BF16)
            ropes = [k.sb(es1, f"p1_rope{i}", [128, 32], F32) for i in range(2)]

            qi = 0
            wv = W["attn_w_in"].rearrange("(kc p) n -> p kc n", p=128)
            for c in range(8):
                load_cast(k, stg, w_in[:, c, :], w_inb, wv[:, c, 0:672], 672, qi); qi += 1
            wv = W["mla_w_q_up"].rearrange("(kc p) n -> p kc n", p=128)
            for c in range(3):
                load_cast(k, stg, w_q[:, c, :], w_qb, wv[:, c, :], 768, qi); qi += 1
            wv = W["mla_w_kv_up"].rearrange("(kc p) n -> p kc n", p=128)
            for c in range(2):
                load_cast(k, stg, w_kv[:, c, :], w_kvb, wv[:, c, :], 1024, qi); qi += 1
            load_bcast(k, gcn[:, 0:384], gcnb, W["mla_g_qa"], 384)
            load_bcast(k, gcn[:, 384:640], gcnb, W["mla_g_kva"], 256)
            load_bcast(k, gq[:], gqb, W["mla_g_q"], 96)
            load_bcast(k, gk[:], gkb, W["mla_g_k"], 96)
            s.op("dve", lambda e: e.tensor_scalar_mul(out=gq[:], in0=gq[:], scalar1=96.0 ** -0.5), reads=[gqb], writes=[gqb])

            for t in range(NT + NCT):
                lat = t >= NCT
                aT, aTb, scr, scrb = front(t)
                if lat:
                    rp, rpb = ropes[t % 2]
                    s.dma("sp", lambda e: e.dma_start(out=rp[:], in_=W["rope"][(t - NCT) * 128:(t - NCT + 1) * 128, :]), writes=[rpb])
                (z0, z0b), (z1, z1b) = banks[0], banks[1]
                for (zb, zbb, c0, c1) in ((z0, z0b, 0, 512), (z1, z1b, 512, 672)):
                    for c in range(8):
                        s.op("pe", lambda e: e.matmul(zb[:, 0:c1 - c0], lhsT=aT[:, c, :], rhs=w_in[:, c, c0:c1], start=(c == 0), stop=(c == 7)),
                             reads=[aTb, w_inb], writes=[zbb])
                    s.op("act", lambda e: e.copy(out=zsb[:, c0:c1], in_=zb[:, 0:c1 - c0]), reads=[zbb], writes=[zsbb])
                if lat:
                    s.op("act", lambda e: e.activation(out=scr[:, 0:384], in_=zsb[:, 0:384], func=AF.Square, accum_out=st2[:, 0:1]),
                         reads=[zsbb], writes=[scrb, st2b])
                    rstd_from_ss(k, st2, st2b, st3, st3b, 1.0 / 384, 1)
                    s.op("dve", lambda e: e.scalar_tensor_tensor(out=cn[:, 0:384], in0=zsb[:, 0:384], scalar=st3[:, 0:1], in1=gcn[:, 0:384],
                                                                 op0=ALU.mult, op1=ALU.mult), reads=[zsbb, st3b, gcnb], writes=[cnb])
                s.op("act", lambda e: e.activation(out=scr[:, 384:640], in_=zsb[:, 384:640], func=AF.Square, accum_out=st2[:, 1:2]),
                     reads=[zsbb], writes=[scrb, st2b])
                s.op("act", lambda e: e.activation(out=st3[:, 1:2], in_=st2[:, 1:2], func=AF.Sqrt, scale=1.0 / 256, bias=k.eps[:, 0:1]),
                     reads=[st2b], writes=[st3b])
                s.op("dve", lambda e: e.reciprocal(out=st3[:, 1:2], in_=st3[:, 1:2]), reads=[st3b], writes=[st3b])
                s.op("dve", lambda e: e.scalar_tensor_tensor(out=cn[:, 384:640], in0=zsb[:, 384:640], scalar=st3[:, 1:2], in1=gcn[:, 384:640],
                                                             op0=ALU.mult, op1=ALU.mult), reads=[zsbb, st3b, gcnb], writes=[cnb])
                s.op("act", lambda e: e.activation(out=scr[:, 640:672], in_=zsb[:, 640:672], func=AF.Square, accum_out=st2[:, 2:3]),
                     reads=[zsbb], writes=[scrb, st2b])
                c_lo = 0 if lat else 3
                bank, bb = banks[7]
                bv = bank[:].bitcast(BF16)
                for c in range(c_lo, 5):
                    s.op("pe", lambda e: e.transpose(out=bv[:, c * 128:(c + 1) * 128], in_=cn[:, c * 128:(c + 1) * 128], identity=k.ident[:]),
                         reads=[cnb, k.identb], writes=[bb])
                s.op("act", lambda e: e.copy(out=cnT[:, c_lo:5, :], in_=bv[:, c_lo * 128:640].rearrange("p (c t) -> p c t", t=128)),
                     reads=[bb], writes=[cnTb])
                if lat:
                    (qa, qab), (qb_, qbb) = banks[2], banks[3]
                    for (qk, qkb, h0, h1) in ((qa, qab, 0, 5), (qb_, qbb, 5, 8)):
                        n = (h1 - h0) * 96
                        for c in range(3):
                            s.op("pe", lambda e: e.matmul(qk[:, 0:n], lhsT=cnT[:, c, :], rhs=w_q[:, c, h0 * 96:h1 * 96], start=(c == 0), stop=(c == 2)),
                                 reads=[cnTb, w_qb], writes=[qkb])
                        s.op("act", lambda e: e.activation(out=scr[:, h0 * 96:h1 * 96], in_=qk[:, 0:n], func=AF.Square), reads=[qkb], writes=[scrb])
                    s.op("dve", lambda e: e.tensor_reduce(out=st2[:, 4:12], in_=scr[:, 0:768].rearrange("p (h d) -> p h d", d=96), axis=AX.X, op=ALU.add),
                         reads=[scrb], writes=[st2b])
                    s.op("act", lambda e: e.activation(out=st3[:, 4:12], in_=st2[:, 4:12], func=AF.Sqrt, scale=1.0 / 96, bias=k.eps[:, 0:1]),
                         reads=[st2b], writes=[st3b])
                    s.op("dve", lambda e: e.reciprocal(out=st3[:, 4:12], in_=st3[:, 4:12]), reads=[st3b], writes=[st3b])
                    for (qk, qkb, h0, h1) in ((qa, qab, 0, 5), (qb_, qbb, 5, 8)):
                        n = (h1 - h0) * 96
                        s.op("dve", lambda e: e.tensor_tensor(out=qn[:, h0:h1, :], in0=qk[:, 0:n].rearrange("p (h d) -> p h d", d=96),
                                                              in1=st3[:, 4 + h0:4 + h1].unsqueeze(2).to_broadcast([128, h1 - h0, 96]), op=ALU.mult),
                             reads=[qkb, st3b], writes=[qnb])
                    s.op("pool", lambda e: e.tensor_tensor(out=qf[:, :, 0:64], in0=qn[:, :, 0:64],
                                                           in1=gq[:, 0:64].unsqueeze(1).to_broadcast([128, 8, 64]), op=ALU.mult),
                         reads=[qnb, gqb], writes=[qfb])
                    s.op("dve", lambda e: e.tensor_tensor(out=qr[:], in0=qn[:, :, 64:96],
                                                          in1=gq[:, 64:96].unsqueeze(1).to_broadcast([128, 8, 32]), op=ALU.mult),
                         reads=[qnb, gqb], writes=[qrb])
                    cosb = rp[:, 0:16].unsqueeze(1).to_broadcast([128, 8, 16])
                    sinb = rp[:, 16:32].unsqueeze(1).to_broadcast([128, 8, 16])
                    s.op("dve", lambda e: e.tensor_tensor(out=rt[:, 0], in0=qr[:, :, 0:16], in1=cosb, op=ALU.mult), reads=[qrb, rpb], writes=[rtb])
                    s.op("dve", lambda e: e.tensor_tensor(out=rt[:, 1], in0=qr[:, :, 16:32], in1=sinb, op=ALU.mult), reads=[qrb, rpb], writes=[rtb])
                    s.op("dve", lambda e: e.tensor_tensor(out=rt[:, 2], in0=qr[:, :, 0:16], in1=sinb, op=ALU.mult), reads=[qrb, rpb], writes=[rtb])
                    s.op("dve", lambda e: e.tensor_tensor(out=rt[:, 3], in0=qr[:, :, 16:32], in1=cosb, op=ALU.mult), reads=[qrb, rpb], writes=[rtb])
                    s.op("dve", lambda e: e.tensor_tensor(out=qf[:, :, 64:80], in0=rt[:, 0], in1=rt[:, 1], op=ALU.subtract), reads=[rtb], writes=[qfb])
                    s.op("dve", lambda e: e.tensor_tensor(out=qf[:, :, 80:96], in0=rt[:, 2], in1=rt[:, 3], op=ALU.add), reads=[rtb], writes=[qfb])
                (ka, kab), (kb_, kbb) = banks[4], banks[5]
                for g, (kk, kkb) in enumerate(((ka, kab), (kb_, kbb))):
                    for c in range(2):
                        s.op("pe", lambda e: e.matmul(kk[:, :], lhsT=cnT[:, 3 + c, :], rhs=w_kv[:, c, g * 512:(g + 1) * 512], start=(c == 0), stop=(c == 1)),
                             reads=[cnTb, w_kvb], writes=[kkb])
                    kv3 = kk[:, :].rearrange("p (h d) -> p h d", d=128)
                    s.op("act", lambda e: e.activation(out=scr[:, g * 256:(g + 1) * 256].rearrange("p (h d) -> p h d", d=64), in_=kv3[:, :, 0:64], func=AF.Square),
                         reads=[kkb], writes=[scrb])
                    s.op("act", lambda e: e.copy(out=Vs[:, t, g * 4:(g + 1) * 4, 0:64], in_=kv3[:, :, 64:128]), reads=[kkb], writes=[Vsb])
                s.op("dve", lambda e: e.tensor_reduce(out=st2[:, 4:12], in_=scr[:, 0:512].rearrange("p (h d) -> p h d", d=64), axis=AX.X, op=ALU.add),
                     reads=[scrb], writes=[st2b])
                s.op("dve", lambda e: e.tensor_scalar(out=st2[:, 4:12], in0=st2[:, 4:12], scalar1=st2[:, 2:3], scalar2=None, op0=ALU.add),
                     reads=[st2b], writes=[st2b])
                s.op("act", lambda e: e.activation(out=st3[:, 4:12], in_=st2[:, 4:12], func=AF.Sqrt, scale=1.0 / 96, bias=k.eps[:, 0:1]),
                     reads=[st2b], writes=[st3b])
                s.op("dve", lambda e: e.reciprocal(out=st3[:, 4:12], in_=st3[:, 4:12]), reads=[st3b], writes=[st3b])
                for g, (kk, kkb) in enumerate(((ka, kab), (kb_, kbb))):
                    kv3 = kk[:, :].rearrange("p (h d) -> p h d", d=128)
                    s.op("dve", lambda e: e.tensor_tensor(out=kn[:, g * 4:(g + 1) * 4, :], in0=kv3[:, :, 0:64],
                                                          in1=st3[:, 4 + g * 4:8 + g * 4].unsqueeze(2).to_broadcast([128, 4, 64]), op=ALU.mult),
                         reads=[kkb, st3b], writes=[knb])
                s.op("pool", lambda e: e.tensor_tensor(out=kf[:, :, 0:64], in0=kn[:], in1=gk[:, 0:64].unsqueeze(1).to_broadcast([128, 8, 64]), op=ALU.mult),
                     reads=[knb, gkb], writes=[kfb])
                s.op("dve", lambda e: e.tensor_tensor(out=krg[:], in0=zsb[:, 640:672], in1=gk[:, 64:96], op=ALU.mult), reads=[zsbb, gkb], writes=[krgb])
                if lat:
                    s.op("dve", lambda e: e.tensor_tensor(out=kt4[:, 0], in0=krg[:, 0:16], in1=rp[:, 0:16], op=ALU.mult), reads=[krgb, rpb], writes=[kt4b])
                    s.op("dve", lambda e: e.tensor_tensor(out=kt4[:, 1], in0=krg[:, 16:32], in1=rp[:, 16:32], op=ALU.mult), reads=[krgb, rpb], writes=[kt4b])
                    s.op("dve", lambda e: e.tensor_tensor(out=kt4[:, 2], in0=krg[:, 0:16], in1=rp[:, 16:32], op=ALU.mult), reads=[krgb, rpb], writes=[kt4b])
                    s.op("dve", lambda e: e.tensor_tensor(out=kt4[:, 3], in0=krg[:, 16:32], in1=rp[:, 0:16], op=ALU.mult), reads=[krgb, rpb], writes=[kt4b])
                    s.op("dve", lambda e: e.tensor_tensor(out=krr[:, 0:16], in0=kt4[:, 0], in1=kt4[:, 1], op=ALU.subtract), reads=[kt4b], writes=[krrb])
                    s.op("dve", lambda e: e.tensor_tensor(out=krr[:, 16:32], in0=kt4[:, 2], in1=kt4[:, 3], op=ALU.add), reads=[kt4b], writes=[krrb])
                else:
                    s.op("dve", lambda e: e.tensor_copy(out=krr[:], in_=krg[:]), reads=[krgb], writes=[krrb])
                s.op("dve", lambda e: e.tensor_tensor(out=kf[:, :, 64:96], in0=krr[:].unsqueeze(1).to_broadcast([128, 8, 32]),
                                                      in1=st3[:, 4:12].unsqueeze(2).to_broadcast([128, 8, 32]), op=ALU.mult),
                     reads=[krrb, st3b], writes=[kfb])
                if lat:
                    tb_, tbb = banks[6]
                    tl = t - NCT
                    transpose_chunks(k, tb_, tbb, lambda h: qf[:, h, :], 8, 96, qT[:, :, tl * 128:(tl + 1) * 128], qTb, qfb, dst_view=True)
                tb_, tbb = banks[0]
                transpose_chunks(k, tb_, tbb, lambda h: kf[:, h, :], 8, 96, kT[:, :, t * 128:(t + 1) * 128], kTb, kfb, dst_view=True)
            s.barrier()

        with ExitStack() as es2:
            pTs = [k.sb(es2, f"p2_pT{i}", [128, 512], BF16) for i in range(3)]
            rc, rcb = k.sb(es2, "p2_rc", [128, 8], F32)
            it = 0
            for h in range(8):
                for g in range(4):
                    for kt in range(NT + NCT):
                        sbk, sbkb = banks[it % 2]
                        pT, pTb = pTs[it % 3]
                        it += 1
                        s.op("pe", lambda e: e.matmul(sbk[:, :], lhsT=kT[:, h, kt * 128:(kt + 1) * 128], rhs=qT[:, h, g * 512:(g + 1) * 512], start=True, stop=True),
                             reads=[kTb, qTb], writes=[sbkb])
                        s.op("act", lambda e: e.activation(out=pT[:], in_=sbk[:, :], func=AF.Exp), reads=[sbkb], writes=[pTb])
                        for qs in range(4):
                            ob, obb = banks[2 + qs]
                            s.op("pe", lambda e: e.matmul(ob[:, 0:65], lhsT=pT[:, qs * 128:(qs + 1) * 128], rhs=Vs[:, kt, h, :],
                                                          start=(kt == 0), stop=(kt == NT + NCT - 1)), reads=[pTb, Vsb], writes=[obb])
                    for qs in range(4):
                        ob, obb = banks[2 + qs]
                        j = (g * 4 + qs) % 8
                        s.op("dve", lambda e: e.reciprocal(out=rc[:, j:j + 1], in_=ob[:, 64:65]), reads=[obb], writes=[rcb])
                        s.op("dve", lambda e: e.tensor_scalar(out=osb[:, g * 4 + qs, h * 64:(h + 1) * 64], in0=ob[:, 0:64], scalar1=rc[:, j:j + 1],
                                                              scalar2=None, op0=ALU.mult), reads=[obb, rcb], writes=[osbb])
            s.barrier()

        with ExitStack() as es3:
            stg = [k.sb(es3, f"p3_stg{i}", [128, 1536], F32) for i in range(2)]
            w_in, w_inb = k.sb(es3, "p3_win", [128, 8, 1536], BF16)
            gqn, gqnb = k.sb(es3, "p3_gq", [128, 64], F32)
            gkn, gknb = k.sb(es3, "p3_gk", [128, 64], F32)
            tn, tnb = k.sb(es3, "p3_tn", [128, 16, 64], F32)
            qkf, qkfb = k.sb(es3, "p3_qkf", [128, 16, 64], BF16)
            wv = W["attn_w_in"].rearrange("(kc p) n -> p kc n", p=128)
            for c in range(8):
                load_cast(k, stg, w_in[:, c, :], w_inb, wv[:, c, 672:2208], 1536, c)
            load_bcast(k, gqn[:], gqnb, W["na_g_q"], 64)
            load_bcast(k, gkn[:], gknb, W["na_g_k"], 64)
            s.op("dve", lambda e: e.tensor_scalar_mul(out=gqn[:], in0=gqn[:], scalar1=64.0 ** -0.5), reads=[gqnb], writes=[gqnb])
            for t in range(NT + NCT):
                lat = t >= NCT
                aT, aTb, scr, scrb = front(t)
                grp = (0, 1, 2) if lat else (1, 2)
                for g in grp:
                    zb, zbb = banks[g]
                    for c in range(8):
                        s.op("pe", lambda e: e.matmul(zb[:, :], lhsT=aT[:, c, :], rhs=w_in[:, c, g * 512:(g + 1) * 512], start=(c == 0), stop=(c == 7)),
                             reads=[aTb, w_inb], writes=[zbb])
                    if g < 2:
                        s.op("act", lambda e: e.activation(out=scr[:, g * 512:(g + 1) * 512], in_=zb[:, :], func=AF.Square), reads=[zbb], writes=[scrb])
                    else:
                        s.op("act", lambda e: e.copy(out=Vs[:, t, :, 0:64], in_=zb[:, :].rearrange("p (h d) -> p h d", d=64)), reads=[zbb], writes=[Vsb])
                g0 = 0 if lat else 1
                s.op("dve", lambda e: e.tensor_reduce(out=st2[:, g0 * 8:16], in_=scr[:, g0 * 512:1024].rearrange("p (h d) -> p h d", d=64), axis=AX.X, op=ALU.add),
                     reads=[scrb], writes=[st2b])
                s.op("act", lambda e: e.activation(out=st3[:, g0 * 8:16], in_=st2[:, g0 * 8:16], func=AF.Sqrt, scale=1.0 / 64, bias=k.eps[:, 0:1]),
                     reads=[st2b], writes=[st3b])
                s.op("dve", lambda e: e.reciprocal(out=st3[:, g0 * 8:16], in_=st3[:, g0 * 8:16]), reads=[st3b], writes=[st3b])
                for g in grp[:-1]:
                    zb, zbb = banks[g]
                    gg, ggb = (gqn, gqnb) if g == 0 else (gkn, gknb)
                    s.op("dve", lambda e: e.tensor_tensor(out=tn[:, g * 8:(g + 1) * 8, :], in0=zb[:, :].rearrange("p (h d) -> p h d", d=64),
                                                          in1=st3[:, g * 8:(g + 1) * 8].unsqueeze(2).to_broadcast([128, 8, 64]), op=ALU.mult),
                         reads=[zbb, st3b], writes=[tnb])
                    s.op("pool", lambda e: e.tensor_tensor(out=qkf[:, g * 8:(g + 1) * 8, :], in0=tn[:, g * 8:(g + 1) * 8, :],
                                                           in1=gg[:].unsqueeze(1).to_broadcast([128, 8, 64]), op=ALU.mult),
                         reads=[tnb, ggb], writes=[qkfb])
                if lat:
                    tb_, tbb = banks[3]
                    tl = t - NCT
                    transpose_chunks(k, tb_, tbb, lambda h: qkf[:, h, :], 8, 64, qT[0:64, :, tl * 128:(tl + 1) * 128], qTb, qkfb, dst_view=True)
                tb_, tbb = banks[4]
                transpose_chunks(k, tb_, tbb, lambda h: qkf[:, 8 + h, :], 8, 64, kT[0:64, :, t * 128:(t + 1) * 128], kTb, qkfb, dst_view=True)
            s.barrier()

        with ExitStack() as es4:
            nbs = [k.sb(es4, f"p4_nb{i}", [128, 21, 128], F32) for i in range(2)]
            sfs = [k.sb(es4, f"p4_sf{i}", [128, 640], F32) for i in range(2)]
            pTs = [k.sb(es4, f"p4_pT{i}", [128, 896], BF16) for i in range(2)]
            rc, rcb = k.sb(es4, "p4_rc", [128, 8], F32)
            it = 0
            for h in range(8):
                nb, nbb = nbs[h % 2]
                s.dma("sp", lambda e: e.dma_start(out=nb[:], in_=W["nabias"][h]), writes=[nbb])
                for qt in range(NT):
                    blocks = na_blocks(qt)
                    nloc = len(blocks)
                    (sa, sab), (sb_, sbb) = banks[(it % 2) * 2], banks[(it % 2) * 2 + 1]
                    sf, sfb = sfs[it % 2]
                    pT, pTb = pTs[it % 2]
                    ob, obb = banks[4 + it % 2]
                    it += 1
                    qsl = qT[0:64, h, qt * 128:(qt + 1) * 128]
                    for j, (kt, bi) in enumerate(blocks):
                        dstb_, dstbb = (sa, sab) if j < 4 else (sb_, sbb)
                        col = (j % 4) * 128
                        s.op("pe", lambda e: e.matmul(dstb_[:, col:col + 128], lhsT=kT[0:64, h, (NCT + kt) * 128:(NCT + kt + 1) * 128], rhs=qsl, start=True, stop=True),
                             reads=[kTb, qTb], writes=[dstbb])
                    for c in range(NCT):
                        s.op("pe", lambda e: e.matmul(sb_[:, 128 + c * 128:256 + c * 128], lhsT=kT[0:64, h, c * 128:(c + 1) * 128], rhs=qsl, start=True, stop=True),
                             reads=[kTb, qTb], writes=[sbb])
                    b0 = blocks[0][1]
                    s.op("dve", lambda e: e.tensor_tensor(out=sf[:, 0:512], in0=sa[:, :], in1=nb[:, b0:b0 + 4, :].rearrange("p b q -> p (b q)"), op=ALU.add),
                         reads=[sab, nbb], writes=[sfb])
                    if nloc == 5:
                        s.op("dve", lambda e: e.tensor_tensor(out=sf[:, 512:640], in0=sb_[:, 0:128], in1=nb[:, b0 + 4, :], op=ALU.add),
                             reads=[sbb, nbb], writes=[sfb])
                    s.op("act", lambda e: e.activation(out=pT[:, 0:nloc * 128], in_=sf[:, 0:nloc * 128], func=AF.Exp), reads=[sfb], writes=[pTb])
                    s.op("act", lambda e: e.activation(out=pT[:, 640:896], in_=sb_[:, 128:384], func=AF.Exp), reads=[sbb], writes=[pTb])
                    nblk = nloc + NCT
                    for j, (kt, bi) in enumerate(blocks):
                        s.op("pe", lambda e: e.matmul(ob[:, 0:65], lhsT=pT[:, j * 128:(j + 1) * 128], rhs=Vs[:, NCT + kt, h, :], start=(j == 0), stop=False),
                             reads=[pTb, Vsb], writes=[obb])
                    for c in range(NCT):
                        s.op("pe", lambda e: e.matmul(ob[:, 0:65], lhsT=pT[:, 640 + c * 128:768 + c * 128], rhs=Vs[:, c, h, :], start=False, stop=(c == NCT - 1)),
                             reads=[pTb, Vsb], writes=[obb])
                    j8 = it % 8
                    s.op("dve", lambda e: e.reciprocal(out=rc[:, j8:j8 + 1], in_=ob[:, 64:65]), reads=[obb], writes=[rcb])
                    s.op("dve", lambda e: e.tensor_scalar(out=osb[:, qt, 512 + h * 64:512 + (h + 1) * 64], in0=ob[:, 0:64], scalar1=rc[:, j8:j8 + 1],
                                                          scalar2=None, op0=ALU.mult), reads=[obb, rcb], writes=[osbb])
            s.barrier()

        with ExitStack() as es5:
            stg = [k.sb(es5, f"p5_stg{i}", [128, 1024], F32) for i in range(2)]
            w_o, w_ob = k.sb(es5, "p5_wo", [128, 8, 1024], BF16)
            G1, G1b = k.sb(es5, "p5_g1", [128, 1024], F32)
            outs = [k.sb(es5, f"p5_out{i}", [128, 1024], F32) for i in range(2)]
            wv = W["attn_w_out"].rearrange("(kc p) n -> p kc n", p=128)
            for c in range(8):
                load_cast(k, stg, w_o[:, c, :], w_ob, wv[:, c, :], 1024, c)
            load_bcast(k, G1[:], G1b, modv0[0:1, 2, :], 1024)
            for t in range(NT):
                xt, xb = xts[t % 2]
                scr, scrb = scrs[t % 2]
                aT, aTb = aTs[t % 2]
                ot, otb = outs[t % 2]
                s.dma("sp", lambda e: e.dma_start(out=xt[:], in_=x[t * 128:(t + 1) * 128, :]), writes=[xb])
                bank, bb = banks[7]
                transpose_chunks(k, bank, bb, lambda c: osb[:, t, c * 128:(c + 1) * 128], 8, 128, aT[:], aTb, osbb, dst_view=True)
                for g in range(2):
                    yb, ybb = banks[g]
                    for c in range(8):
                        s.op("pe", lambda e: e.matmul(yb[:, :], lhsT=aT[:, c, :], rhs=w_o[:, c, g * 512:(g + 1) * 512], start=(c == 0), stop=(c == 7)),
                             reads=[aTb, w_ob], writes=[ybb])
                    s.op("dve", lambda e: e.tensor_tensor(out=scr[:, g * 512:(g + 1) * 512], in0=yb[:, :], in1=G1[:, g * 512:(g + 1) * 512], op=ALU.mult),
                         reads=[ybb, G1b], writes=[scrb])
                s.op("pool", lambda e: e.tensor_tensor(out=ot[:], in0=scr[:], in1=xt[:], op=ALU.add), reads=[scrb, xb], writes=[otb])
                s.dma("sp", lambda e: e.dma_start(out=hout[t * 128:(t + 1) * 128, :], in_=ot[:]), reads=[otb], writes=[houtb[t]])
            s.barrier()


def host_rope_table():
    t = np.arange(SEQ)
    row = (t // 64).astype(np.float32)
    col = (t % 64).astype(np.float32)
    inv = (np.float32(1.0) / (np.float32(10000.0) ** (np.arange(8, dtype=np.float32) / np.float32(8)))).astype(np.float32)
    ang = np.concatenate([row[:, None] * inv, col[:, None] * inv], axis=-1).astype(np.float32)
    return np.concatenate([np.cos(ang), np.sin(ang)], axis=-1).astype(np.float32)


def host_na_bias(rpb):
    pairs = [(2, j) for j in range(5)] + [(0, j) for j in range(4)] + [(1, j) for j in range(4)] \
        + [(14, 12 + j) for j in range(4)] + [(15, 12 + j) for j in range(4)]
    out = np.full((8, 128, 21, 128), NEG, np.float32)
    p = np.arange(128)
    for bi, (qt, kt) in enumerate(pairs):
        tq = qt * 128 + p
        tk = kt * 128 + p
        r, c = tq // 64, tq % 64
        kr, kc = tk // 64, tk % 64
        r0 = np.clip(r - 4, 0, 24)
        c0 = np.clip(c - 8, 0, 48)
        inside = (kr[:, None] >= r0[None, :]) & (kr[:, None] < r0[None, :] + 8) & (kc[:, None] >= c0[None, :]) & (kc[:, None] < c0[None, :] + 16)
        rr = np.clip(kr[:, None] - r[None, :] + 7, 0, 14)
        rc = np.clip(kc[:, None] - c[None, :] + 15, 0, 30)
        vals = rpb[:, rr, rc]
        out[:, :, bi, :] = np.where(inside[None], vals, np.float32(NEG))
    return out


NROW = 8


def stage_peer(k, hin, hinb, modv_l, w_query, skT_d, u_tab, v_tab, hout, houtb, tag):
    nc, s = k.nc, k.s
    banks = k.banks
    with ExitStack() as es:
        stg = [k.sb(es, f"{tag}_stg{i}", [128, 2048], F32) for i in range(2)]
        wq, wqb = k.sb(es, f"{tag}_wq", [128, 8, 2048], BF16)
        skT, skTb = k.sb(es, f"{tag}_skT", [128, 16, 128], BF16)
        ABG, ABGb = k.sb(es, f"{tag}_ABG", [128, 3, 1024], F32)
        iota_i, iota_ib = k.sb(es, f"{tag}_iotai", [128, 16], I32)
        iota, iotab = k.sb(es, f"{tag}_iota", [128, 16], F32)
        st, stb = k.sb(es, f"{tag}_st", [128, 4], F32)
        xts = [k.sb(es, f"{tag}_x{i}", [128, 1024], F32) for i in range(2)]
        scrs = [k.sb(es, f"{tag}_scr{i}", [128, 1024], F32) for i in range(2)]
        hms = [k.sb(es, f"{tag}_hm{i}", [128, 1024], F32) for i in range(2)]
        hbf, hbfb = k.sb(es, f"{tag}_hbf", [128, 1024], BF16)
        hT, hTb = k.sb(es, f"{tag}_hT", [128, 8, 128], BF16)
        qbf, qbfb = k.sb(es, f"{tag}_qbf", [128, 2048], BF16)
        qT, qTb = k.sb(es, f"{tag}_qT", [128, 16, 128], BF16)
        ssb, ssbb = k.sb(es, f"{tag}_s", [128, 16, 128], F32)
        s2, _ = k.sb(es, f"{tag}_s2", [128, 16, 128], F32)
        m16, _ = k.sb(es, f"{tag}_m16", [128, 16, 16], F32)
        i16, _ = k.sb(es, f"{tag}_i16", [128, 16, 16], U32)
        i16f, i16fb = k.sb(es, f"{tag}_i16f", [128, 16, 16], F32)
        cand, candb = k.sb(es, f"{tag}_cand", [128, 8, 256], F32)
        cand2, _ = k.sb(es, f"{tag}_cand2", [128, 8, 256], F32)
        best, _ = k.sb(es, f"{tag}_best", [128, 8, 16], F32)
        pos, _ = k.sb(es, f"{tag}_pos", [128, 8, 16], U32)
        ab_i, ab_ib = k.sb(es, f"{tag}_abi", [128, 2, 128], I32)
        ab_f, ab_fb = k.sb(es, f"{tag}_abf", [128, 2, 128], F32)
        oh, ohb = k.sb(es, f"{tag}_oh", [128, 8, 16, 16], F32)
        e01, e01b = k.sb(es, f"{tag}_e01", [128, 2, 128], F32)
        idxs = [k.sb(es, f"{tag}_idx{i}", [128, 128], I32) for i in range(2)]
        gts = [k.sb(es, f"{tag}_gate{i}", [128, 8, 16], F32) for i in range(2)]
        gsum, gsumb = k.sb(es, f"{tag}_gsum", [128, 8], F32)
        actv, _ = k.sb(es, f"{tag}_act", [128, 128], F32)
        wgt, wgtb = k.sb(es, f"{tag}_wgt", [128, 128], F32)
        junk, _ = k.sb(es, f"{tag}_junk", [128, 1024], BF16)
        rows = [k.sb(es, f"{tag}_row{i}", [128, 1024], F32) for i in range(NROW)]
        accs = [k.sb(es, f"{tag}_acc{i}", [128, 1024], F32) for i in range(4)]
        ot, otb = k.sb(es, f"{tag}_ot", [128, 1024], F32)
        hpb = [[Buf() for _ in range(16)] for _ in range(3)]
        hb = [[Buf() for _ in range(8)] for _ in range(3)]
        actb = [Buf() for _ in range(16)]

        qi = load_w_bf16(k, stg, wq, wqb, w_query, 8, 2048)
        load_cast(k, stg, skT[:].rearrange("p a b -> p (a b)"), skTb, skT_d.rearrange("p a b -> p (a b)"), 2048, qi)
        for j in range(3):
            load_bcast(k, ABG[:, j, :], ABGb, modv_l[0:1, 3 + j, :], 1024)
        s.op("pool", lambda e: e.iota(out=iota_i[:], pattern=[[1, 16]], base=0, channel_multiplier=0), writes=[iota_ib])
        s.op("dve", lambda e: e.tensor_copy(out=iota[:], in_=iota_i[:]), reads=[iota_ib], writes=[iotab])

        def front(t):
            xt, xb = xts[t % 2]
            scr, scrb = scrs[t % 2]
            hm, hmb = hms[t % 2]
            idx, idxb = idxs[t % 2]
            gate, gateb = gts[t % 2]
            s.dma("sp", lambda e: e.dma_start(out=xt[:], in_=hin[t * 128:(t + 1) * 128, :]), reads=[hinb[t]], writes=[xb])
            norm_mod(k, xt, xb, ABG[:, 0, :], ABG[:, 1, :], ABGb, scr, scrb, st, stb, hbf, hbfb, out_f32=(hm, hmb))
            bank, bb = banks[7]
            transpose_chunks(k, bank, bb, lambda c: hbf[:, c * 128:(c + 1) * 128], 8, 128, hT[:], hTb, hbfb, dst_view=True)
            for g in range(4):
                qb_, qbb = banks[g]
                for c in range(8):
                    s.op("pe", lambda e: e.matmul(qb_[:, :], lhsT=hT[:, c, :], rhs=wq[:, c, g * 512:(g + 1) * 512], start=(c == 0), stop=(c == 7)),
                         reads=[hTb, wqb], writes=[qbb])
                s.op("act", lambda e: e.copy(out=qbf[:, g * 512:(g + 1) * 512], in_=qb_[:, :]), reads=[qbb], writes=[qbfb])
            for half in range(2):
                tb_, tbb = banks[4 + half]
                transpose_chunks(k, tb_, tbb, lambda c: qbf[:, (half * 8 + c) * 128:(half * 8 + c + 1) * 128], 8, 128,
                                 qT[:, half * 8:(half + 1) * 8, :], qTb, qbfb, dst_view=True)
            for g in range(4):
                sb_, sbb = banks[g]
                for j in range(4):
                    hp = g * 4 + j
                    s.op("pe", lambda e: e.matmul(sb_[:, j * 128:(j + 1) * 128], lhsT=qT[:, hp, :], rhs=skT[:, hp, :], start=True, stop=True),
                         reads=[qTb, skTb], writes=[sbb])
                s.op("act", lambda e: e.copy(out=ssb[:, g * 4:(g + 1) * 4, :], in_=sb_[:, :].rearrange("p (a b) -> p a b", b=128)), reads=[sbb], writes=[ssbb])
            for hp in range(16):
                s.op("dve", lambda e: e.max(out=m16[:, hp, 0:8], in_=ssb[:, hp, :]), reads=[ssbb], writes=[hpb[0][hp]])
            for hp in range(16):
                s.op("dve", lambda e: e.max_index(out=i16[:, hp, 0:8], in_max=m16[:, hp, 0:8], in_values=ssb[:, hp, :]),
                     reads=[ssbb, hpb[0][hp]], writes=[hpb[1][hp]])
            for hp in range(16):
                s.op("dve", lambda e: e.match_replace(out=s2[:, hp, :], in_to_replace=m16[:, hp, 0:8], in_values=ssb[:, hp, :], imm_value=-1e30),
                     reads=[ssbb, hpb[0][hp]], writes=[hpb[2][hp]])
            for hp in range(16):
                s.op("dve", lambda e: e.max(out=m16[:, hp, 8:16], in_=s2[:, hp, :]), reads=[hpb[2][hp]], writes=[hpb[0][hp]])
            for hp in range(16):
                s.op("dve", lambda e: e.max_index(out=i16[:, hp, 8:16], in_max=m16[:, hp, 8:16], in_values=s2[:, hp, :]),
                     reads=[hpb[2][hp], hpb[0][hp]], writes=[hpb[1][hp]])
            s.op("dve", lambda e: e.tensor_copy(out=i16f[:], in_=i16[:]), reads=hpb[1], writes=[i16fb])
            m4 = m16[:].rearrange("p (h t) a -> p h t a", t=2)
            s.op("dve", lambda e: e.tensor_tensor(out=cand[:].rearrange("p h (a b) -> p h a b", b=16),
                                                  in0=m4[:, :, 0, :].unsqueeze(3).to_broadcast([128, 8, 16, 16]),
                                                  in1=m4[:, :, 1, :].unsqueeze(2).to_broadcast([128, 8, 16, 16]), op=ALU.add),
                 reads=hpb[0], writes=[candb])
            for h in range(8):
                s.op("dve", lambda e: e.max(out=best[:, h, 0:8], in_=cand[:, h, :]), reads=[candb], writes=[hb[0][h]])
            for h in range(8):
                s.op("dve", lambda e: e.max_index(out=pos[:, h, 0:8], in_max=best[:, h, 0:8], in_values=cand[:, h, :]),
                     reads=[candb, hb[0][h]], writes=[hb[1][h]])
            for h in range(8):
                s.op("dve", lambda e: e.match_replace(out=cand2[:, h, :], in_to_replace=best[:, h, 0:8], in_values=cand[:, h, :], imm_value=-1e30),
                     reads=[candb, hb[0][h]], writes=[hb[2][h]])
            for h in range(8):
                s.op("dve", lambda e: e.max(out=best[:, h, 8:16], in_=cand2[:, h, :]), reads=[hb[2][h]], writes=[hb[0][h]])
            for h in range(8):
                s.op("dve", lambda e: e.max_index(out=pos[:, h, 8:16], in_max=best[:, h, 8:16], in_values=cand2[:, h, :]),
                     reads=[hb[2][h], hb[0][h]], writes=[hb[1][h]])
            posi = pos[:].rearrange("p h k -> p (h k)").bitcast(I32)
            s.op("dve", lambda e: e.tensor_single_scalar(out=ab_i[:, 0, :], in_=posi, scalar=4, op=ALU.arith_shift_right), reads=hb[1], writes=[ab_ib])
            s.op("dve", lambda e: e.tensor_single_scalar(out=ab_i[:, 1, :], in_=posi, scalar=15, op=ALU.bitwise_and), reads=hb[1], writes=[ab_ib])
            s.op("dve", lambda e: e.tensor_copy(out=ab_f[:], in_=ab_i[:]), reads=[ab_ib], writes=[ab_fb])
            i4 = i16f[:].rearrange("p (h t) a -> p h t a", t=2)
            for p_ in range(2):
                s.op("dve", lambda e: e.tensor_tensor(out=oh[:], in0=ab_f[:, p_, :].rearrange("p (h k) -> p h k", k=16).unsqueeze(3).to_broadcast([128, 8, 16, 16]),
                                                      in1=iota[:].unsqueeze(1).unsqueeze(1).to_broadcast([128, 8, 16, 16]), op=ALU.is_equal),
                     reads=[ab_fb, iotab], writes=[ohb])
                s.op("dve", lambda e: e.tensor_tensor(out=oh[:], in0=oh[:], in1=i4[:, :, p_, :].unsqueeze(2).to_broadcast([128, 8, 16, 16]), op=ALU.mult),
                     reads=[ohb, i16fb], writes=[ohb])
                s.op("dve", lambda e: e.tensor_reduce(out=e01[:, p_, :].rearrange("p (h k) -> p h k", k=16), in_=oh[:], axis=AX.X, op=ALU.add),
                     reads=[ohb], writes=[e01b])
            s.op("dve", lambda e: e.scalar_tensor_tensor(out=e01[:, 0, :], in0=e01[:, 0, :], scalar=128.0, in1=e01[:, 1, :], op0=ALU.mult, op1=ALU.add),
                 reads=[e01b], writes=[e01b])
            s.op("dve", lambda e: e.tensor_copy(out=idx[:], in_=e01[:, 0, :]), reads=[e01b], writes=[idxb])
            s.op("dve", lambda e: e.tensor_tensor(out=gate[:], in0=best[:], in1=best[:, :, 0:1].to_broadcast([128, 8, 16]), op=ALU.subtract),
                 reads=hb[0], writes=[gateb])
            s.op("act", lambda e: e.activation(out=gate[:], in_=gate[:], func=AF.Exp), reads=[gateb], writes=[gateb])
            s.op("dve", lambda e: e.tensor_reduce(out=gsum[:], in_=gate[:], axis=AX.X, op=ALU.add), reads=[gateb], writes=[gsumb])
            s.op("dve", lambda e: e.reciprocal(out=gsum[:], in_=gsum[:]), reads=[gsumb], writes=[gsumb])
            s.op("dve", lambda e: e.tensor_tensor(out=gate[:], in0=gate[:], in1=gsum[:].unsqueeze(2).to_broadcast([128, 8, 16]), op=ALU.mult),
                 reads=[gateb, gsumb], writes=[gateb])

        ring = [0]

        def gather(tab, idx, idxb, hk):
            rw, rwb = rows[ring[0] % NROW]
            ring[0] += 1
            s.dma("pool", lambda e: e.indirect_dma_start(out=rw[:], out_offset=None, in_=tab,
                                                         in_offset=bass.IndirectOffsetOnAxis(ap=idx[:, hk:hk + 1], axis=0)),
                  reads=[idxb], writes=[rwb])
            return rw, rwb

        def back(t):
            xt, xb = xts[t % 2]
            scr, scrb = scrs[t % 2]
            hm, hmb = hms[t % 2]
            idx, idxb = idxs[t % 2]
            gate, gateb = gts[t % 2]
            for hk in range(128):
                rw, rwb = gather(u_tab, idx, idxb, hk)
                s.op("dve", lambda e: e.scalar_tensor_tensor(out=junk[:], in0=rw[:], scalar=1.0, in1=hm[:], op0=ALU.mult, op1=ALU.mult,
                                                             accum_out=actv[:, hk:hk + 1]), reads=[rwb, hmb], writes=[actb[hk % 16]])
            s.op("act", lambda e: e.activation(out=wgt[:], in_=actv[:], func=AF.Gelu), reads=actb, writes=[wgtb])
            s.op("dve", lambda e: e.tensor_tensor(out=wgt[:], in0=wgt[:], in1=gate[:].rearrange("p h k -> p (h k)"), op=ALU.mult),
                 reads=[wgtb, gateb], writes=[wgtb])
            for hk in range(128):
                rw, rwb = gather(v_tab, idx, idxb, hk)
                ac, acb = accs[hk % 4]
                if hk < 4:
                    s.op("dve", lambda e: e.tensor_scalar(out=ac[:], in0=rw[:], scalar1=wgt[:, hk:hk + 1], scalar2=None, op0=ALU.mult),
                         reads=[rwb, wgtb], writes=[acb])
                else:
                    s.op("dve", lambda e: e.scalar_tensor_tensor(out=ac[:], in0=rw[:], scalar=wgt[:, hk:hk + 1], in1=ac[:], op0=ALU.mult, op1=ALU.add),
                         reads=[rwb, wgtb, acb], writes=[acb])
            (a0, a0b), (a1, a1b), (a2, a2b), (a3, a3b) = accs
            s.op("pool", lambda e: e.tensor_tensor(out=a0[:], in0=a0[:], in1=a1[:], op=ALU.add), reads=[a0b, a1b], writes=[a0b])
            s.op("pool", lambda e: e.tensor_tensor(out=a2[:], in0=a2[:], in1=a3[:], op=ALU.add), reads=[a2b, a3b], writes=[a2b])
            s.op("pool", lambda e: e.tensor_tensor(out=a0[:], in0=a0[:], in1=a2[:], op=ALU.add), reads=[a0b, a2b], writes=[a0b])
            s.op("dve", lambda e: e.tensor_tensor(out=scr[:], in0=a0[:], in1=ABG[:, 2, :], op=ALU.mult), reads=[a0b, ABGb], writes=[scrb])
            s.op("pool", lambda e: e.tensor_tensor(out=ot[:], in0=scr[:], in1=xt[:], op=ALU.add), reads=[scrb, xb], writes=[otb])
            s.dma("sp", lambda e: e.dma_start(out=hout[t * 128:(t + 1) * 128, :], in_=ot[:]), reads=[otb], writes=[houtb[t]])

        front(0)
        for t in range(NT):
            if t + 1 < NT:
                front(t + 1)
            back(t)
        s.barrier()


def stage_conv(k, hin, hinb, modv_l, W, hout, houtb):
    nc, s = k.nc, k.s
    banks = k.banks
    PADW = SEQ + 30
    with ExitStack() as es:
        cbuf, cbufb = k.sb(es, "cv_cbuf", [128, NT, 1024], F32)
        ABG, ABGb = k.sb(es, "cv_ABG", [128, 2, 1024], F32)
        st, stb = k.sb(es, "cv_st", [128, 8], F32)
        xts = [k.sb(es, f"cv_x{i}", [128, 1024], F32) for i in range(2)]
        scrs = [k.sb(es, f"cv_scr{i}", [128, 1024], F32) for i in range(2)]
        for j in range(2):
            load_bcast(k, ABG[:, j, :], ABGb, modv_l[0:1, j, :], 1024)
        cbt = [Buf() for _ in range(NT // 4)]
        with ExitStack() as es1:
            stg = [k.sb(es1, f"cv_stg{i}", [128, 1024], F32) for i in range(2)]
            aTa, aTab = k.sb(es1, "cv_aT", [128, 8, SEQ], BF16)
            w1, w1b = k.sb(es1, "cv_w1", [128, 8, 2048], BF16)
            b1T, b1Tb = k.sb(es1, "cv_b1T", [128, 16], F32)
            wdw, wdwb = k.sb(es1, "cv_wdw", [128, 8, 31], F32)
            bdw, bdwb = k.sb(es1, "cv_bdw", [128, 8], F32)
            abfs = [k.sb(es1, f"cv_a{i}", [128, 1024], BF16) for i in range(2)]
            upads = [k.sb(es1, f"cv_up{i}", [128, PADW], F32) for i in range(2)]
            accs = [k.sb(es1, f"cv_acc{i}", [128, SEQ], F32) for i in range(2)]
            sgs = [k.sb(es1, f"cv_sg{i}", [128, 512], F32) for i in range(2)]
            load_w_bf16(k, stg, w1, w1b, W["conv_w_pw1"], 8, 2048)
            s.dma("sp", lambda e: e.dma_start(out=b1T[:], in_=W["conv_b1T"]), writes=[b1Tb])
            s.dma("sp", lambda e: e.dma_start(out=wdw[:], in_=W["conv_wdwT"]), writes=[wdwb])
            s.dma("sp", lambda e: e.dma_start(out=bdw[:], in_=W["conv_bdwT"]), writes=[bdwb])
            for up, upb in upads:
                s.op("pool", lambda e: e.memset(up[:, 0:15], 0.0), writes=[upb])
                s.op("pool", lambda e: e.memset(up[:, 15 + SEQ:PADW], 0.0), writes=[upb])
            for t in range(NT):
                xt, xb = xts[t % 2]
                scr, scrb = scrs[t % 2]
                abf, abfb = abfs[t % 2]
                s.dma("sp", lambda e: e.dma_start(out=xt[:], in_=hin[t * 128:(t + 1) * 128, :]), reads=[hinb[t]], writes=[xb])
                norm_mod(k, xt, xb, ABG[:, 0, :], ABG[:, 1, :], ABGb, scr, scrb, st, stb, abf, abfb)
                bank, bb = banks[6 + t % 2]
                transpose_chunks(k, bank, bb, lambda c: abf[:, c * 128:(c + 1) * 128], 8, 128, aTa[:, :, t * 128:(t + 1) * 128], aTab, abfb, dst_view=True)
            accbs = [[Buf() for _ in range(4)] for _ in range(2)]
            it = 0
            for m in range(8):
                up, upb = upads[m % 2]
                acc, _ = accs[m % 2]
                accb = accbs[m % 2]
                for tg in range(4):
                    (bv_, bvb), (bg_, bgb) = banks[(it % 2) * 2], banks[(it % 2) * 2 + 1]
                    sg, sgb = sgs[it % 2]
                    it += 1
                    for (bk, bkb, c0) in ((bv_, bvb, m * 128), (bg_, bgb, 1024 + m * 128)):
                        for c in range(8):
                            s.op("pe", lambda e: e.matmul(bk[:, :], lhsT=w1[:, c, c0:c0 + 128], rhs=aTa[:, c, tg * 512:(tg + 1) * 512], start=(c == 0), stop=(c == 7)),
                                 reads=[w1b, aTab], writes=[bkb])
                    s.op("act", lambda e: e.activation(out=sg[:], in_=bg_[:, :], func=AF.Sigmoid, bias=b1T[:, 8 + m:9 + m]), reads=[bgb, b1Tb], writes=[sgb])
                    s.op("dve", lambda e: e.scalar_tensor_tensor(out=up[:, 15 + tg * 512:15 + (tg + 1) * 512], in0=bv_[:, :], scalar=b1T[:, m:m + 1], in1=sg[:],
                                                                 op0=ALU.add, op1=ALU.mult), reads=[bvb, sgb, b1Tb], writes=[upb])
                for j in range(31):
                    for ch in range(4):
                        src = up[:, ch * 512 + j:ch * 512 + j + 512]
                        dst = acc[:, ch * 512:(ch + 1) * 512]
                        if j == 0:
                            s.op("dve", lambda e: e.tensor_scalar(out=dst, in0=src, scalar1=wdw[:, m, 0:1], scalar2=bdw[:, m:m + 1], op0=ALU.mult, op1=ALU.add),
                                 reads=[upb, wdwb, bdwb], writes=[accb[ch]])
                        else:
                            s.op("dve", lambda e: e.scalar_tensor_tensor(out=dst, in0=src, scalar=wdw[:, m, j:j + 1], in1=dst, op0=ALU.mult, op1=ALU.add),
                                 reads=[upb, wdwb, accb[ch]], writes=[accb[ch]])
                for g in range(NT // 4):
                    tb_, tbb = banks[4 + g % 2]
                    for j in range(4):
                        t = g * 4 + j
                        s.op("pe", lambda e: e.transpose(out=tb_[:, j * 128:(j + 1) * 128], in_=acc[:, t * 128:(t + 1) * 128], identity=k.identf[:]),
                             reads=[accb[t // 4], k.identb], writes=[tbb])
                    s.op("act", lambda e: e.copy(out=cbuf[:, g * 4:(g + 1) * 4, m * 128:(m + 1) * 128], in_=tb_[:, :].rearrange("p (a b) -> p a b", b=128)),
                         reads=[tbb], writes=[cbt[g]])
            s.barrier()
        with ExitStack() as es3:
            stg = [k.sb(es3, f"cv3_stg{i}", [128, 1024], F32) for i in range(2)]
            w2, w2b = k.sb(es3, "cv3_w2", [128, 8, 1024], BF16)
            gl, glb = k.sb(es3, "cv3_gl", [128, 4, 1024], F32)
            sbfs = [k.sb(es3, f"cv3_s{i}", [128, 1024], BF16) for i in range(2)]
            sTs = [k.sb(es3, f"cv3_sT{i}", [128, 8, 128], BF16) for i in range(2)]
            outs = [k.sb(es3, f"cv3_o{i}", [128, 1024], F32) for i in range(2)]
            wv = W["conv_w_pw2"].rearrange("(kc p) n -> p kc n", p=128)
            for c in range(8):
                load_cast(k, stg, w2[:, c, :], w2b, wv[:, c, :], 1024, c)
            load_bcast(k, gl[:, 0, :], glb, W["conv_g_ln"], 1024)
            load_bcast(k, gl[:, 1, :], glb, W["conv_b_ln"], 1024)
            load_bcast(k, gl[:, 2, :], glb, W["conv_b_pw2"], 1024)
            load_bcast(k, gl[:, 3, :], glb, modv_l[0:1, 2, :], 1024)
            for t in range(NT):
                xt, xb = xts[t % 2]
                scr, scrb = scrs[t % 2]
                sbf, sbfb = sbfs[t % 2]
                sT, sTb = sTs[t % 2]
                ot, otb = outs[t % 2]
                cb = cbt[t // 4]
                c_t = cbuf[:, t, :]
                s.dma("sp", lambda e: e.dma_start(out=xt[:], in_=hin[t * 128:(t + 1) * 128, :]), reads=[hinb[t]], writes=[xb])
                s.op("act", lambda e: e.activation(out=scr[:], in_=c_t, func=AF.Identity, accum_out=st[:, 0:1]), reads=[cb], writes=[scrb, stb])
                s.op("act", lambda e: e.activation(out=scr[:], in_=c_t, func=AF.Square, accum_out=st[:, 1:2]), reads=[cb], writes=[scrb, stb])
                s.op("dve", lambda e: e.tensor_scalar(out=st[:, 2:3], in0=st[:, 0:1], scalar1=1.0 / D, scalar2=None, op0=ALU.mult), reads=[stb], writes=[stb])
                s.op("dve", lambda e: e.scalar_tensor_tensor(out=st[:, 3:4], in0=st[:, 2:3], scalar=-1.0, in1=st[:, 2:3], op0=ALU.mult, op1=ALU.mult),
                     reads=[stb], writes=[stb])
                s.op("dve", lambda e: e.scalar_tensor_tensor(out=st[:, 4:5], in0=st[:, 1:2], scalar=1.0 / D, in1=st[:, 3:4], op0=ALU.mult, op1=ALU.add),
                     reads=[stb], writes=[stb])
                s.op("act", lambda e: e.activation(out=st[:, 5:6], in_=st[:, 4:5], func=AF.Sqrt, scale=1.0, bias=k.eps[:, 0:1]), reads=[stb], writes=[stb])
                s.op("dve", lambda e: e.reciprocal(out=st[:, 5:6], in_=st[:, 5:6]), reads=[stb], writes=[stb])
                s.op("dve", lambda e: e.tensor_scalar(out=scr[:], in0=c_t, scalar1=st[:, 2:3], scalar2=st[:, 5:6], op0=ALU.subtract, op1=ALU.mult),
                     reads=[cb, stb], writes=[scrb])
                s.op("dve", lambda e: e.tensor_tensor(out=scr[:], in0=scr[:], in1=gl[:, 0, :], op=ALU.mult), reads=[scrb, glb], writes=[scrb])
                s.op("pool", lambda e: e.tensor_tensor(out=scr[:], in0=scr[:], in1=gl[:, 1, :], op=ALU.add), reads=[scrb, glb], writes=[scrb])
                s.op("act", lambda e: e.activation(out=sbf[:], in_=scr[:], func=AF.Silu), reads=[scrb], writes=[sbfb])
                bank, bb = banks[7]
                transpose_chunks(k, bank, bb, lambda c: sbf[:, c * 128:(c + 1) * 128], 8, 128, sT[:], sTb, sbfb, dst_view=True)
                for g in range(2):
                    yb, ybb = banks[g]
                    for c in range(8):
                        s.op("pe", lambda e: e.matmul(yb[:, :], lhsT=sT[:, c, :], rhs=w2[:, c, g * 512:(g + 1) * 512], start=(c == 0), stop=(c == 7)),
                             reads=[sTb, w2b], writes=[ybb])
                    s.op("dve", lambda e: e.tensor_tensor(out=scr[:, g * 512:(g + 1) * 512], in0=yb[:, :], in1=gl[:, 2, g * 512:(g + 1) * 512], op=ALU.add),
                         reads=[ybb, glb], writes=[scrb])
                s.op("pool", lambda e: e.tensor_tensor(out=scr[:], in0=scr[:], in1=gl[:, 3, :], op=ALU.mult), reads=[scrb, glb], writes=[scrb])
                s.op("pool", lambda e: e.tensor_tensor(out=ot[:], in0=scr[:], in1=xt[:], op=ALU.add), reads=[scrb, xb], writes=[otb])
                s.dma("sp", lambda e: e.dma_start(out=hout[t * 128:(t + 1) * 128, :], in_=ot[:]), reads=[otb], writes=[houtb[t]])
            s.barrier()


IN_SPECS = {
    "x": ([SEQ, D], F32), "ctx": ([CTX, D], F32), "cc": ([128, 16], F32),
    "w_ada": ([2, D, 6 * D], F32), "b_ada": ([2, 6 * D], F32), "g_norm": ([4, D], F32),
    "ident": ([128, 128], BF16), "identf": ([128, 128], F32),
    "attn_w_in": ([D, 2208], F32), "mla_w_q_up": ([384, 768], F32), "mla_w_kv_up": ([256, 1024], F32),
    "mla_g_qa": ([1, 384], F32), "mla_g_kva": ([1, 256], F32), "mla_g_q": ([1, 96], F32), "mla_g_k": ([1, 96], F32),
    "na_g_q": ([1, 64], F32), "na_g_k": ([1, 64], F32), "attn_w_out": ([D, D], F32),
    "rope": ([SEQ, 32], F32), "nabias": ([8, 128, 21, 128], F32),
    "conv_w_pw1": ([D, 2 * D], F32), "conv_b1T": ([128, 16], F32), "conv_wdwT": ([128, 8, 31], F32), "conv_bdwT": ([128, 8], F32),
    "conv_g_ln": ([1, D], F32), "conv_b_ln": ([1, D], F32), "conv_w_pw2": ([D, D], F32), "conv_b_pw2": ([1, D], F32),
    "wq0": ([D, 2048], F32), "wq1": ([D, 2048], F32), "skT0": ([128, 16, 128], F32), "skT1": ([128, 16, 128], F32),
    "u0": ([16384, D], F32), "u1": ([16384, D], F32), "v0": ([16384, D], F32), "v1": ([16384, D], F32),
}


def build_program():
    nc = bass.Bass("TRN2", target_bir_lowering=False)
    A = {n: nc.dram_tensor(n, sh, dt, kind="ExternalInput").ap() for n, (sh, dt) in IN_SPECS.items()}
    out = nc.dram_tensor("out", [SEQ, D], F32, kind="ExternalOutput").ap()
    modv = nc.dram_tensor("modv_scr", [2, 2, 6, D], F32, kind="Internal").ap()
    hs = [nc.dram_tensor(f"h_scr{i}", [SEQ, D], F32, kind="Internal").ap() for i in range(3)]
    hb = [[Buf() for _ in range(NT)] for _ in range(4)]
    uvs = [nc.dram_tensor(f"uv_scr{l}", [16384, 2 * D], BF16, kind="Internal").ap() for l in range(2)]
    with ExitStack() as es:
        k = K(nc, es)
        k.modv_buf = Buf()
        setup_consts(k, es, A["ident"], A["identf"])
        uvb = [[], []]
        for l in range(2):
            issue_uv_cast(k, es, A[f"u{l}"], A[f"v{l}"], uvs[l], uvb[l])
        stage_ada(k, A["cc"], A["w_ada"], A["b_ada"], A["g_norm"], modv)
        stage_attn(k, A["x"], A["ctx"], modv[0], A, hs[0], hb[0])
        stage_peer2(k, hs[0], hb[0], modv[0], A["wq0"], A["skT0"], uvs[0], uvb[0], hs[1], hb[1], "pra")
        stage_conv(k, hs[1], hb[1], modv[1], A, hs[2], hb[2])
        stage_peer2(k, hs[2], hb[2], modv[1], A["wq1"], A["skT1"], uvs[1], uvb[1], out, hb[3], "prb")
        k.s.barrier()
    return nc


def kernel(**inp):
    import ml_dtypes
    f = lambda a: np.ascontiguousarray(np.asarray(a, dtype=np.float32))
    nb = inp["x"].shape[0]
    shared = {
        "w_ada": f(inp["w_ada"]), "b_ada": f(inp["b_ada"]),
        "g_norm": f(np.stack([inp["g_norm1"][0], inp["g_norm2"][0], inp["g_norm1"][1], inp["g_norm2"][1]])),
        "ident": np.eye(128).astype(ml_dtypes.bfloat16), "identf": np.eye(128, dtype=np.float32),
        "attn_w_in": f(inp["attn_w_in"][0]), "mla_w_q_up": f(inp["mla_w_q_up"][0]), "mla_w_kv_up": f(inp["mla_w_kv_up"][0]),
        "mla_g_qa": f(inp["mla_g_qa"]), "mla_g_kva": f(inp["mla_g_kva"]), "mla_g_q": f(inp["mla_g_q"]), "mla_g_k": f(inp["mla_g_k"]),
        "na_g_q": f(inp["na_g_q"]), "na_g_k": f(inp["na_g_k"]), "attn_w_out": f(inp["attn_w_out"][0]),
        "rope": host_rope_table(), "nabias": host_na_bias(np.asarray(inp["na_rpb"][0], np.float32)),
        "conv_w_pw1": f(inp["conv_w_pw1"][0]), "conv_b1T": f(np.asarray(inp["conv_b_pw1"][0]).reshape(16, 128).T),
        "conv_wdwT": f(np.asarray(inp["conv_w_dw"][0]).reshape(31, 8, 128).transpose(2, 1, 0)),
        "conv_bdwT": f(np.asarray(inp["conv_b_dw"][0]).reshape(8, 128).T),
        "conv_g_ln": f(inp["conv_g_ln"]), "conv_b_ln": f(inp["conv_b_ln"]), "conv_w_pw2": f(inp["conv_w_pw2"][0]), "conv_b_pw2": f(inp["conv_b_pw2"]),
    }
    for l in range(2):
        shared[f"wq{l}"] = f(inp["peer_w_query"][l])
        shared[f"skT{l}"] = f(np.asarray(inp["peer_sub_keys"][l]).reshape(16, 128, 128).transpose(2, 0, 1))
        shared[f"u{l}"] = f(inp["peer_u"][l])
        shared[f"v{l}"] = f(inp["peer_v"][l])
    in_maps = []
    for b in range(nb):
        cc = np.zeros((128, 16), np.float32)
        cc[:, 0::2] = np.asarray(inp["c"][b], np.float32).reshape(8, 128).T
        cc[:, 1::2] = np.asarray(inp["c_ctx"], np.float32).reshape(8, 128).T
        m = dict(shared)
        m["x"] = f(inp["x"][b])
        m["ctx"] = f(inp["ctx"][b])
        m["cc"] = cc
        in_maps.append(m)
    nc = build_program()
    res = run_bass_kernel_spmd(nc, in_maps, core_ids=list(range(nb)))
    return np.stack([np.asarray(r["out"], dtype=np.float32) for r in res.results], axis=0)


def issue_uv_cast(k, es, u_tab, v_tab, uv, uvb):
    for i in range(4):
        r0, r1 = i * 4096, (i + 1) * 4096
        b0, b1 = Buf(), Buf()
        k.s.bulk_dma("pool", lambda e: e.dma_start(out=uv[r0:r1, 0:1024], in_=u_tab[r0:r1, :]), writes=[b0], es=es)
        k.s.bulk_dma("pool", lambda e: e.dma_start(out=uv[r0:r1, 1024:2048], in_=v_tab[r0:r1, :]), writes=[b1], es=es)
        uvb.extend([b0, b1])


NROW2 = 16


def stage_peer2(k, hin, hinb, modv_l, w_query, skT_d, uv, uvb, hout, houtb, tag):
    nc, s = k.nc, k.s
    banks = k.banks
    with ExitStack() as es:
        wq, wqb = k.sb(es, f"{tag}_wq", [128, 8, 2048], BF16)
        skT, skTb = k.sb(es, f"{tag}_skT", [128, 16, 128], BF16)
        ABG, ABGb = k.sb(es, f"{tag}_ABG", [128, 3, 1024], F32)
        iota_i, iota_ib = k.sb(es, f"{tag}_iotai", [128, 16], I32)
        iota, iotab = k.sb(es, f"{tag}_iota", [128, 16], F32)
        st, stb = k.sb(es, f"{tag}_st", [128, 4], F32)
        xts = [k.sb(es, f"{tag}_x{i}", [128, 1024], F32) for i in range(2)]
        scrs = [k.sb(es, f"{tag}_scr{i}", [128, 1024], F32) for i in range(2)]
        hbfs = [k.sb(es, f"{tag}_hbf{i}", [128, 1024], BF16) for i in range(1)]
        hms = [k.sb(es, f"{tag}_hm{i}", [128, 1024], F32) for i in range(2)]
        hT, hTb = k.sb(es, f"{tag}_hT", [128, 8, 128], BF16)
        qbf, qbfb = k.sb(es, f"{tag}_qbf", [128, 2048], BF16)
        qT, qTb = k.sb(es, f"{tag}_qT", [128, 16, 128], BF16)
        ssb, ssbb = k.sb(es, f"{tag}_s", [128, 16, 128], F32)
        s2, _ = k.sb(es, f"{tag}_s2", [128, 16, 128], F32)
        m16, _ = k.sb(es, f"{tag}_m16", [128, 16, 16], F32)
        i16, _ = k.sb(es, f"{tag}_i16", [128, 16, 16], U32)
        i16f, i16fb = k.sb(es, f"{tag}_i16f", [128, 16, 16], F32)
        cand, candb = k.sb(es, f"{tag}_cand", [128, 8, 256], F32)
        cand2, _ = k.sb(es, f"{tag}_cand2", [128, 8, 256], F32)
        best, _ = k.sb(es, f"{tag}_best", [128, 8, 16], F32)
        pos, _ = k.sb(es, f"{tag}_pos", [128, 8, 16], U32)
        ab_i, ab_ib = k.sb(es, f"{tag}_abi", [128, 2, 128], I32)
        ab_f, ab_fb = k.sb(es, f"{tag}_abf", [128, 2, 128], F32)
        oh, ohb = k.sb(es, f"{tag}_oh", [128, 8, 16, 16], F32)
        e01, e01b = k.sb(es, f"{tag}_e01", [128, 2, 128], F32)
        idxs = [k.sb(es, f"{tag}_idx{i}", [128, 128], I32) for i in range(2)]
        gts = [k.sb(es, f"{tag}_gate{i}", [128, 8, 16], F32) for i in range(2)]
        gsum, gsumb = k.sb(es, f"{tag}_gsum", [128, 8], F32)
        actv, _ = k.sb(es, f"{tag}_act", [128, 128], F32)
        wgt, _ = k.sb(es, f"{tag}_wgt", [128, 128], F32)
        junk, _ = k.sb(es, f"{tag}_junk", [128, 1024], BF16)
        rows = [k.sb(es, f"{tag}_row{i}", [128, 2048], BF16) for i in range(NROW2)]
        dgs = [k.sb(es, f"{tag}_dg{i}", [128, 128], BF16) for i in range(4)]
        ot, otb = k.sb(es, f"{tag}_ot", [128, 1024], F32)
        hpb = [[Buf() for _ in range(16)] for _ in range(3)]
        hb = [[Buf() for _ in range(8)] for _ in range(3)]
        actb = [Buf() for _ in range(16)]
        wgb = [Buf() for _ in range(4)]

        wqv = w_query.rearrange("(kc p) n -> p kc n", p=128)
        for c in range(8):
            s.dma("pool", lambda e: e.dma_start(out=wq[:, c, :], in_=wqv[:, c, :]), writes=[wqb])
        s.dma("pool", lambda e: e.dma_start(out=skT[:].rearrange("p a b -> p (a b)"), in_=skT_d.rearrange("p a b -> p (a b)")), writes=[skTb])
        for j in range(3):
            load_bcast(k, ABG[:, j, :], ABGb, modv_l[0:1, 3 + j, :], 1024)
        s.op("pool", lambda e: e.iota(out=iota_i[:], pattern=[[1, 16]], base=0, channel_multiplier=0), writes=[iota_ib])
        s.op("dve", lambda e: e.tensor_copy(out=iota[:], in_=iota_i[:]), reads=[iota_ib], writes=[iotab])

        def front(t):
            xt, xb = xts[t % 2]
            scr, scrb = scrs[t % 2]
            idx, idxb = idxs[t % 2]
            gate, gateb = gts[t % 2]
            hbf, hbfb = hbfs[0]
            hm, hmb = hms[t % 2]
            s.dma("sp", lambda e: e.dma_start(out=xt[:], in_=hin[t * 128:(t + 1) * 128, :]), reads=[hinb[t]], writes=[xb])
            norm_mod(k, xt, xb, ABG[:, 0, :], ABG[:, 1, :], ABGb, scr, scrb, st, stb, hbf, hbfb, out_f32=(hm, hmb))
            bank, bb = banks[4]
            transpose_chunks(k, bank, bb, lambda c: hbf[:, c * 128:(c + 1) * 128], 8, 128, hT[:], hTb, hbfb, dst_view=True)
            yield
            for g in range(4):
                qb_, qbb = banks[g]
                for c in range(8):
                    s.op("pe", lambda e: e.matmul(qb_[:, :], lhsT=hT[:, c, :], rhs=wq[:, c, g * 512:(g + 1) * 512], start=(c == 0), stop=(c == 7)),
                         reads=[hTb, wqb], writes=[qbb])
                s.op("act", lambda e: e.copy(out=qbf[:, g * 512:(g + 1) * 512], in_=qb_[:, :]), reads=[qbb], writes=[qbfb])
            yield
            for half in range(2):
                tb_, tbb = banks[4 + half]
                transpose_chunks(k, tb_, tbb, lambda c: qbf[:, (half * 8 + c) * 128:(half * 8 + c + 1) * 128], 8, 128,
                                 qT[:, half * 8:(half + 1) * 8, :], qTb, qbfb, dst_view=True)
            for g in range(4):
                sb_, sbb = banks[g]
                for j in range(4):
                    hp = g * 4 + j
                    s.op("pe", lambda e: e.matmul(sb_[:, j * 128:(j + 1) * 128], lhsT=qT[:, hp, :], rhs=skT[:, hp, :], start=True, stop=True),
                         reads=[qTb, skTb], writes=[sbb])
                s.op("act", lambda e: e.copy(out=ssb[:, g * 4:(g + 1) * 4, :], in_=sb_[:, :].rearrange("p (a b) -> p a b", b=128)), reads=[sbb], writes=[ssbb])
            yield
            for hp in range(16):
                s.op("dve", lambda e: e.max(out=m16[:, hp, 0:8], in_=ssb[:, hp, :]), reads=[ssbb], writes=[hpb[0][hp]])
            yield
            for hp in range(16):
                s.op("dve", lambda e: e.max_index(out=i16[:, hp, 0:8], in_max=m16[:, hp, 0:8], in_values=ssb[:, hp, :]),
                     reads=[ssbb, hpb[0][hp]], writes=[hpb[1][hp]])
            yield
            for hp in range(16):
                s.op("dve", lambda e: e.match_replace(out=s2[:, hp, :], in_to_replace=m16[:, hp, 0:8], in_values=ssb[:, hp, :], imm_value=-1e30),
                     reads=[ssbb, hpb[0][hp]], writes=[hpb[2][hp]])
            yield
            for hp in range(16):
                s.op("dve", lambda e: e.max(out=m16[:, hp, 8:16], in_=s2[:, hp, :]), reads=[hpb[2][hp]], writes=[hpb[0][hp]])
            yield
            for hp in range(16):
                s.op("dve", lambda e: e.max_index(out=i16[:, hp, 8:16], in_max=m16[:, hp, 8:16], in_values=s2[:, hp, :]),
                     reads=[hpb[2][hp], hpb[0][hp]], writes=[hpb[1][hp]])
            yield
            s.op("dve", lambda e: e.tensor_copy(out=i16f[:], in_=i16[:]), reads=hpb[1], writes=[i16fb])
            m4 = m16[:].rearrange("p (h t) a -> p h t a", t=2)
            s.op("dve", lambda e: e.tensor_tensor(out=cand[:].rearrange("p h (a b) -> p h a b", b=16),
                                                  in0=m4[:, :, 0, :].unsqueeze(3).to_broadcast([128, 8, 16, 16]),
                                                  in1=m4[:, :, 1, :].unsqueeze(2).to_broadcast([128, 8, 16, 16]), op=ALU.add),
                 reads=hpb[0], writes=[candb])
            yield
            for h in range(8):
                s.op("dve", lambda e: e.max(out=best[:, h, 0:8], in_=cand[:, h, :]), reads=[candb], writes=[hb[0][h]])
            for h in range(8):
                s.op("dve", lambda e: e.max_index(out=pos[:, h, 0:8], in_max=best[:, h, 0:8], in_values=cand[:, h, :]),
                     reads=[candb, hb[0][h]], writes=[hb[1][h]])
            yield
            for h in range(8):
                s.op("dve", lambda e: e.match_replace(out=cand2[:, h, :], in_to_replace=best[:, h, 0:8], in_values=cand[:, h, :], imm_value=-1e30),
                     reads=[candb, hb[0][h]], writes=[hb[2][h]])
            for h in range(8):
                s.op("dve", lambda e: e.max(out=best[:, h, 8:16], in_=cand2[:, h, :]), reads=[hb[2][h]], writes=[hb[0][h]])
            yield
            for h in range(8):
                s.op("dve", lambda e: e.max_index(out=pos[:, h, 8:16], in_max=best[:, h, 8:16], in_values=cand2[:, h, :]),
                     reads=[hb[2][h], hb[0][h]], writes=[hb[1][h]])
            posi = pos[:].rearrange("p h k -> p (h k)").bitcast(I32)
            s.op("dve", lambda e: e.tensor_single_scalar(out=ab_i[:, 0, :], in_=posi, scalar=4, op=ALU.arith_shift_right), reads=hb[1], writes=[ab_ib])
            s.op("dve", lambda e: e.tensor_single_scalar(out=ab_i[:, 1, :], in_=posi, scalar=15, op=ALU.bitwise_and), reads=hb[1], writes=[ab_ib])
            s.op("dve", lambda e: e.tensor_copy(out=ab_f[:], in_=ab_i[:]), reads=[ab_ib], writes=[ab_fb])
            yield
            i4 = i16f[:].rearrange("p (h t) a -> p h t a", t=2)
            for p_ in range(2):
                s.op("dve", lambda e: e.tensor_tensor(out=oh[:], in0=ab_f[:, p_, :].rearrange("p (h k) -> p h k", k=16).unsqueeze(3).to_broadcast([128, 8, 16, 16]),
                                                      in1=iota[:].unsqueeze(1).unsqueeze(1).to_broadcast([128, 8, 16, 16]), op=ALU.is_equal),
                     reads=[ab_fb, iotab], writes=[ohb])
                s.op("dve", lambda e: e.tensor_tensor(out=oh[:], in0=oh[:], in1=i4[:, :, p_, :].unsqueeze(2).to_broadcast([128, 8, 16, 16]), op=ALU.mult),
                     reads=[ohb, i16fb], writes=[ohb])
                s.op("dve", lambda e: e.tensor_reduce(out=e01[:, p_, :].rearrange("p (h k) -> p h k", k=16), in_=oh[:], axis=AX.X, op=ALU.add),
                     reads=[ohb], writes=[e01b])
                yield
            s.op("dve", lambda e: e.scalar_tensor_tensor(out=e01[:, 0, :], in0=e01[:, 0, :], scalar=128.0, in1=e01[:, 1, :], op0=ALU.mult, op1=ALU.add),
                 reads=[e01b], writes=[e01b])
            s.op("dve", lambda e: e.tensor_copy(out=idx[:], in_=e01[:, 0, :]), reads=[e01b], writes=[idxb])
            s.op("dve", lambda e: e.tensor_tensor(out=gate[:], in0=best[:], in1=best[:, :, 0:1].to_broadcast([128, 8, 16]), op=ALU.subtract),
                 reads=hb[0], writes=[gateb])
            s.op("act", lambda e: e.activation(out=gate[:], in_=gate[:], func=AF.Exp), reads=[gateb], writes=[gateb])
            s.op("dve", lambda e: e.tensor_reduce(out=gsum[:], in_=gate[:], axis=AX.X, op=ALU.add), reads=[gateb], writes=[gsumb])
            s.op("dve", lambda e: e.reciprocal(out=gsum[:], in_=gsum[:]), reads=[gsumb], writes=[gsumb])
            s.op("dve", lambda e: e.tensor_tensor(out=gate[:], in0=gate[:], in1=gsum[:].unsqueeze(2).to_broadcast([128, 8, 16]), op=ALU.mult),
                 reads=[gateb, gsumb], writes=[gateb])

        ring = [0]

        def back(t, fg):
            xt, xb = xts[t % 2]
            scr, scrb = scrs[t % 2]
            idx, idxb = idxs[t % 2]
            gate, gateb = gts[t % 2]
            hm, hmb = hms[t % 2]
            (o0, o0b), (o1, o1b) = banks[6], banks[7]
            for grp in range(16):
                held = []
                for j in range(8):
                    hk = grp * 8 + j
                    rw, rwb = rows[ring[0] % NROW2]
                    ring[0] += 1
                    s.dma("pool", lambda e: e.indirect_dma_start(out=rw[:], out_offset=None, in_=uv,
                                                                 in_offset=bass.IndirectOffsetOnAxis(ap=idx[:, hk:hk + 1], axis=0)),
                          reads=[idxb] + uvb, writes=[rwb])
                    s.op("dve", lambda e: e.scalar_tensor_tensor(out=junk[:], in0=rw[:, 0:1024], scalar=1.0, in1=hm[:], op0=ALU.mult, op1=ALU.mult,
                                                                 accum_out=actv[:, hk:hk + 1]), reads=[rwb, hmb], writes=[actb[hk % 16]])
                    held.append((hk, rw, rwb))
                g8 = slice(grp * 8, grp * 8 + 8)
                wb_ = wgb[grp % 4]
                s.op("act", lambda e: e.activation(out=wgt[:, g8], in_=actv[:, g8], func=AF.Gelu), reads=actb[(grp % 2) * 8:(grp % 2) * 8 + 8], writes=[wb_])
                s.op("dve", lambda e: e.tensor_tensor(out=wgt[:, g8], in0=wgt[:, g8], in1=gate[:].rearrange("p h k -> p (h k)")[:, g8], op=ALU.mult),
                     reads=[wb_, gateb], writes=[wb_])
                for (hk, rw, rwb) in held:
                    dg, dgb = dgs[hk % 4]
                    s.op("act", lambda e: e.activation(out=dg[:], in_=k.ident[:], func=AF.Copy, scale=wgt[:, hk:hk + 1]),
                         reads=[k.identb, wb_], writes=[dgb])
                    s.op("pe", lambda e: e.matmul(o0[:, :], lhsT=dg[:], rhs=rw[:, 1024:1536], start=(hk == 0), stop=(hk == 127)),
                         reads=[dgb, rwb], writes=[o0b])
                    s.op("pe", lambda e: e.matmul(o1[:, :], lhsT=dg[:], rhs=rw[:, 1536:2048], start=(hk == 0), stop=(hk == 127)),
                         reads=[dgb, rwb], writes=[o1b])
                if fg is not None:
                    next(fg, None)
            s.op("dve", lambda e: e.tensor_tensor(out=scr[:, 0:512], in0=o0[:, :], in1=ABG[:, 2, 0:512], op=ALU.mult), reads=[o0b, ABGb], writes=[scrb])
            s.op("dve", lambda e: e.tensor_tensor(out=scr[:, 512:1024], in0=o1[:, :], in1=ABG[:, 2, 512:1024], op=ALU.mult), reads=[o1b, ABGb], writes=[scrb])
            s.op("pool", lambda e: e.tensor_tensor(out=ot[:], in0=scr[:], in1=xt[:], op=ALU.add), reads=[scrb, xb], writes=[otb])
            s.dma("sp", lambda e: e.dma_start(out=hout[t * 128:(t + 1) * 128, :], in_=ot[:]), reads=[otb], writes=[houtb[t]])

        for _ in front(0):
            pass
        for t in range(NT):
            fg = front(t + 1) if t + 1 < NT else None
            back(t, fg)
            if fg is not None:
                for _ in fg:
                    pass
        s.barrier()
```

```python
import numpy as np
from contextlib import ExitStack
import concourse.bass as bass
import concourse.mybir as mybir
from concourse.bass_utils import run_bass_kernel_spmd

F32 = mybir.dt.float32
BF16 = mybir.dt.bfloat16
I32 = mybir.dt.int32
U32 = mybir.dt.uint32
ALU = mybir.AluOpType
AF = mybir.ActivationFunctionType
AX = mybir.AxisListType

D = 1024
SEQ = 2048
NT = SEQ // 128
CTX = 256
NCT = CTX // 128
EPS = 1e-6
NEG = -30000.0


class Buf:
    __slots__ = ("w", "r")

    def __init__(self):
        self.w = None
        self.r = {}


class Sched:
    RING = 12

    def __init__(self, nc, es):
        self.nc = nc
        self.eng = {"pe": nc.tensor, "act": nc.scalar, "dve": nc.vector, "pool": nc.gpsimd, "sp": nc.sync}
        self.semobj = {}
        self.cnt = {}
        for k in self.eng:
            self.semobj[k] = es.enter_context(nc.semaphore("s_" + k))
            self.cnt[k] = 0
        self.waited = {k: {} for k in self.eng}
        self.bulk = []
        self.dq = {}
        for q in ("sp", "pool", "act"):
            slots = []
            for i in range(self.RING):
                key = ("d", q, i)
                self.semobj[key] = es.enter_context(nc.semaphore(f"d_{q}_{i}"))
                slots.append(key)
            self.dq[q] = {"slots": slots, "uses": [0] * self.RING, "next": 0}

    def _wait(self, ek, tok):
        if tok is None:
            return
        sk, v = tok
        if self.waited[ek].get(sk, 0) >= v:
            return
        self.eng[ek].wait_ge(self.semobj[sk], v)
        self.waited[ek][sk] = v

    def _deps(self, ek, reads, writes):
        for b in reads:
            self._wait(ek, b.w)
        for b in writes:
            self._wait(ek, b.w)
            for sk, v in b.r.items():
                self._wait(ek, (sk, v))

    def _mark(self, tok, reads, writes):
        sk, v = tok
        for b in reads:
            if b.r.get(sk, 0) < v:
                b.r[sk] = v
        for b in writes:
            b.w = tok
            b.r = {}

    def op(self, ek, fn, reads=(), writes=()):
        self._deps(ek, reads, writes)
        ins = fn(self.eng[ek])
        self.cnt[ek] += 1
        ins.then_inc(self.semobj[ek], 1)
        tok = (ek, self.cnt[ek])
        self._mark(tok, reads, writes)
        return tok

    def dma(self, q, fn, reads=(), writes=()):
        dq = self.dq[q]
        slot = dq["next"]
        dq["next"] = (slot + 1) % self.RING
        key = dq["slots"][slot]
        uses = dq["uses"][slot]
        if uses:
            self._wait(q, (key, 16 * uses))
        self._deps(q, reads, writes)
        ins = fn(self.eng[q])
        ins.then_inc(self.semobj[key], 16)
        dq["uses"][slot] = uses + 1
        tok = (key, 16 * (uses + 1))
        self._mark(tok, reads, writes)
        return tok

    def bulk_dma(self, q, fn, reads=(), writes=(), es=None):
        key = ("bulk", len(self.semobj))
        self.semobj[key] = es.enter_context(self.nc.semaphore(f"bulk{len(self.semobj)}"))
        self._deps(q, reads, writes)
        ins = fn(self.eng[q])
        ins.then_inc(self.semobj[key], 16)
        tok = (key, 16)
        self.bulk.append(tok)
        self._mark(tok, reads, writes)
        return tok

    def barrier(self):
        toks = [(k, self.cnt[k]) for k in self.eng if self.cnt[k]]
        for q, dq in self.dq.items():
            for key, u in zip(dq["slots"], dq["uses"]):
                if u:
                    toks.append((key, 16 * u))
        toks.extend(self.bulk)
        for ek in self.eng:
            for t in toks:
                self._wait(ek, t)


class K:
    def __init__(self, nc, es):
        self.nc = nc
        self.es = es
        self.s = Sched(nc, es)
        self.banks = []
        for i in range(8):
            t = es.enter_context(nc.psum_tensor(f"bank{i}", [128, 512], F32))
            self.banks.append((t, Buf()))
        self.ident = None

    def sb(self, es, name, shape, dt):
        t = es.enter_context(self.nc.sbuf_tensor(name, list(shape), dt))
        return t, Buf()


def bcast_row(ap_row, parts):
    return ap_row.partition_broadcast(parts) if len(ap_row.shape) == 1 else ap_row.to_broadcast([parts, ap_row.shape[-1]])


def stage_ada(k, cc, w_ada, b_ada, g_norm, modv):
    nc, s = k.nc, k.s
    with ExitStack() as es:
        cct, ccb = k.sb(es, "ada_cc", [128, 16], F32)
        sil, silb = k.sb(es, "ada_sil", [128, 16], F32)
        wt = [k.sb(es, f"ada_w{i}", [128, 8, 512], F32) for i in range(2)]
        brow, browb = k.sb(es, "ada_b", [2, 6144], F32)
        grow, growb = k.sb(es, "ada_g", [2, 2, 1024], F32)
        mrow, mrowb = k.sb(es, "ada_m", [2, 6144], F32)
        orow, orowb = k.sb(es, "ada_o", [2, 6, 1024], F32)

        s.dma("sp", lambda e: e.dma_start(out=cct[:], in_=cc), writes=[ccb])
        s.op("act", lambda e: e.activation(out=sil[:], in_=cct[:], func=AF.Silu), reads=[ccb], writes=[silb])
        for l in range(2):
            s.dma("sp", lambda e: e.dma_start(out=brow[:], in_=b_ada[l:l + 1, :].to_broadcast([2, 6144])), writes=[browb])
            s.dma("sp", lambda e: e.dma_start(out=grow[:], in_=g_norm[2 * l:2 * l + 2, :].rearrange("(o a) d -> o a d", o=1).to_broadcast([2, 2, 1024])), writes=[growb])
            wv = w_ada[l].rearrange("(kc p) n -> p kc n", p=128)
            for g in range(12):
                wtile, wbuf = wt[g % 2]
                q = "sp" if g % 2 == 0 else "pool"
                s.dma(q, lambda e: e.dma_start(out=wtile[:], in_=wv[:, :, g * 512:(g + 1) * 512]), writes=[wbuf])
                bank, bb = k.banks[g % 2]
                for kc in range(8):
                    s.op("pe", lambda e: e.matmul(bank[0:2, :], lhsT=sil[:, 2 * kc:2 * kc + 2], rhs=wtile[:, kc, :],
                                                  start=(kc == 0), stop=(kc == 7)),
                         reads=[silb, wbuf], writes=[bb])
                s.op("dve", lambda e: e.tensor_tensor(out=mrow[:, g * 512:(g + 1) * 512], in0=bank[0:2, :],
                                                      in1=brow[:, g * 512:(g + 1) * 512], op=ALU.add),
                     reads=[bb, browb], writes=[mrowb])
            for j in range(2):
                sh = mrow[:, (3 * j) * 1024:(3 * j + 1) * 1024]
                sc = mrow[:, (3 * j + 1) * 1024:(3 * j + 2) * 1024]
                gt = mrow[:, (3 * j + 2) * 1024:(3 * j + 3) * 1024]
                s.op("dve", lambda e: e.scalar_tensor_tensor(out=orow[:, 3 * j, :], in0=sc, scalar=1.0, in1=grow[:, j, :],
                                                             op0=ALU.add, op1=ALU.mult),
                     reads=[mrowb, growb], writes=[orowb])
                s.op("dve", lambda e: e.tensor_copy(out=orow[:, 3 * j + 1, :], in_=sh), reads=[mrowb], writes=[orowb])
                s.op("dve", lambda e: e.tensor_copy(out=orow[:, 3 * j + 2, :], in_=gt), reads=[mrowb], writes=[orowb])
            s.dma("sp", lambda e: e.dma_start(out=modv[l], in_=orow[:]), reads=[orowb], writes=[k.modv_buf])
        s.barrier()


def load_cast(k, stg, dst, dstb, src, n, qi=0):
    s = k.s
    st, stb = stg[qi % len(stg)]
    s.dma("sp" if qi % 2 == 0 else "pool", lambda e: e.dma_start(out=st[:, 0:n], in_=src), writes=[stb])
    ek = ("act", "pool", "dve")[qi % 3]
    if ek == "act":
        s.op("act", lambda e: e.copy(out=dst, in_=st[:, 0:n]), reads=[stb], writes=[dstb])
    else:
        s.op(ek, lambda e: e.tensor_copy(out=dst, in_=st[:, 0:n]), reads=[stb], writes=[dstb])


def load_w_bf16(k, stg, wt, wb, wdram, kc, n, q0=0):
    qi = q0
    v = wdram.rearrange("(kc p) n -> p kc n", p=128)
    cw = stg[0][0].shape[1]
    for c in range(kc):
        for c0 in range(0, n, cw):
            c1 = min(n, c0 + cw)
            load_cast(k, stg, wt[:, c, c0:c1], wb, v[:, c, c0:c1], c1 - c0, qi)
            qi += 1
    return qi


def rstd_from_ss(k, ss, ssb, rs, rsb, inv_n, w):
    s = k.s
    s.op("act", lambda e: e.activation(out=rs[:, 0:w], in_=ss[:, 0:w], func=AF.Sqrt, scale=inv_n, bias=k.eps[:, 0:1]),
         reads=[ssb], writes=[rsb])
    s.op("dve", lambda e: e.reciprocal(out=rs[:, 0:w], in_=rs[:, 0:w]), reads=[rsb], writes=[rsb])


def norm_mod(k, xt, xb, A, B, ABb, scr, scrb, st, stb, out, outb, out_f32=None):
    s = k.s
    s.op("act", lambda e: e.activation(out=scr[:], in_=xt[:], func=AF.Square, accum_out=st[:, 0:1]),
         reads=[xb], writes=[scrb, stb])
    rstd_from_ss(k, st, stb, st, stb, 1.0 / D, 1)
    s.op("dve", lambda e: e.scalar_tensor_tensor(out=scr[:], in0=xt[:], scalar=st[:, 0:1], in1=A, op0=ALU.mult, op1=ALU.mult),
         reads=[xb, stb, ABb], writes=[scrb])
    if out_f32 is not None:
        of, ofb = out_f32
        s.op("pool", lambda e: e.tensor_tensor(out=of[:], in0=scr[:], in1=B, op=ALU.add), reads=[scrb, ABb], writes=[ofb])
        s.op("act", lambda e: e.copy(out=out[:], in_=of[:]), reads=[ofb], writes=[outb])
    else:
        s.op("pool", lambda e: e.tensor_tensor(out=out[:], in0=scr[:], in1=B, op=ALU.add), reads=[scrb, ABb], writes=[outb])


def transpose_chunks(k, bank, bankb, src_fn, nchunks, rows, dst, dstb, srcb, dst_view=None):
    s = k.s
    bv = bank[:].bitcast(BF16)
    for c in range(nchunks):
        s.op("pe", lambda e: e.transpose(out=bv[0:rows, c * 128:(c + 1) * 128], in_=src_fn(c), identity=k.ident[:]),
             reads=[srcb, k.identb], writes=[bankb])
    src = bv[0:rows, 0:nchunks * 128]
    if dst_view is not None:
        src = src.rearrange("p (c t) -> p c t", t=128)
    s.op("act", lambda e: e.copy(out=dst, in_=src), reads=[bankb], writes=[dstb])


def setup_consts(k, es, ident_d, identf_d=None):
    s = k.s
    k.ident, k.identb = k.sb(es, "ident_sb", [128, 128], BF16)
    k.eps, k.epsb = k.sb(es, "epsc", [128, 1], F32)
    s.dma("sp", lambda e: e.dma_start(out=k.ident[:], in_=ident_d), writes=[k.identb])
    if identf_d is not None:
        k.identf, _ = k.sb(es, "identf_sb", [128, 128], F32)
        s.dma("sp", lambda e: e.dma_start(out=k.identf[:], in_=identf_d), writes=[k.identb])
    s.op("dve", lambda e: e.memset(k.eps[:], EPS), writes=[k.epsb])


def load_bcast(k, tile, tb, row, n):
    k.s.dma("sp", lambda e: e.dma_start(out=tile, in_=row.to_broadcast([128, n])), writes=[tb])


def na_blocks(qt):
    if 2 <= qt <= 13:
        return [(qt - 2 + j, j) for j in range(5)]
    if qt == 0:
        return [(j, 5 + j) for j in range(4)]
    if qt == 1:
        return [(j, 9 + j) for j in range(4)]
    if qt == 14:
        return [(12 + j, 13 + j) for j in range(4)]
    return [(12 + j, 17 + j) for j in range(4)]


def stage_attn(k, x, ctx, modv0, W, hout, houtb):
    nc, s = k.nc, k.s
    banks = k.banks
    with ExitStack() as es:
        AB, ABb = k.sb(es, "at_AB", [128, 4, 1024], F32)
        osb, osbb = k.sb(es, "at_o", [128, NT, 1024], BF16)
        qT, qTb = k.sb(es, "at_qT", [96, 8, SEQ], BF16)
        kT, kTb = k.sb(es, "at_kT", [96, 8, SEQ + CTX], BF16)
        Vs, Vsb = k.sb(es, "at_V", [128, NT + NCT, 8, 65], BF16)
        st, stb = k.sb(es, "at_st", [128, 4], F32)
        st2, st2b = k.sb(es, "at_st2", [128, 16], F32)
        st3, st3b = k.sb(es, "at_st3", [128, 16], F32)
        xts = [k.sb(es, f"at_x{i}", [128, 1024], F32) for i in range(2)]
        scrs = [k.sb(es, f"at_scr{i}", [128, 1024], F32) for i in range(2)]
        abfs = [k.sb(es, f"at_a{i}", [128, 1024], BF16) for i in range(2)]
        aTs = [k.sb(es, f"at_aT{i}", [128, 8, 128], BF16) for i in range(2)]

        load_bcast(k, AB[:, 0, :], ABb, modv0[0:1, 0, :], 1024)
        load_bcast(k, AB[:, 1, :], ABb, modv0[0:1, 1, :], 1024)
        load_bcast(k, AB[:, 2, :], ABb, modv0[1:2, 0, :], 1024)
        load_bcast(k, AB[:, 3, :], ABb, modv0[1:2, 1, :], 1024)
        s.op("pool", lambda e: e.memset(Vs[:, :, :, 64:65], 1.0), writes=[Vsb])

        def src_tile(t):
            return ctx[t * 128:(t + 1) * 128, :] if t < NCT else x[(t - NCT) * 128:(t - NCT + 1) * 128, :]

        def front(t):
            xt, xb = xts[t % 2]
            scr, scrb = scrs[t % 2]
            abf, abfb = abfs[t % 2]
            aT, aTb = aTs[t % 2]
            s.dma("sp", lambda e: e.dma_start(out=xt[:], in_=src_tile(t)), writes=[xb])
            j = 2 if t < NCT else 0
            norm_mod(k, xt, xb, AB[:, j, :], AB[:, j + 1, :], ABb, scr, scrb, st, stb, abf, abfb)
            bank, bb = banks[7]
            transpose_chunks(k, bank, bb, lambda c: abf[:, c * 128:(c + 1) * 128], 8, 128, aT[:], aTb, abfb, dst_view=True)
            return aT, aTb, scr, scrb

        with ExitStack() as es1:
            stg = [k.sb(es1, f"p1_stg{i}", [128, 1024], F32) for i in range(2)]
            w_in, w_inb = k.sb(es1, "p1_win", [128, 8, 672], BF16)
            w_q, w_qb = k.sb(es1, "p1_wq", [128, 3, 768], BF16)
            w_kv, w_kvb = k.sb(es1, "p1_wkv", [128, 2, 1024], BF16)
            gcn, gcnb = k.sb(es1, "p1_gcn", [128, 640], F32)
            gq, gqb = k.sb(es1, "p1_gq", [128, 96], F32)
            gk, gkb = k.sb(es1, "p1_gk", [128, 96], F32)
            zsb, zsbb = k.sb(es1, "p1_z", [128, 672], F32)
            cn, cnb = k.sb(es1, "p1_cn", [128, 640], BF16)
            cnT, cnTb = k.sb(es1, "p1_cnT", [128, 5, 128], BF16)
            qn, qnb = k.sb(es1, "p1_qn", [128, 8, 96], F32)
            kn, knb = k.sb(es1, "p1_kn", [128, 8, 64], F32)
            qr, qrb = k.sb(es1, "p1_qr", [128, 8, 32], F32)
            rt, rtb = k.sb(es1, "p1_rt", [128, 4, 8, 16], F32)
            krg, krgb = k.sb(es1, "p1_krg", [128, 32], F32)
            krr, krrb = k.sb(es1, "p1_krr", [128, 32], F32)
            kt4, kt4b = k.sb(es1, "p1_kt4", [128, 4, 16], F32)
            qf, qfb = k.sb(es1, "p1_qf", [128, 8, 96], BF16)
            kf, kfb = k.sb(es1, "p1_kf", [128, 8, 96], BF16)
            ropes = [k.sb(es1, f"p1_rope{i}", [128, 32], F32) for i in range(2)]

            qi = 0
            wv = W["attn_w_in"].rearrange("(kc p) n -> p kc n", p=128)
            for c in range(8):
                load_cast(k, stg, w_in[:, c, :], w_inb, wv[:, c, 0:672], 672, qi); qi += 1
            wv = W["mla_w_q_up"].rearrange("(kc p) n -> p kc n", p=128)
            for c in range(3):
                load_cast(k, stg, w_q[:, c, :], w_qb, wv[:, c, :], 768, qi); qi += 1
            wv = W["mla_w_kv_up"].rearrange("(kc p) n -> p kc n", p=128)
            for c in range(2):
                load_cast(k, stg, w_kv[:, c, :], w_kvb, wv[:, c, :], 1024, qi); qi += 1
            load_bcast(k, gcn[:, 0:384], gcnb, W["mla_g_qa"], 384)
            load_bcast(k, gcn[:, 384:640], gcnb, W["mla_g_kva"], 256)
            load_bcast(k, gq[:], gqb, W["mla_g_q"], 96)
            load_bcast(k, gk[:], gkb, W["mla_g_k"], 96)
            s.op("dve", lambda e: e.tensor_scalar_mul(out=gq[:], in0=gq[:], scalar1=96.0 ** -0.5), reads=[gqb], writes=[gqb])

            for t in range(NT + NCT):
                lat = t >= NCT
                aT, aTb, scr, scrb = front(t)
                if lat:
                    rp, rpb = ropes[t % 2]
                    s.dma("sp", lambda e: e.dma_start(out=rp[:], in_=W["rope"][(t - NCT) * 128:(t - NCT + 1) * 128, :]), writes=[rpb])
                (z0, z0b), (z1, z1b) = banks[0], banks[1]
                for (zb, zbb, c0, c1) in ((z0, z0b, 0, 512), (z1, z1b, 512, 672)):
                    for c in range(8):
                        s.op("pe", lambda e: e.matmul(zb[:, 0:c1 - c0], lhsT=aT[:, c, :], rhs=w_in[:, c, c0:c1], start=(c == 0), stop=(c == 7)),
                             reads=[aTb, w_inb], writes=[zbb])
                    s.op("act", lambda e: e.copy(out=zsb[:, c0:c1], in_=zb[:, 0:c1 - c0]), reads=[zbb], writes=[zsbb])
                if lat:
                    s.op("act", lambda e: e.activation(out=scr[:, 0:384], in_=zsb[:, 0:384], func=AF.Square, accum_out=st2[:, 0:1]),
                         reads=[zsbb], writes=[scrb, st2b])
                    rstd_from_ss(k, st2, st2b, st3, st3b, 1.0 / 384, 1)
                    s.op("dve", lambda e: e.scalar_tensor_tensor(out=cn[:, 0:384], in0=zsb[:, 0:384], scalar=st3[:, 0:1], in1=gcn[:, 0:384],
                                                                 op0=ALU.mult, op1=ALU.mult), reads=[zsbb, st3b, gcnb], writes=[cnb])
                s.op("act", lambda e: e.activation(out=scr[:, 384:640], in_=zsb[:, 384:640], func=AF.Square, accum_out=st2[:, 1:2]),
                     reads=[zsbb], writes=[scrb, st2b])
                s.op("act", lambda e: e.activation(out=st3[:, 1:2], in_=st2[:, 1:2], func=AF.Sqrt, scale=1.0 / 256, bias=k.eps[:, 0:1]),
                     reads=[st2b], writes=[st3b])
                s.op("dve", lambda e: e.reciprocal(out=st3[:, 1:2], in_=st3[:, 1:2]), reads=[st3b], writes=[st3b])
                s.op("dve", lambda e: e.scalar_tensor_tensor(out=cn[:, 384:640], in0=zsb[:, 384:640], scalar=st3[:, 1:2], in1=gcn[:, 384:640],
                                                             op0=ALU.mult, op1=ALU.mult), reads=[zsbb, st3b, gcnb], writes=[cnb])
                s.op("act", lambda e: e.activation(out=scr[:, 640:672], in_=zsb[:, 640:672], func=AF.Square, accum_out=st2[:, 2:3]),
                     reads=[zsbb], writes=[scrb, st2b])
                c_lo = 0 if lat else 3
                bank, bb = banks[7]
                bv = bank[:].bitcast(BF16)
                for c in range(c_lo, 5):
                    s.op("pe", lambda e: e.transpose(out=bv[:, c * 128:(c + 1) * 128], in_=cn[:, c * 128:(c + 1) * 128], identity=k.ident[:]),
                         reads=[cnb, k.identb], writes=[bb])
                s.op("act", lambda e: e.copy(out=cnT[:, c_lo:5, :], in_=bv[:, c_lo * 128:640].rearrange("p (c t) -> p c t", t=128)),
                     reads=[bb], writes=[cnTb])
                if lat:
                    (qa, qab), (qb_, qbb) = banks[2], banks[3]
                    for (qk, qkb, h0, h1) in ((qa, qab, 0, 5), (qb_, qbb, 5, 8)):
                        n = (h1 - h0) * 96
                        for c in range(3):
                            s.op("pe", lambda e: e.matmul(qk[:, 0:n], lhsT=cnT[:, c, :], rhs=w_q[:, c, h0 * 96:h1 * 96], start=(c == 0), stop=(c == 2)),
                                 reads=[cnTb, w_qb], writes=[qkb])
                        s.op("act", lambda e: e.activation(out=scr[:, h0 * 96:h1 * 96], in_=qk[:, 0:n], func=AF.Square), reads=[qkb], writes=[scrb])
                    s.op("dve", lambda e: e.tensor_reduce(out=st2[:, 4:12], in_=scr[:, 0:768].rearrange("p (h d) -> p h d", d=96), axis=AX.X, op=ALU.add),
                         reads=[scrb], writes=[st2b])
                    s.op("act", lambda e: e.activation(out=st3[:, 4:12], in_=st2[:, 4:12], func=AF.Sqrt, scale=1.0 / 96, bias=k.eps[:, 0:1]),
                         reads=[st2b], writes=[st3b])
                    s.op("dve", lambda e: e.reciprocal(out=st3[:, 4:12], in_=st3[:, 4:12]), reads=[st3b], writes=[st3b])
                    for (qk, qkb, h0, h1) in ((qa, qab, 0, 5), (qb_, qbb, 5, 8)):
                        n = (h1 - h0) * 96
                        s.op("dve", lambda e: e.tensor_tensor(out=qn[:, h0:h1, :], in0=qk[:, 0:n].rearrange("p (h d) -> p h d", d=96),
                                                              in1=st3[:, 4 + h0:4 + h1].unsqueeze(2).to_broadcast([128, h1 - h0, 96]), op=ALU.mult),
                             reads=[qkb, st3b], writes=[qnb])
                    s.op("pool", lambda e: e.tensor_tensor(out=qf[:, :, 0:64], in0=qn[:, :, 0:64],
                                                           in1=gq[:, 0:64].unsqueeze(1).to_broadcast([128, 8, 64]), op=ALU.mult),
                         reads=[qnb, gqb], writes=[qfb])
                    s.op("dve", lambda e: e.tensor_tensor(out=qr[:], in0=qn[:, :, 64:96],
                                                          in1=gq[:, 64:96].unsqueeze(1).to_broadcast([128, 8, 32]), op=ALU.mult),
                         reads=[qnb, gqb], writes=[qrb])
                    cosb = rp[:, 0:16].unsqueeze(1).to_broadcast([128, 8, 16])
                    sinb = rp[:, 16:32].unsqueeze(1).to_broadcast([128, 8, 16])
                    s.op("dve", lambda e: e.tensor_tensor(out=rt[:, 0], in0=qr[:, :, 0:16], in1=cosb, op=ALU.mult), reads=[qrb, rpb], writes=[rtb])
                    s.op("dve", lambda e: e.tensor_tensor(out=rt[:, 1], in0=qr[:, :, 16:32], in1=sinb, op=ALU.mult), reads=[qrb, rpb], writes=[rtb])
                    s.op("dve", lambda e: e.tensor_tensor(out=rt[:, 2], in0=qr[:, :, 0:16], in1=sinb, op=ALU.mult), reads=[qrb, rpb], writes=[rtb])
                    s.op("dve", lambda e: e.tensor_tensor(out=rt[:, 3], in0=qr[:, :, 16:32], in1=cosb, op=ALU.mult), reads=[qrb, rpb], writes=[rtb])
                    s.op("dve", lambda e: e.tensor_tensor(out=qf[:, :, 64:80], in0=rt[:, 0], in1=rt[:, 1], op=ALU.subtract), reads=[rtb], writes=[qfb])
                    s.op("dve", lambda e: e.tensor_tensor(out=qf[:, :, 80:96], in0=rt[:, 2], in1=rt[:, 3], op=ALU.add), reads=[rtb], writes=[qfb])
                (ka, kab), (kb_, kbb) = banks[4], banks[5]
                for g, (kk, kkb) in enumerate(((ka, kab), (kb_, kbb))):
                    for c in range(2):
                        s.op("pe", lambda e: e.matmul(kk[:, :], lhsT=cnT[:, 3 + c, :], rhs=w_kv[:, c, g * 512:(g + 1) * 512], start=(c == 0), stop=(c == 1)),
                             reads=[cnTb, w_kvb], writes=[kkb])
                    kv3 = kk[:, :].rearrange("p (h d) -> p h d", d=128)
                    s.op("act", lambda e: e.activation(out=scr[:, g * 256:(g + 1) * 256].rearrange("p (h d) -> p h d", d=64), in_=kv3[:, :, 0:64], func=AF.Square),
                         reads=[kkb], writes=[scrb])
                    s.op("act", lambda e: e.copy(out=Vs[:, t, g * 4:(g + 1) * 4, 0:64], in_=kv3[:, :, 64:128]), reads=[kkb], writes=[Vsb])
                s.op("dve", lambda e: e.tensor_reduce(out=st2[:, 4:12], in_=scr[:, 0:512].rearrange("p (h d) -> p h d", d=64), axis=AX.X, op=ALU.add),
                     reads=[scrb], writes=[st2b])
                s.op("dve", lambda e: e.tensor_scalar(out=st2[:, 4:12], in0=st2[:, 4:12], scalar1=st2[:, 2:3], scalar2=None, op0=ALU.add),
                     reads=[st2b], writes=[st2b])
                s.op("act", lambda e: e.activation(out=st3[:, 4:12], in_=st2[:, 4:12], func=AF.Sqrt, scale=1.0 / 96, bias=k.eps[:, 0:1]),
                     reads=[st2b], writes=[st3b])
                s.op("dve", lambda e: e.reciprocal(out=st3[:, 4:12], in_=st3[:, 4:12]), reads=[st3b], writes=[st3b])
                for g, (kk, kkb) in enumerate(((ka, kab), (kb_, kbb))):
                    kv3 = kk[:, :].rearrange("p (h d) -> p h d", d=128)
                    s.op("dve", lambda e: e.tensor_tensor(out=kn[:, g * 4:(g + 1) * 4, :], in0=kv3[:, :, 0:64],
                                                          in1=st3[:, 4 + g * 4:8 + g * 4].unsqueeze(2).to_broadcast([128, 4, 64]), op=ALU.mult),
                         reads=[kkb, st3b], writes=[knb])
                s.op("pool", lambda e: e.tensor_tensor(out=kf[:, :, 0:64], in0=kn[:], in1=gk[:, 0:64].unsqueeze(1).to_broadcast([128, 8, 64]), op=ALU.mult),
                     reads=[knb, gkb], writes=[kfb])
                s.op("dve", lambda e: e.tensor_tensor(out=krg[:], in0=zsb[:, 640:672], in1=gk[:, 64:96], op=ALU.mult), reads=[zsbb, gkb], writes=[krgb])
                if lat:
                    s.op("dve", lambda e: e.tensor_tensor(out=kt4[:, 0], in0=krg[:, 0:16], in1=rp[:, 0:16], op=ALU.mult), reads=[krgb, rpb], writes=[kt4b])
                    s.op("dve", lambda e: e.tensor_tensor(out=kt4[:, 1], in0=krg[:, 16:32], in1=rp[:, 16:32], op=ALU.mult), reads=[krgb, rpb], writes=[kt4b])
                    s.op("dve", lambda e: e.tensor_tensor(out=kt4[:, 2], in0=krg[:, 0:16], in1=rp[:, 16:32], op=ALU.mult), reads=[krgb, rpb], writes=[kt4b])
                    s.op("dve", lambda e: e.tensor_tensor(out=kt4[:, 3], in0=krg[:, 16:32], in1=rp[:, 0:16], op=ALU.mult), reads=[krgb, rpb], writes=[kt4b])
                    s.op("dve", lambda e: e.tensor_tensor(out=krr[:, 0:16], in0=kt4[:, 0], in1=kt4[:, 1], op=ALU.subtract), reads=[kt4b], writes=[krrb])
                    s.op("dve", lambda e: e.tensor_tensor(out=krr[:, 16:32], in0=kt4[:, 2], in1=kt4[:, 3], op=ALU.add), reads=[kt4b], writes=[krrb])
                else:
                    s.op("dve", lambda e: e.tensor_copy(out=krr[:], in_=krg[:]), reads=[krgb], writes=[krrb])
                s.op("dve", lambda e: e.tensor_tensor(out=kf[:, :, 64:96], in0=krr[:].unsqueeze(1).to_broadcast([128, 8, 32]),
                                                      in1=st3[:, 4:12].unsqueeze(2).to_broadcast([128, 8, 32]), op=ALU.mult),
                     reads=[krrb, st3b], writes=[kfb])
                if lat:
                    tb_, tbb = banks[6]
                    tl = t - NCT
                    transpose_chunks(k, tb_, tbb, lambda h: qf[:, h, :], 8, 96, qT[:, :, tl * 128:(tl + 1) * 128], qTb, qfb, dst_view=True)
                tb_, tbb = banks[0]
                transpose_chunks(k, tb_, tbb, lambda h: kf[:, h, :], 8, 96, kT[:, :, t * 128:(t + 1) * 128], kTb, kfb, dst_view=True)
            s.barrier()

        with ExitStack() as es2:
            pTs = [k.sb(es2, f"p2_pT{i}", [128, 512], BF16) for i in range(3)]
            rc, rcb = k.sb(es2, "p2_rc", [128, 8], F32)
            steps = [(h, g, kt) for h in range(8) for g in range(4) for kt in range(NT + NCT)]

            def emit_s(i):
                h, g, kt = steps[i]
                sbk, sbkb = banks[i % 2]
                pT, pTb = pTs[i % 3]
                s.op("pe", lambda e: e.matmul(sbk[:, :], lhsT=kT[:, h, kt * 128:(kt + 1) * 128], rhs=qT[:, h, g * 512:(g + 1) * 512], start=True, stop=True),
                     reads=[kTb, qTb], writes=[sbkb])
                s.op("act", lambda e: e.activation(out=pT[:], in_=sbk[:, :], func=AF.Exp), reads=[sbkb], writes=[pTb])

            def emit_pv(i):
                h, g, kt = steps[i]
                pT, pTb = pTs[i % 3]
                for qs in range(4):
                    ob, obb = banks[2 + qs]
                    s.op("pe", lambda e: e.matmul(ob[:, 0:65], lhsT=pT[:, qs * 128:(qs + 1) * 128], rhs=Vs[:, kt, h, :],
                                                  start=(kt == 0), stop=(kt == NT + NCT - 1)), reads=[pTb, Vsb], writes=[obb])
                if kt == NT + NCT - 1:
                    for qs in range(4):
                        ob, obb = banks[2 + qs]
                        j = (g * 4 + qs) % 8
                        s.op("dve", lambda e: e.reciprocal(out=rc[:, j:j + 1], in_=ob[:, 64:65]), reads=[obb], writes=[rcb])
                        s.op("dve", lambda e: e.tensor_scalar(out=osb[:, g * 4 + qs, h * 64:(h + 1) * 64], in0=ob[:, 0:64], scalar1=rc[:, j:j + 1],
                                                              scalar2=None, op0=ALU.mult), reads=[obb, rcb], writes=[osbb])

            emit_s(0)
            for i in range(len(steps)):
                if i + 1 < len(steps):
                    emit_s(i + 1)
                emit_pv(i)
            s.barrier()

        with ExitStack() as es3:
            stg = [k.sb(es3, f"p3_stg{i}", [128, 1536], F32) for i in range(2)]
            w_in, w_inb = k.sb(es3, "p3_win", [128, 8, 1536], BF16)
            gqn, gqnb = k.sb(es3, "p3_gq", [128, 64], F32)
            gkn, gknb = k.sb(es3, "p3_gk", [128, 64], F32)
            tn, tnb = k.sb(es3, "p3_tn", [128, 16, 64], F32)
            qkf, qkfb = k.sb(es3, "p3_qkf", [128, 16, 64], BF16)
            wv = W["attn_w_in"].rearrange("(kc p) n -> p kc n", p=128)
            for c in range(8):
                load_cast(k, stg, w_in[:, c, :], w_inb, wv[:, c, 672:2208], 1536, c)
            load_bcast(k, gqn[:], gqnb, W["na_g_q"], 64)
            load_bcast(k, gkn[:], gknb, W["na_g_k"], 64)
            s.op("dve", lambda e: e.tensor_scalar_mul(out=gqn[:], in0=gqn[:], scalar1=64.0 ** -0.5), reads=[gqnb], writes=[gqnb])
            for t in range(NT + NCT):
                lat = t >= NCT
                aT, aTb, scr, scrb = front(t)
                grp = (0, 1, 2) if lat else (1, 2)
                for g in grp:
                    zb, zbb = banks[g]
                    for c in range(8):
                        s.op("pe", lambda e: e.matmul(zb[:, :], lhsT=aT[:, c, :], rhs=w_in[:, c, g * 512:(g + 1) * 512], start=(c == 0), stop=(c == 7)),
                             reads=[aTb, w_inb], writes=[zbb])
                    if g < 2:
                        s.op("act", lambda e: e.activation(out=scr[:, g * 512:(g + 1) * 512], in_=zb[:, :], func=AF.Square), reads=[zbb], writes=[scrb])
                    else:
                        s.op("act", lambda e: e.copy(out=Vs[:, t, :, 0:64], in_=zb[:, :].rearrange("p (h d) -> p h d", d=64)), reads=[zbb], writes=[Vsb])
                g0 = 0 if lat else 1
                s.op("dve", lambda e: e.tensor_reduce(out=st2[:, g0 * 8:16], in_=scr[:, g0 * 512:1024].rearrange("p (h d) -> p h d", d=64), axis=AX.X, op=ALU.add),
                     reads=[scrb], writes=[st2b])
                s.op("act", lambda e: e.activation(out=st3[:, g0 * 8:16], in_=st2[:, g0 * 8:16], func=AF.Sqrt, scale=1.0 / 64, bias=k.eps[:, 0:1]),
                     reads=[st2b], writes=[st3b])
                s.op("dve", lambda e: e.reciprocal(out=st3[:, g0 * 8:16], in_=st3[:, g0 * 8:16]), reads=[st3b], writes=[st3b])
                for g in grp[:-1]:
                    zb, zbb = banks[g]
                    gg, ggb = (gqn, gqnb) if g == 0 else (gkn, gknb)
                    s.op("dve", lambda e: e.tensor_tensor(out=tn[:, g * 8:(g + 1) * 8, :], in0=zb[:, :].rearrange("p (h d) -> p h d", d=64),
                                                          in1=st3[:, g * 8:(g + 1) * 8].unsqueeze(2).to_broadcast([128, 8, 64]), op=ALU.mult),
                         reads=[zbb, st3b], writes=[tnb])
                    s.op("pool", lambda e: e.tensor_tensor(out=qkf[:, g * 8:(g + 1) * 8, :], in0=tn[:, g * 8:(g + 1) * 8, :],
                                                           in1=gg[:].unsqueeze(1).to_broadcast([128, 8, 64]), op=ALU.mult),
                         reads=[tnb, ggb], writes=[qkfb])
                if lat:
                    tb_, tbb = banks[3]
                    tl = t - NCT
                    transpose_chunks(k, tb_, tbb, lambda h: qkf[:, h, :], 8, 64, qT[0:64, :, tl * 128:(tl + 1) * 128], qTb, qkfb, dst_view=True)
                tb_, tbb = banks[4]
                transpose_chunks(k, tb_, tbb, lambda h: qkf[:, 8 + h, :], 8, 64, kT[0:64, :, t * 128:(t + 1) * 128], kTb, qkfb, dst_view=True)
            s.barrier()

        with ExitStack() as es4:
            nbs = [k.sb(es4, f"p4_nb{i}", [128, 21, 128], F32) for i in range(2)]
            sfs = [k.sb(es4, f"p4_sf{i}", [128, 640], F32) for i in range(2)]
            pTs = [k.sb(es4, f"p4_pT{i}", [128, 896], BF16) for i in range(2)]
            rc, rcb = k.sb(es4, "p4_rc", [128, 8], F32)
            steps = [(h, qt) for h in range(8) for qt in range(NT)]

            def res(i):
                return (banks[(i % 2) * 2], banks[(i % 2) * 2 + 1], sfs[i % 2], pTs[i % 2], banks[4 + i % 2])

            def emit_s(i):
                h, qt = steps[i]
                nb, nbb = nbs[h % 2]
                if qt == 0:
                    s.dma("sp", lambda e: e.dma_start(out=nb[:], in_=W["nabias"][h]), writes=[nbb])
                blocks = na_blocks(qt)
                nloc = len(blocks)
                (sa, sab), (sb_, sbb), (sf, sfb), (pT, pTb), _ = res(i)
                qsl = qT[0:64, h, qt * 128:(qt + 1) * 128]
                for j, (kt, bi) in enumerate(blocks):
                    dstb_, dstbb = (sa, sab) if j < 4 else (sb_, sbb)
                    col = (j % 4) * 128
                    s.op("pe", lambda e: e.matmul(dstb_[:, col:col + 128], lhsT=kT[0:64, h, (NCT + kt) * 128:(NCT + kt + 1) * 128], rhs=qsl, start=True, stop=True),
                         reads=[kTb, qTb], writes=[dstbb])
                for c in range(NCT):
                    s.op("pe", lambda e: e.matmul(sb_[:, 128 + c * 128:256 + c * 128], lhsT=kT[0:64, h, c * 128:(c + 1) * 128], rhs=qsl, start=True, stop=True),
                         reads=[kTb, qTb], writes=[sbb])
                b0 = blocks[0][1]
                s.op("dve", lambda e: e.tensor_tensor(out=sf[:, 0:512], in0=sa[:, :], in1=nb[:, b0:b0 + 4, :].rearrange("p b q -> p (b q)"), op=ALU.add),
                     reads=[sab, nbb], writes=[sfb])
                if nloc == 5:
                    s.op("dve", lambda e: e.tensor_tensor(out=sf[:, 512:640], in0=sb_[:, 0:128], in1=nb[:, b0 + 4, :], op=ALU.add),
                         reads=[sbb, nbb], writes=[sfb])
                s.op("act", lambda e: e.activation(out=pT[:, 0:nloc * 128], in_=sf[:, 0:nloc * 128], func=AF.Exp), reads=[sfb], writes=[pTb])
                s.op("act", lambda e: e.activation(out=pT[:, 640:896], in_=sb_[:, 128:384], func=AF.Exp), reads=[sbb], writes=[pTb])

            def emit_pv(i):
                h, qt = steps[i]
                blocks = na_blocks(qt)
                _, _, _, (pT, pTb), (ob, obb) = res(i)
                for j, (kt, bi) in enumerate(blocks):
                    s.op("pe", lambda e: e.matmul(ob[:, 0:65], lhsT=pT[:, j * 128:(j + 1) * 128], rhs=Vs[:, NCT + kt, h, :], start=(j == 0), stop=False),
                         reads=[pTb, Vsb], writes=[obb])
                for c in range(NCT):
                    s.op("pe", lambda e: e.matmul(ob[:, 0:65], lhsT=pT[:, 640 + c * 128:768 + c * 128], rhs=Vs[:, c, h, :], start=False, stop=(c == NCT - 1)),
                         reads=[pTb, Vsb], writes=[obb])
                j8 = i % 8
                s.op("dve", lambda e: e.reciprocal(out=rc[:, j8:j8 + 1], in_=ob[:, 64:65]), reads=[obb], writes=[rcb])
                s.op("dve", lambda e: e.tensor_scalar(out=osb[:, qt, 512 + h * 64:512 + (h + 1) * 64], in0=ob[:, 0:64], scalar1=rc[:, j8:j8 + 1],
                                                      scalar2=None, op0=ALU.mult), reads=[obb, rcb], writes=[osbb])

            emit_s(0)
            for i in range(len(steps)):
                if i + 1 < len(steps):
                    emit_s(i + 1)
                emit_pv(i)
            s.barrier()

        with ExitStack() as es5:
            stg = [k.sb(es5, f"p5_stg{i}", [128, 1024], F32) for i in range(2)]
            w_o, w_ob = k.sb(es5, "p5_wo", [128, 8, 1024], BF16)
            G1, G1b = k.sb(es5, "p5_g1", [128, 1024], F32)
            outs = [k.sb(es5, f"p5_out{i}", [128, 1024], F32) for i in range(2)]
            wv = W["attn_w_out"].rearrange("(kc p) n -> p kc n", p=128)
            for c in range(8):
                load_cast(k, stg, w_o[:, c, :], w_ob, wv[:, c, :], 1024, c)
            load_bcast(k, G1[:], G1b, modv0[0:1, 2, :], 1024)
            for t in range(NT):
                xt, xb = xts[t % 2]
                scr, scrb = scrs[t % 2]
                aT, aTb = aTs[t % 2]
                ot, otb = outs[t % 2]
                s.dma("sp", lambda e: e.dma_start(out=xt[:], in_=x[t * 128:(t + 1) * 128, :]), writes=[xb])
                bank, bb = banks[7]
                transpose_chunks(k, bank, bb, lambda c: osb[:, t, c * 128:(c + 1) * 128], 8, 128, aT[:], aTb, osbb, dst_view=True)
                for g in range(2):
                    yb, ybb = banks[g]
                    for c in range(8):
                        s.op("pe", lambda e: e.matmul(yb[:, :], lhsT=aT[:, c, :], rhs=w_o[:, c, g * 512:(g + 1) * 512], start=(c == 0), stop=(c == 7)),
                             reads=[aTb, w_ob], writes=[ybb])
                    s.op("dve", lambda e: e.tensor_tensor(out=scr[:, g * 512:(g + 1) * 512], in0=yb[:, :], in1=G1[:, g * 512:(g + 1) * 512], op=ALU.mult),
                         reads=[ybb, G1b], writes=[scrb])
                s.op("pool", lambda e: e.tensor_tensor(out=ot[:], in0=scr[:], in1=xt[:], op=ALU.add), reads=[scrb, xb], writes=[otb])
                s.dma("sp", lambda e: e.dma_start(out=hout[t * 128:(t + 1) * 128, :], in_=ot[:]), reads=[otb], writes=[houtb[t]])
            s.barrier()


def host_rope_table():
    t = np.arange(SEQ)
    row = (t // 64).astype(np.float32)
    col = (t % 64).astype(np.float32)
    inv = (np.float32(1.0) / (np.float32(10000.0) ** (np.arange(8, dtype=np.float32) / np.float32(8)))).astype(np.float32)
    ang = np.concatenate([row[:, None] * inv, col[:, None] * inv], axis=-1).astype(np.float32)
    return np.concatenate([np.cos(ang), np.sin(ang)], axis=-1).astype(np.float32)


def host_na_bias(rpb):
    pairs = [(2, j) for j in range(5)] + [(0, j) for j in range(4)] + [(1, j) for j in range(4)] \
        + [(14, 12 + j) for j in range(4)] + [(15, 12 + j) for j in range(4)]
    out = np.full((8, 128, 21, 128), NEG, np.float32)
    p = np.arange(128)
    for bi, (qt, kt) in enumerate(pairs):
        tq = qt * 128 + p
        tk = kt * 128 + p
        r, c = tq // 64, tq % 64
        kr, kc = tk // 64, tk % 64
        r0 = np.clip(r - 4, 0, 24)
        c0 = np.clip(c - 8, 0, 48)
        inside = (kr[:, None] >= r0[None, :]) & (kr[:, None] < r0[None, :] + 8) & (kc[:, None] >= c0[None, :]) & (kc[:, None] < c0[None, :] + 16)
        rr = np.clip(kr[:, None] - r[None, :] + 7, 0, 14)
        rc = np.clip(kc[:, None] - c[None, :] + 15, 0, 30)
        vals = rpb[:, rr, rc]
        out[:, :, bi, :] = np.where(inside[None], vals, np.float32(NEG))
    return out


NROW = 8


def stage_peer(k, hin, hinb, modv_l, w_query, skT_d, u_tab, v_tab, hout, houtb, tag):
    nc, s = k.nc, k.s
    banks = k.banks
    with ExitStack() as es:
        stg = [k.sb(es, f"{tag}_stg{i}", [128, 2048], F32) for i in range(2)]
        wq, wqb = k.sb(es, f"{tag}_wq", [128, 8, 2048], BF16)
        skT, skTb = k.sb(es, f"{tag}_skT", [128, 16, 128], BF16)
        ABG, ABGb = k.sb(es, f"{tag}_ABG", [128, 3, 1024], F32)
        iota_i, iota_ib = k.sb(es, f"{tag}_iotai", [128, 16], I32)
        iota, iotab = k.sb(es, f"{tag}_iota", [128, 16], F32)
        st, stb = k.sb(es, f"{tag}_st", [128, 4], F32)
        xts = [k.sb(es, f"{tag}_x{i}", [128, 1024], F32) for i in range(2)]
        scrs = [k.sb(es, f"{tag}_scr{i}", [128, 1024], F32) for i in range(2)]
        hms = [k.sb(es, f"{tag}_hm{i}", [128, 1024], F32) for i in range(2)]
        hbf, hbfb = k.sb(es, f"{tag}_hbf", [128, 1024], BF16)
        hT, hTb = k.sb(es, f"{tag}_hT", [128, 8, 128], BF16)
        qbf, qbfb = k.sb(es, f"{tag}_qbf", [128, 2048], BF16)
        qT, qTb = k.sb(es, f"{tag}_qT", [128, 16, 128], BF16)
        ssb, ssbb = k.sb(es, f"{tag}_s", [128, 16, 128], F32)
        s2, _ = k.sb(es, f"{tag}_s2", [128, 16, 128], F32)
        m16, _ = k.sb(es, f"{tag}_m16", [128, 16, 16], F32)
        i16, _ = k.sb(es, f"{tag}_i16", [128, 16, 16], U32)
        i16f, i16fb = k.sb(es, f"{tag}_i16f", [128, 16, 16], F32)
        cand, candb = k.sb(es, f"{tag}_cand", [128, 8, 256], F32)
        cand2, _ = k.sb(es, f"{tag}_cand2", [128, 8, 256], F32)
        best, _ = k.sb(es, f"{tag}_best", [128, 8, 16], F32)
        pos, _ = k.sb(es, f"{tag}_pos", [128, 8, 16], U32)
        ab_i, ab_ib = k.sb(es, f"{tag}_abi", [128, 2, 128], I32)
        ab_f, ab_fb = k.sb(es, f"{tag}_abf", [128, 2, 128], F32)
        oh, ohb = k.sb(es, f"{tag}_oh", [128, 8, 16, 16], F32)
        e01, e01b = k.sb(es, f"{tag}_e01", [128, 2, 128], F32)
        idxs = [k.sb(es, f"{tag}_idx{i}", [128, 128], I32) for i in range(2)]
        gts = [k.sb(es, f"{tag}_gate{i}", [128, 8, 16], F32) for i in range(2)]
        gsum, gsumb = k.sb(es, f"{tag}_gsum", [128, 8], F32)
        actv, _ = k.sb(es, f"{tag}_act", [128, 128], F32)
        wgt, wgtb = k.sb(es, f"{tag}_wgt", [128, 128], F32)
        junk, _ = k.sb(es, f"{tag}_junk", [128, 1024], BF16)
        rows = [k.sb(es, f"{tag}_row{i}", [128, 1024], F32) for i in range(NROW)]
        accs = [k.sb(es, f"{tag}_acc{i}", [128, 1024], F32) for i in range(4)]
        ot, otb = k.sb(es, f"{tag}_ot", [128, 1024], F32)
        hpb = [[Buf() for _ in range(16)] for _ in range(3)]
        hb = [[Buf() for _ in range(8)] for _ in range(3)]
        actb = [Buf() for _ in range(16)]

        qi = load_w_bf16(k, stg, wq, wqb, w_query, 8, 2048)
        load_cast(k, stg, skT[:].rearrange("p a b -> p (a b)"), skTb, skT_d.rearrange("p a b -> p (a b)"), 2048, qi)
        for j in range(3):
            load_bcast(k, ABG[:, j, :], ABGb, modv_l[0:1, 3 + j, :], 1024)
        s.op("pool", lambda e: e.iota(out=iota_i[:], pattern=[[1, 16]], base=0, channel_multiplier=0), writes=[iota_ib])
        s.op("dve", lambda e: e.tensor_copy(out=iota[:], in_=iota_i[:]), reads=[iota_ib], writes=[iotab])

        def front(t):
            xt, xb = xts[t % 2]
            scr, scrb = scrs[t % 2]
            hm, hmb = hms[t % 2]
            idx, idxb = idxs[t % 2]
            gate, gateb = gts[t % 2]
            s.dma("sp", lambda e: e.dma_start(out=xt[:], in_=hin[t * 128:(t + 1) * 128, :]), reads=[hinb[t]], writes=[xb])
            norm_mod(k, xt, xb, ABG[:, 0, :], ABG[:, 1, :], ABGb, scr, scrb, st, stb, hbf, hbfb, out_f32=(hm, hmb))
            bank, bb = banks[7]
            transpose_chunks(k, bank, bb, lambda c: hbf[:, c * 128:(c + 1) * 128], 8, 128, hT[:], hTb, hbfb, dst_view=True)
            for g in range(4):
                qb_, qbb = banks[g]
                for c in range(8):
                    s.op("pe", lambda e: e.matmul(qb_[:, :], lhsT=hT[:, c, :], rhs=wq[:, c, g * 512:(g + 1) * 512], start=(c == 0), stop=(c == 7)),
                         reads=[hTb, wqb], writes=[qbb])
                s.op("act", lambda e: e.copy(out=qbf[:, g * 512:(g + 1) * 512], in_=qb_[:, :]), reads=[qbb], writes=[qbfb])
            for half in range(2):
                tb_, tbb = banks[4 + half]
                transpose_chunks(k, tb_, tbb, lambda c: qbf[:, (half * 8 + c) * 128:(half * 8 + c + 1) * 128], 8, 128,
                                 qT[:, half * 8:(half + 1) * 8, :], qTb, qbfb, dst_view=True)
            for g in range(4):
                sb_, sbb = banks[g]
                for j in range(4):
                    hp = g * 4 + j
                    s.op("pe", lambda e: e.matmul(sb_[:, j * 128:(j + 1) * 128], lhsT=qT[:, hp, :], rhs=skT[:, hp, :], start=True, stop=True),
                         reads=[qTb, skTb], writes=[sbb])
                s.op("act", lambda e: e.copy(out=ssb[:, g * 4:(g + 1) * 4, :], in_=sb_[:, :].rearrange("p (a b) -> p a b", b=128)), reads=[sbb], writes=[ssbb])
            for hp in range(16):
                s.op("dve", lambda e: e.max(out=m16[:, hp, 0:8], in_=ssb[:, hp, :]), reads=[ssbb], writes=[hpb[0][hp]])
            for hp in range(16):
                s.op("dve", lambda e: e.max_index(out=i16[:, hp, 0:8], in_max=m16[:, hp, 0:8], in_values=ssb[:, hp, :]),
                     reads=[ssbb, hpb[0][hp]], writes=[hpb[1][hp]])
            for hp in range(16):
                s.op("dve", lambda e: e.match_replace(out=s2[:, hp, :], in_to_replace=m16[:, hp, 0:8], in_values=ssb[:, hp, :], imm_value=-1e30),
                     reads=[ssbb, hpb[0][hp]], writes=[hpb[2][hp]])
            for hp in range(16):
                s.op("dve", lambda e: e.max(out=m16[:, hp, 8:16], in_=s2[:, hp, :]), reads=[hpb[2][hp]], writes=[hpb[0][hp]])
            for hp in range(16):
                s.op("dve", lambda e: e.max_index(out=i16[:, hp, 8:16], in_max=m16[:, hp, 8:16], in_values=s2[:, hp, :]),
                     reads=[hpb[2][hp], hpb[0][hp]], writes=[hpb[1][hp]])
            s.op("dve", lambda e: e.tensor_copy(out=i16f[:], in_=i16[:]), reads=hpb[1], writes=[i16fb])
            m4 = m16[:].rearrange("p (h t) a -> p h t a", t=2)
            s.op("dve", lambda e: e.tensor_tensor(out=cand[:].rearrange("p h (a b) -> p h a b", b=16),
                                                  in0=m4[:, :, 0, :].unsqueeze(3).to_broadcast([128, 8, 16, 16]),
                                                  in1=m4[:, :, 1, :].unsqueeze(2).to_broadcast([128, 8, 16, 16]), op=ALU.add),
                 reads=hpb[0], writes=[candb])
            for h in range(8):
                s.op("dve", lambda e: e.max(out=best[:, h, 0:8], in_=cand[:, h, :]), reads=[candb], writes=[hb[0][h]])
            for h in range(8):
                s.op("dve", lambda e: e.max_index(out=pos[:, h, 0:8], in_max=best[:, h, 0:8], in_values=cand[:, h, :]),
                     reads=[candb, hb[0][h]], writes=[hb[1][h]])
            for h in range(8):
                s.op("dve", lambda e: e.match_replace(out=cand2[:, h, :], in_to_replace=best[:, h, 0:8], in_values=cand[:, h, :], imm_value=-1e30),
                     reads=[candb, hb[0][h]], writes=[hb[2][h]])
            for h in range(8):
                s.op("dve", lambda e: e.max(out=best[:, h, 8:16], in_=cand2[:, h, :]), reads=[hb[2][h]], writes=[hb[0][h]])
            for h in range(8):
                s.op("dve", lambda e: e.max_index(out=pos[:, h, 8:16], in_max=best[:, h, 8:16], in_values=cand2[:, h, :]),
                     reads=[hb[2][h], hb[0][h]], writes=[hb[1][h]])
            posi = pos[:].rearrange("p h k -> p (h k)").bitcast(I32)
            s.op("dve", lambda e: e.tensor_single_scalar(out=ab_i[:, 0, :], in_=posi, scalar=4, op=ALU.arith_shift_right), reads=hb[1], writes=[ab_ib])
            s.op("dve", lambda e: e.tensor_single_scalar(out=ab_i[:, 1, :], in_=posi, scalar=15, op=ALU.bitwise_and), reads=hb[1], writes=[ab_ib])
            s.op("dve", lambda e: e.tensor_copy(out=ab_f[:], in_=ab_i[:]), reads=[ab_ib], writes=[ab_fb])
            i4 = i16f[:].rearrange("p (h t) a -> p h t a", t=2)
            for p_ in range(2):
                s.op("dve", lambda e: e.tensor_tensor(out=oh[:], in0=ab_f[:, p_, :].rearrange("p (h k) -> p h k", k=16).unsqueeze(3).to_broadcast([128, 8, 16, 16]),
                                                      in1=iota[:].unsqueeze(1).unsqueeze(1).to_broadcast([128, 8, 16, 16]), op=ALU.is_equal),
                     reads=[ab_fb, iotab], writes=[ohb])
                s.op("dve", lambda e: e.tensor_tensor(out=oh[:], in0=oh[:], in1=i4[:, :, p_, :].unsqueeze(2).to_broadcast([128, 8, 16, 16]), op=ALU.mult),
                     reads=[ohb, i16fb], writes=[ohb])
                s.op("dve", lambda e: e.tensor_reduce(out=e01[:, p_, :].rearrange("p (h k) -> p h k", k=16), in_=oh[:], axis=AX.X, op=ALU.add),
                     reads=[ohb], writes=[e01b])
            s.op("dve", lambda e: e.scalar_tensor_tensor(out=e01[:, 0, :], in0=e01[:, 0, :], scalar=128.0, in1=e01[:, 1, :], op0=ALU.mult, op1=ALU.add),
                 reads=[e01b], writes=[e01b])
            s.op("dve", lambda e: e.tensor_copy(out=idx[:], in_=e01[:, 0, :]), reads=[e01b], writes=[idxb])
            s.op("dve", lambda e: e.tensor_tensor(out=gate[:], in0=best[:], in1=best[:, :, 0:1].to_broadcast([128, 8, 16]), op=ALU.subtract),
                 reads=hb[0], writes=[gateb])
            s.op("act", lambda e: e.activation(out=gate[:], in_=gate[:], func=AF.Exp), reads=[gateb], writes=[gateb])
            s.op("dve", lambda e: e.tensor_reduce(out=gsum[:], in_=gate[:], axis=AX.X, op=ALU.add), reads=[gateb], writes=[gsumb])
            s.op("dve", lambda e: e.reciprocal(out=gsum[:], in_=gsum[:]), reads=[gsumb], writes=[gsumb])
            s.op("dve", lambda e: e.tensor_tensor(out=gate[:], in0=gate[:], in1=gsum[:].unsqueeze(2).to_broadcast([128, 8, 16]), op=ALU.mult),
                 reads=[gateb, gsumb], writes=[gateb])

        ring = [0]

        def gather(tab, idx, idxb, hk):
            rw, rwb = rows[ring[0] % NROW]
            ring[0] += 1
            s.dma("pool", lambda e: e.indirect_dma_start(out=rw[:], out_offset=None, in_=tab,
                                                         in_offset=bass.IndirectOffsetOnAxis(ap=idx[:, hk:hk + 1], axis=0)),
                  reads=[idxb], writes=[rwb])
            return rw, rwb

        def back(t):
            xt, xb = xts[t % 2]
            scr, scrb = scrs[t % 2]
            hm, hmb = hms[t % 2]
            idx, idxb = idxs[t % 2]
            gate, gateb = gts[t % 2]
            for hk in range(128):
                rw, rwb = gather(u_tab, idx, idxb, hk)
                s.op("dve", lambda e: e.scalar_tensor_tensor(out=junk[:], in0=rw[:], scalar=1.0, in1=hm[:], op0=ALU.mult, op1=ALU.mult,
                                                             accum_out=actv[:, hk:hk + 1]), reads=[rwb, hmb], writes=[actb[hk % 16]])
            s.op("act", lambda e: e.activation(out=wgt[:], in_=actv[:], func=AF.Gelu), reads=actb, writes=[wgtb])
            s.op("dve", lambda e: e.tensor_tensor(out=wgt[:], in0=wgt[:], in1=gate[:].rearrange("p h k -> p (h k)"), op=ALU.mult),
                 reads=[wgtb, gateb], writes=[wgtb])
            for hk in range(128):
                rw, rwb = gather(v_tab, idx, idxb, hk)
                ac, acb = accs[hk % 4]
                if hk < 4:
                    s.op("dve", lambda e: e.tensor_scalar(out=ac[:], in0=rw[:], scalar1=wgt[:, hk:hk + 1], scalar2=None, op0=ALU.mult),
                         reads=[rwb, wgtb], writes=[acb])
                else:
                    s.op("dve", lambda e: e.scalar_tensor_tensor(out=ac[:], in0=rw[:], scalar=wgt[:, hk:hk + 1], in1=ac[:], op0=ALU.mult, op1=ALU.add),
                         reads=[rwb, wgtb, acb], writes=[acb])
            (a0, a0b), (a1, a1b), (a2, a2b), (a3, a3b) = accs
            s.op("pool", lambda e: e.tensor_tensor(out=a0[:], in0=a0[:], in1=a1[:], op=ALU.add), reads=[a0b, a1b], writes=[a0b])
            s.op("pool", lambda e: e.tensor_tensor(out=a2[:], in0=a2[:], in1=a3[:], op=ALU.add), reads=[a2b, a3b], writes=[a2b])
            s.op("pool", lambda e: e.tensor_tensor(out=a0[:], in0=a0[:], in1=a2[:], op=ALU.add), reads=[a0b, a2b], writes=[a0b])
            s.op("dve", lambda e: e.tensor_tensor(out=scr[:], in0=a0[:], in1=ABG[:, 2, :], op=ALU.mult), reads=[a0b, ABGb], writes=[scrb])
            s.op("pool", lambda e: e.tensor_tensor(out=ot[:], in0=scr[:], in1=xt[:], op=ALU.add), reads=[scrb, xb], writes=[otb])
            s.dma("sp", lambda e: e.dma_start(out=hout[t * 128:(t + 1) * 128, :], in_=ot[:]), reads=[otb], writes=[houtb[t]])

        front(0)
        for t in range(NT):
            if t + 1 < NT:
                front(t + 1)
            back(t)
        s.barrier()


def stage_conv(k, hin, hinb, modv_l, W, hout, houtb):
    nc, s = k.nc, k.s
    banks = k.banks
    PADW = SEQ + 30
    with ExitStack() as es:
        cbuf, cbufb = k.sb(es, "cv_cbuf", [128, NT, 1024], F32)
        ABG, ABGb = k.sb(es, "cv_ABG", [128, 2, 1024], F32)
        st, stb = k.sb(es, "cv_st", [128, 8], F32)
        xts = [k.sb(es, f"cv_x{i}", [128, 1024], F32) for i in range(2)]
        scrs = [k.sb(es, f"cv_scr{i}", [128, 1024], F32) for i in range(2)]
        for j in range(2):
            load_bcast(k, ABG[:, j, :], ABGb, modv_l[0:1, j, :], 1024)
        cbt = [Buf() for _ in range(NT // 4)]
        with ExitStack() as es1:
            stg = [k.sb(es1, f"cv_stg{i}", [128, 1024], F32) for i in range(2)]
            aTa, aTab = k.sb(es1, "cv_aT", [128, 8, SEQ], BF16)
            w1, w1b = k.sb(es1, "cv_w1", [128, 8, 2048], BF16)
            b1T, b1Tb = k.sb(es1, "cv_b1T", [128, 16], F32)
            wdw, wdwb = k.sb(es1, "cv_wdw", [128, 8, 31], F32)
            bdw, bdwb = k.sb(es1, "cv_bdw", [128, 8], F32)
            abfs = [k.sb(es1, f"cv_a{i}", [128, 1024], BF16) for i in range(2)]
            upads = [k.sb(es1, f"cv_up{i}", [128, PADW], F32) for i in range(2)]
            accs = [k.sb(es1, f"cv_acc{i}", [128, SEQ], F32) for i in range(2)]
            sgs = [k.sb(es1, f"cv_sg{i}", [128, 512], F32) for i in range(2)]
            load_w_bf16(k, stg, w1, w1b, W["conv_w_pw1"], 8, 2048)
            s.dma("sp", lambda e: e.dma_start(out=b1T[:], in_=W["conv_b1T"]), writes=[b1Tb])
            s.dma("sp", lambda e: e.dma_start(out=wdw[:], in_=W["conv_wdwT"]), writes=[wdwb])
            s.dma("sp", lambda e: e.dma_start(out=bdw[:], in_=W["conv_bdwT"]), writes=[bdwb])
            for up, upb in upads:
                s.op("pool", lambda e: e.memset(up[:, 0:15], 0.0), writes=[upb])
                s.op("pool", lambda e: e.memset(up[:, 15 + SEQ:PADW], 0.0), writes=[upb])
            for t in range(NT):
                xt, xb = xts[t % 2]
                scr, scrb = scrs[t % 2]
                abf, abfb = abfs[t % 2]
                s.dma("sp", lambda e: e.dma_start(out=xt[:], in_=hin[t * 128:(t + 1) * 128, :]), reads=[hinb[t]], writes=[xb])
                norm_mod(k, xt, xb, ABG[:, 0, :], ABG[:, 1, :], ABGb, scr, scrb, st, stb, abf, abfb)
                bank, bb = banks[6 + t % 2]
                transpose_chunks(k, bank, bb, lambda c: abf[:, c * 128:(c + 1) * 128], 8, 128, aTa[:, :, t * 128:(t + 1) * 128], aTab, abfb, dst_view=True)
            accbs = [[Buf() for _ in range(4)] for _ in range(2)]
            it = 0
            for m in range(8):
                up, upb = upads[m % 2]
                acc, _ = accs[m % 2]
                accb = accbs[m % 2]
                for tg in range(4):
                    (bv_, bvb), (bg_, bgb) = banks[(it % 2) * 2], banks[(it % 2) * 2 + 1]
                    sg, sgb = sgs[it % 2]
                    it += 1
                    for (bk, bkb, c0) in ((bv_, bvb, m * 128), (bg_, bgb, 1024 + m * 128)):
                        for c in range(8):
                            s.op("pe", lambda e: e.matmul(bk[:, :], lhsT=w1[:, c, c0:c0 + 128], rhs=aTa[:, c, tg * 512:(tg + 1) * 512], start=(c == 0), stop=(c == 7)),
                                 reads=[w1b, aTab], writes=[bkb])
                    s.op("act", lambda e: e.activation(out=sg[:], in_=bg_[:, :], func=AF.Sigmoid, bias=b1T[:, 8 + m:9 + m]), reads=[bgb, b1Tb], writes=[sgb])
                    s.op("dve", lambda e: e.scalar_tensor_tensor(out=up[:, 15 + tg * 512:15 + (tg + 1) * 512], in0=bv_[:, :], scalar=b1T[:, m:m + 1], in1=sg[:],
                                                                 op0=ALU.add, op1=ALU.mult), reads=[bvb, sgb, b1Tb], writes=[upb])
                for j in range(31):
                    for ch in range(4):
                        src = up[:, ch * 512 + j:ch * 512 + j + 512]
                        dst = acc[:, ch * 512:(ch + 1) * 512]
                        if j == 0:
                            s.op("dve", lambda e: e.tensor_scalar(out=dst, in0=src, scalar1=wdw[:, m, 0:1], scalar2=bdw[:, m:m + 1], op0=ALU.mult, op1=ALU.add),
                                 reads=[upb, wdwb, bdwb], writes=[accb[ch]])
                        else:
                            s.op("dve", lambda e: e.scalar_tensor_tensor(out=dst, in0=src, scalar=wdw[:, m, j:j + 1], in1=dst, op0=ALU.mult, op1=ALU.add),
                                 reads=[upb, wdwb, accb[ch]], writes=[accb[ch]])
                for g in range(NT // 4):
                    tb_, tbb = banks[4 + g % 2]
                    for j in range(4):
                        t = g * 4 + j
                        s.op("pe", lambda e: e.transpose(out=tb_[:, j * 128:(j + 1) * 128], in_=acc[:, t * 128:(t + 1) * 128], identity=k.identf[:]),
                             reads=[accb[t // 4], k.identb], writes=[tbb])
                    s.op("act", lambda e: e.copy(out=cbuf[:, g * 4:(g + 1) * 4, m * 128:(m + 1) * 128], in_=tb_[:, :].rearrange("p (a b) -> p a b", b=128)),
                         reads=[tbb], writes=[cbt[g]])
            s.barrier()
        with ExitStack() as es3:
            stg = [k.sb(es3, f"cv3_stg{i}", [128, 1024], F32) for i in range(2)]
            w2, w2b = k.sb(es3, "cv3_w2", [128, 8, 1024], BF16)
            gl, glb = k.sb(es3, "cv3_gl", [128, 4, 1024], F32)
            sbfs = [k.sb(es3, f"cv3_s{i}", [128, 1024], BF16) for i in range(2)]
            sTs = [k.sb(es3, f"cv3_sT{i}", [128, 8, 128], BF16) for i in range(2)]
            outs = [k.sb(es3, f"cv3_o{i}", [128, 1024], F32) for i in range(2)]
            wv = W["conv_w_pw2"].rearrange("(kc p) n -> p kc n", p=128)
            for c in range(8):
                load_cast(k, stg, w2[:, c, :], w2b, wv[:, c, :], 1024, c)
            load_bcast(k, gl[:, 0, :], glb, W["conv_g_ln"], 1024)
            load_bcast(k, gl[:, 1, :], glb, W["conv_b_ln"], 1024)
            load_bcast(k, gl[:, 2, :], glb, W["conv_b_pw2"], 1024)
            load_bcast(k, gl[:, 3, :], glb, modv_l[0:1, 2, :], 1024)
            for t in range(NT):
                xt, xb = xts[t % 2]
                scr, scrb = scrs[t % 2]
                sbf, sbfb = sbfs[t % 2]
                sT, sTb = sTs[t % 2]
                ot, otb = outs[t % 2]
                cb = cbt[t // 4]
                c_t = cbuf[:, t, :]
                s.dma("sp", lambda e: e.dma_start(out=xt[:], in_=hin[t * 128:(t + 1) * 128, :]), reads=[hinb[t]], writes=[xb])
                s.op("act", lambda e: e.activation(out=scr[:], in_=c_t, func=AF.Identity, accum_out=st[:, 0:1]), reads=[cb], writes=[scrb, stb])
                s.op("act", lambda e: e.activation(out=scr[:], in_=c_t, func=AF.Square, accum_out=st[:, 1:2]), reads=[cb], writes=[scrb, stb])
                s.op("dve", lambda e: e.tensor_scalar(out=st[:, 2:3], in0=st[:, 0:1], scalar1=1.0 / D, scalar2=None, op0=ALU.mult), reads=[stb], writes=[stb])
                s.op("dve", lambda e: e.scalar_tensor_tensor(out=st[:, 3:4], in0=st[:, 2:3], scalar=-1.0, in1=st[:, 2:3], op0=ALU.mult, op1=ALU.mult),
                     reads=[stb], writes=[stb])
                s.op("dve", lambda e: e.scalar_tensor_tensor(out=st[:, 4:5], in0=st[:, 1:2], scalar=1.0 / D, in1=st[:, 3:4], op0=ALU.mult, op1=ALU.add),
                     reads=[stb], writes=[stb])
                s.op("act", lambda e: e.activation(out=st[:, 5:6], in_=st[:, 4:5], func=AF.Sqrt, scale=1.0, bias=k.eps[:, 0:1]), reads=[stb], writes=[stb])
                s.op("dve", lambda e: e.reciprocal(out=st[:, 5:6], in_=st[:, 5:6]), reads=[stb], writes=[stb])
                s.op("dve", lambda e: e.tensor_scalar(out=scr[:], in0=c_t, scalar1=st[:, 2:3], scalar2=st[:, 5:6], op0=ALU.subtract, op1=ALU.mult),
                     reads=[cb, stb], writes=[scrb])
                s.op("dve", lambda e: e.tensor_tensor(out=scr[:], in0=scr[:], in1=gl[:, 0, :], op=ALU.mult), reads=[scrb, glb], writes=[scrb])
                s.op("pool", lambda e: e.tensor_tensor(out=scr[:], in0=scr[:], in1=gl[:, 1, :], op=ALU.add), reads=[scrb, glb], writes=[scrb])
                s.op("act", lambda e: e.activation(out=sbf[:], in_=scr[:], func=AF.Silu), reads=[scrb], writes=[sbfb])
                bank, bb = banks[7]
                transpose_chunks(k, bank, bb, lambda c: sbf[:, c * 128:(c + 1) * 128], 8, 128, sT[:], sTb, sbfb, dst_view=True)
                for g in range(2):
                    yb, ybb = banks[g]
                    for c in range(8):
                        s.op("pe", lambda e: e.matmul(yb[:, :], lhsT=sT[:, c, :], rhs=w2[:, c, g * 512:(g + 1) * 512], start=(c == 0), stop=(c == 7)),
                             reads=[sTb, w2b], writes=[ybb])
                    s.op("dve", lambda e: e.tensor_tensor(out=scr[:, g * 512:(g + 1) * 512], in0=yb[:, :], in1=gl[:, 2, g * 512:(g + 1) * 512], op=ALU.add),
                         reads=[ybb, glb], writes=[scrb])
                s.op("pool", lambda e: e.tensor_tensor(out=scr[:], in0=scr[:], in1=gl[:, 3, :], op=ALU.mult), reads=[scrb, glb], writes=[scrb])
                s.op("pool", lambda e: e.tensor_tensor(out=ot[:], in0=scr[:], in1=xt[:], op=ALU.add), reads=[scrb, xb], writes=[otb])
                s.dma("sp", lambda e: e.dma_start(out=hout[t * 128:(t + 1) * 128, :], in_=ot[:]), reads=[otb], writes=[houtb[t]])
            s.barrier()


IN_SPECS = {
    "x": ([SEQ, D], F32), "ctx": ([CTX, D], F32), "cc": ([128, 16], F32),
    "w_ada": ([2, D, 6 * D], F32), "b_ada": ([2, 6 * D], F32), "g_norm": ([4, D], F32),
    "ident": ([128, 128], BF16), "identf": ([128, 128], F32),
    "attn_w_in": ([D, 2208], F32), "mla_w_q_up": ([384, 768], F32), "mla_w_kv_up": ([256, 1024], F32),
    "mla_g_qa": ([1, 384], F32), "mla_g_kva": ([1, 256], F32), "mla_g_q": ([1, 96], F32), "mla_g_k": ([1, 96], F32),
    "na_g_q": ([1, 64], F32), "na_g_k": ([1, 64], F32), "attn_w_out": ([D, D], F32),
    "rope": ([SEQ, 32], F32), "nabias": ([8, 128, 21, 128], F32),
    "conv_w_pw1": ([D, 2 * D], F32), "conv_b1T": ([128, 16], F32), "conv_wdwT": ([128, 8, 31], F32), "conv_bdwT": ([128, 8], F32),
    "conv_g_ln": ([1, D], F32), "conv_b_ln": ([1, D], F32), "conv_w_pw2": ([D, D], F32), "conv_b_pw2": ([1, D], F32),
    "wq0": ([D, 2048], F32), "wq1": ([D, 2048], F32), "skT0": ([128, 16, 128], F32), "skT1": ([128, 16, 128], F32),
    "u0": ([16384, D], F32), "u1": ([16384, D], F32), "v0": ([16384, D], F32), "v1": ([16384, D], F32),
}


def build_program():
    nc = bass.Bass("TRN2", target_bir_lowering=False)
    A = {n: nc.dram_tensor(n, sh, dt, kind="ExternalInput").ap() for n, (sh, dt) in IN_SPECS.items()}
    out = nc.dram_tensor("out", [SEQ, D], F32, kind="ExternalOutput").ap()
    modv = nc.dram_tensor("modv_scr", [2, 2, 6, D], F32, kind="Internal").ap()
    hs = [nc.dram_tensor(f"h_scr{i}", [SEQ, D], F32, kind="Internal").ap() for i in range(3)]
    hb = [[Buf() for _ in range(NT)] for _ in range(4)]
    uvs = [nc.dram_tensor(f"uv_scr{l}", [16384, 2 * D], BF16, kind="Internal").ap() for l in range(2)]
    with ExitStack() as es:
        k = K(nc, es)
        k.modv_buf = Buf()
        setup_consts(k, es, A["ident"], A["identf"])
        uvb = [[], []]
        for l in range(2):
            issue_uv_cast(k, es, A[f"u{l}"], A[f"v{l}"], uvs[l], uvb[l])
        stage_ada(k, A["cc"], A["w_ada"], A["b_ada"], A["g_norm"], modv)
        stage_attn(k, A["x"], A["ctx"], modv[0], A, hs[0], hb[0])
        stage_peer2(k, hs[0], hb[0], modv[0], A["wq0"], A["skT0"], uvs[0], uvb[0], hs[1], hb[1], "pra")
        stage_conv(k, hs[1], hb[1], modv[1], A, hs[2], hb[2])
        stage_peer2(k, hs[2], hb[2], modv[1], A["wq1"], A["skT1"], uvs[1], uvb[1], out, hb[3], "prb")
        k.s.barrier()
    return nc


def kernel(**inp):
    import ml_dtypes
    f = lambda a: np.ascontiguousarray(np.asarray(a, dtype=np.float32))
    nb = inp["x"].shape[0]
    shared = {
        "w_ada": f(inp["w_ada"]), "b_ada": f(inp["b_ada"]),
        "g_norm": f(np.stack([inp["g_norm1"][0], inp["g_norm2"][0], inp["g_norm1"][1], inp["g_norm2"][1]])),
        "ident": np.eye(128).astype(ml_dtypes.bfloat16), "identf": np.eye(128, dtype=np.float32),
        "attn_w_in": f(inp["attn_w_in"][0]), "mla_w_q_up": f(inp["mla_w_q_up"][0]), "mla_w_kv_up": f(inp["mla_w_kv_up"][0]),
        "mla_g_qa": f(inp["mla_g_qa"]), "mla_g_kva": f(inp["mla_g_kva"]), "mla_g_q": f(inp["mla_g_q"]), "mla_g_k": f(inp["mla_g_k"]),
        "na_g_q": f(inp["na_g_q"]), "na_g_k": f(inp["na_g_k"]), "attn_w_out": f(inp["attn_w_out"][0]),
        "rope": host_rope_table(), "nabias": host_na_bias(np.asarray(inp["na_rpb"][0], np.float32)),
        "conv_w_pw1": f(inp["conv_w_pw1"][0]), "conv_b1T": f(np.asarray(inp["conv_b_pw1"][0]).reshape(16, 128).T),
        "conv_wdwT": f(np.asarray(inp["conv_w_dw"][0]).reshape(31, 8, 128).transpose(2, 1, 0)),
        "conv_bdwT": f(np.asarray(inp["conv_b_dw"][0]).reshape(8, 128).T),
        "conv_g_ln": f(inp["conv_g_ln"]), "conv_b_ln": f(inp["conv_b_ln"]), "conv_w_pw2": f(inp["conv_w_pw2"][0]), "conv_b_pw2": f(inp["conv_b_pw2"]),
    }
    for l in range(2):
        shared[f"wq{l}"] = f(inp["peer_w_query"][l])
        shared[f"skT{l}"] = f(np.asarray(inp["peer_sub_keys"][l]).reshape(16, 128, 128).transpose(2, 0, 1))
        shared[f"u{l}"] = f(inp["peer_u"][l])
        shared[f"v{l}"] = f(inp["peer_v"][l])
    in_maps = []
    for b in range(nb):
        cc = np.zeros((128, 16), np.float32)
        cc[:, 0::2] = np.asarray(inp["c"][b], np.float32).reshape(8, 128).T
        cc[:, 1::2] = np.asarray(inp["c_ctx"], np.float32).reshape(8, 128).T
        m = dict(shared)
        m["x"] = f(inp["x"][b])
        m["ctx"] = f(inp["ctx"][b])
        m["cc"] = cc
        in_maps.append(m)
    nc = build_program()
    res = run_bass_kernel_spmd(nc, in_maps, core_ids=list(range(nb)))
    return np.stack([np.asarray(r["out"], dtype=np.float32) for r in res.results], axis=0)


def issue_uv_cast(k, es, u_tab, v_tab, uv, uvb):
    for i in range(4):
        r0, r1 = i * 4096, (i + 1) * 4096
        b0, b1 = Buf(), Buf()
        k.s.bulk_dma("pool", lambda e: e.dma_start(out=uv[r0:r1, 0:1024], in_=u_tab[r0:r1, :]), writes=[b0], es=es)
        k.s.bulk_dma("pool", lambda e: e.dma_start(out=uv[r0:r1, 1024:2048], in_=v_tab[r0:r1, :]), writes=[b1], es=es)
        uvb.extend([b0, b1])


NROW2 = 16


def stage_peer2(k, hin, hinb, modv_l, w_query, skT_d, uv, uvb, hout, houtb, tag):
    nc, s = k.nc, k.s
    banks = k.banks
    with ExitStack() as es:
        wq, wqb = k.sb(es, f"{tag}_wq", [128, 8, 2048], BF16)
        skT, skTb = k.sb(es, f"{tag}_skT", [128, 16, 128], BF16)
        ABG, ABGb = k.sb(es, f"{tag}_ABG", [128, 3, 1024], F32)
        iota_i, iota_ib = k.sb(es, f"{tag}_iotai", [128, 16], I32)
        iota, iotab = k.sb(es, f"{tag}_iota", [128, 16], F32)
        st, stb = k.sb(es, f"{tag}_st", [128, 4], F32)
        xts = [k.sb(es, f"{tag}_x{i}", [128, 1024], F32) for i in range(2)]
        scrs = [k.sb(es, f"{tag}_scr{i}", [128, 1024], F32) for i in range(2)]
        hbfs = [k.sb(es, f"{tag}_hbf{i}", [128, 1024], BF16) for i in range(1)]
        hms = [k.sb(es, f"{tag}_hm{i}", [128, 1024], F32) for i in range(2)]
        hT, hTb = k.sb(es, f"{tag}_hT", [128, 8, 128], BF16)
        qbf, qbfb = k.sb(es, f"{tag}_qbf", [128, 2048], BF16)
        qT, qTb = k.sb(es, f"{tag}_qT", [128, 16, 128], BF16)
        ssb, ssbb = k.sb(es, f"{tag}_s", [128, 16, 128], F32)
        s2, _ = k.sb(es, f"{tag}_s2", [128, 16, 128], F32)
        m16, _ = k.sb(es, f"{tag}_m16", [128, 16, 16], F32)
        i16, _ = k.sb(es, f"{tag}_i16", [128, 16, 16], U32)
        i16f, i16fb = k.sb(es, f"{tag}_i16f", [128, 16, 16], F32)
        cand, candb = k.sb(es, f"{tag}_cand", [128, 8, 256], F32)
        cand2, _ = k.sb(es, f"{tag}_cand2", [128, 8, 256], F32)
        best, _ = k.sb(es, f"{tag}_best", [128, 8, 16], F32)
        pos, _ = k.sb(es, f"{tag}_pos", [128, 8, 16], U32)
        ab_i, ab_ib = k.sb(es, f"{tag}_abi", [128, 2, 128], I32)
        ab_f, ab_fb = k.sb(es, f"{tag}_abf", [128, 2, 128], F32)
        oh, ohb = k.sb(es, f"{tag}_oh", [128, 8, 16, 16], F32)
        e01, e01b = k.sb(es, f"{tag}_e01", [128, 2, 128], F32)
        idxs = [k.sb(es, f"{tag}_idx{i}", [128, 128], I32) for i in range(2)]
        gts = [k.sb(es, f"{tag}_gate{i}", [128, 8, 16], F32) for i in range(2)]
        gsum, gsumb = k.sb(es, f"{tag}_gsum", [128, 8], F32)
        actv, _ = k.sb(es, f"{tag}_act", [128, 128], F32)
        wgt, _ = k.sb(es, f"{tag}_wgt", [128, 128], F32)
        junk, _ = k.sb(es, f"{tag}_junk", [128, 1024], BF16)
        rows = [k.sb(es, f"{tag}_row{i}", [128, 2048], BF16) for i in range(NROW2)]
        dgs = [k.sb(es, f"{tag}_dg{i}", [128, 128], BF16) for i in range(4)]
        ot, otb = k.sb(es, f"{tag}_ot", [128, 1024], F32)
        hpb = [[Buf() for _ in range(16)] for _ in range(3)]
        hb = [[Buf() for _ in range(8)] for _ in range(3)]
        actb = [Buf() for _ in range(16)]
        wgb = [Buf() for _ in range(4)]

        wqv = w_query.rearrange("(kc p) n -> p kc n", p=128)
        for c in range(8):
            s.dma("pool", lambda e: e.dma_start(out=wq[:, c, :], in_=wqv[:, c, :]), writes=[wqb])
        s.dma("pool", lambda e: e.dma_start(out=skT[:].rearrange("p a b -> p (a b)"), in_=skT_d.rearrange("p a b -> p (a b)")), writes=[skTb])
        for j in range(3):
            load_bcast(k, ABG[:, j, :], ABGb, modv_l[0:1, 3 + j, :], 1024)
        s.op("pool", lambda e: e.iota(out=iota_i[:], pattern=[[1, 16]], base=0, channel_multiplier=0), writes=[iota_ib])
        s.op("dve", lambda e: e.tensor_copy(out=iota[:], in_=iota_i[:]), reads=[iota_ib], writes=[iotab])

        def front(t):
            xt, xb = xts[t % 2]
            scr, scrb = scrs[t % 2]
            idx, idxb = idxs[t % 2]
            gate, gateb = gts[t % 2]
            hbf, hbfb = hbfs[0]
            hm, hmb = hms[t % 2]
            s.dma("sp", lambda e: e.dma_start(out=xt[:], in_=hin[t * 128:(t + 1) * 128, :]), reads=[hinb[t]], writes=[xb])
            norm_mod(k, xt, xb, ABG[:, 0, :], ABG[:, 1, :], ABGb, scr, scrb, st, stb, hbf, hbfb, out_f32=(hm, hmb))
            bank, bb = banks[4]
            transpose_chunks(k, bank, bb, lambda c: hbf[:, c * 128:(c + 1) * 128], 8, 128, hT[:], hTb, hbfb, dst_view=True)
            yield
            for g in range(4):
                qb_, qbb = banks[g]
                for c in range(8):
                    s.op("pe", lambda e: e.matmul(qb_[:, :], lhsT=hT[:, c, :], rhs=wq[:, c, g * 512:(g + 1) * 512], start=(c == 0), stop=(c == 7)),
                         reads=[hTb, wqb], writes=[qbb])
                s.op("act", lambda e: e.copy(out=qbf[:, g * 512:(g + 1) * 512], in_=qb_[:, :]), reads=[qbb], writes=[qbfb])
            yield
            for half in range(2):
                tb_, tbb = banks[4 + half]
                transpose_chunks(k, tb_, tbb, lambda c: qbf[:, (half * 8 + c) * 128:(half * 8 + c + 1) * 128], 8, 128,
                                 qT[:, half * 8:(half + 1) * 8, :], qTb, qbfb, dst_view=True)
            for g in range(4):
                sb_, sbb = banks[g]
                for j in range(4):
                    hp = g * 4 + j
                    s.op("pe", lambda e: e.matmul(sb_[:, j * 128:(j + 1) * 128], lhsT=qT[:, hp, :], rhs=skT[:, hp, :], start=True, stop=True),
                         reads=[qTb, skTb], writes=[sbb])
                s.op("act", lambda e: e.copy(out=ssb[:, g * 4:(g + 1) * 4, :], in_=sb_[:, :].rearrange("p (a b) -> p a b", b=128)), reads=[sbb], writes=[ssbb])
            yield
            for hp in range(16):
                s.op("dve", lambda e: e.max(out=m16[:, hp, 0:8], in_=ssb[:, hp, :]), reads=[ssbb], writes=[hpb[0][hp]])
            yield
            for hp in range(16):
                s.op("dve", lambda e: e.max_index(out=i16[:, hp, 0:8], in_max=m16[:, hp, 0:8], in_values=ssb[:, hp, :]),
                     reads=[ssbb, hpb[0][hp]], writes=[hpb[1][hp]])
            yield
            for hp in range(16):
                s.op("dve", lambda e: e.match_replace(out=s2[:, hp, :], in_to_replace=m16[:, hp, 0:8], in_values=ssb[:, hp, :], imm_value=-1e30),
                     reads=[ssbb, hpb[0][hp]], writes=[hpb[2][hp]])
            yield
            for hp in range(16):
                s.op("dve", lambda e: e.max(out=m16[:, hp, 8:16], in_=s2[:, hp, :]), reads=[hpb[2][hp]], writes=[hpb[0][hp]])
            yield
            for hp in range(16):
                s.op("dve", lambda e: e.max_index(out=i16[:, hp, 8:16], in_max=m16[:, hp, 8:16], in_values=s2[:, hp, :]),
                     reads=[hpb[2][hp], hpb[0][hp]], writes=[hpb[1][hp]])
            yield
            s.op("dve", lambda e: e.tensor_copy(out=i16f[:], in_=i16[:]), reads=hpb[1], writes=[i16fb])
            m4 = m16[:].rearrange("p (h t) a -> p h t a", t=2)
            s.op("dve", lambda e: e.tensor_tensor(out=cand[:].rearrange("p h (a b) -> p h a b", b=16),
                                                  in0=m4[:, :, 0, :].unsqueeze(3).to_broadcast([128, 8, 16, 16]),
                                                  in1=m4[:, :, 1, :].unsqueeze(2).to_broadcast([128, 8, 16, 16]), op=ALU.add),
                 reads=hpb[0], writes=[candb])
            yield
            for h in range(8):
                s.op("dve", lambda e: e.max(out=best[:, h, 0:8], in_=cand[:, h, :]), reads=[candb], writes=[hb[0][h]])
            for h in range(8):
                s.op("dve", lambda e: e.max_index(out=pos[:, h, 0:8], in_max=best[:, h, 0:8], in_values=cand[:, h, :]),
                     reads=[candb, hb[0][h]], writes=[hb[1][h]])
            yield
            for h in range(8):
                s.op("dve", lambda e: e.match_replace(out=cand2[:, h, :], in_to_replace=best[:, h, 0:8], in_values=cand[:, h, :], imm_value=-1e30),
                     reads=[candb, hb[0][h]], writes=[hb[2][h]])
            for h in range(8):
                s.op("dve", lambda e: e.max(out=best[:, h, 8:16], in_=cand2[:, h, :]), reads=[hb[2][h]], writes=[hb[0][h]])
            yield
            for h in range(8):
                s.op("dve", lambda e: e.max_index(out=pos[:, h, 8:16], in_max=best[:, h, 8:16], in_values=cand2[:, h, :]),
                     reads=[hb[2][h], hb[0][h]], writes=[hb[1][h]])
            posi = pos[:].rearrange("p h k -> p (h k)").bitcast(I32)
            s.op("dve", lambda e: e.tensor_single_scalar(out=ab_i[:, 0, :], in_=posi, scalar=4, op=ALU.arith_shift_right), reads=hb[1], writes=[ab_ib])
            s.op("dve", lambda e: e.tensor_single_scalar(out=ab_i[:, 1, :], in_=posi, scalar=15, op=ALU.bitwise_and), reads=hb[1], writes=[ab_ib])
            s.op("dve", lambda e: e.tensor_copy(out=ab_f[:], in_=ab_i[:]), reads=[ab_ib], writes=[ab_fb])
            yield
            i4 = i16f[:].rearrange("p (h t) a -> p h t a", t=2)
            for p_ in range(2):
                s.op("dve", lambda e: e.tensor_tensor(out=oh[:], in0=ab_f[:, p_, :].rearrange("p (h k) -> p h k", k=16).unsqueeze(3).to_broadcast([128, 8, 16, 16]),
                                                      in1=iota[:].unsqueeze(1).unsqueeze(1).to_broadcast([128, 8, 16, 16]), op=ALU.is_equal),
                     reads=[ab_fb, iotab], writes=[ohb])
                s.op("dve", lambda e: e.tensor_tensor(out=oh[:], in0=oh[:], in1=i4[:, :, p_, :].unsqueeze(2).to_broadcast([128, 8, 16, 16]), op=ALU.mult),
                     reads=[ohb, i16fb], writes=[ohb])
                s.op("dve", lambda e: e.tensor_reduce(out=e01[:, p_, :].rearrange("p (h k) -> p h k", k=16), in_=oh[:], axis=AX.X, op=ALU.add),
                     reads=[ohb], writes=[e01b])
                yield
            s.op("dve", lambda e: e.scalar_tensor_tensor(out=e01[:, 0, :], in0=e01[:, 0, :], scalar=128.0, in1=e01[:, 1, :], op0=ALU.mult, op1=ALU.add),
                 reads=[e01b], writes=[e01b])
            s.op("dve", lambda e: e.tensor_copy(out=idx[:], in_=e01[:, 0, :]), reads=[e01b], writes=[idxb])
            s.op("dve", lambda e: e.tensor_tensor(out=gate[:], in0=best[:], in1=best[:, :, 0:1].to_broadcast([128, 8, 16]), op=ALU.subtract),
                 reads=hb[0], writes=[gateb])
            s.op("act", lambda e: e.activation(out=gate[:], in_=gate[:], func=AF.Exp), reads=[gateb], writes=[gateb])
            s.op("dve", lambda e: e.tensor_reduce(out=gsum[:], in_=gate[:], axis=AX.X, op=ALU.add), reads=[gateb], writes=[gsumb])
            s.op("dve", lambda e: e.reciprocal(out=gsum[:], in_=gsum[:]), reads=[gsumb], writes=[gsumb])
            s.op("dve", lambda e: e.tensor_tensor(out=gate[:], in0=gate[:], in1=gsum[:].unsqueeze(2).to_broadcast([128, 8, 16]), op=ALU.mult),
                 reads=[gateb, gsumb], writes=[gateb])

        ring = [0]

        def back(t, fg):
            xt, xb = xts[t % 2]
            scr, scrb = scrs[t % 2]
            idx, idxb = idxs[t % 2]
            gate, gateb = gts[t % 2]
            hm, hmb = hms[t % 2]
            (o0, o0b), (o1, o1b) = banks[6], banks[7]
            for grp in range(16):
                held = []
                for j in range(8):
                    hk = grp * 8 + j
                    rw, rwb = rows[ring[0] % NROW2]
                    ring[0] += 1
                    s.dma("pool", lambda e: e.indirect_dma_start(out=rw[:], out_offset=None, in_=uv,
                                                                 in_offset=bass.IndirectOffsetOnAxis(ap=idx[:, hk:hk + 1], axis=0)),
                          reads=[idxb] + uvb, writes=[rwb])
                    s.op("dve", lambda e: e.scalar_tensor_tensor(out=junk[:], in0=rw[:, 0:1024], scalar=1.0, in1=hm[:], op0=ALU.mult, op1=ALU.mult,
                                                                 accum_out=actv[:, hk:hk + 1]), reads=[rwb, hmb], writes=[actb[hk % 16]])
                    held.append((hk, rw, rwb))
                g8 = slice(grp * 8, grp * 8 + 8)
                wb_ = wgb[grp % 4]
                s.op("act", lambda e: e.activation(out=wgt[:, g8], in_=actv[:, g8], func=AF.Gelu), reads=actb[(grp % 2) * 8:(grp % 2) * 8 + 8], writes=[wb_])
                s.op("dve", lambda e: e.tensor_tensor(out=wgt[:, g8], in0=wgt[:, g8], in1=gate[:].rearrange("p h k -> p (h k)")[:, g8], op=ALU.mult),
                     reads=[wb_, gateb], writes=[wb_])
                for (hk, rw, rwb) in held:
                    dg, dgb = dgs[hk % 4]
                    s.op("act", lambda e: e.activation(out=dg[:], in_=k.ident[:], func=AF.Copy, scale=wgt[:, hk:hk + 1]),
                         reads=[k.identb, wb_], writes=[dgb])
                    s.op("pe", lambda e: e.matmul(o0[:, :], lhsT=dg[:], rhs=rw[:, 1024:1536], start=(hk == 0), stop=(hk == 127)),
                         reads=[dgb, rwb], writes=[o0b])
                    s.op("pe", lambda e: e.matmul(o1[:, :], lhsT=dg[:], rhs=rw[:, 1536:2048], start=(hk == 0), stop=(hk == 127)),
                         reads=[dgb, rwb], writes=[o1b])
                if fg is not None:
                    next(fg, None)
            s.op("dve", lambda e: e.tensor_tensor(out=scr[:, 0:512], in0=o0[:, :], in1=ABG[:, 2, 0:512], op=ALU.mult), reads=[o0b, ABGb], writes=[scrb])
            s.op("dve", lambda e: e.tensor_tensor(out=scr[:, 512:1024], in0=o1[:, :], in1=ABG[:, 2, 512:1024], op=ALU.mult), reads=[o1b, ABGb], writes=[scrb])
            s.op("pool", lambda e: e.tensor_tensor(out=ot[:], in0=scr[:], in1=xt[:], op=ALU.add), reads=[scrb, xb], writes=[otb])
            s.dma("sp", lambda e: e.dma_start(out=hout[t * 128:(t + 1) * 128, :], in_=ot[:]), reads=[otb], writes=[houtb[t]])

        for _ in front(0):
            pass
        for t in range(NT):
            fg = front(t + 1) if t + 1 < NT else None
            back(t, fg)
            if fg is not None:
                for _ in fg:
                    pass
        s.barrier()
```

```python
import numpy as np
from contextlib import ExitStack
import concourse.bass as bass
import concourse.mybir as mybir
from concourse.bass_utils import run_bass_kernel_spmd

F32 = mybir.dt.float32
BF16 = mybir.dt.bfloat16
I32 = mybir.dt.int32
U32 = mybir.dt.uint32
ALU = mybir.AluOpType
AF = mybir.ActivationFunctionType
AX = mybir.AxisListType

D = 1024
SEQ = 2048
NT = SEQ // 128
CTX = 256
NCT = CTX // 128
EPS = 1e-6
NEG = -30000.0


class Buf:
    __slots__ = ("w", "r")

    def __init__(self):
        self.w = None
        self.r = {}


class Sched:
    RING = 12

    def __init__(self, nc, es):
        self.nc = nc
        self.eng = {"pe": nc.tensor, "act": nc.scalar, "dve": nc.vector, "pool": nc.gpsimd, "sp": nc.sync}
        self.semobj = {}
        self.cnt = {}
        for k in self.eng:
            self.semobj[k] = es.enter_context(nc.semaphore("s_" + k))
            self.cnt[k] = 0
        self.waited = {k: {} for k in self.eng}
        self.bulk = []
        self.dq = {}
        for q in ("sp", "pool", "act"):
            slots = []
            for i in range(self.RING):
                key = ("d", q, i)
                self.semobj[key] = es.enter_context(nc.semaphore(f"d_{q}_{i}"))
                slots.append(key)
            self.dq[q] = {"slots": slots, "uses": [0] * self.RING, "next": 0}

    def _wait(self, ek, tok):
        if tok is None:
            return
        sk, v = tok
        if self.waited[ek].get(sk, 0) >= v:
            return
        self.eng[ek].wait_ge(self.semobj[sk], v)
        self.waited[ek][sk] = v

    def _deps(self, ek, reads, writes):
        for b in reads:
            self._wait(ek, b.w)
        for b in writes:
            self._wait(ek, b.w)
            for sk, v in b.r.items():
                self._wait(ek, (sk, v))

    def _mark(self, tok, reads, writes):
        sk, v = tok
        for b in reads:
            if b.r.get(sk, 0) < v:
                b.r[sk] = v
        for b in writes:
            b.w = tok
            b.r = {}

    def op(self, ek, fn, reads=(), writes=()):
        self._deps(ek, reads, writes)
        ins = fn(self.eng[ek])
        self.cnt[ek] += 1
        ins.then_inc(self.semobj[ek], 1)
        tok = (ek, self.cnt[ek])
        self._mark(tok, reads, writes)
        return tok

    def dma(self, q, fn, reads=(), writes=()):
        dq = self.dq[q]
        slot = dq["next"]
        dq["next"] = (slot + 1) % self.RING
        key = dq["slots"][slot]
        uses = dq["uses"][slot]
        if uses:
            self._wait(q, (key, 16 * uses))
        self._deps(q, reads, writes)
        ins = fn(self.eng[q])
        ins.then_inc(self.semobj[key], 16)
        dq["uses"][slot] = uses + 1
        tok = (key, 16 * (uses + 1))
        self._mark(tok, reads, writes)
        return tok

    def bulk_dma(self, q, fn, reads=(), writes=(), es=None):
        key = ("bulk", len(self.semobj))
        self.semobj[key] = es.enter_context(self.nc.semaphore(f"bulk{len(self.semobj)}"))
        self._deps(q, reads, writes)
        ins = fn(self.eng[q])
        ins.then_inc(self.semobj[key], 16)
        tok = (key, 16)
        self.bulk.append(tok)
        self._mark(tok, reads, writes)
        return tok

    def barrier(self):
        toks = [(k, self.cnt[k]) for k in self.eng if self.cnt[k]]
        for q, dq in self.dq.items():
            for key, u in zip(dq["slots"], dq["uses"]):
                if u:
                    toks.append((key, 16 * u))
        toks.extend(self.bulk)
        for ek in self.eng:
            for t in toks:
                self._wait(ek, t)


class K:
    def __init__(self, nc, es):
        self.nc = nc
        self.es = es
        self.s = Sched(nc, es)
        self.banks = []
        for i in range(8):
            t = es.enter_context(nc.psum_tensor(f"bank{i}", [128, 512], F32))
            self.banks.append((t, Buf()))
        self.ident = None

    def sb(self, es, name, shape, dt):
        t = es.enter_context(self.nc.sbuf_tensor(name, list(shape), dt))
        return t, Buf()


def bcast_row(ap_row, parts):
    return ap_row.partition_broadcast(parts) if len(ap_row.shape) == 1 else ap_row.to_broadcast([parts, ap_row.shape[-1]])


def stage_ada(k, cc, w_ada, b_ada, g_norm, modv):
    nc, s = k.nc, k.s
    with ExitStack() as es:
        cct, ccb = k.sb(es, "ada_cc", [128, 16], F32)
        sil, silb = k.sb(es, "ada_sil", [128, 16], F32)
        wt = [k.sb(es, f"ada_w{i}", [128, 8, 512], F32) for i in range(2)]
        brow, browb = k.sb(es, "ada_b", [2, 6144], F32)
        grow, growb = k.sb(es, "ada_g", [2, 2, 1024], F32)
        mrow, mrowb = k.sb(es, "ada_m", [2, 6144], F32)
        orow, orowb = k.sb(es, "ada_o", [2, 6, 1024], F32)

        s.dma("sp", lambda e: e.dma_start(out=cct[:], in_=cc), writes=[ccb])
        s.op("act", lambda e: e.activation(out=sil[:], in_=cct[:], func=AF.Silu), reads=[ccb], writes=[silb])
        for l in range(2):
            s.dma("sp", lambda e: e.dma_start(out=brow[:], in_=b_ada[l:l + 1, :].to_broadcast([2, 6144])), writes=[browb])
            s.dma("sp", lambda e: e.dma_start(out=grow[:], in_=g_norm[2 * l:2 * l + 2, :].rearrange("(o a) d -> o a d", o=1).to_broadcast([2, 2, 1024])), writes=[growb])
            wv = w_ada[l].rearrange("(kc p) n -> p kc n", p=128)
            for g in range(12):
                wtile, wbuf = wt[g % 2]
                q = "sp" if g % 2 == 0 else "pool"
                s.dma(q, lambda e: e.dma_start(out=wtile[:], in_=wv[:, :, g * 512:(g + 1) * 512]), writes=[wbuf])
                bank, bb = k.banks[g % 2]
                for kc in range(8):
                    s.op("pe", lambda e: e.matmul(bank[0:2, :], lhsT=sil[:, 2 * kc:2 * kc + 2], rhs=wtile[:, kc, :],
                                                  start=(kc == 0), stop=(kc == 7)),
                         reads=[silb, wbuf], writes=[bb])
                s.op("dve", lambda e: e.tensor_tensor(out=mrow[:, g * 512:(g + 1) * 512], in0=bank[0:2, :],
                                                      in1=brow[:, g * 512:(g + 1) * 512], op=ALU.add),
                     reads=[bb, browb], writes=[mrowb])
            for j in range(2):
                sh = mrow[:, (3 * j) * 1024:(3 * j + 1) * 1024]
                sc = mrow[:, (3 * j + 1) * 1024:(3 * j + 2) * 1024]
                gt = mrow[:, (3 * j + 2) * 1024:(3 * j + 3) * 1024]
                s.op("dve", lambda e: e.scalar_tensor_tensor(out=orow[:, 3 * j, :], in0=sc, scalar=1.0, in1=grow[:, j, :],
                                                             op0=ALU.add, op1=ALU.mult),
                     reads=[mrowb, growb], writes=[orowb])
                s.op("dve", lambda e: e.tensor_copy(out=orow[:, 3 * j + 1, :], in_=sh), reads=[mrowb], writes=[orowb])
                s.op("dve", lambda e: e.tensor_copy(out=orow[:, 3 * j + 2, :], in_=gt), reads=[mrowb], writes=[orowb])
            s.dma("sp", lambda e: e.dma_start(out=modv[l], in_=orow[:]), reads=[orowb], writes=[k.modv_buf])
        s.barrier()


def load_cast(k, stg, dst, dstb, src, n, qi=0):
    s = k.s
    st, stb = stg[qi % len(stg)]
    s.dma("sp" if qi % 2 == 0 else "pool", lambda e: e.dma_start(out=st[:, 0:n], in_=src), writes=[stb])
    ek = ("act", "pool", "dve")[qi % 3]
    if ek == "act":
        s.op("act", lambda e: e.copy(out=dst, in_=st[:, 0:n]), reads=[stb], writes=[dstb])
    else:
        s.op(ek, lambda e: e.tensor_copy(out=dst, in_=st[:, 0:n]), reads=[stb], writes=[dstb])


def load_w_bf16(k, stg, wt, wb, wdram, kc, n, q0=0):
    qi = q0
    v = wdram.rearrange("(kc p) n -> p kc n", p=128)
    cw = stg[0][0].shape[1]
    for c in range(kc):
        for c0 in range(0, n, cw):
            c1 = min(n, c0 + cw)
            load_cast(k, stg, wt[:, c, c0:c1], wb, v[:, c, c0:c1], c1 - c0, qi)
            qi += 1
    return qi


def rstd_from_ss(k, ss, ssb, rs, rsb, inv_n, w):
    s = k.s
    s.op("act", lambda e: e.activation(out=rs[:, 0:w], in_=ss[:, 0:w], func=AF.Sqrt, scale=inv_n, bias=k.eps[:, 0:1]),
         reads=[ssb], writes=[rsb])
    s.op("dve", lambda e: e.reciprocal(out=rs[:, 0:w], in_=rs[:, 0:w]), reads=[rsb], writes=[rsb])


def norm_mod(k, xt, xb, A, B, ABb, scr, scrb, st, stb, out, outb, out_f32=None):
    s = k.s
    s.op("act", lambda e: e.activation(out=scr[:], in_=xt[:], func=AF.Square, accum_out=st[:, 0:1]),
         reads=[xb], writes=[scrb, stb])
    rstd_from_ss(k, st, stb, st, stb, 1.0 / D, 1)
    s.op("dve", lambda e: e.scalar_tensor_tensor(out=scr[:], in0=xt[:], scalar=st[:, 0:1], in1=A, op0=ALU.mult, op1=ALU.mult),
         reads=[xb, stb, ABb], writes=[scrb])
    if out_f32 is not None:
        of, ofb = out_f32
        s.op("pool", lambda e: e.tensor_tensor(out=of[:], in0=scr[:], in1=B, op=ALU.add), reads=[scrb, ABb], writes=[ofb])
        s.op("act", lambda e: e.copy(out=out[:], in_=of[:]), reads=[ofb], writes=[outb])
    else:
        s.op("pool", lambda e: e.tensor_tensor(out=out[:], in0=scr[:], in1=B, op=ALU.add), reads=[scrb, ABb], writes=[outb])


def transpose_chunks(k, bank, bankb, src_fn, nchunks, rows, dst, dstb, srcb, dst_view=None):
    s = k.s
    bv = bank[:].bitcast(BF16)
    for c in range(nchunks):
        s.op("pe", lambda e: e.transpose(out=bv[0:rows, c * 128:(c + 1) * 128], in_=src_fn(c), identity=k.ident[:]),
             reads=[srcb, k.identb], writes=[bankb])
    src = bv[0:rows, 0:nchunks * 128]
    if dst_view is not None:
        src = src.rearrange("p (c t) -> p c t", t=128)
    s.op("act", lambda e: e.copy(out=dst, in_=src), reads=[bankb], writes=[dstb])


def setup_consts(k, es, ident_d, identf_d=None):
    s = k.s
    k.ident, k.identb = k.sb(es, "ident_sb", [128, 128], BF16)
    k.eps, k.epsb = k.sb(es, "epsc", [128, 1], F32)
    s.dma("sp", lambda e: e.dma_start(out=k.ident[:], in_=ident_d), writes=[k.identb])
    if identf_d is not None:
        k.identf, _ = k.sb(es, "identf_sb", [128, 128], F32)
        s.dma("sp", lambda e: e.dma_start(out=k.identf[:], in_=identf_d), writes=[k.identb])
    s.op("dve", lambda e: e.memset(k.eps[:], EPS), writes=[k.epsb])


def load_bcast(k, tile, tb, row, n):
    k.s.dma("sp", lambda e: e.dma_start(out=tile, in_=row.to_broadcast([128, n])), writes=[tb])


def na_blocks(qt):
    if 2 <= qt <= 13:
        return [(qt - 2 + j, j) for j in range(5)]
    if qt == 0:
        return [(j, 5 + j) for j in range(4)]
    if qt == 1:
        return [(j, 9 + j) for j in range(4)]
    if qt == 14:
        return [(12 + j, 13 + j) for j in range(4)]
    return [(12 + j, 17 + j) for j in range(4)]


def stage_attn(k, x, ctx, modv0, W, hout, houtb):
    nc, s = k.nc, k.s
    banks = k.banks
    with ExitStack() as es:
        AB, ABb = k.sb(es, "at_AB", [128, 4, 1024], F32)
        osb, osbb = k.sb(es, "at_o", [128, NT, 1024], BF16)
        qT, qTb = k.sb(es, "at_qT", [96, 8, SEQ], BF16)
        kT, kTb = k.sb(es, "at_kT", [96, 8, SEQ + CTX], BF16)
        Vs, Vsb = k.sb(es, "at_V", [128, NT + NCT, 8, 65], BF16)
        st, stb = k.sb(es, "at_st", [128, 4], F32)
        st2, st2b = k.sb(es, "at_st2", [128, 16], F32)
        st3, st3b = k.sb(es, "at_st3", [128, 16], F32)
        xts = [k.sb(es, f"at_x{i}", [128, 1024], F32) for i in range(2)]
        scrs = [k.sb(es, f"at_scr{i}", [128, 1024], F32) for i in range(2)]
        abfs = [k.sb(es, f"at_a{i}", [128, 1024], BF16) for i in range(2)]
        aTs = [k.sb(es, f"at_aT{i}", [128, 8, 128], BF16) for i in range(2)]

        load_bcast(k, AB[:, 0, :], ABb, modv0[0:1, 0, :], 1024)
        load_bcast(k, AB[:, 1, :], ABb, modv0[0:1, 1, :], 1024)
        load_bcast(k, AB[:, 2, :], ABb, modv0[1:2, 0, :], 1024)
        load_bcast(k, AB[:, 3, :], ABb, modv0[1:2, 1, :], 1024)
        s.op("pool", lambda e: e.memset(Vs[:, :, :, 64:65], 1.0), writes=[Vsb])

        def src_tile(t):
            return ctx[t * 128:(t + 1) * 128, :] if t < NCT else x[(t - NCT) * 128:(t - NCT + 1) * 128, :]

        def front(t):
            xt, xb = xts[t % 2]
            scr, scrb = scrs[t % 2]
            abf, abfb = abfs[t % 2]
            aT, aTb = aTs[t % 2]
            s.dma("sp", lambda e: e.dma_start(out=xt[:], in_=src_tile(t)), writes=[xb])
            j = 2 if t < NCT else 0
            norm_mod(k, xt, xb, AB[:, j, :], AB[:, j + 1, :], ABb, scr, scrb, st, stb, abf, abfb)
            bank, bb = banks[7]
            transpose_chunks(k, bank, bb, lambda c: abf[:, c * 128:(c + 1) * 128], 8, 128, aT[:], aTb, abfb, dst_view=True)
            return aT, aTb, scr, scrb

        with ExitStack() as es1:
            stg = [k.sb(es1, f"p1_stg{i}", [128, 1024], F32) for i in range(2)]
            w_in, w_inb = k.sb(es1, "p1_win", [128, 8, 672], BF16)
            w_q, w_qb = k.sb(es1, "p1_wq", [128, 3, 768], BF16)
            w_kv, w_kvb = k.sb(es1, "p1_wkv", [128, 2, 1024], BF16)
            gcn, gcnb = k.sb(es1, "p1_gcn", [128, 640], F32)
            gq, gqb = k.sb(es1, "p1_gq", [128, 96], F32)
            gk, gkb = k.sb(es1, "p1_gk", [128, 96], F32)
            zsb, zsbb = k.sb(es1, "p1_z", [128, 672], F32)
            cn, cnb = k.sb(es1, "p1_cn", [128, 640], BF16)
            cnT, cnTb = k.sb(es1, "p1_cnT", [128, 5, 128], BF16)
            qn, qnb = k.sb(es1, "p1_qn", [128, 8, 96], F32)
            kn, knb = k.sb(es1, "p1_kn", [128, 8, 64], F32)
            qr, qrb = k.sb(es1, "p1_qr", [128, 8, 32], F32)
            rt, rtb = k.sb(es1, "p1_rt", [128, 4, 8, 16], F32)
            krg, krgb = k.sb(es1, "p1_krg", [128, 32], F32)
            krr, krrb = k.sb(es1, "p1_krr", [128, 32], F32)
            kt4, kt4b = k.sb(es1, "p1_kt4", [128, 4, 16], F32)
            qf, qfb = k.sb(es1, "p1_qf", [128, 8, 96], BF16)
            kf, kfb = k.sb(es1, "p1_kf", [128, 8, 96], BF16)
            ropes = [k.sb(es1, f"p1_rope{i}", [128, 32], F32) for i in range(2)]

            qi = 0
            wv = W["attn_w_in"].rearrange("(kc p) n -> p kc n", p=128)
            for c in range(8):
                load_cast(k, stg, w_in[:, c, :], w_inb, wv[:, c, 0:672], 672, qi); qi += 1
            wv = W["mla_w_q_up"].rearrange("(kc p) n -> p kc n", p=128)
            for c in range(3):
                load_cast(k, stg, w_q[:, c, :], w_qb, wv[:, c, :], 768, qi); qi += 1
            wv = W["mla_w_kv_up"].rearrange("(kc p) n -> p kc n", p=128)
            for c in range(2):
                load_cast(k, stg, w_kv[:, c, :], w_kvb, wv[:, c, :], 1024, qi); qi += 1
            load_bcast(k, gcn[:, 0:384], gcnb, W["mla_g_qa"], 384)
            load_bcast(k, gcn[:, 384:640], gcnb, W["mla_g_kva"], 256)
            load_bcast(k, gq[:], gqb, W["mla_g_q"], 96)
            load_bcast(k, gk[:], gkb, W["mla_g_k"], 96)
            s.op("dve", lambda e: e.tensor_scalar_mul(out=gq[:], in0=gq[:], scalar1=96.0 ** -0.5), reads=[gqb], writes=[gqb])

            for t in range(NT + NCT):
                lat = t >= NCT
                aT, aTb, scr, scrb = front(t)
                if lat:
                    rp, rpb = ropes[t % 2]
                    s.dma("sp", lambda e: e.dma_start(out=rp[:], in_=W["rope"][(t - NCT) * 128:(t - NCT + 1) * 128, :]), writes=[rpb])
                (z0, z0b), (z1, z1b) = banks[0], banks[1]
                for (zb, zbb, c0, c1) in ((z0, z0b, 0, 512), (z1, z1b, 512, 672)):
                    for c in range(8):
                        s.op("pe", lambda e: e.matmul(zb[:, 0:c1 - c0], lhsT=aT[:, c, :], rhs=w_in[:, c, c0:c1], start=(c == 0), stop=(c == 7)),
                             reads=[aTb, w_inb], writes=[zbb])
                    s.op("act", lambda e: e.copy(out=zsb[:, c0:c1], in_=zb[:, 0:c1 - c0]), reads=[zbb], writes=[zsbb])
                if lat:
                    s.op("act", lambda e: e.activation(out=scr[:, 0:384], in_=zsb[:, 0:384], func=AF.Square, accum_out=st2[:, 0:1]),
                         reads=[zsbb], writes=[scrb, st2b])
                    rstd_from_ss(k, st2, st2b, st3, st3b, 1.0 / 384, 1)
                    s.op("dve", lambda e: e.scalar_tensor_tensor(out=cn[:, 0:384], in0=zsb[:, 0:384], scalar=st3[:, 0:1], in1=gcn[:, 0:384],
                                                                 op0=ALU.mult, op1=ALU.mult), reads=[zsbb, st3b, gcnb], writes=[cnb])
                s.op("act", lambda e: e.activation(out=scr[:, 384:640], in_=zsb[:, 384:640], func=AF.Square, accum_out=st2[:, 1:2]),
                     reads=[zsbb], writes=[scrb, st2b])
                s.op("act", lambda e: e.activation(out=st3[:, 1:2], in_=st2[:, 1:2], func=AF.Sqrt, scale=1.0 / 256, bias=k.eps[:, 0:1]),
                     reads=[st2b], writes=[st3b])
                s.op("dve", lambda e: e.reciprocal(out=st3[:, 1:2], in_=st3[:, 1:2]), reads=[st3b], writes=[st3b])
                s.op("dve", lambda e: e.scalar_tensor_tensor(out=cn[:, 384:640], in0=zsb[:, 384:640], scalar=st3[:, 1:2], in1=gcn[:, 384:640],
                                                             op0=ALU.mult, op1=ALU.mult), reads=[zsbb, st3b, gcnb], writes=[cnb])
                s.op("act", lambda e: e.activation(out=scr[:, 640:672], in_=zsb[:, 640:672], func=AF.Square, accum_out=st2[:, 2:3]),
                     reads=[zsbb], writes=[scrb, st2b])
                c_lo = 0 if lat else 3
                bank, bb = banks[7]
                bv = bank[:].bitcast(BF16)
                for c in range(c_lo, 5):
                    s.op("pe", lambda e: e.transpose(out=bv[:, c * 128:(c + 1) * 128], in_=cn[:, c * 128:(c + 1) * 128], identity=k.ident[:]),
                         reads=[cnb, k.identb], writes=[bb])
                s.op("act", lambda e: e.copy(out=cnT[:, c_lo:5, :], in_=bv[:, c_lo * 128:640].rearrange("p (c t) -> p c t", t=128)),
                     reads=[bb], writes=[cnTb])
                if lat:
                    (qa, qab), (qb_, qbb) = banks[2], banks[3]
                    for (qk, qkb, h0, h1) in ((qa, qab, 0, 5), (qb_, qbb, 5, 8)):
                        n = (h1 - h0) * 96
                        for c in range(3):
                            s.op("pe", lambda e: e.matmul(qk[:, 0:n], lhsT=cnT[:, c, :], rhs=w_q[:, c, h0 * 96:h1 * 96], start=(c == 0), stop=(c == 2)),
                                 reads=[cnTb, w_qb], writes=[qkb])
                        s.op("act", lambda e: e.activation(out=scr[:, h0 * 96:h1 * 96], in_=qk[:, 0:n], func=AF.Square), reads=[qkb], writes=[scrb])
                    s.op("dve", lambda e: e.tensor_reduce(out=st2[:, 4:12], in_=scr[:, 0:768].rearrange("p (h d) -> p h d", d=96), axis=AX.X, op=ALU.add),
                         reads=[scrb], writes=[st2b])
                    s.op("act", lambda e: e.activation(out=st3[:, 4:12], in_=st2[:, 4:12], func=AF.Sqrt, scale=1.0 / 96, bias=k.eps[:, 0:1]),
                         reads=[st2b], writes=[st3b])
                    s.op("dve", lambda e: e.reciprocal(out=st3[:, 4:12], in_=st3[:, 4:12]), reads=[st3b], writes=[st3b])
                    for (qk, qkb, h0, h1) in ((qa, qab, 0, 5), (qb_, qbb, 5, 8)):
                        n = (h1 - h0) * 96
                        s.op("dve", lambda e: e.tensor_tensor(out=qn[:, h0:h1, :], in0=qk[:, 0:n].rearrange("p (h d) -> p h d", d=96),
                                                              in1=st3[:, 4 + h0:4 + h1].unsqueeze(2).to_broadcast([128, h1 - h0, 96]), op=ALU.mult),
                             reads=[qkb, st3b], writes=[qnb])
                    s.op("pool", lambda e: e.tensor_tensor(out=qf[:, :, 0:64], in0=qn[:, :, 0:64],
                                                           in1=gq[:, 0:64].unsqueeze(1).to_broadcast([128, 8, 64]), op=ALU.mult),
                         reads=[qnb, gqb], writes=[qfb])
                    s.op("dve", lambda e: e.tensor_tensor(out=qr[:], in0=qn[:, :, 64:96],
                                                          in1=gq[:, 64:96].unsqueeze(1).to_broadcast([128, 8, 32]), op=ALU.mult),
                         reads=[qnb, gqb], writes=[qrb])
                    cosb = rp[:, 0:16].unsqueeze(1).to_broadcast([128, 8, 16])
                    sinb = rp[:, 16:32].unsqueeze(1).to_broadcast([128, 8, 16])
                    s.op("dve", lambda e: e.tensor_tensor(out=rt[:, 0], in0=qr[:, :, 0:16], in1=cosb, op=ALU.mult), reads=[qrb, rpb], writes=[rtb])
                    s.op("dve", lambda e: e.tensor_tensor(out=rt[:, 1], in0=qr[:, :, 16:32], in1=sinb, op=ALU.mult), reads=[qrb, rpb], writes=[rtb])
                    s.op("dve", lambda e: e.tensor_tensor(out=rt[:, 2], in0=qr[:, :, 0:16], in1=sinb, op=ALU.mult), reads=[qrb, rpb], writes=[rtb])
                    s.op("dve", lambda e: e.tensor_tensor(out=rt[:, 3], in0=qr[:, :, 16:32], in1=cosb, op=ALU.mult), reads=[qrb, rpb], writes=[rtb])
                    s.op("dve", lambda e: e.tensor_tensor(out=qf[:, :, 64:80], in0=rt[:, 0], in1=rt[:, 1], op=ALU.subtract), reads=[rtb], writes=[qfb])
                    s.op("dve", lambda e: e.tensor_tensor(out=qf[:, :, 80:96], in0=rt[:, 2], in1=rt[:, 3], op=ALU.add), reads=[rtb], writes=[qfb])
                (ka, kab), (kb_, kbb) = banks[4], banks[5]
                for g, (kk, kkb) in enumerate(((ka, kab), (kb_, kbb))):
                    for c in range(2):
                        s.op("pe", lambda e: e.matmul(kk[:, :], lhsT=cnT[:, 3 + c, :], rhs=w_kv[:, c, g * 512:(g + 1) * 512], start=(c == 0), stop=(c == 1)),
                             reads=[cnTb, w_kvb], writes=[kkb])
                    kv3 = kk[:, :].rearrange("p (h d) -> p h d", d=128)
                    s.op("act", lambda e: e.activation(out=scr[:, g * 256:(g + 1) * 256].rearrange("p (h d) -> p h d", d=64), in_=kv3[:, :, 0:64], func=AF.Square),
                         reads=[kkb], writes=[scrb])
                    s.op("act", lambda e: e.copy(out=Vs[:, t, g * 4:(g + 1) * 4, 0:64], in_=kv3[:, :, 64:128]), reads=[kkb], writes=[Vsb])
                s.op("dve", lambda e: e.tensor_reduce(out=st2[:, 4:12], in_=scr[:, 0:512].rearrange("p (h d) -> p h d", d=64), axis=AX.X, op=ALU.add),
                     reads=[scrb], writes=[st2b])
                s.op("dve", lambda e: e.tensor_scalar(out=st2[:, 4:12], in0=st2[:, 4:12], scalar1=st2[:, 2:3], scalar2=None, op0=ALU.add),
                     reads=[st2b], writes=[st2b])
                s.op("act", lambda e: e.activation(out=st3[:, 4:12], in_=st2[:, 4:12], func=AF.Sqrt, scale=1.0 / 96, bias=k.eps[:, 0:1]),
                     reads=[st2b], writes=[st3b])
                s.op("dve", lambda e: e.reciprocal(out=st3[:, 4:12], in_=st3[:, 4:12]), reads=[st3b], writes=[st3b])
                for g, (kk, kkb) in enumerate(((ka, kab), (kb_, kbb))):
                    kv3 = kk[:, :].rearrange("p (h d) -> p h d", d=128)
                    s.op("dve", lambda e: e.tensor_tensor(out=kn[:, g * 4:(g + 1) * 4, :], in0=kv3[:, :, 0:64],
                                                          in1=st3[:, 4 + g * 4:8 + g * 4].unsqueeze(2).to_broadcast([128, 4, 64]), op=ALU.mult),
                         reads=[kkb, st3b], writes=[knb])
                s.op("pool", lambda e: e.tensor_tensor(out=kf[:, :, 0:64], in0=kn[:], in1=gk[:, 0:64].unsqueeze(1).to_broadcast([128, 8, 64]), op=ALU.mult),
                     reads=[knb, gkb], writes=[kfb])
                s.op("dve", lambda e: e.tensor_tensor(out=krg[:], in0=zsb[:, 640:672], in1=gk[:, 64:96], op=ALU.mult), reads=[zsbb, gkb], writes=[krgb])
                if lat:
                    s.op("dve", lambda e: e.tensor_tensor(out=kt4[:, 0], in0=krg[:, 0:16], in1=rp[:, 0:16], op=ALU.mult), reads=[krgb, rpb], writes=[kt4b])
                    s.op("dve", lambda e: e.tensor_tensor(out=kt4[:, 1], in0=krg[:, 16:32], in1=rp[:, 16:32], op=ALU.mult), reads=[krgb, rpb], writes=[kt4b])
                    s.op("dve", lambda e: e.tensor_tensor(out=kt4[:, 2], in0=krg[:, 0:16], in1=rp[:, 16:32], op=ALU.mult), reads=[krgb, rpb], writes=[kt4b])
                    s.op("dve", lambda e: e.tensor_tensor(out=kt4[:, 3], in0=krg[:, 16:32], in1=rp[:, 0:16], op=ALU.mult), reads=[krgb, rpb], writes=[kt4b])
                    s.op("dve", lambda e: e.tensor_tensor(out=krr[:, 0:16], in0=kt4[:, 0], in1=kt4[:, 1], op=ALU.subtract), reads=[kt4b], writes=[krrb])
                    s.op("dve", lambda e: e.tensor_tensor(out=krr[:, 16:32], in0=kt4[:, 2], in1=kt4[:, 3], op=ALU.add), reads=[kt4b], writes=[krrb])
                else:
                    s.op("dve", lambda e: e.tensor_copy(out=krr[:], in_=krg[:]), reads=[krgb], writes=[krrb])
                s.op("dve", lambda e: e.tensor_tensor(out=kf[:, :, 64:96], in0=krr[:].unsqueeze(1).to_broadcast([128, 8, 32]),
                                                      in1=st3[:, 4:12].unsqueeze(2).to_broadcast([128, 8, 32]), op=ALU.mult),
                     reads=[krrb, st3b], writes=[kfb])
                if lat:
                    tb_, tbb = banks[6]
                    tl = t - NCT
                    transpose_chunks(k, tb_, tbb, lambda h: qf[:, h, :], 8, 96, qT[:, :, tl * 128:(tl + 1) * 128], qTb, qfb, dst_view=True)
                tb_, tbb = banks[0]
                transpose_chunks(k, tb_, tbb, lambda h: kf[:, h, :], 8, 96, kT[:, :, t * 128:(t + 1) * 128], kTb, kfb, dst_view=True)
            s.barrier()

        with ExitStack() as es2:
            pTs = [k.sb(es2, f"p2_pT{i}", [128, 512], BF16) for i in range(3)]
            rc, rcb = k.sb(es2, "p2_rc", [128, 8], F32)
            steps = [(h, g, kt) for h in range(8) for g in range(4) for kt in range(NT + NCT)]

            def emit_s(i):
                h, g, kt = steps[i]
                sbk, sbkb = banks[i % 2]
                pT, pTb = pTs[i % 3]
                s.op("pe", lambda e: e.matmul(sbk[:, :], lhsT=kT[:, h, kt * 128:(kt + 1) * 128], rhs=qT[:, h, g * 512:(g + 1) * 512], start=True, stop=True),
                     reads=[kTb, qTb], writes=[sbkb])
                s.op("act", lambda e: e.activation(out=pT[:], in_=sbk[:, :], func=AF.Exp), reads=[sbkb], writes=[pTb])

            def emit_pv(i):
                h, g, kt = steps[i]
                pT, pTb = pTs[i % 3]
                for qs in range(4):
                    ob, obb = banks[2 + qs]
                    s.op("pe", lambda e: e.matmul(ob[:, 0:65], lhsT=pT[:, qs * 128:(qs + 1) * 128], rhs=Vs[:, kt, h, :],
                                                  start=(kt == 0), stop=(kt == NT + NCT - 1)), reads=[pTb, Vsb], writes=[obb])
                if kt == NT + NCT - 1:
                    for qs in range(4):
                        ob, obb = banks[2 + qs]
                        j = (g * 4 + qs) % 8
                        s.op("dve", lambda e: e.reciprocal(out=rc[:, j:j + 1], in_=ob[:, 64:65]), reads=[obb], writes=[rcb])
                        s.op("dve", lambda e: e.tensor_scalar(out=osb[:, g * 4 + qs, h * 64:(h + 1) * 64], in0=ob[:, 0:64], scalar1=rc[:, j:j + 1],
                                                              scalar2=None, op0=ALU.mult), reads=[obb, rcb], writes=[osbb])

            emit_s(0)
            for i in range(len(steps)):
                if i + 1 < len(steps):
                    emit_s(i + 1)
                emit_pv(i)
            s.barrier()

        with ExitStack() as es3:
            stg = [k.sb(es3, f"p3_stg{i}", [128, 1536], F32) for i in range(2)]
            w_in, w_inb = k.sb(es3, "p3_win", [128, 8, 1536], BF16)
            gqn, gqnb = k.sb(es3, "p3_gq", [128, 64], F32)
            gkn, gknb = k.sb(es3, "p3_gk", [128, 64], F32)
            tn, tnb = k.sb(es3, "p3_tn", [128, 16, 64], F32)
            qkf, qkfb = k.sb(es3, "p3_qkf", [128, 16, 64], BF16)
            wv = W["attn_w_in"].rearrange("(kc p) n -> p kc n", p=128)
            for c in range(8):
                load_cast(k, stg, w_in[:, c, :], w_inb, wv[:, c, 672:2208], 1536, c)
            load_bcast(k, gqn[:], gqnb, W["na_g_q"], 64)
            load_bcast(k, gkn[:], gknb, W["na_g_k"], 64)
            s.op("dve", lambda e: e.tensor_scalar_mul(out=gqn[:], in0=gqn[:], scalar1=64.0 ** -0.5), reads=[gqnb], writes=[gqnb])
            for t in range(NT + NCT):
                lat = t >= NCT
                aT, aTb, scr, scrb = front(t)
                grp = (0, 1, 2) if lat else (1, 2)
                for g in grp:
                    zb, zbb = banks[g]
                    for c in range(8):
                        s.op("pe", lambda e: e.matmul(zb[:, :], lhsT=aT[:, c, :], rhs=w_in[:, c, g * 512:(g + 1) * 512], start=(c == 0), stop=(c == 7)),
                             reads=[aTb, w_inb], writes=[zbb])
                    if g < 2:
                        s.op("act", lambda e: e.activation(out=scr[:, g * 512:(g + 1) * 512], in_=zb[:, :], func=AF.Square), reads=[zbb], writes=[scrb])
                    else:
                        s.op("act", lambda e: e.copy(out=Vs[:, t, :, 0:64], in_=zb[:, :].rearrange("p (h d) -> p h d", d=64)), reads=[zbb], writes=[Vsb])
                g0 = 0 if lat else 1
                s.op("dve", lambda e: e.tensor_reduce(out=st2[:, g0 * 8:16], in_=scr[:, g0 * 512:1024].rearrange("p (h d) -> p h d", d=64), axis=AX.X, op=ALU.add),
                     reads=[scrb], writes=[st2b])
                s.op("act", lambda e: e.activation(out=st3[:, g0 * 8:16], in_=st2[:, g0 * 8:16], func=AF.Sqrt, scale=1.0 / 64, bias=k.eps[:, 0:1]),
                     reads=[st2b], writes=[st3b])
                s.op("dve", lambda e: e.reciprocal(out=st3[:, g0 * 8:16], in_=st3[:, g0 * 8:16]), reads=[st3b], writes=[st3b])
                for g in grp[:-1]:
                    zb, zbb = banks[g]
                    gg, ggb = (gqn, gqnb) if g == 0 else (gkn, gknb)
                    s.op("dve", lambda e: e.tensor_tensor(out=tn[:, g * 8:(g + 1) * 8, :], in0=zb[:, :].rearrange("p (h d) -> p h d", d=64),
                                                          in1=st3[:, g * 8:(g + 1) * 8].unsqueeze(2).to_broadcast([128, 8, 64]), op=ALU.mult),
                         reads=[zbb, st3b], writes=[tnb])
                    s.op("pool", lambda e: e.tensor_tensor(out=qkf[:, g * 8:(g + 1) * 8, :], in0=tn[:, g * 8:(g + 1) * 8, :],
                                                           in1=gg[:].unsqueeze(1).to_broadcast([128, 8, 64]), op=ALU.mult),
                         reads=[tnb, ggb], writes=[qkfb])
                if lat:
                    tb_, tbb = banks[3]
                    tl = t - NCT
                    transpose_chunks(k, tb_, tbb, lambda h: qkf[:, h, :], 8, 64, qT[0:64, :, tl * 128:(tl + 1) * 128], qTb, qkfb, dst_view=True)
                tb_, tbb = banks[4]
                transpose_chunks(k, tb_, tbb, lambda h: qkf[:, 8 + h, :], 8, 64, kT[0:64, :, t * 128:(t + 1) * 128], kTb, qkfb, dst_view=True)
            s.barrier()

        with ExitStack() as es4:
            nbs = [k.sb(es4, f"p4_nb{i}", [128, 21, 128], F32) for i in range(2)]
            sfs = [k.sb(es4, f"p4_sf{i}", [128, 640], F32) for i in range(2)]
            pTs = [k.sb(es4, f"p4_pT{i}", [128, 896], BF16) for i in range(2)]
            rc, rcb = k.sb(es4, "p4_rc", [128, 8], F32)
            steps = [(h, qt) for h in range(8) for qt in range(NT)]

            def res(i):
                return (banks[(i % 2) * 2], banks[(i % 2) * 2 + 1], sfs[i % 2], pTs[i % 2], banks[4 + i % 2])

            def emit_s(i):
                h, qt = steps[i]
                nb, nbb = nbs[h % 2]
                if qt == 0:
                    s.dma("sp", lambda e: e.dma_start(out=nb[:], in_=W["nabias"][h]), writes=[nbb])
                blocks = na_blocks(qt)
                nloc = len(blocks)
                (sa, sab), (sb_, sbb), (sf, sfb), (pT, pTb), _ = res(i)
                qsl = qT[0:64, h, qt * 128:(qt + 1) * 128]
                for j, (kt, bi) in enumerate(blocks):
                    dstb_, dstbb = (sa, sab) if j < 4 else (sb_, sbb)
                    col = (j % 4) * 128
                    s.op("pe", lambda e: e.matmul(dstb_[:, col:col + 128], lhsT=kT[0:64, h, (NCT + kt) * 128:(NCT + kt + 1) * 128], rhs=qsl, start=True, stop=True),
                         reads=[kTb, qTb], writes=[dstbb])
                for c in range(NCT):
                    s.op("pe", lambda e: e.matmul(sb_[:, 128 + c * 128:256 + c * 128], lhsT=kT[0:64, h, c * 128:(c + 1) * 128], rhs=qsl, start=True, stop=True),
                         reads=[kTb, qTb], writes=[sbb])
                b0 = blocks[0][1]
                s.op("dve", lambda e: e.tensor_tensor(out=sf[:, 0:512], in0=sa[:, :], in1=nb[:, b0:b0 + 4, :].rearrange("p b q -> p (b q)"), op=ALU.add),
                     reads=[sab, nbb], writes=[sfb])
                if nloc == 5:
                    s.op("dve", lambda e: e.tensor_tensor(out=sf[:, 512:640], in0=sb_[:, 0:128], in1=nb[:, b0 + 4, :], op=ALU.add),
                         reads=[sbb, nbb], writes=[sfb])
                s.op("act", lambda e: e.activation(out=pT[:, 0:nloc * 128], in_=sf[:, 0:nloc * 128], func=AF.Exp), reads=[sfb], writes=[pTb])
                s.op("act", lambda e: e.activation(out=pT[:, 640:896], in_=sb_[:, 128:384], func=AF.Exp), reads=[sbb], writes=[pTb])

            def emit_pv(i):
                h, qt = steps[i]
                blocks = na_blocks(qt)
                _, _, _, (pT, pTb), (ob, obb) = res(i)
                for j, (kt, bi) in enumerate(blocks):
                    s.op("pe", lambda e: e.matmul(ob[:, 0:65], lhsT=pT[:, j * 128:(j + 1) * 128], rhs=Vs[:, NCT + kt, h, :], start=(j == 0), stop=False),
                         reads=[pTb, Vsb], writes=[obb])
                for c in range(NCT):
                    s.op("pe", lambda e: e.matmul(ob[:, 0:65], lhsT=pT[:, 640 + c * 128:768 + c * 128], rhs=Vs[:, c, h, :], start=False, stop=(c == NCT - 1)),
                         reads=[pTb, Vsb], writes=[obb])
                j8 = i % 8
                s.op("dve", lambda e: e.reciprocal(out=rc[:, j8:j8 + 1], in_=ob[:, 64:65]), reads=[obb], writes=[rcb])
                s.op("dve", lambda e: e.tensor_scalar(out=osb[:, qt, 512 + h * 64:512 + (h + 1) * 64], in0=ob[:, 0:64], scalar1=rc[:, j8:j8 + 1],
                                                      scalar2=None, op0=ALU.mult), reads=[obb, rcb], writes=[osbb])

            emit_s(0)
            for i in range(len(steps)):
                if i + 1 < len(steps):
                    emit_s(i + 1)
                emit_pv(i)
            s.barrier()

        with ExitStack() as es5:
            stg = [k.sb(es5, f"p5_stg{i}", [128, 1024], F32) for i in range(2)]
            w_o, w_ob = k.sb(es5, "p5_wo", [128, 8, 1024], BF16)
            G1, G1b = k.sb(es5, "p5_g1", [128, 1024], F32)
            outs = [k.sb(es5, f"p5_out{i}", [128, 1024], F32) for i in range(2)]
            wv = W["attn_w_out"].rearrange("(kc p) n -> p kc n", p=128)
            for c in range(8):
                load_cast(k, stg, w_o[:, c, :], w_ob, wv[:, c, :], 1024, c)
            load_bcast(k, G1[:], G1b, modv0[0:1, 2, :], 1024)
            for t in range(NT):
                xt, xb = xts[t % 2]
                scr, scrb = scrs[t % 2]
                aT, aTb = aTs[t % 2]
                ot, otb = outs[t % 2]
                s.dma("sp", lambda e: e.dma_start(out=xt[:], in_=x[t * 128:(t + 1) * 128, :]), writes=[xb])
                bank, bb = banks[7]
                transpose_chunks(k, bank, bb, lambda c: osb[:, t, c * 128:(c + 1) * 128], 8, 128, aT[:], aTb, osbb, dst_view=True)
                for g in range(2):
                    yb, ybb = banks[g]
                    for c in range(8):
                        s.op("pe", lambda e: e.matmul(yb[:, :], lhsT=aT[:, c, :], rhs=w_o[:, c, g * 512:(g + 1) * 512], start=(c == 0), stop=(c == 7)),
                             reads=[aTb, w_ob], writes=[ybb])
                    s.op("dve", lambda e: e.tensor_tensor(out=scr[:, g * 512:(g + 1) * 512], in0=yb[:, :], in1=G1[:, g * 512:(g + 1) * 512], op=ALU.mult),
                         reads=[ybb, G1b], writes=[scrb])
                s.op("pool", lambda e: e.tensor_tensor(out=ot[:], in0=scr[:], in1=xt[:], op=ALU.add), reads=[scrb, xb], writes=[otb])
                s.dma("sp", lambda e: e.dma_start(out=hout[t * 128:(t + 1) * 128, :], in_=ot[:]), reads=[otb], writes=[houtb[t]])
            s.barrier()


def host_rope_table():
    t = np.arange(SEQ)
    row = (t // 64).astype(np.float32)
    col = (t % 64).astype(np.float32)
    inv = (np.float32(1.0) / (np.float32(10000.0) ** (np.arange(8, dtype=np.float32) / np.float32(8)))).astype(np.float32)
    ang = np.concatenate([row[:, None] * inv, col[:, None] * inv], axis=-1).astype(np.float32)
    return np.concatenate([np.cos(ang), np.sin(ang)], axis=-1).astype(np.float32)


def host_na_bias(rpb):
    pairs = [(2, j) for j in range(5)] + [(0, j) for j in range(4)] + [(1, j) for j in range(4)] \
        + [(14, 12 + j) for j in range(4)] + [(15, 12 + j) for j in range(4)]
    out = np.full((8, 128, 21, 128), NEG, np.float32)
    p = np.arange(128)
    for bi, (qt, kt) in enumerate(pairs):
        tq = qt * 128 + p
        tk = kt * 128 + p
        r, c = tq // 64, tq % 64
        kr, kc = tk // 64, tk % 64
        r0 = np.clip(r - 4, 0, 24)
        c0 = np.clip(c - 8, 0, 48)
        inside = (kr[:, None] >= r0[None, :]) & (kr[:, None] < r0[None, :] + 8) & (kc[:, None] >= c0[None, :]) & (kc[:, None] < c0[None, :] + 16)
        rr = np.clip(kr[:, None] - r[None, :] + 7, 0, 14)
        rc = np.clip(kc[:, None] - c[None, :] + 15, 0, 30)
        vals = rpb[:, rr, rc]
        out[:, :, bi, :] = np.where(inside[None], vals, np.float32(NEG))
    return out


NROW = 8


def stage_peer(k, hin, hinb, modv_l, w_query, skT_d, u_tab, v_tab, hout, houtb, tag):
    nc, s = k.nc, k.s
    banks = k.banks
    with ExitStack() as es:
        stg = [k.sb(es, f"{tag}_stg{i}", [128, 2048], F32) for i in range(2)]
        wq, wqb = k.sb(es, f"{tag}_wq", [128, 8, 2048], BF16)
        skT, skTb = k.sb(es, f"{tag}_skT", [128, 16, 128], BF16)
        ABG, ABGb = k.sb(es, f"{tag}_ABG", [128, 3, 1024], F32)
        iota_i, iota_ib = k.sb(es, f"{tag}_iotai", [128, 16], I32)
        iota, iotab = k.sb(es, f"{tag}_iota", [128, 16], F32)
        st, stb = k.sb(es, f"{tag}_st", [128, 4], F32)
        xts = [k.sb(es, f"{tag}_x{i}", [128, 1024], F32) for i in range(2)]
        scrs = [k.sb(es, f"{tag}_scr{i}", [128, 1024], F32) for i in range(2)]
        hms = [k.sb(es, f"{tag}_hm{i}", [128, 1024], F32) for i in range(2)]
        hbf, hbfb = k.sb(es, f"{tag}_hbf", [128, 1024], BF16)
        hT, hTb = k.sb(es, f"{tag}_hT", [128, 8, 128], BF16)
        qbf, qbfb = k.sb(es, f"{tag}_qbf", [128, 2048], BF16)
        qT, qTb = k.sb(es, f"{tag}_qT", [128, 16, 128], BF16)
        ssb, ssbb = k.sb(es, f"{tag}_s", [128, 16, 128], F32)
        s2, _ = k.sb(es, f"{tag}_s2", [128, 16, 128], F32)
        m16, _ = k.sb(es, f"{tag}_m16", [128, 16, 16], F32)
        i16, _ = k.sb(es, f"{tag}_i16", [128, 16, 16], U32)
        i16f, i16fb = k.sb(es, f"{tag}_i16f", [128, 16, 16], F32)
        cand, candb = k.sb(es, f"{tag}_cand", [128, 8, 256], F32)
        cand2, _ = k.sb(es, f"{tag}_cand2", [128, 8, 256], F32)
        best, _ = k.sb(es, f"{tag}_best", [128, 8, 16], F32)
        pos, _ = k.sb(es, f"{tag}_pos", [128, 8, 16], U32)
        ab_i, ab_ib = k.sb(es, f"{tag}_abi", [128, 2, 128], I32)
        ab_f, ab_fb = k.sb(es, f"{tag}_abf", [128, 2, 128], F32)
        oh, ohb = k.sb(es, f"{tag}_oh", [128, 8, 16, 16], F32)
        e01, e01b = k.sb(es, f"{tag}_e01", [128, 2, 128], F32)
        idxs = [k.sb(es, f"{tag}_idx{i}", [128, 128], I32) for i in range(2)]
        gts = [k.sb(es, f"{tag}_gate{i}", [128, 8, 16], F32) for i in range(2)]
        gsum, gsumb = k.sb(es, f"{tag}_gsum", [128, 8], F32)
        actv, _ = k.sb(es, f"{tag}_act", [128, 128], F32)
        wgt, wgtb = k.sb(es, f"{tag}_wgt", [128, 128], F32)
        junk, _ = k.sb(es, f"{tag}_junk", [128, 1024], BF16)
        rows = [k.sb(es, f"{tag}_row{i}", [128, 1024], F32) for i in range(NROW)]
        accs = [k.sb(es, f"{tag}_acc{i}", [128, 1024], F32) for i in range(4)]
        ot, otb = k.sb(es, f"{tag}_ot", [128, 1024], F32)
        hpb = [[Buf() for _ in range(16)] for _ in range(3)]
        hb = [[Buf() for _ in range(8)] for _ in range(3)]
        actb = [Buf() for _ in range(16)]

        qi = load_w_bf16(k, stg, wq, wqb, w_query, 8, 2048)
        load_cast(k, stg, skT[:].rearrange("p a b -> p (a b)"), skTb, skT_d.rearrange("p a b -> p (a b)"), 2048, qi)
        for j in range(3):
            load_bcast(k, ABG[:, j, :], ABGb, modv_l[0:1, 3 + j, :], 1024)
        s.op("pool", lambda e: e.iota(out=iota_i[:], pattern=[[1, 16]], base=0, channel_multiplier=0), writes=[iota_ib])
        s.op("dve", lambda e: e.tensor_copy(out=iota[:], in_=iota_i[:]), reads=[iota_ib], writes=[iotab])

        def front(t):
            xt, xb = xts[t % 2]
            scr, scrb = scrs[t % 2]
            hm, hmb = hms[t % 2]
            idx, idxb = idxs[t % 2]
            gate, gateb = gts[t % 2]
            s.dma("sp", lambda e: e.dma_start(out=xt[:], in_=hin[t * 128:(t + 1) * 128, :]), reads=[hinb[t]], writes=[xb])
            norm_mod(k, xt, xb, ABG[:, 0, :], ABG[:, 1, :], ABGb, scr, scrb, st, stb, hbf, hbfb, out_f32=(hm, hmb))
            bank, bb = banks[7]
            transpose_chunks(k, bank, bb, lambda c: hbf[:, c * 128:(c + 1) * 128], 8, 128, hT[:], hTb, hbfb, dst_view=True)
            for g in range(4):
                qb_, qbb = banks[g]
                for c in range(8):
                    s.op("pe", lambda e: e.matmul(qb_[:, :], lhsT=hT[:, c, :], rhs=wq[:, c, g * 512:(g + 1) * 512], start=(c == 0), stop=(c == 7)),
                         reads=[hTb, wqb], writes=[qbb])
                s.op("act", lambda e: e.copy(out=qbf[:, g * 512:(g + 1) * 512], in_=qb_[:, :]), reads=[qbb], writes=[qbfb])
            for half in range(2):
                tb_, tbb = banks[4 + half]
                transpose_chunks(k, tb_, tbb, lambda c: qbf[:, (half * 8 + c) * 128:(half * 8 + c + 1) * 128], 8, 128,
                                 qT[:, half * 8:(half + 1) * 8, :], qTb, qbfb, dst_view=True)
            for g in range(4):
                sb_, sbb = banks[g]
                for j in range(4):
                    hp = g * 4 + j
                    s.op("pe", lambda e: e.matmul(sb_[:, j * 128:(j + 1) * 128], lhsT=qT[:, hp, :], rhs=skT[:, hp, :], start=True, stop=True),
                         reads=[qTb, skTb], writes=[sbb])
                s.op("act", lambda e: e.copy(out=ssb[:, g * 4:(g + 1) * 4, :], in_=sb_[:, :].rearrange("p (a b) -> p a b", b=128)), reads=[sbb], writes=[ssbb])
            for hp in range(16):
                s.op("dve", lambda e: e.max(out=m16[:, hp, 0:8], in_=ssb[:, hp, :]), reads=[ssbb], writes=[hpb[0][hp]])
            for hp in range(16):
                s.op("dve", lambda e: e.max_index(out=i16[:, hp, 0:8], in_max=m16[:, hp, 0:8], in_values=ssb[:, hp, :]),
                     reads=[ssbb, hpb[0][hp]], writes=[hpb[1][hp]])
            for hp in range(16):
                s.op("dve", lambda e: e.match_replace(out=s2[:, hp, :], in_to_replace=m16[:, hp, 0:8], in_values=ssb[:, hp, :], imm_value=-1e30),
                     reads=[ssbb, hpb[0][hp]], writes=[hpb[2][hp]])
            for hp in range(16):
                s.op("dve", lambda e: e.max(out=m16[:, hp, 8:16], in_=s2[:, hp, :]), reads=[hpb[2][hp]], writes=[hpb[0][hp]])
            for hp in range(16):
                s.op("dve", lambda e: e.max_index(out=i16[:, hp, 8:16], in_max=m16[:, hp, 8:16], in_values=s2[:, hp, :]),
                     reads=[hpb[2][hp], hpb[0][hp]], writes=[hpb[1][hp]])
            s.op("dve", lambda e: e.tensor_copy(out=i16f[:], in_=i16[:]), reads=hpb[1], writes=[i16fb])
            m4 = m16[:].rearrange("p (h t) a -> p h t a", t=2)
            s.op("dve", lambda e: e.tensor_tensor(out=cand[:].rearrange("p h (a b) -> p h a b", b=16),
                                                  in0=m4[:, :, 0, :].unsqueeze(3).to_broadcast([128, 8, 16, 16]),
                                                  in1=m4[:, :, 1, :].unsqueeze(2).to_broadcast([128, 8, 16, 16]), op=ALU.add),
                 reads=hpb[0], writes=[candb])
            for h in range(8):
                s.op("dve", lambda e: e.max(out=best[:, h, 0:8], in_=cand[:, h, :]), reads=[candb], writes=[hb[0][h]])
            for h in range(8):
                s.op("dve", lambda e: e.max_index(out=pos[:, h, 0:8], in_max=best[:, h, 0:8], in_values=cand[:, h, :]),
                     reads=[candb, hb[0][h]], writes=[hb[1][h]])
            for h in range(8):
                s.op("dve", lambda e: e.match_replace(out=cand2[:, h, :], in_to_replace=best[:, h, 0:8], in_values=cand[:, h, :], imm_value=-1e30),
                     reads=[candb, hb[0][h]], writes=[hb[2][h]])
            for h in range(8):
                s.op("dve", lambda e: e.max(out=best[:, h, 8:16], in_=cand2[:, h, :]), reads=[hb[2][h]], writes=[hb[0][h]])
            for h in range(8):
                s.op("dve", lambda e: e.max_index(out=pos[:, h, 8:16], in_max=best[:, h, 8:16], in_values=cand2[:, h, :]),
                     reads=[hb[2][h], hb[0][h]], writes=[hb[1][h]])
            posi = pos[:].rearrange("p h k -> p (h k)").bitcast(I32)
            s.op("dve", lambda e: e.tensor_single_scalar(out=ab_i[:, 0, :], in_=posi, scalar=4, op=ALU.arith_shift_right), reads=hb[1], writes=[ab_ib])
            s.op("dve", lambda e: e.tensor_single_scalar(out=ab_i[:, 1, :], in_=posi, scalar=15, op=ALU.bitwise_and), reads=hb[1], writes=[ab_ib])
            s.op("dve", lambda e: e.tensor_copy(out=ab_f[:], in_=ab_i[:]), reads=[ab_ib], writes=[ab_fb])
            i4 = i16f[:].rearrange("p (h t) a -> p h t a", t=2)
            for p_ in range(2):
                s.op("dve", lambda e: e.tensor_tensor(out=oh[:], in0=ab_f[:, p_, :].rearrange("p (h k) -> p h k", k=16).unsqueeze(3).to_broadcast([128, 8, 16, 16]),
                                                      in1=iota[:].unsqueeze(1).unsqueeze(1).to_broadcast([128, 8, 16, 16]), op=ALU.is_equal),
                     reads=[ab_fb, iotab], writes=[ohb])
                s.op("dve", lambda e: e.tensor_tensor(out=oh[:], in0=oh[:], in1=i4[:, :, p_, :].unsqueeze(2).to_broadcast([128, 8, 16, 16]), op=ALU.mult),
                     reads=[ohb, i16fb], writes=[ohb])
                s.op("dve", lambda e: e.tensor_reduce(out=e01[:, p_, :].rearrange("p (h k) -> p h k", k=16), in_=oh[:], axis=AX.X, op=ALU.add),
                     reads=[ohb], writes=[e01b])
            s.op("dve", lambda e: e.scalar_tensor_tensor(out=e01[:, 0, :], in0=e01[:, 0, :], scalar=128.0, in1=e01[:, 1, :], op0=ALU.mult, op1=ALU.add),
                 reads=[e01b], writes=[e01b])
            s.op("dve", lambda e: e.tensor_copy(out=idx[:], in_=e01[:, 0, :]), reads=[e01b], writes=[idxb])
            s.op("dve", lambda e: e.tensor_tensor(out=gate[:], in0=best[:], in1=best[:, :, 0:1].to_broadcast([128, 8, 16]), op=ALU.subtract),
                 reads=hb[0], writes=[gateb])
            s.op("act", lambda e: e.activation(out=gate[:], in_=gate[:], func=AF.Exp), reads=[gateb], writes=[gateb])
            s.op("dve", lambda e: e.tensor_reduce(out=gsum[:], in_=gate[:], axis=AX.X, op=ALU.add), reads=[gateb], writes=[gsumb])
            s.op("dve", lambda e: e.reciprocal(out=gsum[:], in_=gsum[:]), reads=[gsumb], writes=[gsumb])
            s.op("dve", lambda e: e.tensor_tensor(out=gate[:], in0=gate[:], in1=gsum[:].unsqueeze(2).to_broadcast([128, 8, 16]), op=ALU.mult),
                 reads=[gateb, gsumb], writes=[gateb])

        ring = [0]

        def gather(tab, idx, idxb, hk):
            rw, rwb = rows[ring[0] % NROW]
            ring[0] += 1
            s.dma("pool", lambda e: e.indirect_dma_start(out=rw[:], out_offset=None, in_=tab,
                                                         in_offset=bass.IndirectOffsetOnAxis(ap=idx[:, hk:hk + 1], axis=0)),
                  reads=[idxb], writes=[rwb])
            return rw, rwb

        def back(t):
            xt, xb = xts[t % 2]
            scr, scrb = scrs[t % 2]
            hm, hmb = hms[t % 2]
            idx, idxb = idxs[t % 2]
            gate, gateb = gts[t % 2]
            for hk in range(128):
                rw, rwb = gather(u_tab, idx, idxb, hk)
                s.op("dve", lambda e: e.scalar_tensor_tensor(out=junk[:], in0=rw[:], scalar=1.0, in1=hm[:], op0=ALU.mult, op1=ALU.mult,
                                                             accum_out=actv[:, hk:hk + 1]), reads=[rwb, hmb], writes=[actb[hk % 16]])
            s.op("act", lambda e: e.activation(out=wgt[:], in_=actv[:], func=AF.Gelu), reads=actb, writes=[wgtb])
            s.op("dve", lambda e: e.tensor_tensor(out=wgt[:], in0=wgt[:], in1=gate[:].rearrange("p h k -> p (h k)"), op=ALU.mult),
                 reads=[wgtb, gateb], writes=[wgtb])
            for hk in range(128):
                rw, rwb = gather(v_tab, idx, idxb, hk)
                ac, acb = accs[hk % 4]
                if hk < 4:
                    s.op("dve", lambda e: e.tensor_scalar(out=ac[:], in0=rw[:], scalar1=wgt[:, hk:hk + 1], scalar2=None, op0=ALU.mult),
                         reads=[rwb, wgtb], writes=[acb])
                else:
                    s.op("dve", lambda e: e.scalar_tensor_tensor(out=ac[:], in0=rw[:], scalar=wgt[:, hk:hk + 1], in1=ac[:], op0=ALU.mult, op1=ALU.add),
                         reads=[rwb, wgtb, acb], writes=[acb])
            (a0, a0b), (a1, a1b), (a2, a2b), (a3, a3b) = accs
            s.op("pool", lambda e: e.tensor_tensor(out=a0[:], in0=a0[:], in1=a1[:], op=ALU.add), reads=[a0b, a1b], writes=[a0b])
            s.op("pool", lambda e: e.tensor_tensor(out=a2[:], in0=a2[:], in1=a3[:], op=ALU.add), reads=[a2b, a3b], writes=[a2b])
            s.op("pool", lambda e: e.tensor_tensor(out=a0[:], in0=a0[:], in1=a2[:], op=ALU.add), reads=[a0b, a2b], writes=[a0b])
            s.op("dve", lambda e: e.tensor_tensor(out=scr[:], in0=a0[:], in1=ABG[:, 2, :], op=ALU.mult), reads=[a0b, ABGb], writes=[scrb])
            s.op("pool", lambda e: e.tensor_tensor(out=ot[:], in0=scr[:], in1=xt[:], op=ALU.add), reads=[scrb, xb], writes=[otb])
            s.dma("sp", lambda e: e.dma_start(out=hout[t * 128:(t + 1) * 128, :], in_=ot[:]), reads=[otb], writes=[houtb[t]])

        front(0)
        for t in range(NT):
            if t + 1 < NT:
                front(t + 1)
            back(t)
        s.barrier()


def stage_conv(k, hin, hinb, modv_l, W, hout, houtb):
    nc, s = k.nc, k.s
    banks = k.banks
    PADW = SEQ + 30
    with ExitStack() as es:
        cbuf, cbufb = k.sb(es, "cv_cbuf", [128, NT, 1024], F32)
        ABG, ABGb = k.sb(es, "cv_ABG", [128, 2, 1024], F32)
        st, stb = k.sb(es, "cv_st", [128, 8], F32)
        xts = [k.sb(es, f"cv_x{i}", [128, 1024], F32) for i in range(2)]
        scrs = [k.sb(es, f"cv_scr{i}", [128, 1024], F32) for i in range(2)]
        for j in range(2):
            load_bcast(k, ABG[:, j, :], ABGb, modv_l[0:1, j, :], 1024)
        cbt = [Buf() for _ in range(NT // 4)]
        with ExitStack() as es1:
            stg = [k.sb(es1, f"cv_stg{i}", [128, 1024], F32) for i in range(2)]
            aTa, aTab = k.sb(es1, "cv_aT", [128, 8, SEQ], BF16)
            w1, w1b = k.sb(es1, "cv_w1", [128, 8, 2048], BF16)
            b1T, b1Tb = k.sb(es1, "cv_b1T", [128, 16], F32)
            wdw, wdwb = k.sb(es1, "cv_wdw", [128, 8, 31], F32)
            bdw, bdwb = k.sb(es1, "cv_bdw", [128, 8], F32)
            abfs = [k.sb(es1, f"cv_a{i}", [128, 1024], BF16) for i in range(2)]
            upads = [k.sb(es1, f"cv_up{i}", [128, PADW], F32) for i in range(2)]
            accs = [k.sb(es1, f"cv_acc{i}", [128, SEQ], F32) for i in range(2)]
            sgs = [k.sb(es1, f"cv_sg{i}", [128, 512], F32) for i in range(2)]
            load_w_bf16(k, stg, w1, w1b, W["conv_w_pw1"], 8, 2048)
            s.dma("sp", lambda e: e.dma_start(out=b1T[:], in_=W["conv_b1T"]), writes=[b1Tb])
            s.dma("sp", lambda e: e.dma_start(out=wdw[:], in_=W["conv_wdwT"]), writes=[wdwb])
            s.dma("sp", lambda e: e.dma_start(out=bdw[:], in_=W["conv_bdwT"]), writes=[bdwb])
            for up, upb in upads:
                s.op("pool", lambda e: e.memset(up[:, 0:15], 0.0), writes=[upb])
                s.op("pool", lambda e: e.memset(up[:, 15 + SEQ:PADW], 0.0), writes=[upb])
            for t in range(NT):
                xt, xb = xts[t % 2]
                scr, scrb = scrs[t % 2]
                abf, abfb = abfs[t % 2]
                s.dma("sp", lambda e: e.dma_start(out=xt[:], in_=hin[t * 128:(t + 1) * 128, :]), reads=[hinb[t]], writes=[xb])
                norm_mod(k, xt, xb, ABG[:, 0, :], ABG[:, 1, :], ABGb, scr, scrb, st, stb, abf, abfb)
                bank, bb = banks[6 + t % 2]
                transpose_chunks(k, bank, bb, lambda c: abf[:, c * 128:(c + 1) * 128], 8, 128, aTa[:, :, t * 128:(t + 1) * 128], aTab, abfb, dst_view=True)
            accbs = [[Buf() for _ in range(4)] for _ in range(2)]
            it = 0
            for m in range(8):
                up, upb = upads[m % 2]
                acc, _ = accs[m % 2]
                accb = accbs[m % 2]
                for tg in range(4):
                    (bv_, bvb), (bg_, bgb) = banks[(it % 2) * 2], banks[(it % 2) * 2 + 1]
                    sg, sgb = sgs[it % 2]
                    it += 1
                    for (bk, bkb, c0) in ((bv_, bvb, m * 128), (bg_, bgb, 1024 + m * 128)):
                        for c in range(8):
                            s.op("pe", lambda e: e.matmul(bk[:, :], lhsT=w1[:, c, c0:c0 + 128], rhs=aTa[:, c, tg * 512:(tg + 1) * 512], start=(c == 0), stop=(c == 7)),
                                 reads=[w1b, aTab], writes=[bkb])
                    s.op("act", lambda e: e.activation(out=sg[:], in_=bg_[:, :], func=AF.Sigmoid, bias=b1T[:, 8 + m:9 + m]), reads=[bgb, b1Tb], writes=[sgb])
                    s.op("dve", lambda e: e.scalar_tensor_tensor(out=up[:, 15 + tg * 512:15 + (tg + 1) * 512], in0=bv_[:, :], scalar=b1T[:, m:m + 1], in1=sg[:],
                                                                 op0=ALU.add, op1=ALU.mult), reads=[bvb, sgb, b1Tb], writes=[upb])
                for j in range(31):
                    for ch in range(4):
                        src = up[:, ch * 512 + j:ch * 512 + j + 512]
                        dst = acc[:, ch * 512:(ch + 1) * 512]
                        if j == 0:
                            s.op("dve", lambda e: e.tensor_scalar(out=dst, in0=src, scalar1=wdw[:, m, 0:1], scalar2=bdw[:, m:m + 1], op0=ALU.mult, op1=ALU.add),
                                 reads=[upb, wdwb, bdwb], writes=[accb[ch]])
                        else:
                            s.op("dve", lambda e: e.scalar_tensor_tensor(out=dst, in0=src, scalar=wdw[:, m, j:j + 1], in1=dst, op0=ALU.mult, op1=ALU.add),
                                 reads=[upb, wdwb, accb[ch]], writes=[accb[ch]])
                for g in range(NT // 4):
                    tb_, tbb = banks[4 + g % 2]
                    for j in range(4):
                        t = g * 4 + j
                        s.op("pe", lambda e: e.transpose(out=tb_[:, j * 128:(j + 1) * 128], in_=acc[:, t * 128:(t + 1) * 128], identity=k.identf[:]),
                             reads=[accb[t // 4], k.identb], writes=[tbb])
                    s.op("act", lambda e: e.copy(out=cbuf[:, g * 4:(g + 1) * 4, m * 128:(m + 1) * 128], in_=tb_[:, :].rearrange("p (a b) -> p a b", b=128)),
                         reads=[tbb], writes=[cbt[g]])
            s.barrier()
        with ExitStack() as es3:
            stg = [k.sb(es3, f"cv3_stg{i}", [128, 1024], F32) for i in range(2)]
            w2, w2b = k.sb(es3, "cv3_w2", [128, 8, 1024], BF16)
            gl, glb = k.sb(es3, "cv3_gl", [128, 4, 1024], F32)
            sbfs = [k.sb(es3, f"cv3_s{i}", [128, 1024], BF16) for i in range(2)]
            sTs = [k.sb(es3, f"cv3_sT{i}", [128, 8, 128], BF16) for i in range(2)]
            outs = [k.sb(es3, f"cv3_o{i}", [128, 1024], F32) for i in range(2)]
            wv = W["conv_w_pw2"].rearrange("(kc p) n -> p kc n", p=128)
            for c in range(8):
                load_cast(k, stg, w2[:, c, :], w2b, wv[:, c, :], 1024, c)
            load_bcast(k, gl[:, 0, :], glb, W["conv_g_ln"], 1024)
            load_bcast(k, gl[:, 1, :], glb, W["conv_b_ln"], 1024)
            load_bcast(k, gl[:, 2, :], glb, W["conv_b_pw2"], 1024)
            load_bcast(k, gl[:, 3, :], glb, modv_l[0:1, 2, :], 1024)
            for t in range(NT):
                xt, xb = xts[t % 2]
                scr, scrb = scrs[t % 2]
                sbf, sbfb = sbfs[t % 2]
                sT, sTb = sTs[t % 2]
                ot, otb = outs[t % 2]
                cb = cbt[t // 4]
                c_t = cbuf[:, t, :]
                s.dma("sp", lambda e: e.dma_start(out=xt[:], in_=hin[t * 128:(t + 1) * 128, :]), reads=[hinb[t]], writes=[xb])
                s.op("act", lambda e: e.activation(out=scr[:], in_=c_t, func=AF.Identity, accum_out=st[:, 0:1]), reads=[cb], writes=[scrb, stb])
                s.op("act", lambda e: e.activation(out=scr[:], in_=c_t, func=AF.Square, accum_out=st[:, 1:2]), reads=[cb], writes=[scrb, stb])
                s.op("dve", lambda e: e.tensor_scalar(out=st[:, 2:3], in0=st[:, 0:1], scalar1=1.0 / D, scalar2=None, op0=ALU.mult), reads=[stb], writes=[stb])
                s.op("dve", lambda e: e.scalar_tensor_tensor(out=st[:, 3:4], in0=st[:, 2:3], scalar=-1.0, in1=st[:, 2:3], op0=ALU.mult, op1=ALU.mult),
                     reads=[stb], writes=[stb])
                s.op("dve", lambda e: e.scalar_tensor_tensor(out=st[:, 4:5], in0=st[:, 1:2], scalar=1.0 / D, in1=st[:, 3:4], op0=ALU.mult, op1=ALU.add),
                     reads=[stb], writes=[stb])
                s.op("act", lambda e: e.activation(out=st[:, 5:6], in_=st[:, 4:5], func=AF.Sqrt, scale=1.0, bias=k.eps[:, 0:1]), reads=[stb], writes=[stb])
                s.op("dve", lambda e: e.reciprocal(out=st[:, 5:6], in_=st[:, 5:6]), reads=[stb], writes=[stb])
                s.op("dve", lambda e: e.tensor_scalar(out=scr[:], in0=c_t, scalar1=st[:, 2:3], scalar2=st[:, 5:6], op0=ALU.subtract, op1=ALU.mult),
                     reads=[cb, stb], writes=[scrb])
                s.op("dve", lambda e: e.tensor_tensor(out=scr[:], in0=scr[:], in1=gl[:, 0, :], op=ALU.mult), reads=[scrb, glb], writes=[scrb])
                s.op("pool", lambda e: e.tensor_tensor(out=scr[:], in0=scr[:], in1=gl[:, 1, :], op=ALU.add), reads=[scrb, glb], writes=[scrb])
                s.op("act", lambda e: e.activation(out=sbf[:], in_=scr[:], func=AF.Silu), reads=[scrb], writes=[sbfb])
                bank, bb = banks[7]
                transpose_chunks(k, bank, bb, lambda c: sbf[:, c * 128:(c + 1) * 128], 8, 128, sT[:], sTb, sbfb, dst_view=True)
                for g in range(2):
                    yb, ybb = banks[g]
                    for c in range(8):
                        s.op("pe", lambda e: e.matmul(yb[:, :], lhsT=sT[:, c, :], rhs=w2[:, c, g * 512:(g + 1) * 512], start=(c == 0), stop=(c == 7)),
                             reads=[sTb, w2b], writes=[ybb])
                    s.op("dve", lambda e: e.tensor_tensor(out=scr[:, g * 512:(g + 1) * 512], in0=yb[:, :], in1=gl[:, 2, g * 512:(g + 1) * 512], op=ALU.add),
                         reads=[ybb, glb], writes=[scrb])
                s.op("pool", lambda e: e.tensor_tensor(out=scr[:], in0=scr[:], in1=gl[:, 3, :], op=ALU.mult), reads=[scrb, glb], writes=[scrb])
                s.op("pool", lambda e: e.tensor_tensor(out=ot[:], in0=scr[:], in1=xt[:], op=ALU.add), reads=[scrb, xb], writes=[otb])
                s.dma("sp", lambda e: e.dma_start(out=hout[t * 128:(t + 1) * 128, :], in_=ot[:]), reads=[otb], writes=[houtb[t]])
            s.barrier()


IN_SPECS = {
    "x": ([SEQ, D], F32), "ctx": ([CTX, D], F32), "cc": ([128, 16], F32),
    "w_ada": ([2, D, 6 * D], F32), "b_ada": ([2, 6 * D], F32), "g_norm": ([4, D], F32),
    "ident": ([128, 128], BF16), "identf": ([128, 128], F32),
    "attn_w_in": ([D, 2208], F32), "mla_w_q_up": ([384, 768], F32), "mla_w_kv_up": ([256, 1024], F32),
    "mla_g_qa": ([1, 384], F32), "mla_g_kva": ([1, 256], F32), "mla_g_q": ([1, 96], F32), "mla_g_k": ([1, 96], F32),
    "na_g_q": ([1, 64], F32), "na_g_k": ([1, 64], F32), "attn_w_out": ([D, D], F32),
    "rope": ([SEQ, 32], F32), "nabias": ([8, 128, 21, 128], F32),
    "conv_w_pw1": ([D, 2 * D], F32), "conv_b1T": ([128, 16], F32), "conv_wdwT": ([128, 8, 31], F32), "conv_bdwT": ([128, 8], F32),
    "conv_g_ln": ([1, D], F32), "conv_b_ln": ([1, D], F32), "conv_w_pw2": ([D, D], F32), "conv_b_pw2": ([1, D], F32),
    "wq0": ([D, 2048], F32), "wq1": ([D, 2048], F32), "skT0": ([128, 16, 128], F32), "skT1": ([128, 16, 128], F32),
    "u0": ([16384, D], F32), "u1": ([16384, D], F32), "v0": ([16384, D], F32), "v1": ([16384, D], F32),
}


def build_program():
    nc = bass.Bass("TRN2", target_bir_lowering=False)
    A = {n: nc.dram_tensor(n, sh, dt, kind="ExternalInput").ap() for n, (sh, dt) in IN_SPECS.items()}
    out = nc.dram_tensor("out", [SEQ, D], F32, kind="ExternalOutput").ap()
    modv = nc.dram_tensor("modv_scr", [2, 2, 6, D], F32, kind="Internal").ap()
    hs = [nc.dram_tensor(f"h_scr{i}", [SEQ, D], F32, kind="Internal").ap() for i in range(3)]
    hb = [[Buf() for _ in range(NT)] for _ in range(4)]
    uvs = [nc.dram_tensor(f"uv_scr{l}", [16384, 2 * D], BF16, kind="Internal").ap() for l in range(2)]
    with ExitStack() as es:
        k = K(nc, es)
        k.modv_buf = Buf()
        setup_consts(k, es, A["ident"], A["identf"])
        uvb = [[], []]
        for l in range(2):
            issue_uv_cast(k, es, A[f"u{l}"], A[f"v{l}"], uvs[l], uvb[l])
        stage_ada(k, A["cc"], A["w_ada"], A["b_ada"], A["g_norm"], modv)
        stage_attn(k, A["x"], A["ctx"], modv[0], A, hs[0], hb[0])
        stage_peer3(k, hs[0], hb[0], modv[0], A["wq0"], A["skT0"], uvs[0], uvb[0], hs[1], hb[1], "pra")
        stage_conv(k, hs[1], hb[1], modv[1], A, hs[2], hb[2])
        stage_peer3(k, hs[2], hb[2], modv[1], A["wq1"], A["skT1"], uvs[1], uvb[1], out, hb[3], "prb")
        k.s.barrier()
    return nc


def kernel(**inp):
    import ml_dtypes
    f = lambda a: np.ascontiguousarray(np.asarray(a, dtype=np.float32))
    nb = inp["x"].shape[0]
    shared = {
        "w_ada": f(inp["w_ada"]), "b_ada": f(inp["b_ada"]),
        "g_norm": f(np.stack([inp["g_norm1"][0], inp["g_norm2"][0], inp["g_norm1"][1], inp["g_norm2"][1]])),
        "ident": np.eye(128).astype(ml_dtypes.bfloat16), "identf": np.eye(128, dtype=np.float32),
        "attn_w_in": f(inp["attn_w_in"][0]), "mla_w_q_up": f(inp["mla_w_q_up"][0]), "mla_w_kv_up": f(inp["mla_w_kv_up"][0]),
        "mla_g_qa": f(inp["mla_g_qa"]), "mla_g_kva": f(inp["mla_g_kva"]), "mla_g_q": f(inp["mla_g_q"]), "mla_g_k": f(inp["mla_g_k"]),
        "na_g_q": f(inp["na_g_q"]), "na_g_k": f(inp["na_g_k"]), "attn_w_out": f(inp["attn_w_out"][0]),
        "rope": host_rope_table(), "nabias": host_na_bias(np.asarray(inp["na_rpb"][0], np.float32)),
        "conv_w_pw1": f(inp["conv_w_pw1"][0]), "conv_b1T": f(np.asarray(inp["conv_b_pw1"][0]).reshape(16, 128).T),
        "conv_wdwT": f(np.asarray(inp["conv_w_dw"][0]).reshape(31, 8, 128).transpose(2, 1, 0)),
        "conv_bdwT": f(np.asarray(inp["conv_b_dw"][0]).reshape(8, 128).T),
        "conv_g_ln": f(inp["conv_g_ln"]), "conv_b_ln": f(inp["conv_b_ln"]), "conv_w_pw2": f(inp["conv_w_pw2"][0]), "conv_b_pw2": f(inp["conv_b_pw2"]),
    }
    for l in range(2):
        shared[f"wq{l}"] = f(inp["peer_w_query"][l])
        shared[f"skT{l}"] = f(np.asarray(inp["peer_sub_keys"][l]).reshape(16, 128, 128).transpose(2, 0, 1))
        shared[f"u{l}"] = f(inp["peer_u"][l])
        shared[f"v{l}"] = f(inp["peer_v"][l])
    in_maps = []
    for b in range(nb):
        cc = np.zeros((128, 16), np.float32)
        cc[:, 0::2] = np.asarray(inp["c"][b], np.float32).reshape(8, 128).T
        cc[:, 1::2] = np.asarray(inp["c_ctx"], np.float32).reshape(8, 128).T
        m = dict(shared)
        m["x"] = f(inp["x"][b])
        m["ctx"] = f(inp["ctx"][b])
        m["cc"] = cc
        in_maps.append(m)
    nc = build_program()
    res = run_bass_kernel_spmd(nc, in_maps, core_ids=list(range(nb)))
    return np.stack([np.asarray(r["out"], dtype=np.float32) for r in res.results], axis=0)


def issue_uv_cast(k, es, u_tab, v_tab, uv, uvb):
    for i in range(4):
        r0, r1 = i * 4096, (i + 1) * 4096
        b0, b1 = Buf(), Buf()
        k.s.bulk_dma("pool", lambda e: e.dma_start(out=uv[r0:r1, 0:1024], in_=u_tab[r0:r1, :]), writes=[b0], es=es)
        k.s.bulk_dma("pool", lambda e: e.dma_start(out=uv[r0:r1, 1024:2048], in_=v_tab[r0:r1, :]), writes=[b1], es=es)
        uvb.extend([b0, b1])


NROW2 = 16


def stage_peer2(k, hin, hinb, modv_l, w_query, skT_d, uv, uvb, hout, houtb, tag):
    nc, s = k.nc, k.s
    banks = k.banks
    with ExitStack() as es:
        wq, wqb = k.sb(es, f"{tag}_wq", [128, 8, 2048], BF16)
        skT, skTb = k.sb(es, f"{tag}_skT", [128, 16, 128], BF16)
        ABG, ABGb = k.sb(es, f"{tag}_ABG", [128, 3, 1024], F32)
        iota_i, iota_ib = k.sb(es, f"{tag}_iotai", [128, 16], I32)
        iota, iotab = k.sb(es, f"{tag}_iota", [128, 16], F32)
        st, stb = k.sb(es, f"{tag}_st", [128, 4], F32)
        xts = [k.sb(es, f"{tag}_x{i}", [128, 1024], F32) for i in range(2)]
        scrs = [k.sb(es, f"{tag}_scr{i}", [128, 1024], F32) for i in range(2)]
        hbfs = [k.sb(es, f"{tag}_hbf{i}", [128, 1024], BF16) for i in range(1)]
        hms = [k.sb(es, f"{tag}_hm{i}", [128, 1024], F32) for i in range(2)]
        hT, hTb = k.sb(es, f"{tag}_hT", [128, 8, 128], BF16)
        qbf, qbfb = k.sb(es, f"{tag}_qbf", [128, 2048], BF16)
        qT, qTb = k.sb(es, f"{tag}_qT", [128, 16, 128], BF16)
        ssb, ssbb = k.sb(es, f"{tag}_s", [128, 16, 128], F32)
        s2, _ = k.sb(es, f"{tag}_s2", [128, 16, 128], F32)
        m16, _ = k.sb(es, f"{tag}_m16", [128, 16, 16], F32)
        i16, _ = k.sb(es, f"{tag}_i16", [128, 16, 16], U32)
        i16f, i16fb = k.sb(es, f"{tag}_i16f", [128, 16, 16], F32)
        cand, candb = k.sb(es, f"{tag}_cand", [128, 8, 256], F32)
        cand2, _ = k.sb(es, f"{tag}_cand2", [128, 8, 256], F32)
        best, _ = k.sb(es, f"{tag}_best", [128, 8, 16], F32)
        pos, _ = k.sb(es, f"{tag}_pos", [128, 8, 16], U32)
        ab_i, ab_ib = k.sb(es, f"{tag}_abi", [128, 2, 128], I32)
        ab_f, ab_fb = k.sb(es, f"{tag}_abf", [128, 2, 128], F32)
        oh, ohb = k.sb(es, f"{tag}_oh", [128, 8, 16, 16], F32)
        e01, e01b = k.sb(es, f"{tag}_e01", [128, 2, 128], F32)
        idxs = [k.sb(es, f"{tag}_idx{i}", [128, 128], I32) for i in range(2)]
        gts = [k.sb(es, f"{tag}_gate{i}", [128, 8, 16], F32) for i in range(2)]
        gsum, gsumb = k.sb(es, f"{tag}_gsum", [128, 8], F32)
        actv, _ = k.sb(es, f"{tag}_act", [128, 128], F32)
        wgt, _ = k.sb(es, f"{tag}_wgt", [128, 128], F32)
        junk, _ = k.sb(es, f"{tag}_junk", [128, 1024], BF16)
        rows = [k.sb(es, f"{tag}_row{i}", [128, 2048], BF16) for i in range(NROW2)]
        dgs = [k.sb(es, f"{tag}_dg{i}", [128, 128], BF16) for i in range(4)]
        ot, otb = k.sb(es, f"{tag}_ot", [128, 1024], F32)
        hpb = [[Buf() for _ in range(16)] for _ in range(3)]
        hb = [[Buf() for _ in range(8)] for _ in range(3)]
        actb = [Buf() for _ in range(16)]
        wgb = [Buf() for _ in range(4)]

        wqv = w_query.rearrange("(kc p) n -> p kc n", p=128)
        for c in range(8):
            s.dma("pool", lambda e: e.dma_start(out=wq[:, c, :], in_=wqv[:, c, :]), writes=[wqb])
        s.dma("pool", lambda e: e.dma_start(out=skT[:].rearrange("p a b -> p (a b)"), in_=skT_d.rearrange("p a b -> p (a b)")), writes=[skTb])
        for j in range(3):
            load_bcast(k, ABG[:, j, :], ABGb, modv_l[0:1, 3 + j, :], 1024)
        s.op("pool", lambda e: e.iota(out=iota_i[:], pattern=[[1, 16]], base=0, channel_multiplier=0), writes=[iota_ib])
        s.op("dve", lambda e: e.tensor_copy(out=iota[:], in_=iota_i[:]), reads=[iota_ib], writes=[iotab])

        def front(t):
            xt, xb = xts[t % 2]
            scr, scrb = scrs[t % 2]
            idx, idxb = idxs[t % 2]
            gate, gateb = gts[t % 2]
            hbf, hbfb = hbfs[0]
            hm, hmb = hms[t % 2]
            s.dma("sp", lambda e: e.dma_start(out=xt[:], in_=hin[t * 128:(t + 1) * 128, :]), reads=[hinb[t]], writes=[xb])
            norm_mod(k, xt, xb, ABG[:, 0, :], ABG[:, 1, :], ABGb, scr, scrb, st, stb, hbf, hbfb, out_f32=(hm, hmb))
            bank, bb = banks[4]
            transpose_chunks(k, bank, bb, lambda c: hbf[:, c * 128:(c + 1) * 128], 8, 128, hT[:], hTb, hbfb, dst_view=True)
            yield
            for g in range(4):
                qb_, qbb = banks[g]
                for c in range(8):
                    s.op("pe", lambda e: e.matmul(qb_[:, :], lhsT=hT[:, c, :], rhs=wq[:, c, g * 512:(g + 1) * 512], start=(c == 0), stop=(c == 7)),
                         reads=[hTb, wqb], writes=[qbb])
                s.op("act", lambda e: e.copy(out=qbf[:, g * 512:(g + 1) * 512], in_=qb_[:, :]), reads=[qbb], writes=[qbfb])
            yield
            for half in range(2):
                tb_, tbb = banks[4 + half]
                transpose_chunks(k, tb_, tbb, lambda c: qbf[:, (half * 8 + c) * 128:(half * 8 + c + 1) * 128], 8, 128,
                                 qT[:, half * 8:(half + 1) * 8, :], qTb, qbfb, dst_view=True)
            for g in range(4):
                sb_, sbb = banks[g]
                for j in range(4):
                    hp = g * 4 + j
                    s.op("pe", lambda e: e.matmul(sb_[:, j * 128:(j + 1) * 128], lhsT=qT[:, hp, :], rhs=skT[:, hp, :], start=True, stop=True),
                         reads=[qTb, skTb], writes=[sbb])
                s.op("act", lambda e: e.copy(out=ssb[:, g * 4:(g + 1) * 4, :], in_=sb_[:, :].rearrange("p (a b) -> p a b", b=128)), reads=[sbb], writes=[ssbb])
            yield
            for hp in range(16):
                s.op("dve", lambda e: e.max(out=m16[:, hp, 0:8], in_=ssb[:, hp, :]), reads=[ssbb], writes=[hpb[0][hp]])
            yield
            for hp in range(16):
                s.op("dve", lambda e: e.max_index(out=i16[:, hp, 0:8], in_max=m16[:, hp, 0:8], in_values=ssb[:, hp, :]),
                     reads=[ssbb, hpb[0][hp]], writes=[hpb[1][hp]])
            yield
            for hp in range(16):
                s.op("dve", lambda e: e.match_replace(out=s2[:, hp, :], in_to_replace=m16[:, hp, 0:8], in_values=ssb[:, hp, :], imm_value=-1e30),
                     reads=[ssbb, hpb[0][hp]], writes=[hpb[2][hp]])
            yield
            for hp in range(16):
                s.op("dve", lambda e: e.max(out=m16[:, hp, 8:16], in_=s2[:, hp, :]), reads=[hpb[2][hp]], writes=[hpb[0][hp]])
            yield
            for hp in range(16):
                s.op("dve", lambda e: e.max_index(out=i16[:, hp, 8:16], in_max=m16[:, hp, 8:16], in_values=s2[:, hp, :]),
                     reads=[hpb[2][hp], hpb[0][hp]], writes=[hpb[1][hp]])
            yield
            s.op("dve", lambda e: e.tensor_copy(out=i16f[:], in_=i16[:]), reads=hpb[1], writes=[i16fb])
            m4 = m16[:].rearrange("p (h t) a -> p h t a", t=2)
            s.op("dve", lambda e: e.tensor_tensor(out=cand[:].rearrange("p h (a b) -> p h a b", b=16),
                                                  in0=m4[:, :, 0, :].unsqueeze(3).to_broadcast([128, 8, 16, 16]),
                                                  in1=m4[:, :, 1, :].unsqueeze(2).to_broadcast([128, 8, 16, 16]), op=ALU.add),
                 reads=hpb[0], writes=[candb])
            yield
            for h in range(8):
                s.op("dve", lambda e: e.max(out=best[:, h, 0:8], in_=cand[:, h, :]), reads=[candb], writes=[hb[0][h]])
            for h in range(8):
                s.op("dve", lambda e: e.max_index(out=pos[:, h, 0:8], in_max=best[:, h, 0:8], in_values=cand[:, h, :]),
                     reads=[candb, hb[0][h]], writes=[hb[1][h]])
            yield
            for h in range(8):
                s.op("dve", lambda e: e.match_replace(out=cand2[:, h, :], in_to_replace=best[:, h, 0:8], in_values=cand[:, h, :], imm_value=-1e30),
                     reads=[candb, hb[0][h]], writes=[hb[2][h]])
            for h in range(8):
                s.op("dve", lambda e: e.max(out=best[:, h, 8:16], in_=cand2[:, h, :]), reads=[hb[2][h]], writes=[hb[0][h]])
            yield
            for h in range(8):
                s.op("dve", lambda e: e.max_index(out=pos[:, h, 8:16], in_max=best[:, h, 8:16], in_values=cand2[:, h, :]),
                     reads=[hb[2][h], hb[0][h]], writes=[hb[1][h]])
            posi = pos[:].rearrange("p h k -> p (h k)").bitcast(I32)
            s.op("dve", lambda e: e.tensor_single_scalar(out=ab_i[:, 0, :], in_=posi, scalar=4, op=ALU.arith_shift_right), reads=hb[1], writes=[ab_ib])
            s.op("dve", lambda e: e.tensor_single_scalar(out=ab_i[:, 1, :], in_=posi, scalar=15, op=ALU.bitwise_and), reads=hb[1], writes=[ab_ib])
            s.op("dve", lambda e: e.tensor_copy(out=ab_f[:], in_=ab_i[:]), reads=[ab_ib], writes=[ab_fb])
            yield
            i4 = i16f[:].rearrange("p (h t) a -> p h t a", t=2)
            for p_ in range(2):
                s.op("dve", lambda e: e.tensor_tensor(out=oh[:], in0=ab_f[:, p_, :].rearrange("p (h k) -> p h k", k=16).unsqueeze(3).to_broadcast([128, 8, 16, 16]),
                                                      in1=iota[:].unsqueeze(1).unsqueeze(1).to_broadcast([128, 8, 16, 16]), op=ALU.is_equal),
                     reads=[ab_fb, iotab], writes=[ohb])
                s.op("dve", lambda e: e.tensor_tensor(out=oh[:], in0=oh[:], in1=i4[:, :, p_, :].unsqueeze(2).to_broadcast([128, 8, 16, 16]), op=ALU.mult),
                     reads=[ohb, i16fb], writes=[ohb])
                s.op("dve", lambda e: e.tensor_reduce(out=e01[:, p_, :].rearrange("p (h k) -> p h k", k=16), in_=oh[:], axis=AX.X, op=ALU.add),
                     reads=[ohb], writes=[e01b])
                yield
            s.op("dve", lambda e: e.scalar_tensor_tensor(out=e01[:, 0, :], in0=e01[:, 0, :], scalar=128.0, in1=e01[:, 1, :], op0=ALU.mult, op1=ALU.add),
                 reads=[e01b], writes=[e01b])
            s.op("dve", lambda e: e.tensor_copy(out=idx[:], in_=e01[:, 0, :]), reads=[e01b], writes=[idxb])
            s.op("dve", lambda e: e.tensor_tensor(out=gate[:], in0=best[:], in1=best[:, :, 0:1].to_broadcast([128, 8, 16]), op=ALU.subtract),
                 reads=hb[0], writes=[gateb])
            s.op("act", lambda e: e.activation(out=gate[:], in_=gate[:], func=AF.Exp), reads=[gateb], writes=[gateb])
            s.op("dve", lambda e: e.tensor_reduce(out=gsum[:], in_=gate[:], axis=AX.X, op=ALU.add), reads=[gateb], writes=[gsumb])
            s.op("dve", lambda e: e.reciprocal(out=gsum[:], in_=gsum[:]), reads=[gsumb], writes=[gsumb])
            s.op("dve", lambda e: e.tensor_tensor(out=gate[:], in0=gate[:], in1=gsum[:].unsqueeze(2).to_broadcast([128, 8, 16]), op=ALU.mult),
                 reads=[gateb, gsumb], writes=[gateb])

        ring = [0]

        def back(t, fg):
            xt, xb = xts[t % 2]
            scr, scrb = scrs[t % 2]
            idx, idxb = idxs[t % 2]
            gate, gateb = gts[t % 2]
            hm, hmb = hms[t % 2]
            (o0, o0b), (o1, o1b) = banks[6], banks[7]
            for grp in range(16):
                held = []
                for j in range(8):
                    hk = grp * 8 + j
                    rw, rwb = rows[ring[0] % NROW2]
                    ring[0] += 1
                    s.dma("pool", lambda e: e.indirect_dma_start(out=rw[:], out_offset=None, in_=uv,
                                                                 in_offset=bass.IndirectOffsetOnAxis(ap=idx[:, hk:hk + 1], axis=0)),
                          reads=[idxb] + uvb, writes=[rwb])
                    s.op("dve", lambda e: e.scalar_tensor_tensor(out=junk[:], in0=rw[:, 0:1024], scalar=1.0, in1=hm[:], op0=ALU.mult, op1=ALU.mult,
                                                                 accum_out=actv[:, hk:hk + 1]), reads=[rwb, hmb], writes=[actb[hk % 16]])
                    held.append((hk, rw, rwb))
                g8 = slice(grp * 8, grp * 8 + 8)
                wb_ = wgb[grp % 4]
                s.op("act", lambda e: e.activation(out=wgt[:, g8], in_=actv[:, g8], func=AF.Gelu), reads=actb[(grp % 2) * 8:(grp % 2) * 8 + 8], writes=[wb_])
                s.op("dve", lambda e: e.tensor_tensor(out=wgt[:, g8], in0=wgt[:, g8], in1=gate[:].rearrange("p h k -> p (h k)")[:, g8], op=ALU.mult),
                     reads=[wb_, gateb], writes=[wb_])
                for (hk, rw, rwb) in held:
                    dg, dgb = dgs[hk % 4]
                    s.op("act", lambda e: e.activation(out=dg[:], in_=k.ident[:], func=AF.Copy, scale=wgt[:, hk:hk + 1]),
                         reads=[k.identb, wb_], writes=[dgb])
                    s.op("pe", lambda e: e.matmul(o0[:, :], lhsT=dg[:], rhs=rw[:, 1024:1536], start=(hk == 0), stop=(hk == 127)),
                         reads=[dgb, rwb], writes=[o0b])
                    s.op("pe", lambda e: e.matmul(o1[:, :], lhsT=dg[:], rhs=rw[:, 1536:2048], start=(hk == 0), stop=(hk == 127)),
                         reads=[dgb, rwb], writes=[o1b])
                if fg is not None:
                    next(fg, None)
            s.op("dve", lambda e: e.tensor_tensor(out=scr[:, 0:512], in0=o0[:, :], in1=ABG[:, 2, 0:512], op=ALU.mult), reads=[o0b, ABGb], writes=[scrb])
            s.op("dve", lambda e: e.tensor_tensor(out=scr[:, 512:1024], in0=o1[:, :], in1=ABG[:, 2, 512:1024], op=ALU.mult), reads=[o1b, ABGb], writes=[scrb])
            s.op("pool", lambda e: e.tensor_tensor(out=ot[:], in0=scr[:], in1=xt[:], op=ALU.add), reads=[scrb, xb], writes=[otb])
            s.dma("sp", lambda e: e.dma_start(out=hout[t * 128:(t + 1) * 128, :], in_=ot[:]), reads=[otb], writes=[houtb[t]])

        for _ in front(0):
            pass
        for t in range(NT):
            fg = front(t + 1) if t + 1 < NT else None
            back(t, fg)
            if fg is not None:
                for _ in fg:
                    pass
        s.barrier()
NROW3 = 16


def stage_peer3(k, hin, hinb, modv_l, w_query, skT_d, uv, uvb, hout, houtb, tag):
    nc, s = k.nc, k.s
    banks = k.banks
    with ExitStack() as es:
        wq, wqb = k.sb(es, f"{tag}_wq", [128, 8, 2048], BF16)
        skT, skTb = k.sb(es, f"{tag}_skT", [128, 16, 128], BF16)
        ABG, ABGb = k.sb(es, f"{tag}_ABG", [128, 3, 1024], F32)
        iota_i, iota_ib = k.sb(es, f"{tag}_iotai", [128, 16], I32)
        iota, iotab = k.sb(es, f"{tag}_iota", [128, 16], F32)
        st, stb = k.sb(es, f"{tag}_st", [128, 4], F32)
        xts = [k.sb(es, f"{tag}_x{i}", [128, 1024], F32) for i in range(2)]
        scrs = [k.sb(es, f"{tag}_scr{i}", [128, 1024], F32) for i in range(2)]
        hbfs = [k.sb(es, f"{tag}_hbf{i}", [128, 1024], BF16) for i in range(1)]
        hT, hTb = k.sb(es, f"{tag}_hT", [128, 8, 128], BF16)
        qbf, qbfb = k.sb(es, f"{tag}_qbf", [128, 2048], BF16)
        qT, qTb = k.sb(es, f"{tag}_qT", [128, 16, 128], BF16)
        ssb, ssbb = k.sb(es, f"{tag}_s", [128, 16, 128], F32)
        s2, _ = k.sb(es, f"{tag}_s2", [128, 16, 128], F32)
        m16, _ = k.sb(es, f"{tag}_m16", [128, 16, 16], F32)
        i16, _ = k.sb(es, f"{tag}_i16", [128, 16, 16], U32)
        i16f, i16fb = k.sb(es, f"{tag}_i16f", [128, 16, 16], F32)
        cand, candb = k.sb(es, f"{tag}_cand", [128, 8, 256], F32)
        cand2, _ = k.sb(es, f"{tag}_cand2", [128, 8, 256], F32)
        best, _ = k.sb(es, f"{tag}_best", [128, 8, 16], F32)
        pos, _ = k.sb(es, f"{tag}_pos", [128, 8, 16], U32)
        ab_i, ab_ib = k.sb(es, f"{tag}_abi", [128, 2, 128], I32)
        ab_f, ab_fb = k.sb(es, f"{tag}_abf", [128, 2, 128], F32)
        oh, ohb = k.sb(es, f"{tag}_oh", [128, 8, 16, 16], F32)
        e01, e01b = k.sb(es, f"{tag}_e01", [128, 2, 128], F32)
        idxs = [k.sb(es, f"{tag}_idx{i}", [128, 128], I32) for i in range(2)]
        gts = [k.sb(es, f"{tag}_gate{i}", [128, 8, 16], F32) for i in range(2)]
        gsum, gsumb = k.sb(es, f"{tag}_gsum", [128, 8], F32)
        actv, _ = k.sb(es, f"{tag}_act", [128, 128], F32)
        wgt, _ = k.sb(es, f"{tag}_wgt", [128, 128], F32)
        junk, _ = k.sb(es, f"{tag}_junk", [128, 1024], BF16)
        rows = [k.sb(es, f"{tag}_row{i}", [128, 2048], BF16) for i in range(NROW3)]
        dgs = [k.sb(es, f"{tag}_dg{i}", [128, 128], BF16) for i in range(4)]
        ot, otb = k.sb(es, f"{tag}_ot", [128, 1024], F32)
        hpb = [[Buf() for _ in range(16)] for _ in range(3)]
        hb = [[Buf() for _ in range(8)] for _ in range(3)]
        actb = [Buf() for _ in range(16)]
        wgb = [Buf() for _ in range(4)]

        wqv = w_query.rearrange("(kc p) n -> p kc n", p=128)
        for c in range(8):
            s.dma("pool", lambda e: e.dma_start(out=wq[:, c, :], in_=wqv[:, c, :]), writes=[wqb])
        s.dma("pool", lambda e: e.dma_start(out=skT[:].rearrange("p a b -> p (a b)"), in_=skT_d.rearrange("p a b -> p (a b)")), writes=[skTb])
        for j in range(3):
            load_bcast(k, ABG[:, j, :], ABGb, modv_l[0:1, 3 + j, :], 1024)
        s.op("pool", lambda e: e.iota(out=iota_i[:], pattern=[[1, 16]], base=0, channel_multiplier=0), writes=[iota_ib])
        s.op("dve", lambda e: e.tensor_copy(out=iota[:], in_=iota_i[:]), reads=[iota_ib], writes=[iotab])

        def front(t):
            xt, xb = xts[t % 2]
            scr, scrb = scrs[t % 2]
            idx, idxb = idxs[t % 2]
            gate, gateb = gts[t % 2]
            hbf, hbfb = hbfs[0]
            s.dma("sp", lambda e: e.dma_start(out=xt[:], in_=hin[t * 128:(t + 1) * 128, :]), reads=[hinb[t]], writes=[xb])
            yield
            s.op("act", lambda e: e.activation(out=scr[:], in_=xt[:], func=AF.Square, accum_out=st[:, 0:1]), reads=[xb], writes=[scrb, stb])
            s.op("act", lambda e: e.activation(out=st[:, 0:1], in_=st[:, 0:1], func=AF.Sqrt, scale=1.0 / D, bias=k.eps[:, 0:1]), reads=[stb], writes=[stb])
            yield
            s.op("dve", lambda e: e.reciprocal(out=st[:, 0:1], in_=st[:, 0:1]), reads=[stb], writes=[stb])
            s.op("dve", lambda e: e.scalar_tensor_tensor(out=scr[:], in0=xt[:], scalar=st[:, 0:1], in1=ABG[:, 0, :], op0=ALU.mult, op1=ALU.mult),
                 reads=[xb, stb, ABGb], writes=[scrb])
            yield
            s.op("pool", lambda e: e.tensor_tensor(out=hbf[:], in0=scr[:], in1=ABG[:, 1, :], op=ALU.add), reads=[scrb, ABGb], writes=[hbfb])
            yield
            hmp, hmpb = banks[4 + t % 2]
            bank, bb = banks[2]
            bv = bank[:].bitcast(BF16)
            for c in range(8):
                s.op("pe", lambda e: e.transpose(out=bv[:, c * 128:(c + 1) * 128], in_=hbf[:, c * 128:(c + 1) * 128], identity=k.ident[:]),
                     reads=[hbfb, k.identb], writes=[bb])
            yield
            s.op("act", lambda e: e.copy(out=hT[:], in_=bv[:, :].rearrange("p (c t) -> p c t", t=128)), reads=[bb], writes=[hTb])
            yield
            hmv_ = hmp[:].bitcast(BF16)
            for c in range(8):
                s.op("pe", lambda e: e.transpose(out=hmv_[:, c * 128:(c + 1) * 128], in_=hT[:, c, :], identity=k.ident[:]),
                     reads=[hTb, k.identb], writes=[hmpb])
            for g in range(5):
                if g < 4:
                    qb_, qbb = banks[g % 2]
                    for c in range(8):
                        s.op("pe", lambda e: e.matmul(qb_[:, :], lhsT=hT[:, c, :], rhs=wq[:, c, g * 512:(g + 1) * 512], start=(c == 0), stop=(c == 7)),
                             reads=[hTb, wqb], writes=[qbb])
                if g > 0:
                    g1 = g - 1
                    qb1, qbb1 = banks[g1 % 2]
                    s.op("act", lambda e: e.copy(out=qbf[:, g1 * 512:(g1 + 1) * 512], in_=qb1[:, :]), reads=[qbb1], writes=[qbfb])
                yield
            for half in range(3):
                if half < 2:
                    tb_, tbb = banks[2 + half]
                    bv2 = tb_[:].bitcast(BF16)
                    for c in range(8):
                        s.op("pe", lambda e: e.transpose(out=bv2[:, c * 128:(c + 1) * 128], in_=qbf[:, (half * 8 + c) * 128:(half * 8 + c + 1) * 128], identity=k.ident[:]),
                             reads=[qbfb, k.identb], writes=[tbb])
                if half > 0:
                    h1 = half - 1
                    tb1, tbb1 = banks[2 + h1]
                    s.op("act", lambda e: e.copy(out=qT[:, h1 * 8:(h1 + 1) * 8, :], in_=tb1[:].bitcast(BF16)[:, :].rearrange("p (c t) -> p c t", t=128)),
                         reads=[tbb1], writes=[qTb])
                yield
            for g in range(5):
                if g < 4:
                    sb_, sbb = banks[g % 2]
                    for j in range(4):
                        hp = g * 4 + j
                        s.op("pe", lambda e: e.matmul(sb_[:, j * 128:(j + 1) * 128], lhsT=qT[:, hp, :], rhs=skT[:, hp, :], start=True, stop=True),
                             reads=[qTb, skTb], writes=[sbb])
                if g > 0:
                    g1 = g - 1
                    sb1, sbb1 = banks[g1 % 2]
                    s.op("act", lambda e: e.copy(out=ssb[:, g1 * 4:(g1 + 1) * 4, :], in_=sb1[:, :].rearrange("p (a b) -> p a b", b=128)), reads=[sbb1], writes=[ssbb])
                if g % 2 == 1:
                    yield
            yield
            for hp in range(16):
                s.op("dve", lambda e: e.max(out=m16[:, hp, 0:8], in_=ssb[:, hp, :]), reads=[ssbb], writes=[hpb[0][hp]])
            yield
            for hp in range(16):
                s.op("dve", lambda e: e.max_index(out=i16[:, hp, 0:8], in_max=m16[:, hp, 0:8], in_values=ssb[:, hp, :]),
                     reads=[ssbb, hpb[0][hp]], writes=[hpb[1][hp]])
            yield
            for hp in range(16):
                s.op("dve", lambda e: e.match_replace(out=s2[:, hp, :], in_to_replace=m16[:, hp, 0:8], in_values=ssb[:, hp, :], imm_value=-1e30),
                     reads=[ssbb, hpb[0][hp]], writes=[hpb[2][hp]])
            yield
            for hp in range(16):
                s.op("dve", lambda e: e.max(out=m16[:, hp, 8:16], in_=s2[:, hp, :]), reads=[hpb[2][hp]], writes=[hpb[0][hp]])
            yield
            for hp in range(16):
                s.op("dve", lambda e: e.max_index(out=i16[:, hp, 8:16], in_max=m16[:, hp, 8:16], in_values=s2[:, hp, :]),
                     reads=[hpb[2][hp], hpb[0][hp]], writes=[hpb[1][hp]])
            yield
            s.op("dve", lambda e: e.tensor_copy(out=i16f[:], in_=i16[:]), reads=hpb[1], writes=[i16fb])
            m4 = m16[:].rearrange("p (h t) a -> p h t a", t=2)
            s.op("dve", lambda e: e.tensor_tensor(out=cand[:].rearrange("p h (a b) -> p h a b", b=16),
                                                  in0=m4[:, :, 0, :].unsqueeze(3).to_broadcast([128, 8, 16, 16]),
                                                  in1=m4[:, :, 1, :].unsqueeze(2).to_broadcast([128, 8, 16, 16]), op=ALU.add),
                 reads=hpb[0], writes=[candb])
            yield
            for h in range(8):
                s.op("dve", lambda e: e.max(out=best[:, h, 0:8], in_=cand[:, h, :]), reads=[candb], writes=[hb[0][h]])
            for h in range(8):
                s.op("dve", lambda e: e.max_index(out=pos[:, h, 0:8], in_max=best[:, h, 0:8], in_values=cand[:, h, :]),
                     reads=[candb, hb[0][h]], writes=[hb[1][h]])
            yield
            for h in range(8):
                s.op("dve", lambda e: e.match_replace(out=cand2[:, h, :], in_to_replace=best[:, h, 0:8], in_values=cand[:, h, :], imm_value=-1e30),
                     reads=[candb, hb[0][h]], writes=[hb[2][h]])
            for h in range(8):
                s.op("dve", lambda e: e.max(out=best[:, h, 8:16], in_=cand2[:, h, :]), reads=[hb[2][h]], writes=[hb[0][h]])
            yield
            for h in range(8):
                s.op("dve", lambda e: e.max_index(out=pos[:, h, 8:16], in_max=best[:, h, 8:16], in_values=cand2[:, h, :]),
                     reads=[hb[2][h], hb[0][h]], writes=[hb[1][h]])
            posi = pos[:].rearrange("p h k -> p (h k)").bitcast(I32)
            s.op("dve", lambda e: e.tensor_single_scalar(out=ab_i[:, 0, :], in_=posi, scalar=4, op=ALU.arith_shift_right), reads=hb[1], writes=[ab_ib])
            s.op("dve", lambda e: e.tensor_single_scalar(out=ab_i[:, 1, :], in_=posi, scalar=15, op=ALU.bitwise_and), reads=hb[1], writes=[ab_ib])
            s.op("dve", lambda e: e.tensor_copy(out=ab_f[:], in_=ab_i[:]), reads=[ab_ib], writes=[ab_fb])
            s.op("dve", lambda e: e.tensor_tensor(out=gate[:], in0=best[:], in1=best[:, :, 0:1].to_broadcast([128, 8, 16]), op=ALU.subtract),
                 reads=hb[0], writes=[gateb])
            yield
            s.op("act", lambda e: e.activation(out=gate[:], in_=gate[:], func=AF.Exp), reads=[gateb], writes=[gateb])
            i4 = i16f[:].rearrange("p (h t) a -> p h t a", t=2)
            for p_ in range(2):
                s.op("dve", lambda e: e.tensor_tensor(out=oh[:], in0=ab_f[:, p_, :].rearrange("p (h k) -> p h k", k=16).unsqueeze(3).to_broadcast([128, 8, 16, 16]),
                                                      in1=iota[:].unsqueeze(1).unsqueeze(1).to_broadcast([128, 8, 16, 16]), op=ALU.is_equal),
                     reads=[ab_fb, iotab], writes=[ohb])
                yield
                s.op("dve", lambda e: e.tensor_tensor(out=oh[:], in0=oh[:], in1=i4[:, :, p_, :].unsqueeze(2).to_broadcast([128, 8, 16, 16]), op=ALU.mult),
                     reads=[ohb, i16fb], writes=[ohb])
                yield
                s.op("dve", lambda e: e.tensor_reduce(out=e01[:, p_, :].rearrange("p (h k) -> p h k", k=16), in_=oh[:], axis=AX.X, op=ALU.add),
                     reads=[ohb], writes=[e01b])
                yield
            s.op("dve", lambda e: e.scalar_tensor_tensor(out=e01[:, 0, :], in0=e01[:, 0, :], scalar=128.0, in1=e01[:, 1, :], op0=ALU.mult, op1=ALU.add),
                 reads=[e01b], writes=[e01b])
            s.op("dve", lambda e: e.tensor_copy(out=idx[:], in_=e01[:, 0, :]), reads=[e01b], writes=[idxb])
            s.op("dve", lambda e: e.tensor_reduce(out=gsum[:], in_=gate[:], axis=AX.X, op=ALU.add), reads=[gateb], writes=[gsumb])
            s.op("dve", lambda e: e.reciprocal(out=gsum[:], in_=gsum[:]), reads=[gsumb], writes=[gsumb])
            s.op("dve", lambda e: e.tensor_tensor(out=gate[:], in0=gate[:], in1=gsum[:].unsqueeze(2).to_broadcast([128, 8, 16]), op=ALU.mult),
                 reads=[gateb, gsumb], writes=[gateb])

        ring = [0]

        glv, _ = k.sb(es, f"{tag}_glv", [128, 128], F32)
        glb = [Buf() for _ in range(16)]
        wgb16 = [Buf() for _ in range(16)]

        def back(t, fg):
            xt, xb = xts[t % 2]
            scr, scrb = scrs[t % 2]
            idx, idxb = idxs[t % 2]
            gate, gateb = gts[t % 2]
            hmp, hmpb = banks[4 + t % 2]
            hmv = hmp[:].bitcast(BF16)
            gflat = gate[:].rearrange("p h k -> p (h k)")
            (o0, o0b), (o1, o1b) = banks[6], banks[7]
            held = {}

            def st_a(hk):
                s.op("act", lambda e: e.activation(out=glv[:, hk:hk + 1], in_=actv[:, hk:hk + 1], func=AF.Gelu), reads=[actb[hk % 16]], writes=[glb[hk % 16]])

            def st_b(hk):
                rw, rwb = held.pop(hk)
                dg, dgb = dgs[hk % 4]
                s.op("act", lambda e: e.activation(out=wgt[:, hk:hk + 1], in_=glv[:, hk:hk + 1], func=AF.Copy, scale=gflat[:, hk:hk + 1]),
                     reads=[glb[hk % 16], gateb], writes=[wgb16[hk % 16]])
                s.op("act", lambda e: e.activation(out=dg[:], in_=k.ident[:], func=AF.Copy, scale=wgt[:, hk:hk + 1]),
                     reads=[k.identb, wgb16[hk % 16]], writes=[dgb])
                s.op("pe", lambda e: e.matmul(o0[:, :], lhsT=dg[:], rhs=rw[:, 1024:1536], start=(hk == 0), stop=(hk == 127)),
                     reads=[dgb, rwb], writes=[o0b])
                s.op("pe", lambda e: e.matmul(o1[:, :], lhsT=dg[:], rhs=rw[:, 1536:2048], start=(hk == 0), stop=(hk == 127)),
                     reads=[dgb, rwb], writes=[o1b])

            for hk in range(128 + 3):
                if hk < 128:
                    rw, rwb = rows[ring[0] % NROW3]
                    ring[0] += 1
                    s.dma("pool", lambda e: e.indirect_dma_start(out=rw[:], out_offset=None, in_=uv,
                                                                 in_offset=bass.IndirectOffsetOnAxis(ap=idx[:, hk:hk + 1], axis=0)),
                          reads=[idxb] + uvb, writes=[rwb])
                    s.op("dve", lambda e: e.scalar_tensor_tensor(out=junk[:], in0=rw[:, 0:1024], scalar=1.0, in1=hmv, op0=ALU.mult, op1=ALU.mult,
                                                                 accum_out=actv[:, hk:hk + 1]), reads=[rwb, hmpb], writes=[actb[hk % 16]])
                    held[hk] = (rw, rwb)
                if 0 <= hk - 1 < 128:
                    st_a(hk - 1)
                if 0 <= hk - 3 < 128:
                    st_b(hk - 3)
                if fg is not None and hk % 4 == 3:
                    next(fg, None)
            s.op("dve", lambda e: e.tensor_tensor(out=scr[:, 0:512], in0=o0[:, :], in1=ABG[:, 2, 0:512], op=ALU.mult), reads=[o0b, ABGb], writes=[scrb])
            s.op("dve", lambda e: e.tensor_tensor(out=scr[:, 512:1024], in0=o1[:, :], in1=ABG[:, 2, 512:1024], op=ALU.mult), reads=[o1b, ABGb], writes=[scrb])
            s.op("pool", lambda e: e.tensor_tensor(out=ot[:], in0=scr[:], in1=xt[:], op=ALU.add), reads=[scrb, xb], writes=[otb])
            s.dma("sp", lambda e: e.dma_start(out=hout[t * 128:(t + 1) * 128, :], in_=ot[:]), reads=[otb], writes=[houtb[t]])

        for _ in front(0):
            pass
        for t in range(NT):
            fg = front(t + 1) if t + 1 < NT else None
            back(t, fg)
            if fg is not None:
                for _ in fg:
                    pass
        s.barrier()
```

```python
import numpy as np
from contextlib import ExitStack
import concourse.bass as bass
import concourse.mybir as mybir
from concourse.bass_utils import run_bass_kernel_spmd

F32 = mybir.dt.float32
BF16 = mybir.dt.bfloat16
I32 = mybir.dt.int32
U32 = mybir.dt.uint32
ALU = mybir.AluOpType
AF = mybir.ActivationFunctionType
AX = mybir.AxisListType

D = 1024
SEQ = 2048
NT = SEQ // 128
CTX = 256
NCT = CTX // 128
EPS = 1e-6
NEG = -30000.0


class Buf:
    __slots__ = ("w", "r")

    def __init__(self):
        self.w = None
        self.r = {}


class Sched:
    RING = 12

    def __init__(self, nc, es):
        self.nc = nc
        self.eng = {"pe": nc.tensor, "act": nc.scalar, "dve": nc.vector, "pool": nc.gpsimd, "sp": nc.sync}
        self.semobj = {}
        self.cnt = {}
        for k in self.eng:
            self.semobj[k] = es.enter_context(nc.semaphore("s_" + k))
            self.cnt[k] = 0
        self.waited = {k: {} for k in self.eng}
        self.bulk = []
        self.dq = {}
        for q in ("sp", "pool", "act"):
            slots = []
            for i in range(self.RING):
                key = ("d", q, i)
                self.semobj[key] = es.enter_context(nc.semaphore(f"d_{q}_{i}"))
                slots.append(key)
            self.dq[q] = {"slots": slots, "uses": [0] * self.RING, "next": 0}

    def _wait(self, ek, tok):
        if tok is None:
            return
        sk, v = tok
        if self.waited[ek].get(sk, 0) >= v:
            return
        self.eng[ek].wait_ge(self.semobj[sk], v)
        self.waited[ek][sk] = v

    def _deps(self, ek, reads, writes):
        for b in reads:
            self._wait(ek, b.w)
        for b in writes:
            self._wait(ek, b.w)
            for sk, v in b.r.items():
                self._wait(ek, (sk, v))

    def _mark(self, tok, reads, writes):
        sk, v = tok
        for b in reads:
            if b.r.get(sk, 0) < v:
                b.r[sk] = v
        for b in writes:
            b.w = tok
            b.r = {}

    def op(self, ek, fn, reads=(), writes=()):
        self._deps(ek, reads, writes)
        ins = fn(self.eng[ek])
        self.cnt[ek] += 1
        ins.then_inc(self.semobj[ek], 1)
        tok = (ek, self.cnt[ek])
        self._mark(tok, reads, writes)
        return tok

    def dma(self, q, fn, reads=(), writes=()):
        dq = self.dq[q]
        slot = dq["next"]
        dq["next"] = (slot + 1) % self.RING
        key = dq["slots"][slot]
        uses = dq["uses"][slot]
        if uses:
            self._wait(q, (key, 16 * uses))
        self._deps(q, reads, writes)
        ins = fn(self.eng[q])
        ins.then_inc(self.semobj[key], 16)
        dq["uses"][slot] = uses + 1
        tok = (key, 16 * (uses + 1))
        self._mark(tok, reads, writes)
        return tok

    def bulk_dma(self, q, fn, reads=(), writes=(), es=None):
        key = ("bulk", len(self.semobj))
        self.semobj[key] = es.enter_context(self.nc.semaphore(f"bulk{len(self.semobj)}"))
        self._deps(q, reads, writes)
        ins = fn(self.eng[q])
        ins.then_inc(self.semobj[key], 16)
        tok = (key, 16)
        self.bulk.append(tok)
        self._mark(tok, reads, writes)
        return tok

    def barrier(self):
        toks = [(k, self.cnt[k]) for k in self.eng if self.cnt[k]]
        for q, dq in self.dq.items():
            for key, u in zip(dq["slots"], dq["uses"]):
                if u:
                    toks.append((key, 16 * u))
        toks.extend(self.bulk)
        for ek in self.eng:
            for t in toks:
                self._wait(ek, t)


class K:
    def __init__(self, nc, es):
        self.nc = nc
        self.es = es
        self.s = Sched(nc, es)
        self.banks = []
        for i in range(8):
            t = es.enter_context(nc.psum_tensor(f"bank{i}", [128, 512], F32))
            self.banks.append((t, Buf()))
        self.ident = None

    def sb(self, es, name, shape, dt):
        t = es.enter_context(self.nc.sbuf_tensor(name, list(shape), dt))
        return t, Buf()

    def poke(self):
        bg = getattr(self, "bg", None)
        if bg is not None:
            next(bg, None)


def bcast_row(ap_row, parts):
    return ap_row.partition_broadcast(parts) if len(ap_row.shape) == 1 else ap_row.to_broadcast([parts, ap_row.shape[-1]])


def stage_ada(k, cc, w_ada, b_ada, g_norm, modv):
    nc, s = k.nc, k.s
    with ExitStack() as es:
        cct, ccb = k.sb(es, "ada_cc", [128, 16], F32)
        sil, silb = k.sb(es, "ada_sil", [128, 16], F32)
        wt = [k.sb(es, f"ada_w{i}", [128, 8, 512], F32) for i in range(2)]
        brow, browb = k.sb(es, "ada_b", [2, 6144], F32)
        grow, growb = k.sb(es, "ada_g", [2, 2, 1024], F32)
        mrow, mrowb = k.sb(es, "ada_m", [2, 6144], F32)
        orow, orowb = k.sb(es, "ada_o", [2, 6, 1024], F32)

        s.dma("sp", lambda e: e.dma_start(out=cct[:], in_=cc), writes=[ccb])
        s.op("act", lambda e: e.activation(out=sil[:], in_=cct[:], func=AF.Silu), reads=[ccb], writes=[silb])
        for l in range(2):
            s.dma("sp", lambda e: e.dma_start(out=brow[:], in_=b_ada[l:l + 1, :].to_broadcast([2, 6144])), writes=[browb])
            s.dma("sp", lambda e: e.dma_start(out=grow[:], in_=g_norm[2 * l:2 * l + 2, :].rearrange("(o a) d -> o a d", o=1).to_broadcast([2, 2, 1024])), writes=[growb])
            wv = w_ada[l].rearrange("(kc p) n -> p kc n", p=128)
            for g in range(12):
                wtile, wbuf = wt[g % 2]
                q = "sp" if g % 2 == 0 else "act"
                s.dma(q, lambda e: e.dma_start(out=wtile[:], in_=wv[:, :, g * 512:(g + 1) * 512]), writes=[wbuf])
                bank, bb = k.banks[g % 2]
                for kc in range(8):
                    s.op("pe", lambda e: e.matmul(bank[0:2, :], lhsT=sil[:, 2 * kc:2 * kc + 2], rhs=wtile[:, kc, :],
                                                  start=(kc == 0), stop=(kc == 7)),
                         reads=[silb, wbuf], writes=[bb])
                s.op("dve", lambda e: e.tensor_tensor(out=mrow[:, g * 512:(g + 1) * 512], in0=bank[0:2, :],
                                                      in1=brow[:, g * 512:(g + 1) * 512], op=ALU.add),
                     reads=[bb, browb], writes=[mrowb])
            for j in range(2):
                sh = mrow[:, (3 * j) * 1024:(3 * j + 1) * 1024]
                sc = mrow[:, (3 * j + 1) * 1024:(3 * j + 2) * 1024]
                gt = mrow[:, (3 * j + 2) * 1024:(3 * j + 3) * 1024]
                s.op("dve", lambda e: e.scalar_tensor_tensor(out=orow[:, 3 * j, :], in0=sc, scalar=1.0, in1=grow[:, j, :],
                                                             op0=ALU.add, op1=ALU.mult),
                     reads=[mrowb, growb], writes=[orowb])
                s.op("dve", lambda e: e.tensor_copy(out=orow[:, 3 * j + 1, :], in_=sh), reads=[mrowb], writes=[orowb])
                s.op("dve", lambda e: e.tensor_copy(out=orow[:, 3 * j + 2, :], in_=gt), reads=[mrowb], writes=[orowb])
            s.dma("sp", lambda e: e.dma_start(out=modv[l], in_=orow[:]), reads=[orowb], writes=[k.modv_buf])
        s.barrier()


def load_cast(k, stg, dst, dstb, src, n, qi=0):
    s = k.s
    st, stb = stg[qi % len(stg)]
    s.dma("sp" if qi % 2 == 0 else "pool", lambda e: e.dma_start(out=st[:, 0:n], in_=src), writes=[stb])
    ek = ("act", "pool", "dve")[qi % 3]
    if ek == "act":
        s.op("act", lambda e: e.copy(out=dst, in_=st[:, 0:n]), reads=[stb], writes=[dstb])
    else:
        s.op(ek, lambda e: e.tensor_copy(out=dst, in_=st[:, 0:n]), reads=[stb], writes=[dstb])


def load_w_bf16(k, stg, wt, wb, wdram, kc, n, q0=0):
    qi = q0
    v = wdram.rearrange("(kc p) n -> p kc n", p=128)
    cw = stg[0][0].shape[1]
    for c in range(kc):
        for c0 in range(0, n, cw):
            c1 = min(n, c0 + cw)
            load_cast(k, stg, wt[:, c, c0:c1], wb, v[:, c, c0:c1], c1 - c0, qi)
            qi += 1
    return qi


def rstd_from_ss(k, ss, ssb, rs, rsb, inv_n, w):
    s = k.s
    s.op("act", lambda e: e.activation(out=rs[:, 0:w], in_=ss[:, 0:w], func=AF.Sqrt, scale=inv_n, bias=k.eps[:, 0:1]),
         reads=[ssb], writes=[rsb])
    s.op("dve", lambda e: e.reciprocal(out=rs[:, 0:w], in_=rs[:, 0:w]), reads=[rsb], writes=[rsb])


def norm_mod(k, xt, xb, A, B, ABb, scr, scrb, st, stb, out, outb, out_f32=None):
    s = k.s
    s.op("act", lambda e: e.activation(out=scr[:], in_=xt[:], func=AF.Square, accum_out=st[:, 0:1]),
         reads=[xb], writes=[scrb, stb])
    rstd_from_ss(k, st, stb, st, stb, 1.0 / D, 1)
    s.op("dve", lambda e: e.scalar_tensor_tensor(out=scr[:], in0=xt[:], scalar=st[:, 0:1], in1=A, op0=ALU.mult, op1=ALU.mult),
         reads=[xb, stb, ABb], writes=[scrb])
    if out_f32 is not None:
        of, ofb = out_f32
        s.op("pool", lambda e: e.tensor_tensor(out=of[:], in0=scr[:], in1=B, op=ALU.add), reads=[scrb, ABb], writes=[ofb])
        s.op("act", lambda e: e.copy(out=out[:], in_=of[:]), reads=[ofb], writes=[outb])
    else:
        s.op("pool", lambda e: e.tensor_tensor(out=out[:], in0=scr[:], in1=B, op=ALU.add), reads=[scrb, ABb], writes=[outb])


def transpose_chunks(k, bank, bankb, src_fn, nchunks, rows, dst, dstb, srcb, dst_view=None):
    s = k.s
    bv = bank[:].bitcast(BF16)
    for c in range(nchunks):
        s.op("pe", lambda e: e.transpose(out=bv[0:rows, c * 128:(c + 1) * 128], in_=src_fn(c), identity=k.ident[:]),
             reads=[srcb, k.identb], writes=[bankb])
    src = bv[0:rows, 0:nchunks * 128]
    if dst_view is not None:
        src = src.rearrange("p (c t) -> p c t", t=128)
    s.op("act", lambda e: e.copy(out=dst, in_=src), reads=[bankb], writes=[dstb])


def setup_consts(k, es, ident_d, identf_d=None):
    s = k.s
    k.ident, k.identb = k.sb(es, "ident_sb", [128, 128], BF16)
    k.eps, k.epsb = k.sb(es, "epsc", [128, 1], F32)
    s.dma("sp", lambda e: e.dma_start(out=k.ident[:], in_=ident_d), writes=[k.identb])
    if identf_d is not None:
        k.identf, _ = k.sb(es, "identf_sb", [128, 128], F32)
        s.dma("sp", lambda e: e.dma_start(out=k.identf[:], in_=identf_d), writes=[k.identb])
    s.op("dve", lambda e: e.memset(k.eps[:], EPS), writes=[k.epsb])


def load_bcast(k, tile, tb, row, n):
    k.s.dma("sp", lambda e: e.dma_start(out=tile, in_=row.to_broadcast([128, n])), writes=[tb])


def na_blocks(qt):
    if 2 <= qt <= 13:
        return [(qt - 2 + j, j) for j in range(5)]
    if qt == 0:
        return [(j, 5 + j) for j in range(4)]
    if qt == 1:
        return [(j, 9 + j) for j in range(4)]
    if qt == 14:
        return [(12 + j, 13 + j) for j in range(4)]
    return [(12 + j, 17 + j) for j in range(4)]


def run_pipelined(gens, lag, k=None):
    gens = list(gens)
    active = []
    nxt = 0
    since = lag
    while active or nxt < len(gens):
        if nxt < len(gens) and since >= lag:
            active.append(gens[nxt])
            nxt += 1
            since = 0
        for g in list(active):
            try:
                next(g)
            except StopIteration:
                active.remove(g)
        since += 1
        if k is not None:
            k.poke()


def stage_attn(k, x, ctx, modv0, W, hout, houtb):
    nc, s = k.nc, k.s
    banks = k.banks
    with ExitStack() as es:
        AB, ABb = k.sb(es, "at_AB", [128, 4, 1024], F32)
        osb, osbb = k.sb(es, "at_o", [128, NT, 1024], BF16)
        qT, qTb = k.sb(es, "at_qT", [96, 8, SEQ], BF16)
        kT, kTb = k.sb(es, "at_kT", [96, 8, SEQ + CTX], BF16)
        Vs, Vsb = k.sb(es, "at_V", [128, NT + NCT, 8, 65], BF16)
        st, stb = k.sb(es, "at_st", [128, 4], F32)
        st2, st2b = k.sb(es, "at_st2", [128, 16], F32)
        st3, st3b = k.sb(es, "at_st3", [128, 16], F32)
        xts = [k.sb(es, f"at_x{i}", [128, 1024], F32) for i in range(2)]
        scrs = [k.sb(es, f"at_scr{i}", [128, 1024], F32) for i in range(2)]
        abfs = [k.sb(es, f"at_a{i}", [128, 1024], BF16) for i in range(2)]
        aTs = [k.sb(es, f"at_aT{i}", [128, 8, 128], BF16) for i in range(2)]

        load_bcast(k, AB[:, 0, :], ABb, modv0[0:1, 0, :], 1024)
        load_bcast(k, AB[:, 1, :], ABb, modv0[0:1, 1, :], 1024)
        load_bcast(k, AB[:, 2, :], ABb, modv0[1:2, 0, :], 1024)
        load_bcast(k, AB[:, 3, :], ABb, modv0[1:2, 1, :], 1024)
        s.op("pool", lambda e: e.memset(Vs[:, :, :, 64:65], 1.0), writes=[Vsb])

        def src_tile(t):
            return ctx[t * 128:(t + 1) * 128, :] if t < NCT else x[(t - NCT) * 128:(t - NCT + 1) * 128, :]

        def front(t):
            xt, xb = xts[t % 2]
            scr, scrb = scrs[t % 2]
            abf, abfb = abfs[t % 2]
            aT, aTb = aTs[t % 2]
            s.dma("sp", lambda e: e.dma_start(out=xt[:], in_=src_tile(t)), writes=[xb])
            j = 2 if t < NCT else 0
            norm_mod(k, xt, xb, AB[:, j, :], AB[:, j + 1, :], ABb, scr, scrb, st, stb, abf, abfb)
            bank, bb = banks[7]
            transpose_chunks(k, bank, bb, lambda c: abf[:, c * 128:(c + 1) * 128], 8, 128, aT[:], aTb, abfb, dst_view=True)
            return aT, aTb, scr, scrb

        with ExitStack() as es1:
            w_in, w_inb = k.sb(es1, "p1_win", [128, 8, 672], BF16)
            w_q, w_qb = k.sb(es1, "p1_wq", [128, 3, 768], BF16)
            w_kv, w_kvb = k.sb(es1, "p1_wkv", [128, 2, 1024], BF16)
            gcn, gcnb = k.sb(es1, "p1_gcn", [128, 640], F32)
            gq, gqb = k.sb(es1, "p1_gq", [128, 96], F32)
            gk, gkb = k.sb(es1, "p1_gk", [128, 96], F32)
            zsbs = [k.sb(es1, f"p1_z{i}", [128, 672], F32) for i in range(2)]
            cn, cnb = k.sb(es1, "p1_cn", [128, 640], BF16)
            cnT, cnTb = k.sb(es1, "p1_cnT", [128, 5, 128], BF16)
            qn, qnb = k.sb(es1, "p1_qn", [128, 8, 96], F32)
            kn, knb = k.sb(es1, "p1_kn", [128, 8, 64], F32)
            qr, qrb = k.sb(es1, "p1_qr", [128, 8, 32], F32)
            rt, rtb = k.sb(es1, "p1_rt", [128, 4, 8, 16], F32)
            krg, krgb = k.sb(es1, "p1_krg", [128, 32], F32)
            krr, krrb = k.sb(es1, "p1_krr", [128, 32], F32)
            kt4, kt4b = k.sb(es1, "p1_kt4", [128, 4, 16], F32)
            qf, qfb = k.sb(es1, "p1_qf", [128, 8, 96], BF16)
            kf, kfb = k.sb(es1, "p1_kf", [128, 8, 96], BF16)
            ropes = [k.sb(es1, f"p1_rope{i}", [128, 32], F32) for i in range(2)]

            wv = W["attn_w_in"].rearrange("(kc p) n -> p kc n", p=128)
            for c in range(8):
                s.dma("pool", lambda e: e.dma_start(out=w_in[:, c, :], in_=wv[:, c, 0:672]), writes=[w_inb])
            wv = W["mla_w_q_up"].rearrange("(kc p) n -> p kc n", p=128)
            for c in range(3):
                s.dma("pool", lambda e: e.dma_start(out=w_q[:, c, :], in_=wv[:, c, :]), writes=[w_qb])
            wv = W["mla_w_kv_up"].rearrange("(kc p) n -> p kc n", p=128)
            for c in range(2):
                s.dma("pool", lambda e: e.dma_start(out=w_kv[:, c, :], in_=wv[:, c, :]), writes=[w_kvb])
            load_bcast(k, gcn[:, 0:384], gcnb, W["mla_g_qa"], 384)
            load_bcast(k, gcn[:, 384:640], gcnb, W["mla_g_kva"], 256)
            load_bcast(k, gq[:], gqb, W["mla_g_q"], 96)
            load_bcast(k, gk[:], gkb, W["mla_g_k"], 96)
            s.op("dve", lambda e: e.tensor_scalar_mul(out=gq[:], in0=gq[:], scalar1=96.0 ** -0.5), reads=[gqb], writes=[gqb])

            def p1_tile(t):
                lat = t >= NCT
                zsb, zsbb = zsbs[t % 2]
                aT, aTb, scr, scrb = front(t)
                if lat:
                    rp, rpb = ropes[t % 2]
                    s.dma("sp", lambda e: e.dma_start(out=rp[:], in_=W["rope"][(t - NCT) * 128:(t - NCT + 1) * 128, :]), writes=[rpb])
                yield
                (z0, z0b), (z1, z1b) = banks[0], banks[1]
                for (zb, zbb, c0, c1) in ((z0, z0b, 0, 512), (z1, z1b, 512, 672)):
                    for c in range(8):
                        s.op("pe", lambda e: e.matmul(zb[:, 0:c1 - c0], lhsT=aT[:, c, :], rhs=w_in[:, c, c0:c1], start=(c == 0), stop=(c == 7)),
                             reads=[aTb, w_inb], writes=[zbb])
                    s.op("act", lambda e: e.copy(out=zsb[:, c0:c1], in_=zb[:, 0:c1 - c0]), reads=[zbb], writes=[zsbb])
                yield
                if lat:
                    s.op("act", lambda e: e.activation(out=scr[:, 0:384], in_=zsb[:, 0:384], func=AF.Square, accum_out=st2[:, 0:1]),
                         reads=[zsbb], writes=[scrb, st2b])
                    rstd_from_ss(k, st2, st2b, st3, st3b, 1.0 / 384, 1)
                    s.op("dve", lambda e: e.scalar_tensor_tensor(out=cn[:, 0:384], in0=zsb[:, 0:384], scalar=st3[:, 0:1], in1=gcn[:, 0:384],
                                                                 op0=ALU.mult, op1=ALU.mult), reads=[zsbb, st3b, gcnb], writes=[cnb])
                s.op("act", lambda e: e.activation(out=scr[:, 384:640], in_=zsb[:, 384:640], func=AF.Square, accum_out=st2[:, 1:2]),
                     reads=[zsbb], writes=[scrb, st2b])
                s.op("act", lambda e: e.activation(out=st3[:, 1:2], in_=st2[:, 1:2], func=AF.Sqrt, scale=1.0 / 256, bias=k.eps[:, 0:1]),
                     reads=[st2b], writes=[st3b])
                s.op("dve", lambda e: e.reciprocal(out=st3[:, 1:2], in_=st3[:, 1:2]), reads=[st3b], writes=[st3b])
                s.op("dve", lambda e: e.scalar_tensor_tensor(out=cn[:, 384:640], in0=zsb[:, 384:640], scalar=st3[:, 1:2], in1=gcn[:, 384:640],
                                                             op0=ALU.mult, op1=ALU.mult), reads=[zsbb, st3b, gcnb], writes=[cnb])
                s.op("act", lambda e: e.activation(out=scr[:, 640:672], in_=zsb[:, 640:672], func=AF.Square, accum_out=st2[:, 2:3]),
                     reads=[zsbb], writes=[scrb, st2b])
                yield
                c_lo = 0 if lat else 3
                bank, bb = banks[7]
                bv = bank[:].bitcast(BF16)
                for c in range(c_lo, 5):
                    s.op("pe", lambda e: e.transpose(out=bv[:, c * 128:(c + 1) * 128], in_=cn[:, c * 128:(c + 1) * 128], identity=k.ident[:]),
                         reads=[cnb, k.identb], writes=[bb])
                s.op("act", lambda e: e.copy(out=cnT[:, c_lo:5, :], in_=bv[:, c_lo * 128:640].rearrange("p (c t) -> p c t", t=128)),
                     reads=[bb], writes=[cnTb])
                yield
                if lat:
                    (qa, qab), (qb_, qbb) = banks[2], banks[3]
                    for (qk, qkb, h0, h1) in ((qa, qab, 0, 5), (qb_, qbb, 5, 8)):
                        n = (h1 - h0) * 96
                        for c in range(3):
                            s.op("pe", lambda e: e.matmul(qk[:, 0:n], lhsT=cnT[:, c, :], rhs=w_q[:, c, h0 * 96:h1 * 96], start=(c == 0), stop=(c == 2)),
                                 reads=[cnTb, w_qb], writes=[qkb])
                        s.op("act", lambda e: e.activation(out=scr[:, h0 * 96:h1 * 96], in_=qk[:, 0:n], func=AF.Square), reads=[qkb], writes=[scrb])
                    s.op("dve", lambda e: e.tensor_reduce(out=st2[:, 4:12], in_=scr[:, 0:768].rearrange("p (h d) -> p h d", d=96), axis=AX.X, op=ALU.add),
                         reads=[scrb], writes=[st2b])
                    s.op("act", lambda e: e.activation(out=st3[:, 4:12], in_=st2[:, 4:12], func=AF.Sqrt, scale=1.0 / 96, bias=k.eps[:, 0:1]),
                         reads=[st2b], writes=[st3b])
                    s.op("dve", lambda e: e.reciprocal(out=st3[:, 4:12], in_=st3[:, 4:12]), reads=[st3b], writes=[st3b])
                    for (qk, qkb, h0, h1) in ((qa, qab, 0, 5), (qb_, qbb, 5, 8)):
                        n = (h1 - h0) * 96
                        s.op("dve", lambda e: e.tensor_tensor(out=qn[:, h0:h1, :], in0=qk[:, 0:n].rearrange("p (h d) -> p h d", d=96),
                                                              in1=st3[:, 4 + h0:4 + h1].unsqueeze(2).to_broadcast([128, h1 - h0, 96]), op=ALU.mult),
                             reads=[qkb, st3b], writes=[qnb])
                    s.op("pool", lambda e: e.tensor_tensor(out=qf[:, :, 0:64], in0=qn[:, :, 0:64],
                                                           in1=gq[:, 0:64].unsqueeze(1).to_broadcast([128, 8, 64]), op=ALU.mult),
                         reads=[qnb, gqb], writes=[qfb])
                    s.op("dve", lambda e: e.tensor_tensor(out=qr[:], in0=qn[:, :, 64:96],
                                                          in1=gq[:, 64:96].unsqueeze(1).to_broadcast([128, 8, 32]), op=ALU.mult),
                         reads=[qnb, gqb], writes=[qrb])
                    cosb = rp[:, 0:16].unsqueeze(1).to_broadcast([128, 8, 16])
                    sinb = rp[:, 16:32].unsqueeze(1).to_broadcast([128, 8, 16])
                    s.op("dve", lambda e: e.tensor_tensor(out=rt[:, 0], in0=qr[:, :, 0:16], in1=cosb, op=ALU.mult), reads=[qrb, rpb], writes=[rtb])
                    s.op("dve", lambda e: e.tensor_tensor(out=rt[:, 1], in0=qr[:, :, 16:32], in1=sinb, op=ALU.mult), reads=[qrb, rpb], writes=[rtb])
                    s.op("dve", lambda e: e.tensor_tensor(out=rt[:, 2], in0=qr[:, :, 0:16], in1=sinb, op=ALU.mult), reads=[qrb, rpb], writes=[rtb])
                    s.op("dve", lambda e: e.tensor_tensor(out=rt[:, 3], in0=qr[:, :, 16:32], in1=cosb, op=ALU.mult), reads=[qrb, rpb], writes=[rtb])
                    s.op("dve", lambda e: e.tensor_tensor(out=qf[:, :, 64:80], in0=rt[:, 0], in1=rt[:, 1], op=ALU.subtract), reads=[rtb], writes=[qfb])
                    s.op("dve", lambda e: e.tensor_tensor(out=qf[:, :, 80:96], in0=rt[:, 2], in1=rt[:, 3], op=ALU.add), reads=[rtb], writes=[qfb])
                yield
                (ka, kab), (kb_, kbb) = banks[4], banks[5]
                for g, (kk, kkb) in enumerate(((ka, kab), (kb_, kbb))):
                    for c in range(2):
                        s.op("pe", lambda e: e.matmul(kk[:, :], lhsT=cnT[:, 3 + c, :], rhs=w_kv[:, c, g * 512:(g + 1) * 512], start=(c == 0), stop=(c == 1)),
                             reads=[cnTb, w_kvb], writes=[kkb])
                    kv3 = kk[:, :].rearrange("p (h d) -> p h d", d=128)
                    s.op("act", lambda e: e.activation(out=scr[:, g * 256:(g + 1) * 256].rearrange("p (h d) -> p h d", d=64), in_=kv3[:, :, 0:64], func=AF.Square),
                         reads=[kkb], writes=[scrb])
                    s.op("act", lambda e: e.copy(out=Vs[:, t, g * 4:(g + 1) * 4, 0:64], in_=kv3[:, :, 64:128]), reads=[kkb], writes=[Vsb])
                yield
                s.op("dve", lambda e: e.tensor_reduce(out=st2[:, 4:12], in_=scr[:, 0:512].rearrange("p (h d) -> p h d", d=64), axis=AX.X, op=ALU.add),
                     reads=[scrb], writes=[st2b])
                s.op("dve", lambda e: e.tensor_scalar(out=st2[:, 4:12], in0=st2[:, 4:12], scalar1=st2[:, 2:3], scalar2=None, op0=ALU.add),
                     reads=[st2b], writes=[st2b])
                s.op("act", lambda e: e.activation(out=st3[:, 4:12], in_=st2[:, 4:12], func=AF.Sqrt, scale=1.0 / 96, bias=k.eps[:, 0:1]),
                     reads=[st2b], writes=[st3b])
                s.op("dve", lambda e: e.reciprocal(out=st3[:, 4:12], in_=st3[:, 4:12]), reads=[st3b], writes=[st3b])
                for g, (kk, kkb) in enumerate(((ka, kab), (kb_, kbb))):
                    kv3 = kk[:, :].rearrange("p (h d) -> p h d", d=128)
                    s.op("dve", lambda e: e.tensor_tensor(out=kn[:, g * 4:(g + 1) * 4, :], in0=kv3[:, :, 0:64],
                                                          in1=st3[:, 4 + g * 4:8 + g * 4].unsqueeze(2).to_broadcast([128, 4, 64]), op=ALU.mult),
                         reads=[kkb, st3b], writes=[knb])
                s.op("pool", lambda e: e.tensor_tensor(out=kf[:, :, 0:64], in0=kn[:], in1=gk[:, 0:64].unsqueeze(1).to_broadcast([128, 8, 64]), op=ALU.mult),
                     reads=[knb, gkb], writes=[kfb])
                s.op("dve", lambda e: e.tensor_tensor(out=krg[:], in0=zsb[:, 640:672], in1=gk[:, 64:96], op=ALU.mult), reads=[zsbb, gkb], writes=[krgb])
                if lat:
                    s.op("dve", lambda e: e.tensor_tensor(out=kt4[:, 0], in0=krg[:, 0:16], in1=rp[:, 0:16], op=ALU.mult), reads=[krgb, rpb], writes=[kt4b])
                    s.op("dve", lambda e: e.tensor_tensor(out=kt4[:, 1], in0=krg[:, 16:32], in1=rp[:, 16:32], op=ALU.mult), reads=[krgb, rpb], writes=[kt4b])
                    s.op("dve", lambda e: e.tensor_tensor(out=kt4[:, 2], in0=krg[:, 0:16], in1=rp[:, 16:32], op=ALU.mult), reads=[krgb, rpb], writes=[kt4b])
                    s.op("dve", lambda e: e.tensor_tensor(out=kt4[:, 3], in0=krg[:, 16:32], in1=rp[:, 0:16], op=ALU.mult), reads=[krgb, rpb], writes=[kt4b])
                    s.op("dve", lambda e: e.tensor_tensor(out=krr[:, 0:16], in0=kt4[:, 0], in1=kt4[:, 1], op=ALU.subtract), reads=[kt4b], writes=[krrb])
                    s.op("dve", lambda e: e.tensor_tensor(out=krr[:, 16:32], in0=kt4[:, 2], in1=kt4[:, 3], op=ALU.add), reads=[kt4b], writes=[krrb])
                else:
                    s.op("dve", lambda e: e.tensor_copy(out=krr[:], in_=krg[:]), reads=[krgb], writes=[krrb])
                s.op("dve", lambda e: e.tensor_tensor(out=kf[:, :, 64:96], in0=krr[:].unsqueeze(1).to_broadcast([128, 8, 32]),
                                                      in1=st3[:, 4:12].unsqueeze(2).to_broadcast([128, 8, 32]), op=ALU.mult),
                     reads=[krrb, st3b], writes=[kfb])
                yield
                if lat:
                    tb_, tbb = banks[6]
                    tl = t - NCT
                    transpose_chunks(k, tb_, tbb, lambda h: qf[:, h, :], 8, 96, qT[:, :, tl * 128:(tl + 1) * 128], qTb, qfb, dst_view=True)
                tb_, tbb = banks[0]
                transpose_chunks(k, tb_, tbb, lambda h: kf[:, h, :], 8, 96, kT[:, :, t * 128:(t + 1) * 128], kTb, kfb, dst_view=True)
            run_pipelined([p1_tile(t) for t in range(NT + NCT)], 4, k)
            s.barrier()

        with ExitStack() as es2:
            pTs = [k.sb(es2, f"p2_pT{i}", [128, 512], BF16) for i in range(3)]
            rc, rcb = k.sb(es2, "p2_rc", [128, 8], F32)
            steps = [(h, g, kt) for h in range(8) for g in range(4) for kt in range(NT + NCT)]

            def emit_s(i):
                h, g, kt = steps[i]
                sbk, sbkb = banks[i % 2]
                pT, pTb = pTs[i % 3]
                s.op("pe", lambda e: e.matmul(sbk[:, :], lhsT=kT[:, h, kt * 128:(kt + 1) * 128], rhs=qT[:, h, g * 512:(g + 1) * 512], start=True, stop=True),
                     reads=[kTb, qTb], writes=[sbkb])
                s.op("act", lambda e: e.activation(out=pT[:], in_=sbk[:, :], func=AF.Exp), reads=[sbkb], writes=[pTb])

            def emit_pv(i):
                h, g, kt = steps[i]
                pT, pTb = pTs[i % 3]
                for qs in range(4):
                    ob, obb = banks[2 + qs]
                    s.op("pe", lambda e: e.matmul(ob[:, 0:65], lhsT=pT[:, qs * 128:(qs + 1) * 128], rhs=Vs[:, kt, h, :],
                                                  start=(kt == 0), stop=(kt == NT + NCT - 1)), reads=[pTb, Vsb], writes=[obb])
                if kt == NT + NCT - 1:
                    for qs in range(4):
                        ob, obb = banks[2 + qs]
                        j = (g * 4 + qs) % 8
                        s.op("dve", lambda e: e.reciprocal(out=rc[:, j:j + 1], in_=ob[:, 64:65]), reads=[obb], writes=[rcb])
                        s.op("dve", lambda e: e.tensor_scalar(out=osb[:, g * 4 + qs, h * 64:(h + 1) * 64], in0=ob[:, 0:64], scalar1=rc[:, j:j + 1],
                                                              scalar2=None, op0=ALU.mult), reads=[obb, rcb], writes=[osbb])

            emit_s(0)
            for i in range(len(steps)):
                if i + 1 < len(steps):
                    emit_s(i + 1)
                emit_pv(i)
                if i % 8 == 7:
                    k.poke()
            s.barrier()

        with ExitStack() as es3:
            stg = [k.sb(es3, f"p3_stg{i}", [128, 1536], F32) for i in range(2)]
            w_in, w_inb = k.sb(es3, "p3_win", [128, 8, 1536], BF16)
            gqn, gqnb = k.sb(es3, "p3_gq", [128, 64], F32)
            gkn, gknb = k.sb(es3, "p3_gk", [128, 64], F32)
            tn, tnb = k.sb(es3, "p3_tn", [128, 16, 64], F32)
            qkf, qkfb = k.sb(es3, "p3_qkf", [128, 16, 64], BF16)
            wv = W["attn_w_in"].rearrange("(kc p) n -> p kc n", p=128)
            for c in range(8):
                load_cast(k, stg, w_in[:, c, :], w_inb, wv[:, c, 672:2208], 1536, c)
            load_bcast(k, gqn[:], gqnb, W["na_g_q"], 64)
            load_bcast(k, gkn[:], gknb, W["na_g_k"], 64)
            s.op("dve", lambda e: e.tensor_scalar_mul(out=gqn[:], in0=gqn[:], scalar1=64.0 ** -0.5), reads=[gqnb], writes=[gqnb])
            def p3_tile(t):
                lat = t >= NCT
                aT, aTb, scr, scrb = front(t)
                yield
                grp = (0, 1, 2) if lat else (1, 2)
                for g in grp:
                    zb, zbb = banks[g]
                    for c in range(8):
                        s.op("pe", lambda e: e.matmul(zb[:, :], lhsT=aT[:, c, :], rhs=w_in[:, c, g * 512:(g + 1) * 512], start=(c == 0), stop=(c == 7)),
                             reads=[aTb, w_inb], writes=[zbb])
                    if g < 2:
                        s.op("act", lambda e: e.activation(out=scr[:, g * 512:(g + 1) * 512], in_=zb[:, :], func=AF.Square), reads=[zbb], writes=[scrb])
                    else:
                        s.op("act", lambda e: e.copy(out=Vs[:, t, :, 0:64], in_=zb[:, :].rearrange("p (h d) -> p h d", d=64)), reads=[zbb], writes=[Vsb])
                yield
                g0 = 0 if lat else 1
                s.op("dve", lambda e: e.tensor_reduce(out=st2[:, g0 * 8:16], in_=scr[:, g0 * 512:1024].rearrange("p (h d) -> p h d", d=64), axis=AX.X, op=ALU.add),
                     reads=[scrb], writes=[st2b])
                s.op("act", lambda e: e.activation(out=st3[:, g0 * 8:16], in_=st2[:, g0 * 8:16], func=AF.Sqrt, scale=1.0 / 64, bias=k.eps[:, 0:1]),
                     reads=[st2b], writes=[st3b])
                s.op("dve", lambda e: e.reciprocal(out=st3[:, g0 * 8:16], in_=st3[:, g0 * 8:16]), reads=[st3b], writes=[st3b])
                for g in grp[:-1]:
                    zb, zbb = banks[g]
                    gg, ggb = (gqn, gqnb) if g == 0 else (gkn, gknb)
                    s.op("dve", lambda e: e.tensor_tensor(out=tn[:, g * 8:(g + 1) * 8, :], in0=zb[:, :].rearrange("p (h d) -> p h d", d=64),
                                                          in1=st3[:, g * 8:(g + 1) * 8].unsqueeze(2).to_broadcast([128, 8, 64]), op=ALU.mult),
                         reads=[zbb, st3b], writes=[tnb])
                    s.op("pool", lambda e: e.tensor_tensor(out=qkf[:, g * 8:(g + 1) * 8, :], in0=tn[:, g * 8:(g + 1) * 8, :],
                                                           in1=gg[:].unsqueeze(1).to_broadcast([128, 8, 64]), op=ALU.mult),
                         reads=[tnb, ggb], writes=[qkfb])
                yield
                if lat:
                    tb_, tbb = banks[3]
                    tl = t - NCT
                    transpose_chunks(k, tb_, tbb, lambda h: qkf[:, h, :], 8, 64, qT[0:64, :, tl * 128:(tl + 1) * 128], qTb, qkfb, dst_view=True)
                tb_, tbb = banks[4]
                transpose_chunks(k, tb_, tbb, lambda h: qkf[:, 8 + h, :], 8, 64, kT[0:64, :, t * 128:(t + 1) * 128], kTb, qkfb, dst_view=True)
            run_pipelined([p3_tile(t) for t in range(NT + NCT)], 2, k)
            s.barrier()

        with ExitStack() as es4:
            nbs = [k.sb(es4, f"p4_nb{i}", [128, 21, 128], F32) for i in range(2)]
            sfs = [k.sb(es4, f"p4_sf{i}", [128, 640], F32) for i in range(2)]
            pTs = [k.sb(es4, f"p4_pT{i}", [128, 896], BF16) for i in range(2)]
            rc, rcb = k.sb(es4, "p4_rc", [128, 8], F32)
            steps = [(h, qt) for h in range(8) for qt in range(NT)]

            def res(i):
                return (banks[(i % 2) * 2], banks[(i % 2) * 2 + 1], sfs[i % 2], pTs[i % 2], banks[4 + i % 2])

            def emit_s(i):
                h, qt = steps[i]
                nb, nbb = nbs[h % 2]
                if qt == 0:
                    s.dma("sp", lambda e: e.dma_start(out=nb[:], in_=W["nabias"][h]), writes=[nbb])
                blocks = na_blocks(qt)
                nloc = len(blocks)
                (sa, sab), (sb_, sbb), (sf, sfb), (pT, pTb), _ = res(i)
                qsl = qT[0:64, h, qt * 128:(qt + 1) * 128]
                for j, (kt, bi) in enumerate(blocks):
                    dstb_, dstbb = (sa, sab) if j < 4 else (sb_, sbb)
                    col = (j % 4) * 128
                    s.op("pe", lambda e: e.matmul(dstb_[:, col:col + 128], lhsT=kT[0:64, h, (NCT + kt) * 128:(NCT + kt + 1) * 128], rhs=qsl, start=True, stop=True),
                         reads=[kTb, qTb], writes=[dstbb])
                for c in range(NCT):
                    s.op("pe", lambda e: e.matmul(sb_[:, 128 + c * 128:256 + c * 128], lhsT=kT[0:64, h, c * 128:(c + 1) * 128], rhs=qsl, start=True, stop=True),
                         reads=[kTb, qTb], writes=[sbb])
                b0 = blocks[0][1]
                s.op("dve", lambda e: e.tensor_tensor(out=sf[:, 0:512], in0=sa[:, :], in1=nb[:, b0:b0 + 4, :].rearrange("p b q -> p (b q)"), op=ALU.add),
                     reads=[sab, nbb], writes=[sfb])
                if nloc == 5:
                    s.op("dve", lambda e: e.tensor_tensor(out=sf[:, 512:640], in0=sb_[:, 0:128], in1=nb[:, b0 + 4, :], op=ALU.add),
                         reads=[sbb, nbb], writes=[sfb])
                s.op("act", lambda e: e.activation(out=pT[:, 0:nloc * 128], in_=sf[:, 0:nloc * 128], func=AF.Exp), reads=[sfb], writes=[pTb])
                s.op("act", lambda e: e.activation(out=pT[:, 640:896], in_=sb_[:, 128:384], func=AF.Exp), reads=[sbb], writes=[pTb])

            def emit_pv(i):
                h, qt = steps[i]
                blocks = na_blocks(qt)
                _, _, _, (pT, pTb), (ob, obb) = res(i)
                for j, (kt, bi) in enumerate(blocks):
                    s.op("pe", lambda e: e.matmul(ob[:, 0:65], lhsT=pT[:, j * 128:(j + 1) * 128], rhs=Vs[:, NCT + kt, h, :], start=(j == 0), stop=False),
                         reads=[pTb, Vsb], writes=[obb])
                for c in range(NCT):
                    s.op("pe", lambda e: e.matmul(ob[:, 0:65], lhsT=pT[:, 640 + c * 128:768 + c * 128], rhs=Vs[:, c, h, :], start=False, stop=(c == NCT - 1)),
                         reads=[pTb, Vsb], writes=[obb])
                j8 = i % 8
                s.op("dve", lambda e: e.reciprocal(out=rc[:, j8:j8 + 1], in_=ob[:, 64:65]), reads=[obb], writes=[rcb])
                s.op("dve", lambda e: e.tensor_scalar(out=osb[:, qt, 512 + h * 64:512 + (h + 1) * 64], in0=ob[:, 0:64], scalar1=rc[:, j8:j8 + 1],
                                                      scalar2=None, op0=ALU.mult), reads=[obb, rcb], writes=[osbb])

            emit_s(0)
            for i in range(len(steps)):
                if i + 1 < len(steps):
                    emit_s(i + 1)
                emit_pv(i)
                if i % 8 == 7:
                    k.poke()
            s.barrier()

        with ExitStack() as es5:
            stg = [k.sb(es5, f"p5_stg{i}", [128, 1024], F32) for i in range(2)]
            w_o, w_ob = k.sb(es5, "p5_wo", [128, 8, 1024], BF16)
            G1, G1b = k.sb(es5, "p5_g1", [128, 1024], F32)
            outs = [k.sb(es5, f"p5_out{i}", [128, 1024], F32) for i in range(2)]
            wv = W["attn_w_out"].rearrange("(kc p) n -> p kc n", p=128)
            for c in range(8):
                load_cast(k, stg, w_o[:, c, :], w_ob, wv[:, c, :], 1024, c)
            load_bcast(k, G1[:], G1b, modv0[0:1, 2, :], 1024)
            for t in range(NT):
                xt, xb = xts[t % 2]
                scr, scrb = scrs[t % 2]
                aT, aTb = aTs[t % 2]
                ot, otb = outs[t % 2]
                s.dma("sp", lambda e: e.dma_start(out=xt[:], in_=x[t * 128:(t + 1) * 128, :]), writes=[xb])
                bank, bb = banks[7]
                transpose_chunks(k, bank, bb, lambda c: osb[:, t, c * 128:(c + 1) * 128], 8, 128, aT[:], aTb, osbb, dst_view=True)
                for g in range(2):
                    yb, ybb = banks[g]
                    for c in range(8):
                        s.op("pe", lambda e: e.matmul(yb[:, :], lhsT=aT[:, c, :], rhs=w_o[:, c, g * 512:(g + 1) * 512], start=(c == 0), stop=(c == 7)),
                             reads=[aTb, w_ob], writes=[ybb])
                    s.op("dve", lambda e: e.tensor_tensor(out=scr[:, g * 512:(g + 1) * 512], in0=yb[:, :], in1=G1[:, g * 512:(g + 1) * 512], op=ALU.mult),
                         reads=[ybb, G1b], writes=[scrb])
                s.op("pool", lambda e: e.tensor_tensor(out=ot[:], in0=scr[:], in1=xt[:], op=ALU.add), reads=[scrb, xb], writes=[otb])
                s.dma("sp", lambda e: e.dma_start(out=hout[t * 128:(t + 1) * 128, :], in_=ot[:]), reads=[otb], writes=[houtb[t]])
            s.barrier()


def host_rope_table():
    t = np.arange(SEQ)
    row = (t // 64).astype(np.float32)
    col = (t % 64).astype(np.float32)
    inv = (np.float32(1.0) / (np.float32(10000.0) ** (np.arange(8, dtype=np.float32) / np.float32(8)))).astype(np.float32)
    ang = np.concatenate([row[:, None] * inv, col[:, None] * inv], axis=-1).astype(np.float32)
    return np.concatenate([np.cos(ang), np.sin(ang)], axis=-1).astype(np.float32)


def host_na_bias(rpb):
    pairs = [(2, j) for j in range(5)] + [(0, j) for j in range(4)] + [(1, j) for j in range(4)] \
        + [(14, 12 + j) for j in range(4)] + [(15, 12 + j) for j in range(4)]
    out = np.full((8, 128, 21, 128), NEG, np.float32)
    p = np.arange(128)
    for bi, (qt, kt) in enumerate(pairs):
        tq = qt * 128 + p
        tk = kt * 128 + p
        r, c = tq // 64, tq % 64
        kr, kc = tk // 64, tk % 64
        r0 = np.clip(r - 4, 0, 24)
        c0 = np.clip(c - 8, 0, 48)
        inside = (kr[:, None] >= r0[None, :]) & (kr[:, None] < r0[None, :] + 8) & (kc[:, None] >= c0[None, :]) & (kc[:, None] < c0[None, :] + 16)
        rr = np.clip(kr[:, None] - r[None, :] + 7, 0, 14)
        rc = np.clip(kc[:, None] - c[None, :] + 15, 0, 30)
        vals = rpb[:, rr, rc]
        out[:, :, bi, :] = np.where(inside[None], vals, np.float32(NEG))
    return out


NROW = 8


def stage_peer(k, hin, hinb, modv_l, w_query, skT_d, u_tab, v_tab, hout, houtb, tag):
    nc, s = k.nc, k.s
    banks = k.banks
    with ExitStack() as es:
        stg = [k.sb(es, f"{tag}_stg{i}", [128, 2048], F32) for i in range(2)]
        wq, wqb = k.sb(es, f"{tag}_wq", [128, 8, 2048], BF16)
        skT, skTb = k.sb(es, f"{tag}_skT", [128, 16, 128], BF16)
        ABG, ABGb = k.sb(es, f"{tag}_ABG", [128, 3, 1024], F32)
        iota_i, iota_ib = k.sb(es, f"{tag}_iotai", [128, 16], I32)
        iota, iotab = k.sb(es, f"{tag}_iota", [128, 16], F32)
        st, stb = k.sb(es, f"{tag}_st", [128, 4], F32)
        xts = [k.sb(es, f"{tag}_x{i}", [128, 1024], F32) for i in range(2)]
        scrs = [k.sb(es, f"{tag}_scr{i}", [128, 1024], F32) for i in range(2)]
        hms = [k.sb(es, f"{tag}_hm{i}", [128, 1024], F32) for i in range(2)]
        hbf, hbfb = k.sb(es, f"{tag}_hbf", [128, 1024], BF16)
        hT, hTb = k.sb(es, f"{tag}_hT", [128, 8, 128], BF16)
        qbf, qbfb = k.sb(es, f"{tag}_qbf", [128, 2048], BF16)
        qT, qTb = k.sb(es, f"{tag}_qT", [128, 16, 128], BF16)
        ssb, ssbb = k.sb(es, f"{tag}_s", [128, 16, 128], F32)
        s2, _ = k.sb(es, f"{tag}_s2", [128, 16, 128], F32)
        m16, _ = k.sb(es, f"{tag}_m16", [128, 16, 16], F32)
        i16, _ = k.sb(es, f"{tag}_i16", [128, 16, 16], U32)
        i16f, i16fb = k.sb(es, f"{tag}_i16f", [128, 16, 16], F32)
        cand, candb = k.sb(es, f"{tag}_cand", [128, 8, 256], F32)
        cand2, _ = k.sb(es, f"{tag}_cand2", [128, 8, 256], F32)
        best, _ = k.sb(es, f"{tag}_best", [128, 8, 16], F32)
        pos, _ = k.sb(es, f"{tag}_pos", [128, 8, 16], U32)
        ab_i, ab_ib = k.sb(es, f"{tag}_abi", [128, 2, 128], I32)
        ab_f, ab_fb = k.sb(es, f"{tag}_abf", [128, 2, 128], F32)
        oh, ohb = k.sb(es, f"{tag}_oh", [128, 8, 16, 16], F32)
        e01, e01b = k.sb(es, f"{tag}_e01", [128, 2, 128], F32)
        idxs = [k.sb(es, f"{tag}_idx{i}", [128, 128], I32) for i in range(2)]
        gts = [k.sb(es, f"{tag}_gate{i}", [128, 8, 16], F32) for i in range(2)]
        gsum, gsumb = k.sb(es, f"{tag}_gsum", [128, 8], F32)
        actv, _ = k.sb(es, f"{tag}_act", [128, 128], F32)
        wgt, wgtb = k.sb(es, f"{tag}_wgt", [128, 128], F32)
        junk, _ = k.sb(es, f"{tag}_junk", [128, 1024], BF16)
        rows = [k.sb(es, f"{tag}_row{i}", [128, 1024], F32) for i in range(NROW)]
        accs = [k.sb(es, f"{tag}_acc{i}", [128, 1024], F32) for i in range(4)]
        ot, otb = k.sb(es, f"{tag}_ot", [128, 1024], F32)
        hpb = [[Buf() for _ in range(16)] for _ in range(3)]
        hb = [[Buf() for _ in range(8)] for _ in range(3)]
        actb = [Buf() for _ in range(16)]

        qi = load_w_bf16(k, stg, wq, wqb, w_query, 8, 2048)
        load_cast(k, stg, skT[:].rearrange("p a b -> p (a b)"), skTb, skT_d.rearrange("p a b -> p (a b)"), 2048, qi)
        for j in range(3):
            load_bcast(k, ABG[:, j, :], ABGb, modv_l[0:1, 3 + j, :], 1024)
        s.op("pool", lambda e: e.iota(out=iota_i[:], pattern=[[1, 16]], base=0, channel_multiplier=0), writes=[iota_ib])
        s.op("dve", lambda e: e.tensor_copy(out=iota[:], in_=iota_i[:]), reads=[iota_ib], writes=[iotab])

        def front(t):
            xt, xb = xts[t % 2]
            scr, scrb = scrs[t % 2]
            hm, hmb = hms[t % 2]
            idx, idxb = idxs[t % 2]
            gate, gateb = gts[t % 2]
            s.dma("sp", lambda e: e.dma_start(out=xt[:], in_=hin[t * 128:(t + 1) * 128, :]), reads=[hinb[t]], writes=[xb])
            norm_mod(k, xt, xb, ABG[:, 0, :], ABG[:, 1, :], ABGb, scr, scrb, st, stb, hbf, hbfb, out_f32=(hm, hmb))
            bank, bb = banks[7]
            transpose_chunks(k, bank, bb, lambda c: hbf[:, c * 128:(c + 1) * 128], 8, 128, hT[:], hTb, hbfb, dst_view=True)
            for g in range(4):
                qb_, qbb = banks[g]
                for c in range(8):
                    s.op("pe", lambda e: e.matmul(qb_[:, :], lhsT=hT[:, c, :], rhs=wq[:, c, g * 512:(g + 1) * 512], start=(c == 0), stop=(c == 7)),
                         reads=[hTb, wqb], writes=[qbb])
                s.op("act", lambda e: e.copy(out=qbf[:, g * 512:(g + 1) * 512], in_=qb_[:, :]), reads=[qbb], writes=[qbfb])
            for half in range(2):
                tb_, tbb = banks[4 + half]
                transpose_chunks(k, tb_, tbb, lambda c: qbf[:, (half * 8 + c) * 128:(half * 8 + c + 1) * 128], 8, 128,
                                 qT[:, half * 8:(half + 1) * 8, :], qTb, qbfb, dst_view=True)
            for g in range(4):
                sb_, sbb = banks[g]
                for j in range(4):
                    hp = g * 4 + j
                    s.op("pe", lambda e: e.matmul(sb_[:, j * 128:(j + 1) * 128], lhsT=qT[:, hp, :], rhs=skT[:, hp, :], start=True, stop=True),
                         reads=[qTb, skTb], writes=[sbb])
                s.op("act", lambda e: e.copy(out=ssb[:, g * 4:(g + 1) * 4, :], in_=sb_[:, :].rearrange("p (a b) -> p a b", b=128)), reads=[sbb], writes=[ssbb])
            for hp in range(16):
                s.op("dve", lambda e: e.max(out=m16[:, hp, 0:8], in_=ssb[:, hp, :]), reads=[ssbb], writes=[hpb[0][hp]])
            for hp in range(16):
                s.op("dve", lambda e: e.max_index(out=i16[:, hp, 0:8], in_max=m16[:, hp, 0:8], in_values=ssb[:, hp, :]),
                     reads=[ssbb, hpb[0][hp]], writes=[hpb[1][hp]])
            for hp in range(16):
                s.op("dve", lambda e: e.match_replace(out=s2[:, hp, :], in_to_replace=m16[:, hp, 0:8], in_values=ssb[:, hp, :], imm_value=-1e30),
                     reads=[ssbb, hpb[0][hp]], writes=[hpb[2][hp]])
            for hp in range(16):
                s.op("dve", lambda e: e.max(out=m16[:, hp, 8:16], in_=s2[:, hp, :]), reads=[hpb[2][hp]], writes=[hpb[0][hp]])
            for hp in range(16):
                s.op("dve", lambda e: e.max_index(out=i16[:, hp, 8:16], in_max=m16[:, hp, 8:16], in_values=s2[:, hp, :]),
                     reads=[hpb[2][hp], hpb[0][hp]], writes=[hpb[1][hp]])
            s.op("dve", lambda e: e.tensor_copy(out=i16f[:], in_=i16[:]), reads=hpb[1], writes=[i16fb])
            m4 = m16[:].rearrange("p (h t) a -> p h t a", t=2)
            s.op("dve", lambda e: e.tensor_tensor(out=cand[:].rearrange("p h (a b) -> p h a b", b=16),
                                                  in0=m4[:, :, 0, :].unsqueeze(3).to_broadcast([128, 8, 16, 16]),
                                                  in1=m4[:, :, 1, :].unsqueeze(2).to_broadcast([128, 8, 16, 16]), op=ALU.add),
                 reads=hpb[0], writes=[candb])
            for h in range(8):
                s.op("dve", lambda e: e.max(out=best[:, h, 0:8], in_=cand[:, h, :]), reads=[candb], writes=[hb[0][h]])
            for h in range(8):
                s.op("dve", lambda e: e.max_index(out=pos[:, h, 0:8], in_max=best[:, h, 0:8], in_values=cand[:, h, :]),
                     reads=[candb, hb[0][h]], writes=[hb[1][h]])
            for h in range(8):
                s.op("dve", lambda e: e.match_replace(out=cand2[:, h, :], in_to_replace=best[:, h, 0:8], in_values=cand[:, h, :], imm_value=-1e30),
                     reads=[candb, hb[0][h]], writes=[hb[2][h]])
            for h in range(8):
                s.op("dve", lambda e: e.max(out=best[:, h, 8:16], in_=cand2[:, h, :]), reads=[hb[2][h]], writes=[hb[0][h]])
            for h in range(8):
                s.op("dve", lambda e: e.max_index(out=pos[:, h, 8:16], in_max=best[:, h, 8:16], in_values=cand2[:, h, :]),
                     reads=[hb[2][h], hb[0][h]], writes=[hb[1][h]])
            posi = pos[:].rearrange("p h k -> p (h k)").bitcast(I32)
            s.op("dve", lambda e: e.tensor_single_scalar(out=ab_i[:, 0, :], in_=posi, scalar=4, op=ALU.arith_shift_right), reads=hb[1], writes=[ab_ib])
            s.op("dve", lambda e: e.tensor_single_scalar(out=ab_i[:, 1, :], in_=posi, scalar=15, op=ALU.bitwise_and), reads=hb[1], writes=[ab_ib])
            s.op("dve", lambda e: e.tensor_copy(out=ab_f[:], in_=ab_i[:]), reads=[ab_ib], writes=[ab_fb])
            i4 = i16f[:].rearrange("p (h t) a -> p h t a", t=2)
            for p_ in range(2):
                s.op("dve", lambda e: e.tensor_tensor(out=oh[:], in0=ab_f[:, p_, :].rearrange("p (h k) -> p h k", k=16).unsqueeze(3).to_broadcast([128, 8, 16, 16]),
                                                      in1=iota[:].unsqueeze(1).unsqueeze(1).to_broadcast([128, 8, 16, 16]), op=ALU.is_equal),
                     reads=[ab_fb, iotab], writes=[ohb])
                s.op("dve", lambda e: e.tensor_tensor(out=oh[:], in0=oh[:], in1=i4[:, :, p_, :].unsqueeze(2).to_broadcast([128, 8, 16, 16]), op=ALU.mult),
                     reads=[ohb, i16fb], writes=[ohb])
                s.op("dve", lambda e: e.tensor_reduce(out=e01[:, p_, :].rearrange("p (h k) -> p h k", k=16), in_=oh[:], axis=AX.X, op=ALU.add),
                     reads=[ohb], writes=[e01b])
            s.op("dve", lambda e: e.scalar_tensor_tensor(out=e01[:, 0, :], in0=e01[:, 0, :], scalar=128.0, in1=e01[:, 1, :], op0=ALU.mult, op1=ALU.add),
                 reads=[e01b], writes=[e01b])
            s.op("dve", lambda e: e.tensor_copy(out=idx[:], in_=e01[:, 0, :]), reads=[e01b], writes=[idxb])
            s.op("dve", lambda e: e.tensor_tensor(out=gate[:], in0=best[:], in1=best[:, :, 0:1].to_broadcast([128, 8, 16]), op=ALU.subtract),
                 reads=hb[0], writes=[gateb])
            s.op("act", lambda e: e.activation(out=gate[:], in_=gate[:], func=AF.Exp), reads=[gateb], writes=[gateb])
            s.op("dve", lambda e: e.tensor_reduce(out=gsum[:], in_=gate[:], axis=AX.X, op=ALU.add), reads=[gateb], writes=[gsumb])
            s.op("dve", lambda e: e.reciprocal(out=gsum[:], in_=gsum[:]), reads=[gsumb], writes=[gsumb])
            s.op("dve", lambda e: e.tensor_tensor(out=gate[:], in0=gate[:], in1=gsum[:].unsqueeze(2).to_broadcast([128, 8, 16]), op=ALU.mult),
                 reads=[gateb, gsumb], writes=[gateb])

        ring = [0]

        def gather(tab, idx, idxb, hk):
            rw, rwb = rows[ring[0] % NROW]
            ring[0] += 1
            s.dma("pool", lambda e: e.indirect_dma_start(out=rw[:], out_offset=None, in_=tab,
                                                         in_offset=bass.IndirectOffsetOnAxis(ap=idx[:, hk:hk + 1], axis=0)),
                  reads=[idxb], writes=[rwb])
            return rw, rwb

        def back(t):
            xt, xb = xts[t % 2]
            scr, scrb = scrs[t % 2]
            hm, hmb = hms[t % 2]
            idx, idxb = idxs[t % 2]
            gate, gateb = gts[t % 2]
            for hk in range(128):
                rw, rwb = gather(u_tab, idx, idxb, hk)
                s.op("dve", lambda e: e.scalar_tensor_tensor(out=junk[:], in0=rw[:], scalar=1.0, in1=hm[:], op0=ALU.mult, op1=ALU.mult,
                                                             accum_out=actv[:, hk:hk + 1]), reads=[rwb, hmb], writes=[actb[hk % 16]])
            s.op("act", lambda e: e.activation(out=wgt[:], in_=actv[:], func=AF.Gelu), reads=actb, writes=[wgtb])
            s.op("dve", lambda e: e.tensor_tensor(out=wgt[:], in0=wgt[:], in1=gate[:].rearrange("p h k -> p (h k)"), op=ALU.mult),
                 reads=[wgtb, gateb], writes=[wgtb])
            for hk in range(128):
                rw, rwb = gather(v_tab, idx, idxb, hk)
                ac, acb = accs[hk % 4]
                if hk < 4:
                    s.op("dve", lambda e: e.tensor_scalar(out=ac[:], in0=rw[:], scalar1=wgt[:, hk:hk + 1], scalar2=None, op0=ALU.mult),
                         reads=[rwb, wgtb], writes=[acb])
                else:
                    s.op("dve", lambda e: e.scalar_tensor_tensor(out=ac[:], in0=rw[:], scalar=wgt[:, hk:hk + 1], in1=ac[:], op0=ALU.mult, op1=ALU.add),
                         reads=[rwb, wgtb, acb], writes=[acb])
            (a0, a0b), (a1, a1b), (a2, a2b), (a3, a3b) = accs
            s.op("pool", lambda e: e.tensor_tensor(out=a0[:], in0=a0[:], in1=a1[:], op=ALU.add), reads=[a0b, a1b], writes=[a0b])
            s.op("pool", lambda e: e.tensor_tensor(out=a2[:], in0=a2[:], in1=a3[:], op=ALU.add), reads=[a2b, a3b], writes=[a2b])
            s.op("pool", lambda e: e.tensor_tensor(out=a0[:], in0=a0[:], in1=a2[:], op=ALU.add), reads=[a0b, a2b], writes=[a0b])
            s.op("dve", lambda e: e.tensor_tensor(out=scr[:], in0=a0[:], in1=ABG[:, 2, :], op=ALU.mult), reads=[a0b, ABGb], writes=[scrb])
            s.op("pool", lambda e: e.tensor_tensor(out=ot[:], in0=scr[:], in1=xt[:], op=ALU.add), reads=[scrb, xb], writes=[otb])
            s.dma("sp", lambda e: e.dma_start(out=hout[t * 128:(t + 1) * 128, :], in_=ot[:]), reads=[otb], writes=[houtb[t]])

        front(0)
        for t in range(NT):
            if t + 1 < NT:
                front(t + 1)
            back(t)
        s.barrier()


def stage_conv(k, hin, hinb, modv_l, W, hout, houtb):
    nc, s = k.nc, k.s
    banks = k.banks
    PADW = SEQ + 30
    with ExitStack() as es:
        cbuf, cbufb = k.sb(es, "cv_cbuf", [128, NT, 1024], F32)
        ABG, ABGb = k.sb(es, "cv_ABG", [128, 2, 1024], F32)
        st, stb = k.sb(es, "cv_st", [128, 8], F32)
        xts = [k.sb(es, f"cv_x{i}", [128, 1024], F32) for i in range(2)]
        scrs = [k.sb(es, f"cv_scr{i}", [128, 1024], F32) for i in range(2)]
        for j in range(2):
            load_bcast(k, ABG[:, j, :], ABGb, modv_l[0:1, j, :], 1024)
        cbt = [Buf() for _ in range(NT // 4)]
        with ExitStack() as es1:
            aTa, aTab = k.sb(es1, "cv_aT", [128, 8, SEQ], BF16)
            ubfs = [k.sb(es1, f"cv_ub{i}", [128, PADW], BF16) for i in range(2)]
            dgt, dgtb = k.sb(es1, "cv_dgt", [128, 12, 128], BF16)
            w1, w1b = k.sb(es1, "cv_w1", [128, 8, 2048], BF16)
            b1T, b1Tb = k.sb(es1, "cv_b1T", [128, 16], F32)
            wdw, wdwb = k.sb(es1, "cv_wdw", [128, 8, 31], F32)
            bdw, bdwb = k.sb(es1, "cv_bdw", [128, 8], F32)
            abfs = [k.sb(es1, f"cv_a{i}", [128, 1024], BF16) for i in range(1)]
            upads = [k.sb(es1, f"cv_up{i}", [128, PADW], F32) for i in range(2)]
            accs = [k.sb(es1, f"cv_acc{i}", [128, SEQ], F32) for i in range(2)]
            sgs = [k.sb(es1, f"cv_sg{i}", [128, 512], F32) for i in range(2)]
            w1v = W["conv_w_pw1"].rearrange("(kc p) n -> p kc n", p=128)
            for c in range(8):
                s.dma("pool", lambda e: e.dma_start(out=w1[:, c, :], in_=w1v[:, c, :]), writes=[w1b])
            s.dma("sp", lambda e: e.dma_start(out=b1T[:], in_=W["conv_b1T"]), writes=[b1Tb])
            s.dma("sp", lambda e: e.dma_start(out=wdw[:], in_=W["conv_wdwT"]), writes=[wdwb])
            s.dma("sp", lambda e: e.dma_start(out=bdw[:], in_=W["conv_bdwT"]), writes=[bdwb])
            for up, upb in upads + ubfs:
                s.op("pool", lambda e: e.memset(up[:, 0:15], 0.0), writes=[upb])
                s.op("pool", lambda e: e.memset(up[:, 15 + SEQ:PADW], 0.0), writes=[upb])
            for t in range(NT):
                xt, xb = xts[t % 2]
                scr, scrb = scrs[t % 2]
                abf, abfb = abfs[0]
                s.dma("sp", lambda e: e.dma_start(out=xt[:], in_=hin[t * 128:(t + 1) * 128, :]), reads=[hinb[t]], writes=[xb])
                norm_mod(k, xt, xb, ABG[:, 0, :], ABG[:, 1, :], ABGb, scr, scrb, st, stb, abf, abfb)
                bank, bb = banks[6 + t % 2]
                transpose_chunks(k, bank, bb, lambda c: abf[:, c * 128:(c + 1) * 128], 8, 128, aTa[:, :, t * 128:(t + 1) * 128], aTab, abfb, dst_view=True)
            accbs = [[Buf() for _ in range(4)] for _ in range(2)]
            NPE = 12
            tapbanks = [banks[2], banks[3], banks[6], banks[7]]

            def pw1_gen(m):
                up, upb = upads[m % 2]
                ub, ubb = ubfs[m % 2]
                for j in range(NPE):
                    s.op("act", lambda e: e.activation(out=dgt[:, j, :], in_=k.ident[:], func=AF.Copy, scale=wdw[:, m, j:j + 1]),
                         reads=[k.identb, wdwb], writes=[dgtb])
                for tg in range(4):
                    (bv_, bvb), (bg_, bgb) = banks[0], banks[1]
                    sg, sgb = sgs[tg % 2]
                    for (bk, bkb, c0) in ((bv_, bvb, m * 128), (bg_, bgb, 1024 + m * 128)):
                        for c in range(8):
                            s.op("pe", lambda e: e.matmul(bk[:, :], lhsT=w1[:, c, c0:c0 + 128], rhs=aTa[:, c, tg * 512:(tg + 1) * 512], start=(c == 0), stop=(c == 7)),
                                 reads=[w1b, aTab], writes=[bkb])
                    s.op("act", lambda e: e.activation(out=sg[:], in_=bg_[:, :], func=AF.Sigmoid, bias=b1T[:, 8 + m:9 + m]), reads=[bgb, b1Tb], writes=[sgb])
                    s.op("dve", lambda e: e.scalar_tensor_tensor(out=up[:, 15 + tg * 512:15 + (tg + 1) * 512], in0=bv_[:, :], scalar=b1T[:, m:m + 1], in1=sg[:],
                                                                 op0=ALU.add, op1=ALU.mult), reads=[bvb, sgb, b1Tb], writes=[upb])
                    if tg == 3:
                        s.op("act", lambda e: e.copy(out=ub[:, 15:15 + SEQ], in_=up[:, 15:15 + SEQ]), reads=[upb], writes=[ubb])
                    yield

            def taps_pe(m):
                ub, ubb = ubfs[m % 2]
                for ch in range(4):
                    tbk, tbkb = tapbanks[ch]
                    for j in range(NPE):
                        s.op("pe", lambda e: e.matmul(tbk[:, :], lhsT=dgt[:, j, :], rhs=ub[:, ch * 512 + j:ch * 512 + j + 512], start=(j == 0), stop=(j == NPE - 1)),
                             reads=[dgtb, ubb], writes=[tbkb])

            def taps_dve_gen(m):
                up, upb = upads[m % 2]
                acc, _ = accs[m % 2]
                accb = accbs[m % 2]
                for j in range(NPE, 31):
                    for ch in range(4):
                        src = up[:, ch * 512 + j:ch * 512 + j + 512]
                        dst = acc[:, ch * 512:(ch + 1) * 512]
                        if j == NPE:
                            s.op("dve", lambda e: e.tensor_scalar(out=dst, in0=src, scalar1=wdw[:, m, j:j + 1], scalar2=bdw[:, m:m + 1], op0=ALU.mult, op1=ALU.add),
                                 reads=[upb, wdwb, bdwb], writes=[accb[ch]])
                        else:
                            s.op("dve", lambda e: e.scalar_tensor_tensor(out=dst, in0=src, scalar=wdw[:, m, j:j + 1], in1=dst, op0=ALU.mult, op1=ALU.add),
                                 reads=[upb, wdwb, accb[ch]], writes=[accb[ch]])
                    if (j - NPE) % 5 == 4:
                        yield

            def finish(m):
                acc, _ = accs[m % 2]
                accb = accbs[m % 2]
                for ch in range(4):
                    tbk, tbkb = tapbanks[ch]
                    dst = acc[:, ch * 512:(ch + 1) * 512]
                    s.op("dve", lambda e: e.tensor_tensor(out=dst, in0=dst, in1=tbk[:, :], op=ALU.add), reads=[accb[ch], tbkb], writes=[accb[ch]])
                for g in range(NT // 4):
                    tb_, tbb = banks[4 + g % 2]
                    for j in range(4):
                        t = g * 4 + j
                        s.op("pe", lambda e: e.transpose(out=tb_[:, j * 128:(j + 1) * 128], in_=acc[:, t * 128:(t + 1) * 128], identity=k.identf[:]),
                             reads=[accb[t // 4], k.identb], writes=[tbb])
                    s.op("act", lambda e: e.copy(out=cbuf[:, g * 4:(g + 1) * 4, m * 128:(m + 1) * 128], in_=tb_[:, :].rearrange("p (a b) -> p a b", b=128)),
                         reads=[tbb], writes=[cbt[g]])

            for _ in pw1_gen(0):
                pass
            for m in range(8):
                taps_pe(m)
                gn = pw1_gen(m + 1) if m + 1 < 8 else None
                for _ in taps_dve_gen(m):
                    if gn is not None:
                        next(gn, None)
                if gn is not None:
                    for _ in gn:
                        pass
                finish(m)
            s.barrier()
        with ExitStack() as es3:
            stg = [k.sb(es3, f"cv3_stg{i}", [128, 1024], F32) for i in range(2)]
            w2, w2b = k.sb(es3, "cv3_w2", [128, 8, 1024], BF16)
            gl, glb = k.sb(es3, "cv3_gl", [128, 4, 1024], F32)
            sbfs = [k.sb(es3, f"cv3_s{i}", [128, 1024], BF16) for i in range(2)]
            sTs = [k.sb(es3, f"cv3_sT{i}", [128, 8, 128], BF16) for i in range(2)]
            outs = [k.sb(es3, f"cv3_o{i}", [128, 1024], F32) for i in range(2)]
            wv = W["conv_w_pw2"].rearrange("(kc p) n -> p kc n", p=128)
            for c in range(8):
                load_cast(k, stg, w2[:, c, :], w2b, wv[:, c, :], 1024, c)
            load_bcast(k, gl[:, 0, :], glb, W["conv_g_ln"], 1024)
            load_bcast(k, gl[:, 1, :], glb, W["conv_b_ln"], 1024)
            load_bcast(k, gl[:, 2, :], glb, W["conv_b_pw2"], 1024)
            load_bcast(k, gl[:, 3, :], glb, modv_l[0:1, 2, :], 1024)
            for t in range(NT):
                xt, xb = xts[t % 2]
                scr, scrb = scrs[t % 2]
                sbf, sbfb = sbfs[t % 2]
                sT, sTb = sTs[t % 2]
                ot, otb = outs[t % 2]
                cb = cbt[t // 4]
                c_t = cbuf[:, t, :]
                s.dma("sp", lambda e: e.dma_start(out=xt[:], in_=hin[t * 128:(t + 1) * 128, :]), reads=[hinb[t]], writes=[xb])
                s.op("act", lambda e: e.activation(out=scr[:], in_=c_t, func=AF.Identity, accum_out=st[:, 0:1]), reads=[cb], writes=[scrb, stb])
                s.op("act", lambda e: e.activation(out=scr[:], in_=c_t, func=AF.Square, accum_out=st[:, 1:2]), reads=[cb], writes=[scrb, stb])
                s.op("dve", lambda e: e.tensor_scalar(out=st[:, 2:3], in0=st[:, 0:1], scalar1=1.0 / D, scalar2=None, op0=ALU.mult), reads=[stb], writes=[stb])
                s.op("dve", lambda e: e.scalar_tensor_tensor(out=st[:, 3:4], in0=st[:, 2:3], scalar=-1.0, in1=st[:, 2:3], op0=ALU.mult, op1=ALU.mult),
                     reads=[stb], writes=[stb])
                s.op("dve", lambda e: e.scalar_tensor_tensor(out=st[:, 4:5], in0=st[:, 1:2], scalar=1.0 / D, in1=st[:, 3:4], op0=ALU.mult, op1=ALU.add),
                     reads=[stb], writes=[stb])
                s.op("act", lambda e: e.activation(out=st[:, 5:6], in_=st[:, 4:5], func=AF.Sqrt, scale=1.0, bias=k.eps[:, 0:1]), reads=[stb], writes=[stb])
                s.op("dve", lambda e: e.reciprocal(out=st[:, 5:6], in_=st[:, 5:6]), reads=[stb], writes=[stb])
                s.op("dve", lambda e: e.tensor_scalar(out=scr[:], in0=c_t, scalar1=st[:, 2:3], scalar2=st[:, 5:6], op0=ALU.subtract, op1=ALU.mult),
                     reads=[cb, stb], writes=[scrb])
                s.op("dve", lambda e: e.tensor_tensor(out=scr[:], in0=scr[:], in1=gl[:, 0, :], op=ALU.mult), reads=[scrb, glb], writes=[scrb])
                s.op("pool", lambda e: e.tensor_tensor(out=scr[:], in0=scr[:], in1=gl[:, 1, :], op=ALU.add), reads=[scrb, glb], writes=[scrb])
                s.op("act", lambda e: e.activation(out=sbf[:], in_=scr[:], func=AF.Silu), reads=[scrb], writes=[sbfb])
                bank, bb = banks[7]
                transpose_chunks(k, bank, bb, lambda c: sbf[:, c * 128:(c + 1) * 128], 8, 128, sT[:], sTb, sbfb, dst_view=True)
                for g in range(2):
                    yb, ybb = banks[g]
                    for c in range(8):
                        s.op("pe", lambda e: e.matmul(yb[:, :], lhsT=sT[:, c, :], rhs=w2[:, c, g * 512:(g + 1) * 512], start=(c == 0), stop=(c == 7)),
                             reads=[sTb, w2b], writes=[ybb])
                    s.op("dve", lambda e: e.tensor_tensor(out=scr[:, g * 512:(g + 1) * 512], in0=yb[:, :], in1=gl[:, 2, g * 512:(g + 1) * 512], op=ALU.add),
                         reads=[ybb, glb], writes=[scrb])
                s.op("pool", lambda e: e.tensor_tensor(out=scr[:], in0=scr[:], in1=gl[:, 3, :], op=ALU.mult), reads=[scrb, glb], writes=[scrb])
                s.op("pool", lambda e: e.tensor_tensor(out=ot[:], in0=scr[:], in1=xt[:], op=ALU.add), reads=[scrb, xb], writes=[otb])
                s.dma("sp", lambda e: e.dma_start(out=hout[t * 128:(t + 1) * 128, :], in_=ot[:]), reads=[otb], writes=[houtb[t]])
            s.barrier()


IN_SPECS = {
    "x": ([SEQ, D], F32), "ctx": ([CTX, D], F32), "cc": ([128, 16], F32),
    "w_ada": ([2, D, 6 * D], F32), "b_ada": ([2, 6 * D], F32), "g_norm": ([4, D], F32),
    "ident": ([128, 128], BF16), "identf": ([128, 128], F32),
    "attn_w_in": ([D, 2208], F32), "mla_w_q_up": ([384, 768], F32), "mla_w_kv_up": ([256, 1024], F32),
    "mla_g_qa": ([1, 384], F32), "mla_g_kva": ([1, 256], F32), "mla_g_q": ([1, 96], F32), "mla_g_k": ([1, 96], F32),
    "na_g_q": ([1, 64], F32), "na_g_k": ([1, 64], F32), "attn_w_out": ([D, D], F32),
    "rope": ([SEQ, 32], F32), "nabias": ([8, 128, 21, 128], F32),
    "conv_w_pw1": ([D, 2 * D], F32), "conv_b1T": ([128, 16], F32), "conv_wdwT": ([128, 8, 31], F32), "conv_bdwT": ([128, 8], F32),
    "conv_g_ln": ([1, D], F32), "conv_b_ln": ([1, D], F32), "conv_w_pw2": ([D, D], F32), "conv_b_pw2": ([1, D], F32),
    "wq0": ([D, 2048], F32), "wq1": ([D, 2048], F32), "skT0": ([128, 16, 128], F32), "skT1": ([128, 16, 128], F32),
    "u0": ([16384, D], F32), "u1": ([16384, D], F32), "v0": ([16384, D], F32), "v1": ([16384, D], F32),
}


def build_program():
    nc = bass.Bass("TRN2", target_bir_lowering=False)
    A = {n: nc.dram_tensor(n, sh, dt, kind="ExternalInput").ap() for n, (sh, dt) in IN_SPECS.items()}
    out = nc.dram_tensor("out", [SEQ, D], F32, kind="ExternalOutput").ap()
    modv = nc.dram_tensor("modv_scr", [2, 2, 6, D], F32, kind="Internal").ap()
    hs = [nc.dram_tensor(f"h_scr{i}", [SEQ, D], F32, kind="Internal").ap() for i in range(3)]
    hb = [[Buf() for _ in range(NT)] for _ in range(4)]
    uvs = [nc.dram_tensor(f"uv_scr{l}", [16384, 2 * D], BF16, kind="Internal").ap() for l in range(2)]
    with ExitStack() as es:
        k = K(nc, es)
        k.modv_buf = Buf()
        setup_consts(k, es, A["ident"], A["identf"])
        uvb = [[], []]
        k.bg = uv_cast_gen(k, [(A[f"u{l}"], A[f"v{l}"], uvs[l], uvb[l]) for l in range(2)])
        stage_ada(k, A["cc"], A["w_ada"], A["b_ada"], A["g_norm"], modv)
        stage_attn(k, A["x"], A["ctx"], modv[0], A, hs[0], hb[0])
        for _ in k.bg:
            pass
        stage_peer3(k, hs[0], hb[0], modv[0], A["wq0"], A["skT0"], uvs[0], uvb[0], hs[1], hb[1], "pra")
        stage_conv(k, hs[1], hb[1], modv[1], A, hs[2], hb[2])
        stage_peer3(k, hs[2], hb[2], modv[1], A["wq1"], A["skT1"], uvs[1], uvb[1], out, hb[3], "prb")
        k.s.barrier()
    return nc


def kernel(**inp):
    import ml_dtypes
    f = lambda a: np.ascontiguousarray(np.asarray(a, dtype=np.float32))
    nb = inp["x"].shape[0]
    shared = {
        "w_ada": f(inp["w_ada"]), "b_ada": f(inp["b_ada"]),
        "g_norm": f(np.stack([inp["g_norm1"][0], inp["g_norm2"][0], inp["g_norm1"][1], inp["g_norm2"][1]])),
        "ident": np.eye(128).astype(ml_dtypes.bfloat16), "identf": np.eye(128, dtype=np.float32),
        "attn_w_in": f(inp["attn_w_in"][0]), "mla_w_q_up": f(inp["mla_w_q_up"][0]), "mla_w_kv_up": f(inp["mla_w_kv_up"][0]),
        "mla_g_qa": f(inp["mla_g_qa"]), "mla_g_kva": f(inp["mla_g_kva"]), "mla_g_q": f(inp["mla_g_q"]), "mla_g_k": f(inp["mla_g_k"]),
        "na_g_q": f(inp["na_g_q"]), "na_g_k": f(inp["na_g_k"]), "attn_w_out": f(inp["attn_w_out"][0]),
        "rope": host_rope_table(), "nabias": host_na_bias(np.asarray(inp["na_rpb"][0], np.float32)),
        "conv_w_pw1": f(inp["conv_w_pw1"][0]), "conv_b1T": f(np.asarray(inp["conv_b_pw1"][0]).reshape(16, 128).T),
        "conv_wdwT": f(np.asarray(inp["conv_w_dw"][0]).reshape(31, 8, 128).transpose(2, 1, 0)),
        "conv_bdwT": f(np.asarray(inp["conv_b_dw"][0]).reshape(8, 128).T),
        "conv_g_ln": f(inp["conv_g_ln"]), "conv_b_ln": f(inp["conv_b_ln"]), "conv_w_pw2": f(inp["conv_w_pw2"][0]), "conv_b_pw2": f(inp["conv_b_pw2"]),
    }
    for l in range(2):
        shared[f"wq{l}"] = f(inp["peer_w_query"][l])
        shared[f"skT{l}"] = f(np.asarray(inp["peer_sub_keys"][l]).reshape(16, 128, 128).transpose(2, 0, 1))
        shared[f"u{l}"] = f(inp["peer_u"][l])
        shared[f"v{l}"] = f(inp["peer_v"][l])
    in_maps = []
    for b in range(nb):
        cc = np.zeros((128, 16), np.float32)
        cc[:, 0::2] = np.asarray(inp["c"][b], np.float32).reshape(8, 128).T
        cc[:, 1::2] = np.asarray(inp["c_ctx"], np.float32).reshape(8, 128).T
        m = dict(shared)
        m["x"] = f(inp["x"][b])
        m["ctx"] = f(inp["ctx"][b])
        m["cc"] = cc
        in_maps.append(m)
    nc = build_program()
    res = run_bass_kernel_spmd(nc, in_maps, core_ids=list(range(nb)))
    return np.stack([np.asarray(r["out"], dtype=np.float32) for r in res.results], axis=0)


def uv_cast_gen(k, tabs):
    PIECE = 1024
    for (u_tab, v_tab, uv, uvb) in tabs:
        for r0 in range(0, 16384, PIECE):
            r1 = r0 + PIECE
            b0, b1 = Buf(), Buf()
            k.s.dma("pool", lambda e: e.dma_start(out=uv[r0:r1, 0:1024], in_=u_tab[r0:r1, :]), writes=[b0])
            uvb.append(b0)
            yield
            k.s.dma("pool", lambda e: e.dma_start(out=uv[r0:r1, 1024:2048], in_=v_tab[r0:r1, :]), writes=[b1])
            uvb.append(b1)
            yield


def issue_uv_cast(k, es, u_tab, v_tab, uv, uvb):
    for i in range(4):
        r0, r1 = i * 4096, (i + 1) * 4096
        b0, b1 = Buf(), Buf()
        k.s.bulk_dma("pool", lambda e: e.dma_start(out=uv[r0:r1, 0:1024], in_=u_tab[r0:r1, :]), writes=[b0], es=es)
        k.s.bulk_dma("pool", lambda e: e.dma_start(out=uv[r0:r1, 1024:2048], in_=v_tab[r0:r1, :]), writes=[b1], es=es)
        uvb.extend([b0, b1])


NROW2 = 16


def stage_peer2(k, hin, hinb, modv_l, w_query, skT_d, uv, uvb, hout, houtb, tag):
    nc, s = k.nc, k.s
    banks = k.banks
    with ExitStack() as es:
        wq, wqb = k.sb(es, f"{tag}_wq", [128, 8, 2048], BF16)
        skT, skTb = k.sb(es, f"{tag}_skT", [128, 16, 128], BF16)
        ABG, ABGb = k.sb(es, f"{tag}_ABG", [128, 3, 1024], F32)
        iota_i, iota_ib = k.sb(es, f"{tag}_iotai", [128, 16], I32)
        iota, iotab = k.sb(es, f"{tag}_iota", [128, 16], F32)
        st, stb = k.sb(es, f"{tag}_st", [128, 4], F32)
        xts = [k.sb(es, f"{tag}_x{i}", [128, 1024], F32) for i in range(2)]
        scrs = [k.sb(es, f"{tag}_scr{i}", [128, 1024], F32) for i in range(2)]
        hbfs = [k.sb(es, f"{tag}_hbf{i}", [128, 1024], BF16) for i in range(1)]
        hms = [k.sb(es, f"{tag}_hm{i}", [128, 1024], F32) for i in range(2)]
        hT, hTb = k.sb(es, f"{tag}_hT", [128, 8, 128], BF16)
        qbf, qbfb = k.sb(es, f"{tag}_qbf", [128, 2048], BF16)
        qT, qTb = k.sb(es, f"{tag}_qT", [128, 16, 128], BF16)
        ssb, ssbb = k.sb(es, f"{tag}_s", [128, 16, 128], F32)
        s2, _ = k.sb(es, f"{tag}_s2", [128, 16, 128], F32)
        m16, _ = k.sb(es, f"{tag}_m16", [128, 16, 16], F32)
        i16, _ = k.sb(es, f"{tag}_i16", [128, 16, 16], U32)
        i16f, i16fb = k.sb(es, f"{tag}_i16f", [128, 16, 16], F32)
        cand, candb = k.sb(es, f"{tag}_cand", [128, 8, 256], F32)
        cand2, _ = k.sb(es, f"{tag}_cand2", [128, 8, 256], F32)
        best, _ = k.sb(es, f"{tag}_best", [128, 8, 16], F32)
        pos, _ = k.sb(es, f"{tag}_pos", [128, 8, 16], U32)
        ab_i, ab_ib = k.sb(es, f"{tag}_abi", [128, 2, 128], I32)
        ab_f, ab_fb = k.sb(es, f"{tag}_abf", [128, 2, 128], F32)
        oh, ohb = k.sb(es, f"{tag}_oh", [128, 8, 16, 16], F32)
        e01, e01b = k.sb(es, f"{tag}_e01", [128, 2, 128], F32)
        idxs = [k.sb(es, f"{tag}_idx{i}", [128, 128], I32) for i in range(2)]
        gts = [k.sb(es, f"{tag}_gate{i}", [128, 8, 16], F32) for i in range(2)]
        gsum, gsumb = k.sb(es, f"{tag}_gsum", [128, 8], F32)
        actv, _ = k.sb(es, f"{tag}_act", [128, 128], F32)
        wgt, _ = k.sb(es, f"{tag}_wgt", [128, 128], F32)
        junk, _ = k.sb(es, f"{tag}_junk", [128, 1024], BF16)
        rows = [k.sb(es, f"{tag}_row{i}", [128, 2048], BF16) for i in range(NROW2)]
        dgs = [k.sb(es, f"{tag}_dg{i}", [128, 128], BF16) for i in range(4)]
        ot, otb = k.sb(es, f"{tag}_ot", [128, 1024], F32)
        hpb = [[Buf() for _ in range(16)] for _ in range(3)]
        hb = [[Buf() for _ in range(8)] for _ in range(3)]
        actb = [Buf() for _ in range(16)]
        wgb = [Buf() for _ in range(4)]

        wqv = w_query.rearrange("(kc p) n -> p kc n", p=128)
        for c in range(8):
            s.dma("pool", lambda e: e.dma_start(out=wq[:, c, :], in_=wqv[:, c, :]), writes=[wqb])
        s.dma("pool", lambda e: e.dma_start(out=skT[:].rearrange("p a b -> p (a b)"), in_=skT_d.rearrange("p a b -> p (a b)")), writes=[skTb])
        for j in range(3):
            load_bcast(k, ABG[:, j, :], ABGb, modv_l[0:1, 3 + j, :], 1024)
        s.op("pool", lambda e: e.iota(out=iota_i[:], pattern=[[1, 16]], base=0, channel_multiplier=0), writes=[iota_ib])
        s.op("dve", lambda e: e.tensor_copy(out=iota[:], in_=iota_i[:]), reads=[iota_ib], writes=[iotab])

        def front(t):
            xt, xb = xts[t % 2]
            scr, scrb = scrs[t % 2]
            idx, idxb = idxs[t % 2]
            gate, gateb = gts[t % 2]
            hbf, hbfb = hbfs[0]
            hm, hmb = hms[t % 2]
            s.dma("sp", lambda e: e.dma_start(out=xt[:], in_=hin[t * 128:(t + 1) * 128, :]), reads=[hinb[t]], writes=[xb])
            norm_mod(k, xt, xb, ABG[:, 0, :], ABG[:, 1, :], ABGb, scr, scrb, st, stb, hbf, hbfb, out_f32=(hm, hmb))
            bank, bb = banks[4]
            transpose_chunks(k, bank, bb, lambda c: hbf[:, c * 128:(c + 1) * 128], 8, 128, hT[:], hTb, hbfb, dst_view=True)
            yield
            for g in range(4):
                qb_, qbb = banks[g]
                for c in range(8):
                    s.op("pe", lambda e: e.matmul(qb_[:, :], lhsT=hT[:, c, :], rhs=wq[:, c, g * 512:(g + 1) * 512], start=(c == 0), stop=(c == 7)),
                         reads=[hTb, wqb], writes=[qbb])
                s.op("act", lambda e: e.copy(out=qbf[:, g * 512:(g + 1) * 512], in_=qb_[:, :]), reads=[qbb], writes=[qbfb])
            yield
            for half in range(2):
                tb_, tbb = banks[4 + half]
                transpose_chunks(k, tb_, tbb, lambda c: qbf[:, (half * 8 + c) * 128:(half * 8 + c + 1) * 128], 8, 128,
                                 qT[:, half * 8:(half + 1) * 8, :], qTb, qbfb, dst_view=True)
            for g in range(4):
                sb_, sbb = banks[g]
                for j in range(4):
                    hp = g * 4 + j
                    s.op("pe", lambda e: e.matmul(sb_[:, j * 128:(j + 1) * 128], lhsT=qT[:, hp, :], rhs=skT[:, hp, :], start=True, stop=True),
                         reads=[qTb, skTb], writes=[sbb])
                s.op("act", lambda e: e.copy(out=ssb[:, g * 4:(g + 1) * 4, :], in_=sb_[:, :].rearrange("p (a b) -> p a b", b=128)), reads=[sbb], writes=[ssbb])
            yield
            for hp in range(16):
                s.op("dve", lambda e: e.max(out=m16[:, hp, 0:8], in_=ssb[:, hp, :]), reads=[ssbb], writes=[hpb[0][hp]])
            yield
            for hp in range(16):
                s.op("dve", lambda e: e.max_index(out=i16[:, hp, 0:8], in_max=m16[:, hp, 0:8], in_values=ssb[:, hp, :]),
                     reads=[ssbb, hpb[0][hp]], writes=[hpb[1][hp]])
            yield
            for hp in range(16):
                s.op("dve", lambda e: e.match_replace(out=s2[:, hp, :], in_to_replace=m16[:, hp, 0:8], in_values=ssb[:, hp, :], imm_value=-1e30),
                     reads=[ssbb, hpb[0][hp]], writes=[hpb[2][hp]])
            yield
            for hp in range(16):
                s.op("dve", lambda e: e.max(out=m16[:, hp, 8:16], in_=s2[:, hp, :]), reads=[hpb[2][hp]], writes=[hpb[0][hp]])
            yield
            for hp in range(16):
                s.op("dve", lambda e: e.max_index(out=i16[:, hp, 8:16], in_max=m16[:, hp, 8:16], in_values=s2[:, hp, :]),
                     reads=[hpb[2][hp], hpb[0][hp]], writes=[hpb[1][hp]])
            yield
            s.op("dve", lambda e: e.tensor_copy(out=i16f[:], in_=i16[:]), reads=hpb[1], writes=[i16fb])
            m4 = m16[:].rearrange("p (h t) a -> p h t a", t=2)
            s.op("dve", lambda e: e.tensor_tensor(out=cand[:].rearrange("p h (a b) -> p h a b", b=16),
                                                  in0=m4[:, :, 0, :].unsqueeze(3).to_broadcast([128, 8, 16, 16]),
                                                  in1=m4[:, :, 1, :].unsqueeze(2).to_broadcast([128, 8, 16, 16]), op=ALU.add),
                 reads=hpb[0], writes=[candb])
            yield
            for h in range(8):
                s.op("dve", lambda e: e.max(out=best[:, h, 0:8], in_=cand[:, h, :]), reads=[candb], writes=[hb[0][h]])
            for h in range(8):
                s.op("dve", lambda e: e.max_index(out=pos[:, h, 0:8], in_max=best[:, h, 0:8], in_values=cand[:, h, :]),
                     reads=[candb, hb[0][h]], writes=[hb[1][h]])
            yield
            for h in range(8):
                s.op("dve", lambda e: e.match_replace(out=cand2[:, h, :], in_to_replace=best[:, h, 0:8], in_values=cand[:, h, :], imm_value=-1e30),
                     reads=[candb, hb[0][h]], writes=[hb[2][h]])
            for h in range(8):
                s.op("dve", lambda e: e.max(out=best[:, h, 8:16], in_=cand2[:, h, :]), reads=[hb[2][h]], writes=[hb[0][h]])
            yield
            for h in range(8):
                s.op("dve", lambda e: e.max_index(out=pos[:, h, 8:16], in_max=best[:, h, 8:16], in_values=cand2[:, h, :]),
                     reads=[hb[2][h], hb[0][h]], writes=[hb[1][h]])
            posi = pos[:].rearrange("p h k -> p (h k)").bitcast(I32)
            s.op("dve", lambda e: e.tensor_single_scalar(out=ab_i[:, 0, :], in_=posi, scalar=4, op=ALU.arith_shift_right), reads=hb[1], writes=[ab_ib])
            s.op("dve", lambda e: e.tensor_single_scalar(out=ab_i[:, 1, :], in_=posi, scalar=15, op=ALU.bitwise_and), reads=hb[1], writes=[ab_ib])
            s.op("dve", lambda e: e.tensor_copy(out=ab_f[:], in_=ab_i[:]), reads=[ab_ib], writes=[ab_fb])
            yield
            i4 = i16f[:].rearrange("p (h t) a -> p h t a", t=2)
            for p_ in range(2):
                s.op("dve", lambda e: e.tensor_tensor(out=oh[:], in0=ab_f[:, p_, :].rearrange("p (h k) -> p h k", k=16).unsqueeze(3).to_broadcast([128, 8, 16, 16]),
                                                      in1=iota[:].unsqueeze(1).unsqueeze(1).to_broadcast([128, 8, 16, 16]), op=ALU.is_equal),
                     reads=[ab_fb, iotab], writes=[ohb])
                s.op("dve", lambda e: e.tensor_tensor(out=oh[:], in0=oh[:], in1=i4[:, :, p_, :].unsqueeze(2).to_broadcast([128, 8, 16, 16]), op=ALU.mult),
                     reads=[ohb, i16fb], writes=[ohb])
                s.op("dve", lambda e: e.tensor_reduce(out=e01[:, p_, :].rearrange("p (h k) -> p h k", k=16), in_=oh[:], axis=AX.X, op=ALU.add),
                     reads=[ohb], writes=[e01b])
                yield
            s.op("dve", lambda e: e.scalar_tensor_tensor(out=e01[:, 0, :], in0=e01[:, 0, :], scalar=128.0, in1=e01[:, 1, :], op0=ALU.mult, op1=ALU.add),
                 reads=[e01b], writes=[e01b])
            s.op("dve", lambda e: e.tensor_copy(out=idx[:], in_=e01[:, 0, :]), reads=[e01b], writes=[idxb])
            s.op("dve", lambda e: e.tensor_tensor(out=gate[:], in0=best[:], in1=best[:, :, 0:1].to_broadcast([128, 8, 16]), op=ALU.subtract),
                 reads=hb[0], writes=[gateb])
            s.op("act", lambda e: e.activation(out=gate[:], in_=gate[:], func=AF.Exp), reads=[gateb], writes=[gateb])
            s.op("dve", lambda e: e.tensor_reduce(out=gsum[:], in_=gate[:], axis=AX.X, op=ALU.add), reads=[gateb], writes=[gsumb])
            s.op("dve", lambda e: e.reciprocal(out=gsum[:], in_=gsum[:]), reads=[gsumb], writes=[gsumb])
            s.op("dve", lambda e: e.tensor_tensor(out=gate[:], in0=gate[:], in1=gsum[:].unsqueeze(2).to_broadcast([128, 8, 16]), op=ALU.mult),
                 reads=[gateb, gsumb], writes=[gateb])

        ring = [0]

        def back(t, fg):
            xt, xb = xts[t % 2]
            scr, scrb = scrs[t % 2]
            idx, idxb = idxs[t % 2]
            gate, gateb = gts[t % 2]
            hm, hmb = hms[t % 2]
            (o0, o0b), (o1, o1b) = banks[6], banks[7]
            for grp in range(16):
                held = []
                for j in range(8):
                    hk = grp * 8 + j
                    rw, rwb = rows[ring[0] % NROW2]
                    ring[0] += 1
                    s.dma("pool", lambda e: e.indirect_dma_start(out=rw[:], out_offset=None, in_=uv,
                                                                 in_offset=bass.IndirectOffsetOnAxis(ap=idx[:, hk:hk + 1], axis=0)),
                          reads=[idxb] + uvb, writes=[rwb])
                    s.op("dve", lambda e: e.scalar_tensor_tensor(out=junk[:], in0=rw[:, 0:1024], scalar=1.0, in1=hm[:], op0=ALU.mult, op1=ALU.mult,
                                                                 accum_out=actv[:, hk:hk + 1]), reads=[rwb, hmb], writes=[actb[hk % 16]])
                    held.append((hk, rw, rwb))
                g8 = slice(grp * 8, grp * 8 + 8)
                wb_ = wgb[grp % 4]
                s.op("act", lambda e: e.activation(out=wgt[:, g8], in_=actv[:, g8], func=AF.Gelu), reads=actb[(grp % 2) * 8:(grp % 2) * 8 + 8], writes=[wb_])
                s.op("dve", lambda e: e.tensor_tensor(out=wgt[:, g8], in0=wgt[:, g8], in1=gate[:].rearrange("p h k -> p (h k)")[:, g8], op=ALU.mult),
                     reads=[wb_, gateb], writes=[wb_])
                for (hk, rw, rwb) in held:
                    dg, dgb = dgs[hk % 4]
                    s.op("act", lambda e: e.activation(out=dg[:], in_=k.ident[:], func=AF.Copy, scale=wgt[:, hk:hk + 1]),
                         reads=[k.identb, wb_], writes=[dgb])
                    s.op("pe", lambda e: e.matmul(o0[:, :], lhsT=dg[:], rhs=rw[:, 1024:1536], start=(hk == 0), stop=(hk == 127)),
                         reads=[dgb, rwb], writes=[o0b])
                    s.op("pe", lambda e: e.matmul(o1[:, :], lhsT=dg[:], rhs=rw[:, 1536:2048], start=(hk == 0), stop=(hk == 127)),
                         reads=[dgb, rwb], writes=[o1b])
                if fg is not None:
                    next(fg, None)
            s.op("dve", lambda e: e.tensor_tensor(out=scr[:, 0:512], in0=o0[:, :], in1=ABG[:, 2, 0:512], op=ALU.mult), reads=[o0b, ABGb], writes=[scrb])
            s.op("dve", lambda e: e.tensor_tensor(out=scr[:, 512:1024], in0=o1[:, :], in1=ABG[:, 2, 512:1024], op=ALU.mult), reads=[o1b, ABGb], writes=[scrb])
            s.op("pool", lambda e: e.tensor_tensor(out=ot[:], in0=scr[:], in1=xt[:], op=ALU.add), reads=[scrb, xb], writes=[otb])
            s.dma("sp", lambda e: e.dma_start(out=hout[t * 128:(t + 1) * 128, :], in_=ot[:]), reads=[otb], writes=[houtb[t]])

        for _ in front(0):
            pass
        for t in range(NT):
            fg = front(t + 1) if t + 1 < NT else None
            back(t, fg)
            if fg is not None:
                for _ in fg:
                    pass
        s.barrier()
NROW3 = 16


def stage_peer3(k, hin, hinb, modv_l, w_query, skT_d, uv, uvb, hout, houtb, tag):
    nc, s = k.nc, k.s
    banks = k.banks
    with ExitStack() as es:
        wq, wqb = k.sb(es, f"{tag}_wq", [128, 8, 2048], BF16)
        skT, skTb = k.sb(es, f"{tag}_skT", [128, 16, 128], BF16)
        ABG, ABGb = k.sb(es, f"{tag}_ABG", [128, 3, 1024], F32)
        iota_i, iota_ib = k.sb(es, f"{tag}_iotai", [128, 16], I32)
        iota, iotab = k.sb(es, f"{tag}_iota", [128, 16], F32)
        st, stb = k.sb(es, f"{tag}_st", [128, 4], F32)
        xts = [k.sb(es, f"{tag}_x{i}", [128, 1024], F32) for i in range(2)]
        scrs = [k.sb(es, f"{tag}_scr{i}", [128, 1024], F32) for i in range(2)]
        hbfs = [k.sb(es, f"{tag}_hbf{i}", [128, 1024], BF16) for i in range(1)]
        hT, hTb = k.sb(es, f"{tag}_hT", [128, 8, 128], BF16)
        qbf, qbfb = k.sb(es, f"{tag}_qbf", [128, 2048], BF16)
        qT, qTb = k.sb(es, f"{tag}_qT", [128, 16, 128], BF16)
        ssb, ssbb = k.sb(es, f"{tag}_s", [128, 16, 128], F32)
        s2, _ = k.sb(es, f"{tag}_s2", [128, 16, 128], F32)
        m16, _ = k.sb(es, f"{tag}_m16", [128, 16, 16], F32)
        i16, _ = k.sb(es, f"{tag}_i16", [128, 16, 16], U32)
        i16f, i16fb = k.sb(es, f"{tag}_i16f", [128, 16, 16], F32)
        cand, candb = k.sb(es, f"{tag}_cand", [128, 8, 256], F32)
        cand2, _ = k.sb(es, f"{tag}_cand2", [128, 8, 256], F32)
        best, _ = k.sb(es, f"{tag}_best", [128, 8, 16], F32)
        pos, _ = k.sb(es, f"{tag}_pos", [128, 8, 16], U32)
        ab_i, ab_ib = k.sb(es, f"{tag}_abi", [128, 2, 128], I32)
        ab_f, ab_fb = k.sb(es, f"{tag}_abf", [128, 2, 128], F32)
        oh, ohb = k.sb(es, f"{tag}_oh", [128, 8, 16, 16], F32)
        e01, e01b = k.sb(es, f"{tag}_e01", [128, 2, 128], F32)
        idxs = [k.sb(es, f"{tag}_idx{i}", [128, 128], I32) for i in range(2)]
        gts = [k.sb(es, f"{tag}_gate{i}", [128, 8, 16], F32) for i in range(2)]
        gsum, gsumb = k.sb(es, f"{tag}_gsum", [128, 8], F32)
        actv, _ = k.sb(es, f"{tag}_act", [128, 128], F32)
        wgt, _ = k.sb(es, f"{tag}_wgt", [128, 128], F32)
        junk, _ = k.sb(es, f"{tag}_junk", [128, 1024], BF16)
        rows = [k.sb(es, f"{tag}_row{i}", [128, 2048], BF16) for i in range(NROW3)]
        dgs = [k.sb(es, f"{tag}_dg{i}", [128, 128], BF16) for i in range(4)]
        ot, otb = k.sb(es, f"{tag}_ot", [128, 1024], F32)
        hpb = [[Buf() for _ in range(16)] for _ in range(3)]
        hb = [[Buf() for _ in range(8)] for _ in range(3)]
        actb = [Buf() for _ in range(16)]
        wgb = [Buf() for _ in range(4)]

        wqv = w_query.rearrange("(kc p) n -> p kc n", p=128)
        for c in range(8):
            s.dma("pool", lambda e: e.dma_start(out=wq[:, c, :], in_=wqv[:, c, :]), writes=[wqb])
        s.dma("pool", lambda e: e.dma_start(out=skT[:].rearrange("p a b -> p (a b)"), in_=skT_d.rearrange("p a b -> p (a b)")), writes=[skTb])
        for j in range(3):
            load_bcast(k, ABG[:, j, :], ABGb, modv_l[0:1, 3 + j, :], 1024)
        s.op("pool", lambda e: e.iota(out=iota_i[:], pattern=[[1, 16]], base=0, channel_multiplier=0), writes=[iota_ib])
        s.op("dve", lambda e: e.tensor_copy(out=iota[:], in_=iota_i[:]), reads=[iota_ib], writes=[iotab])

        def front(t):
            xt, xb = xts[t % 2]
            scr, scrb = scrs[t % 2]
            idx, idxb = idxs[t % 2]
            gate, gateb = gts[t % 2]
            hbf, hbfb = hbfs[0]
            s.dma("sp", lambda e: e.dma_start(out=xt[:], in_=hin[t * 128:(t + 1) * 128, :]), reads=[hinb[t]], writes=[xb])
            yield
            s.op("act", lambda e: e.activation(out=scr[:], in_=xt[:], func=AF.Square, accum_out=st[:, 0:1]), reads=[xb], writes=[scrb, stb])
            s.op("act", lambda e: e.activation(out=st[:, 0:1], in_=st[:, 0:1], func=AF.Sqrt, scale=1.0 / D, bias=k.eps[:, 0:1]), reads=[stb], writes=[stb])
            yield
            s.op("dve", lambda e: e.reciprocal(out=st[:, 0:1], in_=st[:, 0:1]), reads=[stb], writes=[stb])
            s.op("dve", lambda e: e.scalar_tensor_tensor(out=scr[:], in0=xt[:], scalar=st[:, 0:1], in1=ABG[:, 0, :], op0=ALU.mult, op1=ALU.mult),
                 reads=[xb, stb, ABGb], writes=[scrb])
            yield
            s.op("pool", lambda e: e.tensor_tensor(out=hbf[:], in0=scr[:], in1=ABG[:, 1, :], op=ALU.add), reads=[scrb, ABGb], writes=[hbfb])
            yield
            hmp, hmpb = banks[4 + t % 2]
            bank, bb = banks[2]
            bv = bank[:].bitcast(BF16)
            for c in range(8):
                s.op("pe", lambda e: e.transpose(out=bv[:, c * 128:(c + 1) * 128], in_=hbf[:, c * 128:(c + 1) * 128], identity=k.ident[:]),
                     reads=[hbfb, k.identb], writes=[bb])
            yield
            s.op("act", lambda e: e.copy(out=hT[:], in_=bv[:, :].rearrange("p (c t) -> p c t", t=128)), reads=[bb], writes=[hTb])
            yield
            hmv_ = hmp[:].bitcast(BF16)
            for c in range(8):
                s.op("pe", lambda e: e.transpose(out=hmv_[:, c * 128:(c + 1) * 128], in_=hT[:, c, :], identity=k.ident[:]),
                     reads=[hTb, k.identb], writes=[hmpb])
            for g in range(5):
                if g < 4:
                    qb_, qbb = banks[g % 2]
                    for c in range(8):
                        s.op("pe", lambda e: e.matmul(qb_[:, :], lhsT=hT[:, c, :], rhs=wq[:, c, g * 512:(g + 1) * 512], start=(c == 0), stop=(c == 7)),
                             reads=[hTb, wqb], writes=[qbb])
                if g > 0:
                    g1 = g - 1
                    qb1, qbb1 = banks[g1 % 2]
                    s.op("act", lambda e: e.copy(out=qbf[:, g1 * 512:(g1 + 1) * 512], in_=qb1[:, :]), reads=[qbb1], writes=[qbfb])
                yield
            for half in range(3):
                if half < 2:
                    tb_, tbb = banks[2 + half]
                    bv2 = tb_[:].bitcast(BF16)
                    for c in range(8):
                        s.op("pe", lambda e: e.transpose(out=bv2[:, c * 128:(c + 1) * 128], in_=qbf[:, (half * 8 + c) * 128:(half * 8 + c + 1) * 128], identity=k.ident[:]),
                             reads=[qbfb, k.identb], writes=[tbb])
                if half > 0:
                    h1 = half - 1
                    tb1, tbb1 = banks[2 + h1]
                    s.op("act", lambda e: e.copy(out=qT[:, h1 * 8:(h1 + 1) * 8, :], in_=tb1[:].bitcast(BF16)[:, :].rearrange("p (c t) -> p c t", t=128)),
                         reads=[tbb1], writes=[qTb])
                yield
            for g in range(5):
                if g < 4:
                    sb_, sbb = banks[g % 2]
                    for j in range(4):
                        hp = g * 4 + j
                        s.op("pe", lambda e: e.matmul(sb_[:, j * 128:(j + 1) * 128], lhsT=qT[:, hp, :], rhs=skT[:, hp, :], start=True, stop=True),
                             reads=[qTb, skTb], writes=[sbb])
                if g > 0:
                    g1 = g - 1
                    sb1, sbb1 = banks[g1 % 2]
                    s.op("act", lambda e: e.copy(out=ssb[:, g1 * 4:(g1 + 1) * 4, :], in_=sb1[:, :].rearrange("p (a b) -> p a b", b=128)), reads=[sbb1], writes=[ssbb])
                if g % 2 == 1:
                    yield
            yield
            for hp in range(16):
                s.op("dve", lambda e: e.max(out=m16[:, hp, 0:8], in_=ssb[:, hp, :]), reads=[ssbb], writes=[hpb[0][hp]])
            yield
            for hp in range(16):
                s.op("dve", lambda e: e.max_index(out=i16[:, hp, 0:8], in_max=m16[:, hp, 0:8], in_values=ssb[:, hp, :]),
                     reads=[ssbb, hpb[0][hp]], writes=[hpb[1][hp]])
            yield
            for hp in range(16):
                s.op("dve", lambda e: e.match_replace(out=s2[:, hp, :], in_to_replace=m16[:, hp, 0:8], in_values=ssb[:, hp, :], imm_value=-1e30),
                     reads=[ssbb, hpb[0][hp]], writes=[hpb[2][hp]])
            yield
            for hp in range(16):
                s.op("dve", lambda e: e.max(out=m16[:, hp, 8:16], in_=s2[:, hp, :]), reads=[hpb[2][hp]], writes=[hpb[0][hp]])
            yield
            for hp in range(16):
                s.op("dve", lambda e: e.max_index(out=i16[:, hp, 8:16], in_max=m16[:, hp, 8:16], in_values=s2[:, hp, :]),
                     reads=[hpb[2][hp], hpb[0][hp]], writes=[hpb[1][hp]])
            yield
            s.op("dve", lambda e: e.tensor_copy(out=i16f[:], in_=i16[:]), reads=hpb[1], writes=[i16fb])
            m4 = m16[:].rearrange("p (h t) a -> p h t a", t=2)
            s.op("dve", lambda e: e.tensor_tensor(out=cand[:].rearrange("p h (a b) -> p h a b", b=16),
                                                  in0=m4[:, :, 0, :].unsqueeze(3).to_broadcast([128, 8, 16, 16]),
                                                  in1=m4[:, :, 1, :].unsqueeze(2).to_broadcast([128, 8, 16, 16]), op=ALU.add),
                 reads=hpb[0], writes=[candb])
            yield
            for h in range(8):
                s.op("dve", lambda e: e.max(out=best[:, h, 0:8], in_=cand[:, h, :]), reads=[candb], writes=[hb[0][h]])
            for h in range(8):
                s.op("dve", lambda e: e.max_index(out=pos[:, h, 0:8], in_max=best[:, h, 0:8], in_values=cand[:, h, :]),
                     reads=[candb, hb[0][h]], writes=[hb[1][h]])
            yield
            for h in range(8):
                s.op("dve", lambda e: e.match_replace(out=cand2[:, h, :], in_to_replace=best[:, h, 0:8], in_values=cand[:, h, :], imm_value=-1e30),
                     reads=[candb, hb[0][h]], writes=[hb[2][h]])
            for h in range(8):
                s.op("dve", lambda e: e.max(out=best[:, h, 8:16], in_=cand2[:, h, :]), reads=[hb[2][h]], writes=[hb[0][h]])
            yield
            for h in range(8):
                s.op("dve", lambda e: e.max_index(out=pos[:, h, 8:16], in_max=best[:, h, 8:16], in_values=cand2[:, h, :]),
                     reads=[hb[2][h], hb[0][h]], writes=[hb[1][h]])
            posi = pos[:].rearrange("p h k -> p (h k)").bitcast(I32)
            s.op("dve", lambda e: e.tensor_single_scalar(out=ab_i[:, 0, :], in_=posi, scalar=4, op=ALU.arith_shift_right), reads=hb[1], writes=[ab_ib])
            s.op("dve", lambda e: e.tensor_single_scalar(out=ab_i[:, 1, :], in_=posi, scalar=15, op=ALU.bitwise_and), reads=hb[1], writes=[ab_ib])
            s.op("dve", lambda e: e.tensor_copy(out=ab_f[:], in_=ab_i[:]), reads=[ab_ib], writes=[ab_fb])
            s.op("dve", lambda e: e.tensor_tensor(out=gate[:], in0=best[:], in1=best[:, :, 0:1].to_broadcast([128, 8, 16]), op=ALU.subtract),
                 reads=hb[0], writes=[gateb])
            yield
            s.op("act", lambda e: e.activation(out=gate[:], in_=gate[:], func=AF.Exp), reads=[gateb], writes=[gateb])
            i4 = i16f[:].rearrange("p (h t) a -> p h t a", t=2)
            for p_ in range(2):
                s.op("dve", lambda e: e.tensor_tensor(out=oh[:], in0=ab_f[:, p_, :].rearrange("p (h k) -> p h k", k=16).unsqueeze(3).to_broadcast([128, 8, 16, 16]),
                                                      in1=iota[:].unsqueeze(1).unsqueeze(1).to_broadcast([128, 8, 16, 16]), op=ALU.is_equal),
                     reads=[ab_fb, iotab], writes=[ohb])
                yield
                s.op("dve", lambda e: e.tensor_tensor(out=oh[:], in0=oh[:], in1=i4[:, :, p_, :].unsqueeze(2).to_broadcast([128, 8, 16, 16]), op=ALU.mult),
                     reads=[ohb, i16fb], writes=[ohb])
                yield
                s.op("dve", lambda e: e.tensor_reduce(out=e01[:, p_, :].rearrange("p (h k) -> p h k", k=16), in_=oh[:], axis=AX.X, op=ALU.add),
                     reads=[ohb], writes=[e01b])
                yield
            s.op("dve", lambda e: e.scalar_tensor_tensor(out=e01[:, 0, :], in0=e01[:, 0, :], scalar=128.0, in1=e01[:, 1, :], op0=ALU.mult, op1=ALU.add),
                 reads=[e01b], writes=[e01b])
            s.op("dve", lambda e: e.tensor_copy(out=idx[:], in_=e01[:, 0, :]), reads=[e01b], writes=[idxb])
            s.op("dve", lambda e: e.tensor_reduce(out=gsum[:], in_=gate[:], axis=AX.X, op=ALU.add), reads=[gateb], writes=[gsumb])
            s.op("dve", lambda e: e.reciprocal(out=gsum[:], in_=gsum[:]), reads=[gsumb], writes=[gsumb])
            s.op("dve", lambda e: e.tensor_tensor(out=gate[:], in0=gate[:], in1=gsum[:].unsqueeze(2).to_broadcast([128, 8, 16]), op=ALU.mult),
                 reads=[gateb, gsumb], writes=[gateb])

        ring = [0]

        glv, _ = k.sb(es, f"{tag}_glv", [128, 128], F32)
        glb = [Buf() for _ in range(16)]
        wgb16 = [Buf() for _ in range(16)]

        def back(t, fg):
            xt, xb = xts[t % 2]
            scr, scrb = scrs[t % 2]
            idx, idxb = idxs[t % 2]
            gate, gateb = gts[t % 2]
            hmp, hmpb = banks[4 + t % 2]
            hmv = hmp[:].bitcast(BF16)
            gflat = gate[:].rearrange("p h k -> p (h k)")
            (o0, o0b), (o1, o1b) = banks[6], banks[7]
            held = {}

            def st_a(hk):
                s.op("act", lambda e: e.activation(out=glv[:, hk:hk + 1], in_=actv[:, hk:hk + 1], func=AF.Gelu), reads=[actb[hk % 16]], writes=[glb[hk % 16]])

            def st_b(hk):
                rw, rwb = held.pop(hk)
                dg, dgb = dgs[hk % 4]
                s.op("act", lambda e: e.activation(out=wgt[:, hk:hk + 1], in_=glv[:, hk:hk + 1], func=AF.Copy, scale=gflat[:, hk:hk + 1]),
                     reads=[glb[hk % 16], gateb], writes=[wgb16[hk % 16]])
                s.op("act", lambda e: e.activation(out=dg[:], in_=k.ident[:], func=AF.Copy, scale=wgt[:, hk:hk + 1]),
                     reads=[k.identb, wgb16[hk % 16]], writes=[dgb])
                s.op("pe", lambda e: e.matmul(o0[:, :], lhsT=dg[:], rhs=rw[:, 1024:1536], start=(hk == 0), stop=(hk == 127)),
                     reads=[dgb, rwb], writes=[o0b])
                s.op("pe", lambda e: e.matmul(o1[:, :], lhsT=dg[:], rhs=rw[:, 1536:2048], start=(hk == 0), stop=(hk == 127)),
                     reads=[dgb, rwb], writes=[o1b])

            for hk in range(128 + 3):
                if hk < 128:
                    rw, rwb = rows[ring[0] % NROW3]
                    ring[0] += 1
                    s.dma("pool", lambda e: e.indirect_dma_start(out=rw[:], out_offset=None, in_=uv,
                                                                 in_offset=bass.IndirectOffsetOnAxis(ap=idx[:, hk:hk + 1], axis=0)),
                          reads=[idxb] + uvb, writes=[rwb])
                    s.op("dve", lambda e: e.scalar_tensor_tensor(out=junk[:], in0=rw[:, 0:1024], scalar=1.0, in1=hmv, op0=ALU.mult, op1=ALU.mult,
                                                                 accum_out=actv[:, hk:hk + 1]), reads=[rwb, hmpb], writes=[actb[hk % 16]])
                    held[hk] = (rw, rwb)
                if 0 <= hk - 1 < 128:
                    st_a(hk - 1)
                if 0 <= hk - 3 < 128:
                    st_b(hk - 3)
                if fg is not None and hk % 4 == 3:
                    next(fg, None)
            s.op("dve", lambda e: e.tensor_tensor(out=scr[:, 0:512], in0=o0[:, :], in1=ABG[:, 2, 0:512], op=ALU.mult), reads=[o0b, ABGb], writes=[scrb])
            s.op("dve", lambda e: e.tensor_tensor(out=scr[:, 512:1024], in0=o1[:, :], in1=ABG[:, 2, 512:1024], op=ALU.mult), reads=[o1b, ABGb], writes=[scrb])
            s.op("pool", lambda e: e.tensor_tensor(out=ot[:], in0=scr[:], in1=xt[:], op=ALU.add), reads=[scrb, xb], writes=[otb])
            s.dma("sp", lambda e: e.dma_start(out=hout[t * 128:(t + 1) * 128, :], in_=ot[:]), reads=[otb], writes=[houtb[t]])

        for _ in front(0):
            pass
        for t in range(NT):
            fg = front(t + 1) if t + 1 < NT else None
            back(t, fg)
            if fg is not None:
                for _ in fg:
                    pass
        s.barrier()
```

```python
import numpy as np
from contextlib import ExitStack
import concourse.bass as bass
import concourse.mybir as mybir
from concourse.bass_utils import run_bass_kernel_spmd

F32 = mybir.dt.float32
BF16 = mybir.dt.bfloat16
I32 = mybir.dt.int32
U32 = mybir.dt.uint32
ALU = mybir.AluOpType
AF = mybir.ActivationFunctionType
AX = mybir.AxisListType

D = 1024
SEQ = 2048
NT = SEQ // 128
CTX = 256
NCT = CTX // 128
EPS = 1e-6
NEG = -30000.0


class Buf:
    __slots__ = ("w", "r")

    def __init__(self):
        self.w = None
        self.r = {}


class Sched:
    RING = 12

    def __init__(self, nc, es):
        self.nc = nc
        self.eng = {"pe": nc.tensor, "act": nc.scalar, "dve": nc.vector, "pool": nc.gpsimd, "sp": nc.sync}
        self.semobj = {}
        self.cnt = {}
        for k in self.eng:
            self.semobj[k] = es.enter_context(nc.semaphore("s_" + k))
            self.cnt[k] = 0
        self.waited = {k: {} for k in self.eng}
        self.bulk = []
        self.dq = {}
        for q in ("sp", "pool", "act"):
            slots = []
            for i in range(self.RING):
                key = ("d", q, i)
                self.semobj[key] = es.enter_context(nc.semaphore(f"d_{q}_{i}"))
                slots.append(key)
            self.dq[q] = {"slots": slots, "uses": [0] * self.RING, "next": 0}

    def _wait(self, ek, tok):
        if tok is None:
            return
        sk, v = tok
        if ek == "pe" and sk == "pe":
            return
        if self.waited[ek].get(sk, 0) >= v:
            return
        self.eng[ek].wait_ge(self.semobj[sk], v)
        self.waited[ek][sk] = v

    def _deps(self, ek, reads, writes):
        for b in reads:
            self._wait(ek, b.w)
        for b in writes:
            self._wait(ek, b.w)
            for sk, v in b.r.items():
                self._wait(ek, (sk, v))

    def _mark(self, tok, reads, writes):
        sk, v = tok
        for b in reads:
            if b.r.get(sk, 0) < v:
                b.r[sk] = v
        for b in writes:
            b.w = tok
            b.r = {}

    def op(self, ek, fn, reads=(), writes=()):
        self._deps(ek, reads, writes)
        ins = fn(self.eng[ek])
        self.cnt[ek] += 1
        ins.then_inc(self.semobj[ek], 1)
        tok = (ek, self.cnt[ek])
        self._mark(tok, reads, writes)
        return tok

    def dma(self, q, fn, reads=(), writes=()):
        dq = self.dq[q]
        slot = dq["next"]
        dq["next"] = (slot + 1) % self.RING
        key = dq["slots"][slot]
        uses = dq["uses"][slot]
        if uses:
            self._wait(q, (key, 16 * uses))
        self._deps(q, reads, writes)
        ins = fn(self.eng[q])
        ins.then_inc(self.semobj[key], 16)
        dq["uses"][slot] = uses + 1
        tok = (key, 16 * (uses + 1))
        self._mark(tok, reads, writes)
        return tok

    def bulk_dma(self, q, fn, reads=(), writes=(), es=None):
        key = ("bulk", len(self.semobj))
        self.semobj[key] = es.enter_context(self.nc.semaphore(f"bulk{len(self.semobj)}"))
        self._deps(q, reads, writes)
        ins = fn(self.eng[q])
        ins.then_inc(self.semobj[key], 16)
        tok = (key, 16)
        self.bulk.append(tok)
        self._mark(tok, reads, writes)
        return tok

    def barrier(self):
        toks = [(k, self.cnt[k]) for k in self.eng if self.cnt[k]]
        for q, dq in self.dq.items():
            for key, u in zip(dq["slots"], dq["uses"]):
                if u:
                    toks.append((key, 16 * u))
        toks.extend(self.bulk)
        for ek in self.eng:
            for t in toks:
                self._wait(ek, t)


class K:
    def __init__(self, nc, es):
        self.nc = nc
        self.es = es
        self.s = Sched(nc, es)
        self.banks = []
        for i in range(8):
            t = es.enter_context(nc.psum_tensor(f"bank{i}", [128, 512], F32))
            self.banks.append((t, Buf()))
        self.ident = None

    def sb(self, es, name, shape, dt):
        t = es.enter_context(self.nc.sbuf_tensor(name, list(shape), dt))
        return t, Buf()

    def poke(self):
        bg = getattr(self, "bg", None)
        if bg is not None:
            next(bg, None)


def bcast_row(ap_row, parts):
    return ap_row.partition_broadcast(parts) if len(ap_row.shape) == 1 else ap_row.to_broadcast([parts, ap_row.shape[-1]])


def stage_ada(k, cc, w_ada, b_ada, g_norm, modv):
    nc, s = k.nc, k.s
    with ExitStack() as es:
        cct, ccb = k.sb(es, "ada_cc", [128, 16], F32)
        sil, silb = k.sb(es, "ada_sil", [128, 16], F32)
        wt = [k.sb(es, f"ada_w{i}", [128, 8, 512], F32) for i in range(2)]
        brow, browb = k.sb(es, "ada_b", [2, 6144], F32)
        grow, growb = k.sb(es, "ada_g", [2, 2, 1024], F32)
        mrow, mrowb = k.sb(es, "ada_m", [2, 6144], F32)
        orow, orowb = k.sb(es, "ada_o", [2, 6, 1024], F32)

        s.dma("sp", lambda e: e.dma_start(out=cct[:], in_=cc), writes=[ccb])
        s.op("act", lambda e: e.activation(out=sil[:], in_=cct[:], func=AF.Silu), reads=[ccb], writes=[silb])
        for l in range(2):
            s.dma("sp", lambda e: e.dma_start(out=brow[:], in_=b_ada[l:l + 1, :].to_broadcast([2, 6144])), writes=[browb])
            s.dma("sp", lambda e: e.dma_start(out=grow[:], in_=g_norm[2 * l:2 * l + 2, :].rearrange("(o a) d -> o a d", o=1).to_broadcast([2, 2, 1024])), writes=[growb])
            wv = w_ada[l].rearrange("(kc p) n -> p kc n", p=128)
            for g in range(12):
                wtile, wbuf = wt[g % 2]
                q = "sp" if g % 2 == 0 else "act"
                s.dma(q, lambda e: e.dma_start(out=wtile[:], in_=wv[:, :, g * 512:(g + 1) * 512]), writes=[wbuf])
                bank, bb = k.banks[g % 2]
                for kc in range(8):
                    s.op("pe", lambda e: e.matmul(bank[0:2, :], lhsT=sil[:, 2 * kc:2 * kc + 2], rhs=wtile[:, kc, :],
                                                  start=(kc == 0), stop=(kc == 7)),
                         reads=[silb, wbuf], writes=[bb])
                s.op("dve", lambda e: e.tensor_tensor(out=mrow[:, g * 512:(g + 1) * 512], in0=bank[0:2, :],
                                                      in1=brow[:, g * 512:(g + 1) * 512], op=ALU.add),
                     reads=[bb, browb], writes=[mrowb])
            for j in range(2):
                sh = mrow[:, (3 * j) * 1024:(3 * j + 1) * 1024]
                sc = mrow[:, (3 * j + 1) * 1024:(3 * j + 2) * 1024]
                gt = mrow[:, (3 * j + 2) * 1024:(3 * j + 3) * 1024]
                s.op("dve", lambda e: e.scalar_tensor_tensor(out=orow[:, 3 * j, :], in0=sc, scalar=1.0, in1=grow[:, j, :],
                                                             op0=ALU.add, op1=ALU.mult),
                     reads=[mrowb, growb], writes=[orowb])
                s.op("dve", lambda e: e.tensor_copy(out=orow[:, 3 * j + 1, :], in_=sh), reads=[mrowb], writes=[orowb])
                s.op("dve", lambda e: e.tensor_copy(out=orow[:, 3 * j + 2, :], in_=gt), reads=[mrowb], writes=[orowb])
            s.dma("sp", lambda e: e.dma_start(out=modv[l], in_=orow[:]), reads=[orowb], writes=[k.modv_buf])
        s.barrier()


def load_cast(k, stg, dst, dstb, src, n, qi=0):
    s = k.s
    st, stb = stg[qi % len(stg)]
    s.dma("sp" if qi % 2 == 0 else "pool", lambda e: e.dma_start(out=st[:, 0:n], in_=src), writes=[stb])
    ek = ("act", "pool", "dve")[qi % 3]
    if ek == "act":
        s.op("act", lambda e: e.copy(out=dst, in_=st[:, 0:n]), reads=[stb], writes=[dstb])
    else:
        s.op(ek, lambda e: e.tensor_copy(out=dst, in_=st[:, 0:n]), reads=[stb], writes=[dstb])


def load_w_bf16(k, stg, wt, wb, wdram, kc, n, q0=0):
    qi = q0
    v = wdram.rearrange("(kc p) n -> p kc n", p=128)
    cw = stg[0][0].shape[1]
    for c in range(kc):
        for c0 in range(0, n, cw):
            c1 = min(n, c0 + cw)
            load_cast(k, stg, wt[:, c, c0:c1], wb, v[:, c, c0:c1], c1 - c0, qi)
            qi += 1
    return qi


def rstd_from_ss(k, ss, ssb, rs, rsb, inv_n, w):
    s = k.s
    s.op("act", lambda e: e.activation(out=rs[:, 0:w], in_=ss[:, 0:w], func=AF.Sqrt, scale=inv_n, bias=k.eps[:, 0:1]),
         reads=[ssb], writes=[rsb])
    s.op("dve", lambda e: e.reciprocal(out=rs[:, 0:w], in_=rs[:, 0:w]), reads=[rsb], writes=[rsb])


def norm_mod(k, xt, xb, A, B, ABb, scr, scrb, st, stb, out, outb, out_f32=None):
    s = k.s
    s.op("act", lambda e: e.activation(out=scr[:], in_=xt[:], func=AF.Square, accum_out=st[:, 0:1]),
         reads=[xb], writes=[scrb, stb])
    rstd_from_ss(k, st, stb, st, stb, 1.0 / D, 1)
    s.op("dve", lambda e: e.scalar_tensor_tensor(out=scr[:], in0=xt[:], scalar=st[:, 0:1], in1=A, op0=ALU.mult, op1=ALU.mult),
         reads=[xb, stb, ABb], writes=[scrb])
    if out_f32 is not None:
        of, ofb = out_f32
        s.op("pool", lambda e: e.tensor_tensor(out=of[:], in0=scr[:], in1=B, op=ALU.add), reads=[scrb, ABb], writes=[ofb])
        s.op("act", lambda e: e.copy(out=out[:], in_=of[:]), reads=[ofb], writes=[outb])
    else:
        s.op("pool", lambda e: e.tensor_tensor(out=out[:], in0=scr[:], in1=B, op=ALU.add), reads=[scrb, ABb], writes=[outb])


def transpose_chunks(k, bank, bankb, src_fn, nchunks, rows, dst, dstb, srcb, dst_view=None):
    s = k.s
    bv = bank[:].bitcast(BF16)
    for c in range(nchunks):
        s.op("pe", lambda e: e.transpose(out=bv[0:rows, c * 128:(c + 1) * 128], in_=src_fn(c), identity=k.ident[:]),
             reads=[srcb, k.identb], writes=[bankb])
    src = bv[0:rows, 0:nchunks * 128]
    if dst_view is not None:
        src = src.rearrange("p (c t) -> p c t", t=128)
    s.op("act", lambda e: e.copy(out=dst, in_=src), reads=[bankb], writes=[dstb])


def setup_consts(k, es, ident_d, identf_d=None):
    s = k.s
    k.ident, k.identb = k.sb(es, "ident_sb", [128, 128], BF16)
    k.eps, k.epsb = k.sb(es, "epsc", [128, 1], F32)
    s.dma("sp", lambda e: e.dma_start(out=k.ident[:], in_=ident_d), writes=[k.identb])
    if identf_d is not None:
        k.identf, _ = k.sb(es, "identf_sb", [128, 128], F32)
        s.dma("sp", lambda e: e.dma_start(out=k.identf[:], in_=identf_d), writes=[k.identb])
    s.op("dve", lambda e: e.memset(k.eps[:], EPS), writes=[k.epsb])


def load_bcast(k, tile, tb, row, n):
    k.s.dma("sp", lambda e: e.dma_start(out=tile, in_=row.to_broadcast([128, n])), writes=[tb])


def na_blocks(qt):
    if 2 <= qt <= 13:
        return [(qt - 2 + j, j) for j in range(5)]
    if qt == 0:
        return [(j, 5 + j) for j in range(4)]
    if qt == 1:
        return [(j, 9 + j) for j in range(4)]
    if qt == 14:
        return [(12 + j, 13 + j) for j in range(4)]
    return [(12 + j, 17 + j) for j in range(4)]


def run_pipelined(gens, lag, k=None):
    gens = list(gens)
    active = []
    nxt = 0
    since = lag
    while active or nxt < len(gens):
        if nxt < len(gens) and since >= lag:
            active.append(gens[nxt])
            nxt += 1
            since = 0
        for g in list(active):
            try:
                next(g)
            except StopIteration:
                active.remove(g)
        since += 1
        if k is not None:
            k.poke()


def stage_attn(k, x, ctx, modv0, W, hout, houtb):
    nc, s = k.nc, k.s
    banks = k.banks
    with ExitStack() as es:
        AB, ABb = k.sb(es, "at_AB", [128, 4, 1024], F32)
        osb, osbb = k.sb(es, "at_o", [128, NT, 1024], BF16)
        qT, qTb = k.sb(es, "at_qT", [96, 8, SEQ], BF16)
        kT, kTb = k.sb(es, "at_kT", [96, 8, SEQ + CTX], BF16)
        Vs, Vsb = k.sb(es, "at_V", [128, NT + NCT, 8, 65], BF16)
        st, stb = k.sb(es, "at_st", [128, 4], F32)
        st2, st2b = k.sb(es, "at_st2", [128, 16], F32)
        st3, st3b = k.sb(es, "at_st3", [128, 16], F32)
        xts = [k.sb(es, f"at_x{i}", [128, 1024], F32) for i in range(2)]
        scrs = [k.sb(es, f"at_scr{i}", [128, 1024], F32) for i in range(2)]
        abfs = [k.sb(es, f"at_a{i}", [128, 1024], BF16) for i in range(2)]
        aTs = [k.sb(es, f"at_aT{i}", [128, 8, 128], BF16) for i in range(2)]

        load_bcast(k, AB[:, 0, :], ABb, modv0[0:1, 0, :], 1024)
        load_bcast(k, AB[:, 1, :], ABb, modv0[0:1, 1, :], 1024)
        load_bcast(k, AB[:, 2, :], ABb, modv0[1:2, 0, :], 1024)
        load_bcast(k, AB[:, 3, :], ABb, modv0[1:2, 1, :], 1024)
        s.op("pool", lambda e: e.memset(Vs[:, :, :, 64:65], 1.0), writes=[Vsb])

        def src_tile(t):
            return ctx[t * 128:(t + 1) * 128, :] if t < NCT else x[(t - NCT) * 128:(t - NCT + 1) * 128, :]

        def front(t):
            xt, xb = xts[t % 2]
            scr, scrb = scrs[t % 2]
            abf, abfb = abfs[t % 2]
            aT, aTb = aTs[t % 2]
            s.dma("sp", lambda e: e.dma_start(out=xt[:], in_=src_tile(t)), writes=[xb])
            j = 2 if t < NCT else 0
            norm_mod(k, xt, xb, AB[:, j, :], AB[:, j + 1, :], ABb, scr, scrb, st, stb, abf, abfb)
            bank, bb = banks[7]
            transpose_chunks(k, bank, bb, lambda c: abf[:, c * 128:(c + 1) * 128], 8, 128, aT[:], aTb, abfb, dst_view=True)
            return aT, aTb, scr, scrb

        with ExitStack() as es1:
            w_in, w_inb = k.sb(es1, "p1_win", [128, 8, 672], BF16)
            w_q, w_qb = k.sb(es1, "p1_wq", [128, 3, 768], BF16)
            w_kv, w_kvb = k.sb(es1, "p1_wkv", [128, 2, 1024], BF16)
            gcn, gcnb = k.sb(es1, "p1_gcn", [128, 640], F32)
            gq, gqb = k.sb(es1, "p1_gq", [128, 96], F32)
            gk, gkb = k.sb(es1, "p1_gk", [128, 96], F32)
            zsbs = [k.sb(es1, f"p1_z{i}", [128, 672], F32) for i in range(2)]
            cn, cnb = k.sb(es1, "p1_cn", [128, 640], BF16)
            cnT, cnTb = k.sb(es1, "p1_cnT", [128, 5, 128], BF16)
            qn, qnb = k.sb(es1, "p1_qn", [128, 8, 96], F32)
            kn, knb = k.sb(es1, "p1_kn", [128, 8, 64], F32)
            qr, qrb = k.sb(es1, "p1_qr", [128, 8, 32], F32)
            rt, rtb = k.sb(es1, "p1_rt", [128, 4, 8, 16], F32)
            krg, krgb = k.sb(es1, "p1_krg", [128, 32], F32)
            krr, krrb = k.sb(es1, "p1_krr", [128, 32], F32)
            kt4, kt4b = k.sb(es1, "p1_kt4", [128, 4, 16], F32)
            qf, qfb = k.sb(es1, "p1_qf", [128, 8, 96], BF16)
            kf, kfb = k.sb(es1, "p1_kf", [128, 8, 96], BF16)
            ropes = [k.sb(es1, f"p1_rope{i}", [128, 32], F32) for i in range(2)]

            wv = W["attn_w_in"].rearrange("(kc p) n -> p kc n", p=128)
            for c in range(8):
                s.dma("pool", lambda e: e.dma_start(out=w_in[:, c, :], in_=wv[:, c, 0:672]), writes=[w_inb])
            wv = W["mla_w_q_up"].rearrange("(kc p) n -> p kc n", p=128)
            for c in range(3):
                s.dma("pool", lambda e: e.dma_start(out=w_q[:, c, :], in_=wv[:, c, :]), writes=[w_qb])
            wv = W["mla_w_kv_up"].rearrange("(kc p) n -> p kc n", p=128)
            for c in range(2):
                s.dma("pool", lambda e: e.dma_start(out=w_kv[:, c, :], in_=wv[:, c, :]), writes=[w_kvb])
            load_bcast(k, gcn[:, 0:384], gcnb, W["mla_g_qa"], 384)
            load_bcast(k, gcn[:, 384:640], gcnb, W["mla_g_kva"], 256)
            load_bcast(k, gq[:], gqb, W["mla_g_q"], 96)
            load_bcast(k, gk[:], gkb, W["mla_g_k"], 96)
            s.op("dve", lambda e: e.tensor_scalar_mul(out=gq[:], in0=gq[:], scalar1=96.0 ** -0.5), reads=[gqb], writes=[gqb])

            def p1_tile(t):
                lat = t >= NCT
                zsb, zsbb = zsbs[t % 2]
                aT, aTb, scr, scrb = front(t)
                if lat:
                    rp, rpb = ropes[t % 2]
                    s.dma("sp", lambda e: e.dma_start(out=rp[:], in_=W["rope"][(t - NCT) * 128:(t - NCT + 1) * 128, :]), writes=[rpb])
                yield
                (z0, z0b), (z1, z1b) = banks[0], banks[1]
                for (zb, zbb, c0, c1) in ((z0, z0b, 0, 512), (z1, z1b, 512, 672)):
                    for c in range(8):
                        s.op("pe", lambda e: e.matmul(zb[:, 0:c1 - c0], lhsT=aT[:, c, :], rhs=w_in[:, c, c0:c1], start=(c == 0), stop=(c == 7)),
                             reads=[aTb, w_inb], writes=[zbb])
                    s.op("act", lambda e: e.copy(out=zsb[:, c0:c1], in_=zb[:, 0:c1 - c0]), reads=[zbb], writes=[zsbb])
                yield
                if lat:
                    s.op("act", lambda e: e.activation(out=scr[:, 0:384], in_=zsb[:, 0:384], func=AF.Square, accum_out=st2[:, 0:1]),
                         reads=[zsbb], writes=[scrb, st2b])
                    rstd_from_ss(k, st2, st2b, st3, st3b, 1.0 / 384, 1)
                    s.op("dve", lambda e: e.scalar_tensor_tensor(out=cn[:, 0:384], in0=zsb[:, 0:384], scalar=st3[:, 0:1], in1=gcn[:, 0:384],
                                                                 op0=ALU.mult, op1=ALU.mult), reads=[zsbb, st3b, gcnb], writes=[cnb])
                s.op("act", lambda e: e.activation(out=scr[:, 384:640], in_=zsb[:, 384:640], func=AF.Square, accum_out=st2[:, 1:2]),
                     reads=[zsbb], writes=[scrb, st2b])
                s.op("act", lambda e: e.activation(out=st3[:, 1:2], in_=st2[:, 1:2], func=AF.Sqrt, scale=1.0 / 256, bias=k.eps[:, 0:1]),
                     reads=[st2b], writes=[st3b])
                s.op("dve", lambda e: e.reciprocal(out=st3[:, 1:2], in_=st3[:, 1:2]), reads=[st3b], writes=[st3b])
                s.op("dve", lambda e: e.scalar_tensor_tensor(out=cn[:, 384:640], in0=zsb[:, 384:640], scalar=st3[:, 1:2], in1=gcn[:, 384:640],
                                                             op0=ALU.mult, op1=ALU.mult), reads=[zsbb, st3b, gcnb], writes=[cnb])
                s.op("act", lambda e: e.activation(out=scr[:, 640:672], in_=zsb[:, 640:672], func=AF.Square, accum_out=st2[:, 2:3]),
                     reads=[zsbb], writes=[scrb, st2b])
                yield
                c_lo = 0 if lat else 3
                bank, bb = banks[7]
                bv = bank[:].bitcast(BF16)
                for c in range(c_lo, 5):
                    s.op("pe", lambda e: e.transpose(out=bv[:, c * 128:(c + 1) * 128], in_=cn[:, c * 128:(c + 1) * 128], identity=k.ident[:]),
                         reads=[cnb, k.identb], writes=[bb])
                s.op("act", lambda e: e.copy(out=cnT[:, c_lo:5, :], in_=bv[:, c_lo * 128:640].rearrange("p (c t) -> p c t", t=128)),
                     reads=[bb], writes=[cnTb])
                yield
                if lat:
                    (qa, qab), (qb_, qbb) = banks[2], banks[3]
                    for (qk, qkb, h0, h1) in ((qa, qab, 0, 5), (qb_, qbb, 5, 8)):
                        n = (h1 - h0) * 96
                        for c in range(3):
                            s.op("pe", lambda e: e.matmul(qk[:, 0:n], lhsT=cnT[:, c, :], rhs=w_q[:, c, h0 * 96:h1 * 96], start=(c == 0), stop=(c == 2)),
                                 reads=[cnTb, w_qb], writes=[qkb])
                        s.op("act", lambda e: e.activation(out=scr[:, h0 * 96:h1 * 96], in_=qk[:, 0:n], func=AF.Square), reads=[qkb], writes=[scrb])
                    s.op("dve", lambda e: e.tensor_reduce(out=st2[:, 4:12], in_=scr[:, 0:768].rearrange("p (h d) -> p h d", d=96), axis=AX.X, op=ALU.add),
                         reads=[scrb], writes=[st2b])
                    s.op("act", lambda e: e.activation(out=st3[:, 4:12], in_=st2[:, 4:12], func=AF.Sqrt, scale=1.0 / 96, bias=k.eps[:, 0:1]),
                         reads=[st2b], writes=[st3b])
                    s.op("dve", lambda e: e.reciprocal(out=st3[:, 4:12], in_=st3[:, 4:12]), reads=[st3b], writes=[st3b])
                    for (qk, qkb, h0, h1) in ((qa, qab, 0, 5), (qb_, qbb, 5, 8)):
                        n = (h1 - h0) * 96
                        s.op("dve", lambda e: e.tensor_tensor(out=qn[:, h0:h1, :], in0=qk[:, 0:n].rearrange("p (h d) -> p h d", d=96),
                                                              in1=st3[:, 4 + h0:4 + h1].unsqueeze(2).to_broadcast([128, h1 - h0, 96]), op=ALU.mult),
                             reads=[qkb, st3b], writes=[qnb])
                    s.op("pool", lambda e: e.tensor_tensor(out=qf[:, :, 0:64], in0=qn[:, :, 0:64],
                                                           in1=gq[:, 0:64].unsqueeze(1).to_broadcast([128, 8, 64]), op=ALU.mult),
                         reads=[qnb, gqb], writes=[qfb])
                    s.op("dve", lambda e: e.tensor_tensor(out=qr[:], in0=qn[:, :, 64:96],
                                                          in1=gq[:, 64:96].unsqueeze(1).to_broadcast([128, 8, 32]), op=ALU.mult),
                         reads=[qnb, gqb], writes=[qrb])
                    cosb = rp[:, 0:16].unsqueeze(1).to_broadcast([128, 8, 16])
                    sinb = rp[:, 16:32].unsqueeze(1).to_broadcast([128, 8, 16])
                    s.op("dve", lambda e: e.tensor_tensor(out=rt[:, 0], in0=qr[:, :, 0:16], in1=cosb, op=ALU.mult), reads=[qrb, rpb], writes=[rtb])
                    s.op("dve", lambda e: e.tensor_tensor(out=rt[:, 1], in0=qr[:, :, 16:32], in1=sinb, op=ALU.mult), reads=[qrb, rpb], writes=[rtb])
                    s.op("dve", lambda e: e.tensor_tensor(out=rt[:, 2], in0=qr[:, :, 0:16], in1=sinb, op=ALU.mult), reads=[qrb, rpb], writes=[rtb])
                    s.op("dve", lambda e: e.tensor_tensor(out=rt[:, 3], in0=qr[:, :, 16:32], in1=cosb, op=ALU.mult), reads=[qrb, rpb], writes=[rtb])
                    s.op("dve", lambda e: e.tensor_tensor(out=qf[:, :, 64:80], in0=rt[:, 0], in1=rt[:, 1], op=ALU.subtract), reads=[rtb], writes=[qfb])
                    s.op("dve", lambda e: e.tensor_tensor(out=qf[:, :, 80:96], in0=rt[:, 2], in1=rt[:, 3], op=ALU.add), reads=[rtb], writes=[qfb])
                yield
                (ka, kab), (kb_, kbb) = banks[4], banks[5]
                for g, (kk, kkb) in enumerate(((ka, kab), (kb_, kbb))):
                    for c in range(2):
                        s.op("pe", lambda e: e.matmul(kk[:, :], lhsT=cnT[:, 3 + c, :], rhs=w_kv[:, c, g * 512:(g + 1) * 512], start=(c == 0), stop=(c == 1)),
                             reads=[cnTb, w_kvb], writes=[kkb])
                    kv3 = kk[:, :].rearrange("p (h d) -> p h d", d=128)
                    s.op("act", lambda e: e.activation(out=scr[:, g * 256:(g + 1) * 256].rearrange("p (h d) -> p h d", d=64), in_=kv3[:, :, 0:64], func=AF.Square),
                         reads=[kkb], writes=[scrb])
                    s.op("act", lambda e: e.copy(out=Vs[:, t, g * 4:(g + 1) * 4, 0:64], in_=kv3[:, :, 64:128]), reads=[kkb], writes=[Vsb])
                yield
                s.op("dve", lambda e: e.tensor_reduce(out=st2[:, 4:12], in_=scr[:, 0:512].rearrange("p (h d) -> p h d", d=64), axis=AX.X, op=ALU.add),
                     reads=[scrb], writes=[st2b])
                s.op("dve", lambda e: e.tensor_scalar(out=st2[:, 4:12], in0=st2[:, 4:12], scalar1=st2[:, 2:3], scalar2=None, op0=ALU.add),
                     reads=[st2b], writes=[st2b])
                s.op("act", lambda e: e.activation(out=st3[:, 4:12], in_=st2[:, 4:12], func=AF.Sqrt, scale=1.0 / 96, bias=k.eps[:, 0:1]),
                     reads=[st2b], writes=[st3b])
                s.op("dve", lambda e: e.reciprocal(out=st3[:, 4:12], in_=st3[:, 4:12]), reads=[st3b], writes=[st3b])
                for g, (kk, kkb) in enumerate(((ka, kab), (kb_, kbb))):
                    kv3 = kk[:, :].rearrange("p (h d) -> p h d", d=128)
                    s.op("dve", lambda e: e.tensor_tensor(out=kn[:, g * 4:(g + 1) * 4, :], in0=kv3[:, :, 0:64],
                                                          in1=st3[:, 4 + g * 4:8 + g * 4].unsqueeze(2).to_broadcast([128, 4, 64]), op=ALU.mult),
                         reads=[kkb, st3b], writes=[knb])
                s.op("pool", lambda e: e.tensor_tensor(out=kf[:, :, 0:64], in0=kn[:], in1=gk[:, 0:64].unsqueeze(1).to_broadcast([128, 8, 64]), op=ALU.mult),
                     reads=[knb, gkb], writes=[kfb])
                s.op("dve", lambda e: e.tensor_tensor(out=krg[:], in0=zsb[:, 640:672], in1=gk[:, 64:96], op=ALU.mult), reads=[zsbb, gkb], writes=[krgb])
                if lat:
                    s.op("dve", lambda e: e.tensor_tensor(out=kt4[:, 0], in0=krg[:, 0:16], in1=rp[:, 0:16], op=ALU.mult), reads=[krgb, rpb], writes=[kt4b])
                    s.op("dve", lambda e: e.tensor_tensor(out=kt4[:, 1], in0=krg[:, 16:32], in1=rp[:, 16:32], op=ALU.mult), reads=[krgb, rpb], writes=[kt4b])
                    s.op("dve", lambda e: e.tensor_tensor(out=kt4[:, 2], in0=krg[:, 0:16], in1=rp[:, 16:32], op=ALU.mult), reads=[krgb, rpb], writes=[kt4b])
                    s.op("dve", lambda e: e.tensor_tensor(out=kt4[:, 3], in0=krg[:, 16:32], in1=rp[:, 0:16], op=ALU.mult), reads=[krgb, rpb], writes=[kt4b])
                    s.op("dve", lambda e: e.tensor_tensor(out=krr[:, 0:16], in0=kt4[:, 0], in1=kt4[:, 1], op=ALU.subtract), reads=[kt4b], writes=[krrb])
                    s.op("dve", lambda e: e.tensor_tensor(out=krr[:, 16:32], in0=kt4[:, 2], in1=kt4[:, 3], op=ALU.add), reads=[kt4b], writes=[krrb])
                else:
                    s.op("dve", lambda e: e.tensor_copy(out=krr[:], in_=krg[:]), reads=[krgb], writes=[krrb])
                s.op("dve", lambda e: e.tensor_tensor(out=kf[:, :, 64:96], in0=krr[:].unsqueeze(1).to_broadcast([128, 8, 32]),
                                                      in1=st3[:, 4:12].unsqueeze(2).to_broadcast([128, 8, 32]), op=ALU.mult),
                     reads=[krrb, st3b], writes=[kfb])
                yield
                if lat:
                    tb_, tbb = banks[6]
                    tl = t - NCT
                    transpose_chunks(k, tb_, tbb, lambda h: qf[:, h, :], 8, 96, qT[:, :, tl * 128:(tl + 1) * 128], qTb, qfb, dst_view=True)
                tb_, tbb = banks[0]
                transpose_chunks(k, tb_, tbb, lambda h: kf[:, h, :], 8, 96, kT[:, :, t * 128:(t + 1) * 128], kTb, kfb, dst_view=True)
            run_pipelined([p1_tile(t) for t in range(NT + NCT)], 4, k)
            s.barrier()

        with ExitStack() as es2:
            pTs = [k.sb(es2, f"p2_pT{i}", [128, 512], BF16) for i in range(3)]
            rc, rcb = k.sb(es2, "p2_rc", [128, 8], F32)
            steps = [(h, g, kt) for h in range(8) for g in range(4) for kt in range(NT + NCT)]

            def emit_s(i):
                h, g, kt = steps[i]
                sbk, sbkb = banks[i % 2]
                pT, pTb = pTs[i % 3]
                s.op("pe", lambda e: e.matmul(sbk[:, :], lhsT=kT[:, h, kt * 128:(kt + 1) * 128], rhs=qT[:, h, g * 512:(g + 1) * 512], start=True, stop=True),
                     reads=[kTb, qTb], writes=[sbkb])
                s.op("act", lambda e: e.activation(out=pT[:], in_=sbk[:, :], func=AF.Exp), reads=[sbkb], writes=[pTb])

            def emit_pv(i):
                h, g, kt = steps[i]
                pT, pTb = pTs[i % 3]
                for qs in range(4):
                    ob, obb = banks[2 + qs]
                    s.op("pe", lambda e: e.matmul(ob[:, 0:65], lhsT=pT[:, qs * 128:(qs + 1) * 128], rhs=Vs[:, kt, h, :],
                                                  start=(kt == 0), stop=(kt == NT + NCT - 1)), reads=[pTb, Vsb], writes=[obb])
                if kt == NT + NCT - 1:
                    for qs in range(4):
                        ob, obb = banks[2 + qs]
                        j = (g * 4 + qs) % 8
                        s.op("dve", lambda e: e.reciprocal(out=rc[:, j:j + 1], in_=ob[:, 64:65]), reads=[obb], writes=[rcb])
                        s.op("dve", lambda e: e.tensor_scalar(out=osb[:, g * 4 + qs, h * 64:(h + 1) * 64], in0=ob[:, 0:64], scalar1=rc[:, j:j + 1],
                                                              scalar2=None, op0=ALU.mult), reads=[obb, rcb], writes=[osbb])

            emit_s(0)
            for i in range(len(steps)):
                if i + 1 < len(steps):
                    emit_s(i + 1)
                emit_pv(i)
                if i % 8 == 7:
                    k.poke()
            s.barrier()

        with ExitStack() as es3:
            stg = [k.sb(es3, f"p3_stg{i}", [128, 1536], F32) for i in range(2)]
            w_in, w_inb = k.sb(es3, "p3_win", [128, 8, 1536], BF16)
            gqn, gqnb = k.sb(es3, "p3_gq", [128, 64], F32)
            gkn, gknb = k.sb(es3, "p3_gk", [128, 64], F32)
            tn, tnb = k.sb(es3, "p3_tn", [128, 16, 64], F32)
            qkf, qkfb = k.sb(es3, "p3_qkf", [128, 16, 64], BF16)
            wv = W["attn_w_in"].rearrange("(kc p) n -> p kc n", p=128)
            for c in range(8):
                load_cast(k, stg, w_in[:, c, :], w_inb, wv[:, c, 672:2208], 1536, c)
            load_bcast(k, gqn[:], gqnb, W["na_g_q"], 64)
            load_bcast(k, gkn[:], gknb, W["na_g_k"], 64)
            s.op("dve", lambda e: e.tensor_scalar_mul(out=gqn[:], in0=gqn[:], scalar1=64.0 ** -0.5), reads=[gqnb], writes=[gqnb])
            def p3_tile(t):
                lat = t >= NCT
                aT, aTb, scr, scrb = front(t)
                yield
                grp = (0, 1, 2) if lat else (1, 2)
                for g in grp:
                    zb, zbb = banks[g]
                    for c in range(8):
                        s.op("pe", lambda e: e.matmul(zb[:, :], lhsT=aT[:, c, :], rhs=w_in[:, c, g * 512:(g + 1) * 512], start=(c == 0), stop=(c == 7)),
                             reads=[aTb, w_inb], writes=[zbb])
                    if g < 2:
                        s.op("act", lambda e: e.activation(out=scr[:, g * 512:(g + 1) * 512], in_=zb[:, :], func=AF.Square), reads=[zbb], writes=[scrb])
                    else:
                        s.op("act", lambda e: e.copy(out=Vs[:, t, :, 0:64], in_=zb[:, :].rearrange("p (h d) -> p h d", d=64)), reads=[zbb], writes=[Vsb])
                yield
                g0 = 0 if lat else 1
                s.op("dve", lambda e: e.tensor_reduce(out=st2[:, g0 * 8:16], in_=scr[:, g0 * 512:1024].rearrange("p (h d) -> p h d", d=64), axis=AX.X, op=ALU.add),
                     reads=[scrb], writes=[st2b])
                s.op("act", lambda e: e.activation(out=st3[:, g0 * 8:16], in_=st2[:, g0 * 8:16], func=AF.Sqrt, scale=1.0 / 64, bias=k.eps[:, 0:1]),
                     reads=[st2b], writes=[st3b])
                s.op("dve", lambda e: e.reciprocal(out=st3[:, g0 * 8:16], in_=st3[:, g0 * 8:16]), reads=[st3b], writes=[st3b])
                for g in grp[:-1]:
                    zb, zbb = banks[g]
                    gg, ggb = (gqn, gqnb) if g == 0 else (gkn, gknb)
                    s.op("dve", lambda e: e.tensor_tensor(out=tn[:, g * 8:(g + 1) * 8, :], in0=zb[:, :].rearrange("p (h d) -> p h d", d=64),
                                                          in1=st3[:, g * 8:(g + 1) * 8].unsqueeze(2).to_broadcast([128, 8, 64]), op=ALU.mult),
                         reads=[zbb, st3b], writes=[tnb])
                    s.op("pool", lambda e: e.tensor_tensor(out=qkf[:, g * 8:(g + 1) * 8, :], in0=tn[:, g * 8:(g + 1) * 8, :],
                                                           in1=gg[:].unsqueeze(1).to_broadcast([128, 8, 64]), op=ALU.mult),
                         reads=[tnb, ggb], writes=[qkfb])
                yield
                if lat:
                    tb_, tbb = banks[3]
                    tl = t - NCT
                    transpose_chunks(k, tb_, tbb, lambda h: qkf[:, h, :], 8, 64, qT[0:64, :, tl * 128:(tl + 1) * 128], qTb, qkfb, dst_view=True)
                tb_, tbb = banks[4]
                transpose_chunks(k, tb_, tbb, lambda h: qkf[:, 8 + h, :], 8, 64, kT[0:64, :, t * 128:(t + 1) * 128], kTb, qkfb, dst_view=True)
            run_pipelined([p3_tile(t) for t in range(NT + NCT)], 2, k)
            s.barrier()

        with ExitStack() as es4:
            nbs = [k.sb(es4, f"p4_nb{i}", [128, 21, 128], F32) for i in range(2)]
            sfs = [k.sb(es4, f"p4_sf{i}", [128, 640], F32) for i in range(2)]
            pTs = [k.sb(es4, f"p4_pT{i}", [128, 896], BF16) for i in range(2)]
            rc, rcb = k.sb(es4, "p4_rc", [128, 8], F32)
            steps = [(h, qt) for h in range(8) for qt in range(NT)]

            def res(i):
                return (banks[(i % 2) * 2], banks[(i % 2) * 2 + 1], sfs[i % 2], pTs[i % 2], banks[4 + i % 2])

            def emit_s(i):
                h, qt = steps[i]
                nb, nbb = nbs[h % 2]
                if qt == 0:
                    s.dma("sp", lambda e: e.dma_start(out=nb[:], in_=W["nabias"][h]), writes=[nbb])
                blocks = na_blocks(qt)
                nloc = len(blocks)
                (sa, sab), (sb_, sbb), (sf, sfb), (pT, pTb), _ = res(i)
                qsl = qT[0:64, h, qt * 128:(qt + 1) * 128]
                for j, (kt, bi) in enumerate(blocks):
                    dstb_, dstbb = (sa, sab) if j < 4 else (sb_, sbb)
                    col = (j % 4) * 128
                    s.op("pe", lambda e: e.matmul(dstb_[:, col:col + 128], lhsT=kT[0:64, h, (NCT + kt) * 128:(NCT + kt + 1) * 128], rhs=qsl, start=True, stop=True),
                         reads=[kTb, qTb], writes=[dstbb])
                for c in range(NCT):
                    s.op("pe", lambda e: e.matmul(sb_[:, 128 + c * 128:256 + c * 128], lhsT=kT[0:64, h, c * 128:(c + 1) * 128], rhs=qsl, start=True, stop=True),
                         reads=[kTb, qTb], writes=[sbb])
                b0 = blocks[0][1]
                s.op("dve", lambda e: e.tensor_tensor(out=sf[:, 0:512], in0=sa[:, :], in1=nb[:, b0:b0 + 4, :].rearrange("p b q -> p (b q)"), op=ALU.add),
                     reads=[sab, nbb], writes=[sfb])
                if nloc == 5:
                    s.op("dve", lambda e: e.tensor_tensor(out=sf[:, 512:640], in0=sb_[:, 0:128], in1=nb[:, b0 + 4, :], op=ALU.add),
                         reads=[sbb, nbb], writes=[sfb])
                s.op("act", lambda e: e.activation(out=pT[:, 0:nloc * 128], in_=sf[:, 0:nloc * 128], func=AF.Exp), reads=[sfb], writes=[pTb])
                s.op("act", lambda e: e.activation(out=pT[:, 640:896], in_=sb_[:, 128:384], func=AF.Exp), reads=[sbb], writes=[pTb])

            def emit_pv(i):
                h, qt = steps[i]
                blocks = na_blocks(qt)
                _, _, _, (pT, pTb), (ob, obb) = res(i)
                for j, (kt, bi) in enumerate(blocks):
                    s.op("pe", lambda e: e.matmul(ob[:, 0:65], lhsT=pT[:, j * 128:(j + 1) * 128], rhs=Vs[:, NCT + kt, h, :], start=(j == 0), stop=False),
                         reads=[pTb, Vsb], writes=[obb])
                for c in range(NCT):
                    s.op("pe", lambda e: e.matmul(ob[:, 0:65], lhsT=pT[:, 640 + c * 128:768 + c * 128], rhs=Vs[:, c, h, :], start=False, stop=(c == NCT - 1)),
                         reads=[pTb, Vsb], writes=[obb])
                j8 = i % 8
                s.op("dve", lambda e: e.reciprocal(out=rc[:, j8:j8 + 1], in_=ob[:, 64:65]), reads=[obb], writes=[rcb])
                s.op("dve", lambda e: e.tensor_scalar(out=osb[:, qt, 512 + h * 64:512 + (h + 1) * 64], in0=ob[:, 0:64], scalar1=rc[:, j8:j8 + 1],
                                                      scalar2=None, op0=ALU.mult), reads=[obb, rcb], writes=[osbb])

            emit_s(0)
            for i in range(len(steps)):
                if i + 1 < len(steps):
                    emit_s(i + 1)
                emit_pv(i)
                if i % 8 == 7:
                    k.poke()
            s.barrier()

        with ExitStack() as es5:
            stg = [k.sb(es5, f"p5_stg{i}", [128, 1024], F32) for i in range(2)]
            w_o, w_ob = k.sb(es5, "p5_wo", [128, 8, 1024], BF16)
            G1, G1b = k.sb(es5, "p5_g1", [128, 1024], F32)
            outs = [k.sb(es5, f"p5_out{i}", [128, 1024], F32) for i in range(2)]
            wv = W["attn_w_out"].rearrange("(kc p) n -> p kc n", p=128)
            for c in range(8):
                load_cast(k, stg, w_o[:, c, :], w_ob, wv[:, c, :], 1024, c)
            load_bcast(k, G1[:], G1b, modv0[0:1, 2, :], 1024)
            for t in range(NT):
                xt, xb = xts[t % 2]
                scr, scrb = scrs[t % 2]
                aT, aTb = aTs[t % 2]
                ot, otb = outs[t % 2]
                s.dma("sp", lambda e: e.dma_start(out=xt[:], in_=x[t * 128:(t + 1) * 128, :]), writes=[xb])
                bank, bb = banks[7]
                transpose_chunks(k, bank, bb, lambda c: osb[:, t, c * 128:(c + 1) * 128], 8, 128, aT[:], aTb, osbb, dst_view=True)
                for g in range(2):
                    yb, ybb = banks[g]
                    for c in range(8):
                        s.op("pe", lambda e: e.matmul(yb[:, :], lhsT=aT[:, c, :], rhs=w_o[:, c, g * 512:(g + 1) * 512], start=(c == 0), stop=(c == 7)),
                             reads=[aTb, w_ob], writes=[ybb])
                    s.op("dve", lambda e: e.tensor_tensor(out=scr[:, g * 512:(g + 1) * 512], in0=yb[:, :], in1=G1[:, g * 512:(g + 1) * 512], op=ALU.mult),
                         reads=[ybb, G1b], writes=[scrb])
                s.op("pool", lambda e: e.tensor_tensor(out=ot[:], in0=scr[:], in1=xt[:], op=ALU.add), reads=[scrb, xb], writes=[otb])
                s.dma("sp", lambda e: e.dma_start(out=hout[t * 128:(t + 1) * 128, :], in_=ot[:]), reads=[otb], writes=[houtb[t]])
            s.barrier()


def host_rope_table():
    t = np.arange(SEQ)
    row = (t // 64).astype(np.float32)
    col = (t % 64).astype(np.float32)
    inv = (np.float32(1.0) / (np.float32(10000.0) ** (np.arange(8, dtype=np.float32) / np.float32(8)))).astype(np.float32)
    ang = np.concatenate([row[:, None] * inv, col[:, None] * inv], axis=-1).astype(np.float32)
    return np.concatenate([np.cos(ang), np.sin(ang)], axis=-1).astype(np.float32)


def host_na_bias(rpb):
    pairs = [(2, j) for j in range(5)] + [(0, j) for j in range(4)] + [(1, j) for j in range(4)] \
        + [(14, 12 + j) for j in range(4)] + [(15, 12 + j) for j in range(4)]
    out = np.full((8, 128, 21, 128), NEG, np.float32)
    p = np.arange(128)
    for bi, (qt, kt) in enumerate(pairs):
        tq = qt * 128 + p
        tk = kt * 128 + p
        r, c = tq // 64, tq % 64
        kr, kc = tk // 64, tk % 64
        r0 = np.clip(r - 4, 0, 24)
        c0 = np.clip(c - 8, 0, 48)
        inside = (kr[:, None] >= r0[None, :]) & (kr[:, None] < r0[None, :] + 8) & (kc[:, None] >= c0[None, :]) & (kc[:, None] < c0[None, :] + 16)
        rr = np.clip(kr[:, None] - r[None, :] + 7, 0, 14)
        rc = np.clip(kc[:, None] - c[None, :] + 15, 0, 30)
        vals = rpb[:, rr, rc]
        out[:, :, bi, :] = np.where(inside[None], vals, np.float32(NEG))
    return out


NROW = 8


def stage_peer(k, hin, hinb, modv_l, w_query, skT_d, u_tab, v_tab, hout, houtb, tag):
    nc, s = k.nc, k.s
    banks = k.banks
    with ExitStack() as es:
        stg = [k.sb(es, f"{tag}_stg{i}", [128, 2048], F32) for i in range(2)]
        wq, wqb = k.sb(es, f"{tag}_wq", [128, 8, 2048], BF16)
        skT, skTb = k.sb(es, f"{tag}_skT", [128, 16, 128], BF16)
        ABG, ABGb = k.sb(es, f"{tag}_ABG", [128, 3, 1024], F32)
        iota_i, iota_ib = k.sb(es, f"{tag}_iotai", [128, 16], I32)
        iota, iotab = k.sb(es, f"{tag}_iota", [128, 16], F32)
        st, stb = k.sb(es, f"{tag}_st", [128, 4], F32)
        xts = [k.sb(es, f"{tag}_x{i}", [128, 1024], F32) for i in range(2)]
        scrs = [k.sb(es, f"{tag}_scr{i}", [128, 1024], F32) for i in range(2)]
        hms = [k.sb(es, f"{tag}_hm{i}", [128, 1024], F32) for i in range(2)]
        hbf, hbfb = k.sb(es, f"{tag}_hbf", [128, 1024], BF16)
        hT, hTb = k.sb(es, f"{tag}_hT", [128, 8, 128], BF16)
        qbf, qbfb = k.sb(es, f"{tag}_qbf", [128, 2048], BF16)
        qT, qTb = k.sb(es, f"{tag}_qT", [128, 16, 128], BF16)
        ssb, ssbb = k.sb(es, f"{tag}_s", [128, 16, 128], F32)
        s2, _ = k.sb(es, f"{tag}_s2", [128, 16, 128], F32)
        m16, _ = k.sb(es, f"{tag}_m16", [128, 16, 16], F32)
        i16, _ = k.sb(es, f"{tag}_i16", [128, 16, 16], U32)
        i16f, i16fb = k.sb(es, f"{tag}_i16f", [128, 16, 16], F32)
        cand, candb = k.sb(es, f"{tag}_cand", [128, 8, 256], F32)
        cand2, _ = k.sb(es, f"{tag}_cand2", [128, 8, 256], F32)
        best, _ = k.sb(es, f"{tag}_best", [128, 8, 16], F32)
        pos, _ = k.sb(es, f"{tag}_pos", [128, 8, 16], U32)
        ab_i, ab_ib = k.sb(es, f"{tag}_abi", [128, 2, 128], I32)
        ab_f, ab_fb = k.sb(es, f"{tag}_abf", [128, 2, 128], F32)
        oh, ohb = k.sb(es, f"{tag}_oh", [128, 8, 16, 16], F32)
        e01, e01b = k.sb(es, f"{tag}_e01", [128, 2, 128], F32)
        idxs = [k.sb(es, f"{tag}_idx{i}", [128, 128], I32) for i in range(2)]
        gts = [k.sb(es, f"{tag}_gate{i}", [128, 8, 16], F32) for i in range(2)]
        gsum, gsumb = k.sb(es, f"{tag}_gsum", [128, 8], F32)
        actv, _ = k.sb(es, f"{tag}_act", [128, 128], F32)
        wgt, wgtb = k.sb(es, f"{tag}_wgt", [128, 128], F32)
        junk, _ = k.sb(es, f"{tag}_junk", [128, 1024], BF16)
        rows = [k.sb(es, f"{tag}_row{i}", [128, 1024], F32) for i in range(NROW)]
        accs = [k.sb(es, f"{tag}_acc{i}", [128, 1024], F32) for i in range(4)]
        ot, otb = k.sb(es, f"{tag}_ot", [128, 1024], F32)
        hpb = [[Buf() for _ in range(16)] for _ in range(3)]
        hb = [[Buf() for _ in range(8)] for _ in range(3)]
        actb = [Buf() for _ in range(16)]

        qi = load_w_bf16(k, stg, wq, wqb, w_query, 8, 2048)
        load_cast(k, stg, skT[:].rearrange("p a b -> p (a b)"), skTb, skT_d.rearrange("p a b -> p (a b)"), 2048, qi)
        for j in range(3):
            load_bcast(k, ABG[:, j, :], ABGb, modv_l[0:1, 3 + j, :], 1024)
        s.op("pool", lambda e: e.iota(out=iota_i[:], pattern=[[1, 16]], base=0, channel_multiplier=0), writes=[iota_ib])
        s.op("dve", lambda e: e.tensor_copy(out=iota[:], in_=iota_i[:]), reads=[iota_ib], writes=[iotab])

        def front(t):
            xt, xb = xts[t % 2]
            scr, scrb = scrs[t % 2]
            hm, hmb = hms[t % 2]
            idx, idxb = idxs[t % 2]
            gate, gateb = gts[t % 2]
            s.dma("sp", lambda e: e.dma_start(out=xt[:], in_=hin[t * 128:(t + 1) * 128, :]), reads=[hinb[t]], writes=[xb])
            norm_mod(k, xt, xb, ABG[:, 0, :], ABG[:, 1, :], ABGb, scr, scrb, st, stb, hbf, hbfb, out_f32=(hm, hmb))
            bank, bb = banks[7]
            transpose_chunks(k, bank, bb, lambda c: hbf[:, c * 128:(c + 1) * 128], 8, 128, hT[:], hTb, hbfb, dst_view=True)
            for g in range(4):
                qb_, qbb = banks[g]
                for c in range(8):
                    s.op("pe", lambda e: e.matmul(qb_[:, :], lhsT=hT[:, c, :], rhs=wq[:, c, g * 512:(g + 1) * 512], start=(c == 0), stop=(c == 7)),
                         reads=[hTb, wqb], writes=[qbb])
                s.op("act", lambda e: e.copy(out=qbf[:, g * 512:(g + 1) * 512], in_=qb_[:, :]), reads=[qbb], writes=[qbfb])
            for half in range(2):
                tb_, tbb = banks[4 + half]
                transpose_chunks(k, tb_, tbb, lambda c: qbf[:, (half * 8 + c) * 128:(half * 8 + c + 1) * 128], 8, 128,
                                 qT[:, half * 8:(half + 1) * 8, :], qTb, qbfb, dst_view=True)
            for g in range(4):
                sb_, sbb = banks[g]
                for j in range(4):
                    hp = g * 4 + j
                    s.op("pe", lambda e: e.matmul(sb_[:, j * 128:(j + 1) * 128], lhsT=qT[:, hp, :], rhs=skT[:, hp, :], start=True, stop=True),
                         reads=[qTb, skTb], writes=[sbb])
                s.op("act", lambda e: e.copy(out=ssb[:, g * 4:(g + 1) * 4, :], in_=sb_[:, :].rearrange("p (a b) -> p a b", b=128)), reads=[sbb], writes=[ssbb])
            for hp in range(16):
                s.op("dve", lambda e: e.max(out=m16[:, hp, 0:8], in_=ssb[:, hp, :]), reads=[ssbb], writes=[hpb[0][hp]])
            for hp in range(16):
                s.op("dve", lambda e: e.max_index(out=i16[:, hp, 0:8], in_max=m16[:, hp, 0:8], in_values=ssb[:, hp, :]),
                     reads=[ssbb, hpb[0][hp]], writes=[hpb[1][hp]])
            for hp in range(16):
                s.op("dve", lambda e: e.match_replace(out=s2[:, hp, :], in_to_replace=m16[:, hp, 0:8], in_values=ssb[:, hp, :], imm_value=-1e30),
                     reads=[ssbb, hpb[0][hp]], writes=[hpb[2][hp]])
            for hp in range(16):
                s.op("dve", lambda e: e.max(out=m16[:, hp, 8:16], in_=s2[:, hp, :]), reads=[hpb[2][hp]], writes=[hpb[0][hp]])
            for hp in range(16):
                s.op("dve", lambda e: e.max_index(out=i16[:, hp, 8:16], in_max=m16[:, hp, 8:16], in_values=s2[:, hp, :]),
                     reads=[hpb[2][hp], hpb[0][hp]], writes=[hpb[1][hp]])
            s.op("dve", lambda e: e.tensor_copy(out=i16f[:], in_=i16[:]), reads=hpb[1], writes=[i16fb])
            m4 = m16[:].rearrange("p (h t) a -> p h t a", t=2)
            s.op("dve", lambda e: e.tensor_tensor(out=cand[:].rearrange("p h (a b) -> p h a b", b=16),
                                                  in0=m4[:, :, 0, :].unsqueeze(3).to_broadcast([128, 8, 16, 16]),
                                                  in1=m4[:, :, 1, :].unsqueeze(2).to_broadcast([128, 8, 16, 16]), op=ALU.add),
                 reads=hpb[0], writes=[candb])
            for h in range(8):
                s.op("dve", lambda e: e.max(out=best[:, h, 0:8], in_=cand[:, h, :]), reads=[candb], writes=[hb[0][h]])
            for h in range(8):
                s.op("dve", lambda e: e.max_index(out=pos[:, h, 0:8], in_max=best[:, h, 0:8], in_values=cand[:, h, :]),
                     reads=[candb, hb[0][h]], writes=[hb[1][h]])
            for h in range(8):
                s.op("dve", lambda e: e.match_replace(out=cand2[:, h, :], in_to_replace=best[:, h, 0:8], in_values=cand[:, h, :], imm_value=-1e30),
                     reads=[candb, hb[0][h]], writes=[hb[2][h]])
            for h in range(8):
                s.op("dve", lambda e: e.max(out=best[:, h, 8:16], in_=cand2[:, h, :]), reads=[hb[2][h]], writes=[hb[0][h]])
            for h in range(8):
                s.op("dve", lambda e: e.max_index(out=pos[:, h, 8:16], in_max=best[:, h, 8:16], in_values=cand2[:, h, :]),
                     reads=[hb[2][h], hb[0][h]], writes=[hb[1][h]])
            posi = pos[:].rearrange("p h k -> p (h k)").bitcast(I32)
            s.op("dve", lambda e: e.tensor_single_scalar(out=ab_i[:, 0, :], in_=posi, scalar=4, op=ALU.arith_shift_right), reads=hb[1], writes=[ab_ib])
            s.op("dve", lambda e: e.tensor_single_scalar(out=ab_i[:, 1, :], in_=posi, scalar=15, op=ALU.bitwise_and), reads=hb[1], writes=[ab_ib])
            s.op("dve", lambda e: e.tensor_copy(out=ab_f[:], in_=ab_i[:]), reads=[ab_ib], writes=[ab_fb])
            i4 = i16f[:].rearrange("p (h t) a -> p h t a", t=2)
            for p_ in range(2):
                s.op("dve", lambda e: e.tensor_tensor(out=oh[:], in0=ab_f[:, p_, :].rearrange("p (h k) -> p h k", k=16).unsqueeze(3).to_broadcast([128, 8, 16, 16]),
                                                      in1=iota[:].unsqueeze(1).unsqueeze(1).to_broadcast([128, 8, 16, 16]), op=ALU.is_equal),
                     reads=[ab_fb, iotab], writes=[ohb])
                s.op("dve", lambda e: e.tensor_tensor(out=oh[:], in0=oh[:], in1=i4[:, :, p_, :].unsqueeze(2).to_broadcast([128, 8, 16, 16]), op=ALU.mult),
                     reads=[ohb, i16fb], writes=[ohb])
                s.op("dve", lambda e: e.tensor_reduce(out=e01[:, p_, :].rearrange("p (h k) -> p h k", k=16), in_=oh[:], axis=AX.X, op=ALU.add),
                     reads=[ohb], writes=[e01b])
            s.op("dve", lambda e: e.scalar_tensor_tensor(out=e01[:, 0, :], in0=e01[:, 0, :], scalar=128.0, in1=e01[:, 1, :], op0=ALU.mult, op1=ALU.add),
                 reads=[e01b], writes=[e01b])
            s.op("dve", lambda e: e.tensor_copy(out=idx[:], in_=e01[:, 0, :]), reads=[e01b], writes=[idxb])
            s.op("dve", lambda e: e.tensor_tensor(out=gate[:], in0=best[:], in1=best[:, :, 0:1].to_broadcast([128, 8, 16]), op=ALU.subtract),
                 reads=hb[0], writes=[gateb])
            s.op("act", lambda e: e.activation(out=gate[:], in_=gate[:], func=AF.Exp), reads=[gateb], writes=[gateb])
            s.op("dve", lambda e: e.tensor_reduce(out=gsum[:], in_=gate[:], axis=AX.X, op=ALU.add), reads=[gateb], writes=[gsumb])
            s.op("dve", lambda e: e.reciprocal(out=gsum[:], in_=gsum[:]), reads=[gsumb], writes=[gsumb])
            s.op("dve", lambda e: e.tensor_tensor(out=gate[:], in0=gate[:], in1=gsum[:].unsqueeze(2).to_broadcast([128, 8, 16]), op=ALU.mult),
                 reads=[gateb, gsumb], writes=[gateb])

        ring = [0]

        def gather(tab, idx, idxb, hk):
            rw, rwb = rows[ring[0] % NROW]
            ring[0] += 1
            s.dma("pool", lambda e: e.indirect_dma_start(out=rw[:], out_offset=None, in_=tab,
                                                         in_offset=bass.IndirectOffsetOnAxis(ap=idx[:, hk:hk + 1], axis=0)),
                  reads=[idxb], writes=[rwb])
            return rw, rwb

        def back(t):
            xt, xb = xts[t % 2]
            scr, scrb = scrs[t % 2]
            hm, hmb = hms[t % 2]
            idx, idxb = idxs[t % 2]
            gate, gateb = gts[t % 2]
            for hk in range(128):
                rw, rwb = gather(u_tab, idx, idxb, hk)
                s.op("dve", lambda e: e.scalar_tensor_tensor(out=junk[:], in0=rw[:], scalar=1.0, in1=hm[:], op0=ALU.mult, op1=ALU.mult,
                                                             accum_out=actv[:, hk:hk + 1]), reads=[rwb, hmb], writes=[actb[hk % 16]])
            s.op("act", lambda e: e.activation(out=wgt[:], in_=actv[:], func=AF.Gelu), reads=actb, writes=[wgtb])
            s.op("dve", lambda e: e.tensor_tensor(out=wgt[:], in0=wgt[:], in1=gate[:].rearrange("p h k -> p (h k)"), op=ALU.mult),
                 reads=[wgtb, gateb], writes=[wgtb])
            for hk in range(128):
                rw, rwb = gather(v_tab, idx, idxb, hk)
                ac, acb = accs[hk % 4]
                if hk < 4:
                    s.op("dve", lambda e: e.tensor_scalar(out=ac[:], in0=rw[:], scalar1=wgt[:, hk:hk + 1], scalar2=None, op0=ALU.mult),
                         reads=[rwb, wgtb], writes=[acb])
                else:
                    s.op("dve", lambda e: e.scalar_tensor_tensor(out=ac[:], in0=rw[:], scalar=wgt[:, hk:hk + 1], in1=ac[:], op0=ALU.mult, op1=ALU.add),
                         reads=[rwb, wgtb, acb], writes=[acb])
            (a0, a0b), (a1, a1b), (a2, a2b), (a3, a3b) = accs
            s.op("pool", lambda e: e.tensor_tensor(out=a0[:], in0=a0[:], in1=a1[:], op=ALU.add), reads=[a0b, a1b], writes=[a0b])
            s.op("pool", lambda e: e.tensor_tensor(out=a2[:], in0=a2[:], in1=a3[:], op=ALU.add), reads=[a2b, a3b], writes=[a2b])
            s.op("pool", lambda e: e.tensor_tensor(out=a0[:], in0=a0[:], in1=a2[:], op=ALU.add), reads=[a0b, a2b], writes=[a0b])
            s.op("dve", lambda e: e.tensor_tensor(out=scr[:], in0=a0[:], in1=ABG[:, 2, :], op=ALU.mult), reads=[a0b, ABGb], writes=[scrb])
            s.op("pool", lambda e: e.tensor_tensor(out=ot[:], in0=scr[:], in1=xt[:], op=ALU.add), reads=[scrb, xb], writes=[otb])
            s.dma("sp", lambda e: e.dma_start(out=hout[t * 128:(t + 1) * 128, :], in_=ot[:]), reads=[otb], writes=[houtb[t]])

        front(0)
        for t in range(NT):
            if t + 1 < NT:
                front(t + 1)
            back(t)
        s.barrier()


def stage_conv(k, hin, hinb, modv_l, W, hout, houtb):
    nc, s = k.nc, k.s
    banks = k.banks
    PADW = SEQ + 30
    with ExitStack() as es:
        cbuf, cbufb = k.sb(es, "cv_cbuf", [128, NT, 1024], F32)
        ABG, ABGb = k.sb(es, "cv_ABG", [128, 2, 1024], F32)
        st, stb = k.sb(es, "cv_st", [128, 8], F32)
        xts = [k.sb(es, f"cv_x{i}", [128, 1024], F32) for i in range(2)]
        scrs = [k.sb(es, f"cv_scr{i}", [128, 1024], F32) for i in range(2)]
        for j in range(2):
            load_bcast(k, ABG[:, j, :], ABGb, modv_l[0:1, j, :], 1024)
        cbt = [Buf() for _ in range(NT // 4)]
        with ExitStack() as es1:
            aTa, aTab = k.sb(es1, "cv_aT", [128, 8, SEQ], BF16)
            ubfs = [k.sb(es1, f"cv_ub{i}", [128, PADW], BF16) for i in range(2)]
            dgt, dgtb = k.sb(es1, "cv_dgt", [128, 12, 128], BF16)
            w1, w1b = k.sb(es1, "cv_w1", [128, 8, 2048], BF16)
            b1T, b1Tb = k.sb(es1, "cv_b1T", [128, 16], F32)
            wdw, wdwb = k.sb(es1, "cv_wdw", [128, 8, 31], F32)
            bdw, bdwb = k.sb(es1, "cv_bdw", [128, 8], F32)
            abfs = [k.sb(es1, f"cv_a{i}", [128, 1024], BF16) for i in range(1)]
            upads = [k.sb(es1, f"cv_up{i}", [128, PADW], F32) for i in range(2)]
            accs = [k.sb(es1, f"cv_acc{i}", [128, SEQ], F32) for i in range(2)]
            sgs = [k.sb(es1, f"cv_sg{i}", [128, 512], F32) for i in range(2)]
            w1v = W["conv_w_pw1"].rearrange("(kc p) n -> p kc n", p=128)
            for c in range(8):
                s.dma("pool", lambda e: e.dma_start(out=w1[:, c, :], in_=w1v[:, c, :]), writes=[w1b])
            s.dma("sp", lambda e: e.dma_start(out=b1T[:], in_=W["conv_b1T"]), writes=[b1Tb])
            s.dma("sp", lambda e: e.dma_start(out=wdw[:], in_=W["conv_wdwT"]), writes=[wdwb])
            s.dma("sp", lambda e: e.dma_start(out=bdw[:], in_=W["conv_bdwT"]), writes=[bdwb])
            for up, upb in upads + ubfs:
                s.op("pool", lambda e: e.memset(up[:, 0:15], 0.0), writes=[upb])
                s.op("pool", lambda e: e.memset(up[:, 15 + SEQ:PADW], 0.0), writes=[upb])
            for t in range(NT):
                xt, xb = xts[t % 2]
                scr, scrb = scrs[t % 2]
                abf, abfb = abfs[0]
                s.dma("sp", lambda e: e.dma_start(out=xt[:], in_=hin[t * 128:(t + 1) * 128, :]), reads=[hinb[t]], writes=[xb])
                norm_mod(k, xt, xb, ABG[:, 0, :], ABG[:, 1, :], ABGb, scr, scrb, st, stb, abf, abfb)
                bank, bb = banks[6 + t % 2]
                transpose_chunks(k, bank, bb, lambda c: abf[:, c * 128:(c + 1) * 128], 8, 128, aTa[:, :, t * 128:(t + 1) * 128], aTab, abfb, dst_view=True)
            accbs = [[Buf() for _ in range(4)] for _ in range(2)]
            NPE = 12
            tapbanks = [banks[2], banks[3], banks[6], banks[7]]

            def pw1_gen(m):
                up, upb = upads[m % 2]
                ub, ubb = ubfs[m % 2]
                for j in range(NPE):
                    s.op("act", lambda e: e.activation(out=dgt[:, j, :], in_=k.ident[:], func=AF.Copy, scale=wdw[:, m, j:j + 1]),
                         reads=[k.identb, wdwb], writes=[dgtb])
                for tg in range(4):
                    (bv_, bvb), (bg_, bgb) = banks[0], banks[1]
                    sg, sgb = sgs[tg % 2]
                    for (bk, bkb, c0) in ((bv_, bvb, m * 128), (bg_, bgb, 1024 + m * 128)):
                        for c in range(8):
                            s.op("pe", lambda e: e.matmul(bk[:, :], lhsT=w1[:, c, c0:c0 + 128], rhs=aTa[:, c, tg * 512:(tg + 1) * 512], start=(c == 0), stop=(c == 7)),
                                 reads=[w1b, aTab], writes=[bkb])
                    s.op("act", lambda e: e.activation(out=sg[:], in_=bg_[:, :], func=AF.Sigmoid, bias=b1T[:, 8 + m:9 + m]), reads=[bgb, b1Tb], writes=[sgb])
                    s.op("dve", lambda e: e.scalar_tensor_tensor(out=up[:, 15 + tg * 512:15 + (tg + 1) * 512], in0=bv_[:, :], scalar=b1T[:, m:m + 1], in1=sg[:],
                                                                 op0=ALU.add, op1=ALU.mult), reads=[bvb, sgb, b1Tb], writes=[upb])
                    if tg == 3:
                        s.op("act", lambda e: e.copy(out=ub[:, 15:15 + SEQ], in_=up[:, 15:15 + SEQ]), reads=[upb], writes=[ubb])
                    yield

            def taps_pe(m):
                ub, ubb = ubfs[m % 2]
                for ch in range(4):
                    tbk, tbkb = tapbanks[ch]
                    for j in range(NPE):
                        s.op("pe", lambda e: e.matmul(tbk[:, :], lhsT=dgt[:, j, :], rhs=ub[:, ch * 512 + j:ch * 512 + j + 512], start=(j == 0), stop=(j == NPE - 1)),
                             reads=[dgtb, ubb], writes=[tbkb])

            def taps_dve_gen(m):
                up, upb = upads[m % 2]
                acc, _ = accs[m % 2]
                accb = accbs[m % 2]
                for j in range(NPE, 31):
                    for ch in range(4):
                        src = up[:, ch * 512 + j:ch * 512 + j + 512]
                        dst = acc[:, ch * 512:(ch + 1) * 512]
                        if j == NPE:
                            s.op("dve", lambda e: e.tensor_scalar(out=dst, in0=src, scalar1=wdw[:, m, j:j + 1], scalar2=bdw[:, m:m + 1], op0=ALU.mult, op1=ALU.add),
                                 reads=[upb, wdwb, bdwb], writes=[accb[ch]])
                        else:
                            s.op("dve", lambda e: e.scalar_tensor_tensor(out=dst, in0=src, scalar=wdw[:, m, j:j + 1], in1=dst, op0=ALU.mult, op1=ALU.add),
                                 reads=[upb, wdwb, accb[ch]], writes=[accb[ch]])
                    if (j - NPE) % 5 == 4:
                        yield

            def finish(m):
                acc, _ = accs[m % 2]
                accb = accbs[m % 2]
                for ch in range(4):
                    tbk, tbkb = tapbanks[ch]
                    dst = acc[:, ch * 512:(ch + 1) * 512]
                    s.op("dve", lambda e: e.tensor_tensor(out=dst, in0=dst, in1=tbk[:, :], op=ALU.add), reads=[accb[ch], tbkb], writes=[accb[ch]])
                for g in range(NT // 4):
                    tb_, tbb = banks[4 + g % 2]
                    for j in range(4):
                        t = g * 4 + j
                        s.op("pe", lambda e: e.transpose(out=tb_[:, j * 128:(j + 1) * 128], in_=acc[:, t * 128:(t + 1) * 128], identity=k.identf[:]),
                             reads=[accb[t // 4], k.identb], writes=[tbb])
                    s.op("act", lambda e: e.copy(out=cbuf[:, g * 4:(g + 1) * 4, m * 128:(m + 1) * 128], in_=tb_[:, :].rearrange("p (a b) -> p a b", b=128)),
                         reads=[tbb], writes=[cbt[g]])

            for _ in pw1_gen(0):
                pass
            for m in range(8):
                taps_pe(m)
                gn = pw1_gen(m + 1) if m + 1 < 8 else None
                for _ in taps_dve_gen(m):
                    if gn is not None:
                        next(gn, None)
                if gn is not None:
                    for _ in gn:
                        pass
                finish(m)
            s.barrier()
        with ExitStack() as es3:
            stg = [k.sb(es3, f"cv3_stg{i}", [128, 1024], F32) for i in range(2)]
            w2, w2b = k.sb(es3, "cv3_w2", [128, 8, 1024], BF16)
            gl, glb = k.sb(es3, "cv3_gl", [128, 4, 1024], F32)
            sbfs = [k.sb(es3, f"cv3_s{i}", [128, 1024], BF16) for i in range(2)]
            sTs = [k.sb(es3, f"cv3_sT{i}", [128, 8, 128], BF16) for i in range(2)]
            outs = [k.sb(es3, f"cv3_o{i}", [128, 1024], F32) for i in range(2)]
            wv = W["conv_w_pw2"].rearrange("(kc p) n -> p kc n", p=128)
            for c in range(8):
                load_cast(k, stg, w2[:, c, :], w2b, wv[:, c, :], 1024, c)
            load_bcast(k, gl[:, 0, :], glb, W["conv_g_ln"], 1024)
            load_bcast(k, gl[:, 1, :], glb, W["conv_b_ln"], 1024)
            load_bcast(k, gl[:, 2, :], glb, W["conv_b_pw2"], 1024)
            load_bcast(k, gl[:, 3, :], glb, modv_l[0:1, 2, :], 1024)
            for t in range(NT):
                xt, xb = xts[t % 2]
                scr, scrb = scrs[t % 2]
                sbf, sbfb = sbfs[t % 2]
                sT, sTb = sTs[t % 2]
                ot, otb = outs[t % 2]
                cb = cbt[t // 4]
                c_t = cbuf[:, t, :]
                s.dma("sp", lambda e: e.dma_start(out=xt[:], in_=hin[t * 128:(t + 1) * 128, :]), reads=[hinb[t]], writes=[xb])
                s.op("act", lambda e: e.activation(out=scr[:], in_=c_t, func=AF.Identity, accum_out=st[:, 0:1]), reads=[cb], writes=[scrb, stb])
                s.op("act", lambda e: e.activation(out=scr[:], in_=c_t, func=AF.Square, accum_out=st[:, 1:2]), reads=[cb], writes=[scrb, stb])
                s.op("dve", lambda e: e.tensor_scalar(out=st[:, 2:3], in0=st[:, 0:1], scalar1=1.0 / D, scalar2=None, op0=ALU.mult), reads=[stb], writes=[stb])
                s.op("dve", lambda e: e.scalar_tensor_tensor(out=st[:, 3:4], in0=st[:, 2:3], scalar=-1.0, in1=st[:, 2:3], op0=ALU.mult, op1=ALU.mult),
                     reads=[stb], writes=[stb])
                s.op("dve", lambda e: e.scalar_tensor_tensor(out=st[:, 4:5], in0=st[:, 1:2], scalar=1.0 / D, in1=st[:, 3:4], op0=ALU.mult, op1=ALU.add),
                     reads=[stb], writes=[stb])
                s.op("act", lambda e: e.activation(out=st[:, 5:6], in_=st[:, 4:5], func=AF.Sqrt, scale=1.0, bias=k.eps[:, 0:1]), reads=[stb], writes=[stb])
                s.op("dve", lambda e: e.reciprocal(out=st[:, 5:6], in_=st[:, 5:6]), reads=[stb], writes=[stb])
                s.op("dve", lambda e: e.tensor_scalar(out=scr[:], in0=c_t, scalar1=st[:, 2:3], scalar2=st[:, 5:6], op0=ALU.subtract, op1=ALU.mult),
                     reads=[cb, stb], writes=[scrb])
                s.op("dve", lambda e: e.tensor_tensor(out=scr[:], in0=scr[:], in1=gl[:, 0, :], op=ALU.mult), reads=[scrb, glb], writes=[scrb])
                s.op("pool", lambda e: e.tensor_tensor(out=scr[:], in0=scr[:], in1=gl[:, 1, :], op=ALU.add), reads=[scrb, glb], writes=[scrb])
                s.op("act", lambda e: e.activation(out=sbf[:], in_=scr[:], func=AF.Silu), reads=[scrb], writes=[sbfb])
                bank, bb = banks[7]
                transpose_chunks(k, bank, bb, lambda c: sbf[:, c * 128:(c + 1) * 128], 8, 128, sT[:], sTb, sbfb, dst_view=True)
                for g in range(2):
                    yb, ybb = banks[g]
                    for c in range(8):
                        s.op("pe", lambda e: e.matmul(yb[:, :], lhsT=sT[:, c, :], rhs=w2[:, c, g * 512:(g + 1) * 512], start=(c == 0), stop=(c == 7)),
                             reads=[sTb, w2b], writes=[ybb])
                    s.op("dve", lambda e: e.tensor_tensor(out=scr[:, g * 512:(g + 1) * 512], in0=yb[:, :], in1=gl[:, 2, g * 512:(g + 1) * 512], op=ALU.add),
                         reads=[ybb, glb], writes=[scrb])
                s.op("pool", lambda e: e.tensor_tensor(out=scr[:], in0=scr[:], in1=gl[:, 3, :], op=ALU.mult), reads=[scrb, glb], writes=[scrb])
                s.op("pool", lambda e: e.tensor_tensor(out=ot[:], in0=scr[:], in1=xt[:], op=ALU.add), reads=[scrb, xb], writes=[otb])
                s.dma("sp", lambda e: e.dma_start(out=hout[t * 128:(t + 1) * 128, :], in_=ot[:]), reads=[otb], writes=[houtb[t]])
            s.barrier()


IN_SPECS = {
    "x": ([SEQ, D], F32), "ctx": ([CTX, D], F32), "cc": ([128, 16], F32),
    "w_ada": ([2, D, 6 * D], F32), "b_ada": ([2, 6 * D], F32), "g_norm": ([4, D], F32),
    "ident": ([128, 128], BF16), "identf": ([128, 128], F32),
    "attn_w_in": ([D, 2208], F32), "mla_w_q_up": ([384, 768], F32), "mla_w_kv_up": ([256, 1024], F32),
    "mla_g_qa": ([1, 384], F32), "mla_g_kva": ([1, 256], F32), "mla_g_q": ([1, 96], F32), "mla_g_k": ([1, 96], F32),
    "na_g_q": ([1, 64], F32), "na_g_k": ([1, 64], F32), "attn_w_out": ([D, D], F32),
    "rope": ([SEQ, 32], F32), "nabias": ([8, 128, 21, 128], F32),
    "conv_w_pw1": ([D, 2 * D], F32), "conv_b1T": ([128, 16], F32), "conv_wdwT": ([128, 8, 31], F32), "conv_bdwT": ([128, 8], F32),
    "conv_g_ln": ([1, D], F32), "conv_b_ln": ([1, D], F32), "conv_w_pw2": ([D, D], F32), "conv_b_pw2": ([1, D], F32),
    "wq0": ([D, 2048], F32), "wq1": ([D, 2048], F32), "skT0": ([128, 16, 128], F32), "skT1": ([128, 16, 128], F32),
    "u0": ([16384, D], F32), "u1": ([16384, D], F32), "v0": ([16384, D], F32), "v1": ([16384, D], F32),
}


def build_program():
    nc = bass.Bass("TRN2", target_bir_lowering=False)
    A = {n: nc.dram_tensor(n, sh, dt, kind="ExternalInput").ap() for n, (sh, dt) in IN_SPECS.items()}
    out = nc.dram_tensor("out", [SEQ, D], F32, kind="ExternalOutput").ap()
    modv = nc.dram_tensor("modv_scr", [2, 2, 6, D], F32, kind="Internal").ap()
    hs = [nc.dram_tensor(f"h_scr{i}", [SEQ, D], F32, kind="Internal").ap() for i in range(3)]
    hb = [[Buf() for _ in range(NT)] for _ in range(4)]
    uvs = [nc.dram_tensor(f"uv_scr{l}", [16384, 2 * D], BF16, kind="Internal").ap() for l in range(2)]
    with ExitStack() as es:
        k = K(nc, es)
        k.modv_buf = Buf()
        setup_consts(k, es, A["ident"], A["identf"])
        uvb = [[], []]
        k.bg = uv_cast_gen(k, [(A[f"u{l}"], A[f"v{l}"], uvs[l], uvb[l]) for l in range(2)])
        stage_ada(k, A["cc"], A["w_ada"], A["b_ada"], A["g_norm"], modv)
        stage_attn(k, A["x"], A["ctx"], modv[0], A, hs[0], hb[0])
        for _ in k.bg:
            pass
        stage_peer3(k, hs[0], hb[0], modv[0], A["wq0"], A["skT0"], uvs[0], uvb[0], hs[1], hb[1], "pra")
        stage_conv(k, hs[1], hb[1], modv[1], A, hs[2], hb[2])
        stage_peer3(k, hs[2], hb[2], modv[1], A["wq1"], A["skT1"], uvs[1], uvb[1], out, hb[3], "prb")
        k.s.barrier()
    return nc


def kernel(**inp):
    import ml_dtypes
    f = lambda a: np.ascontiguousarray(np.asarray(a, dtype=np.float32))
    nb = inp["x"].shape[0]
    shared = {
        "w_ada": f(inp["w_ada"]), "b_ada": f(inp["b_ada"]),
        "g_norm": f(np.stack([inp["g_norm1"][0], inp["g_norm2"][0], inp["g_norm1"][1], inp["g_norm2"][1]])),
        "ident": np.eye(128).astype(ml_dtypes.bfloat16), "identf": np.eye(128, dtype=np.float32),
        "attn_w_in": f(inp["attn_w_in"][0]), "mla_w_q_up": f(inp["mla_w_q_up"][0]), "mla_w_kv_up": f(inp["mla_w_kv_up"][0]),
        "mla_g_qa": f(inp["mla_g_qa"]), "mla_g_kva": f(inp["mla_g_kva"]), "mla_g_q": f(inp["mla_g_q"]), "mla_g_k": f(inp["mla_g_k"]),
        "na_g_q": f(inp["na_g_q"]), "na_g_k": f(inp["na_g_k"]), "attn_w_out": f(inp["attn_w_out"][0]),
        "rope": host_rope_table(), "nabias": host_na_bias(np.asarray(inp["na_rpb"][0], np.float32)),
        "conv_w_pw1": f(inp["conv_w_pw1"][0]), "conv_b1T": f(np.asarray(inp["conv_b_pw1"][0]).reshape(16, 128).T),
        "conv_wdwT": f(np.asarray(inp["conv_w_dw"][0]).reshape(31, 8, 128).transpose(2, 1, 0)),
        "conv_bdwT": f(np.asarray(inp["conv_b_dw"][0]).reshape(8, 128).T),
        "conv_g_ln": f(inp["conv_g_ln"]), "conv_b_ln": f(inp["conv_b_ln"]), "conv_w_pw2": f(inp["conv_w_pw2"][0]), "conv_b_pw2": f(inp["conv_b_pw2"]),
    }
    for l in range(2):
        shared[f"wq{l}"] = f(inp["peer_w_query"][l])
        shared[f"skT{l}"] = f(np.asarray(inp["peer_sub_keys"][l]).reshape(16, 128, 128).transpose(2, 0, 1))
        shared[f"u{l}"] = f(inp["peer_u"][l])
        shared[f"v{l}"] = f(inp["peer_v"][l])
    in_maps = []
    for b in range(nb):
        cc = np.zeros((128, 16), np.float32)
        cc[:, 0::2] = np.asarray(inp["c"][b], np.float32).reshape(8, 128).T
        cc[:, 1::2] = np.asarray(inp["c_ctx"], np.float32).reshape(8, 128).T
        m = dict(shared)
        m["x"] = f(inp["x"][b])
        m["ctx"] = f(inp["ctx"][b])
        m["cc"] = cc
        in_maps.append(m)
    nc = build_program()
    res = run_bass_kernel_spmd(nc, in_maps, core_ids=list(range(nb)))
    return np.stack([np.asarray(r["out"], dtype=np.float32) for r in res.results], axis=0)


def uv_cast_gen(k, tabs):
    PIECE = 1024
    for (u_tab, v_tab, uv, uvb) in tabs:
        for r0 in range(0, 16384, PIECE):
            r1 = r0 + PIECE
            b0, b1 = Buf(), Buf()
            k.s.dma("pool", lambda e: e.dma_start(out=uv[r0:r1, 0:1024], in_=u_tab[r0:r1, :]), writes=[b0])
            uvb.append(b0)
            yield
            k.s.dma("pool", lambda e: e.dma_start(out=uv[r0:r1, 1024:2048], in_=v_tab[r0:r1, :]), writes=[b1])
            uvb.append(b1)
            yield


def issue_uv_cast(k, es, u_tab, v_tab, uv, uvb):
    for i in range(4):
        r0, r1 = i * 4096, (i + 1) * 4096
        b0, b1 = Buf(), Buf()
        k.s.bulk_dma("pool", lambda e: e.dma_start(out=uv[r0:r1, 0:1024], in_=u_tab[r0:r1, :]), writes=[b0], es=es)
        k.s.bulk_dma("pool", lambda e: e.dma_start(out=uv[r0:r1, 1024:2048], in_=v_tab[r0:r1, :]), writes=[b1], es=es)
        uvb.extend([b0, b1])


NROW2 = 16


def stage_peer2(k, hin, hinb, modv_l, w_query, skT_d, uv, uvb, hout, houtb, tag):
    nc, s = k.nc, k.s
    banks = k.banks
    with ExitStack() as es:
        wq, wqb = k.sb(es, f"{tag}_wq", [128, 8, 2048], BF16)
        skT, skTb = k.sb(es, f"{tag}_skT", [128, 16, 128], BF16)
        ABG, ABGb = k.sb(es, f"{tag}_ABG", [128, 3, 1024], F32)
        iota_i, iota_ib = k.sb(es, f"{tag}_iotai", [128, 16], I32)
        iota, iotab = k.sb(es, f"{tag}_iota", [128, 16], F32)
        st, stb = k.sb(es, f"{tag}_st", [128, 4], F32)
        xts = [k.sb(es, f"{tag}_x{i}", [128, 1024], F32) for i in range(2)]
        scrs = [k.sb(es, f"{tag}_scr{i}", [128, 1024], F32) for i in range(2)]
        hbfs = [k.sb(es, f"{tag}_hbf{i}", [128, 1024], BF16) for i in range(1)]
        hms = [k.sb(es, f"{tag}_hm{i}", [128, 1024], F32) for i in range(2)]
        hT, hTb = k.sb(es, f"{tag}_hT", [128, 8, 128], BF16)
        qbf, qbfb = k.sb(es, f"{tag}_qbf", [128, 2048], BF16)
        qT, qTb = k.sb(es, f"{tag}_qT", [128, 16, 128], BF16)
        ssb, ssbb = k.sb(es, f"{tag}_s", [128, 16, 128], F32)
        s2, _ = k.sb(es, f"{tag}_s2", [128, 16, 128], F32)
        m16, _ = k.sb(es, f"{tag}_m16", [128, 16, 16], F32)
        i16, _ = k.sb(es, f"{tag}_i16", [128, 16, 16], U32)
        i16f, i16fb = k.sb(es, f"{tag}_i16f", [128, 16, 16], F32)
        cand, candb = k.sb(es, f"{tag}_cand", [128, 8, 256], F32)
        cand2, _ = k.sb(es, f"{tag}_cand2", [128, 8, 256], F32)
        best, _ = k.sb(es, f"{tag}_best", [128, 8, 16], F32)
        pos, _ = k.sb(es, f"{tag}_pos", [128, 8, 16], U32)
        ab_i, ab_ib = k.sb(es, f"{tag}_abi", [128, 2, 128], I32)
        ab_f, ab_fb = k.sb(es, f"{tag}_abf", [128, 2, 128], F32)
        oh, ohb = k.sb(es, f"{tag}_oh", [128, 8, 16, 16], F32)
        e01, e01b = k.sb(es, f"{tag}_e01", [128, 2, 128], F32)
        idxs = [k.sb(es, f"{tag}_idx{i}", [128, 128], I32) for i in range(2)]
        gts = [k.sb(es, f"{tag}_gate{i}", [128, 8, 16], F32) for i in range(2)]
        gsum, gsumb = k.sb(es, f"{tag}_gsum", [128, 8], F32)
        actv, _ = k.sb(es, f"{tag}_act", [128, 128], F32)
        wgt, _ = k.sb(es, f"{tag}_wgt", [128, 128], F32)
        junk, _ = k.sb(es, f"{tag}_junk", [128, 1024], BF16)
        rows = [k.sb(es, f"{tag}_row{i}", [128, 2048], BF16) for i in range(NROW2)]
        dgs = [k.sb(es, f"{tag}_dg{i}", [128, 128], BF16) for i in range(4)]
        ot, otb = k.sb(es, f"{tag}_ot", [128, 1024], F32)
        hpb = [[Buf() for _ in range(16)] for _ in range(3)]
        hb = [[Buf() for _ in range(8)] for _ in range(3)]
        actb = [Buf() for _ in range(16)]
        wgb = [Buf() for _ in range(4)]

        wqv = w_query.rearrange("(kc p) n -> p kc n", p=128)
        for c in range(8):
            s.dma("pool", lambda e: e.dma_start(out=wq[:, c, :], in_=wqv[:, c, :]), writes=[wqb])
        s.dma("pool", lambda e: e.dma_start(out=skT[:].rearrange("p a b -> p (a b)"), in_=skT_d.rearrange("p a b -> p (a b)")), writes=[skTb])
        for j in range(3):
            load_bcast(k, ABG[:, j, :], ABGb, modv_l[0:1, 3 + j, :], 1024)
        s.op("pool", lambda e: e.iota(out=iota_i[:], pattern=[[1, 16]], base=0, channel_multiplier=0), writes=[iota_ib])
        s.op("dve", lambda e: e.tensor_copy(out=iota[:], in_=iota_i[:]), reads=[iota_ib], writes=[iotab])

        def front(t):
            xt, xb = xts[t % 2]
            scr, scrb = scrs[t % 2]
            idx, idxb = idxs[t % 2]
            gate, gateb = gts[t % 2]
            hbf, hbfb = hbfs[0]
            hm, hmb = hms[t % 2]
            s.dma("sp", lambda e: e.dma_start(out=xt[:], in_=hin[t * 128:(t + 1) * 128, :]), reads=[hinb[t]], writes=[xb])
            norm_mod(k, xt, xb, ABG[:, 0, :], ABG[:, 1, :], ABGb, scr, scrb, st, stb, hbf, hbfb, out_f32=(hm, hmb))
            bank, bb = banks[4]
            transpose_chunks(k, bank, bb, lambda c: hbf[:, c * 128:(c + 1) * 128], 8, 128, hT[:], hTb, hbfb, dst_view=True)
            yield
            for g in range(4):
                qb_, qbb = banks[g]
                for c in range(8):
                    s.op("pe", lambda e: e.matmul(qb_[:, :], lhsT=hT[:, c, :], rhs=wq[:, c, g * 512:(g + 1) * 512], start=(c == 0), stop=(c == 7)),
                         reads=[hTb, wqb], writes=[qbb])
                s.op("act", lambda e: e.copy(out=qbf[:, g * 512:(g + 1) * 512], in_=qb_[:, :]), reads=[qbb], writes=[qbfb])
            yield
            for half in range(2):
                tb_, tbb = banks[4 + half]
                transpose_chunks(k, tb_, tbb, lambda c: qbf[:, (half * 8 + c) * 128:(half * 8 + c + 1) * 128], 8, 128,
                                 qT[:, half * 8:(half + 1) * 8, :], qTb, qbfb, dst_view=True)
            for g in range(4):
                sb_, sbb = banks[g]
                for j in range(4):
                    hp = g * 4 + j
                    s.op("pe", lambda e: e.matmul(sb_[:, j * 128:(j + 1) * 128], lhsT=qT[:, hp, :], rhs=skT[:, hp, :], start=True, stop=True),
                         reads=[qTb, skTb], writes=[sbb])
                s.op("act", lambda e: e.copy(out=ssb[:, g * 4:(g + 1) * 4, :], in_=sb_[:, :].rearrange("p (a b) -> p a b", b=128)), reads=[sbb], writes=[ssbb])
            yield
            for hp in range(16):
                s.op("dve", lambda e: e.max(out=m16[:, hp, 0:8], in_=ssb[:, hp, :]), reads=[ssbb], writes=[hpb[0][hp]])
            yield
            for hp in range(16):
                s.op("dve", lambda e: e.max_index(out=i16[:, hp, 0:8], in_max=m16[:, hp, 0:8], in_values=ssb[:, hp, :]),
                     reads=[ssbb, hpb[0][hp]], writes=[hpb[1][hp]])
            yield
            for hp in range(16):
                s.op("dve", lambda e: e.match_replace(out=s2[:, hp, :], in_to_replace=m16[:, hp, 0:8], in_values=ssb[:, hp, :], imm_value=-1e30),
                     reads=[ssbb, hpb[0][hp]], writes=[hpb[2][hp]])
            yield
            for hp in range(16):
                s.op("dve", lambda e: e.max(out=m16[:, hp, 8:16], in_=s2[:, hp, :]), reads=[hpb[2][hp]], writes=[hpb[0][hp]])
            yield
            for hp in range(16):
                s.op("dve", lambda e: e.max_index(out=i16[:, hp, 8:16], in_max=m16[:, hp, 8:16], in_values=s2[:, hp, :]),
                     reads=[hpb[2][hp], hpb[0][hp]], writes=[hpb[1][hp]])
            yield
            s.op("dve", lambda e: e.tensor_copy(out=i16f[:], in_=i16[:]), reads=hpb[1], writes=[i16fb])
            m4 = m16[:].rearrange("p (h t) a -> p h t a", t=2)
            s.op("dve", lambda e: e.tensor_tensor(out=cand[:].rearrange("p h (a b) -> p h a b", b=16),
                                                  in0=m4[:, :, 0, :].unsqueeze(3).to_broadcast([128, 8, 16, 16]),
                                                  in1=m4[:, :, 1, :].unsqueeze(2).to_broadcast([128, 8, 16, 16]), op=ALU.add),
                 reads=hpb[0], writes=[candb])
            yield
            for h in range(8):
                s.op("dve", lambda e: e.max(out=best[:, h, 0:8], in_=cand[:, h, :]), reads=[candb], writes=[hb[0][h]])
            for h in range(8):
                s.op("dve", lambda e: e.max_index(out=pos[:, h, 0:8], in_max=best[:, h, 0:8], in_values=cand[:, h, :]),
                     reads=[candb, hb[0][h]], writes=[hb[1][h]])
            yield
            for h in range(8):
                s.op("dve", lambda e: e.match_replace(out=cand2[:, h, :], in_to_replace=best[:, h, 0:8], in_values=cand[:, h, :], imm_value=-1e30),
                     reads=[candb, hb[0][h]], writes=[hb[2][h]])
            for h in range(8):
                s.op("dve", lambda e: e.max(out=best[:, h, 8:16], in_=cand2[:, h, :]), reads=[hb[2][h]], writes=[hb[0][h]])
            yield
            for h in range(8):
                s.op("dve", lambda e: e.max_index(out=pos[:, h, 8:16], in_max=best[:, h, 8:16], in_values=cand2[:, h, :]),
                     reads=[hb[2][h], hb[0][h]], writes=[hb[1][h]])
            posi = pos[:].rearrange("p h k -> p (h k)").bitcast(I32)
            s.op("dve", lambda e: e.tensor_single_scalar(out=ab_i[:, 0, :], in_=posi, scalar=4, op=ALU.arith_shift_right), reads=hb[1], writes=[ab_ib])
            s.op("dve", lambda e: e.tensor_single_scalar(out=ab_i[:, 1, :], in_=posi, scalar=15, op=ALU.bitwise_and), reads=hb[1], writes=[ab_ib])
            s.op("dve", lambda e: e.tensor_copy(out=ab_f[:], in_=ab_i[:]), reads=[ab_ib], writes=[ab_fb])
            yield
            i4 = i16f[:].rearrange("p (h t) a -> p h t a", t=2)
            for p_ in range(2):
                s.op("dve", lambda e: e.tensor_tensor(out=oh[:], in0=ab_f[:, p_, :].rearrange("p (h k) -> p h k", k=16).unsqueeze(3).to_broadcast([128, 8, 16, 16]),
                                                      in1=iota[:].unsqueeze(1).unsqueeze(1).to_broadcast([128, 8, 16, 16]), op=ALU.is_equal),
                     reads=[ab_fb, iotab], writes=[ohb])
                s.op("dve", lambda e: e.tensor_tensor(out=oh[:], in0=oh[:], in1=i4[:, :, p_, :].unsqueeze(2).to_broadcast([128, 8, 16, 16]), op=ALU.mult),
                     reads=[ohb, i16fb], writes=[ohb])
                s.op("dve", lambda e: e.tensor_reduce(out=e01[:, p_, :].rearrange("p (h k) -> p h k", k=16), in_=oh[:], axis=AX.X, op=ALU.add),
                     reads=[ohb], writes=[e01b])
                yield
            s.op("dve", lambda e: e.scalar_tensor_tensor(out=e01[:, 0, :], in0=e01[:, 0, :], scalar=128.0, in1=e01[:, 1, :], op0=ALU.mult, op1=ALU.add),
                 reads=[e01b], writes=[e01b])
            s.op("dve", lambda e: e.tensor_copy(out=idx[:], in_=e01[:, 0, :]), reads=[e01b], writes=[idxb])
            s.op("dve", lambda e: e.tensor_tensor(out=gate[:], in0=best[:], in1=best[:, :, 0:1].to_broadcast([128, 8, 16]), op=ALU.subtract),
                 reads=hb[0], writes=[gateb])
            s.op("act", lambda e: e.activation(out=gate[:], in_=gate[:], func=AF.Exp), reads=[gateb], writes=[gateb])
            s.op("dve", lambda e: e.tensor_reduce(out=gsum[:], in_=gate[:], axis=AX.X, op=ALU.add), reads=[gateb], writes=[gsumb])
            s.op("dve", lambda e: e.reciprocal(out=gsum[:], in_=gsum[:]), reads=[gsumb], writes=[gsumb])
            s.op("dve", lambda e: e.tensor_tensor(out=gate[:], in0=gate[:], in1=gsum[:].unsqueeze(2).to_broadcast([128, 8, 16]), op=ALU.mult),
                 reads=[gateb, gsumb], writes=[gateb])

        ring = [0]

        def back(t, fg):
            xt, xb = xts[t % 2]
            scr, scrb = scrs[t % 2]
            idx, idxb = idxs[t % 2]
            gate, gateb = gts[t % 2]
            hm, hmb = hms[t % 2]
            (o0, o0b), (o1, o1b) = banks[6], banks[7]
            for grp in range(16):
                held = []
                for j in range(8):
                    hk = grp * 8 + j
                    rw, rwb = rows[ring[0] % NROW2]
                    ring[0] += 1
                    s.dma("pool", lambda e: e.indirect_dma_start(out=rw[:], out_offset=None, in_=uv,
                                                                 in_offset=bass.IndirectOffsetOnAxis(ap=idx[:, hk:hk + 1], axis=0)),
                          reads=[idxb] + uvb, writes=[rwb])
                    s.op("dve", lambda e: e.scalar_tensor_tensor(out=junk[:], in0=rw[:, 0:1024], scalar=1.0, in1=hm[:], op0=ALU.mult, op1=ALU.mult,
                                                                 accum_out=actv[:, hk:hk + 1]), reads=[rwb, hmb], writes=[actb[hk % 16]])
                    held.append((hk, rw, rwb))
                g8 = slice(grp * 8, grp * 8 + 8)
                wb_ = wgb[grp % 4]
                s.op("act", lambda e: e.activation(out=wgt[:, g8], in_=actv[:, g8], func=AF.Gelu), reads=actb[(grp % 2) * 8:(grp % 2) * 8 + 8], writes=[wb_])
                s.op("dve", lambda e: e.tensor_tensor(out=wgt[:, g8], in0=wgt[:, g8], in1=gate[:].rearrange("p h k -> p (h k)")[:, g8], op=ALU.mult),
                     reads=[wb_, gateb], writes=[wb_])
                for (hk, rw, rwb) in held:
                    dg, dgb = dgs[hk % 4]
                    s.op("act", lambda e: e.activation(out=dg[:], in_=k.ident[:], func=AF.Copy, scale=wgt[:, hk:hk + 1]),
                         reads=[k.identb, wb_], writes=[dgb])
                    s.op("pe", lambda e: e.matmul(o0[:, :], lhsT=dg[:], rhs=rw[:, 1024:1536], start=(hk == 0), stop=(hk == 127)),
                         reads=[dgb, rwb], writes=[o0b])
                    s.op("pe", lambda e: e.matmul(o1[:, :], lhsT=dg[:], rhs=rw[:, 1536:2048], start=(hk == 0), stop=(hk == 127)),
                         reads=[dgb, rwb], writes=[o1b])
                if fg is not None:
                    next(fg, None)
            s.op("dve", lambda e: e.tensor_tensor(out=scr[:, 0:512], in0=o0[:, :], in1=ABG[:, 2, 0:512], op=ALU.mult), reads=[o0b, ABGb], writes=[scrb])
            s.op("dve", lambda e: e.tensor_tensor(out=scr[:, 512:1024], in0=o1[:, :], in1=ABG[:, 2, 512:1024], op=ALU.mult), reads=[o1b, ABGb], writes=[scrb])
            s.op("pool", lambda e: e.tensor_tensor(out=ot[:], in0=scr[:], in1=xt[:], op=ALU.add), reads=[scrb, xb], writes=[otb])
            s.dma("sp", lambda e: e.dma_start(out=hout[t * 128:(t + 1) * 128, :], in_=ot[:]), reads=[otb], writes=[houtb[t]])

        for _ in front(0):
            pass
        for t in range(NT):
            fg = front(t + 1) if t + 1 < NT else None
            back(t, fg)
            if fg is not None:
                for _ in fg:
                    pass
        s.barrier()
NROW3 = 16


def stage_peer3(k, hin, hinb, modv_l, w_query, skT_d, uv, uvb, hout, houtb, tag):
    nc, s = k.nc, k.s
    banks = k.banks
    with ExitStack() as es:
        wq, wqb = k.sb(es, f"{tag}_wq", [128, 8, 2048], BF16)
        skT, skTb = k.sb(es, f"{tag}_skT", [128, 16, 128], BF16)
        ABG, ABGb = k.sb(es, f"{tag}_ABG", [128, 3, 1024], F32)
        iota_i, iota_ib = k.sb(es, f"{tag}_iotai", [128, 16], I32)
        iota, iotab = k.sb(es, f"{tag}_iota", [128, 16], F32)
        st, stb = k.sb(es, f"{tag}_st", [128, 4], F32)
        xts = [k.sb(es, f"{tag}_x{i}", [128, 1024], F32) for i in range(2)]
        scrs = [k.sb(es, f"{tag}_scr{i}", [128, 1024], F32) for i in range(1)] * 2
        hbfs = [k.sb(es, f"{tag}_hbf{i}", [128, 1024], BF16) for i in range(1)]
        hT, hTb = k.sb(es, f"{tag}_hT", [128, 8, 128], BF16)
        qbf, qbfb = k.sb(es, f"{tag}_qbf", [128, 2048], BF16)
        qT, qTb = k.sb(es, f"{tag}_qT", [128, 16, 128], BF16)
        ssb, ssbb = k.sb(es, f"{tag}_s", [128, 16, 128], F32)
        s2, _ = k.sb(es, f"{tag}_s2", [128, 16, 128], F32)
        m16, _ = k.sb(es, f"{tag}_m16", [128, 16, 16], F32)
        i16, _ = k.sb(es, f"{tag}_i16", [128, 16, 16], U32)
        i16f, i16fb = k.sb(es, f"{tag}_i16f", [128, 16, 16], F32)
        cand, candb = k.sb(es, f"{tag}_cand", [128, 8, 256], F32)
        cand2, _ = k.sb(es, f"{tag}_cand2", [128, 8, 256], F32)
        best, _ = k.sb(es, f"{tag}_best", [128, 8, 16], F32)
        pos, _ = k.sb(es, f"{tag}_pos", [128, 8, 16], U32)
        ab_i, ab_ib = k.sb(es, f"{tag}_abi", [128, 2, 128], I32)
        ab_f, ab_fb = k.sb(es, f"{tag}_abf", [128, 2, 128], F32)
        oh, ohb = k.sb(es, f"{tag}_oh", [128, 8, 16, 16], F32)
        e01, e01b = k.sb(es, f"{tag}_e01", [128, 2, 128], F32)
        idxs = [k.sb(es, f"{tag}_idx{i}", [128, 128], I32) for i in range(2)]
        gts = [k.sb(es, f"{tag}_gate{i}", [128, 8, 16], F32) for i in range(2)]
        gsum, gsumb = k.sb(es, f"{tag}_gsum", [128, 8], F32)
        actv, _ = k.sb(es, f"{tag}_act", [128, 128], F32)
        wgt, _ = k.sb(es, f"{tag}_wgt", [128, 128], F32)
        junks = [k.sb(es, f"{tag}_junk{i}", [128, 1024], BF16) for i in range(3)]
        rows = [k.sb(es, f"{tag}_row{i}", [128, 2048], BF16) for i in range(NROW3)]
        dgs = [k.sb(es, f"{tag}_dg{i}", [128, 128], BF16) for i in range(4)]
        ot, otb = k.sb(es, f"{tag}_ot", [128, 1024], F32)
        hpb = [[Buf() for _ in range(16)] for _ in range(3)]
        hb = [[Buf() for _ in range(8)] for _ in range(3)]
        actb = [Buf() for _ in range(16)]
        wgb = [Buf() for _ in range(4)]

        wqv = w_query.rearrange("(kc p) n -> p kc n", p=128)
        for c in range(8):
            s.dma("pool", lambda e: e.dma_start(out=wq[:, c, :], in_=wqv[:, c, :]), writes=[wqb])
        s.dma("pool", lambda e: e.dma_start(out=skT[:].rearrange("p a b -> p (a b)"), in_=skT_d.rearrange("p a b -> p (a b)")), writes=[skTb])
        for j in range(3):
            load_bcast(k, ABG[:, j, :], ABGb, modv_l[0:1, 3 + j, :], 1024)
        s.op("pool", lambda e: e.iota(out=iota_i[:], pattern=[[1, 16]], base=0, channel_multiplier=0), writes=[iota_ib])
        s.op("dve", lambda e: e.tensor_copy(out=iota[:], in_=iota_i[:]), reads=[iota_ib], writes=[iotab])

        def front(t):
            xt, xb = xts[t % 2]
            scr, scrb = scrs[t % 2]
            idx, idxb = idxs[t % 2]
            gate, gateb = gts[t % 2]
            hbf, hbfb = hbfs[0]
            s.dma("sp", lambda e: e.dma_start(out=xt[:], in_=hin[t * 128:(t + 1) * 128, :]), reads=[hinb[t]], writes=[xb])
            yield
            s.op("act", lambda e: e.activation(out=scr[:], in_=xt[:], func=AF.Square, accum_out=st[:, 0:1]), reads=[xb], writes=[scrb, stb])
            s.op("act", lambda e: e.activation(out=st[:, 0:1], in_=st[:, 0:1], func=AF.Sqrt, scale=1.0 / D, bias=k.eps[:, 0:1]), reads=[stb], writes=[stb])
            yield
            s.op("dve", lambda e: e.reciprocal(out=st[:, 0:1], in_=st[:, 0:1]), reads=[stb], writes=[stb])
            s.op("dve", lambda e: e.scalar_tensor_tensor(out=scr[:], in0=xt[:], scalar=st[:, 0:1], in1=ABG[:, 0, :], op0=ALU.mult, op1=ALU.mult),
                 reads=[xb, stb, ABGb], writes=[scrb])
            yield
            s.op("pool", lambda e: e.tensor_tensor(out=hbf[:], in0=scr[:], in1=ABG[:, 1, :], op=ALU.add), reads=[scrb, ABGb], writes=[hbfb])
            yield
            hmp, hmpb = banks[4 + t % 2]
            bank, bb = banks[2]
            bv = bank[:].bitcast(BF16)
            for c in range(8):
                s.op("pe", lambda e: e.transpose(out=bv[:, c * 128:(c + 1) * 128], in_=hbf[:, c * 128:(c + 1) * 128], identity=k.ident[:]),
                     reads=[hbfb, k.identb], writes=[bb])
            yield
            s.op("act", lambda e: e.copy(out=hT[:], in_=bv[:, :].rearrange("p (c t) -> p c t", t=128)), reads=[bb], writes=[hTb])
            yield
            hmv_ = hmp[:].bitcast(BF16)
            for c in range(8):
                s.op("pe", lambda e: e.transpose(out=hmv_[:, c * 128:(c + 1) * 128], in_=hT[:, c, :], identity=k.ident[:]),
                     reads=[hTb, k.identb], writes=[hmpb])
            for g in range(5):
                if g < 4:
                    qb_, qbb = banks[g % 2]
                    for c in range(8):
                        s.op("pe", lambda e: e.matmul(qb_[:, :], lhsT=hT[:, c, :], rhs=wq[:, c, g * 512:(g + 1) * 512], start=(c == 0), stop=(c == 7)),
                             reads=[hTb, wqb], writes=[qbb])
                if g > 0:
                    g1 = g - 1
                    qb1, qbb1 = banks[g1 % 2]
                    s.op("act", lambda e: e.copy(out=qbf[:, g1 * 512:(g1 + 1) * 512], in_=qb1[:, :]), reads=[qbb1], writes=[qbfb])
                yield
            for half in range(3):
                if half < 2:
                    tb_, tbb = banks[2 + half]
                    bv2 = tb_[:].bitcast(BF16)
                    for c in range(8):
                        s.op("pe", lambda e: e.transpose(out=bv2[:, c * 128:(c + 1) * 128], in_=qbf[:, (half * 8 + c) * 128:(half * 8 + c + 1) * 128], identity=k.ident[:]),
                             reads=[qbfb, k.identb], writes=[tbb])
                if half > 0:
                    h1 = half - 1
                    tb1, tbb1 = banks[2 + h1]
                    s.op("act", lambda e: e.copy(out=qT[:, h1 * 8:(h1 + 1) * 8, :], in_=tb1[:].bitcast(BF16)[:, :].rearrange("p (c t) -> p c t", t=128)),
                         reads=[tbb1], writes=[qTb])
                yield
            for g in range(5):
                if g < 4:
                    sb_, sbb = banks[g % 2]
                    for j in range(4):
                        hp = g * 4 + j
                        s.op("pe", lambda e: e.matmul(sb_[:, j * 128:(j + 1) * 128], lhsT=qT[:, hp, :], rhs=skT[:, hp, :], start=True, stop=True),
                             reads=[qTb, skTb], writes=[sbb])
                if g > 0:
                    g1 = g - 1
                    sb1, sbb1 = banks[g1 % 2]
                    s.op("act", lambda e: e.copy(out=ssb[:, g1 * 4:(g1 + 1) * 4, :], in_=sb1[:, :].rearrange("p (a b) -> p a b", b=128)), reads=[sbb1], writes=[ssbb])
                if g % 2 == 1:
                    yield
            yield
            for hp in range(16):
                s.op("dve", lambda e: e.max(out=m16[:, hp, 0:8], in_=ssb[:, hp, :]), reads=[ssbb], writes=[hpb[0][hp]])
            yield
            for hp in range(16):
                s.op("dve", lambda e: e.max_index(out=i16[:, hp, 0:8], in_max=m16[:, hp, 0:8], in_values=ssb[:, hp, :]),
                     reads=[ssbb, hpb[0][hp]], writes=[hpb[1][hp]])
            yield
            for hp in range(16):
                s.op("dve", lambda e: e.match_replace(out=s2[:, hp, :], in_to_replace=m16[:, hp, 0:8], in_values=ssb[:, hp, :], imm_value=-1e30),
                     reads=[ssbb, hpb[0][hp]], writes=[hpb[2][hp]])
            yield
            for hp in range(16):
                s.op("dve", lambda e: e.max(out=m16[:, hp, 8:16], in_=s2[:, hp, :]), reads=[hpb[2][hp]], writes=[hpb[0][hp]])
            yield
            for hp in range(16):
                s.op("dve", lambda e: e.max_index(out=i16[:, hp, 8:16], in_max=m16[:, hp, 8:16], in_values=s2[:, hp, :]),
                     reads=[hpb[2][hp], hpb[0][hp]], writes=[hpb[1][hp]])
            yield
            s.op("dve", lambda e: e.tensor_copy(out=i16f[:], in_=i16[:]), reads=hpb[1], writes=[i16fb])
            m4 = m16[:].rearrange("p (h t) a -> p h t a", t=2)
            s.op("dve", lambda e: e.tensor_tensor(out=cand[:].rearrange("p h (a b) -> p h a b", b=16),
                                                  in0=m4[:, :, 0, :].unsqueeze(3).to_broadcast([128, 8, 16, 16]),
                                                  in1=m4[:, :, 1, :].unsqueeze(2).to_broadcast([128, 8, 16, 16]), op=ALU.add),
                 reads=hpb[0], writes=[candb])
            yield
            for h in range(8):
                s.op("dve", lambda e: e.max(out=best[:, h, 0:8], in_=cand[:, h, :]), reads=[candb], writes=[hb[0][h]])
            for h in range(8):
                s.op("dve", lambda e: e.max_index(out=pos[:, h, 0:8], in_max=best[:, h, 0:8], in_values=cand[:, h, :]),
                     reads=[candb, hb[0][h]], writes=[hb[1][h]])
            yield
            for h in range(8):
                s.op("dve", lambda e: e.match_replace(out=cand2[:, h, :], in_to_replace=best[:, h, 0:8], in_values=cand[:, h, :], imm_value=-1e30),
                     reads=[candb, hb[0][h]], writes=[hb[2][h]])
            for h in range(8):
                s.op("dve", lambda e: e.max(out=best[:, h, 8:16], in_=cand2[:, h, :]), reads=[hb[2][h]], writes=[hb[0][h]])
            yield
            for h in range(8):
                s.op("dve", lambda e: e.max_index(out=pos[:, h, 8:16], in_max=best[:, h, 8:16], in_values=cand2[:, h, :]),
                     reads=[hb[2][h], hb[0][h]], writes=[hb[1][h]])
            posi = pos[:].rearrange("p h k -> p (h k)").bitcast(I32)
            s.op("dve", lambda e: e.tensor_single_scalar(out=ab_i[:, 0, :], in_=posi, scalar=4, op=ALU.arith_shift_right), reads=hb[1], writes=[ab_ib])
            s.op("dve", lambda e: e.tensor_single_scalar(out=ab_i[:, 1, :], in_=posi, scalar=15, op=ALU.bitwise_and), reads=hb[1], writes=[ab_ib])
            s.op("dve", lambda e: e.tensor_copy(out=ab_f[:], in_=ab_i[:]), reads=[ab_ib], writes=[ab_fb])
            s.op("dve", lambda e: e.tensor_tensor(out=gate[:], in0=best[:], in1=best[:, :, 0:1].to_broadcast([128, 8, 16]), op=ALU.subtract),
                 reads=hb[0], writes=[gateb])
            yield
            s.op("act", lambda e: e.activation(out=gate[:], in_=gate[:], func=AF.Exp), reads=[gateb], writes=[gateb])
            i4 = i16f[:].rearrange("p (h t) a -> p h t a", t=2)
            for p_ in range(2):
                s.op("dve", lambda e: e.tensor_tensor(out=oh[:], in0=ab_f[:, p_, :].rearrange("p (h k) -> p h k", k=16).unsqueeze(3).to_broadcast([128, 8, 16, 16]),
                                                      in1=iota[:].unsqueeze(1).unsqueeze(1).to_broadcast([128, 8, 16, 16]), op=ALU.is_equal),
                     reads=[ab_fb, iotab], writes=[ohb])
                yield
                s.op("dve", lambda e: e.tensor_tensor(out=oh[:], in0=oh[:], in1=i4[:, :, p_, :].unsqueeze(2).to_broadcast([128, 8, 16, 16]), op=ALU.mult),
                     reads=[ohb, i16fb], writes=[ohb])
                yield
                s.op("dve", lambda e: e.tensor_reduce(out=e01[:, p_, :].rearrange("p (h k) -> p h k", k=16), in_=oh[:], axis=AX.X, op=ALU.add),
                     reads=[ohb], writes=[e01b])
                yield
            s.op("dve", lambda e: e.scalar_tensor_tensor(out=e01[:, 0, :], in0=e01[:, 0, :], scalar=128.0, in1=e01[:, 1, :], op0=ALU.mult, op1=ALU.add),
                 reads=[e01b], writes=[e01b])
            s.op("dve", lambda e: e.tensor_copy(out=idx[:], in_=e01[:, 0, :]), reads=[e01b], writes=[idxb])
            s.op("dve", lambda e: e.tensor_reduce(out=gsum[:], in_=gate[:], axis=AX.X, op=ALU.add), reads=[gateb], writes=[gsumb])
            s.op("dve", lambda e: e.reciprocal(out=gsum[:], in_=gsum[:]), reads=[gsumb], writes=[gsumb])
            s.op("dve", lambda e: e.tensor_tensor(out=gate[:], in0=gate[:], in1=gsum[:].unsqueeze(2).to_broadcast([128, 8, 16]), op=ALU.mult),
                 reads=[gateb, gsumb], writes=[gateb])

        ring = [0]

        glv, _ = k.sb(es, f"{tag}_glv", [128, 128], F32)
        glb = [Buf() for _ in range(16)]
        wgb16 = [Buf() for _ in range(16)]

        def back(t, fg):
            xt, xb = xts[t % 2]
            scr, scrb = scrs[t % 2]
            idx, idxb = idxs[t % 2]
            gate, gateb = gts[t % 2]
            hmp, hmpb = banks[4 + t % 2]
            hmv = hmp[:].bitcast(BF16)
            gflat = gate[:].rearrange("p h k -> p (h k)")
            (o0, o0b), (o1, o1b) = banks[6], banks[7]
            held = {}

            def st_a(hk):
                s.op("act", lambda e: e.activation(out=glv[:, hk:hk + 1], in_=actv[:, hk:hk + 1], func=AF.Gelu), reads=[actb[hk % 16]], writes=[glb[hk % 16]])

            def st_b(hk):
                rw, rwb = held.pop(hk)
                dg, dgb = dgs[hk % 4]
                s.op("act", lambda e: e.activation(out=wgt[:, hk:hk + 1], in_=glv[:, hk:hk + 1], func=AF.Copy, scale=gflat[:, hk:hk + 1]),
                     reads=[glb[hk % 16], gateb], writes=[wgb16[hk % 16]])
                s.op("act", lambda e: e.activation(out=dg[:], in_=k.ident[:], func=AF.Copy, scale=wgt[:, hk:hk + 1]),
                     reads=[k.identb, wgb16[hk % 16]], writes=[dgb])
                s.op("pe", lambda e: e.matmul(o0[:, :], lhsT=dg[:], rhs=rw[:, 1024:1536], start=(hk == 0), stop=(hk == 127)),
                     reads=[dgb, rwb], writes=[o0b])
                s.op("pe", lambda e: e.matmul(o1[:, :], lhsT=dg[:], rhs=rw[:, 1536:2048], start=(hk == 0), stop=(hk == 127)),
                     reads=[dgb, rwb], writes=[o1b])

            for hk in range(128 + 3):
                if hk < 128:
                    rw, rwb = rows[ring[0] % NROW3]
                    ring[0] += 1
                    s.dma("pool", lambda e: e.indirect_dma_start(out=rw[:], out_offset=None, in_=uv,
                                                                 in_offset=bass.IndirectOffsetOnAxis(ap=idx[:, hk:hk + 1], axis=0)),
                          reads=[idxb] + uvb, writes=[rwb])
                    junk, junkb = junks[hk % 3]
                    s.op("dve", lambda e: e.scalar_tensor_tensor(out=junk[:], in0=rw[:, 0:1024], scalar=1.0, in1=hmv, op0=ALU.mult, op1=ALU.mult,
                                                                 accum_out=actv[:, hk:hk + 1]), reads=[rwb, hmpb], writes=[actb[hk % 16], junkb])
                    held[hk] = (rw, rwb)
                if 0 <= hk - 1 < 128:
                    st_a(hk - 1)
                if 0 <= hk - 3 < 128:
                    st_b(hk - 3)
                if fg is not None and hk % 4 == 3:
                    next(fg, None)
            s.op("dve", lambda e: e.tensor_tensor(out=scr[:, 0:512], in0=o0[:, :], in1=ABG[:, 2, 0:512], op=ALU.mult), reads=[o0b, ABGb], writes=[scrb])
            s.op("dve", lambda e: e.tensor_tensor(out=scr[:, 512:1024], in0=o1[:, :], in1=ABG[:, 2, 512:1024], op=ALU.mult), reads=[o1b, ABGb], writes=[scrb])
            s.op("pool", lambda e: e.tensor_tensor(out=ot[:], in0=scr[:], in1=xt[:], op=ALU.add), reads=[scrb, xb], writes=[otb])
            s.dma("sp", lambda e: e.dma_start(out=hout[t * 128:(t + 1) * 128, :], in_=ot[:]), reads=[otb], writes=[houtb[t]])

        for _ in front(0):
            pass
        for t in range(NT):
            fg = front(t + 1) if t + 1 < NT else None
            back(t, fg)
            if fg is not None:
                for _ in fg:
                    pass
        s.barrier()
```

```python
import numpy as np
from contextlib import ExitStack
import concourse.bass as bass
import concourse.mybir as mybir
from concourse.bass_utils import run_bass_kernel_spmd

F32 = mybir.dt.float32
BF16 = mybir.dt.bfloat16
I32 = mybir.dt.int32
U32 = mybir.dt.uint32
ALU = mybir.AluOpType
AF = mybir.ActivationFunctionType
AX = mybir.AxisListType

D = 1024
SEQ = 2048
NT = SEQ // 128
CTX = 256
NCT = CTX // 128
EPS = 1e-6
NEG = -30000.0


class Buf:
    __slots__ = ("w", "r")

    def __init__(self):
        self.w = None
        self.r = {}


class Sched:
    RING = 12

    def __init__(self, nc, es):
        self.nc = nc
        self.eng = {"pe": nc.tensor, "act": nc.scalar, "dve": nc.vector, "pool": nc.gpsimd, "sp": nc.sync}
        self.semobj = {}
        self.cnt = {}
        for k in self.eng:
            self.semobj[k] = es.enter_context(nc.semaphore("s_" + k))
            self.cnt[k] = 0
        self.waited = {k: {} for k in self.eng}
        self.bulk = []
        self.dq = {}
        for q in ("sp", "pool", "act"):
            slots = []
            for i in range(self.RING):
                key = ("d", q, i)
                self.semobj[key] = es.enter_context(nc.semaphore(f"d_{q}_{i}"))
                slots.append(key)
            self.dq[q] = {"slots": slots, "uses": [0] * self.RING, "next": 0}

    def _wait(self, ek, tok):
        if tok is None:
            return
        sk, v = tok
        if ek == "pe" and sk == "pe":
            return
        if self.waited[ek].get(sk, 0) >= v:
            return
        self.eng[ek].wait_ge(self.semobj[sk], v)
        self.waited[ek][sk] = v

    def _deps(self, ek, reads, writes):
        for b in reads:
            self._wait(ek, b.w)
        for b in writes:
            self._wait(ek, b.w)
            for sk, v in b.r.items():
                self._wait(ek, (sk, v))

    def _mark(self, tok, reads, writes):
        sk, v = tok
        for b in reads:
            if b.r.get(sk, 0) < v:
                b.r[sk] = v
        for b in writes:
            b.w = tok
            b.r = {}

    def op(self, ek, fn, reads=(), writes=()):
        self._deps(ek, reads, writes)
        ins = fn(self.eng[ek])
        self.cnt[ek] += 1
        ins.then_inc(self.semobj[ek], 1)
        tok = (ek, self.cnt[ek])
        self._mark(tok, reads, writes)
        return tok

    def dma(self, q, fn, reads=(), writes=()):
        dq = self.dq[q]
        slot = dq["next"]
        dq["next"] = (slot + 1) % self.RING
        key = dq["slots"][slot]
        uses = dq["uses"][slot]
        if uses:
            self._wait(q, (key, 16 * uses))
        self._deps(q, reads, writes)
        ins = fn(self.eng[q])
        ins.then_inc(self.semobj[key], 16)
        dq["uses"][slot] = uses + 1
        tok = (key, 16 * (uses + 1))
        self._mark(tok, reads, writes)
        return tok

    def bulk_dma(self, q, fn, reads=(), writes=(), es=None):
        key = ("bulk", len(self.semobj))
        self.semobj[key] = es.enter_context(self.nc.semaphore(f"bulk{len(self.semobj)}"))
        self._deps(q, reads, writes)
        ins = fn(self.eng[q])
        ins.then_inc(self.semobj[key], 16)
        tok = (key, 16)
        self.bulk.append(tok)
        self._mark(tok, reads, writes)
        return tok

    def barrier(self):
        toks = [(k, self.cnt[k]) for k in self.eng if self.cnt[k]]
        for q, dq in self.dq.items():
            for key, u in zip(dq["slots"], dq["uses"]):
                if u:
                    toks.append((key, 16 * u))
        toks.extend(self.bulk)
        for ek in self.eng:
            for t in toks:
                self._wait(ek, t)


class K:
    def __init__(self, nc, es):
        self.nc = nc
        self.es = es
        self.s = Sched(nc, es)
        self.banks = []
        for i in range(8):
            t = es.enter_context(nc.psum_tensor(f"bank{i}", [128, 512], F32))
            self.banks.append((t, Buf()))
        self.ident = None

    def sb(self, es, name, shape, dt):
        t = es.enter_context(self.nc.sbuf_tensor(name, list(shape), dt))
        return t, Buf()

    def poke(self):
        bg = getattr(self, "bg", None)
        if bg is not None:
            next(bg, None)


def stage_ada(k, cc, w_ada, b_ada, g_norm, modv):
    nc, s = k.nc, k.s
    with ExitStack() as es:
        cct, ccb = k.sb(es, "ada_cc", [128, 16], F32)
        sil, silb = k.sb(es, "ada_sil", [128, 16], F32)
        wt = [k.sb(es, f"ada_w{i}", [128, 8, 512], F32) for i in range(2)]
        brow, browb = k.sb(es, "ada_b", [2, 6144], F32)
        grow, growb = k.sb(es, "ada_g", [2, 2, 1024], F32)
        mrow, mrowb = k.sb(es, "ada_m", [2, 6144], F32)
        orow, orowb = k.sb(es, "ada_o", [2, 6, 1024], F32)

        s.dma("sp", lambda e: e.dma_start(out=cct[:], in_=cc), writes=[ccb])
        s.op("act", lambda e: e.activation(out=sil[:], in_=cct[:], func=AF.Silu), reads=[ccb], writes=[silb])
        for l in range(2):
            s.dma("sp", lambda e: e.dma_start(out=brow[:], in_=b_ada[l:l + 1, :].to_broadcast([2, 6144])), writes=[browb])
            s.dma("sp", lambda e: e.dma_start(out=grow[:], in_=g_norm[2 * l:2 * l + 2, :].rearrange("(o a) d -> o a d", o=1).to_broadcast([2, 2, 1024])), writes=[growb])
            wv = w_ada[l].rearrange("(kc p) n -> p kc n", p=128)
            for g in range(12):
                wtile, wbuf = wt[g % 2]
                q = "sp" if g % 2 == 0 else "act"
                s.dma(q, lambda e: e.dma_start(out=wtile[:], in_=wv[:, :, g * 512:(g + 1) * 512]), writes=[wbuf])
                bank, bb = k.banks[g % 2]
                for kc in range(8):
                    s.op("pe", lambda e: e.matmul(bank[0:2, :], lhsT=sil[:, 2 * kc:2 * kc + 2], rhs=wtile[:, kc, :],
                                                  start=(kc == 0), stop=(kc == 7)),
                         reads=[silb, wbuf], writes=[bb])
                s.op("dve", lambda e: e.tensor_tensor(out=mrow[:, g * 512:(g + 1) * 512], in0=bank[0:2, :],
                                                      in1=brow[:, g * 512:(g + 1) * 512], op=ALU.add),
                     reads=[bb, browb], writes=[mrowb])
            for j in range(2):
                sh = mrow[:, (3 * j) * 1024:(3 * j + 1) * 1024]
                sc = mrow[:, (3 * j + 1) * 1024:(3 * j + 2) * 1024]
                gt = mrow[:, (3 * j + 2) * 1024:(3 * j + 3) * 1024]
                s.op("dve", lambda e: e.scalar_tensor_tensor(out=orow[:, 3 * j, :], in0=sc, scalar=1.0, in1=grow[:, j, :],
                                                             op0=ALU.add, op1=ALU.mult),
                     reads=[mrowb, growb], writes=[orowb])
                s.op("dve", lambda e: e.tensor_copy(out=orow[:, 3 * j + 1, :], in_=sh), reads=[mrowb], writes=[orowb])
                s.op("dve", lambda e: e.tensor_copy(out=orow[:, 3 * j + 2, :], in_=gt), reads=[mrowb], writes=[orowb])
            s.dma("sp", lambda e: e.dma_start(out=modv[l], in_=orow[:]), reads=[orowb], writes=[k.modv_buf])
        s.barrier()


def load_cast(k, stg, dst, dstb, src, n, qi=0):
    s = k.s
    st, stb = stg[qi % len(stg)]
    s.dma("sp" if qi % 2 == 0 else "pool", lambda e: e.dma_start(out=st[:, 0:n], in_=src), writes=[stb])
    ek = ("act", "pool", "dve")[qi % 3]
    if ek == "act":
        s.op("act", lambda e: e.copy(out=dst, in_=st[:, 0:n]), reads=[stb], writes=[dstb])
    else:
        s.op(ek, lambda e: e.tensor_copy(out=dst, in_=st[:, 0:n]), reads=[stb], writes=[dstb])


def load_w_bf16(k, stg, wt, wb, wdram, kc, n, q0=0):
    qi = q0
    v = wdram.rearrange("(kc p) n -> p kc n", p=128)
    cw = stg[0][0].shape[1]
    for c in range(kc):
        for c0 in range(0, n, cw):
            c1 = min(n, c0 + cw)
            load_cast(k, stg, wt[:, c, c0:c1], wb, v[:, c, c0:c1], c1 - c0, qi)
            qi += 1
    return qi


def rstd_from_ss(k, ss, ssb, rs, rsb, inv_n, w):
    s = k.s
    s.op("act", lambda e: e.activation(out=rs[:, 0:w], in_=ss[:, 0:w], func=AF.Sqrt, scale=inv_n, bias=k.eps[:, 0:1]),
         reads=[ssb], writes=[rsb])
    s.op("dve", lambda e: e.reciprocal(out=rs[:, 0:w], in_=rs[:, 0:w]), reads=[rsb], writes=[rsb])


def norm_mod(k, xt, xb, A, B, ABb, scr, scrb, st, stb, out, outb, out_f32=None):
    s = k.s
    s.op("act", lambda e: e.activation(out=scr[:], in_=xt[:], func=AF.Square, accum_out=st[:, 0:1]),
         reads=[xb], writes=[scrb, stb])
    rstd_from_ss(k, st, stb, st, stb, 1.0 / D, 1)
    s.op("dve", lambda e: e.scalar_tensor_tensor(out=scr[:], in0=xt[:], scalar=st[:, 0:1], in1=A, op0=ALU.mult, op1=ALU.mult),
         reads=[xb, stb, ABb], writes=[scrb])
    if out_f32 is not None:
        of, ofb = out_f32
        s.op("pool", lambda e: e.tensor_tensor(out=of[:], in0=scr[:], in1=B, op=ALU.add), reads=[scrb, ABb], writes=[ofb])
        s.op("act", lambda e: e.copy(out=out[:], in_=of[:]), reads=[ofb], writes=[outb])
    else:
        s.op("pool", lambda e: e.tensor_tensor(out=out[:], in0=scr[:], in1=B, op=ALU.add), reads=[scrb, ABb], writes=[outb])


def transpose_chunks(k, bank, bankb, src_fn, nchunks, rows, dst, dstb, srcb, dst_view=None):
    s = k.s
    bv = bank[:].bitcast(BF16)
    for c in range(nchunks):
        s.op("pe", lambda e: e.transpose(out=bv[0:rows, c * 128:(c + 1) * 128], in_=src_fn(c), identity=k.ident[:]),
             reads=[srcb, k.identb], writes=[bankb])
    src = bv[0:rows, 0:nchunks * 128]
    if dst_view is not None:
        src = src.rearrange("p (c t) -> p c t", t=128)
    s.op("act", lambda e: e.copy(out=dst, in_=src), reads=[bankb], writes=[dstb])


def setup_consts(k, es, ident_d, identf_d=None):
    s = k.s
    k.ident, k.identb = k.sb(es, "ident_sb", [128, 128], BF16)
    k.eps, k.epsb = k.sb(es, "epsc", [128, 1], F32)
    s.dma("sp", lambda e: e.dma_start(out=k.ident[:], in_=ident_d), writes=[k.identb])
    if identf_d is not None:
        k.identf, _ = k.sb(es, "identf_sb", [128, 128], F32)
        s.dma("sp", lambda e: e.dma_start(out=k.identf[:], in_=identf_d), writes=[k.identb])
    s.op("dve", lambda e: e.memset(k.eps[:], EPS), writes=[k.epsb])


def load_bcast(k, tile, tb, row, n):
    k.s.dma("sp", lambda e: e.dma_start(out=tile, in_=row.to_broadcast([128, n])), writes=[tb])


def na_blocks(qt):
    if 2 <= qt <= 13:
        return [(qt - 2 + j, j) for j in range(5)]
    if qt == 0:
        return [(j, 5 + j) for j in range(4)]
    if qt == 1:
        return [(j, 9 + j) for j in range(4)]
    if qt == 14:
        return [(12 + j, 13 + j) for j in range(4)]
    return [(12 + j, 17 + j) for j in range(4)]


def run_pipelined(gens, lag, k=None):
    gens = list(gens)
    active = []
    nxt = 0
    since = lag
    while active or nxt < len(gens):
        if nxt < len(gens) and since >= lag:
            active.append(gens[nxt])
            nxt += 1
            since = 0
        for g in list(active):
            try:
                next(g)
            except StopIteration:
                active.remove(g)
        since += 1
        if k is not None:
            k.poke()


def stage_attn(k, x, ctx, modv0, W, hout, houtb):
    nc, s = k.nc, k.s
    banks = k.banks
    with ExitStack() as es:
        AB, ABb = k.sb(es, "at_AB", [128, 4, 1024], F32)
        osb, osbb = k.sb(es, "at_o", [128, NT, 1024], BF16)
        qT, qTb = k.sb(es, "at_qT", [96, 8, SEQ], BF16)
        kT, kTb = k.sb(es, "at_kT", [96, 8, SEQ + CTX], BF16)
        Vs, Vsb = k.sb(es, "at_V", [128, NT + NCT, 8, 65], BF16)
        st, stb = k.sb(es, "at_st", [128, 4], F32)
        st2, st2b = k.sb(es, "at_st2", [128, 16], F32)
        st3, st3b = k.sb(es, "at_st3", [128, 16], F32)
        xts = [k.sb(es, f"at_x{i}", [128, 1024], F32) for i in range(2)]
        scrs = [k.sb(es, f"at_scr{i}", [128, 1024], F32) for i in range(2)]
        abfs = [k.sb(es, f"at_a{i}", [128, 1024], BF16) for i in range(2)]
        aTs = [k.sb(es, f"at_aT{i}", [128, 8, 128], BF16) for i in range(2)]

        load_bcast(k, AB[:, 0, :], ABb, modv0[0:1, 0, :], 1024)
        load_bcast(k, AB[:, 1, :], ABb, modv0[0:1, 1, :], 1024)
        load_bcast(k, AB[:, 2, :], ABb, modv0[1:2, 0, :], 1024)
        load_bcast(k, AB[:, 3, :], ABb, modv0[1:2, 1, :], 1024)
        s.op("pool", lambda e: e.memset(Vs[:, :, :, 64:65], 1.0), writes=[Vsb])

        def src_tile(t):
            return ctx[t * 128:(t + 1) * 128, :] if t < NCT else x[(t - NCT) * 128:(t - NCT + 1) * 128, :]

        def front(t):
            xt, xb = xts[t % 2]
            scr, scrb = scrs[t % 2]
            abf, abfb = abfs[t % 2]
            aT, aTb = aTs[t % 2]
            s.dma("sp", lambda e: e.dma_start(out=xt[:], in_=src_tile(t)), writes=[xb])
            j = 2 if t < NCT else 0
            norm_mod(k, xt, xb, AB[:, j, :], AB[:, j + 1, :], ABb, scr, scrb, st, stb, abf, abfb)
            bank, bb = banks[7]
            transpose_chunks(k, bank, bb, lambda c: abf[:, c * 128:(c + 1) * 128], 8, 128, aT[:], aTb, abfb, dst_view=True)
            return aT, aTb, scr, scrb

        with ExitStack() as es1:
            w_in, w_inb = k.sb(es1, "p1_win", [128, 8, 672], BF16)
            w_q, w_qb = k.sb(es1, "p1_wq", [128, 3, 768], BF16)
            w_kv, w_kvb = k.sb(es1, "p1_wkv", [128, 2, 1024], BF16)
            gcn, gcnb = k.sb(es1, "p1_gcn", [128, 640], F32)
            gq, gqb = k.sb(es1, "p1_gq", [128, 96], F32)
            gk, gkb = k.sb(es1, "p1_gk", [128, 96], F32)
            zsbs = [k.sb(es1, f"p1_z{i}", [128, 672], F32) for i in range(2)]
            cn, cnb = k.sb(es1, "p1_cn", [128, 640], BF16)
            cnT, cnTb = k.sb(es1, "p1_cnT", [128, 5, 128], BF16)
            qn, qnb = k.sb(es1, "p1_qn", [128, 8, 96], F32)
            kn, knb = k.sb(es1, "p1_kn", [128, 8, 64], F32)
            qr, qrb = k.sb(es1, "p1_qr", [128, 8, 32], F32)
            rt, rtb = k.sb(es1, "p1_rt", [128, 4, 8, 16], F32)
            krg, krgb = k.sb(es1, "p1_krg", [128, 32], F32)
            krr, krrb = k.sb(es1, "p1_krr", [128, 32], F32)
            kt4, kt4b = k.sb(es1, "p1_kt4", [128, 4, 16], F32)
            qf, qfb = k.sb(es1, "p1_qf", [128, 8, 96], BF16)
            kf, kfb = k.sb(es1, "p1_kf", [128, 8, 96], BF16)
            ropes = [k.sb(es1, f"p1_rope{i}", [128, 32], F32) for i in range(2)]

            wv = W["attn_w_in"].rearrange("(kc p) n -> p kc n", p=128)
            for c in range(8):
                s.dma("pool", lambda e: e.dma_start(out=w_in[:, c, :], in_=wv[:, c, 0:672]), writes=[w_inb])
            wv = W["mla_w_q_up"].rearrange("(kc p) n -> p kc n", p=128)
            for c in range(3):
                s.dma("pool", lambda e: e.dma_start(out=w_q[:, c, :], in_=wv[:, c, :]), writes=[w_qb])
            wv = W["mla_w_kv_up"].rearrange("(kc p) n -> p kc n", p=128)
            for c in range(2):
                s.dma("pool", lambda e: e.dma_start(out=w_kv[:, c, :], in_=wv[:, c, :]), writes=[w_kvb])
            load_bcast(k, gcn[:, 0:384], gcnb, W["mla_g_qa"], 384)
            load_bcast(k, gcn[:, 384:640], gcnb, W["mla_g_kva"], 256)
            load_bcast(k, gq[:], gqb, W["mla_g_q"], 96)
            load_bcast(k, gk[:], gkb, W["mla_g_k"], 96)
            s.op("dve", lambda e: e.tensor_scalar_mul(out=gq[:], in0=gq[:], scalar1=96.0 ** -0.5), reads=[gqb], writes=[gqb])

            def p1_tile(t):
                lat = t >= NCT
                zsb, zsbb = zsbs[t % 2]
                aT, aTb, scr, scrb = front(t)
                if lat:
                    rp, rpb = ropes[t % 2]
                    s.dma("sp", lambda e: e.dma_start(out=rp[:], in_=W["rope"][(t - NCT) * 128:(t - NCT + 1) * 128, :]), writes=[rpb])
                yield
                (z0, z0b), (z1, z1b) = banks[0], banks[1]
                for (zb, zbb, c0, c1) in ((z0, z0b, 0, 512), (z1, z1b, 512, 672)):
                    for c in range(8):
                        s.op("pe", lambda e: e.matmul(zb[:, 0:c1 - c0], lhsT=aT[:, c, :], rhs=w_in[:, c, c0:c1], start=(c == 0), stop=(c == 7)),
                             reads=[aTb, w_inb], writes=[zbb])
                    s.op("act", lambda e: e.copy(out=zsb[:, c0:c1], in_=zb[:, 0:c1 - c0]), reads=[zbb], writes=[zsbb])
                yield
                if lat:
                    s.op("act", lambda e: e.activation(out=scr[:, 0:384], in_=zsb[:, 0:384], func=AF.Square, accum_out=st2[:, 0:1]),
                         reads=[zsbb], writes=[scrb, st2b])
                    rstd_from_ss(k, st2, st2b, st3, st3b, 1.0 / 384, 1)
                    s.op("dve", lambda e: e.scalar_tensor_tensor(out=cn[:, 0:384], in0=zsb[:, 0:384], scalar=st3[:, 0:1], in1=gcn[:, 0:384],
                                                                 op0=ALU.mult, op1=ALU.mult), reads=[zsbb, st3b, gcnb], writes=[cnb])
                s.op("act", lambda e: e.activation(out=scr[:, 384:640], in_=zsb[:, 384:640], func=AF.Square, accum_out=st2[:, 1:2]),
                     reads=[zsbb], writes=[scrb, st2b])
                s.op("act", lambda e: e.activation(out=st3[:, 1:2], in_=st2[:, 1:2], func=AF.Sqrt, scale=1.0 / 256, bias=k.eps[:, 0:1]),
                     reads=[st2b], writes=[st3b])
                s.op("dve", lambda e: e.reciprocal(out=st3[:, 1:2], in_=st3[:, 1:2]), reads=[st3b], writes=[st3b])
                s.op("dve", lambda e: e.scalar_tensor_tensor(out=cn[:, 384:640], in0=zsb[:, 384:640], scalar=st3[:, 1:2], in1=gcn[:, 384:640],
                                                             op0=ALU.mult, op1=ALU.mult), reads=[zsbb, st3b, gcnb], writes=[cnb])
                s.op("act", lambda e: e.activation(out=scr[:, 640:672], in_=zsb[:, 640:672], func=AF.Square, accum_out=st2[:, 2:3]),
                     reads=[zsbb], writes=[scrb, st2b])
                yield
                c_lo = 0 if lat else 3
                bank, bb = banks[7]
                bv = bank[:].bitcast(BF16)
                for c in range(c_lo, 5):
                    s.op("pe", lambda e: e.transpose(out=bv[:, c * 128:(c + 1) * 128], in_=cn[:, c * 128:(c + 1) * 128], identity=k.ident[:]),
                         reads=[cnb, k.identb], writes=[bb])
                s.op("act", lambda e: e.copy(out=cnT[:, c_lo:5, :], in_=bv[:, c_lo * 128:640].rearrange("p (c t) -> p c t", t=128)),
                     reads=[bb], writes=[cnTb])
                yield
                if lat:
                    (qa, qab), (qb_, qbb) = banks[2], banks[3]
                    for (qk, qkb, h0, h1) in ((qa, qab, 0, 5), (qb_, qbb, 5, 8)):
                        n = (h1 - h0) * 96
                        for c in range(3):
                            s.op("pe", lambda e: e.matmul(qk[:, 0:n], lhsT=cnT[:, c, :], rhs=w_q[:, c, h0 * 96:h1 * 96], start=(c == 0), stop=(c == 2)),
                                 reads=[cnTb, w_qb], writes=[qkb])
                        s.op("act", lambda e: e.activation(out=scr[:, h0 * 96:h1 * 96], in_=qk[:, 0:n], func=AF.Square), reads=[qkb], writes=[scrb])
                    s.op("dve", lambda e: e.tensor_reduce(out=st2[:, 4:12], in_=scr[:, 0:768].rearrange("p (h d) -> p h d", d=96), axis=AX.X, op=ALU.add),
                         reads=[scrb], writes=[st2b])
                    s.op("act", lambda e: e.activation(out=st3[:, 4:12], in_=st2[:, 4:12], func=AF.Sqrt, scale=1.0 / 96, bias=k.eps[:, 0:1]),
                         reads=[st2b], writes=[st3b])
                    s.op("dve", lambda e: e.reciprocal(out=st3[:, 4:12], in_=st3[:, 4:12]), reads=[st3b], writes=[st3b])
                    for (qk, qkb, h0, h1) in ((qa, qab, 0, 5), (qb_, qbb, 5, 8)):
                        n = (h1 - h0) * 96
                        s.op("dve", lambda e: e.tensor_tensor(out=qn[:, h0:h1, :], in0=qk[:, 0:n].rearrange("p (h d) -> p h d", d=96),
                                                              in1=st3[:, 4 + h0:4 + h1].unsqueeze(2).to_broadcast([128, h1 - h0, 96]), op=ALU.mult),
                             reads=[qkb, st3b], writes=[qnb])
                    s.op("pool", lambda e: e.tensor_tensor(out=qf[:, :, 0:64], in0=qn[:, :, 0:64],
                                                           in1=gq[:, 0:64].unsqueeze(1).to_broadcast([128, 8, 64]), op=ALU.mult),
                         reads=[qnb, gqb], writes=[qfb])
                    s.op("dve", lambda e: e.tensor_tensor(out=qr[:], in0=qn[:, :, 64:96],
                                                          in1=gq[:, 64:96].unsqueeze(1).to_broadcast([128, 8, 32]), op=ALU.mult),
                         reads=[qnb, gqb], writes=[qrb])
                    cosb = rp[:, 0:16].unsqueeze(1).to_broadcast([128, 8, 16])
                    sinb = rp[:, 16:32].unsqueeze(1).to_broadcast([128, 8, 16])
                    s.op("dve", lambda e: e.tensor_tensor(out=rt[:, 0], in0=qr[:, :, 0:16], in1=cosb, op=ALU.mult), reads=[qrb, rpb], writes=[rtb])
                    s.op("dve", lambda e: e.tensor_tensor(out=rt[:, 1], in0=qr[:, :, 16:32], in1=sinb, op=ALU.mult), reads=[qrb, rpb], writes=[rtb])
                    s.op("dve", lambda e: e.tensor_tensor(out=rt[:, 2], in0=qr[:, :, 0:16], in1=sinb, op=ALU.mult), reads=[qrb, rpb], writes=[rtb])
                    s.op("dve", lambda e: e.tensor_tensor(out=rt[:, 3], in0=qr[:, :, 16:32], in1=cosb, op=ALU.mult), reads=[qrb, rpb], writes=[rtb])
                    s.op("dve", lambda e: e.tensor_tensor(out=qf[:, :, 64:80], in0=rt[:, 0], in1=rt[:, 1], op=ALU.subtract), reads=[rtb], writes=[qfb])
                    s.op("dve", lambda e: e.tensor_tensor(out=qf[:, :, 80:96], in0=rt[:, 2], in1=rt[:, 3], op=ALU.add), reads=[rtb], writes=[qfb])
                yield
                (ka, kab), (kb_, kbb) = banks[4], banks[5]
                for g, (kk, kkb) in enumerate(((ka, kab), (kb_, kbb))):
                    for c in range(2):
                        s.op("pe", lambda e: e.matmul(kk[:, :], lhsT=cnT[:, 3 + c, :], rhs=w_kv[:, c, g * 512:(g + 1) * 512], start=(c == 0), stop=(c == 1)),
                             reads=[cnTb, w_kvb], writes=[kkb])
                    kv3 = kk[:, :].rearrange("p (h d) -> p h d", d=128)
                    s.op("act", lambda e: e.activation(out=scr[:, g * 256:(g + 1) * 256].rearrange("p (h d) -> p h d", d=64), in_=kv3[:, :, 0:64], func=AF.Square),
                         reads=[kkb], writes=[scrb])
                    s.op("act", lambda e: e.copy(out=Vs[:, t, g * 4:(g + 1) * 4, 0:64], in_=kv3[:, :, 64:128]), reads=[kkb], writes=[Vsb])
                yield
                s.op("dve", lambda e: e.tensor_reduce(out=st2[:, 4:12], in_=scr[:, 0:512].rearrange("p (h d) -> p h d", d=64), axis=AX.X, op=ALU.add),
                     reads=[scrb], writes=[st2b])
                s.op("dve", lambda e: e.tensor_scalar(out=st2[:, 4:12], in0=st2[:, 4:12], scalar1=st2[:, 2:3], scalar2=None, op0=ALU.add),
                     reads=[st2b], writes=[st2b])
                s.op("act", lambda e: e.activation(out=st3[:, 4:12], in_=st2[:, 4:12], func=AF.Sqrt, scale=1.0 / 96, bias=k.eps[:, 0:1]),
                     reads=[st2b], writes=[st3b])
                s.op("dve", lambda e: e.reciprocal(out=st3[:, 4:12], in_=st3[:, 4:12]), reads=[st3b], writes=[st3b])
                for g, (kk, kkb) in enumerate(((ka, kab), (kb_, kbb))):
                    kv3 = kk[:, :].rearrange("p (h d) -> p h d", d=128)
                    s.op("dve", lambda e: e.tensor_tensor(out=kn[:, g * 4:(g + 1) * 4, :], in0=kv3[:, :, 0:64],
                                                          in1=st3[:, 4 + g * 4:8 + g * 4].unsqueeze(2).to_broadcast([128, 4, 64]), op=ALU.mult),
                         reads=[kkb, st3b], writes=[knb])
                s.op("pool", lambda e: e.tensor_tensor(out=kf[:, :, 0:64], in0=kn[:], in1=gk[:, 0:64].unsqueeze(1).to_broadcast([128, 8, 64]), op=ALU.mult),
                     reads=[knb, gkb], writes=[kfb])
                s.op("dve", lambda e: e.tensor_tensor(out=krg[:], in0=zsb[:, 640:672], in1=gk[:, 64:96], op=ALU.mult), reads=[zsbb, gkb], writes=[krgb])
                if lat:
                    s.op("dve", lambda e: e.tensor_tensor(out=kt4[:, 0], in0=krg[:, 0:16], in1=rp[:, 0:16], op=ALU.mult), reads=[krgb, rpb], writes=[kt4b])
                    s.op("dve", lambda e: e.tensor_tensor(out=kt4[:, 1], in0=krg[:, 16:32], in1=rp[:, 16:32], op=ALU.mult), reads=[krgb, rpb], writes=[kt4b])
                    s.op("dve", lambda e: e.tensor_tensor(out=kt4[:, 2], in0=krg[:, 0:16], in1=rp[:, 16:32], op=ALU.mult), reads=[krgb, rpb], writes=[kt4b])
                    s.op("dve", lambda e: e.tensor_tensor(out=kt4[:, 3], in0=krg[:, 16:32], in1=rp[:, 0:16], op=ALU.mult), reads=[krgb, rpb], writes=[kt4b])
                    s.op("dve", lambda e: e.tensor_tensor(out=krr[:, 0:16], in0=kt4[:, 0], in1=kt4[:, 1], op=ALU.subtract), reads=[kt4b], writes=[krrb])
                    s.op("dve", lambda e: e.tensor_tensor(out=krr[:, 16:32], in0=kt4[:, 2], in1=kt4[:, 3], op=ALU.add), reads=[kt4b], writes=[krrb])
                else:
                    s.op("dve", lambda e: e.tensor_copy(out=krr[:], in_=krg[:]), reads=[krgb], writes=[krrb])
                s.op("dve", lambda e: e.tensor_tensor(out=kf[:, :, 64:96], in0=krr[:].unsqueeze(1).to_broadcast([128, 8, 32]),
                                                      in1=st3[:, 4:12].unsqueeze(2).to_broadcast([128, 8, 32]), op=ALU.mult),
                     reads=[krrb, st3b], writes=[kfb])
                yield
                if lat:
                    tb_, tbb = banks[6]
                    tl = t - NCT
                    transpose_chunks(k, tb_, tbb, lambda h: qf[:, h, :], 8, 96, qT[:, :, tl * 128:(tl + 1) * 128], qTb, qfb, dst_view=True)
                tb_, tbb = banks[0]
                transpose_chunks(k, tb_, tbb, lambda h: kf[:, h, :], 8, 96, kT[:, :, t * 128:(t + 1) * 128], kTb, kfb, dst_view=True)
            run_pipelined([p1_tile(t) for t in range(NT + NCT)], 4, k)
            s.barrier()

        with ExitStack() as es2:
            pTs = [k.sb(es2, f"p2_pT{i}", [128, 512], BF16) for i in range(3)]
            rc, rcb = k.sb(es2, "p2_rc", [128, 8], F32)
            steps = [(h, g, kt) for h in range(8) for g in range(4) for kt in range(NT + NCT)]

            def emit_s(i):
                h, g, kt = steps[i]
                sbk, sbkb = banks[i % 2]
                pT, pTb = pTs[i % 3]
                s.op("pe", lambda e: e.matmul(sbk[:, :], lhsT=kT[:, h, kt * 128:(kt + 1) * 128], rhs=qT[:, h, g * 512:(g + 1) * 512], start=True, stop=True),
                     reads=[kTb, qTb], writes=[sbkb])
                s.op("act", lambda e: e.activation(out=pT[:], in_=sbk[:, :], func=AF.Exp), reads=[sbkb], writes=[pTb])

            def emit_pv(i):
                h, g, kt = steps[i]
                pT, pTb = pTs[i % 3]
                for qs in range(4):
                    ob, obb = banks[2 + qs]
                    s.op("pe", lambda e: e.matmul(ob[:, 0:65], lhsT=pT[:, qs * 128:(qs + 1) * 128], rhs=Vs[:, kt, h, :],
                                                  start=(kt == 0), stop=(kt == NT + NCT - 1)), reads=[pTb, Vsb], writes=[obb])
                if kt == NT + NCT - 1:
                    for qs in range(4):
                        ob, obb = banks[2 + qs]
                        j = (g * 4 + qs) % 8
                        s.op("dve", lambda e: e.reciprocal(out=rc[:, j:j + 1], in_=ob[:, 64:65]), reads=[obb], writes=[rcb])
                        s.op("dve", lambda e: e.tensor_scalar(out=osb[:, g * 4 + qs, h * 64:(h + 1) * 64], in0=ob[:, 0:64], scalar1=rc[:, j:j + 1],
                                                              scalar2=None, op0=ALU.mult), reads=[obb, rcb], writes=[osbb])

            emit_s(0)
            for i in range(len(steps)):
                if i + 1 < len(steps):
                    emit_s(i + 1)
                emit_pv(i)
                if i % 8 == 7:
                    k.poke()
            s.barrier()

        with ExitStack() as es3:
            stg = [k.sb(es3, f"p3_stg{i}", [128, 1536], F32) for i in range(2)]
            w_in, w_inb = k.sb(es3, "p3_win", [128, 8, 1536], BF16)
            gqn, gqnb = k.sb(es3, "p3_gq", [128, 64], F32)
            gkn, gknb = k.sb(es3, "p3_gk", [128, 64], F32)
            tn, tnb = k.sb(es3, "p3_tn", [128, 16, 64], F32)
            qkf, qkfb = k.sb(es3, "p3_qkf", [128, 16, 64], BF16)
            wv = W["attn_w_in"].rearrange("(kc p) n -> p kc n", p=128)
            for c in range(8):
                load_cast(k, stg, w_in[:, c, :], w_inb, wv[:, c, 672:2208], 1536, c)
            load_bcast(k, gqn[:], gqnb, W["na_g_q"], 64)
            load_bcast(k, gkn[:], gknb, W["na_g_k"], 64)
            s.op("dve", lambda e: e.tensor_scalar_mul(out=gqn[:], in0=gqn[:], scalar1=64.0 ** -0.5), reads=[gqnb], writes=[gqnb])
            def p3_tile(t):
                lat = t >= NCT
                aT, aTb, scr, scrb = front(t)
                yield
                grp = (0, 1, 2) if lat else (1, 2)
                for g in grp:
                    zb, zbb = banks[g]
                    for c in range(8):
                        s.op("pe", lambda e: e.matmul(zb[:, :], lhsT=aT[:, c, :], rhs=w_in[:, c, g * 512:(g + 1) * 512], start=(c == 0), stop=(c == 7)),
                             reads=[aTb, w_inb], writes=[zbb])
                    if g < 2:
                        s.op("act", lambda e: e.activation(out=scr[:, g * 512:(g + 1) * 512], in_=zb[:, :], func=AF.Square), reads=[zbb], writes=[scrb])
                    else:
                        s.op("act", lambda e: e.copy(out=Vs[:, t, :, 0:64], in_=zb[:, :].rearrange("p (h d) -> p h d", d=64)), reads=[zbb], writes=[Vsb])
                yield
                g0 = 0 if lat else 1
                s.op("dve", lambda e: e.tensor_reduce(out=st2[:, g0 * 8:16], in_=scr[:, g0 * 512:1024].rearrange("p (h d) -> p h d", d=64), axis=AX.X, op=ALU.add),
                     reads=[scrb], writes=[st2b])
                s.op("act", lambda e: e.activation(out=st3[:, g0 * 8:16], in_=st2[:, g0 * 8:16], func=AF.Sqrt, scale=1.0 / 64, bias=k.eps[:, 0:1]),
                     reads=[st2b], writes=[st3b])
                s.op("dve", lambda e: e.reciprocal(out=st3[:, g0 * 8:16], in_=st3[:, g0 * 8:16]), reads=[st3b], writes=[st3b])
                for g in grp[:-1]:
                    zb, zbb = banks[g]
                    gg, ggb = (gqn, gqnb) if g == 0 else (gkn, gknb)
                    s.op("dve", lambda e: e.tensor_tensor(out=tn[:, g * 8:(g + 1) * 8, :], in0=zb[:, :].rearrange("p (h d) -> p h d", d=64),
                                                          in1=st3[:, g * 8:(g + 1) * 8].unsqueeze(2).to_broadcast([128, 8, 64]), op=ALU.mult),
                         reads=[zbb, st3b], writes=[tnb])
                    s.op("pool", lambda e: e.tensor_tensor(out=qkf[:, g * 8:(g + 1) * 8, :], in0=tn[:, g * 8:(g + 1) * 8, :],
                                                           in1=gg[:].unsqueeze(1).to_broadcast([128, 8, 64]), op=ALU.mult),
                         reads=[tnb, ggb], writes=[qkfb])
                yield
                if lat:
                    tb_, tbb = banks[3]
                    tl = t - NCT
                    transpose_chunks(k, tb_, tbb, lambda h: qkf[:, h, :], 8, 64, qT[0:64, :, tl * 128:(tl + 1) * 128], qTb, qkfb, dst_view=True)
                tb_, tbb = banks[4]
                transpose_chunks(k, tb_, tbb, lambda h: qkf[:, 8 + h, :], 8, 64, kT[0:64, :, t * 128:(t + 1) * 128], kTb, qkfb, dst_view=True)
            run_pipelined([p3_tile(t) for t in range(NT + NCT)], 2, k)
            s.barrier()

        with ExitStack() as es4:
            nbs = [k.sb(es4, f"p4_nb{i}", [128, 21, 128], F32) for i in range(2)]
            sfs = [k.sb(es4, f"p4_sf{i}", [128, 640], F32) for i in range(2)]
            pTs = [k.sb(es4, f"p4_pT{i}", [128, 896], BF16) for i in range(2)]
            rc, rcb = k.sb(es4, "p4_rc", [128, 8], F32)
            steps = [(h, qt) for h in range(8) for qt in range(NT)]

            def res(i):
                return (banks[(i % 2) * 2], banks[(i % 2) * 2 + 1], sfs[i % 2], pTs[i % 2], banks[4 + i % 2])

            def emit_s(i):
                h, qt = steps[i]
                nb, nbb = nbs[h % 2]
                if qt == 0:
                    s.dma("sp", lambda e: e.dma_start(out=nb[:], in_=W["nabias"][h]), writes=[nbb])
                blocks = na_blocks(qt)
                nloc = len(blocks)
                (sa, sab), (sb_, sbb), (sf, sfb), (pT, pTb), _ = res(i)
                qsl = qT[0:64, h, qt * 128:(qt + 1) * 128]
                for j, (kt, bi) in enumerate(blocks):
                    dstb_, dstbb = (sa, sab) if j < 4 else (sb_, sbb)
                    col = (j % 4) * 128
                    s.op("pe", lambda e: e.matmul(dstb_[:, col:col + 128], lhsT=kT[0:64, h, (NCT + kt) * 128:(NCT + kt + 1) * 128], rhs=qsl, start=True, stop=True),
                         reads=[kTb, qTb], writes=[dstbb])
                for c in range(NCT):
                    s.op("pe", lambda e: e.matmul(sb_[:, 128 + c * 128:256 + c * 128], lhsT=kT[0:64, h, c * 128:(c + 1) * 128], rhs=qsl, start=True, stop=True),
                         reads=[kTb, qTb], writes=[sbb])
                b0 = blocks[0][1]
                s.op("dve", lambda e: e.tensor_tensor(out=sf[:, 0:512], in0=sa[:, :], in1=nb[:, b0:b0 + 4, :].rearrange("p b q -> p (b q)"), op=ALU.add),
                     reads=[sab, nbb], writes=[sfb])
                if nloc == 5:
                    s.op("dve", lambda e: e.tensor_tensor(out=sf[:, 512:640], in0=sb_[:, 0:128], in1=nb[:, b0 + 4, :], op=ALU.add),
                         reads=[sbb, nbb], writes=[sfb])
                s.op("act", lambda e: e.activation(out=pT[:, 0:nloc * 128], in_=sf[:, 0:nloc * 128], func=AF.Exp), reads=[sfb], writes=[pTb])
                s.op("act", lambda e: e.activation(out=pT[:, 640:896], in_=sb_[:, 128:384], func=AF.Exp), reads=[sbb], writes=[pTb])

            def emit_pv(i):
                h, qt = steps[i]
                blocks = na_blocks(qt)
                _, _, _, (pT, pTb), (ob, obb) = res(i)
                for j, (kt, bi) in enumerate(blocks):
                    s.op("pe", lambda e: e.matmul(ob[:, 0:65], lhsT=pT[:, j * 128:(j + 1) * 128], rhs=Vs[:, NCT + kt, h, :], start=(j == 0), stop=False),
                         reads=[pTb, Vsb], writes=[obb])
                for c in range(NCT):
                    s.op("pe", lambda e: e.matmul(ob[:, 0:65], lhsT=pT[:, 640 + c * 128:768 + c * 128], rhs=Vs[:, c, h, :], start=False, stop=(c == NCT - 1)),
                         reads=[pTb, Vsb], writes=[obb])
                j8 = i % 8
                s.op("dve", lambda e: e.reciprocal(out=rc[:, j8:j8 + 1], in_=ob[:, 64:65]), reads=[obb], writes=[rcb])
                s.op("dve", lambda e: e.tensor_scalar(out=osb[:, qt, 512 + h * 64:512 + (h + 1) * 64], in0=ob[:, 0:64], scalar1=rc[:, j8:j8 + 1],
                                                      scalar2=None, op0=ALU.mult), reads=[obb, rcb], writes=[osbb])

            emit_s(0)
            for i in range(len(steps)):
                if i + 1 < len(steps):
                    emit_s(i + 1)
                emit_pv(i)
                if i % 8 == 7:
                    k.poke()
            s.barrier()

        with ExitStack() as es5:
            stg = [k.sb(es5, f"p5_stg{i}", [128, 1024], F32) for i in range(2)]
            w_o, w_ob = k.sb(es5, "p5_wo", [128, 8, 1024], BF16)
            G1, G1b = k.sb(es5, "p5_g1", [128, 1024], F32)
            outs = [k.sb(es5, f"p5_out{i}", [128, 1024], F32) for i in range(2)]
            wv = W["attn_w_out"].rearrange("(kc p) n -> p kc n", p=128)
            for c in range(8):
                load_cast(k, stg, w_o[:, c, :], w_ob, wv[:, c, :], 1024, c)
            load_bcast(k, G1[:], G1b, modv0[0:1, 2, :], 1024)
            for t in range(NT):
                xt, xb = xts[t % 2]
                scr, scrb = scrs[t % 2]
                aT, aTb = aTs[t % 2]
                ot, otb = outs[t % 2]
                s.dma("sp", lambda e: e.dma_start(out=xt[:], in_=x[t * 128:(t + 1) * 128, :]), writes=[xb])
                bank, bb = banks[7]
                transpose_chunks(k, bank, bb, lambda c: osb[:, t, c * 128:(c + 1) * 128], 8, 128, aT[:], aTb, osbb, dst_view=True)
                for g in range(2):
                    yb, ybb = banks[g]
                    for c in range(8):
                        s.op("pe", lambda e: e.matmul(yb[:, :], lhsT=aT[:, c, :], rhs=w_o[:, c, g * 512:(g + 1) * 512], start=(c == 0), stop=(c == 7)),
                             reads=[aTb, w_ob], writes=[ybb])
                    s.op("dve", lambda e: e.tensor_tensor(out=scr[:, g * 512:(g + 1) * 512], in0=yb[:, :], in1=G1[:, g * 512:(g + 1) * 512], op=ALU.mult),
                         reads=[ybb, G1b], writes=[scrb])
                s.op("pool", lambda e: e.tensor_tensor(out=ot[:], in0=scr[:], in1=xt[:], op=ALU.add), reads=[scrb, xb], writes=[otb])
                s.dma("sp", lambda e: e.dma_start(out=hout[t * 128:(t + 1) * 128, :], in_=ot[:]), reads=[otb], writes=[houtb[t]])
            s.barrier()


def host_rope_table():
    t = np.arange(SEQ)
    row = (t // 64).astype(np.float32)
    col = (t % 64).astype(np.float32)
    inv = (np.float32(1.0) / (np.float32(10000.0) ** (np.arange(8, dtype=np.float32) / np.float32(8)))).astype(np.float32)
    ang = np.concatenate([row[:, None] * inv, col[:, None] * inv], axis=-1).astype(np.float32)
    return np.concatenate([np.cos(ang), np.sin(ang)], axis=-1).astype(np.float32)


def host_na_bias(rpb):
    pairs = [(2, j) for j in range(5)] + [(0, j) for j in range(4)] + [(1, j) for j in range(4)] \
        + [(14, 12 + j) for j in range(4)] + [(15, 12 + j) for j in range(4)]
    out = np.full((8, 128, 21, 128), NEG, np.float32)
    p = np.arange(128)
    for bi, (qt, kt) in enumerate(pairs):
        tq = qt * 128 + p
        tk = kt * 128 + p
        r, c = tq // 64, tq % 64
        kr, kc = tk // 64, tk % 64
        r0 = np.clip(r - 4, 0, 24)
        c0 = np.clip(c - 8, 0, 48)
        inside = (kr[:, None] >= r0[None, :]) & (kr[:, None] < r0[None, :] + 8) & (kc[:, None] >= c0[None, :]) & (kc[:, None] < c0[None, :] + 16)
        rr = np.clip(kr[:, None] - r[None, :] + 7, 0, 14)
        rc = np.clip(kc[:, None] - c[None, :] + 15, 0, 30)
        vals = rpb[:, rr, rc]
        out[:, :, bi, :] = np.where(inside[None], vals, np.float32(NEG))
    return out


def stage_conv(k, hin, hinb, modv_l, W, hout, houtb):
    nc, s = k.nc, k.s
    banks = k.banks
    PADW = SEQ + 30
    with ExitStack() as es:
        cbuf, cbufb = k.sb(es, "cv_cbuf", [128, NT, 1024], F32)
        ABG, ABGb = k.sb(es, "cv_ABG", [128, 2, 1024], F32)
        st, stb = k.sb(es, "cv_st", [128, 8], F32)
        xts = [k.sb(es, f"cv_x{i}", [128, 1024], F32) for i in range(2)]
        scrs = [k.sb(es, f"cv_scr{i}", [128, 1024], F32) for i in range(2)]
        for j in range(2):
            load_bcast(k, ABG[:, j, :], ABGb, modv_l[0:1, j, :], 1024)
        cbt = [Buf() for _ in range(NT // 4)]
        with ExitStack() as es1:
            aTa, aTab = k.sb(es1, "cv_aT", [128, 8, SEQ], BF16)
            ubfs = [k.sb(es1, f"cv_ub{i}", [128, PADW], BF16) for i in range(2)]
            dgt, dgtb = k.sb(es1, "cv_dgt", [128, 12, 128], BF16)
            w1, w1b = k.sb(es1, "cv_w1", [128, 8, 2048], BF16)
            b1T, b1Tb = k.sb(es1, "cv_b1T", [128, 16], F32)
            wdw, wdwb = k.sb(es1, "cv_wdw", [128, 8, 31], F32)
            bdw, bdwb = k.sb(es1, "cv_bdw", [128, 8], F32)
            abfs = [k.sb(es1, f"cv_a{i}", [128, 1024], BF16) for i in range(1)]
            upads = [k.sb(es1, f"cv_up{i}", [128, PADW], F32) for i in range(2)]
            accs = [k.sb(es1, f"cv_acc{i}", [128, SEQ], F32) for i in range(2)]
            sgs = [k.sb(es1, f"cv_sg{i}", [128, 512], F32) for i in range(2)]
            w1v = W["conv_w_pw1"].rearrange("(kc p) n -> p kc n", p=128)
            for c in range(8):
                s.dma("pool", lambda e: e.dma_start(out=w1[:, c, :], in_=w1v[:, c, :]), writes=[w1b])
            s.dma("sp", lambda e: e.dma_start(out=b1T[:], in_=W["conv_b1T"]), writes=[b1Tb])
            s.dma("sp", lambda e: e.dma_start(out=wdw[:], in_=W["conv_wdwT"]), writes=[wdwb])
            s.dma("sp", lambda e: e.dma_start(out=bdw[:], in_=W["conv_bdwT"]), writes=[bdwb])
            for up, upb in upads + ubfs:
                s.op("pool", lambda e: e.memset(up[:, 0:15], 0.0), writes=[upb])
                s.op("pool", lambda e: e.memset(up[:, 15 + SEQ:PADW], 0.0), writes=[upb])
            for t in range(NT):
                xt, xb = xts[t % 2]
                scr, scrb = scrs[t % 2]
                abf, abfb = abfs[0]
                s.dma("sp", lambda e: e.dma_start(out=xt[:], in_=hin[t * 128:(t + 1) * 128, :]), reads=[hinb[t]], writes=[xb])
                norm_mod(k, xt, xb, ABG[:, 0, :], ABG[:, 1, :], ABGb, scr, scrb, st, stb, abf, abfb)
                bank, bb = banks[6 + t % 2]
                transpose_chunks(k, bank, bb, lambda c: abf[:, c * 128:(c + 1) * 128], 8, 128, aTa[:, :, t * 128:(t + 1) * 128], aTab, abfb, dst_view=True)
            accbs = [[Buf() for _ in range(4)] for _ in range(2)]
            NPE = 12
            tapbanks = [banks[2], banks[3], banks[6], banks[7]]

            def pw1_gen(m):
                up, upb = upads[m % 2]
                ub, ubb = ubfs[m % 2]
                for j in range(NPE):
                    s.op("act", lambda e: e.activation(out=dgt[:, j, :], in_=k.ident[:], func=AF.Copy, scale=wdw[:, m, j:j + 1]),
                         reads=[k.identb, wdwb], writes=[dgtb])
                for tg in range(4):
                    (bv_, bvb), (bg_, bgb) = banks[0], banks[1]
                    sg, sgb = sgs[tg % 2]
                    for (bk, bkb, c0) in ((bv_, bvb, m * 128), (bg_, bgb, 1024 + m * 128)):
                        for c in range(8):
                            s.op("pe", lambda e: e.matmul(bk[:, :], lhsT=w1[:, c, c0:c0 + 128], rhs=aTa[:, c, tg * 512:(tg + 1) * 512], start=(c == 0), stop=(c == 7)),
                                 reads=[w1b, aTab], writes=[bkb])
                    s.op("act", lambda e: e.activation(out=sg[:], in_=bg_[:, :], func=AF.Sigmoid, bias=b1T[:, 8 + m:9 + m]), reads=[bgb, b1Tb], writes=[sgb])
                    s.op("dve", lambda e: e.scalar_tensor_tensor(out=up[:, 15 + tg * 512:15 + (tg + 1) * 512], in0=bv_[:, :], scalar=b1T[:, m:m + 1], in1=sg[:],
                                                                 op0=ALU.add, op1=ALU.mult), reads=[bvb, sgb, b1Tb], writes=[upb])
                    if tg == 3:
                        s.op("act", lambda e: e.copy(out=ub[:, 15:15 + SEQ], in_=up[:, 15:15 + SEQ]), reads=[upb], writes=[ubb])
                    yield

            def taps_pe(m):
                ub, ubb = ubfs[m % 2]
                for ch in range(4):
                    tbk, tbkb = tapbanks[ch]
                    for j in range(NPE):
                        s.op("pe", lambda e: e.matmul(tbk[:, :], lhsT=dgt[:, j, :], rhs=ub[:, ch * 512 + j:ch * 512 + j + 512], start=(j == 0), stop=(j == NPE - 1)),
                             reads=[dgtb, ubb], writes=[tbkb])

            def taps_dve_gen(m):
                up, upb = upads[m % 2]
                acc, _ = accs[m % 2]
                accb = accbs[m % 2]
                for j in range(NPE, 31):
                    for ch in range(4):
                        src = up[:, ch * 512 + j:ch * 512 + j + 512]
                        dst = acc[:, ch * 512:(ch + 1) * 512]
                        if j == NPE:
                            s.op("dve", lambda e: e.tensor_scalar(out=dst, in0=src, scalar1=wdw[:, m, j:j + 1], scalar2=bdw[:, m:m + 1], op0=ALU.mult, op1=ALU.add),
                                 reads=[upb, wdwb, bdwb], writes=[accb[ch]])
                        else:
                            s.op("dve", lambda e: e.scalar_tensor_tensor(out=dst, in0=src, scalar=wdw[:, m, j:j + 1], in1=dst, op0=ALU.mult, op1=ALU.add),
                                 reads=[upb, wdwb, accb[ch]], writes=[accb[ch]])
                    if (j - NPE) % 5 == 4:
                        yield

            def finish(m):
                acc, _ = accs[m % 2]
                accb = accbs[m % 2]
                for ch in range(4):
                    tbk, tbkb = tapbanks[ch]
                    dst = acc[:, ch * 512:(ch + 1) * 512]
                    s.op("dve", lambda e: e.tensor_tensor(out=dst, in0=dst, in1=tbk[:, :], op=ALU.add), reads=[accb[ch], tbkb], writes=[accb[ch]])
                for g in range(NT // 4):
                    tb_, tbb = banks[4 + g % 2]
                    for j in range(4):
                        t = g * 4 + j
                        s.op("pe", lambda e: e.transpose(out=tb_[:, j * 128:(j + 1) * 128], in_=acc[:, t * 128:(t + 1) * 128], identity=k.identf[:]),
                             reads=[accb[t // 4], k.identb], writes=[tbb])
                    s.op("act", lambda e: e.copy(out=cbuf[:, g * 4:(g + 1) * 4, m * 128:(m + 1) * 128], in_=tb_[:, :].rearrange("p (a b) -> p a b", b=128)),
                         reads=[tbb], writes=[cbt[g]])

            for _ in pw1_gen(0):
                pass
            for m in range(8):
                taps_pe(m)
                gn = pw1_gen(m + 1) if m + 1 < 8 else None
                for _ in taps_dve_gen(m):
                    if gn is not None:
                        next(gn, None)
                if gn is not None:
                    for _ in gn:
                        pass
                finish(m)
            s.barrier()
        with ExitStack() as es3:
            stg = [k.sb(es3, f"cv3_stg{i}", [128, 1024], F32) for i in range(2)]
            w2, w2b = k.sb(es3, "cv3_w2", [128, 8, 1024], BF16)
            gl, glb = k.sb(es3, "cv3_gl", [128, 4, 1024], F32)
            sbfs = [k.sb(es3, f"cv3_s{i}", [128, 1024], BF16) for i in range(2)]
            sTs = [k.sb(es3, f"cv3_sT{i}", [128, 8, 128], BF16) for i in range(2)]
            outs = [k.sb(es3, f"cv3_o{i}", [128, 1024], F32) for i in range(2)]
            wv = W["conv_w_pw2"].rearrange("(kc p) n -> p kc n", p=128)
            for c in range(8):
                load_cast(k, stg, w2[:, c, :], w2b, wv[:, c, :], 1024, c)
            load_bcast(k, gl[:, 0, :], glb, W["conv_g_ln"], 1024)
            load_bcast(k, gl[:, 1, :], glb, W["conv_b_ln"], 1024)
            load_bcast(k, gl[:, 2, :], glb, W["conv_b_pw2"], 1024)
            load_bcast(k, gl[:, 3, :], glb, modv_l[0:1, 2, :], 1024)
            for t in range(NT):
                xt, xb = xts[t % 2]
                scr, scrb = scrs[t % 2]
                sbf, sbfb = sbfs[t % 2]
                sT, sTb = sTs[t % 2]
                ot, otb = outs[t % 2]
                cb = cbt[t // 4]
                c_t = cbuf[:, t, :]
                s.dma("sp", lambda e: e.dma_start(out=xt[:], in_=hin[t * 128:(t + 1) * 128, :]), reads=[hinb[t]], writes=[xb])
                s.op("act", lambda e: e.activation(out=scr[:], in_=c_t, func=AF.Identity, accum_out=st[:, 0:1]), reads=[cb], writes=[scrb, stb])
                s.op("act", lambda e: e.activation(out=scr[:], in_=c_t, func=AF.Square, accum_out=st[:, 1:2]), reads=[cb], writes=[scrb, stb])
                s.op("dve", lambda e: e.tensor_scalar(out=st[:, 2:3], in0=st[:, 0:1], scalar1=1.0 / D, scalar2=None, op0=ALU.mult), reads=[stb], writes=[stb])
                s.op("dve", lambda e: e.scalar_tensor_tensor(out=st[:, 3:4], in0=st[:, 2:3], scalar=-1.0, in1=st[:, 2:3], op0=ALU.mult, op1=ALU.mult),
                     reads=[stb], writes=[stb])
                s.op("dve", lambda e: e.scalar_tensor_tensor(out=st[:, 4:5], in0=st[:, 1:2], scalar=1.0 / D, in1=st[:, 3:4], op0=ALU.mult, op1=ALU.add),
                     reads=[stb], writes=[stb])
                s.op("act", lambda e: e.activation(out=st[:, 5:6], in_=st[:, 4:5], func=AF.Sqrt, scale=1.0, bias=k.eps[:, 0:1]), reads=[stb], writes=[stb])
                s.op("dve", lambda e: e.reciprocal(out=st[:, 5:6], in_=st[:, 5:6]), reads=[stb], writes=[stb])
                s.op("dve", lambda e: e.tensor_scalar(out=scr[:], in0=c_t, scalar1=st[:, 2:3], scalar2=st[:, 5:6], op0=ALU.subtract, op1=ALU.mult),
                     reads=[cb, stb], writes=[scrb])
                s.op("dve", lambda e: e.tensor_tensor(out=scr[:], in0=scr[:], in1=gl[:, 0, :], op=ALU.mult), reads=[scrb, glb], writes=[scrb])
                s.op("pool", lambda e: e.tensor_tensor(out=scr[:], in0=scr[:], in1=gl[:, 1, :], op=ALU.add), reads=[scrb, glb], writes=[scrb])
                s.op("act", lambda e: e.activation(out=sbf[:], in_=scr[:], func=AF.Silu), reads=[scrb], writes=[sbfb])
                bank, bb = banks[7]
                transpose_chunks(k, bank, bb, lambda c: sbf[:, c * 128:(c + 1) * 128], 8, 128, sT[:], sTb, sbfb, dst_view=True)
                for g in range(2):
                    yb, ybb = banks[g]
                    for c in range(8):
                        s.op("pe", lambda e: e.matmul(yb[:, :], lhsT=sT[:, c, :], rhs=w2[:, c, g * 512:(g + 1) * 512], start=(c == 0), stop=(c == 7)),
                             reads=[sTb, w2b], writes=[ybb])
                    s.op("dve", lambda e: e.tensor_tensor(out=scr[:, g * 512:(g + 1) * 512], in0=yb[:, :], in1=gl[:, 2, g * 512:(g + 1) * 512], op=ALU.add),
                         reads=[ybb, glb], writes=[scrb])
                s.op("pool", lambda e: e.tensor_tensor(out=scr[:], in0=scr[:], in1=gl[:, 3, :], op=ALU.mult), reads=[scrb, glb], writes=[scrb])
                s.op("pool", lambda e: e.tensor_tensor(out=ot[:], in0=scr[:], in1=xt[:], op=ALU.add), reads=[scrb, xb], writes=[otb])
                s.dma("sp", lambda e: e.dma_start(out=hout[t * 128:(t + 1) * 128, :], in_=ot[:]), reads=[otb], writes=[houtb[t]])
            s.barrier()


IN_SPECS = {
    "x": ([SEQ, D], F32), "ctx": ([CTX, D], F32), "cc": ([128, 16], F32),
    "w_ada": ([2, D, 6 * D], F32), "b_ada": ([2, 6 * D], F32), "g_norm": ([4, D], F32),
    "ident": ([128, 128], BF16), "identf": ([128, 128], F32),
    "attn_w_in": ([D, 2208], F32), "mla_w_q_up": ([384, 768], F32), "mla_w_kv_up": ([256, 1024], F32),
    "mla_g_qa": ([1, 384], F32), "mla_g_kva": ([1, 256], F32), "mla_g_q": ([1, 96], F32), "mla_g_k": ([1, 96], F32),
    "na_g_q": ([1, 64], F32), "na_g_k": ([1, 64], F32), "attn_w_out": ([D, D], F32),
    "rope": ([SEQ, 32], F32), "nabias": ([8, 128, 21, 128], F32),
    "conv_w_pw1": ([D, 2 * D], F32), "conv_b1T": ([128, 16], F32), "conv_wdwT": ([128, 8, 31], F32), "conv_bdwT": ([128, 8], F32),
    "conv_g_ln": ([1, D], F32), "conv_b_ln": ([1, D], F32), "conv_w_pw2": ([D, D], F32), "conv_b_pw2": ([1, D], F32),
    "wq0": ([D, 2048], F32), "wq1": ([D, 2048], F32), "skT0": ([128, 16, 128], F32), "skT1": ([128, 16, 128], F32),
    "u0": ([16384, D], F32), "u1": ([16384, D], F32), "v0": ([16384, D], F32), "v1": ([16384, D], F32),
}


def build_program():
    nc = bass.Bass("TRN2", target_bir_lowering=False)
    A = {n: nc.dram_tensor(n, sh, dt, kind="ExternalInput").ap() for n, (sh, dt) in IN_SPECS.items()}
    out = nc.dram_tensor("out", [SEQ, D], F32, kind="ExternalOutput").ap()
    modv = nc.dram_tensor("modv_scr", [2, 2, 6, D], F32, kind="Internal").ap()
    hs = [nc.dram_tensor(f"h_scr{i}", [SEQ, D], F32, kind="Internal").ap() for i in range(3)]
    hb = [[Buf() for _ in range(NT)] for _ in range(4)]
    uvs = [nc.dram_tensor(f"uv_scr{l}", [16384, 2 * D], BF16, kind="Internal").ap() for l in range(2)]
    with ExitStack() as es:
        k = K(nc, es)
        k.modv_buf = Buf()
        setup_consts(k, es, A["ident"], A["identf"])
        uvb = [[], []]
        k.bg = uv_cast_gen(k, [(A[f"u{l}"], A[f"v{l}"], uvs[l], uvb[l]) for l in range(2)])
        stage_ada(k, A["cc"], A["w_ada"], A["b_ada"], A["g_norm"], modv)
        stage_attn(k, A["x"], A["ctx"], modv[0], A, hs[0], hb[0])
        for _ in k.bg:
            pass
        stage_peer3(k, hs[0], hb[0], modv[0], A["wq0"], A["skT0"], uvs[0], uvb[0], hs[1], hb[1], "pra")
        stage_conv(k, hs[1], hb[1], modv[1], A, hs[2], hb[2])
        stage_peer3(k, hs[2], hb[2], modv[1], A["wq1"], A["skT1"], uvs[1], uvb[1], out, hb[3], "prb")
        k.s.barrier()
    return nc


def kernel(**inp):
    import ml_dtypes
    f = lambda a: np.ascontiguousarray(np.asarray(a, dtype=np.float32))
    nb = inp["x"].shape[0]
    shared = {
        "w_ada": f(inp["w_ada"]), "b_ada": f(inp["b_ada"]),
        "g_norm": f(np.stack([inp["g_norm1"][0], inp["g_norm2"][0], inp["g_norm1"][1], inp["g_norm2"][1]])),
        "ident": np.eye(128).astype(ml_dtypes.bfloat16), "identf": np.eye(128, dtype=np.float32),
        "attn_w_in": f(inp["attn_w_in"][0]), "mla_w_q_up": f(inp["mla_w_q_up"][0]), "mla_w_kv_up": f(inp["mla_w_kv_up"][0]),
        "mla_g_qa": f(inp["mla_g_qa"]), "mla_g_kva": f(inp["mla_g_kva"]), "mla_g_q": f(inp["mla_g_q"]), "mla_g_k": f(inp["mla_g_k"]),
        "na_g_q": f(inp["na_g_q"]), "na_g_k": f(inp["na_g_k"]), "attn_w_out": f(inp["attn_w_out"][0]),
        "rope": host_rope_table(), "nabias": host_na_bias(np.asarray(inp["na_rpb"][0], np.float32)),
        "conv_w_pw1": f(inp["conv_w_pw1"][0]), "conv_b1T": f(np.asarray(inp["conv_b_pw1"][0]).reshape(16, 128).T),
        "conv_wdwT": f(np.asarray(inp["conv_w_dw"][0]).reshape(31, 8, 128).transpose(2, 1, 0)),
        "conv_bdwT": f(np.asarray(inp["conv_b_dw"][0]).reshape(8, 128).T),
        "conv_g_ln": f(inp["conv_g_ln"]), "conv_b_ln": f(inp["conv_b_ln"]), "conv_w_pw2": f(inp["conv_w_pw2"][0]), "conv_b_pw2": f(inp["conv_b_pw2"]),
    }
    for l in range(2):
        shared[f"wq{l}"] = f(inp["peer_w_query"][l])
        shared[f"skT{l}"] = f(np.asarray(inp["peer_sub_keys"][l]).reshape(16, 128, 128).transpose(2, 0, 1))
        shared[f"u{l}"] = f(inp["peer_u"][l])
        shared[f"v{l}"] = f(inp["peer_v"][l])
    in_maps = []
    for b in range(nb):
        cc = np.zeros((128, 16), np.float32)
        cc[:, 0::2] = np.asarray(inp["c"][b], np.float32).reshape(8, 128).T
        cc[:, 1::2] = np.asarray(inp["c_ctx"], np.float32).reshape(8, 128).T
        m = dict(shared)
        m["x"] = f(inp["x"][b])
        m["ctx"] = f(inp["ctx"][b])
        m["cc"] = cc
        in_maps.append(m)
    nc = build_program()
    res = run_bass_kernel_spmd(nc, in_maps, core_ids=list(range(nb)))
    return np.stack([np.asarray(r["out"], dtype=np.float32) for r in res.results], axis=0)


def uv_cast_gen(k, tabs):
    PIECE = 1024
    for (u_tab, v_tab, uv, uvb) in tabs:
        for r0 in range(0, 16384, PIECE):
            r1 = r0 + PIECE
            b0, b1 = Buf(), Buf()
            k.s.dma("pool", lambda e: e.dma_start(out=uv[r0:r1, 0:1024], in_=u_tab[r0:r1, :]), writes=[b0])
            uvb.append(b0)
            yield
            k.s.dma("pool", lambda e: e.dma_start(out=uv[r0:r1, 1024:2048], in_=v_tab[r0:r1, :]), writes=[b1])
            uvb.append(b1)
            yield


NROW3 = 16


def stage_peer3(k, hin, hinb, modv_l, w_query, skT_d, uv, uvb, hout, houtb, tag):
    nc, s = k.nc, k.s
    banks = k.banks
    with ExitStack() as es:
        wq, wqb = k.sb(es, f"{tag}_wq", [128, 8, 2048], BF16)
        skT, skTb = k.sb(es, f"{tag}_skT", [128, 16, 128], BF16)
        ABG, ABGb = k.sb(es, f"{tag}_ABG", [128, 3, 1024], F32)
        iota_i, iota_ib = k.sb(es, f"{tag}_iotai", [128, 16], I32)
        iota, iotab = k.sb(es, f"{tag}_iota", [128, 16], F32)
        st, stb = k.sb(es, f"{tag}_st", [128, 4], F32)
        xts = [k.sb(es, f"{tag}_x{i}", [128, 1024], F32) for i in range(2)]
        scrs = [k.sb(es, f"{tag}_scr{i}", [128, 1024], F32) for i in range(1)] * 2
        hbfs = [k.sb(es, f"{tag}_hbf{i}", [128, 1024], BF16) for i in range(1)]
        hT, hTb = k.sb(es, f"{tag}_hT", [128, 8, 128], BF16)
        qbf, qbfb = k.sb(es, f"{tag}_qbf", [128, 2048], BF16)
        qT, qTb = k.sb(es, f"{tag}_qT", [128, 16, 128], BF16)
        ssb, ssbb = k.sb(es, f"{tag}_s", [128, 16, 128], F32)
        s2, _ = k.sb(es, f"{tag}_s2", [128, 16, 128], F32)
        m16, _ = k.sb(es, f"{tag}_m16", [128, 16, 16], F32)
        i16, _ = k.sb(es, f"{tag}_i16", [128, 16, 16], U32)
        i16f, i16fb = k.sb(es, f"{tag}_i16f", [128, 16, 16], F32)
        cand, candb = k.sb(es, f"{tag}_cand", [128, 8, 256], F32)
        cand2, _ = k.sb(es, f"{tag}_cand2", [128, 8, 256], F32)
        best, _ = k.sb(es, f"{tag}_best", [128, 8, 16], F32)
        pos, _ = k.sb(es, f"{tag}_pos", [128, 8, 16], U32)
        ab_i, ab_ib = k.sb(es, f"{tag}_abi", [128, 2, 128], I32)
        ab_f, ab_fb = k.sb(es, f"{tag}_abf", [128, 2, 128], F32)
        oh, ohb = k.sb(es, f"{tag}_oh", [128, 8, 16, 16], F32)
        e01, e01b = k.sb(es, f"{tag}_e01", [128, 2, 128], F32)
        idxs = [k.sb(es, f"{tag}_idx{i}", [128, 128], I32) for i in range(2)]
        gts = [k.sb(es, f"{tag}_gate{i}", [128, 8, 16], F32) for i in range(2)]
        gsum, gsumb = k.sb(es, f"{tag}_gsum", [128, 8], F32)
        actv, _ = k.sb(es, f"{tag}_act", [128, 128], F32)
        wgt, _ = k.sb(es, f"{tag}_wgt", [128, 128], F32)
        junks = [k.sb(es, f"{tag}_junk{i}", [128, 1024], BF16) for i in range(3)]
        rows = [k.sb(es, f"{tag}_row{i}", [128, 2048], BF16) for i in range(NROW3)]
        dgs = [k.sb(es, f"{tag}_dg{i}", [128, 128], BF16) for i in range(4)]
        ot, otb = k.sb(es, f"{tag}_ot", [128, 1024], F32)
        hpb = [[Buf() for _ in range(16)] for _ in range(3)]
        hb = [[Buf() for _ in range(8)] for _ in range(3)]
        actb = [Buf() for _ in range(16)]
        wgb = [Buf() for _ in range(4)]

        wqv = w_query.rearrange("(kc p) n -> p kc n", p=128)
        for c in range(8):
            s.dma("pool", lambda e: e.dma_start(out=wq[:, c, :], in_=wqv[:, c, :]), writes=[wqb])
        s.dma("pool", lambda e: e.dma_start(out=skT[:].rearrange("p a b -> p (a b)"), in_=skT_d.rearrange("p a b -> p (a b)")), writes=[skTb])
        for j in range(3):
            load_bcast(k, ABG[:, j, :], ABGb, modv_l[0:1, 3 + j, :], 1024)
        s.op("pool", lambda e: e.iota(out=iota_i[:], pattern=[[1, 16]], base=0, channel_multiplier=0), writes=[iota_ib])
        s.op("dve", lambda e: e.tensor_copy(out=iota[:], in_=iota_i[:]), reads=[iota_ib], writes=[iotab])

        def front(t):
            xt, xb = xts[t % 2]
            scr, scrb = scrs[t % 2]
            idx, idxb = idxs[t % 2]
            gate, gateb = gts[t % 2]
            hbf, hbfb = hbfs[0]
            s.dma("sp", lambda e: e.dma_start(out=xt[:], in_=hin[t * 128:(t + 1) * 128, :]), reads=[hinb[t]], writes=[xb])
            yield
            s.op("act", lambda e: e.activation(out=scr[:], in_=xt[:], func=AF.Square, accum_out=st[:, 0:1]), reads=[xb], writes=[scrb, stb])
            s.op("act", lambda e: e.activation(out=st[:, 0:1], in_=st[:, 0:1], func=AF.Sqrt, scale=1.0 / D, bias=k.eps[:, 0:1]), reads=[stb], writes=[stb])
            yield
            s.op("dve", lambda e: e.reciprocal(out=st[:, 0:1], in_=st[:, 0:1]), reads=[stb], writes=[stb])
            s.op("dve", lambda e: e.scalar_tensor_tensor(out=scr[:], in0=xt[:], scalar=st[:, 0:1], in1=ABG[:, 0, :], op0=ALU.mult, op1=ALU.mult),
                 reads=[xb, stb, ABGb], writes=[scrb])
            yield
            s.op("pool", lambda e: e.tensor_tensor(out=hbf[:], in0=scr[:], in1=ABG[:, 1, :], op=ALU.add), reads=[scrb, ABGb], writes=[hbfb])
            yield
            hmp, hmpb = banks[4 + t % 2]
            bank, bb = banks[2]
            bv = bank[:].bitcast(BF16)
            for c in range(8):
                s.op("pe", lambda e: e.transpose(out=bv[:, c * 128:(c + 1) * 128], in_=hbf[:, c * 128:(c + 1) * 128], identity=k.ident[:]),
                     reads=[hbfb, k.identb], writes=[bb])
            yield
            s.op("act", lambda e: e.copy(out=hT[:], in_=bv[:, :].rearrange("p (c t) -> p c t", t=128)), reads=[bb], writes=[hTb])
            yield
            hmv_ = hmp[:].bitcast(BF16)
            for c in range(8):
                s.op("pe", lambda e: e.transpose(out=hmv_[:, c * 128:(c + 1) * 128], in_=hT[:, c, :], identity=k.ident[:]),
                     reads=[hTb, k.identb], writes=[hmpb])
            for g in range(5):
                if g < 4:
                    qb_, qbb = banks[g % 2]
                    for c in range(8):
                        s.op("pe", lambda e: e.matmul(qb_[:, :], lhsT=hT[:, c, :], rhs=wq[:, c, g * 512:(g + 1) * 512], start=(c == 0), stop=(c == 7)),
                             reads=[hTb, wqb], writes=[qbb])
                if g > 0:
                    g1 = g - 1
                    qb1, qbb1 = banks[g1 % 2]
                    s.op("act", lambda e: e.copy(out=qbf[:, g1 * 512:(g1 + 1) * 512], in_=qb1[:, :]), reads=[qbb1], writes=[qbfb])
                yield
            for half in range(3):
                if half < 2:
                    tb_, tbb = banks[2 + half]
                    bv2 = tb_[:].bitcast(BF16)
                    for c in range(8):
                        s.op("pe", lambda e: e.transpose(out=bv2[:, c * 128:(c + 1) * 128], in_=qbf[:, (half * 8 + c) * 128:(half * 8 + c + 1) * 128], identity=k.ident[:]),
                             reads=[qbfb, k.identb], writes=[tbb])
                if half > 0:
                    h1 = half - 1
                    tb1, tbb1 = banks[2 + h1]
                    s.op("act", lambda e: e.copy(out=qT[:, h1 * 8:(h1 + 1) * 8, :], in_=tb1[:].bitcast(BF16)[:, :].rearrange("p (c t) -> p c t", t=128)),
                         reads=[tbb1], writes=[qTb])
                yield
            for g in range(5):
                if g < 4:
                    sb_, sbb = banks[g % 2]
                    for j in range(4):
                        hp = g * 4 + j
                        s.op("pe", lambda e: e.matmul(sb_[:, j * 128:(j + 1) * 128], lhsT=qT[:, hp, :], rhs=skT[:, hp, :], start=True, stop=True),
                             reads=[qTb, skTb], writes=[sbb])
                if g > 0:
                    g1 = g - 1
                    sb1, sbb1 = banks[g1 % 2]
                    s.op("act", lambda e: e.copy(out=ssb[:, g1 * 4:(g1 + 1) * 4, :], in_=sb1[:, :].rearrange("p (a b) -> p a b", b=128)), reads=[sbb1], writes=[ssbb])
                if g % 2 == 1:
                    yield
            yield
            for hp in range(16):
                s.op("dve", lambda e: e.max(out=m16[:, hp, 0:8], in_=ssb[:, hp, :]), reads=[ssbb], writes=[hpb[0][hp]])
            yield
            for hp in range(16):
                s.op("dve", lambda e: e.max_index(out=i16[:, hp, 0:8], in_max=m16[:, hp, 0:8], in_values=ssb[:, hp, :]),
                     reads=[ssbb, hpb[0][hp]], writes=[hpb[1][hp]])
            yield
            for hp in range(16):
                s.op("dve", lambda e: e.match_replace(out=s2[:, hp, :], in_to_replace=m16[:, hp, 0:8], in_values=ssb[:, hp, :], imm_value=-1e30),
                     reads=[ssbb, hpb[0][hp]], writes=[hpb[2][hp]])
            yield
            for hp in range(16):
                s.op("dve", lambda e: e.max(out=m16[:, hp, 8:16], in_=s2[:, hp, :]), reads=[hpb[2][hp]], writes=[hpb[0][hp]])
            yield
            for hp in range(16):
                s.op("dve", lambda e: e.max_index(out=i16[:, hp, 8:16], in_max=m16[:, hp, 8:16], in_values=s2[:, hp, :]),
                     reads=[hpb[2][hp], hpb[0][hp]], writes=[hpb[1][hp]])
            yield
            s.op("dve", lambda e: e.tensor_copy(out=i16f[:], in_=i16[:]), reads=hpb[1], writes=[i16fb])
            m4 = m16[:].rearrange("p (h t) a -> p h t a", t=2)
            s.op("dve", lambda e: e.tensor_tensor(out=cand[:].rearrange("p h (a b) -> p h a b", b=16),
                                                  in0=m4[:, :, 0, :].unsqueeze(3).to_broadcast([128, 8, 16, 16]),
                                                  in1=m4[:, :, 1, :].unsqueeze(2).to_broadcast([128, 8, 16, 16]), op=ALU.add),
                 reads=hpb[0], writes=[candb])
            yield
            for h in range(8):
                s.op("dve", lambda e: e.max(out=best[:, h, 0:8], in_=cand[:, h, :]), reads=[candb], writes=[hb[0][h]])
            for h in range(8):
                s.op("dve", lambda e: e.max_index(out=pos[:, h, 0:8], in_max=best[:, h, 0:8], in_values=cand[:, h, :]),
                     reads=[candb, hb[0][h]], writes=[hb[1][h]])
            yield
            for h in range(8):
                s.op("dve", lambda e: e.match_replace(out=cand2[:, h, :], in_to_replace=best[:, h, 0:8], in_values=cand[:, h, :], imm_value=-1e30),
                     reads=[candb, hb[0][h]], writes=[hb[2][h]])
            for h in range(8):
                s.op("dve", lambda e: e.max(out=best[:, h, 8:16], in_=cand2[:, h, :]), reads=[hb[2][h]], writes=[hb[0][h]])
            yield
            for h in range(8):
                s.op("dve", lambda e: e.max_index(out=pos[:, h, 8:16], in_max=best[:, h, 8:16], in_values=cand2[:, h, :]),
                     reads=[hb[2][h], hb[0][h]], writes=[hb[1][h]])
            posi = pos[:].rearrange("p h k -> p (h k)").bitcast(I32)
            s.op("dve", lambda e: e.tensor_single_scalar(out=ab_i[:, 0, :], in_=posi, scalar=4, op=ALU.arith_shift_right), reads=hb[1], writes=[ab_ib])
            s.op("dve", lambda e: e.tensor_single_scalar(out=ab_i[:, 1, :], in_=posi, scalar=15, op=ALU.bitwise_and), reads=hb[1], writes=[ab_ib])
            s.op("dve", lambda e: e.tensor_copy(out=ab_f[:], in_=ab_i[:]), reads=[ab_ib], writes=[ab_fb])
            s.op("dve", lambda e: e.tensor_tensor(out=gate[:], in0=best[:], in1=best[:, :, 0:1].to_broadcast([128, 8, 16]), op=ALU.subtract),
                 reads=hb[0], writes=[gateb])
            yield
            s.op("act", lambda e: e.activation(out=gate[:], in_=gate[:], func=AF.Exp), reads=[gateb], writes=[gateb])
            i4 = i16f[:].rearrange("p (h t) a -> p h t a", t=2)
            for p_ in range(2):
                s.op("dve", lambda e: e.tensor_tensor(out=oh[:], in0=ab_f[:, p_, :].rearrange("p (h k) -> p h k", k=16).unsqueeze(3).to_broadcast([128, 8, 16, 16]),
                                                      in1=iota[:].unsqueeze(1).unsqueeze(1).to_broadcast([128, 8, 16, 16]), op=ALU.is_equal),
                     reads=[ab_fb, iotab], writes=[ohb])
                yield
                s.op("dve", lambda e: e.tensor_tensor(out=oh[:], in0=oh[:], in1=i4[:, :, p_, :].unsqueeze(2).to_broadcast([128, 8, 16, 16]), op=ALU.mult),
                     reads=[ohb, i16fb], writes=[ohb])
                yield
                s.op("dve", lambda e: e.tensor_reduce(out=e01[:, p_, :].rearrange("p (h k) -> p h k", k=16), in_=oh[:], axis=AX.X, op=ALU.add),
                     reads=[ohb], writes=[e01b])
                yield
            s.op("dve", lambda e: e.scalar_tensor_tensor(out=e01[:, 0, :], in0=e01[:, 0, :], scalar=128.0, in1=e01[:, 1, :], op0=ALU.mult, op1=ALU.add),
                 reads=[e01b], writes=[e01b])
            s.op("dve", lambda e: e.tensor_copy(out=idx[:], in_=e01[:, 0, :]), reads=[e01b], writes=[idxb])
            s.op("dve", lambda e: e.tensor_reduce(out=gsum[:], in_=gate[:], axis=AX.X, op=ALU.add), reads=[gateb], writes=[gsumb])
            s.op("dve", lambda e: e.reciprocal(out=gsum[:], in_=gsum[:]), reads=[gsumb], writes=[gsumb])
            s.op("dve", lambda e: e.tensor_tensor(out=gate[:], in0=gate[:], in1=gsum[:].unsqueeze(2).to_broadcast([128, 8, 16]), op=ALU.mult),
                 reads=[gateb, gsumb], writes=[gateb])

        ring = [0]

        glv, _ = k.sb(es, f"{tag}_glv", [128, 128], F32)
        glb = [Buf() for _ in range(16)]
        wgb16 = [Buf() for _ in range(16)]

        def back(t, fg):
            xt, xb = xts[t % 2]
            scr, scrb = scrs[t % 2]
            idx, idxb = idxs[t % 2]
            gate, gateb = gts[t % 2]
            hmp, hmpb = banks[4 + t % 2]
            hmv = hmp[:].bitcast(BF16)
            gflat = gate[:].rearrange("p h k -> p (h k)")
            (o0, o0b), (o1, o1b) = banks[6], banks[7]
            held = {}

            def st_a(hk):
                s.op("act", lambda e: e.activation(out=glv[:, hk:hk + 1], in_=actv[:, hk:hk + 1], func=AF.Gelu), reads=[actb[hk % 16]], writes=[glb[hk % 16]])

            def st_b(hk):
                rw, rwb = held.pop(hk)
                dg, dgb = dgs[hk % 4]
                s.op("act", lambda e: e.activation(out=wgt[:, hk:hk + 1], in_=glv[:, hk:hk + 1], func=AF.Copy, scale=gflat[:, hk:hk + 1]),
                     reads=[glb[hk % 16], gateb], writes=[wgb16[hk % 16]])
                s.op("act", lambda e: e.activation(out=dg[:], in_=k.ident[:], func=AF.Copy, scale=wgt[:, hk:hk + 1]),
                     reads=[k.identb, wgb16[hk % 16]], writes=[dgb])
                s.op("pe", lambda e: e.matmul(o0[:, :], lhsT=dg[:], rhs=rw[:, 1024:1536], start=(hk == 0), stop=(hk == 127)),
                     reads=[dgb, rwb], writes=[o0b])
                s.op("pe", lambda e: e.matmul(o1[:, :], lhsT=dg[:], rhs=rw[:, 1536:2048], start=(hk == 0), stop=(hk == 127)),
                     reads=[dgb, rwb], writes=[o1b])

            for hk in range(128 + 3):
                if hk < 128:
                    rw, rwb = rows[ring[0] % NROW3]
                    ring[0] += 1
                    s.dma("pool", lambda e: e.indirect_dma_start(out=rw[:], out_offset=None, in_=uv,
                                                                 in_offset=bass.IndirectOffsetOnAxis(ap=idx[:, hk:hk + 1], axis=0)),
                          reads=[idxb] + uvb, writes=[rwb])
                    junk, junkb = junks[hk % 3]
                    s.op("dve", lambda e: e.scalar_tensor_tensor(out=junk[:], in0=rw[:, 0:1024], scalar=1.0, in1=hmv, op0=ALU.mult, op1=ALU.mult,
                                                                 accum_out=actv[:, hk:hk + 1]), reads=[rwb, hmpb], writes=[actb[hk % 16], junkb])
                    held[hk] = (rw, rwb)
                if 0 <= hk - 1 < 128:
                    st_a(hk - 1)
                if 0 <= hk - 3 < 128:
                    st_b(hk - 3)
                if fg is not None and hk % 4 == 3:
                    next(fg, None)
            s.op("dve", lambda e: e.tensor_tensor(out=scr[:, 0:512], in0=o0[:, :], in1=ABG[:, 2, 0:512], op=ALU.mult), reads=[o0b, ABGb], writes=[scrb])
            s.op("dve", lambda e: e.tensor_tensor(out=scr[:, 512:1024], in0=o1[:, :], in1=ABG[:, 2, 512:1024], op=ALU.mult), reads=[o1b, ABGb], writes=[scrb])
            s.op("pool", lambda e: e.tensor_tensor(out=ot[:], in0=scr[:], in1=xt[:], op=ALU.add), reads=[scrb, xb], writes=[otb])
            s.dma("sp", lambda e: e.dma_start(out=hout[t * 128:(t + 1) * 128, :], in_=ot[:]), reads=[otb], writes=[houtb[t]])

        for _ in front(0):
            pass
        for t in range(NT):
            fg = front(t + 1) if t + 1 < NT else None
            back(t, fg)
            if fg is not None:
                for _ in fg:
                    pass
        s.barrier()
```

```python
import numpy as np
from contextlib import ExitStack
import concourse.bass as bass
import concourse.mybir as mybir
from concourse.bass_utils import run_bass_kernel_spmd

F32 = mybir.dt.float32
BF16 = mybir.dt.bfloat16
I32 = mybir.dt.int32
U32 = mybir.dt.uint32
ALU = mybir.AluOpType
AF = mybir.ActivationFunctionType
AX = mybir.AxisListType

D = 1024
SEQ = 2048
NT = SEQ // 128
CTX = 256
NCT = CTX // 128
EPS = 1e-6
NEG = -30000.0


class Buf:
    __slots__ = ("w", "r")

    def __init__(self):
        self.w = None
        self.r = {}


class Sched:
    RING = 12

    def __init__(self, nc, es):
        self.nc = nc
        self.eng = {"pe": nc.tensor, "act": nc.scalar, "dve": nc.vector, "pool": nc.gpsimd, "sp": nc.sync}
        self.semobj = {}
        self.cnt = {}
        for k in self.eng:
            self.semobj[k] = es.enter_context(nc.semaphore("s_" + k))
            self.cnt[k] = 0
        self.waited = {k: {} for k in self.eng}
        self.bulk = []
        self.dq = {}
        for q in ("sp", "pool", "act"):
            slots = []
            for i in range(self.RING):
                key = ("d", q, i)
                self.semobj[key] = es.enter_context(nc.semaphore(f"d_{q}_{i}"))
                slots.append(key)
            self.dq[q] = {"slots": slots, "uses": [0] * self.RING, "next": 0}

    def _wait(self, ek, tok):
        if tok is None:
            return
        sk, v = tok
        if ek == "pe" and sk == "pe":
            return
        if self.waited[ek].get(sk, 0) >= v:
            return
        self.eng[ek].wait_ge(self.semobj[sk], v)
        self.waited[ek][sk] = v

    def _deps(self, ek, reads, writes):
        for b in reads:
            self._wait(ek, b.w)
        for b in writes:
            self._wait(ek, b.w)
            for sk, v in b.r.items():
                self._wait(ek, (sk, v))

    def _mark(self, tok, reads, writes):
        sk, v = tok
        for b in reads:
            if b.r.get(sk, 0) < v:
                b.r[sk] = v
        for b in writes:
            b.w = tok
            b.r = {}

    def op(self, ek, fn, reads=(), writes=()):
        self._deps(ek, reads, writes)
        ins = fn(self.eng[ek])
        self.cnt[ek] += 1
        ins.then_inc(self.semobj[ek], 1)
        tok = (ek, self.cnt[ek])
        self._mark(tok, reads, writes)
        return tok

    def dma(self, q, fn, reads=(), writes=()):
        dq = self.dq[q]
        slot = dq["next"]
        dq["next"] = (slot + 1) % self.RING
        key = dq["slots"][slot]
        uses = dq["uses"][slot]
        if uses:
            self._wait(q, (key, 16 * uses))
        self._deps(q, reads, writes)
        ins = fn(self.eng[q])
        ins.then_inc(self.semobj[key], 16)
        dq["uses"][slot] = uses + 1
        tok = (key, 16 * (uses + 1))
        self._mark(tok, reads, writes)
        return tok

    def bulk_dma(self, q, fn, reads=(), writes=(), es=None):
        key = ("bulk", len(self.semobj))
        self.semobj[key] = es.enter_context(self.nc.semaphore(f"bulk{len(self.semobj)}"))
        self._deps(q, reads, writes)
        ins = fn(self.eng[q])
        ins.then_inc(self.semobj[key], 16)
        tok = (key, 16)
        self.bulk.append(tok)
        self._mark(tok, reads, writes)
        return tok

    def barrier(self):
        toks = [(k, self.cnt[k]) for k in self.eng if self.cnt[k]]
        for q, dq in self.dq.items():
            for key, u in zip(dq["slots"], dq["uses"]):
                if u:
                    toks.append((key, 16 * u))
        toks.extend(self.bulk)
        for ek in self.eng:
            for t in toks:
                self._wait(ek, t)


class K:
    def __init__(self, nc, es):
        self.nc = nc
        self.es = es
        self.s = Sched(nc, es)
        self.banks = []
        for i in range(8):
            t = es.enter_context(nc.psum_tensor(f"bank{i}", [128, 512], F32))
            self.banks.append((t, Buf()))
        self.ident = None

    def sb(self, es, name, shape, dt):
        t = es.enter_context(self.nc.sbuf_tensor(name, list(shape), dt))
        return t, Buf()

    def poke(self):
        bg = getattr(self, "bg", None)
        if bg is not None:
            next(bg, None)


def bcast_row(ap_row, parts):
    return ap_row.partition_broadcast(parts) if len(ap_row.shape) == 1 else ap_row.to_broadcast([parts, ap_row.shape[-1]])


def stage_ada(k, cc, w_ada, b_ada, g_norm, modv):
    nc, s = k.nc, k.s
    with ExitStack() as es:
        cct, ccb = k.sb(es, "ada_cc", [128, 16], F32)
        sil, silb = k.sb(es, "ada_sil", [128, 16], F32)
        wt = [k.sb(es, f"ada_w{i}", [128, 8, 512], F32) for i in range(3)]
        brow, browb = k.sb(es, "ada_b", [2, 6144], F32)
        grow, growb = k.sb(es, "ada_g", [2, 2, 1024], F32)
        mrow, mrowb = k.sb(es, "ada_m", [2, 6144], F32)
        orow, orowb = k.sb(es, "ada_o", [2, 6, 1024], F32)

        s.dma("sp", lambda e: e.dma_start(out=cct[:], in_=cc), writes=[ccb])
        s.op("act", lambda e: e.activation(out=sil[:], in_=cct[:], func=AF.Silu), reads=[ccb], writes=[silb])
        for l in range(2):
            s.dma("sp", lambda e: e.dma_start(out=brow[:], in_=b_ada[l:l + 1, :].to_broadcast([2, 6144])), writes=[browb])
            s.dma("sp", lambda e: e.dma_start(out=grow[:], in_=g_norm[2 * l:2 * l + 2, :].rearrange("(o a) d -> o a d", o=1).to_broadcast([2, 2, 1024])), writes=[growb])
            wv = w_ada[l].rearrange("(kc p) n -> p kc n", p=128)
            for g in range(12):
                wtile, wbuf = wt[g % 3]
                q = ("sp", "act", "pool")[g % 3]
                s.dma(q, lambda e: e.dma_start(out=wtile[:], in_=wv[:, :, g * 512:(g + 1) * 512]), writes=[wbuf])
                bank, bb = k.banks[g % 2]
                for kc in range(8):
                    s.op("pe", lambda e: e.matmul(bank[0:2, :], lhsT=sil[:, 2 * kc:2 * kc + 2], rhs=wtile[:, kc, :],
                                                  start=(kc == 0), stop=(kc == 7)),
                         reads=[silb, wbuf], writes=[bb])
                s.op("dve", lambda e: e.tensor_tensor(out=mrow[:, g * 512:(g + 1) * 512], in0=bank[0:2, :],
                                                      in1=brow[:, g * 512:(g + 1) * 512], op=ALU.add),
                     reads=[bb, browb], writes=[mrowb])
            for j in range(2):
                sh = mrow[:, (3 * j) * 1024:(3 * j + 1) * 1024]
                sc = mrow[:, (3 * j + 1) * 1024:(3 * j + 2) * 1024]
                gt = mrow[:, (3 * j + 2) * 1024:(3 * j + 3) * 1024]
                s.op("dve", lambda e: e.scalar_tensor_tensor(out=orow[:, 3 * j, :], in0=sc, scalar=1.0, in1=grow[:, j, :],
                                                             op0=ALU.add, op1=ALU.mult),
                     reads=[mrowb, growb], writes=[orowb])
                s.op("dve", lambda e: e.tensor_copy(out=orow[:, 3 * j + 1, :], in_=sh), reads=[mrowb], writes=[orowb])
                s.op("dve", lambda e: e.tensor_copy(out=orow[:, 3 * j + 2, :], in_=gt), reads=[mrowb], writes=[orowb])
            s.dma("sp", lambda e: e.dma_start(out=modv[l], in_=orow[:]), reads=[orowb], writes=[k.modv_buf])
        s.barrier()


def load_cast(k, stg, dst, dstb, src, n, qi=0):
    s = k.s
    st, stb = stg[qi % len(stg)]
    s.dma("sp" if qi % 2 == 0 else "pool", lambda e: e.dma_start(out=st[:, 0:n], in_=src), writes=[stb])
    ek = ("act", "pool", "dve")[qi % 3]
    if ek == "act":
        s.op("act", lambda e: e.copy(out=dst, in_=st[:, 0:n]), reads=[stb], writes=[dstb])
    else:
        s.op(ek, lambda e: e.tensor_copy(out=dst, in_=st[:, 0:n]), reads=[stb], writes=[dstb])


def load_w_bf16(k, stg, wt, wb, wdram, kc, n, q0=0):
    qi = q0
    v = wdram.rearrange("(kc p) n -> p kc n", p=128)
    cw = stg[0][0].shape[1]
    for c in range(kc):
        for c0 in range(0, n, cw):
            c1 = min(n, c0 + cw)
            load_cast(k, stg, wt[:, c, c0:c1], wb, v[:, c, c0:c1], c1 - c0, qi)
            qi += 1
    return qi


def rstd_from_ss(k, ss, ssb, rs, rsb, inv_n, w):
    s = k.s
    s.op("act", lambda e: e.activation(out=rs[:, 0:w], in_=ss[:, 0:w], func=AF.Sqrt, scale=inv_n, bias=k.eps[:, 0:1]),
         reads=[ssb], writes=[rsb])
    s.op("dve", lambda e: e.reciprocal(out=rs[:, 0:w], in_=rs[:, 0:w]), reads=[rsb], writes=[rsb])


def norm_mod(k, xt, xb, A, B, ABb, scr, scrb, st, stb, out, outb, out_f32=None):
    s = k.s
    s.op("act", lambda e: e.activation(out=scr[:], in_=xt[:], func=AF.Square, accum_out=st[:, 0:1]),
         reads=[xb], writes=[scrb, stb])
    rstd_from_ss(k, st, stb, st, stb, 1.0 / D, 1)
    s.op("dve", lambda e: e.scalar_tensor_tensor(out=scr[:], in0=xt[:], scalar=st[:, 0:1], in1=A, op0=ALU.mult, op1=ALU.mult),
         reads=[xb, stb, ABb], writes=[scrb])
    if out_f32 is not None:
        of, ofb = out_f32
        s.op("pool", lambda e: e.tensor_tensor(out=of[:], in0=scr[:], in1=B, op=ALU.add), reads=[scrb, ABb], writes=[ofb])
        s.op("act", lambda e: e.copy(out=out[:], in_=of[:]), reads=[ofb], writes=[outb])
    else:
        s.op("pool", lambda e: e.tensor_tensor(out=out[:], in0=scr[:], in1=B, op=ALU.add), reads=[scrb, ABb], writes=[outb])


def transpose_chunks(k, bank, bankb, src_fn, nchunks, rows, dst, dstb, srcb, dst_view=None):
    s = k.s
    bv = bank[:].bitcast(BF16)
    for c in range(nchunks):
        s.op("pe", lambda e: e.transpose(out=bv[0:rows, c * 128:(c + 1) * 128], in_=src_fn(c), identity=k.ident[:]),
             reads=[srcb, k.identb], writes=[bankb])
    src = bv[0:rows, 0:nchunks * 128]
    if dst_view is not None:
        src = src.rearrange("p (c t) -> p c t", t=128)
    s.op("act", lambda e: e.copy(out=dst, in_=src), reads=[bankb], writes=[dstb])


def setup_consts(k, es, ident_d, identf_d=None):
    s = k.s
    k.ident, k.identb = k.sb(es, "ident_sb", [128, 128], BF16)
    k.eps, k.epsb = k.sb(es, "epsc", [128, 1], F32)
    s.dma("sp", lambda e: e.dma_start(out=k.ident[:], in_=ident_d), writes=[k.identb])
    if identf_d is not None:
        k.identf, _ = k.sb(es, "identf_sb", [128, 128], F32)
        s.dma("sp", lambda e: e.dma_start(out=k.identf[:], in_=identf_d), writes=[k.identb])
    s.op("dve", lambda e: e.memset(k.eps[:], EPS), writes=[k.epsb])


def load_bcast(k, tile, tb, row, n):
    k.s.dma("sp", lambda e: e.dma_start(out=tile, in_=row.to_broadcast([128, n])), writes=[tb])


def na_blocks(qt):
    if 2 <= qt <= 13:
        return [(qt - 2 + j, j) for j in range(5)]
    if qt == 0:
        return [(j, 5 + j) for j in range(4)]
    if qt == 1:
        return [(j, 9 + j) for j in range(4)]
    if qt == 14:
        return [(12 + j, 13 + j) for j in range(4)]
    return [(12 + j, 17 + j) for j in range(4)]


def run_pipelined(gens, lag, k=None):
    gens = list(gens)
    active = []
    nxt = 0
    since = lag
    while active or nxt < len(gens):
        if nxt < len(gens) and since >= lag:
            active.append(gens[nxt])
            nxt += 1
            since = 0
        for g in list(active):
            try:
                next(g)
            except StopIteration:
                active.remove(g)
        since += 1
        if k is not None:
            k.poke()


def stage_attn(k, x, ctx, modv0, W, hout, houtb):
    nc, s = k.nc, k.s
    banks = k.banks
    with ExitStack() as es:
        AB, ABb = k.sb(es, "at_AB", [128, 4, 1024], F32)
        osb, osbb = k.sb(es, "at_o", [128, NT, 1024], BF16)
        qT, qTb = k.sb(es, "at_qT", [96, 8, SEQ], BF16)
        kT, kTb = k.sb(es, "at_kT", [96, 8, SEQ + CTX], BF16)
        Vs, Vsb = k.sb(es, "at_V", [128, NT + NCT, 8, 65], BF16)
        st, stb = k.sb(es, "at_st", [128, 4], F32)
        st2, st2b = k.sb(es, "at_st2", [128, 16], F32)
        st3, st3b = k.sb(es, "at_st3", [128, 16], F32)
        xts = [k.sb(es, f"at_x{i}", [128, 1024], F32) for i in range(2)]
        scrs = [k.sb(es, f"at_scr{i}", [128, 1024], F32) for i in range(2)]
        abfs = [k.sb(es, f"at_a{i}", [128, 1024], BF16) for i in range(2)]
        aTs = [k.sb(es, f"at_aT{i}", [128, 8, 128], BF16) for i in range(2)]

        load_bcast(k, AB[:, 0, :], ABb, modv0[0:1, 0, :], 1024)
        load_bcast(k, AB[:, 1, :], ABb, modv0[0:1, 1, :], 1024)
        load_bcast(k, AB[:, 2, :], ABb, modv0[1:2, 0, :], 1024)
        load_bcast(k, AB[:, 3, :], ABb, modv0[1:2, 1, :], 1024)
        s.op("pool", lambda e: e.memset(Vs[:, :, :, 64:65], 1.0), writes=[Vsb])

        def src_tile(t):
            return ctx[t * 128:(t + 1) * 128, :] if t < NCT else x[(t - NCT) * 128:(t - NCT + 1) * 128, :]

        def front(t):
            xt, xb = xts[t % 2]
            scr, scrb = scrs[t % 2]
            abf, abfb = abfs[t % 2]
            aT, aTb = aTs[t % 2]
            s.dma("sp", lambda e: e.dma_start(out=xt[:], in_=src_tile(t)), writes=[xb])
            j = 2 if t < NCT else 0
            norm_mod(k, xt, xb, AB[:, j, :], AB[:, j + 1, :], ABb, scr, scrb, st, stb, abf, abfb)
            bank, bb = banks[7]
            transpose_chunks(k, bank, bb, lambda c: abf[:, c * 128:(c + 1) * 128], 8, 128, aT[:], aTb, abfb, dst_view=True)
            return aT, aTb, scr, scrb

        with ExitStack() as es1:
            w_in, w_inb = k.sb(es1, "p1_win", [128, 8, 672], BF16)
            w_q, w_qb = k.sb(es1, "p1_wq", [128, 3, 768], BF16)
            w_kv, w_kvb = k.sb(es1, "p1_wkv", [128, 2, 1024], BF16)
            gcn, gcnb = k.sb(es1, "p1_gcn", [128, 640], F32)
            gq, gqb = k.sb(es1, "p1_gq", [128, 96], F32)
            gk, gkb = k.sb(es1, "p1_gk", [128, 96], F32)
            zsbs = [k.sb(es1, f"p1_z{i}", [128, 672], F32) for i in range(2)]
            cn, cnb = k.sb(es1, "p1_cn", [128, 640], BF16)
            cnT, cnTb = k.sb(es1, "p1_cnT", [128, 5, 128], BF16)
            qn, qnb = k.sb(es1, "p1_qn", [128, 8, 96], F32)
            kn, knb = k.sb(es1, "p1_kn", [128, 8, 64], F32)
            qr, qrb = k.sb(es1, "p1_qr", [128, 8, 32], F32)
            rt, rtb = k.sb(es1, "p1_rt", [128, 4, 8, 16], F32)
            krg, krgb = k.sb(es1, "p1_krg", [128, 32], F32)
            krr, krrb = k.sb(es1, "p1_krr", [128, 32], F32)
            kt4, kt4b = k.sb(es1, "p1_kt4", [128, 4, 16], F32)
            qf, qfb = k.sb(es1, "p1_qf", [128, 8, 96], BF16)
            kf, kfb = k.sb(es1, "p1_kf", [128, 8, 96], BF16)
            ropes = [k.sb(es1, f"p1_rope{i}", [128, 32], F32) for i in range(2)]

            wv = W["attn_w_in"].rearrange("(kc p) n -> p kc n", p=128)
            for c in range(8):
                s.dma("pool", lambda e: e.dma_start(out=w_in[:, c, :], in_=wv[:, c, 0:672]), writes=[w_inb])
            wv = W["mla_w_q_up"].rearrange("(kc p) n -> p kc n", p=128)
            for c in range(3):
                s.dma("pool", lambda e: e.dma_start(out=w_q[:, c, :], in_=wv[:, c, :]), writes=[w_qb])
            wv = W["mla_w_kv_up"].rearrange("(kc p) n -> p kc n", p=128)
            for c in range(2):
                s.dma("pool", lambda e: e.dma_start(out=w_kv[:, c, :], in_=wv[:, c, :]), writes=[w_kvb])
            load_bcast(k, gcn[:, 0:384], gcnb, W["mla_g_qa"], 384)
            load_bcast(k, gcn[:, 384:640], gcnb, W["mla_g_kva"], 256)
            load_bcast(k, gq[:], gqb, W["mla_g_q"], 96)
            load_bcast(k, gk[:], gkb, W["mla_g_k"], 96)
            s.op("dve", lambda e: e.tensor_scalar_mul(out=gq[:], in0=gq[:], scalar1=96.0 ** -0.5), reads=[gqb], writes=[gqb])

            def p1_tile(t):
                lat = t >= NCT
                zsb, zsbb = zsbs[t % 2]
                aT, aTb, scr, scrb = front(t)
                if lat:
                    rp, rpb = ropes[t % 2]
                    s.dma("sp", lambda e: e.dma_start(out=rp[:], in_=W["rope"][(t - NCT) * 128:(t - NCT + 1) * 128, :]), writes=[rpb])
                yield
                (z0, z0b), (z1, z1b) = banks[0], banks[1]
                for (zb, zbb, c0, c1) in ((z0, z0b, 0, 512), (z1, z1b, 512, 672)):
                    for c in range(8):
                        s.op("pe", lambda e: e.matmul(zb[:, 0:c1 - c0], lhsT=aT[:, c, :], rhs=w_in[:, c, c0:c1], start=(c == 0), stop=(c == 7)),
                             reads=[aTb, w_inb], writes=[zbb])
                    s.op("act", lambda e: e.copy(out=zsb[:, c0:c1], in_=zb[:, 0:c1 - c0]), reads=[zbb], writes=[zsbb])
                yield
                if lat:
                    s.op("act", lambda e: e.activation(out=scr[:, 0:384], in_=zsb[:, 0:384], func=AF.Square, accum_out=st2[:, 0:1]),
                         reads=[zsbb], writes=[scrb, st2b])
                    rstd_from_ss(k, st2, st2b, st3, st3b, 1.0 / 384, 1)
                    s.op("dve", lambda e: e.scalar_tensor_tensor(out=cn[:, 0:384], in0=zsb[:, 0:384], scalar=st3[:, 0:1], in1=gcn[:, 0:384],
                                                                 op0=ALU.mult, op1=ALU.mult), reads=[zsbb, st3b, gcnb], writes=[cnb])
                s.op("act", lambda e: e.activation(out=scr[:, 384:640], in_=zsb[:, 384:640], func=AF.Square, accum_out=st2[:, 1:2]),
                     reads=[zsbb], writes=[scrb, st2b])
                s.op("act", lambda e: e.activation(out=st3[:, 1:2], in_=st2[:, 1:2], func=AF.Sqrt, scale=1.0 / 256, bias=k.eps[:, 0:1]),
                     reads=[st2b], writes=[st3b])
                s.op("dve", lambda e: e.reciprocal(out=st3[:, 1:2], in_=st3[:, 1:2]), reads=[st3b], writes=[st3b])
                s.op("dve", lambda e: e.scalar_tensor_tensor(out=cn[:, 384:640], in0=zsb[:, 384:640], scalar=st3[:, 1:2], in1=gcn[:, 384:640],
                                                             op0=ALU.mult, op1=ALU.mult), reads=[zsbb, st3b, gcnb], writes=[cnb])
                s.op("act", lambda e: e.activation(out=scr[:, 640:672], in_=zsb[:, 640:672], func=AF.Square, accum_out=st2[:, 2:3]),
                     reads=[zsbb], writes=[scrb, st2b])
                yield
                c_lo = 0 if lat else 3
                bank, bb = banks[7]
                bv = bank[:].bitcast(BF16)
                for c in range(c_lo, 5):
                    s.op("pe", lambda e: e.transpose(out=bv[:, c * 128:(c + 1) * 128], in_=cn[:, c * 128:(c + 1) * 128], identity=k.ident[:]),
                         reads=[cnb, k.identb], writes=[bb])
                s.op("act", lambda e: e.copy(out=cnT[:, c_lo:5, :], in_=bv[:, c_lo * 128:640].rearrange("p (c t) -> p c t", t=128)),
                     reads=[bb], writes=[cnTb])
                yield
                if lat:
                    (qa, qab), (qb_, qbb) = banks[2], banks[3]
                    for (qk, qkb, h0, h1) in ((qa, qab, 0, 5), (qb_, qbb, 5, 8)):
                        n = (h1 - h0) * 96
                        for c in range(3):
                            s.op("pe", lambda e: e.matmul(qk[:, 0:n], lhsT=cnT[:, c, :], rhs=w_q[:, c, h0 * 96:h1 * 96], start=(c == 0), stop=(c == 2)),
                                 reads=[cnTb, w_qb], writes=[qkb])
                        s.op("act", lambda e: e.activation(out=scr[:, h0 * 96:h1 * 96], in_=qk[:, 0:n], func=AF.Square), reads=[qkb], writes=[scrb])
                    s.op("dve", lambda e: e.tensor_reduce(out=st2[:, 4:12], in_=scr[:, 0:768].rearrange("p (h d) -> p h d", d=96), axis=AX.X, op=ALU.add),
                         reads=[scrb], writes=[st2b])
                    s.op("act", lambda e: e.activation(out=st3[:, 4:12], in_=st2[:, 4:12], func=AF.Sqrt, scale=1.0 / 96, bias=k.eps[:, 0:1]),
                         reads=[st2b], writes=[st3b])
                    s.op("dve", lambda e: e.reciprocal(out=st3[:, 4:12], in_=st3[:, 4:12]), reads=[st3b], writes=[st3b])
                    for (qk, qkb, h0, h1) in ((qa, qab, 0, 5), (qb_, qbb, 5, 8)):
                        n = (h1 - h0) * 96
                        s.op("dve", lambda e: e.tensor_tensor(out=qn[:, h0:h1, :], in0=qk[:, 0:n].rearrange("p (h d) -> p h d", d=96),
                                                              in1=st3[:, 4 + h0:4 + h1].unsqueeze(2).to_broadcast([128, h1 - h0, 96]), op=ALU.mult),
                             reads=[qkb, st3b], writes=[qnb])
                    s.op("pool", lambda e: e.tensor_tensor(out=qf[:, :, 0:64], in0=qn[:, :, 0:64],
                                                           in1=gq[:, 0:64].unsqueeze(1).to_broadcast([128, 8, 64]), op=ALU.mult),
                         reads=[qnb, gqb], writes=[qfb])
                    s.op("dve", lambda e: e.tensor_tensor(out=qr[:], in0=qn[:, :, 64:96],
                                                          in1=gq[:, 64:96].unsqueeze(1).to_broadcast([128, 8, 32]), op=ALU.mult),
                         reads=[qnb, gqb], writes=[qrb])
                    cosb = rp[:, 0:16].unsqueeze(1).to_broadcast([128, 8, 16])
                    sinb = rp[:, 16:32].unsqueeze(1).to_broadcast([128, 8, 16])
                    s.op("dve", lambda e: e.tensor_tensor(out=rt[:, 0], in0=qr[:, :, 0:16], in1=cosb, op=ALU.mult), reads=[qrb, rpb], writes=[rtb])
                    s.op("dve", lambda e: e.tensor_tensor(out=rt[:, 1], in0=qr[:, :, 16:32], in1=sinb, op=ALU.mult), reads=[qrb, rpb], writes=[rtb])
                    s.op("dve", lambda e: e.tensor_tensor(out=rt[:, 2], in0=qr[:, :, 0:16], in1=sinb, op=ALU.mult), reads=[qrb, rpb], writes=[rtb])
                    s.op("dve", lambda e: e.tensor_tensor(out=rt[:, 3], in0=qr[:, :, 16:32], in1=cosb, op=ALU.mult), reads=[qrb, rpb], writes=[rtb])
                    s.op("dve", lambda e: e.tensor_tensor(out=qf[:, :, 64:80], in0=rt[:, 0], in1=rt[:, 1], op=ALU.subtract), reads=[rtb], writes=[qfb])
                    s.op("dve", lambda e: e.tensor_tensor(out=qf[:, :, 80:96], in0=rt[:, 2], in1=rt[:, 3], op=ALU.add), reads=[rtb], writes=[qfb])
                yield
                (ka, kab), (kb_, kbb) = banks[4], banks[5]
                for g, (kk, kkb) in enumerate(((ka, kab), (kb_, kbb))):
                    for c in range(2):
                        s.op("pe", lambda e: e.matmul(kk[:, :], lhsT=cnT[:, 3 + c, :], rhs=w_kv[:, c, g * 512:(g + 1) * 512], start=(c == 0), stop=(c == 1)),
                             reads=[cnTb, w_kvb], writes=[kkb])
                    kv3 = kk[:, :].rearrange("p (h d) -> p h d", d=128)
                    s.op("act", lambda e: e.activation(out=scr[:, g * 256:(g + 1) * 256].rearrange("p (h d) -> p h d", d=64), in_=kv3[:, :, 0:64], func=AF.Square),
                         reads=[kkb], writes=[scrb])
                    s.op("act", lambda e: e.copy(out=Vs[:, t, g * 4:(g + 1) * 4, 0:64], in_=kv3[:, :, 64:128]), reads=[kkb], writes=[Vsb])
                yield
                s.op("dve", lambda e: e.tensor_reduce(out=st2[:, 4:12], in_=scr[:, 0:512].rearrange("p (h d) -> p h d", d=64), axis=AX.X, op=ALU.add),
                     reads=[scrb], writes=[st2b])
                s.op("dve", lambda e: e.tensor_scalar(out=st2[:, 4:12], in0=st2[:, 4:12], scalar1=st2[:, 2:3], scalar2=None, op0=ALU.add),
                     reads=[st2b], writes=[st2b])
                s.op("act", lambda e: e.activation(out=st3[:, 4:12], in_=st2[:, 4:12], func=AF.Sqrt, scale=1.0 / 96, bias=k.eps[:, 0:1]),
                     reads=[st2b], writes=[st3b])
                s.op("dve", lambda e: e.reciprocal(out=st3[:, 4:12], in_=st3[:, 4:12]), reads=[st3b], writes=[st3b])
                for g, (kk, kkb) in enumerate(((ka, kab), (kb_, kbb))):
                    kv3 = kk[:, :].rearrange("p (h d) -> p h d", d=128)
                    s.op("dve", lambda e: e.tensor_tensor(out=kn[:, g * 4:(g + 1) * 4, :], in0=kv3[:, :, 0:64],
                                                          in1=st3[:, 4 + g * 4:8 + g * 4].unsqueeze(2).to_broadcast([128, 4, 64]), op=ALU.mult),
                         reads=[kkb, st3b], writes=[knb])
                s.op("pool", lambda e: e.tensor_tensor(out=kf[:, :, 0:64], in0=kn[:], in1=gk[:, 0:64].unsqueeze(1).to_broadcast([128, 8, 64]), op=ALU.mult),
                     reads=[knb, gkb], writes=[kfb])
                s.op("dve", lambda e: e.tensor_tensor(out=krg[:], in0=zsb[:, 640:672], in1=gk[:, 64:96], op=ALU.mult), reads=[zsbb, gkb], writes=[krgb])
                if lat:
                    s.op("dve", lambda e: e.tensor_tensor(out=kt4[:, 0], in0=krg[:, 0:16], in1=rp[:, 0:16], op=ALU.mult), reads=[krgb, rpb], writes=[kt4b])
                    s.op("dve", lambda e: e.tensor_tensor(out=kt4[:, 1], in0=krg[:, 16:32], in1=rp[:, 16:32], op=ALU.mult), reads=[krgb, rpb], writes=[kt4b])
                    s.op("dve", lambda e: e.tensor_tensor(out=kt4[:, 2], in0=krg[:, 0:16], in1=rp[:, 16:32], op=ALU.mult), reads=[krgb, rpb], writes=[kt4b])
                    s.op("dve", lambda e: e.tensor_tensor(out=kt4[:, 3], in0=krg[:, 16:32], in1=rp[:, 0:16], op=ALU.mult), reads=[krgb, rpb], writes=[kt4b])
                    s.op("dve", lambda e: e.tensor_tensor(out=krr[:, 0:16], in0=kt4[:, 0], in1=kt4[:, 1], op=ALU.subtract), reads=[kt4b], writes=[krrb])
                    s.op("dve", lambda e: e.tensor_tensor(out=krr[:, 16:32], in0=kt4[:, 2], in1=kt4[:, 3], op=ALU.add), reads=[kt4b], writes=[krrb])
                else:
                    s.op("dve", lambda e: e.tensor_copy(out=krr[:], in_=krg[:]), reads=[krgb], writes=[krrb])
                s.op("dve", lambda e: e.tensor_tensor(out=kf[:, :, 64:96], in0=krr[:].unsqueeze(1).to_broadcast([128, 8, 32]),
                                                      in1=st3[:, 4:12].unsqueeze(2).to_broadcast([128, 8, 32]), op=ALU.mult),
                     reads=[krrb, st3b], writes=[kfb])
                yield
                if lat:
                    tb_, tbb = banks[6]
                    tl = t - NCT
                    transpose_chunks(k, tb_, tbb, lambda h: qf[:, h, :], 8, 96, qT[:, :, tl * 128:(tl + 1) * 128], qTb, qfb, dst_view=True)
                tb_, tbb = banks[0]
                transpose_chunks(k, tb_, tbb, lambda h: kf[:, h, :], 8, 96, kT[:, :, t * 128:(t + 1) * 128], kTb, kfb, dst_view=True)
            run_pipelined([p1_tile(t) for t in range(NT + NCT)], 4, k)
            s.barrier()

        with ExitStack() as es2:
            pTs = [k.sb(es2, f"p2_pT{i}", [128, 512], BF16) for i in range(3)]
            rc, rcb = k.sb(es2, "p2_rc", [128, 8], F32)
            steps = [(h, g, kt) for h in range(8) for g in range(4) for kt in range(NT + NCT)]

            def emit_s(i):
                h, g, kt = steps[i]
                sbk, sbkb = banks[i % 2]
                pT, pTb = pTs[i % 3]
                s.op("pe", lambda e: e.matmul(sbk[:, :], lhsT=kT[:, h, kt * 128:(kt + 1) * 128], rhs=qT[:, h, g * 512:(g + 1) * 512], start=True, stop=True),
                     reads=[kTb, qTb], writes=[sbkb])
                s.op("act", lambda e: e.activation(out=pT[:], in_=sbk[:, :], func=AF.Exp), reads=[sbkb], writes=[pTb])

            def emit_pv(i):
                h, g, kt = steps[i]
                pT, pTb = pTs[i % 3]
                for qs in range(4):
                    ob, obb = banks[2 + qs]
                    s.op("pe", lambda e: e.matmul(ob[:, 0:65], lhsT=pT[:, qs * 128:(qs + 1) * 128], rhs=Vs[:, kt, h, :],
                                                  start=(kt == 0), stop=(kt == NT + NCT - 1)), reads=[pTb, Vsb], writes=[obb])
                if kt == NT + NCT - 1:
                    for qs in range(4):
                        ob, obb = banks[2 + qs]
                        j = (g * 4 + qs) % 8
                        s.op("dve", lambda e: e.reciprocal(out=rc[:, j:j + 1], in_=ob[:, 64:65]), reads=[obb], writes=[rcb])
                        s.op("dve", lambda e: e.tensor_scalar(out=osb[:, g * 4 + qs, h * 64:(h + 1) * 64], in0=ob[:, 0:64], scalar1=rc[:, j:j + 1],
                                                              scalar2=None, op0=ALU.mult), reads=[obb, rcb], writes=[osbb])

            emit_s(0)
            for i in range(len(steps)):
                if i + 1 < len(steps):
                    emit_s(i + 1)
                emit_pv(i)
                if i % 8 == 7:
                    k.poke()
            s.barrier()

        with ExitStack() as es3:
            stg = [k.sb(es3, f"p3_stg{i}", [128, 1536], F32) for i in range(2)]
            w_in, w_inb = k.sb(es3, "p3_win", [128, 8, 1536], BF16)
            gqn, gqnb = k.sb(es3, "p3_gq", [128, 64], F32)
            gkn, gknb = k.sb(es3, "p3_gk", [128, 64], F32)
            tn, tnb = k.sb(es3, "p3_tn", [128, 16, 64], F32)
            qkf, qkfb = k.sb(es3, "p3_qkf", [128, 16, 64], BF16)
            wv = W["attn_w_in"].rearrange("(kc p) n -> p kc n", p=128)
            for c in range(8):
                load_cast(k, stg, w_in[:, c, :], w_inb, wv[:, c, 672:2208], 1536, c)
            load_bcast(k, gqn[:], gqnb, W["na_g_q"], 64)
            load_bcast(k, gkn[:], gknb, W["na_g_k"], 64)
            s.op("dve", lambda e: e.tensor_scalar_mul(out=gqn[:], in0=gqn[:], scalar1=64.0 ** -0.5), reads=[gqnb], writes=[gqnb])
            def p3_tile(t):
                lat = t >= NCT
                aT, aTb, scr, scrb = front(t)
                yield
                grp = (0, 1, 2) if lat else (1, 2)
                for g in grp:
                    zb, zbb = banks[g]
                    for c in range(8):
                        s.op("pe", lambda e: e.matmul(zb[:, :], lhsT=aT[:, c, :], rhs=w_in[:, c, g * 512:(g + 1) * 512], start=(c == 0), stop=(c == 7)),
                             reads=[aTb, w_inb], writes=[zbb])
                    if g < 2:
                        s.op("act", lambda e: e.activation(out=scr[:, g * 512:(g + 1) * 512], in_=zb[:, :], func=AF.Square), reads=[zbb], writes=[scrb])
                    else:
                        s.op("act", lambda e: e.copy(out=Vs[:, t, :, 0:64], in_=zb[:, :].rearrange("p (h d) -> p h d", d=64)), reads=[zbb], writes=[Vsb])
                yield
                g0 = 0 if lat else 1
                s.op("dve", lambda e: e.tensor_reduce(out=st2[:, g0 * 8:16], in_=scr[:, g0 * 512:1024].rearrange("p (h d) -> p h d", d=64), axis=AX.X, op=ALU.add),
                     reads=[scrb], writes=[st2b])
                s.op("act", lambda e: e.activation(out=st3[:, g0 * 8:16], in_=st2[:, g0 * 8:16], func=AF.Sqrt, scale=1.0 / 64, bias=k.eps[:, 0:1]),
                     reads=[st2b], writes=[st3b])
                s.op("dve", lambda e: e.reciprocal(out=st3[:, g0 * 8:16], in_=st3[:, g0 * 8:16]), reads=[st3b], writes=[st3b])
                for g in grp[:-1]:
                    zb, zbb = banks[g]
                    gg, ggb = (gqn, gqnb) if g == 0 else (gkn, gknb)
                    s.op("dve", lambda e: e.tensor_tensor(out=tn[:, g * 8:(g + 1) * 8, :], in0=zb[:, :].rearrange("p (h d) -> p h d", d=64),
                                                          in1=st3[:, g * 8:(g + 1) * 8].unsqueeze(2).to_broadcast([128, 8, 64]), op=ALU.mult),
                         reads=[zbb, st3b], writes=[tnb])
                    s.op("pool", lambda e: e.tensor_tensor(out=qkf[:, g * 8:(g + 1) * 8, :], in0=tn[:, g * 8:(g + 1) * 8, :],
                                                           in1=gg[:].unsqueeze(1).to_broadcast([128, 8, 64]), op=ALU.mult),
                         reads=[tnb, ggb], writes=[qkfb])
                yield
                if lat:
                    tb_, tbb = banks[3]
                    tl = t - NCT
                    transpose_chunks(k, tb_, tbb, lambda h: qkf[:, h, :], 8, 64, qT[0:64, :, tl * 128:(tl + 1) * 128], qTb, qkfb, dst_view=True)
                tb_, tbb = banks[4]
                transpose_chunks(k, tb_, tbb, lambda h: qkf[:, 8 + h, :], 8, 64, kT[0:64, :, t * 128:(t + 1) * 128], kTb, qkfb, dst_view=True)
            run_pipelined([p3_tile(t) for t in range(NT + NCT)], 2, k)
            s.barrier()

        with ExitStack() as es4:
            nbs = [k.sb(es4, f"p4_nb{i}", [128, 21, 128], F32) for i in range(2)]
            sfs = [k.sb(es4, f"p4_sf{i}", [128, 640], F32) for i in range(2)]
            pTs = [k.sb(es4, f"p4_pT{i}", [128, 896], BF16) for i in range(2)]
            rc, rcb = k.sb(es4, "p4_rc", [128, 8], F32)
            steps = [(h, qt) for h in range(8) for qt in range(NT)]

            def res(i):
                return (banks[(i % 2) * 2], banks[(i % 2) * 2 + 1], sfs[i % 2], pTs[i % 2], banks[4 + i % 2])

            def emit_s(i):
                h, qt = steps[i]
                nb, nbb = nbs[h % 2]
                if qt == 0:
                    s.dma("sp", lambda e: e.dma_start(out=nb[:], in_=W["nabias"][h]), writes=[nbb])
                blocks = na_blocks(qt)
                nloc = len(blocks)
                (sa, sab), (sb_, sbb), (sf, sfb), (pT, pTb), _ = res(i)
                qsl = qT[0:64, h, qt * 128:(qt + 1) * 128]
                for j, (kt, bi) in enumerate(blocks):
                    dstb_, dstbb = (sa, sab) if j < 4 else (sb_, sbb)
                    col = (j % 4) * 128
                    s.op("pe", lambda e: e.matmul(dstb_[:, col:col + 128], lhsT=kT[0:64, h, (NCT + kt) * 128:(NCT + kt + 1) * 128], rhs=qsl, start=True, stop=True),
                         reads=[kTb, qTb], writes=[dstbb])
                for c in range(NCT):
                    s.op("pe", lambda e: e.matmul(sb_[:, 128 + c * 128:256 + c * 128], lhsT=kT[0:64, h, c * 128:(c + 1) * 128], rhs=qsl, start=True, stop=True),
                         reads=[kTb, qTb], writes=[sbb])
                b0 = blocks[0][1]
                s.op("dve", lambda e: e.tensor_tensor(out=sf[:, 0:512], in0=sa[:, :], in1=nb[:, b0:b0 + 4, :].rearrange("p b q -> p (b q)"), op=ALU.add),
                     reads=[sab, nbb], writes=[sfb])
                if nloc == 5:
                    s.op("dve", lambda e: e.tensor_tensor(out=sf[:, 512:640], in0=sb_[:, 0:128], in1=nb[:, b0 + 4, :], op=ALU.add),
                         reads=[sbb, nbb], writes=[sfb])
                s.op("act", lambda e: e.activation(out=pT[:, 0:nloc * 128], in_=sf[:, 0:nloc * 128], func=AF.Exp), reads=[sfb], writes=[pTb])
                s.op("act", lambda e: e.activation(out=pT[:, 640:896], in_=sb_[:, 128:384], func=AF.Exp), reads=[sbb], writes=[pTb])

            def emit_pv(i):
                h, qt = steps[i]
                blocks = na_blocks(qt)
                _, _, _, (pT, pTb), (ob, obb) = res(i)
                for j, (kt, bi) in enumerate(blocks):
                    s.op("pe", lambda e: e.matmul(ob[:, 0:65], lhsT=pT[:, j * 128:(j + 1) * 128], rhs=Vs[:, NCT + kt, h, :], start=(j == 0), stop=False),
                         reads=[pTb, Vsb], writes=[obb])
                for c in range(NCT):
                    s.op("pe", lambda e: e.matmul(ob[:, 0:65], lhsT=pT[:, 640 + c * 128:768 + c * 128], rhs=Vs[:, c, h, :], start=False, stop=(c == NCT - 1)),
                         reads=[pTb, Vsb], writes=[obb])
                j8 = i % 8
                s.op("dve", lambda e: e.reciprocal(out=rc[:, j8:j8 + 1], in_=ob[:, 64:65]), reads=[obb], writes=[rcb])
                s.op("dve", lambda e: e.tensor_scalar(out=osb[:, qt, 512 + h * 64:512 + (h + 1) * 64], in0=ob[:, 0:64], scalar1=rc[:, j8:j8 + 1],
                                                      scalar2=None, op0=ALU.mult), reads=[obb, rcb], writes=[osbb])

            emit_s(0)
            for i in range(len(steps)):
                if i + 1 < len(steps):
                    emit_s(i + 1)
                emit_pv(i)
                if i % 8 == 7:
                    k.poke()
            s.barrier()

        with ExitStack() as es5:
            stg = [k.sb(es5, f"p5_stg{i}", [128, 1024], F32) for i in range(2)]
            w_o, w_ob = k.sb(es5, "p5_wo", [128, 8, 1024], BF16)
            G1, G1b = k.sb(es5, "p5_g1", [128, 1024], F32)
            outs = [k.sb(es5, f"p5_out{i}", [128, 1024], F32) for i in range(2)]
            wv = W["attn_w_out"].rearrange("(kc p) n -> p kc n", p=128)
            for c in range(8):
                load_cast(k, stg, w_o[:, c, :], w_ob, wv[:, c, :], 1024, c)
            load_bcast(k, G1[:], G1b, modv0[0:1, 2, :], 1024)
            for t in range(NT):
                xt, xb = xts[t % 2]
                scr, scrb = scrs[t % 2]
                aT, aTb = aTs[t % 2]
                ot, otb = outs[t % 2]
                s.dma("sp", lambda e: e.dma_start(out=xt[:], in_=x[t * 128:(t + 1) * 128, :]), writes=[xb])
                bank, bb = banks[7]
                transpose_chunks(k, bank, bb, lambda c: osb[:, t, c * 128:(c + 1) * 128], 8, 128, aT[:], aTb, osbb, dst_view=True)
                for g in range(2):
                    yb, ybb = banks[g]
                    for c in range(8):
                        s.op("pe", lambda e: e.matmul(yb[:, :], lhsT=aT[:, c, :], rhs=w_o[:, c, g * 512:(g + 1) * 512], start=(c == 0), stop=(c == 7)),
                             reads=[aTb, w_ob], writes=[ybb])
                    s.op("dve", lambda e: e.tensor_tensor(out=scr[:, g * 512:(g + 1) * 512], in0=yb[:, :], in1=G1[:, g * 512:(g + 1) * 512], op=ALU.mult),
                         reads=[ybb, G1b], writes=[scrb])
                s.op("pool", lambda e: e.tensor_tensor(out=ot[:], in0=scr[:], in1=xt[:], op=ALU.add), reads=[scrb, xb], writes=[otb])
                s.dma("sp", lambda e: e.dma_start(out=hout[t * 128:(t + 1) * 128, :], in_=ot[:]), reads=[otb], writes=[houtb[t]])
            s.barrier()


def host_rope_table():
    t = np.arange(SEQ)
    row = (t // 64).astype(np.float32)
    col = (t % 64).astype(np.float32)
    inv = (np.float32(1.0) / (np.float32(10000.0) ** (np.arange(8, dtype=np.float32) / np.float32(8)))).astype(np.float32)
    ang = np.concatenate([row[:, None] * inv, col[:, None] * inv], axis=-1).astype(np.float32)
    return np.concatenate([np.cos(ang), np.sin(ang)], axis=-1).astype(np.float32)


def host_na_bias(rpb):
    pairs = [(2, j) for j in range(5)] + [(0, j) for j in range(4)] + [(1, j) for j in range(4)] \
        + [(14, 12 + j) for j in range(4)] + [(15, 12 + j) for j in range(4)]
    out = np.full((8, 128, 21, 128), NEG, np.float32)
    p = np.arange(128)
    for bi, (qt, kt) in enumerate(pairs):
        tq = qt * 128 + p
        tk = kt * 128 + p
        r, c = tq // 64, tq % 64
        kr, kc = tk // 64, tk % 64
        r0 = np.clip(r - 4, 0, 24)
        c0 = np.clip(c - 8, 0, 48)
        inside = (kr[:, None] >= r0[None, :]) & (kr[:, None] < r0[None, :] + 8) & (kc[:, None] >= c0[None, :]) & (kc[:, None] < c0[None, :] + 16)
        rr = np.clip(kr[:, None] - r[None, :] + 7, 0, 14)
        rc = np.clip(kc[:, None] - c[None, :] + 15, 0, 30)
        vals = rpb[:, rr, rc]
        out[:, :, bi, :] = np.where(inside[None], vals, np.float32(NEG))
    return out


NROW = 8


def stage_peer(k, hin, hinb, modv_l, w_query, skT_d, u_tab, v_tab, hout, houtb, tag):
    nc, s = k.nc, k.s
    banks = k.banks
    with ExitStack() as es:
        stg = [k.sb(es, f"{tag}_stg{i}", [128, 2048], F32) for i in range(2)]
        wq, wqb = k.sb(es, f"{tag}_wq", [128, 8, 2048], BF16)
        skT, skTb = k.sb(es, f"{tag}_skT", [128, 16, 128], BF16)
        ABG, ABGb = k.sb(es, f"{tag}_ABG", [128, 3, 1024], F32)
        iota_i, iota_ib = k.sb(es, f"{tag}_iotai", [128, 16], I32)
        iota, iotab = k.sb(es, f"{tag}_iota", [128, 16], F32)
        st, stb = k.sb(es, f"{tag}_st", [128, 4], F32)
        xts = [k.sb(es, f"{tag}_x{i}", [128, 1024], F32) for i in range(2)]
        scrs = [k.sb(es, f"{tag}_scr{i}", [128, 1024], F32) for i in range(2)]
        hms = [k.sb(es, f"{tag}_hm{i}", [128, 1024], F32) for i in range(2)]
        hbf, hbfb = k.sb(es, f"{tag}_hbf", [128, 1024], BF16)
        hT, hTb = k.sb(es, f"{tag}_hT", [128, 8, 128], BF16)
        qbf, qbfb = k.sb(es, f"{tag}_qbf", [128, 2048], BF16)
        qT, qTb = k.sb(es, f"{tag}_qT", [128, 16, 128], BF16)
        ssb, ssbb = k.sb(es, f"{tag}_s", [128, 16, 128], F32)
        s2, _ = k.sb(es, f"{tag}_s2", [128, 16, 128], F32)
        m16, _ = k.sb(es, f"{tag}_m16", [128, 16, 16], F32)
        i16, _ = k.sb(es, f"{tag}_i16", [128, 16, 16], U32)
        i16f, i16fb = k.sb(es, f"{tag}_i16f", [128, 16, 16], F32)
        cand, candb = k.sb(es, f"{tag}_cand", [128, 8, 256], F32)
        cand2, _ = k.sb(es, f"{tag}_cand2", [128, 8, 256], F32)
        best, _ = k.sb(es, f"{tag}_best", [128, 8, 16], F32)
        pos, _ = k.sb(es, f"{tag}_pos", [128, 8, 16], U32)
        ab_i, ab_ib = k.sb(es, f"{tag}_abi", [128, 2, 128], I32)
        ab_f, ab_fb = k.sb(es, f"{tag}_abf", [128, 2, 128], F32)
        oh, ohb = k.sb(es, f"{tag}_oh", [128, 8, 16, 16], F32)
        e01, e01b = k.sb(es, f"{tag}_e01", [128, 2, 128], F32)
        idxs = [k.sb(es, f"{tag}_idx{i}", [128, 128], I32) for i in range(2)]
        gts = [k.sb(es, f"{tag}_gate{i}", [128, 8, 16], F32) for i in range(2)]
        gsum, gsumb = k.sb(es, f"{tag}_gsum", [128, 8], F32)
        actv, _ = k.sb(es, f"{tag}_act", [128, 128], F32)
        wgt, wgtb = k.sb(es, f"{tag}_wgt", [128, 128], F32)
        junk, _ = k.sb(es, f"{tag}_junk", [128, 1024], BF16)
        rows = [k.sb(es, f"{tag}_row{i}", [128, 1024], F32) for i in range(NROW)]
        accs = [k.sb(es, f"{tag}_acc{i}", [128, 1024], F32) for i in range(4)]
        ot, otb = k.sb(es, f"{tag}_ot", [128, 1024], F32)
        hpb = [[Buf() for _ in range(16)] for _ in range(3)]
        hb = [[Buf() for _ in range(8)] for _ in range(3)]
        actb = [Buf() for _ in range(16)]

        qi = load_w_bf16(k, stg, wq, wqb, w_query, 8, 2048)
        load_cast(k, stg, skT[:].rearrange("p a b -> p (a b)"), skTb, skT_d.rearrange("p a b -> p (a b)"), 2048, qi)
        for j in range(3):
            load_bcast(k, ABG[:, j, :], ABGb, modv_l[0:1, 3 + j, :], 1024)
        s.op("pool", lambda e: e.iota(out=iota_i[:], pattern=[[1, 16]], base=0, channel_multiplier=0), writes=[iota_ib])
        s.op("dve", lambda e: e.tensor_copy(out=iota[:], in_=iota_i[:]), reads=[iota_ib], writes=[iotab])

        def front(t):
            xt, xb = xts[t % 2]
            scr, scrb = scrs[t % 2]
            hm, hmb = hms[t % 2]
            idx, idxb = idxs[t % 2]
            gate, gateb = gts[t % 2]
            s.dma("sp", lambda e: e.dma_start(out=xt[:], in_=hin[t * 128:(t + 1) * 128, :]), reads=[hinb[t]], writes=[xb])
            norm_mod(k, xt, xb, ABG[:, 0, :], ABG[:, 1, :], ABGb, scr, scrb, st, stb, hbf, hbfb, out_f32=(hm, hmb))
            bank, bb = banks[7]
            transpose_chunks(k, bank, bb, lambda c: hbf[:, c * 128:(c + 1) * 128], 8, 128, hT[:], hTb, hbfb, dst_view=True)
            for g in range(4):
                qb_, qbb = banks[g]
                for c in range(8):
                    s.op("pe", lambda e: e.matmul(qb_[:, :], lhsT=hT[:, c, :], rhs=wq[:, c, g * 512:(g + 1) * 512], start=(c == 0), stop=(c == 7)),
                         reads=[hTb, wqb], writes=[qbb])
                s.op("act", lambda e: e.copy(out=qbf[:, g * 512:(g + 1) * 512], in_=qb_[:, :]), reads=[qbb], writes=[qbfb])
            for half in range(2):
                tb_, tbb = banks[4 + half]
                transpose_chunks(k, tb_, tbb, lambda c: qbf[:, (half * 8 + c) * 128:(half * 8 + c + 1) * 128], 8, 128,
                                 qT[:, half * 8:(half + 1) * 8, :], qTb, qbfb, dst_view=True)
            for g in range(4):
                sb_, sbb = banks[g]
                for j in range(4):
                    hp = g * 4 + j
                    s.op("pe", lambda e: e.matmul(sb_[:, j * 128:(j + 1) * 128], lhsT=qT[:, hp, :], rhs=skT[:, hp, :], start=True, stop=True),
                         reads=[qTb, skTb], writes=[sbb])
                s.op("act", lambda e: e.copy(out=ssb[:, g * 4:(g + 1) * 4, :], in_=sb_[:, :].rearrange("p (a b) -> p a b", b=128)), reads=[sbb], writes=[ssbb])
            for hp in range(16):
                s.op("dve", lambda e: e.max(out=m16[:, hp, 0:8], in_=ssb[:, hp, :]), reads=[ssbb], writes=[hpb[0][hp]])
            for hp in range(16):
                s.op("dve", lambda e: e.max_index(out=i16[:, hp, 0:8], in_max=m16[:, hp, 0:8], in_values=ssb[:, hp, :]),
                     reads=[ssbb, hpb[0][hp]], writes=[hpb[1][hp]])
            for hp in range(16):
                s.op("dve", lambda e: e.match_replace(out=s2[:, hp, :], in_to_replace=m16[:, hp, 0:8], in_values=ssb[:, hp, :], imm_value=-1e30),
                     reads=[ssbb, hpb[0][hp]], writes=[hpb[2][hp]])
            for hp in range(16):
                s.op("dve", lambda e: e.max(out=m16[:, hp, 8:16], in_=s2[:, hp, :]), reads=[hpb[2][hp]], writes=[hpb[0][hp]])
            for hp in range(16):
                s.op("dve", lambda e: e.max_index(out=i16[:, hp, 8:16], in_max=m16[:, hp, 8:16], in_values=s2[:, hp, :]),
                     reads=[hpb[2][hp], hpb[0][hp]], writes=[hpb[1][hp]])
            s.op("dve", lambda e: e.tensor_copy(out=i16f[:], in_=i16[:]), reads=hpb[1], writes=[i16fb])
            m4 = m16[:].rearrange("p (h t) a -> p h t a", t=2)
            s.op("dve", lambda e: e.tensor_tensor(out=cand[:].rearrange("p h (a b) -> p h a b", b=16),
                                                  in0=m4[:, :, 0, :].unsqueeze(3).to_broadcast([128, 8, 16, 16]),
                                                  in1=m4[:, :, 1, :].unsqueeze(2).to_broadcast([128, 8, 16, 16]), op=ALU.add),
                 reads=hpb[0], writes=[candb])
            for h in range(8):
                s.op("dve", lambda e: e.max(out=best[:, h, 0:8], in_=cand[:, h, :]), reads=[candb], writes=[hb[0][h]])
            for h in range(8):
                s.op("dve", lambda e: e.max_index(out=pos[:, h, 0:8], in_max=best[:, h, 0:8], in_values=cand[:, h, :]),
                     reads=[candb, hb[0][h]], writes=[hb[1][h]])
            for h in range(8):
                s.op("dve", lambda e: e.match_replace(out=cand2[:, h, :], in_to_replace=best[:, h, 0:8], in_values=cand[:, h, :], imm_value=-1e30),
                     reads=[candb, hb[0][h]], writes=[hb[2][h]])
            for h in range(8):
                s.op("dve", lambda e: e.max(out=best[:, h, 8:16], in_=cand2[:, h, :]), reads=[hb[2][h]], writes=[hb[0][h]])
            for h in range(8):
                s.op("dve", lambda e: e.max_index(out=pos[:, h, 8:16], in_max=best[:, h, 8:16], in_values=cand2[:, h, :]),
                     reads=[hb[2][h], hb[0][h]], writes=[hb[1][h]])
            posi = pos[:].rearrange("p h k -> p (h k)").bitcast(I32)
            s.op("dve", lambda e: e.tensor_single_scalar(out=ab_i[:, 0, :], in_=posi, scalar=4, op=ALU.arith_shift_right), reads=hb[1], writes=[ab_ib])
            s.op("dve", lambda e: e.tensor_single_scalar(out=ab_i[:, 1, :], in_=posi, scalar=15, op=ALU.bitwise_and), reads=hb[1], writes=[ab_ib])
            s.op("dve", lambda e: e.tensor_copy(out=ab_f[:], in_=ab_i[:]), reads=[ab_ib], writes=[ab_fb])
            i4 = i16f[:].rearrange("p (h t) a -> p h t a", t=2)
            for p_ in range(2):
                s.op("dve", lambda e: e.tensor_tensor(out=oh[:], in0=ab_f[:, p_, :].rearrange("p (h k) -> p h k", k=16).unsqueeze(3).to_broadcast([128, 8, 16, 16]),
                                                      in1=iota[:].unsqueeze(1).unsqueeze(1).to_broadcast([128, 8, 16, 16]), op=ALU.is_equal),
                     reads=[ab_fb, iotab], writes=[ohb])
                s.op("dve", lambda e: e.tensor_tensor(out=oh[:], in0=oh[:], in1=i4[:, :, p_, :].unsqueeze(2).to_broadcast([128, 8, 16, 16]), op=ALU.mult),
                     reads=[ohb, i16fb], writes=[ohb])
                s.op("dve", lambda e: e.tensor_reduce(out=e01[:, p_, :].rearrange("p (h k) -> p h k", k=16), in_=oh[:], axis=AX.X, op=ALU.add),
                     reads=[ohb], writes=[e01b])
            s.op("dve", lambda e: e.scalar_tensor_tensor(out=e01[:, 0, :], in0=e01[:, 0, :], scalar=128.0, in1=e01[:, 1, :], op0=ALU.mult, op1=ALU.add),
                 reads=[e01b], writes=[e01b])
            s.op("dve", lambda e: e.tensor_copy(out=idx[:], in_=e01[:, 0, :]), reads=[e01b], writes=[idxb])
            s.op("dve", lambda e: e.tensor_tensor(out=gate[:], in0=best[:], in1=best[:, :, 0:1].to_broadcast([128, 8, 16]), op=ALU.subtract),
                 reads=hb[0], writes=[gateb])
            s.op("act", lambda e: e.activation(out=gate[:], in_=gate[:], func=AF.Exp), reads=[gateb], writes=[gateb])
            s.op("dve", lambda e: e.tensor_reduce(out=gsum[:], in_=gate[:], axis=AX.X, op=ALU.add), reads=[gateb], writes=[gsumb])
            s.op("dve", lambda e: e.reciprocal(out=gsum[:], in_=gsum[:]), reads=[gsumb], writes=[gsumb])
            s.op("dve", lambda e: e.tensor_tensor(out=gate[:], in0=gate[:], in1=gsum[:].unsqueeze(2).to_broadcast([128, 8, 16]), op=ALU.mult),
                 reads=[gateb, gsumb], writes=[gateb])

        ring = [0]

        def gather(tab, idx, idxb, hk):
            rw, rwb = rows[ring[0] % NROW]
            ring[0] += 1
            s.dma("pool", lambda e: e.indirect_dma_start(out=rw[:], out_offset=None, in_=tab,
                                                         in_offset=bass.IndirectOffsetOnAxis(ap=idx[:, hk:hk + 1], axis=0)),
                  reads=[idxb], writes=[rwb])
            return rw, rwb

        def back(t):
            xt, xb = xts[t % 2]
            scr, scrb = scrs[t % 2]
            hm, hmb = hms[t % 2]
            idx, idxb = idxs[t % 2]
            gate, gateb = gts[t % 2]
            for hk in range(128):
                rw, rwb = gather(u_tab, idx, idxb, hk)
                s.op("dve", lambda e: e.scalar_tensor_tensor(out=junk[:], in0=rw[:], scalar=1.0, in1=hm[:], op0=ALU.mult, op1=ALU.mult,
                                                             accum_out=actv[:, hk:hk + 1]), reads=[rwb, hmb], writes=[actb[hk % 16]])
            s.op("act", lambda e: e.activation(out=wgt[:], in_=actv[:], func=AF.Gelu), reads=actb, writes=[wgtb])
            s.op("dve", lambda e: e.tensor_tensor(out=wgt[:], in0=wgt[:], in1=gate[:].rearrange("p h k -> p (h k)"), op=ALU.mult),
                 reads=[wgtb, gateb], writes=[wgtb])
            for hk in range(128):
                rw, rwb = gather(v_tab, idx, idxb, hk)
                ac, acb = accs[hk % 4]
                if hk < 4:
                    s.op("dve", lambda e: e.tensor_scalar(out=ac[:], in0=rw[:], scalar1=wgt[:, hk:hk + 1], scalar2=None, op0=ALU.mult),
                         reads=[rwb, wgtb], writes=[acb])
                else:
                    s.op("dve", lambda e: e.scalar_tensor_tensor(out=ac[:], in0=rw[:], scalar=wgt[:, hk:hk + 1], in1=ac[:], op0=ALU.mult, op1=ALU.add),
                         reads=[rwb, wgtb, acb], writes=[acb])
            (a0, a0b), (a1, a1b), (a2, a2b), (a3, a3b) = accs
            s.op("pool", lambda e: e.tensor_tensor(out=a0[:], in0=a0[:], in1=a1[:], op=ALU.add), reads=[a0b, a1b], writes=[a0b])
            s.op("pool", lambda e: e.tensor_tensor(out=a2[:], in0=a2[:], in1=a3[:], op=ALU.add), reads=[a2b, a3b], writes=[a2b])
            s.op("pool", lambda e: e.tensor_tensor(out=a0[:], in0=a0[:], in1=a2[:], op=ALU.add), reads=[a0b, a2b], writes=[a0b])
            s.op("dve", lambda e: e.tensor_tensor(out=scr[:], in0=a0[:], in1=ABG[:, 2, :], op=ALU.mult), reads=[a0b, ABGb], writes=[scrb])
            s.op("pool", lambda e: e.tensor_tensor(out=ot[:], in0=scr[:], in1=xt[:], op=ALU.add), reads=[scrb, xb], writes=[otb])
            s.dma("sp", lambda e: e.dma_start(out=hout[t * 128:(t + 1) * 128, :], in_=ot[:]), reads=[otb], writes=[houtb[t]])

        front(0)
        for t in range(NT):
            if t + 1 < NT:
                front(t + 1)
            back(t)
        s.barrier()


def stage_conv(k, hin, hinb, modv_l, W, hout, houtb):
    nc, s = k.nc, k.s
    banks = k.banks
    PADW = SEQ + 30
    with ExitStack() as es:
        cbuf, cbufb = k.sb(es, "cv_cbuf", [128, NT, 1024], F32)
        ABG, ABGb = k.sb(es, "cv_ABG", [128, 2, 1024], F32)
        st, stb = k.sb(es, "cv_st", [128, 8], F32)
        xts = [k.sb(es, f"cv_x{i}", [128, 1024], F32) for i in range(2)]
        scrs = [k.sb(es, f"cv_scr{i}", [128, 1024], F32) for i in range(2)]
        for j in range(2):
            load_bcast(k, ABG[:, j, :], ABGb, modv_l[0:1, j, :], 1024)
        cbt = [Buf() for _ in range(NT // 4)]
        with ExitStack() as es1:
            aTa, aTab = k.sb(es1, "cv_aT", [128, 8, SEQ], BF16)
            ubfs = [k.sb(es1, f"cv_ub{i}", [128, PADW], BF16) for i in range(2)]
            dgt, dgtb = k.sb(es1, "cv_dgt", [128, 12, 128], BF16)
            w1, w1b = k.sb(es1, "cv_w1", [128, 8, 2048], BF16)
            b1T, b1Tb = k.sb(es1, "cv_b1T", [128, 16], F32)
            wdw, wdwb = k.sb(es1, "cv_wdw", [128, 8, 31], F32)
            bdw, bdwb = k.sb(es1, "cv_bdw", [128, 8], F32)
            abfs = [k.sb(es1, f"cv_a{i}", [128, 1024], BF16) for i in range(1)]
            upads = [k.sb(es1, f"cv_up{i}", [128, PADW], F32) for i in range(2)]
            accs = [k.sb(es1, f"cv_acc{i}", [128, SEQ], F32) for i in range(2)]
            sgs = [k.sb(es1, f"cv_sg{i}", [128, 512], F32) for i in range(2)]
            w1v = W["conv_w_pw1"].rearrange("(kc p) n -> p kc n", p=128)
            for c in range(8):
                s.dma("pool", lambda e: e.dma_start(out=w1[:, c, :], in_=w1v[:, c, :]), writes=[w1b])
            s.dma("sp", lambda e: e.dma_start(out=b1T[:], in_=W["conv_b1T"]), writes=[b1Tb])
            s.dma("sp", lambda e: e.dma_start(out=wdw[:], in_=W["conv_wdwT"]), writes=[wdwb])
            s.dma("sp", lambda e: e.dma_start(out=bdw[:], in_=W["conv_bdwT"]), writes=[bdwb])
            for up, upb in upads + ubfs:
                s.op("pool", lambda e: e.memset(up[:, 0:15], 0.0), writes=[upb])
                s.op("pool", lambda e: e.memset(up[:, 15 + SEQ:PADW], 0.0), writes=[upb])
            for t in range(NT):
                xt, xb = xts[t % 2]
                scr, scrb = scrs[t % 2]
                abf, abfb = abfs[0]
                s.dma("sp", lambda e: e.dma_start(out=xt[:], in_=hin[t * 128:(t + 1) * 128, :]), reads=[hinb[t]], writes=[xb])
                norm_mod(k, xt, xb, ABG[:, 0, :], ABG[:, 1, :], ABGb, scr, scrb, st, stb, abf, abfb)
                bank, bb = banks[6 + t % 2]
                transpose_chunks(k, bank, bb, lambda c: abf[:, c * 128:(c + 1) * 128], 8, 128, aTa[:, :, t * 128:(t + 1) * 128], aTab, abfb, dst_view=True)
            accbs = [[Buf() for _ in range(4)] for _ in range(2)]
            NPE = 12
            tapbanks = [banks[2], banks[3], banks[6], banks[7]]

            def pw1_gen(m):
                up, upb = upads[m % 2]
                ub, ubb = ubfs[m % 2]
                for j in range(NPE):
                    s.op("act", lambda e: e.activation(out=dgt[:, j, :], in_=k.ident[:], func=AF.Copy, scale=wdw[:, m, j:j + 1]),
                         reads=[k.identb, wdwb], writes=[dgtb])
                for tg in range(4):
                    (bv_, bvb), (bg_, bgb) = banks[0], banks[1]
                    sg, sgb = sgs[tg % 2]
                    for (bk, bkb, c0) in ((bv_, bvb, m * 128), (bg_, bgb, 1024 + m * 128)):
                        for c in range(8):
                            s.op("pe", lambda e: e.matmul(bk[:, :], lhsT=w1[:, c, c0:c0 + 128], rhs=aTa[:, c, tg * 512:(tg + 1) * 512], start=(c == 0), stop=(c == 7)),
                                 reads=[w1b, aTab], writes=[bkb])
                    s.op("act", lambda e: e.activation(out=sg[:], in_=bg_[:, :], func=AF.Sigmoid, bias=b1T[:, 8 + m:9 + m]), reads=[bgb, b1Tb], writes=[sgb])
                    s.op("dve", lambda e: e.scalar_tensor_tensor(out=up[:, 15 + tg * 512:15 + (tg + 1) * 512], in0=bv_[:, :], scalar=b1T[:, m:m + 1], in1=sg[:],
                                                                 op0=ALU.add, op1=ALU.mult), reads=[bvb, sgb, b1Tb], writes=[upb])
                    if tg == 3:
                        s.op("act", lambda e: e.copy(out=ub[:, 15:15 + SEQ], in_=up[:, 15:15 + SEQ]), reads=[upb], writes=[ubb])
                    yield

            def taps_pe(m):
                ub, ubb = ubfs[m % 2]
                for ch in range(4):
                    tbk, tbkb = tapbanks[ch]
                    for j in range(NPE):
                        s.op("pe", lambda e: e.matmul(tbk[:, :], lhsT=dgt[:, j, :], rhs=ub[:, ch * 512 + j:ch * 512 + j + 512], start=(j == 0), stop=(j == NPE - 1)),
                             reads=[dgtb, ubb], writes=[tbkb])

            def taps_dve_gen(m):
                up, upb = upads[m % 2]
                acc, _ = accs[m % 2]
                accb = accbs[m % 2]
                for j in range(NPE, 31):
                    for ch in range(4):
                        src = up[:, ch * 512 + j:ch * 512 + j + 512]
                        dst = acc[:, ch * 512:(ch + 1) * 512]
                        if j == NPE:
                            s.op("dve", lambda e: e.tensor_scalar(out=dst, in0=src, scalar1=wdw[:, m, j:j + 1], scalar2=bdw[:, m:m + 1], op0=ALU.mult, op1=ALU.add),
                                 reads=[upb, wdwb, bdwb], writes=[accb[ch]])
                        else:
                            s.op("dve", lambda e: e.scalar_tensor_tensor(out=dst, in0=src, scalar=wdw[:, m, j:j + 1], in1=dst, op0=ALU.mult, op1=ALU.add),
                                 reads=[upb, wdwb, accb[ch]], writes=[accb[ch]])
                    if (j - NPE) % 5 == 4:
                        yield

            def finish(m):
                acc, _ = accs[m % 2]
                accb = accbs[m % 2]
                for ch in range(4):
                    tbk, tbkb = tapbanks[ch]
                    dst = acc[:, ch * 512:(ch + 1) * 512]
                    s.op("dve", lambda e: e.tensor_tensor(out=dst, in0=dst, in1=tbk[:, :], op=ALU.add), reads=[accb[ch], tbkb], writes=[accb[ch]])
                for g in range(NT // 4):
                    tb_, tbb = banks[4 + g % 2]
                    for j in range(4):
                        t = g * 4 + j
                        s.op("pe", lambda e: e.transpose(out=tb_[:, j * 128:(j + 1) * 128], in_=acc[:, t * 128:(t + 1) * 128], identity=k.identf[:]),
                             reads=[accb[t // 4], k.identb], writes=[tbb])
                    s.op("act", lambda e: e.copy(out=cbuf[:, g * 4:(g + 1) * 4, m * 128:(m + 1) * 128], in_=tb_[:, :].rearrange("p (a b) -> p a b", b=128)),
                         reads=[tbb], writes=[cbt[g]])

            for _ in pw1_gen(0):
                pass
            for m in range(8):
                taps_pe(m)
                gn = pw1_gen(m + 1) if m + 1 < 8 else None
                for _ in taps_dve_gen(m):
                    if gn is not None:
                        next(gn, None)
                if gn is not None:
                    for _ in gn:
                        pass
                finish(m)
            s.barrier()
        with ExitStack() as es3:
            stg = [k.sb(es3, f"cv3_stg{i}", [128, 1024], F32) for i in range(2)]
            w2, w2b = k.sb(es3, "cv3_w2", [128, 8, 1024], BF16)
            gl, glb = k.sb(es3, "cv3_gl", [128, 4, 1024], F32)
            sbfs = [k.sb(es3, f"cv3_s{i}", [128, 1024], BF16) for i in range(2)]
            sTs = [k.sb(es3, f"cv3_sT{i}", [128, 8, 128], BF16) for i in range(2)]
            outs = [k.sb(es3, f"cv3_o{i}", [128, 1024], F32) for i in range(2)]
            wv = W["conv_w_pw2"].rearrange("(kc p) n -> p kc n", p=128)
            for c in range(8):
                load_cast(k, stg, w2[:, c, :], w2b, wv[:, c, :], 1024, c)
            load_bcast(k, gl[:, 0, :], glb, W["conv_g_ln"], 1024)
            load_bcast(k, gl[:, 1, :], glb, W["conv_b_ln"], 1024)
            load_bcast(k, gl[:, 2, :], glb, W["conv_b_pw2"], 1024)
            load_bcast(k, gl[:, 3, :], glb, modv_l[0:1, 2, :], 1024)
            for t in range(NT):
                xt, xb = xts[t % 2]
                scr, scrb = scrs[t % 2]
                sbf, sbfb = sbfs[t % 2]
                sT, sTb = sTs[t % 2]
                ot, otb = outs[t % 2]
                cb = cbt[t // 4]
                c_t = cbuf[:, t, :]
                s.dma("sp", lambda e: e.dma_start(out=xt[:], in_=hin[t * 128:(t + 1) * 128, :]), reads=[hinb[t]], writes=[xb])
                s.op("act", lambda e: e.activation(out=scr[:], in_=c_t, func=AF.Identity, accum_out=st[:, 0:1]), reads=[cb], writes=[scrb, stb])
                s.op("act", lambda e: e.activation(out=scr[:], in_=c_t, func=AF.Square, accum_out=st[:, 1:2]), reads=[cb], writes=[scrb, stb])
                s.op("dve", lambda e: e.tensor_scalar(out=st[:, 2:3], in0=st[:, 0:1], scalar1=1.0 / D, scalar2=None, op0=ALU.mult), reads=[stb], writes=[stb])
                s.op("dve", lambda e: e.scalar_tensor_tensor(out=st[:, 3:4], in0=st[:, 2:3], scalar=-1.0, in1=st[:, 2:3], op0=ALU.mult, op1=ALU.mult),
                     reads=[stb], writes=[stb])
                s.op("dve", lambda e: e.scalar_tensor_tensor(out=st[:, 4:5], in0=st[:, 1:2], scalar=1.0 / D, in1=st[:, 3:4], op0=ALU.mult, op1=ALU.add),
                     reads=[stb], writes=[stb])
                s.op("act", lambda e: e.activation(out=st[:, 5:6], in_=st[:, 4:5], func=AF.Sqrt, scale=1.0, bias=k.eps[:, 0:1]), reads=[stb], writes=[stb])
                s.op("dve", lambda e: e.reciprocal(out=st[:, 5:6], in_=st[:, 5:6]), reads=[stb], writes=[stb])
                s.op("dve", lambda e: e.tensor_scalar(out=scr[:], in0=c_t, scalar1=st[:, 2:3], scalar2=st[:, 5:6], op0=ALU.subtract, op1=ALU.mult),
                     reads=[cb, stb], writes=[scrb])
                s.op("dve", lambda e: e.tensor_tensor(out=scr[:], in0=scr[:], in1=gl[:, 0, :], op=ALU.mult), reads=[scrb, glb], writes=[scrb])
                s.op("pool", lambda e: e.tensor_tensor(out=scr[:], in0=scr[:], in1=gl[:, 1, :], op=ALU.add), reads=[scrb, glb], writes=[scrb])
                s.op("act", lambda e: e.activation(out=sbf[:], in_=scr[:], func=AF.Silu), reads=[scrb], writes=[sbfb])
                bank, bb = banks[7]
                transpose_chunks(k, bank, bb, lambda c: sbf[:, c * 128:(c + 1) * 128], 8, 128, sT[:], sTb, sbfb, dst_view=True)
                for g in range(2):
                    yb, ybb = banks[g]
                    for c in range(8):
                        s.op("pe", lambda e: e.matmul(yb[:, :], lhsT=sT[:, c, :], rhs=w2[:, c, g * 512:(g + 1) * 512], start=(c == 0), stop=(c == 7)),
                             reads=[sTb, w2b], writes=[ybb])
                    s.op("dve", lambda e: e.tensor_tensor(out=scr[:, g * 512:(g + 1) * 512], in0=yb[:, :], in1=gl[:, 2, g * 512:(g + 1) * 512], op=ALU.add),
                         reads=[ybb, glb], writes=[scrb])
                s.op("pool", lambda e: e.tensor_tensor(out=scr[:], in0=scr[:], in1=gl[:, 3, :], op=ALU.mult), reads=[scrb, glb], writes=[scrb])
                s.op("pool", lambda e: e.tensor_tensor(out=ot[:], in0=scr[:], in1=xt[:], op=ALU.add), reads=[scrb, xb], writes=[otb])
                s.dma("sp", lambda e: e.dma_start(out=hout[t * 128:(t + 1) * 128, :], in_=ot[:]), reads=[otb], writes=[houtb[t]])
            s.barrier()


IN_SPECS = {
    "x": ([SEQ, D], F32), "ctx": ([CTX, D], F32), "cc": ([128, 16], F32),
    "w_ada": ([2, D, 6 * D], F32), "b_ada": ([2, 6 * D], F32), "g_norm": ([4, D], F32),
    "ident": ([128, 128], BF16), "identf": ([128, 128], F32),
    "attn_w_in": ([D, 2208], F32), "mla_w_q_up": ([384, 768], F32), "mla_w_kv_up": ([256, 1024], F32),
    "mla_g_qa": ([1, 384], F32), "mla_g_kva": ([1, 256], F32), "mla_g_q": ([1, 96], F32), "mla_g_k": ([1, 96], F32),
    "na_g_q": ([1, 64], F32), "na_g_k": ([1, 64], F32), "attn_w_out": ([D, D], F32),
    "rope": ([SEQ, 32], F32), "nabias": ([8, 128, 21, 128], F32),
    "conv_w_pw1": ([D, 2 * D], F32), "conv_b1T": ([128, 16], F32), "conv_wdwT": ([128, 8, 31], F32), "conv_bdwT": ([128, 8], F32),
    "conv_g_ln": ([1, D], F32), "conv_b_ln": ([1, D], F32), "conv_w_pw2": ([D, D], F32), "conv_b_pw2": ([1, D], F32),
    "wq0": ([D, 2048], F32), "wq1": ([D, 2048], F32), "skT0": ([128, 16, 128], F32), "skT1": ([128, 16, 128], F32),
    "u0": ([16384, D], F32), "u1": ([16384, D], F32), "v0": ([16384, D], F32), "v1": ([16384, D], F32),
}


def build_program():
    nc = bass.Bass("TRN2", target_bir_lowering=False)
    A = {n: nc.dram_tensor(n, sh, dt, kind="ExternalInput").ap() for n, (sh, dt) in IN_SPECS.items()}
    out = nc.dram_tensor("out", [SEQ, D], F32, kind="ExternalOutput").ap()
    modv = nc.dram_tensor("modv_scr", [2, 2, 6, D], F32, kind="Internal").ap()
    hs = [nc.dram_tensor(f"h_scr{i}", [SEQ, D], F32, kind="Internal").ap() for i in range(3)]
    hb = [[Buf() for _ in range(NT)] for _ in range(4)]
    uvs = [nc.dram_tensor(f"uv_scr{l}", [16384, 2 * D], BF16, kind="Internal").ap() for l in range(2)]
    with ExitStack() as es:
        k = K(nc, es)
        k.modv_buf = Buf()
        setup_consts(k, es, A["ident"], A["identf"])
        uvb = [[], []]
        k.bg = uv_cast_gen(k, [(A[f"u{l}"], A[f"v{l}"], uvs[l], uvb[l]) for l in range(2)])
        stage_ada(k, A["cc"], A["w_ada"], A["b_ada"], A["g_norm"], modv)
        stage_attn(k, A["x"], A["ctx"], modv[0], A, hs[0], hb[0])
        for _ in k.bg:
            pass
        stage_peer3(k, hs[0], hb[0], modv[0], A["wq0"], A["skT0"], uvs[0], uvb[0], hs[1], hb[1], "pra")
        stage_conv(k, hs[1], hb[1], modv[1], A, hs[2], hb[2])
        stage_peer3(k, hs[2], hb[2], modv[1], A["wq1"], A["skT1"], uvs[1], uvb[1], out, hb[3], "prb")
        k.s.barrier()
    return nc


def kernel(**inp):
    import ml_dtypes
    f = lambda a: np.ascontiguousarray(np.asarray(a, dtype=np.float32))
    nb = inp["x"].shape[0]
    shared = {
        "w_ada": f(inp["w_ada"]), "b_ada": f(inp["b_ada"]),
        "g_norm": f(np.stack([inp["g_norm1"][0], inp["g_norm2"][0], inp["g_norm1"][1], inp["g_norm2"][1]])),
        "ident": np.eye(128).astype(ml_dtypes.bfloat16), "identf": np.eye(128, dtype=np.float32),
        "attn_w_in": f(inp["attn_w_in"][0]), "mla_w_q_up": f(inp["mla_w_q_up"][0]), "mla_w_kv_up": f(inp["mla_w_kv_up"][0]),
        "mla_g_qa": f(inp["mla_g_qa"]), "mla_g_kva": f(inp["mla_g_kva"]), "mla_g_q": f(inp["mla_g_q"]), "mla_g_k": f(inp["mla_g_k"]),
        "na_g_q": f(inp["na_g_q"]), "na_g_k": f(inp["na_g_k"]), "attn_w_out": f(inp["attn_w_out"][0]),
        "rope": host_rope_table(), "nabias": host_na_bias(np.asarray(inp["na_rpb"][0], np.float32)),
        "conv_w_pw1": f(inp["conv_w_pw1"][0]), "conv_b1T": f(np.asarray(inp["conv_b_pw1"][0]).reshape(16, 128).T),
        "conv_wdwT": f(np.asarray(inp["conv_w_dw"][0]).reshape(31, 8, 128).transpose(2, 1, 0)),
        "conv_bdwT": f(np.asarray(inp["conv_b_dw"][0]).reshape(8, 128).T),
        "conv_g_ln": f(inp["conv_g_ln"]), "conv_b_ln": f(inp["conv_b_ln"]), "conv_w_pw2": f(inp["conv_w_pw2"][0]), "conv_b_pw2": f(inp["conv_b_pw2"]),
    }
    for l in range(2):
        shared[f"wq{l}"] = f(inp["peer_w_query"][l])
        shared[f"skT{l}"] = f(np.asarray(inp["peer_sub_keys"][l]).reshape(16, 128, 128).transpose(2, 0, 1))
        shared[f"u{l}"] = f(inp["peer_u"][l])
        shared[f"v{l}"] = f(inp["peer_v"][l])
    in_maps = []
    for b in range(nb):
        cc = np.zeros((128, 16), np.float32)
        cc[:, 0::2] = np.asarray(inp["c"][b], np.float32).reshape(8, 128).T
        cc[:, 1::2] = np.asarray(inp["c_ctx"], np.float32).reshape(8, 128).T
        m = dict(shared)
        m["x"] = f(inp["x"][b])
        m["ctx"] = f(inp["ctx"][b])
        m["cc"] = cc
        in_maps.append(m)
    nc = build_program()
    res = run_bass_kernel_spmd(nc, in_maps, core_ids=list(range(nb)))
    return np.stack([np.asarray(r["out"], dtype=np.float32) for r in res.results], axis=0)


def uv_cast_gen(k, tabs):
    PIECE = 1024
    for (u_tab, v_tab, uv, uvb) in tabs:
        for r0 in range(0, 16384, PIECE):
            r1 = r0 + PIECE
            b0, b1 = Buf(), Buf()
            k.s.dma("pool", lambda e: e.dma_start(out=uv[r0:r1, 0:1024], in_=u_tab[r0:r1, :]), writes=[b0])
            uvb.append(b0)
            yield
            k.s.dma("pool", lambda e: e.dma_start(out=uv[r0:r1, 1024:2048], in_=v_tab[r0:r1, :]), writes=[b1])
            uvb.append(b1)
            yield


def issue_uv_cast(k, es, u_tab, v_tab, uv, uvb):
    for i in range(4):
        r0, r1 = i * 4096, (i + 1) * 4096
        b0, b1 = Buf(), Buf()
        k.s.bulk_dma("pool", lambda e: e.dma_start(out=uv[r0:r1, 0:1024], in_=u_tab[r0:r1, :]), writes=[b0], es=es)
        k.s.bulk_dma("pool", lambda e: e.dma_start(out=uv[r0:r1, 1024:2048], in_=v_tab[r0:r1, :]), writes=[b1], es=es)
        uvb.extend([b0, b1])


NROW2 = 16


def stage_peer2(k, hin, hinb, modv_l, w_query, skT_d, uv, uvb, hout, houtb, tag):
    nc, s = k.nc, k.s
    banks = k.banks
    with ExitStack() as es:
        wq, wqb = k.sb(es, f"{tag}_wq", [128, 8, 2048], BF16)
        skT, skTb = k.sb(es, f"{tag}_skT", [128, 16, 128], BF16)
        ABG, ABGb = k.sb(es, f"{tag}_ABG", [128, 3, 1024], F32)
        iota_i, iota_ib = k.sb(es, f"{tag}_iotai", [128, 16], I32)
        iota, iotab = k.sb(es, f"{tag}_iota", [128, 16], F32)
        st, stb = k.sb(es, f"{tag}_st", [128, 4], F32)
        xts = [k.sb(es, f"{tag}_x{i}", [128, 1024], F32) for i in range(2)]
        scrs = [k.sb(es, f"{tag}_scr{i}", [128, 1024], F32) for i in range(2)]
        hbfs = [k.sb(es, f"{tag}_hbf{i}", [128, 1024], BF16) for i in range(1)]
        hms = [k.sb(es, f"{tag}_hm{i}", [128, 1024], F32) for i in range(2)]
        hT, hTb = k.sb(es, f"{tag}_hT", [128, 8, 128], BF16)
        qbf, qbfb = k.sb(es, f"{tag}_qbf", [128, 2048], BF16)
        qT, qTb = k.sb(es, f"{tag}_qT", [128, 16, 128], BF16)
        ssb, ssbb = k.sb(es, f"{tag}_s", [128, 16, 128], F32)
        s2, _ = k.sb(es, f"{tag}_s2", [128, 16, 128], F32)
        m16, _ = k.sb(es, f"{tag}_m16", [128, 16, 16], F32)
        i16, _ = k.sb(es, f"{tag}_i16", [128, 16, 16], U32)
        i16f, i16fb = k.sb(es, f"{tag}_i16f", [128, 16, 16], F32)
        cand, candb = k.sb(es, f"{tag}_cand", [128, 8, 256], F32)
        cand2, _ = k.sb(es, f"{tag}_cand2", [128, 8, 256], F32)
        best, _ = k.sb(es, f"{tag}_best", [128, 8, 16], F32)
        pos, _ = k.sb(es, f"{tag}_pos", [128, 8, 16], U32)
        ab_i, ab_ib = k.sb(es, f"{tag}_abi", [128, 2, 128], I32)
        ab_f, ab_fb = k.sb(es, f"{tag}_abf", [128, 2, 128], F32)
        oh, ohb = k.sb(es, f"{tag}_oh", [128, 8, 16, 16], F32)
        e01, e01b = k.sb(es, f"{tag}_e01", [128, 2, 128], F32)
        idxs = [k.sb(es, f"{tag}_idx{i}", [128, 128], I32) for i in range(2)]
        gts = [k.sb(es, f"{tag}_gate{i}", [128, 8, 16], F32) for i in range(2)]
        gsum, gsumb = k.sb(es, f"{tag}_gsum", [128, 8], F32)
        actv, _ = k.sb(es, f"{tag}_act", [128, 128], F32)
        wgt, _ = k.sb(es, f"{tag}_wgt", [128, 128], F32)
        junk, _ = k.sb(es, f"{tag}_junk", [128, 1024], BF16)
        rows = [k.sb(es, f"{tag}_row{i}", [128, 2048], BF16) for i in range(NROW2)]
        dgs = [k.sb(es, f"{tag}_dg{i}", [128, 128], BF16) for i in range(4)]
        ot, otb = k.sb(es, f"{tag}_ot", [128, 1024], F32)
        hpb = [[Buf() for _ in range(16)] for _ in range(3)]
        hb = [[Buf() for _ in range(8)] for _ in range(3)]
        actb = [Buf() for _ in range(16)]
        wgb = [Buf() for _ in range(4)]

        wqv = w_query.rearrange("(kc p) n -> p kc n", p=128)
        for c in range(8):
            s.dma("pool", lambda e: e.dma_start(out=wq[:, c, :], in_=wqv[:, c, :]), writes=[wqb])
        s.dma("pool", lambda e: e.dma_start(out=skT[:].rearrange("p a b -> p (a b)"), in_=skT_d.rearrange("p a b -> p (a b)")), writes=[skTb])
        for j in range(3):
            load_bcast(k, ABG[:, j, :], ABGb, modv_l[0:1, 3 + j, :], 1024)
        s.op("pool", lambda e: e.iota(out=iota_i[:], pattern=[[1, 16]], base=0, channel_multiplier=0), writes=[iota_ib])
        s.op("dve", lambda e: e.tensor_copy(out=iota[:], in_=iota_i[:]), reads=[iota_ib], writes=[iotab])

        def front(t):
            xt, xb = xts[t % 2]
            scr, scrb = scrs[t % 2]
            idx, idxb = idxs[t % 2]
            gate, gateb = gts[t % 2]
            hbf, hbfb = hbfs[0]
            hm, hmb = hms[t % 2]
            s.dma("sp", lambda e: e.dma_start(out=xt[:], in_=hin[t * 128:(t + 1) * 128, :]), reads=[hinb[t]], writes=[xb])
            norm_mod(k, xt, xb, ABG[:, 0, :], ABG[:, 1, :], ABGb, scr, scrb, st, stb, hbf, hbfb, out_f32=(hm, hmb))
            bank, bb = banks[4]
            transpose_chunks(k, bank, bb, lambda c: hbf[:, c * 128:(c + 1) * 128], 8, 128, hT[:], hTb, hbfb, dst_view=True)
            yield
            for g in range(4):
                qb_, qbb = banks[g]
                for c in range(8):
                    s.op("pe", lambda e: e.matmul(qb_[:, :], lhsT=hT[:, c, :], rhs=wq[:, c, g * 512:(g + 1) * 512], start=(c == 0), stop=(c == 7)),
                         reads=[hTb, wqb], writes=[qbb])
                s.op("act", lambda e: e.copy(out=qbf[:, g * 512:(g + 1) * 512], in_=qb_[:, :]), reads=[qbb], writes=[qbfb])
            yield
            for half in range(2):
                tb_, tbb = banks[4 + half]
                transpose_chunks(k, tb_, tbb, lambda c: qbf[:, (half * 8 + c) * 128:(half * 8 + c + 1) * 128], 8, 128,
                                 qT[:, half * 8:(half + 1) * 8, :], qTb, qbfb, dst_view=True)
            for g in range(4):
                sb_, sbb = banks[g]
                for j in range(4):
                    hp = g * 4 + j
                    s.op("pe", lambda e: e.matmul(sb_[:, j * 128:(j + 1) * 128], lhsT=qT[:, hp, :], rhs=skT[:, hp, :], start=True, stop=True),
                         reads=[qTb, skTb], writes=[sbb])
                s.op("act", lambda e: e.copy(out=ssb[:, g * 4:(g + 1) * 4, :], in_=sb_[:, :].rearrange("p (a b) -> p a b", b=128)), reads=[sbb], writes=[ssbb])
            yield
            for hp in range(16):
                s.op("dve", lambda e: e.max(out=m16[:, hp, 0:8], in_=ssb[:, hp, :]), reads=[ssbb], writes=[hpb[0][hp]])
            yield
            for hp in range(16):
                s.op("dve", lambda e: e.max_index(out=i16[:, hp, 0:8], in_max=m16[:, hp, 0:8], in_values=ssb[:, hp, :]),
                     reads=[ssbb, hpb[0][hp]], writes=[hpb[1][hp]])
            yield
            for hp in range(16):
                s.op("dve", lambda e: e.match_replace(out=s2[:, hp, :], in_to_replace=m16[:, hp, 0:8], in_values=ssb[:, hp, :], imm_value=-1e30),
                     reads=[ssbb, hpb[0][hp]], writes=[hpb[2][hp]])
            yield
            for hp in range(16):
                s.op("dve", lambda e: e.max(out=m16[:, hp, 8:16], in_=s2[:, hp, :]), reads=[hpb[2][hp]], writes=[hpb[0][hp]])
            yield
            for hp in range(16):
                s.op("dve", lambda e: e.max_index(out=i16[:, hp, 8:16], in_max=m16[:, hp, 8:16], in_values=s2[:, hp, :]),
                     reads=[hpb[2][hp], hpb[0][hp]], writes=[hpb[1][hp]])
            yield
            s.op("dve", lambda e: e.tensor_copy(out=i16f[:], in_=i16[:]), reads=hpb[1], writes=[i16fb])
            m4 = m16[:].rearrange("p (h t) a -> p h t a", t=2)
            s.op("dve", lambda e: e.tensor_tensor(out=cand[:].rearrange("p h (a b) -> p h a b", b=16),
                                                  in0=m4[:, :, 0, :].unsqueeze(3).to_broadcast([128, 8, 16, 16]),
                                                  in1=m4[:, :, 1, :].unsqueeze(2).to_broadcast([128, 8, 16, 16]), op=ALU.add),
                 reads=hpb[0], writes=[candb])
            yield
            for h in range(8):
                s.op("dve", lambda e: e.max(out=best[:, h, 0:8], in_=cand[:, h, :]), reads=[candb], writes=[hb[0][h]])
            for h in range(8):
                s.op("dve", lambda e: e.max_index(out=pos[:, h, 0:8], in_max=best[:, h, 0:8], in_values=cand[:, h, :]),
                     reads=[candb, hb[0][h]], writes=[hb[1][h]])
            yield
            for h in range(8):
                s.op("dve", lambda e: e.match_replace(out=cand2[:, h, :], in_to_replace=best[:, h, 0:8], in_values=cand[:, h, :], imm_value=-1e30),
                     reads=[candb, hb[0][h]], writes=[hb[2][h]])
            for h in range(8):
                s.op("dve", lambda e: e.max(out=best[:, h, 8:16], in_=cand2[:, h, :]), reads=[hb[2][h]], writes=[hb[0][h]])
            yield
            for h in range(8):
                s.op("dve", lambda e: e.max_index(out=pos[:, h, 8:16], in_max=best[:, h, 8:16], in_values=cand2[:, h, :]),
                     reads=[hb[2][h], hb[0][h]], writes=[hb[1][h]])
            posi = pos[:].rearrange("p h k -> p (h k)").bitcast(I32)
            s.op("dve", lambda e: e.tensor_single_scalar(out=ab_i[:, 0, :], in_=posi, scalar=4, op=ALU.arith_shift_right), reads=hb[1], writes=[ab_ib])
            s.op("dve", lambda e: e.tensor_single_scalar(out=ab_i[:, 1, :], in_=posi, scalar=15, op=ALU.bitwise_and), reads=hb[1], writes=[ab_ib])
            s.op("dve", lambda e: e.tensor_copy(out=ab_f[:], in_=ab_i[:]), reads=[ab_ib], writes=[ab_fb])
            yield
            i4 = i16f[:].rearrange("p (h t) a -> p h t a", t=2)
            for p_ in range(2):
                s.op("dve", lambda e: e.tensor_tensor(out=oh[:], in0=ab_f[:, p_, :].rearrange("p (h k) -> p h k", k=16).unsqueeze(3).to_broadcast([128, 8, 16, 16]),
                                                      in1=iota[:].unsqueeze(1).unsqueeze(1).to_broadcast([128, 8, 16, 16]), op=ALU.is_equal),
                     reads=[ab_fb, iotab], writes=[ohb])
                s.op("dve", lambda e: e.tensor_tensor(out=oh[:], in0=oh[:], in1=i4[:, :, p_, :].unsqueeze(2).to_broadcast([128, 8, 16, 16]), op=ALU.mult),
                     reads=[ohb, i16fb], writes=[ohb])
                s.op("dve", lambda e: e.tensor_reduce(out=e01[:, p_, :].rearrange("p (h k) -> p h k", k=16), in_=oh[:], axis=AX.X, op=ALU.add),
                     reads=[ohb], writes=[e01b])
                yield
            s.op("dve", lambda e: e.scalar_tensor_tensor(out=e01[:, 0, :], in0=e01[:, 0, :], scalar=128.0, in1=e01[:, 1, :], op0=ALU.mult, op1=ALU.add),
                 reads=[e01b], writes=[e01b])
            s.op("dve", lambda e: e.tensor_copy(out=idx[:], in_=e01[:, 0, :]), reads=[e01b], writes=[idxb])
            s.op("dve", lambda e: e.tensor_tensor(out=gate[:], in0=best[:], in1=best[:, :, 0:1].to_broadcast([128, 8, 16]), op=ALU.subtract),
                 reads=hb[0], writes=[gateb])
            s.op("act", lambda e: e.activation(out=gate[:], in_=gate[:], func=AF.Exp), reads=[gateb], writes=[gateb])
            s.op("dve", lambda e: e.tensor_reduce(out=gsum[:], in_=gate[:], axis=AX.X, op=ALU.add), reads=[gateb], writes=[gsumb])
            s.op("dve", lambda e: e.reciprocal(out=gsum[:], in_=gsum[:]), reads=[gsumb], writes=[gsumb])
            s.op("dve", lambda e: e.tensor_tensor(out=gate[:], in0=gate[:], in1=gsum[:].unsqueeze(2).to_broadcast([128, 8, 16]), op=ALU.mult),
                 reads=[gateb, gsumb], writes=[gateb])

        ring = [0]

        def back(t, fg):
            xt, xb = xts[t % 2]
            scr, scrb = scrs[t % 2]
            idx, idxb = idxs[t % 2]
            gate, gateb = gts[t % 2]
            hm, hmb = hms[t % 2]
            (o0, o0b), (o1, o1b) = banks[6], banks[7]
            for grp in range(16):
                held = []
                for j in range(8):
                    hk = grp * 8 + j
                    rw, rwb = rows[ring[0] % NROW2]
                    ring[0] += 1
                    s.dma("pool", lambda e: e.indirect_dma_start(out=rw[:], out_offset=None, in_=uv,
                                                                 in_offset=bass.IndirectOffsetOnAxis(ap=idx[:, hk:hk + 1], axis=0)),
                          reads=[idxb] + uvb, writes=[rwb])
                    s.op("dve", lambda e: e.scalar_tensor_tensor(out=junk[:], in0=rw[:, 0:1024], scalar=1.0, in1=hm[:], op0=ALU.mult, op1=ALU.mult,
                                                                 accum_out=actv[:, hk:hk + 1]), reads=[rwb, hmb], writes=[actb[hk % 16]])
                    held.append((hk, rw, rwb))
                g8 = slice(grp * 8, grp * 8 + 8)
                wb_ = wgb[grp % 4]
                s.op("act", lambda e: e.activation(out=wgt[:, g8], in_=actv[:, g8], func=AF.Gelu), reads=actb[(grp % 2) * 8:(grp % 2) * 8 + 8], writes=[wb_])
                s.op("dve", lambda e: e.tensor_tensor(out=wgt[:, g8], in0=wgt[:, g8], in1=gate[:].rearrange("p h k -> p (h k)")[:, g8], op=ALU.mult),
                     reads=[wb_, gateb], writes=[wb_])
                for (hk, rw, rwb) in held:
                    dg, dgb = dgs[hk % 4]
                    s.op("act", lambda e: e.activation(out=dg[:], in_=k.ident[:], func=AF.Copy, scale=wgt[:, hk:hk + 1]),
                         reads=[k.identb, wb_], writes=[dgb])
                    s.op("pe", lambda e: e.matmul(o0[:, :], lhsT=dg[:], rhs=rw[:, 1024:1536], start=(hk == 0), stop=(hk == 127)),
                         reads=[dgb, rwb], writes=[o0b])
                    s.op("pe", lambda e: e.matmul(o1[:, :], lhsT=dg[:], rhs=rw[:, 1536:2048], start=(hk == 0), stop=(hk == 127)),
                         reads=[dgb, rwb], writes=[o1b])
                if fg is not None:
                    next(fg, None)
            s.op("dve", lambda e: e.tensor_tensor(out=scr[:, 0:512], in0=o0[:, :], in1=ABG[:, 2, 0:512], op=ALU.mult), reads=[o0b, ABGb], writes=[scrb])
            s.op("dve", lambda e: e.tensor_tensor(out=scr[:, 512:1024], in0=o1[:, :], in1=ABG[:, 2, 512:1024], op=ALU.mult), reads=[o1b, ABGb], writes=[scrb])
            s.op("pool", lambda e: e.tensor_tensor(out=ot[:], in0=scr[:], in1=xt[:], op=ALU.add), reads=[scrb, xb], writes=[otb])
            s.dma("sp", lambda e: e.dma_start(out=hout[t * 128:(t + 1) * 128, :], in_=ot[:]), reads=[otb], writes=[houtb[t]])

        for _ in front(0):
            pass
        for t in range(NT):
            fg = front(t + 1) if t + 1 < NT else None
            back(t, fg)
            if fg is not None:
                for _ in fg:
                    pass
        s.barrier()
NROW3 = 16


def stage_peer3(k, hin, hinb, modv_l, w_query, skT_d, uv, uvb, hout, houtb, tag):
    nc, s = k.nc, k.s
    banks = k.banks
    with ExitStack() as es:
        wq, wqb = k.sb(es, f"{tag}_wq", [128, 8, 2048], BF16)
        skT, skTb = k.sb(es, f"{tag}_skT", [128, 16, 128], BF16)
        ABG, ABGb = k.sb(es, f"{tag}_ABG", [128, 3, 1024], F32)
        iota_i, iota_ib = k.sb(es, f"{tag}_iotai", [128, 16], I32)
        iota, iotab = k.sb(es, f"{tag}_iota", [128, 16], F32)
        st, stb = k.sb(es, f"{tag}_st", [128, 4], F32)
        xts = [k.sb(es, f"{tag}_x{i}", [128, 1024], F32) for i in range(2)]
        scrs = [k.sb(es, f"{tag}_scr{i}", [128, 1024], F32) for i in range(1)] * 2
        hbfs = [k.sb(es, f"{tag}_hbf{i}", [128, 1024], BF16) for i in range(1)]
        hT, hTb = k.sb(es, f"{tag}_hT", [128, 8, 128], BF16)
        qbf, qbfb = k.sb(es, f"{tag}_qbf", [128, 2048], BF16)
        qT, qTb = k.sb(es, f"{tag}_qT", [128, 16, 128], BF16)
        ssb, ssbb = k.sb(es, f"{tag}_s", [128, 16, 128], F32)
        s2, _ = k.sb(es, f"{tag}_s2", [128, 16, 128], F32)
        m16, _ = k.sb(es, f"{tag}_m16", [128, 16, 16], F32)
        i16, _ = k.sb(es, f"{tag}_i16", [128, 16, 16], U32)
        i16f, i16fb = k.sb(es, f"{tag}_i16f", [128, 16, 16], F32)
        cand, candb = k.sb(es, f"{tag}_cand", [128, 8, 256], F32)
        cand2, _ = k.sb(es, f"{tag}_cand2", [128, 8, 256], F32)
        best, _ = k.sb(es, f"{tag}_best", [128, 8, 16], F32)
        pos, _ = k.sb(es, f"{tag}_pos", [128, 8, 16], U32)
        ab_i, ab_ib = k.sb(es, f"{tag}_abi", [128, 2, 128], I32)
        ab_f, ab_fb = k.sb(es, f"{tag}_abf", [128, 2, 128], F32)
        oh, ohb = k.sb(es, f"{tag}_oh", [128, 8, 16, 16], F32)
        e01, e01b = k.sb(es, f"{tag}_e01", [128, 2, 128], F32)
        idxs = [k.sb(es, f"{tag}_idx{i}", [128, 128], I32) for i in range(2)]
        gts = [k.sb(es, f"{tag}_gate{i}", [128, 8, 16], F32) for i in range(2)]
        gsum, gsumb = k.sb(es, f"{tag}_gsum", [128, 8], F32)
        actv, _ = k.sb(es, f"{tag}_act", [128, 128], F32)
        wgt, _ = k.sb(es, f"{tag}_wgt", [128, 128], F32)
        junks = [k.sb(es, f"{tag}_junk{i}", [128, 1024], BF16) for i in range(3)]
        rows = [k.sb(es, f"{tag}_row{i}", [128, 2048], BF16) for i in range(NROW3)]
        dgs = [k.sb(es, f"{tag}_dg{i}", [128, 128], BF16) for i in range(4)]
        ot, otb = k.sb(es, f"{tag}_ot", [128, 1024], F32)
        hpb = [[Buf() for _ in range(16)] for _ in range(3)]
        hb = [[Buf() for _ in range(8)] for _ in range(3)]
        actb = [Buf() for _ in range(16)]
        wgb = [Buf() for _ in range(4)]

        wqv = w_query.rearrange("(kc p) n -> p kc n", p=128)
        for c in range(8):
            s.dma("pool", lambda e: e.dma_start(out=wq[:, c, :], in_=wqv[:, c, :]), writes=[wqb])
        s.dma("pool", lambda e: e.dma_start(out=skT[:].rearrange("p a b -> p (a b)"), in_=skT_d.rearrange("p a b -> p (a b)")), writes=[skTb])
        for j in range(3):
            load_bcast(k, ABG[:, j, :], ABGb, modv_l[0:1, 3 + j, :], 1024)
        s.op("pool", lambda e: e.iota(out=iota_i[:], pattern=[[1, 16]], base=0, channel_multiplier=0), writes=[iota_ib])
        s.op("dve", lambda e: e.tensor_copy(out=iota[:], in_=iota_i[:]), reads=[iota_ib], writes=[iotab])

        def front(t):
            xt, xb = xts[t % 2]
            scr, scrb = scrs[t % 2]
            idx, idxb = idxs[t % 2]
            gate, gateb = gts[t % 2]
            hbf, hbfb = hbfs[0]
            s.dma("sp", lambda e: e.dma_start(out=xt[:], in_=hin[t * 128:(t + 1) * 128, :]), reads=[hinb[t]], writes=[xb])
            yield
            s.op("act", lambda e: e.activation(out=scr[:], in_=xt[:], func=AF.Square, accum_out=st[:, 0:1]), reads=[xb], writes=[scrb, stb])
            s.op("act", lambda e: e.activation(out=st[:, 0:1], in_=st[:, 0:1], func=AF.Sqrt, scale=1.0 / D, bias=k.eps[:, 0:1]), reads=[stb], writes=[stb])
            yield
            s.op("dve", lambda e: e.reciprocal(out=st[:, 0:1], in_=st[:, 0:1]), reads=[stb], writes=[stb])
            s.op("dve", lambda e: e.scalar_tensor_tensor(out=scr[:], in0=xt[:], scalar=st[:, 0:1], in1=ABG[:, 0, :], op0=ALU.mult, op1=ALU.mult),
                 reads=[xb, stb, ABGb], writes=[scrb])
            yield
            s.op("pool", lambda e: e.tensor_tensor(out=hbf[:], in0=scr[:], in1=ABG[:, 1, :], op=ALU.add), reads=[scrb, ABGb], writes=[hbfb])
            yield
            hmp, hmpb = banks[4 + t % 2]
            bank, bb = banks[2]
            bv = bank[:].bitcast(BF16)
            for c in range(8):
                s.op("pe", lambda e: e.transpose(out=bv[:, c * 128:(c + 1) * 128], in_=hbf[:, c * 128:(c + 1) * 128], identity=k.ident[:]),
                     reads=[hbfb, k.identb], writes=[bb])
            yield
            s.op("act", lambda e: e.copy(out=hT[:], in_=bv[:, :].rearrange("p (c t) -> p c t", t=128)), reads=[bb], writes=[hTb])
            yield
            hmv_ = hmp[:].bitcast(BF16)
            for c in range(8):
                s.op("pe", lambda e: e.transpose(out=hmv_[:, c * 128:(c + 1) * 128], in_=hT[:, c, :], identity=k.ident[:]),
                     reads=[hTb, k.identb], writes=[hmpb])
            for g in range(5):
                if g < 4:
                    qb_, qbb = banks[g % 2]
                    for c in range(8):
                        s.op("pe", lambda e: e.matmul(qb_[:, :], lhsT=hT[:, c, :], rhs=wq[:, c, g * 512:(g + 1) * 512], start=(c == 0), stop=(c == 7)),
                             reads=[hTb, wqb], writes=[qbb])
                if g > 0:
                    g1 = g - 1
                    qb1, qbb1 = banks[g1 % 2]
                    s.op("act", lambda e: e.copy(out=qbf[:, g1 * 512:(g1 + 1) * 512], in_=qb1[:, :]), reads=[qbb1], writes=[qbfb])
                yield
            for half in range(3):
                if half < 2:
                    tb_, tbb = banks[2 + half]
                    bv2 = tb_[:].bitcast(BF16)
                    for c in range(8):
                        s.op("pe", lambda e: e.transpose(out=bv2[:, c * 128:(c + 1) * 128], in_=qbf[:, (half * 8 + c) * 128:(half * 8 + c + 1) * 128], identity=k.ident[:]),
                             reads=[qbfb, k.identb], writes=[tbb])
                if half > 0:
                    h1 = half - 1
                    tb1, tbb1 = banks[2 + h1]
                    s.op("act", lambda e: e.copy(out=qT[:, h1 * 8:(h1 + 1) * 8, :], in_=tb1[:].bitcast(BF16)[:, :].rearrange("p (c t) -> p c t", t=128)),
                         reads=[tbb1], writes=[qTb])
                yield
            for g in range(5):
                if g < 4:
                    sb_, sbb = banks[g % 2]
                    for j in range(4):
                        hp = g * 4 + j
                        s.op("pe", lambda e: e.matmul(sb_[:, j * 128:(j + 1) * 128], lhsT=qT[:, hp, :], rhs=skT[:, hp, :], start=True, stop=True),
                             reads=[qTb, skTb], writes=[sbb])
                if g > 0:
                    g1 = g - 1
                    sb1, sbb1 = banks[g1 % 2]
                    s.op("act", lambda e: e.copy(out=ssb[:, g1 * 4:(g1 + 1) * 4, :], in_=sb1[:, :].rearrange("p (a b) -> p a b", b=128)), reads=[sbb1], writes=[ssbb])
                if g % 2 == 1:
                    yield
            yield
            for hp in range(16):
                s.op("dve", lambda e: e.max(out=m16[:, hp, 0:8], in_=ssb[:, hp, :]), reads=[ssbb], writes=[hpb[0][hp]])
            yield
            for hp in range(16):
                s.op("dve", lambda e: e.max_index(out=i16[:, hp, 0:8], in_max=m16[:, hp, 0:8], in_values=ssb[:, hp, :]),
                     reads=[ssbb, hpb[0][hp]], writes=[hpb[1][hp]])
            yield
            for hp in range(16):
                s.op("dve", lambda e: e.match_replace(out=s2[:, hp, :], in_to_replace=m16[:, hp, 0:8], in_values=ssb[:, hp, :], imm_value=-1e30),
                     reads=[ssbb, hpb[0][hp]], writes=[hpb[2][hp]])
            yield
            for hp in range(16):
                s.op("dve", lambda e: e.max(out=m16[:, hp, 8:16], in_=s2[:, hp, :]), reads=[hpb[2][hp]], writes=[hpb[0][hp]])
            yield
            for hp in range(16):
                s.op("dve", lambda e: e.max_index(out=i16[:, hp, 8:16], in_max=m16[:, hp, 8:16], in_values=s2[:, hp, :]),
                     reads=[hpb[2][hp], hpb[0][hp]], writes=[hpb[1][hp]])
            yield
            s.op("dve", lambda e: e.tensor_copy(out=i16f[:], in_=i16[:]), reads=hpb[1], writes=[i16fb])
            m4 = m16[:].rearrange("p (h t) a -> p h t a", t=2)
            s.op("dve", lambda e: e.tensor_tensor(out=cand[:].rearrange("p h (a b) -> p h a b", b=16),
                                                  in0=m4[:, :, 0, :].unsqueeze(3).to_broadcast([128, 8, 16, 16]),
                                                  in1=m4[:, :, 1, :].unsqueeze(2).to_broadcast([128, 8, 16, 16]), op=ALU.add),
                 reads=hpb[0], writes=[candb])
            yield
            for h in range(8):
                s.op("dve", lambda e: e.max(out=best[:, h, 0:8], in_=cand[:, h, :]), reads=[candb], writes=[hb[0][h]])
            for h in range(8):
                s.op("dve", lambda e: e.max_index(out=pos[:, h, 0:8], in_max=best[:, h, 0:8], in_values=cand[:, h, :]),
                     reads=[candb, hb[0][h]], writes=[hb[1][h]])
            yield
            for h in range(8):
                s.op("dve", lambda e: e.match_replace(out=cand2[:, h, :], in_to_replace=best[:, h, 0:8], in_values=cand[:, h, :], imm_value=-1e30),
                     reads=[candb, hb[0][h]], writes=[hb[2][h]])
            for h in range(8):
                s.op("dve", lambda e: e.max(out=best[:, h, 8:16], in_=cand2[:, h, :]), reads=[hb[2][h]], writes=[hb[0][h]])
            yield
            for h in range(8):
                s.op("dve", lambda e: e.max_index(out=pos[:, h, 8:16], in_max=best[:, h, 8:16], in_values=cand2[:, h, :]),
                     reads=[hb[2][h], hb[0][h]], writes=[hb[1][h]])
            posi = pos[:].rearrange("p h k -> p (h k)").bitcast(I32)
            s.op("dve", lambda e: e.tensor_single_scalar(out=ab_i[:, 0, :], in_=posi, scalar=4, op=ALU.arith_shift_right), reads=hb[1], writes=[ab_ib])
            s.op("dve", lambda e: e.tensor_single_scalar(out=ab_i[:, 1, :], in_=posi, scalar=15, op=ALU.bitwise_and), reads=hb[1], writes=[ab_ib])
            s.op("dve", lambda e: e.tensor_copy(out=ab_f[:], in_=ab_i[:]), reads=[ab_ib], writes=[ab_fb])
            s.op("dve", lambda e: e.tensor_tensor(out=gate[:], in0=best[:], in1=best[:, :, 0:1].to_broadcast([128, 8, 16]), op=ALU.subtract),
                 reads=hb[0], writes=[gateb])
            yield
            s.op("act", lambda e: e.activation(out=gate[:], in_=gate[:], func=AF.Exp), reads=[gateb], writes=[gateb])
            i4 = i16f[:].rearrange("p (h t) a -> p h t a", t=2)
            for p_ in range(2):
                s.op("dve", lambda e: e.tensor_tensor(out=oh[:], in0=ab_f[:, p_, :].rearrange("p (h k) -> p h k", k=16).unsqueeze(3).to_broadcast([128, 8, 16, 16]),
                                                      in1=iota[:].unsqueeze(1).unsqueeze(1).to_broadcast([128, 8, 16, 16]), op=ALU.is_equal),
                     reads=[ab_fb, iotab], writes=[ohb])
                yield
                s.op("dve", lambda e: e.tensor_tensor(out=oh[:], in0=oh[:], in1=i4[:, :, p_, :].unsqueeze(2).to_broadcast([128, 8, 16, 16]), op=ALU.mult),
                     reads=[ohb, i16fb], writes=[ohb])
                yield
                s.op("dve", lambda e: e.tensor_reduce(out=e01[:, p_, :].rearrange("p (h k) -> p h k", k=16), in_=oh[:], axis=AX.X, op=ALU.add),
                     reads=[ohb], writes=[e01b])
                yield
            s.op("dve", lambda e: e.scalar_tensor_tensor(out=e01[:, 0, :], in0=e01[:, 0, :], scalar=128.0, in1=e01[:, 1, :], op0=ALU.mult, op1=ALU.add),
                 reads=[e01b], writes=[e01b])
            s.op("dve", lambda e: e.tensor_copy(out=idx[:], in_=e01[:, 0, :]), reads=[e01b], writes=[idxb])
            s.op("dve", lambda e: e.tensor_reduce(out=gsum[:], in_=gate[:], axis=AX.X, op=ALU.add), reads=[gateb], writes=[gsumb])
            s.op("dve", lambda e: e.reciprocal(out=gsum[:], in_=gsum[:]), reads=[gsumb], writes=[gsumb])
            s.op("dve", lambda e: e.tensor_tensor(out=gate[:], in0=gate[:], in1=gsum[:].unsqueeze(2).to_broadcast([128, 8, 16]), op=ALU.mult),
                 reads=[gateb, gsumb], writes=[gateb])

        ring = [0]

        glv, _ = k.sb(es, f"{tag}_glv", [128, 128], F32)
        glb = [Buf() for _ in range(16)]
        wgb16 = [Buf() for _ in range(16)]

        def back(t, fg):
            xt, xb = xts[t % 2]
            scr, scrb = scrs[t % 2]
            idx, idxb = idxs[t % 2]
            gate, gateb = gts[t % 2]
            hmp, hmpb = banks[4 + t % 2]
            hmv = hmp[:].bitcast(BF16)
            gflat = gate[:].rearrange("p h k -> p (h k)")
            (o0, o0b), (o1, o1b) = banks[6], banks[7]
            held = {}

            def st_a(hk):
                s.op("act", lambda e: e.activation(out=glv[:, hk:hk + 1], in_=actv[:, hk:hk + 1], func=AF.Gelu), reads=[actb[hk % 16]], writes=[glb[hk % 16]])

            def st_b(hk):
                rw, rwb = held.pop(hk)
                dg, dgb = dgs[hk % 4]
                s.op("act", lambda e: e.activation(out=wgt[:, hk:hk + 1], in_=glv[:, hk:hk + 1], func=AF.Copy, scale=gflat[:, hk:hk + 1]),
                     reads=[glb[hk % 16], gateb], writes=[wgb16[hk % 16]])
                s.op("act", lambda e: e.activation(out=dg[:], in_=k.ident[:], func=AF.Copy, scale=wgt[:, hk:hk + 1]),
                     reads=[k.identb, wgb16[hk % 16]], writes=[dgb])
                s.op("pe", lambda e: e.matmul(o0[:, :], lhsT=dg[:], rhs=rw[:, 1024:1536], start=(hk == 0), stop=(hk == 127)),
                     reads=[dgb, rwb], writes=[o0b])
                s.op("pe", lambda e: e.matmul(o1[:, :], lhsT=dg[:], rhs=rw[:, 1536:2048], start=(hk == 0), stop=(hk == 127)),
                     reads=[dgb, rwb], writes=[o1b])

            for hk in range(128 + 3):
                if hk < 128:
                    rw, rwb = rows[ring[0] % NROW3]
                    ring[0] += 1
                    s.dma("pool", lambda e: e.indirect_dma_start(out=rw[:], out_offset=None, in_=uv,
                                                                 in_offset=bass.IndirectOffsetOnAxis(ap=idx[:, hk:hk + 1], axis=0)),
                          reads=[idxb] + uvb, writes=[rwb])
                    junk, junkb = junks[hk % 3]
                    s.op("dve", lambda e: e.scalar_tensor_tensor(out=junk[:], in0=rw[:, 0:1024], scalar=1.0, in1=hmv, op0=ALU.mult, op1=ALU.mult,
                                                                 accum_out=actv[:, hk:hk + 1]), reads=[rwb, hmpb], writes=[actb[hk % 16], junkb])
                    held[hk] = (rw, rwb)
                if 0 <= hk - 1 < 128:
                    st_a(hk - 1)
                if 0 <= hk - 3 < 128:
                    st_b(hk - 3)
                if fg is not None and hk % 4 == 3:
                    next(fg, None)
            s.op("dve", lambda e: e.tensor_tensor(out=scr[:, 0:512], in0=o0[:, :], in1=ABG[:, 2, 0:512], op=ALU.mult), reads=[o0b, ABGb], writes=[scrb])
            s.op("dve", lambda e: e.tensor_tensor(out=scr[:, 512:1024], in0=o1[:, :], in1=ABG[:, 2, 512:1024], op=ALU.mult), reads=[o1b, ABGb], writes=[scrb])
            s.op("pool", lambda e: e.tensor_tensor(out=ot[:], in0=scr[:], in1=xt[:], op=ALU.add), reads=[scrb, xb], writes=[otb])
            s.dma("sp", lambda e: e.dma_start(out=hout[t * 128:(t + 1) * 128, :], in_=ot[:]), reads=[otb], writes=[houtb[t]])

        for _ in front(0):
            pass
        for t in range(NT):
            fg = front(t + 1) if t + 1 < NT else None
            back(t, fg)
            if fg is not None:
                for _ in fg:
                    pass
        s.barrier()
```
